# Optimizing a Trainium2 kernel written in Bass

```python
import math
import jax, jax.numpy as jnp
from jax import lax
import numpy as np

D_MODEL = 1024
BATCH = 2
SEQ = 8192
DEPTH = 1

HEAD_DIM = 64
SWA_Q_HEADS = 8
SWA_KV_HEADS = 2
SWA_GROUP = SWA_Q_HEADS // SWA_KV_HEADS
WINDOW = 128
DIFF_HEADS = 4
DIFF_V_DIM = 2 * HEAD_DIM
MIX_A_WIDTH = SWA_Q_HEADS * HEAD_DIM
MIX_B_WIDTH = DIFF_HEADS * DIFF_V_DIM
MIX_WIDTH = MIX_A_WIDTH + MIX_B_WIDTH
A_Q = SWA_Q_HEADS * HEAD_DIM
A_K = SWA_KV_HEADS * HEAD_DIM
A_V = SWA_KV_HEADS * HEAD_DIM
B_Q = DIFF_HEADS * 2 * HEAD_DIM
B_K = DIFF_HEADS * 2 * HEAD_DIM
B_V = DIFF_HEADS * DIFF_V_DIM
IN_COLS = A_Q + A_K + A_V + B_Q + B_K + B_V
IN_SPLITS = [int(v) for v in np.cumsum([A_Q, A_K, A_V, B_Q, B_K])]
Q_BLOCK = 128
ROPE_THETA = 10000.0
N_EXPERTS = 32
TOP_K = 4
D_FF = D_MODEL
SWIGLU_ALPHA = 1.702
SWIGLU_LIMIT = 7.0
EXPERT_BLOCK = 128
EPS = 1e-5
N_MOD = 6

kernel_name = "hymba_swa_sink_diffattn_moe_adaln"


def rms_norm(x, g):
    xf = x.astype(jnp.float32)
    y = xf * lax.rsqrt(jnp.mean(xf * xf, axis=-1, keepdims=True) + EPS)
    return (y * g.astype(jnp.float32)).astype(x.dtype)


def modulate(h, shift, scale):
    return h * (1 + scale[:, None, :]) + shift[:, None, :]


def rope_tables(positions):
    inv = 1.0 / (ROPE_THETA ** (jnp.arange(0, HEAD_DIM, 2, dtype=jnp.float32) / HEAD_DIM))
    ang = positions.astype(jnp.float32)[..., None] * inv
    return jnp.cos(ang)[:, :, None, :], jnp.sin(ang)[:, :, None, :]


def apply_rope(t, cos, sin):
    tf = t.astype(jnp.float32)
    t1, t2 = jnp.split(tf, 2, axis=-1)
    out = jnp.concatenate([t1 * cos - t2 * sin, t2 * cos + t1 * sin], axis=-1)
    return out.astype(t.dtype)


def sliding_window_sink_attention(q, k, v, sinks):
    B, S, _, D = q.shape
    nb = S // WINDOW
    qb = q.reshape(B, nb, WINDOW, SWA_KV_HEADS, SWA_GROUP, D)

    def with_prev(t):
        tb = t.reshape(B, nb, WINDOW, SWA_KV_HEADS, D)
        prev = jnp.pad(tb, ((0, 0), (1, 0), (0, 0), (0, 0), (0, 0)))[:, :-1]
        return jnp.concatenate([prev, tb], axis=2)

    kb, vb = with_prev(k), with_prev(v)
    s = jnp.einsum('bnikgd,bnjkd->bnkgij', qb, kb,
                   preferred_element_type=jnp.float32) / math.sqrt(D)
    qi = jnp.arange(WINDOW) + WINDOW
    kj = jnp.arange(2 * WINDOW)
    diff = qi[:, None] - kj[None, :]
    band = (diff >= 0) & (diff < WINDOW)
    blk = jnp.arange(nb)[:, None, None]
    mask = band[None] & ((blk > 0) | (kj[None, None, :] >= WINDOW))
    s = jnp.where(mask[None, :, None, None], s, -jnp.inf)
    sink = sinks.astype(jnp.float32).reshape(SWA_KV_HEADS, SWA_GROUP)[None, None, :, :, None, None]
    m = jnp.maximum(jnp.max(s, axis=-1, keepdims=True), sink)
    p = jnp.exp(s - m)
    p = p / (jnp.sum(p, axis=-1, keepdims=True) + jnp.exp(sink - m))
    o = jnp.einsum('bnkgij,bnjkd->bnikgd', p.astype(v.dtype), vb)
    return o.reshape(B, S, SWA_Q_HEADS * D)


def differential_attention(q, k, v, lam, g_subln, lambda_init):
    B, S, H, _, D = q.shape
    nb = S // Q_BLOCK
    qb = jnp.moveaxis(q.reshape(B, nb, Q_BLOCK, H, 2, D), 1, 0)
    kpos = jnp.arange(S)
    scale = 1.0 / math.sqrt(D)

    def block(args):
        qblk, start = args
        s = jnp.einsum('bihmd,bjhmd->bhmij', qblk, k,
                       preferred_element_type=jnp.float32) * scale
        qpos = start + jnp.arange(Q_BLOCK)
        causal = kpos[None, :] <= qpos[:, None]
        p = jax.nn.softmax(jnp.where(causal, s, -jnp.inf), axis=-1)
        a = p[:, :, 0] - lam * p[:, :, 1]
        return jnp.einsum('bhij,bjhe->bihe', a.astype(v.dtype), v)

    o = lax.map(block, (qb, jnp.arange(nb) * Q_BLOCK))
    o = jnp.moveaxis(o, 0, 1).reshape(B, S, H, DIFF_V_DIM)
    o = rms_norm(o, g_subln) * (1.0 - lambda_init)
    return o.reshape(B, S, H * DIFF_V_DIM)


def swiglu_clamped(u):
    x_glu = jnp.minimum(u[..., ::2], SWIGLU_LIMIT)
    x_lin = jnp.clip(u[..., 1::2], -SWIGLU_LIMIT, SWIGLU_LIMIT)
    return x_glu * jax.nn.sigmoid(SWIGLU_ALPHA * x_glu) * (x_lin + 1)


def moe_ffn(h, w_router, b_router, w1, b1, w2, b2):
    B, S, D = h.shape
    N = B * S
    hf = h.reshape(N, D)
    logits = (hf @ w_router + b_router).astype(jnp.float32)
    top_val, top_idx = lax.top_k(logits, TOP_K)
    gate = jax.nn.softmax(top_val, axis=-1)
    flat_e = top_idx.reshape(-1)
    flat_tok = jnp.repeat(jnp.arange(N, dtype=jnp.int32), TOP_K)
    flat_w = gate.reshape(-1)
    order = jnp.argsort(flat_e)
    e_sorted, tok_sorted, w_sorted = flat_e[order], flat_tok[order], flat_w[order]
    counts = jnp.bincount(flat_e, length=N_EXPERTS)
    padded = (counts + EXPERT_BLOCK - 1) // EXPERT_BLOCK * EXPERT_BLOCK
    starts = jnp.cumsum(counts) - counts
    pends = jnp.cumsum(padded)
    pstarts = pends - padded
    dest = pstarts[e_sorted] + (jnp.arange(N * TOP_K) - starts[e_sorted])
    n_rows = (N * TOP_K + N_EXPERTS * (EXPERT_BLOCK - 1) + EXPERT_BLOCK - 1) // EXPERT_BLOCK * EXPERT_BLOCK
    n_blocks = n_rows // EXPERT_BLOCK
    row_tok = jnp.full((n_rows,), N, jnp.int32).at[dest].set(tok_sorted)
    row_w = jnp.zeros((n_rows,), jnp.float32).at[dest].set(w_sorted)
    block_e = jnp.clip(jnp.searchsorted(pends, jnp.arange(n_blocks) * EXPERT_BLOCK, side='right'),
                       0, N_EXPERTS - 1)
    x_rows = jnp.concatenate([hf, jnp.zeros((1, D), hf.dtype)], axis=0)[row_tok]
    x_rows = x_rows.reshape(n_blocks, EXPERT_BLOCK, D)

    def expert_block(args):
        xb, e = args
        u = xb @ w1[e] + b1[e]
        return swiglu_clamped(u) @ w2[e] + b2[e]

    y = lax.map(expert_block, (x_rows, block_e)).reshape(n_rows, D)
    y = y * row_w[:, None].astype(y.dtype)
    out = jax.ops.segment_sum(y, row_tok, num_segments=N + 1)[:N]
    return out.reshape(B, S, D)


def setup_inputs(seed: int = 0) -> dict:
    key = jax.random.key(seed)
    ks = jax.random.split(key, 26)
    nrm = lambda k, shape: jax.random.normal(k, shape, jnp.float32)
    L, D = DEPTH, D_MODEL
    offsets = jax.random.randint(ks[2], (BATCH, 1), 0, 4096, dtype=jnp.int32)
    positions = offsets + jnp.arange(SEQ, dtype=jnp.int32)[None, :]
    return {
        "x": nrm(ks[0], (BATCH, SEQ, D)),
        "c": nrm(ks[1], (BATCH, D)),
        "positions": positions,
        "w_ada": nrm(ks[3], (L, D, N_MOD * D)) * (0.5 * D ** -0.5),
        "b_ada": nrm(ks[4], (L, N_MOD * D)) * 0.02,
        "g_mix": 1.0 + 0.02 * nrm(ks[5], (L, D)),
        "w_in": nrm(ks[6], (L, D, IN_COLS)) * D ** -0.5,
        "b_in": nrm(ks[7], (L, IN_COLS)) * 0.02,
        "attn_sinks": nrm(ks[8], (L, SWA_Q_HEADS)) * 0.5,
        "lambda_q1": nrm(ks[9], (L, HEAD_DIM)) * 0.1,
        "lambda_k1": nrm(ks[10], (L, HEAD_DIM)) * 0.1,
        "lambda_q2": nrm(ks[11], (L, HEAD_DIM)) * 0.1,
        "lambda_k2": nrm(ks[12], (L, HEAD_DIM)) * 0.1,
        "g_subln": 1.0 + 0.02 * nrm(ks[13], (L, DIFF_V_DIM)),
        "w_out": nrm(ks[14], (L, MIX_WIDTH, D)) * MIX_WIDTH ** -0.5,
        "b_out": nrm(ks[15], (L, D)) * 0.02,
        "g_ffn": 1.0 + 0.02 * nrm(ks[16], (L, D)),
        "w_router": nrm(ks[17], (L, D, N_EXPERTS)) * D ** -0.5,
        "b_router": nrm(ks[18], (L, N_EXPERTS)) * 0.01,
        "w1": nrm(ks[19], (L, N_EXPERTS, D, 2 * D_FF)) * D ** -0.5,
        "b1": nrm(ks[20], (L, N_EXPERTS, 2 * D_FF)) * 0.02,
        "w2": nrm(ks[21], (L, N_EXPERTS, D_FF, D)) * D_FF ** -0.5,
        "b2": nrm(ks[22], (L, N_EXPERTS, D)) * 0.02,
        "g_final": 1.0 + 0.02 * nrm(ks[23], (D,)),
    }


def reference(x, c, positions, w_ada, b_ada, g_mix, w_in, b_in, attn_sinks,
              lambda_q1, lambda_k1, lambda_q2, lambda_k2, g_subln, w_out, b_out,
              g_ffn, w_router, b_router, w1, b1, w2, b2, g_final):
    B, S, D = x.shape
    cos, sin = rope_tables(positions)
    cond = jax.nn.silu(c)
    for layer in range(DEPTH):
        mod = cond @ w_ada[layer] + b_ada[layer]
        sh1, sc1, gt1, sh2, sc2, gt2 = jnp.split(mod, N_MOD, axis=-1)

        h = modulate(rms_norm(x, g_mix[layer]), sh1, sc1)
        proj = h @ w_in[layer] + b_in[layer]
        qa, ka, va, qd, kd, vd = jnp.split(proj, IN_SPLITS, axis=-1)
        qa = apply_rope(qa.reshape(B, S, SWA_Q_HEADS, HEAD_DIM), cos, sin)
        ka = apply_rope(ka.reshape(B, S, SWA_KV_HEADS, HEAD_DIM), cos, sin)
        va = va.reshape(B, S, SWA_KV_HEADS, HEAD_DIM)
        out_a = sliding_window_sink_attention(qa, ka, va, attn_sinks[layer])
        qd = apply_rope(qd.reshape(B, S, DIFF_HEADS * 2, HEAD_DIM), cos, sin)
        kd = apply_rope(kd.reshape(B, S, DIFF_HEADS * 2, HEAD_DIM), cos, sin)
        qd = qd.reshape(B, S, DIFF_HEADS, 2, HEAD_DIM)
        kd = kd.reshape(B, S, DIFF_HEADS, 2, HEAD_DIM)
        vd = vd.reshape(B, S, DIFF_HEADS, DIFF_V_DIM)
        lambda_init = 0.8 - 0.6 * math.exp(-0.3 * layer)
        lam = (jnp.exp(jnp.sum(lambda_q1[layer].astype(jnp.float32) * lambda_k1[layer].astype(jnp.float32)))
               - jnp.exp(jnp.sum(lambda_q2[layer].astype(jnp.float32) * lambda_k2[layer].astype(jnp.float32)))
               + lambda_init)
        out_b = differential_attention(qd, kd, vd, lam, g_subln[layer], lambda_init)
        mixed = jnp.concatenate([out_a, out_b], axis=-1)
        x = x + gt1[:, None, :] * (mixed @ w_out[layer] + b_out[layer])

        h = modulate(rms_norm(x, g_ffn[layer]), sh2, sc2)
        x = x + gt2[:, None, :] * moe_ffn(h, w_router[layer], b_router[layer],
                                          w1[layer], b1[layer], w2[layer], b2[layer])
    return rms_norm(x, g_final)
```

```python
import math
import os
from contextlib import ExitStack

import numpy as np
import concourse.bass as bass
import concourse.mybir as mybir
from concourse.bass_utils import run_bass_kernel_spmd

F32 = mybir.dt.float32
BF16 = mybir.dt.bfloat16
I32 = mybir.dt.int32
ALU = mybir.AluOpType
AF = mybir.ActivationFunctionType
AX = mybir.AxisListType

D = 1024
S = 8192
NT = 64
NG = 16
NOWN = 16
NE = 32
SX = S + 2 * NOWN * 128
NGX = SX // 512
NCST = 720
CAP = 2048
U32 = mybir.dt.uint32
EPS = 1e-5
C1 = 6.28125
C2 = 2 * math.pi - 6.28125
INV2PI = float(1.0 / (2 * math.pi))

OFF_QA, OFF_KA, OFF_VA, OFF_QD, OFF_KD, OFF_VD = 0, 512, 640, 768, 1280, 1792


def _swap64(cols):
    cols = np.asarray(cols).reshape(-1, 64)
    return np.concatenate([cols[:, 32:], cols[:, :32]], axis=1).reshape(-1)


def _unit_cols():
    units = []
    k = np.concatenate([np.tile(np.arange(OFF_KA + g * 64, OFF_KA + (g + 1) * 64), 2) for g in range(2)])
    q = np.arange(OFF_QA, OFF_QA + 512)
    v = np.arange(OFF_VA, OFF_VA + 128)
    units.append(dict(nk=2, nq=4, k=k, q=q, v=v))
    for h in range(4):
        k = np.arange(OFF_KD + h * 128, OFF_KD + (h + 1) * 128)
        q = np.arange(OFF_QD + h * 128, OFF_QD + (h + 1) * 128)
        v = np.arange(OFF_VD + h * 128, OFF_VD + (h + 1) * 128)
        units.append(dict(nk=1, nq=1, k=k, q=q, v=v))
    off = 0
    sel = []
    for u in units:
        u["base"] = off
        parts = [u["k"], _swap64(u["k"]), u["q"], _swap64(u["q"]), u["v"]]
        u["o_k"] = 0
        u["o_ks"] = len(u["k"])
        u["o_q"] = u["o_ks"] + len(u["k"])
        u["o_qs"] = u["o_q"] + len(u["q"])
        u["o_v"] = u["o_qs"] + len(u["q"])
        u["ncols"] = u["o_v"] + 128
        sel.append(np.concatenate(parts))
        off += u["ncols"]
    return units, np.concatenate(sel)


UNITS, SEL = _unit_cols()
NSEL = len(SEL)
NCH = NSEL // 128


def own_blocks(j):
    return sorted([8 * m + j for m in range(8)] + [8 * m + 7 - j for m in range(8)])


class Tr:
    __slots__ = ("w", "r")

    def __init__(self):
        self.w = {}
        self.r = {}


def trs(n):
    return [Tr() for _ in range(n)]


class Prog:
    ENG = ("pe", "act", "dve", "pool", "sp")

    def __init__(self, nc, stack, n_dma_sems=48):
        self.nc = nc
        self.q = {e: [] for e in self.ENG}
        self.esem = {e: stack.enter_context(nc.semaphore("s_" + e)) for e in self.ENG}
        self.ecnt = {e: 0 for e in self.ENG}
        self.waited = {e: {} for e in self.ENG}
        self.dsem = [stack.enter_context(nc.semaphore("d%d" % i)) for i in range(n_dma_sems)]
        self.dcnt = [0] * n_dma_sems
        self.dpool = {"sp": list(range(0, n_dma_sems - 24)), "act": list(range(n_dma_sems - 24, n_dma_sems - 16)),
                      "pool": list(range(n_dma_sems - 16, n_dma_sems))}
        self.dnext = {"sp": 0, "pool": 0, "act": 0}
        self.in_cond = False
        self.handles = {"pe": nc.tensor, "act": nc.scalar, "dve": nc.vector, "pool": nc.gpsimd, "sp": nc.sync}

    def _need(self, eng, s, v):
        wd = self.waited[eng]
        if wd.get(s, 0) >= v:
            return
        wd[s] = v
        self.q[eng].append(("wait", s, v))

    def _waits(self, eng, reads, writes):
        need = {}
        for t in reads:
            for s, v in t.w.items():
                if need.get(s, 0) < v:
                    need[s] = v
        for t in writes:
            for s, v in t.w.items():
                if need.get(s, 0) < v:
                    need[s] = v
            for s, v in t.r.items():
                if need.get(s, 0) < v:
                    need[s] = v
        for s, v in need.items():
            if eng == "pe" and s is self.esem["pe"]:
                continue
            self._need(eng, s, v)

    def _record(self, ev, reads, writes):
        s, v = ev
        for t in reads:
            if t.r.get(s, 0) < v:
                t.r[s] = v
        for t in writes:
            if self.in_cond:
                if t.w.get(s, 0) < v:
                    t.w[s] = v
            else:
                t.w = {s: v}
                t.r = {}

    def op(self, eng, fn, reads=(), writes=()):
        self._waits(eng, reads, writes)
        self.ecnt[eng] += 1
        ev = (self.esem[eng], self.ecnt[eng])
        self.q[eng].append(("op", fn, self.esem[eng], 1))
        self._record(ev, reads, writes)

    def group(self, eng, fns, reads=(), writes=()):
        self._waits(eng, reads, writes)
        self.ecnt[eng] += 1
        ev = (self.esem[eng], self.ecnt[eng])
        for f in fns[:-1]:
            self.q[eng].append(("op", f, None, 0))
        self.q[eng].append(("op", fns[-1], self.esem[eng], 1))
        self._record(ev, reads, writes)

    def dma(self, eng, fn, reads=(), writes=(), slot=None):
        pl = self.dpool[eng]
        if slot is None:
            i = pl[self.dnext[eng]]
            self.dnext[eng] = (self.dnext[eng] + 1) % (len(pl) - 4)
        else:
            i = pl[len(pl) - 4 + slot]
        s = self.dsem[i]
        if self.dcnt[i]:
            self._need(eng, s, self.dcnt[i])
        self._waits(eng, reads, writes)
        self.dcnt[i] += 16
        ev = (s, self.dcnt[i])
        self.q[eng].append(("op", fn, s, 16))
        self._record(ev, reads, writes)
        return ev

    CENG = ("pe", "act", "dve")

    def regload(self, ap, reads=()):
        for e in self.CENG:
            self._waits(e, reads, ())
            self.q[e].append(("regload", ap))

    def cond_begin(self, thr):
        self._csnap = ({e: self.ecnt[e] for e in self.ENG}, list(self.dcnt), {e: dict(self.waited[e]) for e in self.ENG})
        self.in_cond = True
        for e in self.CENG:
            self.q[e].append(["if", thr, None])

    def cond_end(self):
        ec0, dc0, wd0 = self._csnap
        assert self.ecnt["pool"] == ec0["pool"] and self.ecnt["sp"] == ec0["sp"], "pool/sp must stay outside conditional regions"
        dd = [(i, self.dcnt[i] - dc0[i]) for i in range(len(self.dcnt)) if self.dcnt[i] != dc0[i]]
        for i, _ in dd:
            assert i in self.dpool["act"]
        for e in self.CENG:
            comp = []
            if self.ecnt[e] != ec0[e]:
                comp.append((self.esem[e], self.ecnt[e] - ec0[e]))
            if e == "act":
                comp += [(self.dsem[i], d, dc0[i]) for i, d in dd]
            for it in reversed(self.q[e]):
                if isinstance(it, list) and it[0] == "if" and it[2] is None:
                    it[2] = comp
                    break
            self.q[e].append(("endif",))
            self.waited[e] = wd0[e]
        self.waited["pool"] = wd0["pool"]
        self.waited["sp"] = wd0["sp"]
        self.in_cond = False

    def barrier(self):
        for e in self.ENG:
            for f in self.ENG:
                if f != e and self.ecnt[f]:
                    self._need(e, self.esem[f], self.ecnt[f])
            for i, s in enumerate(self.dsem):
                if self.dcnt[i]:
                    self._need(e, s, self.dcnt[i])

    def emit(self):
        nc = self.nc
        q = self.q
        self.q = {e: [] for e in self.ENG}
        if not hasattr(self, "regs"):
            self.regs = {}
        with nc.Block() as block:
            def run_items(h, ename, items):
                i = 0
                n = len(items)
                while i < n:
                    it = items[i]
                    k = it[0]
                    if k == "wait":
                        h.wait_ge(it[1], it[2])
                    elif k == "op":
                        ins = it[1]()
                        if it[2] is not None:
                            ins.then_inc(it[2], it[3])
                    elif k == "regload":
                        if ename not in self.regs:
                            self.regs[ename] = h.alloc_register("cnt_" + ename)
                        h.reg_load(self.regs[ename], it[1])
                    elif k == "if":
                        depth = 1
                        j = i + 1
                        while True:
                            if items[j][0] == "if":
                                depth += 1
                            elif items[j][0] == "endif":
                                depth -= 1
                                if depth == 0:
                                    break
                            j += 1
                        body = items[i + 1:j]
                        with h.If_lt(self.regs[ename], it[1]):
                            h.drain()
                            for cp in it[2]:
                                if len(cp) == 3 and cp[2]:
                                    h.wait_ge(cp[0], cp[2])
                                h.sem_inc(cp[0], cp[1])
                        with h.Else():
                            run_items(h, ename, body)
                        i = j
                    i += 1

            def run(ename):
                run_items(self.handles[ename], ename, q[ename])

            @block.tensor
            def _(e):
                run("pe")

            @block.scalar
            def _(e):
                run("act")

            @block.vector
            def _(e):
                run("dve")

            @block.gpsimd
            def _(e):
                run("pool")

            @block.sync
            def _(e):
                run("sp")


def build_program(j_core_unused=None, debug=False):
    nc = bass.Bass("TRN2", target_bir_lowering=False)
    din = lambda name, shape, dt=F32: nc.dram_tensor(name, list(shape), dt, kind="ExternalInput").ap()
    x_d = din("x", [SX, D])
    pos_d = din("pos", [1, SX], I32)
    dmask_d = din("dmask", [128, NOWN * 512])
    smask_d = din("smask", [128, NOWN * 256])
    cT_d = din("cT", [128, 8])
    wada_d = din("w_ada", [D, 6 * D])
    bada_d = din("b_ada", [1, 6 * D])
    gmixT_d = din("gmixT", [128, 8])
    gffnT_d = din("gffnT", [128, 8])
    wsel_d = din("w_sel", [D, NSEL])
    bselT_d = din("b_selT", [128, NCH])
    bsel_d = din("b_sel", [1, NSEL])
    sinks_d = din("sinks", [1, 8])
    lam_d = din("lam4", [4, 64])
    gsub_d = din("g_subln", [1, 128])
    wout_d = din("w_out", [D, D])
    bout_d = din("b_out", [1, D])
    wr_d = din("w_router", [D, NE])
    br_d = din("b_router", [1, NE])
    w1_d = din("w1", [NE, D, 2 * D])
    b1_d = din("b1", [NE, 2 * D])
    gffn_d = din("g_ffn", [1, D])
    w2_d = din("w2", [NE, D, D])
    b2_d = din("b2", [NE, D])
    gfin_d = din("g_final", [1, D])
    cst_d = din("consts", [128, NCST])
    out_d = nc.dram_tensor("out", [NOWN * 128, D], F32, kind="ExternalOutput").ap()
    hT_d = nc.dram_tensor("hT_scr", [8, 128, SX], BF16, kind="Internal").ap()
    cos_d = nc.dram_tensor("cos_scr", [128, SX], F32, kind="Internal").ap()
    sin_d = nc.dram_tensor("sin_scr", [128, SX], F32, kind="Internal").ap()
    x1_d = nc.dram_tensor("x1_scr", [NOWN * 128, D], F32, kind="Internal").ap()
    mod_d = nc.dram_tensor("mod_scr", [1, 6 * D], F32, kind="Internal").ap()
    xbuf_d = nc.dram_tensor("xbuf_scr", [NE * CAP + 128, D], BF16, kind="Internal").ap()
    ybuf_d = nc.dram_tensor("ybuf_scr", [NE * CAP, D], F32, kind="Internal").ap()


    with ExitStack() as st:
        P = Prog(nc, st)
        sbuf = lambda stack, name, shape, dt=F32: stack.enter_context(nc.sbuf_tensor(name, list(shape), dt))
        V, A, T, G_ = nc.vector, nc.scalar, nc.tensor, nc.gpsimd

        bank = [st.enter_context(nc.psum_tensor("bank%d" % i, [128, 512], F32)) for i in range(8)]
        tb = trs(8)

        cst = sbuf(st, "cst", [128, NCST]); t_cst = Tr()
        identb = sbuf(st, "identb", [128, 128], BF16)
        mask256 = sbuf(st, "mask256", [128, 256], BF16)
        onesb = sbuf(st, "onesb", [1, 128], BF16)
        trib = sbuf(st, "trib", [128, 128], BF16)
        ones128b = sbuf(st, "ones128b", [128, 128], BF16)
        A1 = sbuf(st, "A1", [128, 8]); S1 = sbuf(st, "S1", [128, 8])
        A2 = sbuf(st, "A2", [128, 8]); S2 = sbuf(st, "S2", [128, 8])
        t_mod = Tr()
        t_mixed = trs(NOWN)
        small = sbuf(st, "small", [128, 64]); t_small = Tr()
        ident = cst[:, 0:128]
        invf = cst[:, 384:385]
        sgn = cst[:, 385:386]
        halfpi = cst[:, 386:387]
        zero_c = cst[:, 387:388]
        one11 = cst[0:1, 388:389]
        ones_row = cst[0:1, 392:520]
        neglam = small[:, 0:1]
        expsink = small[:, 8:16]

        P.dma("sp", lambda: nc.sync.dma_start(out=cst[:], in_=cst_d[:, :]), writes=[t_cst])
        P.op("dve", lambda: V.tensor_copy(out=identb[:], in_=cst[:, 0:128]), reads=[t_cst], writes=[t_cst])
        P.op("dve", lambda: V.tensor_copy(out=mask256[:, 0:128], in_=cst[:, 256:384]), reads=[t_cst], writes=[t_cst])
        P.op("dve", lambda: V.tensor_copy(out=mask256[:, 128:256], in_=cst[:, 128:256]), reads=[t_cst], writes=[t_cst])
        P.op("dve", lambda: V.tensor_copy(out=onesb[:], in_=cst[0:1, 392:520]), reads=[t_cst], writes=[t_cst])
        P.op("dve", lambda: V.tensor_copy(out=trib[:], in_=cst[:, 520:648]), reads=[t_cst], writes=[t_cst])
        P.op("dve", lambda: V.tensor_copy(out=ones128b[:], in_=cst[:, 392:520]), reads=[t_cst], writes=[t_cst])

        with ExitStack() as s0:
            cT = sbuf(s0, "cT_sb", [128, 8]); t_cT = Tr()
            wad = [sbuf(s0, "wad%d" % i, [128, 8, 512]) for i in range(2)]; t_wad = trs(2)
            modrow = sbuf(s0, "modrow", [1, 6 * D]); t_modrow = Tr()
            badar = sbuf(s0, "badar", [1, 6 * D]); t_bada = Tr()
            gT = sbuf(s0, "gT", [128, 16]); t_gT = Tr()
            lamb = sbuf(s0, "lamb", [128, 256]); t_lam = Tr()
            lamp = sbuf(s0, "lamp", [128, 128])
            P.dma("sp", lambda: nc.sync.dma_start(out=cT[:], in_=cT_d[:, :]), writes=[t_cT])
            P.dma("sp", lambda: nc.sync.dma_start(out=badar[:], in_=bada_d[:, :]), writes=[t_bada])
            P.dma("sp", lambda: nc.sync.dma_start(out=gT[:, 0:8], in_=gmixT_d[:, :]), writes=[t_gT])
            P.dma("sp", lambda: nc.sync.dma_start(out=gT[:, 8:16], in_=gffnT_d[:, :]), writes=[t_gT])
            P.dma("sp", lambda: nc.sync.dma_start(out=lamb[:].rearrange("p (a b) -> p a b", a=4),
                                                  in_=lam_d[:, :].partition_broadcast(128)), writes=[t_lam])
            P.dma("sp", lambda: nc.sync.dma_start(out=small[:, 16:24], in_=sinks_d[0:1, :].partition_broadcast(128)), writes=[t_small])
            P.op("act", lambda: A.activation(out=cT[:], in_=cT[:], func=AF.Silu), reads=[t_cT], writes=[t_cT])
            wada_v = wada_d.rearrange("(k p) n -> p k n", p=128)
            for pc in range(12):
                b = pc % 2
                P.dma("sp", lambda pc=pc, b=b: nc.sync.dma_start(out=wad[b][:], in_=wada_v[:, :, pc * 512:(pc + 1) * 512]), writes=[t_wad[b]])
                bk = pc % 2
                P.group("pe", [(lambda kc=kc, b=b, bk=bk: T.matmul(bank[bk][0:1, :], lhsT=cT[:, kc:kc + 1], rhs=wad[b][:, kc, :],
                                                                    start=(kc == 0), stop=(kc == 7))) for kc in range(8)],
                        reads=[t_cT, t_wad[b]], writes=[tb[bk]])
                P.op("dve", lambda pc=pc, bk=bk: V.tensor_tensor(out=modrow[0:1, pc * 512:(pc + 1) * 512], in0=bank[bk][0:1, :],
                                                                 in1=badar[0:1, pc * 512:(pc + 1) * 512], op=ALU.add),
                     reads=[tb[bk], t_bada], writes=[t_modrow])
            cols = [(0, 0), (1, 8), (3, 16), (4, 24)]
            fns = []
            for mi, dc in cols:
                for kc in range(8):
                    fns.append(lambda mi=mi, dc=dc, kc=kc: T.matmul(bank[2][:, dc + kc:dc + kc + 1],
                                                                    lhsT=modrow[0:1, mi * D + kc * 128: mi * D + (kc + 1) * 128],
                                                                    rhs=one11, start=True, stop=True))
            P.group("pe", fns, reads=[t_modrow, t_cst], writes=[tb[2]])
            P.op("dve", lambda: V.tensor_copy(out=S1[:], in_=bank[2][:, 0:8]), reads=[tb[2]], writes=[t_mod])
            P.op("dve", lambda: V.scalar_tensor_tensor(out=A1[:], in0=bank[2][:, 8:16], scalar=1.0, in1=gT[:, 0:8], op0=ALU.add, op1=ALU.mult),
                 reads=[tb[2], t_gT], writes=[t_mod])
            P.op("dve", lambda: V.tensor_copy(out=S2[:], in_=bank[2][:, 16:24]), reads=[tb[2]], writes=[t_mod])
            P.op("dve", lambda: V.scalar_tensor_tensor(out=A2[:], in0=bank[2][:, 24:32], scalar=1.0, in1=gT[:, 8:16], op0=ALU.add, op1=ALU.mult),
                 reads=[tb[2], t_gT], writes=[t_mod])
            P.dma("sp", lambda: nc.sync.dma_start(out=mod_d[:, :], in_=modrow[:]), reads=[t_modrow])
            P.op("dve", lambda: V.tensor_tensor(out=lamp[:, 0:64], in0=lamb[:, 0:64], in1=lamb[:, 64:128], op=ALU.mult), reads=[t_lam], writes=[t_lam])
            P.op("dve", lambda: V.tensor_tensor(out=lamp[:, 64:128], in0=lamb[:, 128:192], in1=lamb[:, 192:256], op=ALU.mult), reads=[t_lam], writes=[t_lam])
            P.op("dve", lambda: V.tensor_reduce(out=small[:, 1:3], in_=lamp[:].rearrange("p (a b) -> p a b", a=2), axis=AX.X, op=ALU.add),
                 reads=[t_lam], writes=[t_small])
            P.op("act", lambda: A.activation(out=small[:, 1:3], in_=small[:, 1:3], func=AF.Exp), reads=[t_small], writes=[t_small])
            P.op("dve", lambda: V.scalar_tensor_tensor(out=small[:, 0:1], in0=small[:, 2:3], scalar=-0.2, in1=small[:, 1:2], op0=ALU.add, op1=ALU.subtract),
                 reads=[t_small], writes=[t_small])
            P.op("act", lambda: A.activation(out=small[:, 8:16], in_=small[:, 16:24], func=AF.Exp), reads=[t_small], writes=[t_small])
            P.barrier()
            P.emit()

        with ExitStack() as s1:
            xt = [sbuf(s1, "xt%d" % i, [128, D]) for i in range(3)]; t_xt = trs(3)
            xn = [sbuf(s1, "xn%d" % i, [128, D], BF16) for i in range(2)]; t_xn = trs(2)
            junk = sbuf(s1, "junk", [128, D], BF16); t_junk = Tr()
            ssq = sbuf(s1, "ssq", [128, 8]); t_ssq = trs(4)
            hTg = [sbuf(s1, "hTg%d" % i, [128, 8, 512], BF16) for i in range(2)]; t_hTg = trs(2)
            posi = sbuf(s1, "posi", [128, 512], I32); t_posi = Tr()
            ang = sbuf(s1, "ang", [128, 512]); t_ang = Tr()
            ki = sbuf(s1, "ki", [128, 512], I32); kf = sbuf(s1, "kf", [128, 512]); t_k = Tr()
            rr = sbuf(s1, "rr", [128, 512]); t_rr = Tr()
            tab = [sbuf(s1, "tab%d" % i, [128, 512]) for i in range(4)]; t_tab = trs(4)
            hT_v = hT_d.rearrange("k p t -> p k t")
            tcount = 0
            for g in range(NGX):
                hb = g % 2
                for tt in range(4):
                    t = 4 * g + tt
                    xb = tcount % 3
                    nb = tcount % 2
                    sq = tcount % 4
                    tcount += 1
                    P.dma("sp", lambda t=t, xb=xb: nc.sync.dma_start(out=xt[xb][:], in_=x_d[t * 128:(t + 1) * 128, :]), writes=[t_xt[xb]])
                    P.op("act", lambda xb=xb, sq=sq: A.activation(out=junk[:], in_=xt[xb][:], func=AF.Square, accum_out=ssq[:, 2 * sq:2 * sq + 1]),
                         reads=[t_xt[xb]], writes=[t_junk, t_ssq[sq]])
                    P.op("dve", lambda sq=sq: V.tensor_scalar(out=ssq[:, 2 * sq + 1:2 * sq + 2], in0=ssq[:, 2 * sq:2 * sq + 1], scalar1=1.0 / D, scalar2=EPS,
                                                              op0=ALU.mult, op1=ALU.add), reads=[t_ssq[sq]], writes=[t_ssq[sq]])
                    P.op("act", lambda sq=sq: A.activation(out=ssq[:, 2 * sq + 1:2 * sq + 2], in_=ssq[:, 2 * sq + 1:2 * sq + 2], func=AF.Sqrt),
                         reads=[t_ssq[sq]], writes=[t_ssq[sq]])
                    P.op("dve", lambda sq=sq: V.reciprocal(out=ssq[:, 2 * sq + 1:2 * sq + 2], in_=ssq[:, 2 * sq + 1:2 * sq + 2]),
                         reads=[t_ssq[sq]], writes=[t_ssq[sq]])
                    P.op("dve", lambda xb=xb, nb=nb, sq=sq: V.tensor_scalar(out=xn[nb][:], in0=xt[xb][:], scalar1=ssq[:, 2 * sq + 1:2 * sq + 2], scalar2=None, op0=ALU.mult),
                         reads=[t_xt[xb], t_ssq[sq]], writes=[t_xn[nb]])
                    bk = nb
                    pT = bank[bk][:, :].bitcast(BF16)
                    P.group("pe", [(lambda kc=kc, nb=nb, pT=pT: T.transpose(out=pT[:, kc * 128:(kc + 1) * 128], in_=xn[nb][:, kc * 128:(kc + 1) * 128], identity=identb[:]))
                                   for kc in range(8)], reads=[t_xn[nb], t_cst], writes=[tb[bk]])
                    for kc in range(8):
                        if kc % 2 == 0:
                            P.op("act", lambda kc=kc, hb=hb, tt=tt, pT=pT: A.activation(out=hTg[hb][:, kc, tt * 128:(tt + 1) * 128], in_=pT[:, kc * 128:(kc + 1) * 128],
                                                                                      func=AF.Identity, scale=A1[:, kc:kc + 1], bias=S1[:, kc:kc + 1]),
                                 reads=[tb[bk], t_mod], writes=[t_hTg[hb]])
                        else:
                            P.op("dve", lambda kc=kc, hb=hb, tt=tt, pT=pT: V.tensor_scalar(out=hTg[hb][:, kc, tt * 128:(tt + 1) * 128], in0=pT[:, kc * 128:(kc + 1) * 128],
                                                                                         scalar1=A1[:, kc:kc + 1], scalar2=S1[:, kc:kc + 1], op0=ALU.mult, op1=ALU.add),
                                 reads=[tb[bk], t_mod], writes=[t_hTg[hb]])
                P.dma("sp", lambda g=g, hb=hb: nc.sync.dma_start(out=hT_v[:, :, g * 512:(g + 1) * 512], in_=hTg[hb][:]), reads=[t_hTg[hb]])
                P.dma("sp", lambda g=g: nc.sync.dma_start(out=posi[:], in_=pos_d[0:1, g * 512:(g + 1) * 512].partition_broadcast(128)), writes=[t_posi])
                P.op("dve", lambda: V.tensor_copy(out=ang[:], in_=posi[:]), reads=[t_posi], writes=[t_ang])
                P.op("dve", lambda: V.tensor_scalar(out=ang[:], in0=ang[:], scalar1=invf, scalar2=None, op0=ALU.mult), reads=[t_ang, t_cst], writes=[t_ang])
                for which in range(2):
                    tbi = (2 * g + which) % 4
                    if which == 0:
                        P.op("dve", lambda: V.tensor_scalar(out=ki[:], in0=ang[:], scalar1=INV2PI, scalar2=None, op0=ALU.mult), reads=[t_ang], writes=[t_k])
                    else:
                        P.op("dve", lambda: V.tensor_scalar(out=ki[:], in0=ang[:], scalar1=INV2PI, scalar2=0.25, op0=ALU.mult, op1=ALU.add), reads=[t_ang], writes=[t_k])
                    P.op("dve", lambda: V.tensor_copy(out=kf[:], in_=ki[:]), reads=[t_k], writes=[t_k])
                    P.op("dve", lambda: V.scalar_tensor_tensor(out=rr[:], in0=kf[:], scalar=-C1, in1=ang[:], op0=ALU.mult, op1=ALU.add), reads=[t_k, t_ang], writes=[t_rr])
                    P.op("dve", lambda: V.scalar_tensor_tensor(out=rr[:], in0=kf[:], scalar=-C2, in1=rr[:], op0=ALU.mult, op1=ALU.add), reads=[t_k, t_rr], writes=[t_rr])
                    if which == 0:
                        P.op("dve", lambda: V.tensor_scalar(out=rr[:], in0=rr[:], scalar1=-3.1415925, scalar2=3.1415925, op0=ALU.max, op1=ALU.min), reads=[t_rr], writes=[t_rr])
                    else:
                        P.op("dve", lambda: V.tensor_scalar(out=rr[:], in0=rr[:], scalar1=-4.712388, scalar2=1.570796, op0=ALU.max, op1=ALU.min), reads=[t_rr], writes=[t_rr])
                    if which == 0:
                        P.op("act", lambda tbi=tbi: A.activation(out=tab[tbi][:], in_=rr[:], func=AF.Sin, scale=sgn, bias=zero_c), reads=[t_rr, t_cst], writes=[t_tab[tbi]])
                        P.dma("sp", lambda g=g, tbi=tbi: nc.sync.dma_start(out=sin_d[:, g * 512:(g + 1) * 512], in_=tab[tbi][:]), reads=[t_tab[tbi]])
                    else:
                        P.op("act", lambda tbi=tbi: A.activation(out=tab[tbi][:], in_=rr[:], func=AF.Sin, scale=1.0, bias=halfpi), reads=[t_rr, t_cst], writes=[t_tab[tbi]])
                        P.dma("sp", lambda g=g, tbi=tbi: nc.sync.dma_start(out=cos_d[:, g * 512:(g + 1) * 512], in_=tab[tbi][:]), reads=[t_tab[tbi]])
            P.barrier()
            P.emit()

        s34 = st.enter_context(ExitStack())
        dest_i = sbuf(s34, "dest_i", [128, 4 * NOWN], I32); t_dest = trs(NOWN)
        gate4 = sbuf(s34, "gate4", [128, 4 * NOWN]); t_gate4 = trs(NOWN)
        maskb = sbuf(s34, "maskb", [128, NOWN, NE], BF16); t_maskb = trs(NOWN)
        cnt_run = sbuf(s34, "cnt_run", [128, NE]); t_cnt = Tr()
        cnt_i = sbuf(s34, "cnt_i", [1, NE], I32); t_cnti = Tr()
        padidx = sbuf(s34, "padidx", [128, NE], I32); t_pad = Tr()
        t_xbuf = Tr()
        iota32 = cst[:, 648:680]
        e2048 = cst[:, 680:712]
        iota_p = cst[:, 712:713]
        sA = ExitStack()
        bufA = sbuf(sA, "bufA", [128, 16 * 1024], BF16)
        mixed = bufA[:].rearrange("p (a b) -> p a b", a=NOWN)
        with ExitStack() as s2:
            Wu = sbuf(s2, "Wu", [128, 8, 1664], BF16); t_Wu = Tr()
            KT = sbuf(s2, "KT", [128, S], BF16); t_KT = Tr()
            Vb = sbuf(s2, "Vb", [128, 64 * 130], BF16); t_V = Tr()
            QT = sbuf(s2, "QT", [128, 4, NOWN * 128], BF16); t_QT = Tr()
            hTg = [sbuf(s2, "hTg2_%d" % i, [128, 8, 512], BF16) for i in range(2)]; t_hTg = trs(2)
            csg = [sbuf(s2, "csg%d" % i, [128, 2, 512]) for i in range(2)]; t_csg = trs(2)
            tm1 = [sbuf(s2, "tm1_%d" % i, [128, 512]) for i in range(2)]; t_tm1 = trs(2)
            tm2 = [sbuf(s2, "tm2_%d" % i, [128, 512]) for i in range(2)]; t_tm2 = trs(2)
            PT = [sbuf(s2, "PT%d" % i, [128, 512], BF16) for i in range(3)]; t_PT = trs(3)
            dmask = sbuf(s2, "dmask_sb", [128, NOWN, 512], BF16); t_dmask = Tr()
            smask = sbuf(s2, "smask_sb", [128, NOWN, 256], BF16); t_smask = Tr()
            bselT = sbuf(s2, "bselT", [128, NCH]); t_bsel = Tr()
            vbias = sbuf(s2, "vbias", [128, 128]); t_vbias = Tr()
            gsub_b = sbuf(s2, "gsub_b", [128, 128]); t_gsub = Tr()
            fin = sbuf(s2, "fin", [128, 8 * 128]); t_fin = Tr()
            fsm = sbuf(s2, "fsm", [128, 32]); t_fsm = Tr()
            junk2 = sbuf(s2, "junk2", [128, 128], BF16)
            hT_v = hT_d.rearrange("k p t -> p k t")
            wsel_v = wsel_d.rearrange("(k p) n -> p k n", p=128)
            for q4 in range(4):
                P.dma("pool", lambda q4=q4: G_.dma_start(out=dmask[:, 4 * q4:4 * q4 + 4, :], in_=dmask_d[:, q4 * 2048:(q4 + 1) * 2048].rearrange("p (a b) -> p a b", a=4)),
                      writes=[t_dmask])
            for q4 in range(2):
                P.dma("pool", lambda q4=q4: G_.dma_start(out=smask[:, 8 * q4:8 * q4 + 8, :], in_=smask_d[:, q4 * 2048:(q4 + 1) * 2048].rearrange("p (a b) -> p a b", a=8)),
                      writes=[t_smask])
            P.dma("sp", lambda: nc.sync.dma_start(out=bselT[:], in_=bselT_d[:, :]), writes=[t_bsel])
            P.dma("sp", lambda: nc.sync.dma_start(out=gsub_b[:], in_=gsub_d[0:1, :].partition_broadcast(128)), writes=[t_gsub])
            P.op("dve", lambda: V.tensor_scalar(out=gsub_b[:], in0=gsub_b[:], scalar1=0.8, scalar2=None, op0=ALU.mult), reads=[t_gsub], writes=[t_gsub])
            gcount = [0]

            def rope_proj(u, wc, wcs, hb, cb, ccol, ncol, dst, t_dst, par):
                bA, bB = bank[2 * par], bank[2 * par + 1]
                ci = (u["base"] + wc) // 128
                cis = (u["base"] + wcs) // 128
                P.group("pe", [(lambda kc=kc: T.matmul(bA[:, 0:ncol], lhsT=Wu[:, kc, wc:wc + 128], rhs=hTg[hb][:, kc, ccol:ccol + ncol], start=(kc == 0), stop=(kc == 7)))
                               for kc in range(8)], reads=[t_Wu, t_hTg[hb]], writes=[tb[2 * par]])
                P.group("pe", [(lambda kc=kc: T.matmul(bB[:, 0:ncol], lhsT=Wu[:, kc, wcs:wcs + 128], rhs=hTg[hb][:, kc, ccol:ccol + ncol], start=(kc == 0), stop=(kc == 7)))
                               for kc in range(8)], reads=[t_Wu, t_hTg[hb]], writes=[tb[2 * par + 1]])
                P.op("dve", lambda: V.scalar_tensor_tensor(out=tm1[par][:, 0:ncol], in0=bA[:, 0:ncol], scalar=bselT[:, ci:ci + 1], in1=csg[cb][:, 0, ccol:ccol + ncol],
                                                           op0=ALU.add, op1=ALU.mult), reads=[tb[2 * par], t_bsel, t_csg[cb]], writes=[t_tm1[par]])
                P.op("dve", lambda: V.scalar_tensor_tensor(out=tm2[par][:, 0:ncol], in0=bB[:, 0:ncol], scalar=bselT[:, cis:cis + 1], in1=csg[cb][:, 1, ccol:ccol + ncol],
                                                           op0=ALU.add, op1=ALU.mult), reads=[tb[2 * par + 1], t_bsel, t_csg[cb]], writes=[t_tm2[par]])
                P.op("pool", lambda: G_.tensor_tensor(out=dst, in0=tm1[par][:, 0:ncol], in1=tm2[par][:, 0:ncol], op=ALU.add),
                     reads=[t_tm1[par], t_tm2[par]], writes=[t_dst])

            def load_group(g):
                hb = gcount[0] % 2
                gcount[0] += 1
                P.dma("sp", lambda: nc.sync.dma_start(out=hTg[hb][:], in_=hT_v[:, :, g * 512:(g + 1) * 512]), writes=[t_hTg[hb]])
                P.dma("sp", lambda: nc.sync.dma_start(out=csg[hb][:, 0, :], in_=cos_d[:, g * 512:(g + 1) * 512]), writes=[t_csg[hb]])
                P.dma("sp", lambda: nc.sync.dma_start(out=csg[hb][:, 1, :], in_=sin_d[:, g * 512:(g + 1) * 512]), writes=[t_csg[hb]])
                return hb

            pcount = [0]

            def v_proj(u, hb, vt0, vw, swa):
                bk = 4 + (pcount[0] % 2)
                pcount[0] += 1
                ov = u["o_v"]
                fns = []
                for tt in range(4):
                    for kc in range(8):
                        fns.append(lambda tt=tt, kc=kc: T.matmul(bank[bk][:, tt * 128:(tt + 1) * 128], lhsT=hTg[hb][:, kc, tt * 128:(tt + 1) * 128],
                                                                 rhs=Wu[:, kc, ov:ov + 128], start=(kc == 0), stop=(kc == 7)))
                P.group("pe", fns, reads=[t_Wu, t_hTg[hb]], writes=[tb[bk]])
                src = bank[bk][:, :].rearrange("p (a b) -> p a b", a=4)
                vb_b = vbias[:].unsqueeze(1).to_broadcast([128, 4, 128])
                if not swa:
                    dst = Vb[:, vt0 * 129:(vt0 + 4) * 129].rearrange("p (a b) -> p a b", a=4)[:, :, 0:128]
                    P.op("dve", lambda: V.tensor_tensor(out=dst, in0=src, in1=vb_b, op=ALU.add), reads=[tb[bk], t_vbias], writes=[t_V])
                else:
                    for kv in range(2):
                        dst = Vb[:, vt0 * 130:(vt0 + 4) * 130].rearrange("p (a b) -> p a b", a=4)[:, :, kv * 65:kv * 65 + 64]
                        P.op("dve", lambda dst=dst, kv=kv: V.tensor_tensor(out=dst, in0=src[:, :, kv * 64:(kv + 1) * 64],
                                                                          in1=vbias[:, kv * 64:(kv + 1) * 64].unsqueeze(1).to_broadcast([128, 4, 64]), op=ALU.add),
                             reads=[tb[bk], t_vbias], writes=[t_V])

            for ui, u in enumerate(UNITS):
                swa = (ui == 0)
                nc_u = u["ncols"]
                P.dma("pool", lambda u=u, nc_u=nc_u: G_.dma_start(out=Wu[:, :, 0:nc_u], in_=wsel_v[:, :, u["base"]:u["base"] + nc_u]), writes=[t_Wu])
                P.dma("sp", lambda u=u: nc.sync.dma_start(out=vbias[:], in_=bsel_d[0:1, u["base"] + u["o_v"]:u["base"] + u["o_v"] + 128].partition_broadcast(128)),
                      writes=[t_vbias])
                if swa:
                    vv = Vb[:, 0:32 * 130].rearrange("p (a b) -> p a b", a=32)
                    P.op("pool", lambda vv=vv: G_.memset(vv[:, :, 64:65], 1.0), writes=[t_V])
                    P.op("pool", lambda vv=vv: G_.memset(vv[:, :, 129:130], 1.0), writes=[t_V])
                    kv_groups = [(20 + i, i * 512, 4 * i) for i in range(4)] + [(16 + i, 2048 + i * 512, 16 + 4 * i) for i in range(4)]
                elif ui == 1:
                    vv = Vb[:, 0:64 * 129].rearrange("p (a b) -> p a b", a=64)
                    P.op("pool", lambda vv=vv: G_.memset(vv[:, :, 128:129], 1.0), writes=[t_V])
                    kv_groups = [(g, g * 512, 4 * g) for g in range(NG)]
                else:
                    kv_groups = [(g, g * 512, 4 * g) for g in range(NG)]
                par = 0
                for (g, kcol, vt0) in kv_groups:
                    hb = load_group(g)
                    for kc_ in range(u["nk"]):
                        rope_proj(u, u["o_k"] + kc_ * 128, u["o_ks"] + kc_ * 128, hb, hb, 0, 512, KT[:, kc_ * 4096 + kcol:kc_ * 4096 + kcol + 512], t_KT, par)
                        par ^= 1
                    v_proj(u, hb, vt0, None, swa)
                    if swa and g < 20:
                        for qc in range(4):
                            rope_proj(u, u["o_q"] + qc * 128, u["o_qs"] + qc * 128, hb, hb, 0, 512, QT[:, qc, (g - 16) * 512:(g - 15) * 512], t_QT, par)
                            par ^= 1
                if not swa:
                    for g in range(16, 20):
                        hb = load_group(g)
                        rope_proj(u, u["o_q"], u["o_qs"], hb, hb, 0, 512, QT[:, 0, (g - 16) * 512:(g - 15) * 512], t_QT, par)
                        par ^= 1

                items = []
                if swa:
                    for oi in range(NOWN):
                        for hh in range(8):
                            items.append((oi, hh, 0, True))
                else:
                    for oi in range(NOWN):
                        nkb = 8 * (oi // 2) + (4 if oi % 2 == 0 else 8)
                        for m in range(2):
                            for c in range(nkb // 4):
                                items.append((oi, m, c, c == nkb // 4 - 1))

                def qk(n):
                    oi, a, c, last = items[n]
                    sb_ = n % 3
                    if swa:
                        hh = a; half = hh % 2; qc = hh // 2; kvg = hh // 4
                        ps = slice(half * 64, half * 64 + 64)
                        fns = [lambda: T.matmul(bank[sb_][:, 0:128], lhsT=KT[ps, kvg * 4096 + oi * 128:kvg * 4096 + (oi + 1) * 128], rhs=QT[ps, qc, oi * 128:(oi + 1) * 128], start=True, stop=True),
                               lambda: T.matmul(bank[sb_][:, 128:256], lhsT=KT[ps, kvg * 4096 + 2048 + oi * 128:kvg * 4096 + 2048 + (oi + 1) * 128], rhs=QT[ps, qc, oi * 128:(oi + 1) * 128], start=True, stop=True)]
                        ncol = 256
                        mk = smask[:, oi, :]
                        t_mk = t_smask
                    else:
                        m = a
                        ps = slice(m * 64, m * 64 + 64)
                        fns = [(lambda i=i: T.matmul(bank[sb_][:, i * 128:(i + 1) * 128], lhsT=KT[ps, (4 * c + i) * 128:(4 * c + i + 1) * 128],
                                                     rhs=QT[ps, 0, oi * 128:(oi + 1) * 128], start=True, stop=True)) for i in range(4)]
                        ncol = 512
                        mk = dmask[:, oi, :]
                        t_mk = t_dmask
                    P.group("pe", fns, reads=[t_KT, t_QT], writes=[tb[sb_]])
                    P.op("act", lambda: A.activation(out=PT[sb_][:, 0:ncol], in_=bank[sb_][:, 0:ncol], func=AF.Exp, scale=0.125), reads=[tb[sb_]], writes=[t_PT[sb_]])
                    if last:
                        P.op("pool", lambda: G_.tensor_tensor(out=PT[sb_][:, 0:ncol], in0=PT[sb_][:, 0:ncol], in1=mk, op=ALU.mult), reads=[t_PT[sb_], t_mk], writes=[t_PT[sb_]])

                def pv(n):
                    oi, a, c, last = items[n]
                    sb_ = n % 3
                    if swa:
                        hh = a; kvg = hh // 4
                        ob = 3 + (oi % 2) * 2 + (hh // 4)
                        oc = (hh % 4) * 65
                        fns = [lambda: T.matmul(bank[ob][:, oc:oc + 65], lhsT=PT[sb_][:, 0:128], rhs=Vb[:, oi * 130 + kvg * 65: oi * 130 + kvg * 65 + 65], start=True, stop=False),
                               lambda: T.matmul(bank[ob][:, oc:oc + 65], lhsT=PT[sb_][:, 128:256], rhs=Vb[:, (16 + oi) * 130 + kvg * 65: (16 + oi) * 130 + kvg * 65 + 65], start=False, stop=True)]
                    else:
                        m = a
                        ob = 3 + (oi % 2) * 2 + m
                        fns = [(lambda i=i: T.matmul(bank[ob][:, 0:129], lhsT=PT[sb_][:, i * 128:(i + 1) * 128], rhs=Vb[:, (4 * c + i) * 129:(4 * c + i + 1) * 129],
                                                     start=(c == 0 and i == 0), stop=(last and i == 3))) for i in range(4)]
                    P.group("pe", fns, reads=[t_PT[sb_], t_V], writes=[tb[ob]])
                    if swa and a == 7:
                        for hh in range(8):
                            ob2 = 3 + (oi % 2) * 2 + (hh // 4)
                            oc2 = (hh % 4) * 65
                            P.op("dve", lambda hh=hh, ob2=ob2, oc2=oc2: V.tensor_tensor(out=fsm[:, hh:hh + 1], in0=bank[ob2][:, oc2 + 64:oc2 + 65], in1=expsink[:, hh:hh + 1], op=ALU.add),
                                 reads=[tb[ob2], t_small], writes=[t_fsm])
                        P.op("dve", lambda: V.reciprocal(out=fsm[:, 0:8], in_=fsm[:, 0:8]), reads=[t_fsm], writes=[t_fsm])
                        for hh in range(8):
                            ob2 = 3 + (oi % 2) * 2 + (hh // 4)
                            oc2 = (hh % 4) * 65
                            P.op("dve", lambda hh=hh, ob2=ob2, oc2=oc2: V.tensor_scalar(out=mixed[:, oi, hh * 64:(hh + 1) * 64], in0=bank[ob2][:, oc2:oc2 + 64],
                                                                                         scalar1=fsm[:, hh:hh + 1], scalar2=None, op0=ALU.mult),
                                 reads=[tb[ob2], t_fsm], writes=[t_mixed[oi]])
                    if (not swa) and a == 1 and last:
                        h = ui - 1
                        o0 = bank[3 + (oi % 2) * 2]
                        o1 = bank[3 + (oi % 2) * 2 + 1]
                        t0, t1 = tb[3 + (oi % 2) * 2], tb[3 + (oi % 2) * 2 + 1]
                        P.op("dve", lambda: V.reciprocal(out=fsm[:, 16:17], in_=o0[:, 128:129]), reads=[t0], writes=[t_fsm])
                        P.op("dve", lambda: V.reciprocal(out=fsm[:, 17:18], in_=o1[:, 128:129]), reads=[t1], writes=[t_fsm])
                        P.op("dve", lambda: V.tensor_tensor(out=fsm[:, 17:18], in0=fsm[:, 17:18], in1=neglam, op=ALU.mult), reads=[t_fsm, t_small], writes=[t_fsm])
                        P.op("dve", lambda: V.tensor_scalar(out=fin[:, 0:128], in0=o1[:, 0:128], scalar1=fsm[:, 17:18], scalar2=None, op0=ALU.mult), reads=[t1, t_fsm], writes=[t_fin])
                        P.op("dve", lambda: V.scalar_tensor_tensor(out=fin[:, 128:256], in0=o0[:, 0:128], scalar=fsm[:, 16:17], in1=fin[:, 0:128], op0=ALU.mult, op1=ALU.add),
                             reads=[t0, t_fsm, t_fin], writes=[t_fin])
                        P.op("act", lambda: A.activation(out=junk2[:], in_=fin[:, 128:256], func=AF.Square, accum_out=fsm[:, 18:19]), reads=[t_fin], writes=[t_fsm])
                        P.op("dve", lambda: V.tensor_scalar(out=fsm[:, 18:19], in0=fsm[:, 18:19], scalar1=1.0 / 128, scalar2=EPS, op0=ALU.mult, op1=ALU.add), reads=[t_fsm], writes=[t_fsm])
                        P.op("act", lambda: A.activation(out=fsm[:, 18:19], in_=fsm[:, 18:19], func=AF.Sqrt), reads=[t_fsm], writes=[t_fsm])
                        P.op("dve", lambda: V.reciprocal(out=fsm[:, 18:19], in_=fsm[:, 18:19]), reads=[t_fsm], writes=[t_fsm])
                        P.op("dve", lambda: V.scalar_tensor_tensor(out=mixed[:, oi, 512 + h * 128:512 + (h + 1) * 128], in0=fin[:, 128:256], scalar=fsm[:, 18:19], in1=gsub_b[:],
                                                                   op0=ALU.mult, op1=ALU.mult), reads=[t_fin, t_fsm, t_gsub], writes=[t_mixed[oi]])

                LAG = 2
                for n in range(len(items) + LAG):
                    if n < len(items):
                        qk(n)
                    if n >= LAG:
                        pv(n - LAG)
            P.barrier()
            P.emit()

        with ExitStack() as s3:
            gt1_b = sbuf(s3, "gt1_b", [128, D])
            A2b = sbuf(s3, "A2b", [128, D]); S2b = sbuf(s3, "S2b", [128, D]); t_m2 = Tr()
            P.dma("sp", lambda: nc.sync.dma_start(out=gt1_b[:], in_=mod_d[0:1, 2 * D:3 * D].partition_broadcast(128)), writes=[t_mod])
            P.dma("sp", lambda: nc.sync.dma_start(out=S2b[:], in_=mod_d[0:1, 3 * D:4 * D].partition_broadcast(128)), writes=[t_m2])
            P.dma("sp", lambda: nc.sync.dma_start(out=A2b[:], in_=mod_d[0:1, 4 * D:5 * D].partition_broadcast(128)), writes=[t_m2])
            wout = sbuf(s3, "wout", [128, 8, D], BF16); t_wout = Tr()
            boutb = sbuf(s3, "boutb", [1, D], BF16)
            wr = sbuf(s3, "wr", [128, 8, NE], BF16); t_wr = Tr()
            brb = sbuf(s3, "brb", [1, NE], BF16)
            gfb = sbuf(s3, "gfb", [128, D]); t_gfb = Tr()
            P.dma("sp", lambda: nc.sync.dma_start(out=gfb[:], in_=gffn_d[0:1, :].partition_broadcast(128)), writes=[t_gfb])
            P.op("dve", lambda: V.scalar_tensor_tensor(out=A2b[:], in0=A2b[:], scalar=1.0, in1=gfb[:], op0=ALU.add, op1=ALU.mult), reads=[t_m2, t_gfb], writes=[t_m2])
            P.op("dve", lambda: V.memset(cnt_run[:], 0.0), writes=[t_cnt])
            mixT = [sbuf(s3, "mixT%d" % i, [128, 8, 128], BF16) for i in range(2)]; t_mixT = trs(2)
            xo = [sbuf(s3, "xo%d" % i, [128, D]) for i in range(2)]; t_xo = trs(2)
            x1t = [sbuf(s3, "x1t%d" % i, [128, D]) for i in range(2)]; t_x1t = trs(2)
            h2f = [sbuf(s3, "h2f%d" % i, [128, D]) for i in range(2)]; t_h2f = trs(2)
            h2tok = [sbuf(s3, "h2tok%d" % i, [128, D], BF16) for i in range(2)]; t_h2tok = trs(2)
            h2Tt = [sbuf(s3, "h2Tt%d" % i, [128, 8, 128], BF16) for i in range(2)]; t_h2Tt = trs(2)
            zrow = sbuf(s3, "zrow", [128, D], BF16); t_zrow = Tr()
            junk3 = sbuf(s3, "junk3", [128, D], BF16); t_junk3 = Tr()
            rs = sbuf(s3, "rs", [128, 64]); t_rs = trs(2)
            lg = sbuf(s3, "lg", [128, 2, 4 * NE]); t_lg = trs(2)
            idx8 = sbuf(s3, "idx8", [128, 2, 8], U32)
            posb = sbuf(s3, "posb", [128, 2, NE]); junkp = sbuf(s3, "junkp", [128, 2, NE])
            wout_v = wout_d.rearrange("(k p) n -> p k n", p=128)
            wr_v = wr_d.rearrange("(k p) n -> p k n", p=128)
            P.dma("pool", lambda: G_.dma_start(out=wout[:], in_=wout_v), writes=[t_wout])
            P.dma("pool", lambda: G_.dma_start(out=boutb[:], in_=bout_d[:, :]), writes=[t_wout])
            P.dma("pool", lambda: G_.dma_start(out=wr[:], in_=wr_v), writes=[t_wr])
            P.dma("pool", lambda: G_.dma_start(out=brb[:], in_=br_d[:, :]), writes=[t_wr])
            P.op("pool", lambda: G_.memset(zrow[:], 0.0), writes=[t_zrow])

            def p3(oi):
                b = oi % 2
                P.dma("sp", lambda: nc.sync.dma_start(out=xo[b][:], in_=x_d[S + oi * 128:S + (oi + 1) * 128, :]), writes=[t_xo[b]])
                pT = bank[b][:, :].bitcast(BF16)
                P.group("pe", [(lambda kc=kc: T.transpose(out=pT[:, kc * 128:(kc + 1) * 128], in_=mixed[:, oi, kc * 128:(kc + 1) * 128], identity=identb[:])) for kc in range(8)],
                        reads=[t_mixed[oi], t_cst], writes=[tb[b]])
                P.op("act", lambda: A.activation(out=mixT[b][:].rearrange("p a b -> p (a b)"), in_=pT[:, :], func=AF.Copy), reads=[tb[b]], writes=[t_mixT[b]])
                for hf in range(2):
                    bk = 2 + 2 * b + hf
                    fns = [(lambda kc=kc, hf=hf, bk=bk: T.matmul(bank[bk][:, :], lhsT=mixT[b][:, kc, :], rhs=wout[:, kc, hf * 512:(hf + 1) * 512], start=(kc == 0), stop=False)) for kc in range(8)]
                    fns.append(lambda hf=hf, bk=bk: T.matmul(bank[bk][:, :], lhsT=onesb[0:1, :], rhs=boutb[0:1, hf * 512:(hf + 1) * 512], start=False, stop=True))
                    P.group("pe", fns, reads=[t_mixT[b], t_wout, t_cst], writes=[tb[bk]])
                    P.op("dve", lambda hf=hf, bk=bk: V.tensor_tensor(out=x1t[b][:, hf * 512:(hf + 1) * 512], in0=bank[bk][:, :], in1=gt1_b[:, hf * 512:(hf + 1) * 512], op=ALU.mult),
                         reads=[tb[bk], t_mod], writes=[t_x1t[b]])
                P.op("pool", lambda: G_.tensor_tensor(out=x1t[b][:], in0=x1t[b][:], in1=xo[b][:], op=ALU.add), reads=[t_x1t[b], t_xo[b]], writes=[t_x1t[b]])
                P.dma("sp", lambda: nc.sync.dma_start(out=x1_d[oi * 128:(oi + 1) * 128, :], in_=x1t[b][:]), reads=[t_x1t[b]])
                r0 = 32 * b
                P.op("act", lambda: A.activation(out=junk3[:], in_=x1t[b][:], func=AF.Square, accum_out=rs[:, r0:r0 + 1]), reads=[t_x1t[b]], writes=[t_junk3, t_rs[b]])
                P.op("dve", lambda: V.tensor_scalar(out=rs[:, r0 + 1:r0 + 2], in0=rs[:, r0:r0 + 1], scalar1=1.0 / D, scalar2=EPS, op0=ALU.mult, op1=ALU.add), reads=[t_rs[b]], writes=[t_rs[b]])
                P.op("act", lambda: A.activation(out=rs[:, r0 + 1:r0 + 2], in_=rs[:, r0 + 1:r0 + 2], func=AF.Sqrt), reads=[t_rs[b]], writes=[t_rs[b]])
                P.op("dve", lambda: V.reciprocal(out=rs[:, r0 + 1:r0 + 2], in_=rs[:, r0 + 1:r0 + 2]), reads=[t_rs[b]], writes=[t_rs[b]])
                P.op("dve", lambda: V.scalar_tensor_tensor(out=h2f[b][:], in0=x1t[b][:], scalar=rs[:, r0 + 1:r0 + 2], in1=A2b[:], op0=ALU.mult, op1=ALU.mult),
                     reads=[t_x1t[b], t_rs[b], t_m2], writes=[t_h2f[b]])
                P.op("pool", lambda: G_.tensor_tensor(out=h2tok[b][:], in0=h2f[b][:], in1=S2b[:], op=ALU.add), reads=[t_h2f[b], t_m2], writes=[t_h2tok[b]])
                bk = 6 + b
                pT2 = bank[bk][:, :].bitcast(BF16)
                P.group("pe", [(lambda kc=kc: T.transpose(out=pT2[:, kc * 128:(kc + 1) * 128], in_=h2tok[b][:, kc * 128:(kc + 1) * 128], identity=identb[:])) for kc in range(8)],
                        reads=[t_h2tok[b], t_cst], writes=[tb[bk]])
                P.op("act", lambda: A.activation(out=h2Tt[b][:].rearrange("p a b -> p (a b)"), in_=pT2[:, :], func=AF.Copy), reads=[tb[bk]], writes=[t_h2Tt[b]])
                fns = [(lambda kc=kc: T.matmul(bank[b][:, 0:NE], lhsT=h2Tt[b][:, kc, :], rhs=wr[:, kc, :], start=(kc == 0), stop=False)) for kc in range(8)]
                fns.append(lambda: T.matmul(bank[b][:, 0:NE], lhsT=onesb[0:1, :], rhs=brb[0:1, :], start=False, stop=True))
                P.group("pe", fns, reads=[t_h2Tt[b], t_wr, t_cst], writes=[tb[b]])
                L0, L1, L2, L3 = lg[:, b, 0:NE], lg[:, b, NE:NE + 8], lg[:, b, 2 * NE:3 * NE], lg[:, b, 3 * NE:3 * NE + 8]
                P.op("dve", lambda: V.tensor_copy(out=L0, in_=bank[b][:, 0:NE]), reads=[tb[b]], writes=[t_lg[b]])
                P.op("dve", lambda: V.max(out=L1, in_=L0), reads=[t_lg[b]], writes=[t_lg[b]])
                P.op("dve", lambda: V.max_index(out=idx8[:, b, :], in_max=L1, in_values=L0), reads=[t_lg[b]], writes=[t_lg[b]])
                P.op("dve", lambda: V.tensor_scalar(out=maskb[:, oi, :], in0=L0, scalar1=lg[:, b, NE + 3:NE + 4], scalar2=None, op0=ALU.is_ge), reads=[t_lg[b]], writes=[t_maskb[oi]])
                P.op("dve", lambda: V.tensor_scalar(out=rs[:, r0 + 2:r0 + 3], in0=lg[:, b, NE:NE + 1], scalar1=-1.0, scalar2=None, op0=ALU.mult), reads=[t_lg[b]], writes=[t_rs[b]])
                P.op("act", lambda: A.activation(out=L3[:, 0:4], in_=L1[:, 0:4], func=AF.Exp, bias=rs[:, r0 + 2:r0 + 3], scale=1.0, accum_out=rs[:, r0 + 3:r0 + 4]),
                     reads=[t_lg[b], t_rs[b]], writes=[t_lg[b], t_rs[b]])
                P.op("dve", lambda: V.reciprocal(out=rs[:, r0 + 3:r0 + 4], in_=rs[:, r0 + 3:r0 + 4]), reads=[t_rs[b]], writes=[t_rs[b]])
                P.op("dve", lambda: V.tensor_scalar(out=gate4[:, 4 * oi:4 * oi + 4], in0=L3[:, 0:4], scalar1=rs[:, r0 + 3:r0 + 4], scalar2=None, op0=ALU.mult),
                     reads=[t_lg[b], t_rs[b]], writes=[t_gate4[oi]])
                pb = bank[b]
                P.group("pe", [lambda: T.matmul(pb[:, 64:64 + NE], lhsT=trib[:], rhs=maskb[:, oi, :], start=True, stop=True),
                               lambda: T.matmul(pb[:, 128:128 + NE], lhsT=ones128b[:], rhs=maskb[:, oi, :], start=True, stop=True)],
                        reads=[t_maskb[oi], t_cst, t_lg[b]], writes=[tb[b]])
                P.op("dve", lambda: V.tensor_tensor(out=posb[:, b, :], in0=pb[:, 64:64 + NE], in1=cnt_run[:], op=ALU.add), reads=[tb[b], t_cnt], writes=[t_lg[b]])
                P.op("dve", lambda: V.tensor_tensor(out=cnt_run[:], in0=pb[:, 128:128 + NE], in1=cnt_run[:], op=ALU.add), reads=[tb[b], t_cnt, t_lg[b]], writes=[t_cnt])
                EK = rs[:, r0 + 8:r0 + 12]; PK = rs[:, r0 + 12:r0 + 16]; DF = rs[:, r0 + 16:r0 + 20]
                P.op("dve", lambda: V.tensor_copy(out=EK, in_=idx8[:, b, 0:4]), reads=[t_lg[b]], writes=[t_rs[b]])
                for k in range(4):
                    P.op("dve", lambda k=k: V.scalar_tensor_tensor(out=junkp[:, b, :], in0=iota32, scalar=rs[:, r0 + 8 + k:r0 + 9 + k], in1=posb[:, b, :],
                                                                   op0=ALU.is_equal, op1=ALU.mult, accum_out=rs[:, r0 + 12 + k:r0 + 13 + k]),
                         reads=[t_lg[b], t_rs[b], t_cst], writes=[t_rs[b]])
                P.op("dve", lambda: V.scalar_tensor_tensor(out=DF, in0=EK, scalar=float(CAP), in1=PK, op0=ALU.mult, op1=ALU.add), reads=[t_rs[b]], writes=[t_rs[b]])
                P.op("dve", lambda: V.tensor_copy(out=dest_i[:, 4 * oi:4 * oi + 4], in_=DF), reads=[t_rs[b]], writes=[t_dest[oi]])
                for k in range(4):
                    P.dma("pool", lambda k=k: G_.indirect_dma_start(out=xbuf_d[:, :], out_offset=bass.IndirectOffsetOnAxis(ap=dest_i[:, 4 * oi + k:4 * oi + k + 1], axis=0),
                                                                    in_=h2tok[b][:, :], in_offset=None),
                          reads=[t_h2tok[b], t_dest[oi]], writes=[t_xbuf])
            for oi in range(NOWN):
                p3(oi)
            cf = rs[0:1, 0:NE]
            P.op("dve", lambda: V.tensor_scalar(out=cf, in0=cnt_run[0:1, :], scalar1=127.0, scalar2=1.0 / 128, op0=ALU.add, op1=ALU.mult), reads=[t_cnt] + t_rs, writes=t_rs)
            P.op("dve", lambda: V.tensor_scalar(out=cf, in0=cf, scalar1=-0.496, scalar2=None, op0=ALU.add), reads=t_rs, writes=t_rs)
            P.op("dve", lambda: V.tensor_copy(out=cnt_i[:], in_=cf), reads=t_rs, writes=[t_cnti])
            P.op("dve", lambda: V.tensor_scalar(out=posb[:, 0, :], in0=cnt_run[:], scalar1=iota_p, scalar2=None, op0=ALU.add), reads=[t_cnt, t_cst] + t_lg, writes=t_lg)
            P.op("dve", lambda: V.tensor_scalar(out=posb[:, 1, :], in0=posb[:, 0, :], scalar1=float(CAP), scalar2=None, op0=ALU.is_ge), reads=t_lg, writes=t_lg)
            P.op("dve", lambda: V.tensor_tensor(out=posb[:, 0, :], in0=posb[:, 0, :], in1=e2048, op=ALU.add), reads=t_lg + [t_cst], writes=t_lg)
            P.op("dve", lambda: V.tensor_scalar(out=rs[:, 40:41], in0=iota_p, scalar1=float(NE * CAP), scalar2=None, op0=ALU.add), reads=[t_cst] + t_rs, writes=t_rs)
            P.op("dve", lambda: V.tensor_scalar(out=junkp[:, 0, :], in0=posb[:, 0, :], scalar1=-1.0, scalar2=rs[:, 40:41], op0=ALU.mult, op1=ALU.add), reads=t_lg + t_rs, writes=t_lg)
            P.op("dve", lambda: V.tensor_tensor(out=junkp[:, 0, :], in0=junkp[:, 0, :], in1=posb[:, 1, :], op=ALU.mult), reads=t_lg, writes=t_lg)
            P.op("dve", lambda: V.tensor_tensor(out=posb[:, 0, :], in0=posb[:, 0, :], in1=junkp[:, 0, :], op=ALU.add), reads=t_lg, writes=t_lg)
            P.op("dve", lambda: V.tensor_copy(out=padidx[:], in_=posb[:, 0, :]), reads=t_lg, writes=[t_pad])
            for e in range(NE):
                P.dma("pool", lambda e=e: G_.indirect_dma_start(out=xbuf_d[:, :], out_offset=bass.IndirectOffsetOnAxis(ap=padidx[:, e:e + 1], axis=0),
                                                                in_=zrow[:, :], in_offset=None),
                      reads=[t_zrow, t_pad], writes=[t_xbuf])
            P.barrier()
            P.emit()

        sA.close()
        if int(os.environ.get('K_STOP', '9')) <= 3:
            return nc
        with ExitStack() as s4:
            w1b = [sbuf(s4, "w1b%d" % i, [128, 8, 2 * D], BF16) for i in range(2)]
            w2b = [sbuf(s4, "w2b%d" % i, [128, 8, D], BF16) for i in range(2)]
            b1r = [sbuf(s4, "b1r%d" % i, [1, 2 * D], BF16) for i in range(2)]
            b2r = [sbuf(s4, "b2r%d" % i, [1, D], BF16) for i in range(2)]
            t_w = trs(2)
            stg = [sbuf(s4, "stg%d" % i, [128, 8, 512]) for i in range(3)]; t_stg = trs(3)
            stc = [0]
            Xtok = [sbuf(s4, "Xtok%d" % i, [128, D], BF16) for i in range(2)]; t_Xtok = trs(2)
            XT = [sbuf(s4, "XT%d" % i, [128, 8, 128], BF16) for i in range(2)]; t_XT = trs(2)
            gg = [sbuf(s4, "gg%d" % i, [128, 256]) for i in range(2)]; t_gg = trs(2)
            sg = [sbuf(s4, "sg%d" % i, [128, 256]) for i in range(2)]; t_sg = trs(2)
            ll = [sbuf(s4, "ll%d" % i, [128, 256]) for i in range(2)]; t_ll = trs(2)
            atok = [sbuf(s4, "atok%d" % i, [128, D], BF16) for i in range(2)]; t_atok = trs(2)
            aT = [sbuf(s4, "aT%d" % i, [128, 8, 128], BF16) for i in range(2)]; t_aT = trs(2)
            yt = [sbuf(s4, "yt%d" % i, [128, D]) for i in range(2)]; t_yt = trs(2)
            t_ybuf = Tr()
            w1_v = w1_d.rearrange("e (k p) n -> e p k n", p=128)
            w2_v = w2_d.rearrange("e (k p) n -> e p k n", p=128)
            blk = [0]

            def block_body(e, bslot, ws):
                n = blk[0]
                blk[0] += 1
                xb = n % 2
                row0 = e * CAP + bslot * 128
                P.dma("act", lambda: nc.scalar.dma_start(out=Xtok[xb][:], in_=xbuf_d[row0:row0 + 128, :]), reads=[t_xbuf], writes=[t_Xtok[xb]], slot=xb)
                pT = bank[xb][:, :].bitcast(BF16)
                P.group("pe", [(lambda kc=kc: T.transpose(out=pT[:, kc * 128:(kc + 1) * 128], in_=Xtok[xb][:, kc * 128:(kc + 1) * 128], identity=identb[:])) for kc in range(8)],
                        reads=[t_Xtok[xb], t_cst], writes=[tb[xb]])
                P.op("act", lambda: A.activation(out=XT[xb][:].rearrange("p a b -> p (a b)"), in_=pT[:, :], func=AF.Copy), reads=[tb[xb]], writes=[t_XT[xb]])
                def do_cch(cch):
                    bk = 2 + (cch % 2)
                    par = cch % 2
                    fns = [(lambda kc=kc: T.matmul(bank[bk][:, :], lhsT=XT[xb][:, kc, :], rhs=w1b[ws][:, kc, cch * 512:(cch + 1) * 512], start=(kc == 0), stop=False)) for kc in range(8)]
                    fns.append(lambda: T.matmul(bank[bk][:, :], lhsT=onesb[0:1, :], rhs=b1r[ws][0:1, cch * 512:(cch + 1) * 512], start=False, stop=True))
                    P.group("pe", fns, reads=[t_XT[xb], t_w[ws], t_cst], writes=[tb[bk]])
                    P.op("dve", lambda: V.tensor_scalar(out=gg[par][:], in0=bank[bk][:, 0:512:2], scalar1=7.0, scalar2=None, op0=ALU.min), reads=[tb[bk]], writes=[t_gg[par]])
                    P.op("act", lambda: A.activation(out=sg[par][:], in_=gg[par][:], func=AF.Gelu_apprx_sigmoid), reads=[t_gg[par]], writes=[t_sg[par]])
                    P.op("dve", lambda: V.tensor_scalar(out=ll[par][:], in0=bank[bk][:, 1:512:2], scalar1=7.0, scalar2=-7.0, op0=ALU.min, op1=ALU.max), reads=[tb[bk]], writes=[t_ll[par]])
                    P.op("dve", lambda: V.scalar_tensor_tensor(out=atok[xb][:, cch * 256:(cch + 1) * 256], in0=ll[par][:], scalar=1.0, in1=sg[par][:], op0=ALU.add, op1=ALU.mult),
                         reads=[t_ll[par], t_sg[par]], writes=[t_atok[xb]])
                for cch in range(4):
                    do_cch(cch)
                bk = 4 + xb
                pT2 = bank[bk][:, :].bitcast(BF16)
                P.group("pe", [(lambda j=j: T.transpose(out=pT2[:, j * 128:(j + 1) * 128], in_=atok[xb][:, j * 128:(j + 1) * 128], identity=identb[:])) for j in range(8)],
                        reads=[t_atok[xb], t_cst], writes=[tb[bk]])
                P.op("act", lambda: A.activation(out=aT[xb][:].rearrange("p a b -> p (a b)"), in_=pT2[:, :], func=AF.Copy), reads=[tb[bk]], writes=[t_aT[xb]])
                def do_hf(hf):
                    bk2 = 6 + hf
                    fns = [(lambda j=j: T.matmul(bank[bk2][:, :], lhsT=aT[xb][:, j, :], rhs=w2b[ws][:, j, hf * 512:(hf + 1) * 512], start=(j == 0), stop=False)) for j in range(8)]
                    fns.append(lambda: T.matmul(bank[bk2][:, :], lhsT=onesb[0:1, :], rhs=b2r[ws][0:1, hf * 512:(hf + 1) * 512], start=False, stop=True))
                    P.group("pe", fns, reads=[t_aT[xb], t_w[ws], t_cst], writes=[tb[bk2]])
                    if hf == 0:
                        P.op("act", lambda: A.activation(out=yt[xb][:, 0:512], in_=bank[bk2][:, :], func=AF.Copy), reads=[tb[bk2]], writes=[t_yt[xb]])
                    else:
                        P.op("dve", lambda: V.tensor_copy(out=yt[xb][:, 512:1024], in_=bank[bk2][:, :]), reads=[tb[bk2]], writes=[t_yt[xb]])
                for hf in range(2):
                    do_hf(hf)
                P.dma("act", lambda: nc.scalar.dma_start(out=ybuf_d[row0:row0 + 128, :], in_=yt[xb][:]), reads=[t_yt[xb]], writes=[t_ybuf], slot=2 + xb)

            def load_expert(e):
                ws = e % 2
                for pc in range(6):
                    sl = stc[0] % 3
                    stc[0] += 1
                    if pc < 4:
                        src = w1_v[e, :, :, pc * 512:(pc + 1) * 512]
                        dst = w1b[ws][:, :, pc * 512:(pc + 1) * 512]
                    else:
                        src = w2_v[e, :, :, (pc - 4) * 512:(pc - 3) * 512]
                        dst = w2b[ws][:, :, (pc - 4) * 512:(pc - 3) * 512]
                    P.dma("sp", lambda src=src, sl=sl: nc.sync.dma_start(out=stg[sl][:], in_=src), writes=[t_stg[sl]])
                    P.op("pool", lambda dst=dst, sl=sl: G_.tensor_copy(out=dst, in_=stg[sl][:]), reads=[t_stg[sl]], writes=[t_w[ws]])
                P.dma("pool", lambda: G_.dma_start(out=b1r[ws][:], in_=b1_d[e:e + 1, :]), writes=[t_w[ws]])
                P.dma("pool", lambda: G_.dma_start(out=b2r[ws][:], in_=b2_d[e:e + 1, :]), writes=[t_w[ws]])

            NEX = int(os.environ.get('K_NE', NE))
            load_expert(0)
            for e in range(NEX):
                ws = e % 2
                if e + 1 < NEX:
                    load_expert(e + 1)
                P.regload(cnt_i[0:1, e:e + 1], reads=[t_cnti])
                for bslot in range(int(os.environ.get('K_NB', CAP // 128))):
                    P.cond_begin(bslot + 1)
                    block_body(e, bslot, ws)
                    P.cond_end()
            P.barrier()
            P.emit()

        if int(os.environ.get('K_STOP', '9')) <= 4:
            return nc
        with ExitStack() as s5:
            gt2_b = sbuf(s5, "gt2_b", [128, D]); gfin_b = sbuf(s5, "gfin_b", [128, D]); t_g5 = Tr()
            yk = [sbuf(s5, "yk%d" % i, [128, D]) for i in range(4)]; t_yk = trs(4)
            acc = [sbuf(s5, "acc%d" % i, [128, D]) for i in range(2)]; t_acc = trs(2)
            x1b = [sbuf(s5, "x1b%d" % i, [128, D]) for i in range(2)]; t_x1b = trs(2)
            ob_ = [sbuf(s5, "ob%d" % i, [128, D]) for i in range(2)]; t_ob = trs(2)
            junk5 = sbuf(s5, "junk5", [128, D], BF16); t_junk5 = Tr()
            fs5 = sbuf(s5, "fs5", [128, 8]); t_fs5 = trs(2)
            P.dma("sp", lambda: nc.sync.dma_start(out=gt2_b[:], in_=mod_d[0:1, 5 * D:6 * D].partition_broadcast(128)), writes=[t_g5])
            P.dma("sp", lambda: nc.sync.dma_start(out=gfin_b[:], in_=gfin_d[0:1, :].partition_broadcast(128)), writes=[t_g5])

            def p5(oi):
                b = oi % 2
                P.dma("sp", lambda: nc.sync.dma_start(out=x1b[b][:], in_=x1_d[oi * 128:(oi + 1) * 128, :]), writes=[t_x1b[b]])
                for k in range(4):
                    P.dma("pool", lambda k=k: G_.indirect_dma_start(out=yk[k][:, :], out_offset=None, in_=ybuf_d[:, :],
                                                                    in_offset=bass.IndirectOffsetOnAxis(ap=dest_i[:, 4 * oi + k:4 * oi + k + 1], axis=0),
                                                                    ), reads=[t_dest[oi]], writes=[t_yk[k]])
                    if k == 0:
                        P.op("dve", lambda: V.tensor_scalar(out=acc[b][:], in0=yk[0][:], scalar1=gate4[:, 4 * oi:4 * oi + 1], scalar2=None, op0=ALU.mult),
                             reads=[t_yk[0], t_gate4[oi]], writes=[t_acc[b]])
                    else:
                        P.op("dve", lambda k=k: V.scalar_tensor_tensor(out=acc[b][:], in0=yk[k][:], scalar=gate4[:, 4 * oi + k:4 * oi + k + 1], in1=acc[b][:], op0=ALU.mult, op1=ALU.add),
                             reads=[t_yk[k], t_gate4[oi], t_acc[b]], writes=[t_acc[b]])
                P.op("pool", lambda: G_.tensor_tensor(out=acc[b][:], in0=acc[b][:], in1=gt2_b[:], op=ALU.mult), reads=[t_acc[b], t_g5], writes=[t_acc[b]])
                P.op("pool", lambda: G_.tensor_tensor(out=x1b[b][:], in0=acc[b][:], in1=x1b[b][:], op=ALU.add), reads=[t_acc[b], t_x1b[b]], writes=[t_x1b[b]])
                P.op("act", lambda: A.activation(out=junk5[:], in_=x1b[b][:], func=AF.Square, accum_out=fs5[:, 4 * b:4 * b + 1]), reads=[t_x1b[b]], writes=[t_junk5, t_fs5[b]])
                P.op("dve", lambda: V.tensor_scalar(out=fs5[:, 4 * b + 1:4 * b + 2], in0=fs5[:, 4 * b:4 * b + 1], scalar1=1.0 / D, scalar2=EPS, op0=ALU.mult, op1=ALU.add),
                     reads=[t_fs5[b]], writes=[t_fs5[b]])
                P.op("act", lambda: A.activation(out=fs5[:, 4 * b + 1:4 * b + 2], in_=fs5[:, 4 * b + 1:4 * b + 2], func=AF.Sqrt), reads=[t_fs5[b]], writes=[t_fs5[b]])
                P.op("dve", lambda: V.reciprocal(out=fs5[:, 4 * b + 1:4 * b + 2], in_=fs5[:, 4 * b + 1:4 * b + 2]), reads=[t_fs5[b]], writes=[t_fs5[b]])
                P.op("dve", lambda: V.scalar_tensor_tensor(out=ob_[b][:], in0=x1b[b][:], scalar=fs5[:, 4 * b + 1:4 * b + 2], in1=gfin_b[:], op0=ALU.mult, op1=ALU.mult),
                     reads=[t_x1b[b], t_fs5[b], t_g5], writes=[t_ob[b]])
                P.dma("sp", lambda: nc.sync.dma_start(out=out_d[oi * 128:(oi + 1) * 128, :], in_=ob_[b][:]), reads=[t_ob[b]])
            for oi in range(NOWN):
                p5(oi)
            P.barrier()
            P.emit()
    return nc


def _consts():
    c = np.zeros((128, NCST), np.float32)
    c[:, 0:128] = np.eye(128, dtype=np.float32)
    k = np.arange(128)[:, None]
    q = np.arange(128)[None, :]
    c[:, 128:256] = (k <= q)
    c[:, 256:384] = (k > q)
    inv = (1.0 / (np.float32(10000.0) ** (np.arange(0, 64, 2, dtype=np.float32) / np.float32(64)))).astype(np.float32)
    p = np.arange(128)
    c[:, 384] = inv[p % 32]
    c[:, 385] = np.where((p % 64) < 32, -1.0, 1.0)
    c[:, 386] = np.float32(math.pi / 2)
    c[:, 387] = 0.0
    c[:, 388] = 1.0
    c[:, 392:520] = 1.0
    c[:, 520:648] = (k < q)
    c[:, 648:680] = np.arange(32)[None, :]
    c[:, 680:712] = (np.arange(32) * CAP)[None, :]
    c[:, 712] = np.arange(128)
    return c


def _core_masks(j):
    own = own_blocks(j)
    k = np.arange(128)[:, None]
    q = np.arange(128)[None, :]
    tri = (k <= q).astype(np.float32)
    low = (k > q).astype(np.float32)
    dm = np.zeros((128, NOWN, 4, 128), np.float32)
    sm = np.zeros((128, NOWN, 2, 128), np.float32)
    for oi, gb in enumerate(own):
        nkb = 8 * (oi // 2) + (4 if oi % 2 == 0 else 8)
        for i in range(4):
            kb = nkb - 4 + i
            if kb < gb:
                dm[:, oi, i, :] = 1.0
            elif kb == gb:
                dm[:, oi, i, :] = tri
        sm[:, oi, 1, :] = tri
        if gb > 0:
            sm[:, oi, 0, :] = low
    return dm.reshape(128, NOWN * 512), sm.reshape(128, NOWN * 256)


_NC_CACHE = {}


def kernel(x, c, positions, w_ada, b_ada, g_mix, w_in, b_in, attn_sinks, lambda_q1, lambda_k1, lambda_q2, lambda_k2,
           g_subln, w_out, b_out, g_ffn, w_router, b_router, w1, b1, w2, b2, g_final):
    f = lambda a: np.ascontiguousarray(np.asarray(a))
    x = f(x); positions = f(positions)
    if "nc" not in _NC_CACHE:
        _NC_CACHE["nc"] = build_program()
    nc = _NC_CACHE["nc"]
    colT = lambda v: f(np.asarray(v).reshape(-1, 128).T)
    w_sel = f(np.asarray(w_in)[0][:, SEL])
    b_sel = f(np.asarray(b_in)[0][SEL])
    b1_ = np.asarray(b1)[0]
    shared = {
        "w_ada": f(np.asarray(w_ada)[0]), "b_ada": f(np.asarray(b_ada)[0][None, :]),
        "gmixT": colT(np.asarray(g_mix)[0]), "gffnT": colT(np.asarray(g_ffn)[0]),
        "w_sel": w_sel, "b_selT": colT(b_sel), "b_sel": f(b_sel[None, :]),
        "sinks": f(np.asarray(attn_sinks)[0][None, :]),
        "lam4": f(np.stack([np.asarray(lambda_q1)[0], np.asarray(lambda_k1)[0], np.asarray(lambda_q2)[0], np.asarray(lambda_k2)[0]])),
        "g_subln": f(np.asarray(g_subln)[0][None, :]),
        "w_out": f(np.asarray(w_out)[0]), "b_out": f(np.asarray(b_out)[0][None, :]),
        "w_router": f(np.asarray(w_router)[0]), "b_router": f(np.asarray(b_router)[0][None, :]),
        "w1": f(np.asarray(w1)[0]), "w2": f(np.asarray(w2)[0]), "b2": f(np.asarray(b2)[0]),
        "b1": f(b1_), "g_ffn": f(np.asarray(g_ffn)[0][None, :]),
        "g_final": f(np.asarray(g_final)[None, :]),
        "consts": _consts(),
    }
    in_maps = []
    rows_all = []
    for core in range(8):
        b, j = core // 4, core % 4
        own = own_blocks(j)
        rows_own = np.concatenate([np.arange(g * 128, (g + 1) * 128) for g in own])
        rows_prev = np.concatenate([np.arange(max(g - 1, 0) * 128, (max(g - 1, 0) + 1) * 128) for g in own])
        rows_all.append(rows_own)
        xb = x[b]
        x_ext = np.concatenate([xb, xb[rows_own], xb[rows_prev]], axis=0)
        pb = positions[b]
        pos_ext = np.concatenate([pb, pb[rows_own], pb[rows_prev]])[None, :].astype(np.int32)
        dm, sm = _core_masks(j)
        m = dict(shared)
        m.update({"x": f(x_ext), "pos": f(pos_ext), "cT": colT(np.asarray(c)[b]), "dmask": dm, "smask": sm})
        in_maps.append(m)
    res = run_bass_kernel_spmd(nc, in_maps, core_ids=list(range(8)))
    out = np.zeros((2, S, D), np.float32)
    for core in range(8):
        out[core // 4, rows_all[core], :] = np.asarray(res.results[core]["out"])
    return out
```

```python
import math
import os
from contextlib import ExitStack

import numpy as np
import concourse.bass as bass
import concourse.mybir as mybir
from concourse.bass_utils import run_bass_kernel_spmd

F32 = mybir.dt.float32
BF16 = mybir.dt.bfloat16
I32 = mybir.dt.int32
ALU = mybir.AluOpType
AF = mybir.ActivationFunctionType
AX = mybir.AxisListType

D = 1024
S = 8192
NT = 64
NG = 16
NOWN = 16
NE = 32
SX = S + 2 * NOWN * 128
NGX = SX // 512
NCST = 720
CAP = 2048
U32 = mybir.dt.uint32
EPS = 1e-5
C1 = 6.28125
C2 = 2 * math.pi - 6.28125
INV2PI = float(1.0 / (2 * math.pi))

OFF_QA, OFF_KA, OFF_VA, OFF_QD, OFF_KD, OFF_VD = 0, 512, 640, 768, 1280, 1792


def _swap64(cols):
    cols = np.asarray(cols).reshape(-1, 64)
    return np.concatenate([cols[:, 32:], cols[:, :32]], axis=1).reshape(-1)


def _unit_cols():
    units = []
    k = np.concatenate([np.tile(np.arange(OFF_KA + g * 64, OFF_KA + (g + 1) * 64), 2) for g in range(2)])
    q = np.arange(OFF_QA, OFF_QA + 512)
    v = np.arange(OFF_VA, OFF_VA + 128)
    units.append(dict(nk=2, nq=4, k=k, q=q, v=v))
    for h in range(4):
        k = np.arange(OFF_KD + h * 128, OFF_KD + (h + 1) * 128)
        q = np.arange(OFF_QD + h * 128, OFF_QD + (h + 1) * 128)
        v = np.arange(OFF_VD + h * 128, OFF_VD + (h + 1) * 128)
        units.append(dict(nk=1, nq=1, k=k, q=q, v=v))
    off = 0
    sel = []
    for u in units:
        u["base"] = off
        parts = [u["k"], _swap64(u["k"]), u["q"], _swap64(u["q"]), u["v"]]
        u["o_k"] = 0
        u["o_ks"] = len(u["k"])
        u["o_q"] = u["o_ks"] + len(u["k"])
        u["o_qs"] = u["o_q"] + len(u["q"])
        u["o_v"] = u["o_qs"] + len(u["q"])
        u["ncols"] = u["o_v"] + 128
        sel.append(np.concatenate(parts))
        off += u["ncols"]
    return units, np.concatenate(sel)


UNITS, SEL = _unit_cols()
NSEL = len(SEL)
NCH = NSEL // 128


def own_blocks(j):
    return sorted([8 * m + j for m in range(8)] + [8 * m + 7 - j for m in range(8)])


class Tr:
    __slots__ = ("w", "r")

    def __init__(self):
        self.w = {}
        self.r = {}


def trs(n):
    return [Tr() for _ in range(n)]


class Prog:
    ENG = ("pe", "act", "dve", "pool", "sp")

    def __init__(self, nc, stack, n_dma_sems=48):
        self.nc = nc
        self.q = {e: [] for e in self.ENG}
        self.esem = {e: stack.enter_context(nc.semaphore("s_" + e)) for e in self.ENG}
        self.ecnt = {e: 0 for e in self.ENG}
        self.waited = {e: {} for e in self.ENG}
        self.dsem = [stack.enter_context(nc.semaphore("d%d" % i)) for i in range(n_dma_sems)]
        self.dcnt = [0] * n_dma_sems
        self.dpool = {"sp": list(range(0, n_dma_sems - 24)), "act": list(range(n_dma_sems - 24, n_dma_sems - 16)),
                      "pool": list(range(n_dma_sems - 16, n_dma_sems))}
        self.dnext = {"sp": 0, "pool": 0, "act": 0}
        self.in_cond = False
        self.handles = {"pe": nc.tensor, "act": nc.scalar, "dve": nc.vector, "pool": nc.gpsimd, "sp": nc.sync}

    def _need(self, eng, s, v):
        wd = self.waited[eng]
        if wd.get(s, 0) >= v:
            return
        wd[s] = v
        self.q[eng].append(("wait", s, v))

    def _waits(self, eng, reads, writes):
        need = {}
        for t in reads:
            for s, v in t.w.items():
                if need.get(s, 0) < v:
                    need[s] = v
        for t in writes:
            for s, v in t.w.items():
                if need.get(s, 0) < v:
                    need[s] = v
            for s, v in t.r.items():
                if need.get(s, 0) < v:
                    need[s] = v
        for s, v in need.items():
            if eng == "pe" and s is self.esem["pe"]:
                continue
            self._need(eng, s, v)

    def _record(self, ev, reads, writes):
        s, v = ev
        for t in reads:
            if t.r.get(s, 0) < v:
                t.r[s] = v
        for t in writes:
            if self.in_cond:
                if t.w.get(s, 0) < v:
                    t.w[s] = v
            else:
                t.w = {s: v}
                t.r = {}

    def op(self, eng, fn, reads=(), writes=()):
        self._waits(eng, reads, writes)
        self.ecnt[eng] += 1
        ev = (self.esem[eng], self.ecnt[eng])
        self.q[eng].append(("op", fn, self.esem[eng], 1))
        self._record(ev, reads, writes)

    def group(self, eng, fns, reads=(), writes=()):
        self._waits(eng, reads, writes)
        self.ecnt[eng] += 1
        ev = (self.esem[eng], self.ecnt[eng])
        for f in fns[:-1]:
            self.q[eng].append(("op", f, None, 0))
        self.q[eng].append(("op", fns[-1], self.esem[eng], 1))
        self._record(ev, reads, writes)

    def dma(self, eng, fn, reads=(), writes=(), slot=None):
        pl = self.dpool[eng]
        if slot is None:
            i = pl[self.dnext[eng]]
            self.dnext[eng] = (self.dnext[eng] + 1) % (len(pl) - 4)
        else:
            i = pl[len(pl) - 4 + slot]
        s = self.dsem[i]
        if self.dcnt[i]:
            self._need(eng, s, self.dcnt[i])
        self._waits(eng, reads, writes)
        self.dcnt[i] += 16
        ev = (s, self.dcnt[i])
        self.q[eng].append(("op", fn, s, 16))
        self._record(ev, reads, writes)
        return ev

    CENG = ("pe", "act", "dve")

    def regload(self, ap, reads=()):
        for e in self.CENG:
            self._waits(e, reads, ())
            self.q[e].append(("regload", ap))

    def cond_begin(self, thr):
        self._csnap = ({e: self.ecnt[e] for e in self.ENG}, list(self.dcnt), {e: dict(self.waited[e]) for e in self.ENG})
        self.in_cond = True
        for e in self.CENG:
            self.q[e].append(["if", thr, None])

    def cond_end(self):
        ec0, dc0, wd0 = self._csnap
        assert self.ecnt["pool"] == ec0["pool"] and self.ecnt["sp"] == ec0["sp"], "pool/sp must stay outside conditional regions"
        dd = [(i, self.dcnt[i] - dc0[i]) for i in range(len(self.dcnt)) if self.dcnt[i] != dc0[i]]
        for i, _ in dd:
            assert i in self.dpool["act"]
        for e in self.CENG:
            comp = []
            if self.ecnt[e] != ec0[e]:
                comp.append((self.esem[e], self.ecnt[e] - ec0[e]))
            if e == "act":
                comp += [(self.dsem[i], d, dc0[i]) for i, d in dd]
            for it in reversed(self.q[e]):
                if isinstance(it, list) and it[0] == "if" and it[2] is None:
                    it[2] = comp
                    break
            self.q[e].append(("endif",))
            self.waited[e] = wd0[e]
        self.waited["pool"] = wd0["pool"]
        self.waited["sp"] = wd0["sp"]
        self.in_cond = False

    def barrier(self):
        for e in self.ENG:
            for f in self.ENG:
                if f != e and self.ecnt[f]:
                    self._need(e, self.esem[f], self.ecnt[f])
            for i, s in enumerate(self.dsem):
                if self.dcnt[i]:
                    self._need(e, s, self.dcnt[i])

    def emit(self):
        nc = self.nc
        q = self.q
        self.q = {e: [] for e in self.ENG}
        if not hasattr(self, "regs"):
            self.regs = {}
        with nc.Block() as block:
            def run_items(h, ename, items):
                i = 0
                n = len(items)
                while i < n:
                    it = items[i]
                    k = it[0]
                    if k == "wait":
                        h.wait_ge(it[1], it[2])
                    elif k == "op":
                        ins = it[1]()
                        if it[2] is not None:
                            ins.then_inc(it[2], it[3])
                    elif k == "regload":
                        if ename not in self.regs:
                            self.regs[ename] = h.alloc_register("cnt_" + ename)
                        h.reg_load(self.regs[ename], it[1])
                    elif k == "if":
                        depth = 1
                        j = i + 1
                        while True:
                            if items[j][0] == "if":
                                depth += 1
                            elif items[j][0] == "endif":
                                depth -= 1
                                if depth == 0:
                                    break
                            j += 1
                        body = items[i + 1:j]
                        with h.If_lt(self.regs[ename], it[1]):
                            h.drain()
                            for cp in it[2]:
                                if len(cp) == 3 and cp[2]:
                                    h.wait_ge(cp[0], cp[2])
                                h.sem_inc(cp[0], cp[1])
                        with h.Else():
                            run_items(h, ename, body)
                        i = j
                    i += 1

            def run(ename):
                run_items(self.handles[ename], ename, q[ename])

            @block.tensor
            def _(e):
                run("pe")

            @block.scalar
            def _(e):
                run("act")

            @block.vector
            def _(e):
                run("dve")

            @block.gpsimd
            def _(e):
                run("pool")

            @block.sync
            def _(e):
                run("sp")


def build_program(j_core_unused=None, debug=False):
    nc = bass.Bass("TRN2", target_bir_lowering=False)
    din = lambda name, shape, dt=F32: nc.dram_tensor(name, list(shape), dt, kind="ExternalInput").ap()
    x_d = din("x", [SX, D])
    pos_d = din("pos", [1, SX], I32)
    dmask_d = din("dmask", [128, NOWN * 512])
    smask_d = din("smask", [128, NOWN * 256])
    cT_d = din("cT", [128, 8])
    wada_d = din("w_ada", [D, 6 * D])
    bada_d = din("b_ada", [1, 6 * D])
    gmixT_d = din("gmixT", [128, 8])
    gffnT_d = din("gffnT", [128, 8])
    wsel_d = din("w_sel", [D, NSEL])
    bselT_d = din("b_selT", [128, NCH])
    bsel_d = din("b_sel", [1, NSEL])
    sinks_d = din("sinks", [1, 8])
    lam_d = din("lam4", [4, 64])
    gsub_d = din("g_subln", [1, 128])
    wout_d = din("w_out", [D, D])
    bout_d = din("b_out", [1, D])
    wr_d = din("w_router", [D, NE])
    br_d = din("b_router", [1, NE])
    w1_d = din("w1", [NE, D, 2 * D])
    b1_d = din("b1", [NE, 2 * D])
    gffn_d = din("g_ffn", [1, D])
    w2_d = din("w2", [NE, D, D])
    b2_d = din("b2", [NE, D])
    gfin_d = din("g_final", [1, D])
    cst_d = din("consts", [128, NCST])
    out_d = nc.dram_tensor("out", [NOWN * 128, D], F32, kind="ExternalOutput").ap()
    hT_d = nc.dram_tensor("hT_scr", [8, 128, SX], BF16, kind="Internal").ap()
    cos_d = nc.dram_tensor("cos_scr", [128, SX], F32, kind="Internal").ap()
    sin_d = nc.dram_tensor("sin_scr", [128, SX], F32, kind="Internal").ap()
    x1_d = nc.dram_tensor("x1_scr", [NOWN * 128, D], F32, kind="Internal").ap()
    mod_d = nc.dram_tensor("mod_scr", [1, 6 * D], F32, kind="Internal").ap()
    xbuf_d = nc.dram_tensor("xbuf_scr", [NE * CAP + 128, D], BF16, kind="Internal").ap()
    ybuf_d = nc.dram_tensor("ybuf_scr", [NE * CAP, D], F32, kind="Internal").ap()


    with ExitStack() as st:
        P = Prog(nc, st)
        sbuf = lambda stack, name, shape, dt=F32: stack.enter_context(nc.sbuf_tensor(name, list(shape), dt))
        V, A, T, G_ = nc.vector, nc.scalar, nc.tensor, nc.gpsimd

        bank = [st.enter_context(nc.psum_tensor("bank%d" % i, [128, 512], F32)) for i in range(8)]
        tb = trs(8)

        cst = sbuf(st, "cst", [128, NCST]); t_cst = Tr()
        identb = sbuf(st, "identb", [128, 128], BF16)
        mask256 = sbuf(st, "mask256", [128, 256], BF16)
        onesb = sbuf(st, "onesb", [1, 128], BF16)
        trib = sbuf(st, "trib", [128, 128], BF16)
        ones128b = sbuf(st, "ones128b", [128, 128], BF16)
        A1 = sbuf(st, "A1", [128, 8]); S1 = sbuf(st, "S1", [128, 8])
        A2 = sbuf(st, "A2", [128, 8]); S2 = sbuf(st, "S2", [128, 8])
        t_mod = Tr()
        t_mixed = trs(NOWN)
        small = sbuf(st, "small", [128, 64]); t_small = Tr()
        ident = cst[:, 0:128]
        invf = cst[:, 384:385]
        sgn = cst[:, 385:386]
        halfpi = cst[:, 386:387]
        zero_c = cst[:, 387:388]
        one11 = cst[0:1, 388:389]
        ones_row = cst[0:1, 392:520]
        neglam = small[:, 0:1]
        expsink = small[:, 8:16]

        P.dma("sp", lambda: nc.sync.dma_start(out=cst[:], in_=cst_d[:, :]), writes=[t_cst])
        P.op("dve", lambda: V.tensor_copy(out=identb[:], in_=cst[:, 0:128]), reads=[t_cst], writes=[t_cst])
        P.op("dve", lambda: V.tensor_copy(out=mask256[:, 0:128], in_=cst[:, 256:384]), reads=[t_cst], writes=[t_cst])
        P.op("dve", lambda: V.tensor_copy(out=mask256[:, 128:256], in_=cst[:, 128:256]), reads=[t_cst], writes=[t_cst])
        P.op("dve", lambda: V.tensor_copy(out=onesb[:], in_=cst[0:1, 392:520]), reads=[t_cst], writes=[t_cst])
        P.op("dve", lambda: V.tensor_copy(out=trib[:], in_=cst[:, 520:648]), reads=[t_cst], writes=[t_cst])
        P.op("dve", lambda: V.tensor_copy(out=ones128b[:], in_=cst[:, 392:520]), reads=[t_cst], writes=[t_cst])

        with ExitStack() as s0:
            cT = sbuf(s0, "cT_sb", [128, 8]); t_cT = Tr()
            wad = [sbuf(s0, "wad%d" % i, [128, 8, 512]) for i in range(2)]; t_wad = trs(2)
            modrow = sbuf(s0, "modrow", [1, 6 * D]); t_modrow = Tr()
            badar = sbuf(s0, "badar", [1, 6 * D]); t_bada = Tr()
            gT = sbuf(s0, "gT", [128, 16]); t_gT = Tr()
            lamb = sbuf(s0, "lamb", [128, 256]); t_lam = Tr()
            lamp = sbuf(s0, "lamp", [128, 128])
            P.dma("sp", lambda: nc.sync.dma_start(out=cT[:], in_=cT_d[:, :]), writes=[t_cT])
            P.dma("sp", lambda: nc.sync.dma_start(out=badar[:], in_=bada_d[:, :]), writes=[t_bada])
            P.dma("sp", lambda: nc.sync.dma_start(out=gT[:, 0:8], in_=gmixT_d[:, :]), writes=[t_gT])
            P.dma("sp", lambda: nc.sync.dma_start(out=gT[:, 8:16], in_=gffnT_d[:, :]), writes=[t_gT])
            P.dma("sp", lambda: nc.sync.dma_start(out=lamb[:].rearrange("p (a b) -> p a b", a=4),
                                                  in_=lam_d[:, :].partition_broadcast(128)), writes=[t_lam])
            P.dma("sp", lambda: nc.sync.dma_start(out=small[:, 16:24], in_=sinks_d[0:1, :].partition_broadcast(128)), writes=[t_small])
            P.op("act", lambda: A.activation(out=cT[:], in_=cT[:], func=AF.Silu), reads=[t_cT], writes=[t_cT])
            wada_v = wada_d.rearrange("(k p) n -> p k n", p=128)
            for pc in range(12):
                b = pc % 2
                P.dma("sp", lambda pc=pc, b=b: nc.sync.dma_start(out=wad[b][:], in_=wada_v[:, :, pc * 512:(pc + 1) * 512]), writes=[t_wad[b]])
                bk = pc % 2
                P.group("pe", [(lambda kc=kc, b=b, bk=bk: T.matmul(bank[bk][0:1, :], lhsT=cT[:, kc:kc + 1], rhs=wad[b][:, kc, :],
                                                                    start=(kc == 0), stop=(kc == 7))) for kc in range(8)],
                        reads=[t_cT, t_wad[b]], writes=[tb[bk]])
                P.op("dve", lambda pc=pc, bk=bk: V.tensor_tensor(out=modrow[0:1, pc * 512:(pc + 1) * 512], in0=bank[bk][0:1, :],
                                                                 in1=badar[0:1, pc * 512:(pc + 1) * 512], op=ALU.add),
                     reads=[tb[bk], t_bada], writes=[t_modrow])
            cols = [(0, 0), (1, 8), (3, 16), (4, 24)]
            fns = []
            for mi, dc in cols:
                for kc in range(8):
                    fns.append(lambda mi=mi, dc=dc, kc=kc: T.matmul(bank[2][:, dc + kc:dc + kc + 1],
                                                                    lhsT=modrow[0:1, mi * D + kc * 128: mi * D + (kc + 1) * 128],
                                                                    rhs=one11, start=True, stop=True))
            P.group("pe", fns, reads=[t_modrow, t_cst], writes=[tb[2]])
            P.op("dve", lambda: V.tensor_copy(out=S1[:], in_=bank[2][:, 0:8]), reads=[tb[2]], writes=[t_mod])
            P.op("dve", lambda: V.scalar_tensor_tensor(out=A1[:], in0=bank[2][:, 8:16], scalar=1.0, in1=gT[:, 0:8], op0=ALU.add, op1=ALU.mult),
                 reads=[tb[2], t_gT], writes=[t_mod])
            P.op("dve", lambda: V.tensor_copy(out=S2[:], in_=bank[2][:, 16:24]), reads=[tb[2]], writes=[t_mod])
            P.op("dve", lambda: V.scalar_tensor_tensor(out=A2[:], in0=bank[2][:, 24:32], scalar=1.0, in1=gT[:, 8:16], op0=ALU.add, op1=ALU.mult),
                 reads=[tb[2], t_gT], writes=[t_mod])
            P.dma("sp", lambda: nc.sync.dma_start(out=mod_d[:, :], in_=modrow[:]), reads=[t_modrow])
            P.op("dve", lambda: V.tensor_tensor(out=lamp[:, 0:64], in0=lamb[:, 0:64], in1=lamb[:, 64:128], op=ALU.mult), reads=[t_lam], writes=[t_lam])
            P.op("dve", lambda: V.tensor_tensor(out=lamp[:, 64:128], in0=lamb[:, 128:192], in1=lamb[:, 192:256], op=ALU.mult), reads=[t_lam], writes=[t_lam])
            P.op("dve", lambda: V.tensor_reduce(out=small[:, 1:3], in_=lamp[:].rearrange("p (a b) -> p a b", a=2), axis=AX.X, op=ALU.add),
                 reads=[t_lam], writes=[t_small])
            P.op("act", lambda: A.activation(out=small[:, 1:3], in_=small[:, 1:3], func=AF.Exp), reads=[t_small], writes=[t_small])
            P.op("dve", lambda: V.scalar_tensor_tensor(out=small[:, 0:1], in0=small[:, 2:3], scalar=-0.2, in1=small[:, 1:2], op0=ALU.add, op1=ALU.subtract),
                 reads=[t_small], writes=[t_small])
            P.op("act", lambda: A.activation(out=small[:, 8:16], in_=small[:, 16:24], func=AF.Exp), reads=[t_small], writes=[t_small])
            P.barrier()
            P.emit()

        with ExitStack() as s1:
            xt = [sbuf(s1, "xt%d" % i, [128, D]) for i in range(3)]; t_xt = trs(3)
            xn = [sbuf(s1, "xn%d" % i, [128, D], BF16) for i in range(2)]; t_xn = trs(2)
            junk = sbuf(s1, "junk", [128, D], BF16); t_junk = Tr()
            ssq = sbuf(s1, "ssq", [128, 8]); t_ssq = trs(4)
            hTg = [sbuf(s1, "hTg%d" % i, [128, 8, 512], BF16) for i in range(2)]; t_hTg = trs(2)
            posi = sbuf(s1, "posi", [128, 512], I32); t_posi = Tr()
            ang = sbuf(s1, "ang", [128, 512]); t_ang = Tr()
            ki = sbuf(s1, "ki", [128, 512], I32); kf = sbuf(s1, "kf", [128, 512]); t_k = Tr()
            rr = sbuf(s1, "rr", [128, 512]); t_rr = Tr()
            tab = [sbuf(s1, "tab%d" % i, [128, 512]) for i in range(4)]; t_tab = trs(4)
            hT_v = hT_d.rearrange("k p t -> p k t")
            tcount = 0
            for g in range(NGX):
                hb = g % 2
                for tt in range(4):
                    t = 4 * g + tt
                    xb = tcount % 3
                    nb = tcount % 2
                    sq = tcount % 4
                    tcount += 1
                    P.dma("sp", lambda t=t, xb=xb: nc.sync.dma_start(out=xt[xb][:], in_=x_d[t * 128:(t + 1) * 128, :]), writes=[t_xt[xb]])
                    P.op("act", lambda xb=xb, sq=sq: A.activation(out=junk[:], in_=xt[xb][:], func=AF.Square, accum_out=ssq[:, 2 * sq:2 * sq + 1]),
                         reads=[t_xt[xb]], writes=[t_junk, t_ssq[sq]])
                    P.op("dve", lambda sq=sq: V.tensor_scalar(out=ssq[:, 2 * sq + 1:2 * sq + 2], in0=ssq[:, 2 * sq:2 * sq + 1], scalar1=1.0 / D, scalar2=EPS,
                                                              op0=ALU.mult, op1=ALU.add), reads=[t_ssq[sq]], writes=[t_ssq[sq]])
                    P.op("act", lambda sq=sq: A.activation(out=ssq[:, 2 * sq + 1:2 * sq + 2], in_=ssq[:, 2 * sq + 1:2 * sq + 2], func=AF.Sqrt),
                         reads=[t_ssq[sq]], writes=[t_ssq[sq]])
                    P.op("dve", lambda sq=sq: V.reciprocal(out=ssq[:, 2 * sq + 1:2 * sq + 2], in_=ssq[:, 2 * sq + 1:2 * sq + 2]),
                         reads=[t_ssq[sq]], writes=[t_ssq[sq]])
                    P.op("dve", lambda xb=xb, nb=nb, sq=sq: V.tensor_scalar(out=xn[nb][:], in0=xt[xb][:], scalar1=ssq[:, 2 * sq + 1:2 * sq + 2], scalar2=None, op0=ALU.mult),
                         reads=[t_xt[xb], t_ssq[sq]], writes=[t_xn[nb]])
                    bk = nb
                    pT = bank[bk][:, :].bitcast(BF16)
                    P.group("pe", [(lambda kc=kc, nb=nb, pT=pT: T.transpose(out=pT[:, kc * 128:(kc + 1) * 128], in_=xn[nb][:, kc * 128:(kc + 1) * 128], identity=identb[:]))
                                   for kc in range(8)], reads=[t_xn[nb], t_cst], writes=[tb[bk]])
                    for kc in range(8):
                        if kc % 2 == 0:
                            P.op("act", lambda kc=kc, hb=hb, tt=tt, pT=pT: A.activation(out=hTg[hb][:, kc, tt * 128:(tt + 1) * 128], in_=pT[:, kc * 128:(kc + 1) * 128],
                                                                                      func=AF.Identity, scale=A1[:, kc:kc + 1], bias=S1[:, kc:kc + 1]),
                                 reads=[tb[bk], t_mod], writes=[t_hTg[hb]])
                        else:
                            P.op("dve", lambda kc=kc, hb=hb, tt=tt, pT=pT: V.tensor_scalar(out=hTg[hb][:, kc, tt * 128:(tt + 1) * 128], in0=pT[:, kc * 128:(kc + 1) * 128],
                                                                                         scalar1=A1[:, kc:kc + 1], scalar2=S1[:, kc:kc + 1], op0=ALU.mult, op1=ALU.add),
                                 reads=[tb[bk], t_mod], writes=[t_hTg[hb]])
                P.dma("sp", lambda g=g, hb=hb: nc.sync.dma_start(out=hT_v[:, :, g * 512:(g + 1) * 512], in_=hTg[hb][:]), reads=[t_hTg[hb]])
                P.dma("sp", lambda g=g: nc.sync.dma_start(out=posi[:], in_=pos_d[0:1, g * 512:(g + 1) * 512].partition_broadcast(128)), writes=[t_posi])
                P.op("dve", lambda: V.tensor_copy(out=ang[:], in_=posi[:]), reads=[t_posi], writes=[t_ang])
                P.op("dve", lambda: V.tensor_scalar(out=ang[:], in0=ang[:], scalar1=invf, scalar2=None, op0=ALU.mult), reads=[t_ang, t_cst], writes=[t_ang])
                for which in range(2):
                    tbi = (2 * g + which) % 4
                    if which == 0:
                        P.op("dve", lambda: V.tensor_scalar(out=ki[:], in0=ang[:], scalar1=INV2PI, scalar2=None, op0=ALU.mult), reads=[t_ang], writes=[t_k])
                    else:
                        P.op("dve", lambda: V.tensor_scalar(out=ki[:], in0=ang[:], scalar1=INV2PI, scalar2=0.25, op0=ALU.mult, op1=ALU.add), reads=[t_ang], writes=[t_k])
                    P.op("dve", lambda: V.tensor_copy(out=kf[:], in_=ki[:]), reads=[t_k], writes=[t_k])
                    P.op("dve", lambda: V.scalar_tensor_tensor(out=rr[:], in0=kf[:], scalar=-C1, in1=ang[:], op0=ALU.mult, op1=ALU.add), reads=[t_k, t_ang], writes=[t_rr])
                    P.op("dve", lambda: V.scalar_tensor_tensor(out=rr[:], in0=kf[:], scalar=-C2, in1=rr[:], op0=ALU.mult, op1=ALU.add), reads=[t_k, t_rr], writes=[t_rr])
                    if which == 0:
                        P.op("dve", lambda: V.tensor_scalar(out=rr[:], in0=rr[:], scalar1=-3.1415925, scalar2=3.1415925, op0=ALU.max, op1=ALU.min), reads=[t_rr], writes=[t_rr])
                    else:
                        P.op("dve", lambda: V.tensor_scalar(out=rr[:], in0=rr[:], scalar1=-4.712388, scalar2=1.570796, op0=ALU.max, op1=ALU.min), reads=[t_rr], writes=[t_rr])
                    if which == 0:
                        P.op("act", lambda tbi=tbi: A.activation(out=tab[tbi][:], in_=rr[:], func=AF.Sin, scale=sgn, bias=zero_c), reads=[t_rr, t_cst], writes=[t_tab[tbi]])
                        P.dma("sp", lambda g=g, tbi=tbi: nc.sync.dma_start(out=sin_d[:, g * 512:(g + 1) * 512], in_=tab[tbi][:]), reads=[t_tab[tbi]])
                    else:
                        P.op("act", lambda tbi=tbi: A.activation(out=tab[tbi][:], in_=rr[:], func=AF.Sin, scale=1.0, bias=halfpi), reads=[t_rr, t_cst], writes=[t_tab[tbi]])
                        P.dma("sp", lambda g=g, tbi=tbi: nc.sync.dma_start(out=cos_d[:, g * 512:(g + 1) * 512], in_=tab[tbi][:]), reads=[t_tab[tbi]])
            P.barrier()
            P.emit()

        s34 = st.enter_context(ExitStack())
        dest_i = sbuf(s34, "dest_i", [128, 4 * NOWN], I32); t_dest = trs(NOWN)
        gate4 = sbuf(s34, "gate4", [128, 4 * NOWN]); t_gate4 = trs(NOWN)
        maskb = sbuf(s34, "maskb", [128, NOWN, NE], BF16); t_maskb = trs(NOWN)
        cnt_run = sbuf(s34, "cnt_run", [128, NE]); t_cnt = Tr()
        cnt_i = sbuf(s34, "cnt_i", [1, NE], I32); t_cnti = Tr()
        padidx = sbuf(s34, "padidx", [128, NE], I32); t_pad = Tr()
        t_xbuf = Tr()
        iota32 = cst[:, 648:680]
        e2048 = cst[:, 680:712]
        iota_p = cst[:, 712:713]
        sA = ExitStack()
        bufA = sbuf(sA, "bufA", [128, 16 * 1024], BF16)
        mixed = bufA[:].rearrange("p (a b) -> p a b", a=NOWN)
        with ExitStack() as s2:
            Wu = sbuf(s2, "Wu", [128, 8, 1664], BF16); t_Wu = Tr()
            KT = sbuf(s2, "KT", [128, S], BF16); t_KT = Tr()
            Vb = sbuf(s2, "Vb", [128, 64 * 130], BF16); t_V = Tr()
            QT = sbuf(s2, "QT", [128, 4, NOWN * 128], BF16); t_QT = Tr()
            hTg = [sbuf(s2, "hTg2_%d" % i, [128, 8, 512], BF16) for i in range(2)]; t_hTg = trs(2)
            csg = [sbuf(s2, "csg%d" % i, [128, 2, 512]) for i in range(2)]; t_csg = trs(2)
            tm1 = [sbuf(s2, "tm1_%d" % i, [128, 512]) for i in range(2)]; t_tm1 = trs(2)
            tm2 = [sbuf(s2, "tm2_%d" % i, [128, 512]) for i in range(2)]; t_tm2 = trs(2)
            PT = [sbuf(s2, "PT%d" % i, [128, 512], BF16) for i in range(3)]; t_PT = trs(3)
            dmask = sbuf(s2, "dmask_sb", [128, NOWN, 512], BF16); t_dmask = Tr()
            smask = sbuf(s2, "smask_sb", [128, NOWN, 256], BF16); t_smask = Tr()
            bselT = sbuf(s2, "bselT", [128, NCH]); t_bsel = Tr()
            vbias = sbuf(s2, "vbias", [128, 128]); t_vbias = Tr()
            gsub_b = sbuf(s2, "gsub_b", [128, 128]); t_gsub = Tr()
            fin = sbuf(s2, "fin", [128, 8 * 128]); t_fin = Tr()
            fsm = sbuf(s2, "fsm", [128, 32]); t_fsm = Tr()
            junk2 = sbuf(s2, "junk2", [128, 128], BF16)
            hT_v = hT_d.rearrange("k p t -> p k t")
            wsel_v = wsel_d.rearrange("(k p) n -> p k n", p=128)
            for q4 in range(4):
                P.dma("pool", lambda q4=q4: G_.dma_start(out=dmask[:, 4 * q4:4 * q4 + 4, :], in_=dmask_d[:, q4 * 2048:(q4 + 1) * 2048].rearrange("p (a b) -> p a b", a=4)),
                      writes=[t_dmask])
            for q4 in range(2):
                P.dma("pool", lambda q4=q4: G_.dma_start(out=smask[:, 8 * q4:8 * q4 + 8, :], in_=smask_d[:, q4 * 2048:(q4 + 1) * 2048].rearrange("p (a b) -> p a b", a=8)),
                      writes=[t_smask])
            P.dma("sp", lambda: nc.sync.dma_start(out=bselT[:], in_=bselT_d[:, :]), writes=[t_bsel])
            P.dma("sp", lambda: nc.sync.dma_start(out=gsub_b[:], in_=gsub_d[0:1, :].partition_broadcast(128)), writes=[t_gsub])
            P.op("dve", lambda: V.tensor_scalar(out=gsub_b[:], in0=gsub_b[:], scalar1=0.8, scalar2=None, op0=ALU.mult), reads=[t_gsub], writes=[t_gsub])
            gcount = [0]

            def rope_proj(u, wc, wcs, hb, cb, ccol, ncol, dst, t_dst, par):
                bA, bB = bank[2 * par], bank[2 * par + 1]
                ci = (u["base"] + wc) // 128
                cis = (u["base"] + wcs) // 128
                P.group("pe", [(lambda kc=kc: T.matmul(bA[:, 0:ncol], lhsT=Wu[:, kc, wc:wc + 128], rhs=hTg[hb][:, kc, ccol:ccol + ncol], start=(kc == 0), stop=(kc == 7)))
                               for kc in range(8)], reads=[t_Wu, t_hTg[hb]], writes=[tb[2 * par]])
                P.group("pe", [(lambda kc=kc: T.matmul(bB[:, 0:ncol], lhsT=Wu[:, kc, wcs:wcs + 128], rhs=hTg[hb][:, kc, ccol:ccol + ncol], start=(kc == 0), stop=(kc == 7)))
                               for kc in range(8)], reads=[t_Wu, t_hTg[hb]], writes=[tb[2 * par + 1]])
                P.op("dve", lambda: V.scalar_tensor_tensor(out=tm1[par][:, 0:ncol], in0=bA[:, 0:ncol], scalar=bselT[:, ci:ci + 1], in1=csg[cb][:, 0, ccol:ccol + ncol],
                                                           op0=ALU.add, op1=ALU.mult), reads=[tb[2 * par], t_bsel, t_csg[cb]], writes=[t_tm1[par]])
                P.op("dve", lambda: V.scalar_tensor_tensor(out=tm2[par][:, 0:ncol], in0=bB[:, 0:ncol], scalar=bselT[:, cis:cis + 1], in1=csg[cb][:, 1, ccol:ccol + ncol],
                                                           op0=ALU.add, op1=ALU.mult), reads=[tb[2 * par + 1], t_bsel, t_csg[cb]], writes=[t_tm2[par]])
                P.op("pool", lambda: G_.tensor_tensor(out=dst, in0=tm1[par][:, 0:ncol], in1=tm2[par][:, 0:ncol], op=ALU.add),
                     reads=[t_tm1[par], t_tm2[par]], writes=[t_dst])

            def load_group(g):
                hb = gcount[0] % 2
                gcount[0] += 1
                P.dma("sp", lambda: nc.sync.dma_start(out=hTg[hb][:], in_=hT_v[:, :, g * 512:(g + 1) * 512]), writes=[t_hTg[hb]])
                P.dma("sp", lambda: nc.sync.dma_start(out=csg[hb][:, 0, :], in_=cos_d[:, g * 512:(g + 1) * 512]), writes=[t_csg[hb]])
                P.dma("sp", lambda: nc.sync.dma_start(out=csg[hb][:, 1, :], in_=sin_d[:, g * 512:(g + 1) * 512]), writes=[t_csg[hb]])
                return hb

            pcount = [0]

            def v_proj(u, hb, vt0, vw, swa):
                bk = 4 + (pcount[0] % 2)
                pcount[0] += 1
                ov = u["o_v"]
                fns = []
                for tt in range(4):
                    for kc in range(8):
                        fns.append(lambda tt=tt, kc=kc: T.matmul(bank[bk][:, tt * 128:(tt + 1) * 128], lhsT=hTg[hb][:, kc, tt * 128:(tt + 1) * 128],
                                                                 rhs=Wu[:, kc, ov:ov + 128], start=(kc == 0), stop=(kc == 7)))
                P.group("pe", fns, reads=[t_Wu, t_hTg[hb]], writes=[tb[bk]])
                src = bank[bk][:, :].rearrange("p (a b) -> p a b", a=4)
                vb_b = vbias[:].unsqueeze(1).to_broadcast([128, 4, 128])
                if not swa:
                    dst = Vb[:, vt0 * 129:(vt0 + 4) * 129].rearrange("p (a b) -> p a b", a=4)[:, :, 0:128]
                    P.op("dve", lambda: V.tensor_tensor(out=dst, in0=src, in1=vb_b, op=ALU.add), reads=[tb[bk], t_vbias], writes=[t_V])
                else:
                    for kv in range(2):
                        dst = Vb[:, vt0 * 130:(vt0 + 4) * 130].rearrange("p (a b) -> p a b", a=4)[:, :, kv * 65:kv * 65 + 64]
                        P.op("dve", lambda dst=dst, kv=kv: V.tensor_tensor(out=dst, in0=src[:, :, kv * 64:(kv + 1) * 64],
                                                                          in1=vbias[:, kv * 64:(kv + 1) * 64].unsqueeze(1).to_broadcast([128, 4, 64]), op=ALU.add),
                             reads=[tb[bk], t_vbias], writes=[t_V])

            for ui, u in enumerate(UNITS):
                swa = (ui == 0)
                nc_u = u["ncols"]
                P.dma("pool", lambda u=u, nc_u=nc_u: G_.dma_start(out=Wu[:, :, 0:nc_u], in_=wsel_v[:, :, u["base"]:u["base"] + nc_u]), writes=[t_Wu])
                P.dma("sp", lambda u=u: nc.sync.dma_start(out=vbias[:], in_=bsel_d[0:1, u["base"] + u["o_v"]:u["base"] + u["o_v"] + 128].partition_broadcast(128)),
                      writes=[t_vbias])
                if swa:
                    vv = Vb[:, 0:32 * 130].rearrange("p (a b) -> p a b", a=32)
                    P.op("pool", lambda vv=vv: G_.memset(vv[:, :, 64:65], 1.0), writes=[t_V])
                    P.op("pool", lambda vv=vv: G_.memset(vv[:, :, 129:130], 1.0), writes=[t_V])
                    kv_groups = [(20 + i, i * 512, 4 * i) for i in range(4)] + [(16 + i, 2048 + i * 512, 16 + 4 * i) for i in range(4)]
                elif ui == 1:
                    vv = Vb[:, 0:64 * 129].rearrange("p (a b) -> p a b", a=64)
                    P.op("pool", lambda vv=vv: G_.memset(vv[:, :, 128:129], 1.0), writes=[t_V])
                    kv_groups = [(g, g * 512, 4 * g) for g in range(NG)]
                else:
                    kv_groups = [(g, g * 512, 4 * g) for g in range(NG)]
                par = 0
                for (g, kcol, vt0) in kv_groups:
                    hb = load_group(g)
                    for kc_ in range(u["nk"]):
                        rope_proj(u, u["o_k"] + kc_ * 128, u["o_ks"] + kc_ * 128, hb, hb, 0, 512, KT[:, kc_ * 4096 + kcol:kc_ * 4096 + kcol + 512], t_KT, par)
                        par ^= 1
                    v_proj(u, hb, vt0, None, swa)
                    if swa and g < 20:
                        for qc in range(4):
                            rope_proj(u, u["o_q"] + qc * 128, u["o_qs"] + qc * 128, hb, hb, 0, 512, QT[:, qc, (g - 16) * 512:(g - 15) * 512], t_QT, par)
                            par ^= 1
                if not swa:
                    for g in range(16, 20):
                        hb = load_group(g)
                        rope_proj(u, u["o_q"], u["o_qs"], hb, hb, 0, 512, QT[:, 0, (g - 16) * 512:(g - 15) * 512], t_QT, par)
                        par ^= 1

                items = []
                if swa:
                    for oi in range(NOWN):
                        for hh in range(8):
                            items.append((oi, hh, 0, True))
                else:
                    for oi in range(NOWN):
                        nkb = 8 * (oi // 2) + (4 if oi % 2 == 0 else 8)
                        for m in range(2):
                            for c in range(nkb // 4):
                                items.append((oi, m, c, c == nkb // 4 - 1))

                def qk(n):
                    oi, a, c, last = items[n]
                    sb_ = n % 3
                    if swa:
                        hh = a; half = hh % 2; qc = hh // 2; kvg = hh // 4
                        ps = slice(half * 64, half * 64 + 64)
                        fns = [lambda: T.matmul(bank[sb_][:, 0:128], lhsT=KT[ps, kvg * 4096 + oi * 128:kvg * 4096 + (oi + 1) * 128], rhs=QT[ps, qc, oi * 128:(oi + 1) * 128], start=True, stop=True),
                               lambda: T.matmul(bank[sb_][:, 128:256], lhsT=KT[ps, kvg * 4096 + 2048 + oi * 128:kvg * 4096 + 2048 + (oi + 1) * 128], rhs=QT[ps, qc, oi * 128:(oi + 1) * 128], start=True, stop=True)]
                        ncol = 256
                        mk = smask[:, oi, :]
                        t_mk = t_smask
                    else:
                        m = a
                        ps = slice(m * 64, m * 64 + 64)
                        fns = [(lambda i=i: T.matmul(bank[sb_][:, i * 128:(i + 1) * 128], lhsT=KT[ps, (4 * c + i) * 128:(4 * c + i + 1) * 128],
                                                     rhs=QT[ps, 0, oi * 128:(oi + 1) * 128], start=True, stop=True)) for i in range(4)]
                        ncol = 512
                        mk = dmask[:, oi, :]
                        t_mk = t_dmask
                    P.group("pe", fns, reads=[t_KT, t_QT], writes=[tb[sb_]])
                    P.op("act", lambda: A.activation(out=PT[sb_][:, 0:ncol], in_=bank[sb_][:, 0:ncol], func=AF.Exp, scale=0.125), reads=[tb[sb_]], writes=[t_PT[sb_]])
                    if last:
                        P.op("pool", lambda: G_.tensor_tensor(out=PT[sb_][:, 0:ncol], in0=PT[sb_][:, 0:ncol], in1=mk, op=ALU.mult), reads=[t_PT[sb_], t_mk], writes=[t_PT[sb_]])

                def pv(n):
                    oi, a, c, last = items[n]
                    sb_ = n % 3
                    if swa:
                        hh = a; kvg = hh // 4
                        ob = 3 + (oi % 2) * 2 + (hh // 4)
                        oc = (hh % 4) * 65
                        fns = [lambda: T.matmul(bank[ob][:, oc:oc + 65], lhsT=PT[sb_][:, 0:128], rhs=Vb[:, oi * 130 + kvg * 65: oi * 130 + kvg * 65 + 65], start=True, stop=False),
                               lambda: T.matmul(bank[ob][:, oc:oc + 65], lhsT=PT[sb_][:, 128:256], rhs=Vb[:, (16 + oi) * 130 + kvg * 65: (16 + oi) * 130 + kvg * 65 + 65], start=False, stop=True)]
                    else:
                        m = a
                        ob = 3 + (oi % 2) * 2 + m
                        fns = [(lambda i=i: T.matmul(bank[ob][:, 0:129], lhsT=PT[sb_][:, i * 128:(i + 1) * 128], rhs=Vb[:, (4 * c + i) * 129:(4 * c + i + 1) * 129],
                                                     start=(c == 0 and i == 0), stop=(last and i == 3))) for i in range(4)]
                    P.group("pe", fns, reads=[t_PT[sb_], t_V], writes=[tb[ob]])
                    if swa and a == 7:
                        for hh in range(8):
                            ob2 = 3 + (oi % 2) * 2 + (hh // 4)
                            oc2 = (hh % 4) * 65
                            P.op("dve", lambda hh=hh, ob2=ob2, oc2=oc2: V.tensor_tensor(out=fsm[:, hh:hh + 1], in0=bank[ob2][:, oc2 + 64:oc2 + 65], in1=expsink[:, hh:hh + 1], op=ALU.add),
                                 reads=[tb[ob2], t_small], writes=[t_fsm])
                        P.op("dve", lambda: V.reciprocal(out=fsm[:, 0:8], in_=fsm[:, 0:8]), reads=[t_fsm], writes=[t_fsm])
                        for hh in range(8):
                            ob2 = 3 + (oi % 2) * 2 + (hh // 4)
                            oc2 = (hh % 4) * 65
                            P.op("dve", lambda hh=hh, ob2=ob2, oc2=oc2: V.tensor_scalar(out=mixed[:, oi, hh * 64:(hh + 1) * 64], in0=bank[ob2][:, oc2:oc2 + 64],
                                                                                         scalar1=fsm[:, hh:hh + 1], scalar2=None, op0=ALU.mult),
                                 reads=[tb[ob2], t_fsm], writes=[t_mixed[oi]])
                    if (not swa) and a == 1 and last:
                        h = ui - 1
                        o0 = bank[3 + (oi % 2) * 2]
                        o1 = bank[3 + (oi % 2) * 2 + 1]
                        t0, t1 = tb[3 + (oi % 2) * 2], tb[3 + (oi % 2) * 2 + 1]
                        P.op("dve", lambda: V.reciprocal(out=fsm[:, 16:17], in_=o0[:, 128:129]), reads=[t0], writes=[t_fsm])
                        P.op("dve", lambda: V.reciprocal(out=fsm[:, 17:18], in_=o1[:, 128:129]), reads=[t1], writes=[t_fsm])
                        P.op("dve", lambda: V.tensor_tensor(out=fsm[:, 17:18], in0=fsm[:, 17:18], in1=neglam, op=ALU.mult), reads=[t_fsm, t_small], writes=[t_fsm])
                        P.op("dve", lambda: V.tensor_scalar(out=fin[:, 0:128], in0=o1[:, 0:128], scalar1=fsm[:, 17:18], scalar2=None, op0=ALU.mult), reads=[t1, t_fsm], writes=[t_fin])
                        P.op("dve", lambda: V.scalar_tensor_tensor(out=fin[:, 128:256], in0=o0[:, 0:128], scalar=fsm[:, 16:17], in1=fin[:, 0:128], op0=ALU.mult, op1=ALU.add),
                             reads=[t0, t_fsm, t_fin], writes=[t_fin])
                        P.op("act", lambda: A.activation(out=junk2[:], in_=fin[:, 128:256], func=AF.Square, accum_out=fsm[:, 18:19]), reads=[t_fin], writes=[t_fsm])
                        P.op("dve", lambda: V.tensor_scalar(out=fsm[:, 18:19], in0=fsm[:, 18:19], scalar1=1.0 / 128, scalar2=EPS, op0=ALU.mult, op1=ALU.add), reads=[t_fsm], writes=[t_fsm])
                        P.op("act", lambda: A.activation(out=fsm[:, 18:19], in_=fsm[:, 18:19], func=AF.Sqrt), reads=[t_fsm], writes=[t_fsm])
                        P.op("dve", lambda: V.reciprocal(out=fsm[:, 18:19], in_=fsm[:, 18:19]), reads=[t_fsm], writes=[t_fsm])
                        P.op("dve", lambda: V.scalar_tensor_tensor(out=mixed[:, oi, 512 + h * 128:512 + (h + 1) * 128], in0=fin[:, 128:256], scalar=fsm[:, 18:19], in1=gsub_b[:],
                                                                   op0=ALU.mult, op1=ALU.mult), reads=[t_fin, t_fsm, t_gsub], writes=[t_mixed[oi]])

                LAG = 2
                for n in range(len(items) + LAG):
                    if n < len(items):
                        qk(n)
                    if n >= LAG:
                        pv(n - LAG)
            P.barrier()
            P.emit()

        with ExitStack() as s3:
            gt1_b = sbuf(s3, "gt1_b", [128, D])
            A2b = sbuf(s3, "A2b", [128, D]); S2b = sbuf(s3, "S2b", [128, D]); t_m2 = Tr()
            P.dma("sp", lambda: nc.sync.dma_start(out=gt1_b[:], in_=mod_d[0:1, 2 * D:3 * D].partition_broadcast(128)), writes=[t_mod])
            P.dma("sp", lambda: nc.sync.dma_start(out=S2b[:], in_=mod_d[0:1, 3 * D:4 * D].partition_broadcast(128)), writes=[t_m2])
            P.dma("sp", lambda: nc.sync.dma_start(out=A2b[:], in_=mod_d[0:1, 4 * D:5 * D].partition_broadcast(128)), writes=[t_m2])
            wout = sbuf(s3, "wout", [128, 8, D], BF16); t_wout = Tr()
            boutb = sbuf(s3, "boutb", [1, D], BF16)
            wr = sbuf(s3, "wr", [128, 8, NE], BF16); t_wr = Tr()
            brb = sbuf(s3, "brb", [1, NE], BF16)
            gfb = sbuf(s3, "gfb", [128, D]); t_gfb = Tr()
            P.dma("sp", lambda: nc.sync.dma_start(out=gfb[:], in_=gffn_d[0:1, :].partition_broadcast(128)), writes=[t_gfb])
            P.op("dve", lambda: V.scalar_tensor_tensor(out=A2b[:], in0=A2b[:], scalar=1.0, in1=gfb[:], op0=ALU.add, op1=ALU.mult), reads=[t_m2, t_gfb], writes=[t_m2])
            P.op("dve", lambda: V.memset(cnt_run[:], 0.0), writes=[t_cnt])
            mixT = [sbuf(s3, "mixT%d" % i, [128, 8, 128], BF16) for i in range(2)]; t_mixT = trs(2)
            xo = [sbuf(s3, "xo%d" % i, [128, D]) for i in range(2)]; t_xo = trs(2)
            x1t = [sbuf(s3, "x1t%d" % i, [128, D]) for i in range(2)]; t_x1t = trs(2)
            h2f = [sbuf(s3, "h2f%d" % i, [128, D]) for i in range(2)]; t_h2f = trs(2)
            h2tok = [sbuf(s3, "h2tok%d" % i, [128, D], BF16) for i in range(2)]; t_h2tok = trs(2)
            h2Tt = [sbuf(s3, "h2Tt%d" % i, [128, 8, 128], BF16) for i in range(2)]; t_h2Tt = trs(2)
            zrow = sbuf(s3, "zrow", [128, D], BF16); t_zrow = Tr()
            junk3 = sbuf(s3, "junk3", [128, D], BF16); t_junk3 = Tr()
            rs = sbuf(s3, "rs", [128, 64]); t_rs = trs(2)
            lg = sbuf(s3, "lg", [128, 2, 4 * NE]); t_lg = trs(2)
            idx8 = sbuf(s3, "idx8", [128, 2, 8], U32)
            posb = sbuf(s3, "posb", [128, 2, NE]); junkp = sbuf(s3, "junkp", [128, 2, NE])
            wout_v = wout_d.rearrange("(k p) n -> p k n", p=128)
            wr_v = wr_d.rearrange("(k p) n -> p k n", p=128)
            P.dma("pool", lambda: G_.dma_start(out=wout[:], in_=wout_v), writes=[t_wout])
            P.dma("pool", lambda: G_.dma_start(out=boutb[:], in_=bout_d[:, :]), writes=[t_wout])
            P.dma("pool", lambda: G_.dma_start(out=wr[:], in_=wr_v), writes=[t_wr])
            P.dma("pool", lambda: G_.dma_start(out=brb[:], in_=br_d[:, :]), writes=[t_wr])
            P.op("pool", lambda: G_.memset(zrow[:], 0.0), writes=[t_zrow])

            def p3(oi):
                b = oi % 2
                P.dma("sp", lambda: nc.sync.dma_start(out=xo[b][:], in_=x_d[S + oi * 128:S + (oi + 1) * 128, :]), writes=[t_xo[b]])
                pT = bank[b][:, :].bitcast(BF16)
                P.group("pe", [(lambda kc=kc: T.transpose(out=pT[:, kc * 128:(kc + 1) * 128], in_=mixed[:, oi, kc * 128:(kc + 1) * 128], identity=identb[:])) for kc in range(8)],
                        reads=[t_mixed[oi], t_cst], writes=[tb[b]])
                P.op("act", lambda: A.activation(out=mixT[b][:].rearrange("p a b -> p (a b)"), in_=pT[:, :], func=AF.Copy), reads=[tb[b]], writes=[t_mixT[b]])
                for hf in range(2):
                    bk = 2 + 2 * b + hf
                    fns = [(lambda kc=kc, hf=hf, bk=bk: T.matmul(bank[bk][:, :], lhsT=mixT[b][:, kc, :], rhs=wout[:, kc, hf * 512:(hf + 1) * 512], start=(kc == 0), stop=False)) for kc in range(8)]
                    fns.append(lambda hf=hf, bk=bk: T.matmul(bank[bk][:, :], lhsT=onesb[0:1, :], rhs=boutb[0:1, hf * 512:(hf + 1) * 512], start=False, stop=True))
                    P.group("pe", fns, reads=[t_mixT[b], t_wout, t_cst], writes=[tb[bk]])
                    P.op("dve", lambda hf=hf, bk=bk: V.tensor_tensor(out=x1t[b][:, hf * 512:(hf + 1) * 512], in0=bank[bk][:, :], in1=gt1_b[:, hf * 512:(hf + 1) * 512], op=ALU.mult),
                         reads=[tb[bk], t_mod], writes=[t_x1t[b]])
                P.op("pool", lambda: G_.tensor_tensor(out=x1t[b][:], in0=x1t[b][:], in1=xo[b][:], op=ALU.add), reads=[t_x1t[b], t_xo[b]], writes=[t_x1t[b]])
                P.dma("sp", lambda: nc.sync.dma_start(out=x1_d[oi * 128:(oi + 1) * 128, :], in_=x1t[b][:]), reads=[t_x1t[b]])
                r0 = 32 * b
                P.op("act", lambda: A.activation(out=junk3[:], in_=x1t[b][:], func=AF.Square, accum_out=rs[:, r0:r0 + 1]), reads=[t_x1t[b]], writes=[t_junk3, t_rs[b]])
                P.op("dve", lambda: V.tensor_scalar(out=rs[:, r0 + 1:r0 + 2], in0=rs[:, r0:r0 + 1], scalar1=1.0 / D, scalar2=EPS, op0=ALU.mult, op1=ALU.add), reads=[t_rs[b]], writes=[t_rs[b]])
                P.op("act", lambda: A.activation(out=rs[:, r0 + 1:r0 + 2], in_=rs[:, r0 + 1:r0 + 2], func=AF.Sqrt), reads=[t_rs[b]], writes=[t_rs[b]])
                P.op("dve", lambda: V.reciprocal(out=rs[:, r0 + 1:r0 + 2], in_=rs[:, r0 + 1:r0 + 2]), reads=[t_rs[b]], writes=[t_rs[b]])
                P.op("dve", lambda: V.scalar_tensor_tensor(out=h2f[b][:], in0=x1t[b][:], scalar=rs[:, r0 + 1:r0 + 2], in1=A2b[:], op0=ALU.mult, op1=ALU.mult),
                     reads=[t_x1t[b], t_rs[b], t_m2], writes=[t_h2f[b]])
                P.op("pool", lambda: G_.tensor_tensor(out=h2tok[b][:], in0=h2f[b][:], in1=S2b[:], op=ALU.add), reads=[t_h2f[b], t_m2], writes=[t_h2tok[b]])
                bk = 6 + b
                pT2 = bank[bk][:, :].bitcast(BF16)
                P.group("pe", [(lambda kc=kc: T.transpose(out=pT2[:, kc * 128:(kc + 1) * 128], in_=h2tok[b][:, kc * 128:(kc + 1) * 128], identity=identb[:])) for kc in range(8)],
                        reads=[t_h2tok[b], t_cst], writes=[tb[bk]])
                P.op("act", lambda: A.activation(out=h2Tt[b][:].rearrange("p a b -> p (a b)"), in_=pT2[:, :], func=AF.Copy), reads=[tb[bk]], writes=[t_h2Tt[b]])
                fns = [(lambda kc=kc: T.matmul(bank[b][:, 0:NE], lhsT=h2Tt[b][:, kc, :], rhs=wr[:, kc, :], start=(kc == 0), stop=False)) for kc in range(8)]
                fns.append(lambda: T.matmul(bank[b][:, 0:NE], lhsT=onesb[0:1, :], rhs=brb[0:1, :], start=False, stop=True))
                P.group("pe", fns, reads=[t_h2Tt[b], t_wr, t_cst], writes=[tb[b]])
                L0, L1, L2, L3 = lg[:, b, 0:NE], lg[:, b, NE:NE + 8], lg[:, b, 2 * NE:3 * NE], lg[:, b, 3 * NE:3 * NE + 8]
                P.op("dve", lambda: V.tensor_copy(out=L0, in_=bank[b][:, 0:NE]), reads=[tb[b]], writes=[t_lg[b]])
                P.op("dve", lambda: V.max(out=L1, in_=L0), reads=[t_lg[b]], writes=[t_lg[b]])
                P.op("dve", lambda: V.max_index(out=idx8[:, b, :], in_max=L1, in_values=L0), reads=[t_lg[b]], writes=[t_lg[b]])
                P.op("dve", lambda: V.tensor_scalar(out=maskb[:, oi, :], in0=L0, scalar1=lg[:, b, NE + 3:NE + 4], scalar2=None, op0=ALU.is_ge), reads=[t_lg[b]], writes=[t_maskb[oi]])
                P.op("dve", lambda: V.tensor_scalar(out=rs[:, r0 + 2:r0 + 3], in0=lg[:, b, NE:NE + 1], scalar1=-1.0, scalar2=None, op0=ALU.mult), reads=[t_lg[b]], writes=[t_rs[b]])
                P.op("act", lambda: A.activation(out=L3[:, 0:4], in_=L1[:, 0:4], func=AF.Exp, bias=rs[:, r0 + 2:r0 + 3], scale=1.0, accum_out=rs[:, r0 + 3:r0 + 4]),
                     reads=[t_lg[b], t_rs[b]], writes=[t_lg[b], t_rs[b]])
                P.op("dve", lambda: V.reciprocal(out=rs[:, r0 + 3:r0 + 4], in_=rs[:, r0 + 3:r0 + 4]), reads=[t_rs[b]], writes=[t_rs[b]])
                P.op("dve", lambda: V.tensor_scalar(out=gate4[:, 4 * oi:4 * oi + 4], in0=L3[:, 0:4], scalar1=rs[:, r0 + 3:r0 + 4], scalar2=None, op0=ALU.mult),
                     reads=[t_lg[b], t_rs[b]], writes=[t_gate4[oi]])
                pb = bank[b]
                P.group("pe", [lambda: T.matmul(pb[:, 64:64 + NE], lhsT=trib[:], rhs=maskb[:, oi, :], start=True, stop=True),
                               lambda: T.matmul(pb[:, 128:128 + NE], lhsT=ones128b[:], rhs=maskb[:, oi, :], start=True, stop=True)],
                        reads=[t_maskb[oi], t_cst, t_lg[b]], writes=[tb[b]])
                P.op("dve", lambda: V.tensor_tensor(out=posb[:, b, :], in0=pb[:, 64:64 + NE], in1=cnt_run[:], op=ALU.add), reads=[tb[b], t_cnt], writes=[t_lg[b]])
                P.op("dve", lambda: V.tensor_tensor(out=cnt_run[:], in0=pb[:, 128:128 + NE], in1=cnt_run[:], op=ALU.add), reads=[tb[b], t_cnt, t_lg[b]], writes=[t_cnt])
                EK = rs[:, r0 + 8:r0 + 12]; PK = rs[:, r0 + 12:r0 + 16]; DF = rs[:, r0 + 16:r0 + 20]
                P.op("dve", lambda: V.tensor_copy(out=EK, in_=idx8[:, b, 0:4]), reads=[t_lg[b]], writes=[t_rs[b]])
                for k in range(4):
                    P.op("dve", lambda k=k: V.scalar_tensor_tensor(out=junkp[:, b, :], in0=iota32, scalar=rs[:, r0 + 8 + k:r0 + 9 + k], in1=posb[:, b, :],
                                                                   op0=ALU.is_equal, op1=ALU.mult, accum_out=rs[:, r0 + 12 + k:r0 + 13 + k]),
                         reads=[t_lg[b], t_rs[b], t_cst], writes=[t_rs[b]])
                P.op("dve", lambda: V.scalar_tensor_tensor(out=DF, in0=EK, scalar=float(CAP), in1=PK, op0=ALU.mult, op1=ALU.add), reads=[t_rs[b]], writes=[t_rs[b]])
                P.op("dve", lambda: V.tensor_copy(out=dest_i[:, 4 * oi:4 * oi + 4], in_=DF), reads=[t_rs[b]], writes=[t_dest[oi]])
                for k in range(4):
                    P.dma("pool", lambda k=k: G_.indirect_dma_start(out=xbuf_d[:, :], out_offset=bass.IndirectOffsetOnAxis(ap=dest_i[:, 4 * oi + k:4 * oi + k + 1], axis=0),
                                                                    in_=h2tok[b][:, :], in_offset=None),
                          reads=[t_h2tok[b], t_dest[oi]], writes=[t_xbuf])
            for oi in range(NOWN):
                p3(oi)
            cf = rs[0:1, 0:NE]
            P.op("dve", lambda: V.tensor_scalar(out=cf, in0=cnt_run[0:1, :], scalar1=127.0, scalar2=1.0 / 128, op0=ALU.add, op1=ALU.mult), reads=[t_cnt] + t_rs, writes=t_rs)
            P.op("dve", lambda: V.tensor_scalar(out=cf, in0=cf, scalar1=-0.496, scalar2=None, op0=ALU.add), reads=t_rs, writes=t_rs)
            P.op("dve", lambda: V.tensor_copy(out=cnt_i[:], in_=cf), reads=t_rs, writes=[t_cnti])
            P.op("dve", lambda: V.tensor_scalar(out=posb[:, 0, :], in0=cnt_run[:], scalar1=iota_p, scalar2=None, op0=ALU.add), reads=[t_cnt, t_cst] + t_lg, writes=t_lg)
            P.op("dve", lambda: V.tensor_scalar(out=posb[:, 1, :], in0=posb[:, 0, :], scalar1=float(CAP), scalar2=None, op0=ALU.is_ge), reads=t_lg, writes=t_lg)
            P.op("dve", lambda: V.tensor_tensor(out=posb[:, 0, :], in0=posb[:, 0, :], in1=e2048, op=ALU.add), reads=t_lg + [t_cst], writes=t_lg)
            P.op("dve", lambda: V.tensor_scalar(out=rs[:, 40:41], in0=iota_p, scalar1=float(NE * CAP), scalar2=None, op0=ALU.add), reads=[t_cst] + t_rs, writes=t_rs)
            P.op("dve", lambda: V.tensor_scalar(out=junkp[:, 0, :], in0=posb[:, 0, :], scalar1=-1.0, scalar2=rs[:, 40:41], op0=ALU.mult, op1=ALU.add), reads=t_lg + t_rs, writes=t_lg)
            P.op("dve", lambda: V.tensor_tensor(out=junkp[:, 0, :], in0=junkp[:, 0, :], in1=posb[:, 1, :], op=ALU.mult), reads=t_lg, writes=t_lg)
            P.op("dve", lambda: V.tensor_tensor(out=posb[:, 0, :], in0=posb[:, 0, :], in1=junkp[:, 0, :], op=ALU.add), reads=t_lg, writes=t_lg)
            P.op("dve", lambda: V.tensor_copy(out=padidx[:], in_=posb[:, 0, :]), reads=t_lg, writes=[t_pad])
            for e in range(NE):
                P.dma("pool", lambda e=e: G_.indirect_dma_start(out=xbuf_d[:, :], out_offset=bass.IndirectOffsetOnAxis(ap=padidx[:, e:e + 1], axis=0),
                                                                in_=zrow[:, :], in_offset=None),
                      reads=[t_zrow, t_pad], writes=[t_xbuf])
            P.barrier()
            P.emit()

        sA.close()
        if int(os.environ.get('K_STOP', '9')) <= 3:
            return nc
        with ExitStack() as s4:
            w1b = [sbuf(s4, "w1b%d" % i, [128, 8, 2 * D], BF16) for i in range(2)]
            w2b = [sbuf(s4, "w2b%d" % i, [128, 8, D], BF16) for i in range(2)]
            b1r = [sbuf(s4, "b1r%d" % i, [1, 2 * D], BF16) for i in range(2)]
            b2r = [sbuf(s4, "b2r%d" % i, [1, D], BF16) for i in range(2)]
            t_w = trs(2)
            stg = [sbuf(s4, "stg%d" % i, [128, 8, 512]) for i in range(3)]; t_stg = trs(3)
            stc = [0]
            Xtok = [sbuf(s4, "Xtok%d" % i, [128, D], BF16) for i in range(2)]; t_Xtok = trs(2)
            XT = [sbuf(s4, "XT%d" % i, [128, 8, 128], BF16) for i in range(2)]; t_XT = trs(2)
            gg = [sbuf(s4, "gg%d" % i, [128, 256]) for i in range(2)]; t_gg = trs(2)
            sg = [sbuf(s4, "sg%d" % i, [128, 256]) for i in range(2)]; t_sg = trs(2)
            ll = [sbuf(s4, "ll%d" % i, [128, 256]) for i in range(2)]; t_ll = trs(2)
            atok = [sbuf(s4, "atok%d" % i, [128, D], BF16) for i in range(2)]; t_atok = trs(2)
            aT = [sbuf(s4, "aT%d" % i, [128, 8, 128], BF16) for i in range(2)]; t_aT = trs(2)
            yt = [sbuf(s4, "yt%d" % i, [128, D]) for i in range(2)]; t_yt = trs(2)
            t_ybuf = Tr()
            w1_v = w1_d.rearrange("e (k p) n -> e p k n", p=128)
            w2_v = w2_d.rearrange("e (k p) n -> e p k n", p=128)
            blk = [0]

            def block_body(e, bslot, ws):
                n = blk[0]
                blk[0] += 1
                xb = n % 2
                row0 = e * CAP + bslot * 128
                P.dma("act", lambda: nc.scalar.dma_start(out=Xtok[xb][:], in_=xbuf_d[row0:row0 + 128, :]), reads=[t_xbuf], writes=[t_Xtok[xb]], slot=xb)
                pT = bank[xb][:, :].bitcast(BF16)
                P.group("pe", [(lambda kc=kc: T.transpose(out=pT[:, kc * 128:(kc + 1) * 128], in_=Xtok[xb][:, kc * 128:(kc + 1) * 128], identity=identb[:])) for kc in range(8)],
                        reads=[t_Xtok[xb], t_cst], writes=[tb[xb]])
                P.op("act", lambda: A.activation(out=XT[xb][:].rearrange("p a b -> p (a b)"), in_=pT[:, :], func=AF.Copy), reads=[tb[xb]], writes=[t_XT[xb]])
                def do_cch(cch):
                    bk = 2 + (cch % 2)
                    par = cch % 2
                    fns = [(lambda kc=kc: T.matmul(bank[bk][:, :], lhsT=XT[xb][:, kc, :], rhs=w1b[ws][:, kc, cch * 512:(cch + 1) * 512], start=(kc == 0), stop=False)) for kc in range(8)]
                    fns.append(lambda: T.matmul(bank[bk][:, :], lhsT=onesb[0:1, :], rhs=b1r[ws][0:1, cch * 512:(cch + 1) * 512], start=False, stop=True))
                    P.group("pe", fns, reads=[t_XT[xb], t_w[ws], t_cst], writes=[tb[bk]])
                    P.op("dve", lambda: V.tensor_scalar(out=gg[par][:], in0=bank[bk][:, 0:512:2], scalar1=7.0, scalar2=None, op0=ALU.min), reads=[tb[bk]], writes=[t_gg[par]])
                    P.op("act", lambda: A.activation(out=sg[par][:], in_=gg[par][:], func=AF.Gelu_apprx_sigmoid), reads=[t_gg[par]], writes=[t_sg[par]])
                    P.op("dve", lambda: V.tensor_scalar(out=ll[par][:], in0=bank[bk][:, 1:512:2], scalar1=7.0, scalar2=-7.0, op0=ALU.min, op1=ALU.max), reads=[tb[bk]], writes=[t_ll[par]])
                    P.op("dve", lambda: V.scalar_tensor_tensor(out=atok[xb][:, cch * 256:(cch + 1) * 256], in0=ll[par][:], scalar=1.0, in1=sg[par][:], op0=ALU.add, op1=ALU.mult),
                         reads=[t_ll[par], t_sg[par]], writes=[t_atok[xb]])
                for cch in range(4):
                    do_cch(cch)
                bk = 4 + xb
                pT2 = bank[bk][:, :].bitcast(BF16)
                P.group("pe", [(lambda j=j: T.transpose(out=pT2[:, j * 128:(j + 1) * 128], in_=atok[xb][:, j * 128:(j + 1) * 128], identity=identb[:])) for j in range(8)],
                        reads=[t_atok[xb], t_cst], writes=[tb[bk]])
                P.op("act", lambda: A.activation(out=aT[xb][:].rearrange("p a b -> p (a b)"), in_=pT2[:, :], func=AF.Copy), reads=[tb[bk]], writes=[t_aT[xb]])
                def do_hf(hf):
                    bk2 = 6 + hf
                    fns = [(lambda j=j: T.matmul(bank[bk2][:, :], lhsT=aT[xb][:, j, :], rhs=w2b[ws][:, j, hf * 512:(hf + 1) * 512], start=(j == 0), stop=False)) for j in range(8)]
                    fns.append(lambda: T.matmul(bank[bk2][:, :], lhsT=onesb[0:1, :], rhs=b2r[ws][0:1, hf * 512:(hf + 1) * 512], start=False, stop=True))
                    P.group("pe", fns, reads=[t_aT[xb], t_w[ws], t_cst], writes=[tb[bk2]])
                    if hf == 0:
                        P.op("act", lambda: A.activation(out=yt[xb][:, 0:512], in_=bank[bk2][:, :], func=AF.Copy), reads=[tb[bk2]], writes=[t_yt[xb]])
                    else:
                        P.op("dve", lambda: V.tensor_copy(out=yt[xb][:, 512:1024], in_=bank[bk2][:, :]), reads=[tb[bk2]], writes=[t_yt[xb]])
                for hf in range(2):
                    do_hf(hf)
                P.dma("act", lambda: nc.scalar.dma_start(out=ybuf_d[row0:row0 + 128, :], in_=yt[xb][:]), reads=[t_yt[xb]], writes=[t_ybuf], slot=2 + xb)

            def piece(e, pc):
                ws = e % 2
                if pc < 4:
                    return w1_v[e, :, :, pc * 512:(pc + 1) * 512], w1b[ws][:, :, pc * 512:(pc + 1) * 512]
                return w2_v[e, :, :, (pc - 4) * 512:(pc - 3) * 512], w2b[ws][:, :, (pc - 4) * 512:(pc - 3) * 512]

            pend = {}

            def issue_load(e, pc):
                sl = stc[0] % 3
                stc[0] += 1
                src, _ = piece(e, pc)
                P.dma("sp", lambda: nc.sync.dma_start(out=stg[sl][:], in_=src), writes=[t_stg[sl]])
                pend[(e, pc)] = sl

            def issue_bias(e):
                ws = e % 2
                P.dma("pool", lambda: G_.dma_start(out=b1r[ws][:], in_=b1_d[e:e + 1, :]), writes=[t_w[ws]])
                P.dma("pool", lambda: G_.dma_start(out=b2r[ws][:], in_=b2_d[e:e + 1, :]), writes=[t_w[ws]])

            def cast_piece(e, pc):
                ws = e % 2
                sl = pend.pop((e, pc))
                _, dst = piece(e, pc)
                if pc % 2 == 0:
                    P.op("act", lambda: A.activation(out=dst, in_=stg[sl][:], func=AF.Copy), reads=[t_stg[sl]], writes=[t_w[ws]])
                else:
                    P.op("dve", lambda: V.tensor_copy(out=dst, in_=stg[sl][:]), reads=[t_stg[sl]], writes=[t_w[ws]])

            NEX = int(os.environ.get('K_NE', NE))
            for pc in range(3):
                pass
            issue_order = []
            for e in range(NEX):
                ws = e % 2
                if e == 0:
                    for pc in range(3):
                        issue_load(0, pc)
                    for pc in range(6):
                        cast_piece(0, pc)
                        if pc + 3 < 6:
                            issue_load(0, pc + 3)
                    issue_bias(0)
                if e + 1 < NEX:
                    for pc in range(3):
                        issue_load(e + 1, pc)
                    issue_bias(e + 1)
                P.regload(cnt_i[0:1, e:e + 1], reads=[t_cnti])
                for bslot in range(int(os.environ.get('K_NB', CAP // 128))):
                    P.cond_begin(bslot + 1)
                    block_body(e, bslot, ws)
                    P.cond_end()
                    if e + 1 < NEX and bslot < 6:
                        cast_piece(e + 1, bslot)
                        if bslot + 3 < 6:
                            issue_load(e + 1, bslot + 3)
            P.barrier()
            P.emit()

        if int(os.environ.get('K_STOP', '9')) <= 4:
            return nc
        with ExitStack() as s5:
            gt2_b = sbuf(s5, "gt2_b", [128, D]); gfin_b = sbuf(s5, "gfin_b", [128, D]); t_g5 = Tr()
            yk = [sbuf(s5, "yk%d" % i, [128, D]) for i in range(4)]; t_yk = trs(4)
            acc = [sbuf(s5, "acc%d" % i, [128, D]) for i in range(2)]; t_acc = trs(2)
            x1b = [sbuf(s5, "x1b%d" % i, [128, D]) for i in range(2)]; t_x1b = trs(2)
            ob_ = [sbuf(s5, "ob%d" % i, [128, D]) for i in range(2)]; t_ob = trs(2)
            junk5 = sbuf(s5, "junk5", [128, D], BF16); t_junk5 = Tr()
            fs5 = sbuf(s5, "fs5", [128, 8]); t_fs5 = trs(2)
            P.dma("sp", lambda: nc.sync.dma_start(out=gt2_b[:], in_=mod_d[0:1, 5 * D:6 * D].partition_broadcast(128)), writes=[t_g5])
            P.dma("sp", lambda: nc.sync.dma_start(out=gfin_b[:], in_=gfin_d[0:1, :].partition_broadcast(128)), writes=[t_g5])

            def p5(oi):
                b = oi % 2
                P.dma("sp", lambda: nc.sync.dma_start(out=x1b[b][:], in_=x1_d[oi * 128:(oi + 1) * 128, :]), writes=[t_x1b[b]])
                for k in range(4):
                    P.dma("pool", lambda k=k: G_.indirect_dma_start(out=yk[k][:, :], out_offset=None, in_=ybuf_d[:, :],
                                                                    in_offset=bass.IndirectOffsetOnAxis(ap=dest_i[:, 4 * oi + k:4 * oi + k + 1], axis=0),
                                                                    ), reads=[t_dest[oi]], writes=[t_yk[k]])
                    if k == 0:
                        P.op("dve", lambda: V.tensor_scalar(out=acc[b][:], in0=yk[0][:], scalar1=gate4[:, 4 * oi:4 * oi + 1], scalar2=None, op0=ALU.mult),
                             reads=[t_yk[0], t_gate4[oi]], writes=[t_acc[b]])
                    else:
                        P.op("dve", lambda k=k: V.scalar_tensor_tensor(out=acc[b][:], in0=yk[k][:], scalar=gate4[:, 4 * oi + k:4 * oi + k + 1], in1=acc[b][:], op0=ALU.mult, op1=ALU.add),
                             reads=[t_yk[k], t_gate4[oi], t_acc[b]], writes=[t_acc[b]])
                P.op("pool", lambda: G_.tensor_tensor(out=acc[b][:], in0=acc[b][:], in1=gt2_b[:], op=ALU.mult), reads=[t_acc[b], t_g5], writes=[t_acc[b]])
                P.op("pool", lambda: G_.tensor_tensor(out=x1b[b][:], in0=acc[b][:], in1=x1b[b][:], op=ALU.add), reads=[t_acc[b], t_x1b[b]], writes=[t_x1b[b]])
                P.op("act", lambda: A.activation(out=junk5[:], in_=x1b[b][:], func=AF.Square, accum_out=fs5[:, 4 * b:4 * b + 1]), reads=[t_x1b[b]], writes=[t_junk5, t_fs5[b]])
                P.op("dve", lambda: V.tensor_scalar(out=fs5[:, 4 * b + 1:4 * b + 2], in0=fs5[:, 4 * b:4 * b + 1], scalar1=1.0 / D, scalar2=EPS, op0=ALU.mult, op1=ALU.add),
                     reads=[t_fs5[b]], writes=[t_fs5[b]])
                P.op("act", lambda: A.activation(out=fs5[:, 4 * b + 1:4 * b + 2], in_=fs5[:, 4 * b + 1:4 * b + 2], func=AF.Sqrt), reads=[t_fs5[b]], writes=[t_fs5[b]])
                P.op("dve", lambda: V.reciprocal(out=fs5[:, 4 * b + 1:4 * b + 2], in_=fs5[:, 4 * b + 1:4 * b + 2]), reads=[t_fs5[b]], writes=[t_fs5[b]])
                P.op("dve", lambda: V.scalar_tensor_tensor(out=ob_[b][:], in0=x1b[b][:], scalar=fs5[:, 4 * b + 1:4 * b + 2], in1=gfin_b[:], op0=ALU.mult, op1=ALU.mult),
                     reads=[t_x1b[b], t_fs5[b], t_g5], writes=[t_ob[b]])
                P.dma("sp", lambda: nc.sync.dma_start(out=out_d[oi * 128:(oi + 1) * 128, :], in_=ob_[b][:]), reads=[t_ob[b]])
            for oi in range(NOWN):
                p5(oi)
            P.barrier()
            P.emit()
    return nc


def _consts():
    c = np.zeros((128, NCST), np.float32)
    c[:, 0:128] = np.eye(128, dtype=np.float32)
    k = np.arange(128)[:, None]
    q = np.arange(128)[None, :]
    c[:, 128:256] = (k <= q)
    c[:, 256:384] = (k > q)
    inv = (1.0 / (np.float32(10000.0) ** (np.arange(0, 64, 2, dtype=np.float32) / np.float32(64)))).astype(np.float32)
    p = np.arange(128)
    c[:, 384] = inv[p % 32]
    c[:, 385] = np.where((p % 64) < 32, -1.0, 1.0)
    c[:, 386] = np.float32(math.pi / 2)
    c[:, 387] = 0.0
    c[:, 388] = 1.0
    c[:, 392:520] = 1.0
    c[:, 520:648] = (k < q)
    c[:, 648:680] = np.arange(32)[None, :]
    c[:, 680:712] = (np.arange(32) * CAP)[None, :]
    c[:, 712] = np.arange(128)
    return c


def _core_masks(j):
    own = own_blocks(j)
    k = np.arange(128)[:, None]
    q = np.arange(128)[None, :]
    tri = (k <= q).astype(np.float32)
    low = (k > q).astype(np.float32)
    dm = np.zeros((128, NOWN, 4, 128), np.float32)
    sm = np.zeros((128, NOWN, 2, 128), np.float32)
    for oi, gb in enumerate(own):
        nkb = 8 * (oi // 2) + (4 if oi % 2 == 0 else 8)
        for i in range(4):
            kb = nkb - 4 + i
            if kb < gb:
                dm[:, oi, i, :] = 1.0
            elif kb == gb:
                dm[:, oi, i, :] = tri
        sm[:, oi, 1, :] = tri
        if gb > 0:
            sm[:, oi, 0, :] = low
    return dm.reshape(128, NOWN * 512), sm.reshape(128, NOWN * 256)


_NC_CACHE = {}


def kernel(x, c, positions, w_ada, b_ada, g_mix, w_in, b_in, attn_sinks, lambda_q1, lambda_k1, lambda_q2, lambda_k2,
           g_subln, w_out, b_out, g_ffn, w_router, b_router, w1, b1, w2, b2, g_final):
    f = lambda a: np.ascontiguousarray(np.asarray(a))
    x = f(x); positions = f(positions)
    if "nc" not in _NC_CACHE:
        _NC_CACHE["nc"] = build_program()
    nc = _NC_CACHE["nc"]
    colT = lambda v: f(np.asarray(v).reshape(-1, 128).T)
    w_sel = f(np.asarray(w_in)[0][:, SEL])
    b_sel = f(np.asarray(b_in)[0][SEL])
    b1_ = np.asarray(b1)[0]
    shared = {
        "w_ada": f(np.asarray(w_ada)[0]), "b_ada": f(np.asarray(b_ada)[0][None, :]),
        "gmixT": colT(np.asarray(g_mix)[0]), "gffnT": colT(np.asarray(g_ffn)[0]),
        "w_sel": w_sel, "b_selT": colT(b_sel), "b_sel": f(b_sel[None, :]),
        "sinks": f(np.asarray(attn_sinks)[0][None, :]),
        "lam4": f(np.stack([np.asarray(lambda_q1)[0], np.asarray(lambda_k1)[0], np.asarray(lambda_q2)[0], np.asarray(lambda_k2)[0]])),
        "g_subln": f(np.asarray(g_subln)[0][None, :]),
        "w_out": f(np.asarray(w_out)[0]), "b_out": f(np.asarray(b_out)[0][None, :]),
        "w_router": f(np.asarray(w_router)[0]), "b_router": f(np.asarray(b_router)[0][None, :]),
        "w1": f(np.asarray(w1)[0]), "w2": f(np.asarray(w2)[0]), "b2": f(np.asarray(b2)[0]),
        "b1": f(b1_), "g_ffn": f(np.asarray(g_ffn)[0][None, :]),
        "g_final": f(np.asarray(g_final)[None, :]),
        "consts": _consts(),
    }
    in_maps = []
    rows_all = []
    for core in range(8):
        b, j = core // 4, core % 4
        own = own_blocks(j)
        rows_own = np.concatenate([np.arange(g * 128, (g + 1) * 128) for g in own])
        rows_prev = np.concatenate([np.arange(max(g - 1, 0) * 128, (max(g - 1, 0) + 1) * 128) for g in own])
        rows_all.append(rows_own)
        xb = x[b]
        x_ext = np.concatenate([xb, xb[rows_own], xb[rows_prev]], axis=0)
        pb = positions[b]
        pos_ext = np.concatenate([pb, pb[rows_own], pb[rows_prev]])[None, :].astype(np.int32)
        dm, sm = _core_masks(j)
        m = dict(shared)
        m.update({"x": f(x_ext), "pos": f(pos_ext), "cT": colT(np.asarray(c)[b]), "dmask": dm, "smask": sm})
        in_maps.append(m)
    res = run_bass_kernel_spmd(nc, in_maps, core_ids=list(range(8)))
    out = np.zeros((2, S, D), np.float32)
    for core in range(8):
        out[core // 4, rows_all[core], :] = np.asarray(res.results[core]["out"])
    return out
```

```python
import math
import os
from contextlib import ExitStack

import numpy as np
import concourse.bass as bass
import concourse.mybir as mybir
from concourse.bass_utils import run_bass_kernel_spmd

F32 = mybir.dt.float32
BF16 = mybir.dt.bfloat16
I32 = mybir.dt.int32
ALU = mybir.AluOpType
AF = mybir.ActivationFunctionType
AX = mybir.AxisListType

D = 1024
S = 8192
NT = 64
NG = 16
NOWN = 16
NE = 32
SX = S + 2 * NOWN * 128
NGX = SX // 512
NCST = 720
CAP = 2048
U32 = mybir.dt.uint32
EPS = 1e-5
C1 = 6.28125
C2 = 2 * math.pi - 6.28125
INV2PI = float(1.0 / (2 * math.pi))

OFF_QA, OFF_KA, OFF_VA, OFF_QD, OFF_KD, OFF_VD = 0, 512, 640, 768, 1280, 1792


def _swap64(cols):
    cols = np.asarray(cols).reshape(-1, 64)
    return np.concatenate([cols[:, 32:], cols[:, :32]], axis=1).reshape(-1)


def _unit_cols():
    units = []
    k = np.concatenate([np.tile(np.arange(OFF_KA + g * 64, OFF_KA + (g + 1) * 64), 2) for g in range(2)])
    q = np.arange(OFF_QA, OFF_QA + 512)
    v = np.arange(OFF_VA, OFF_VA + 128)
    units.append(dict(nk=2, nq=4, k=k, q=q, v=v))
    for h in range(4):
        k = np.arange(OFF_KD + h * 128, OFF_KD + (h + 1) * 128)
        q = np.arange(OFF_QD + h * 128, OFF_QD + (h + 1) * 128)
        v = np.arange(OFF_VD + h * 128, OFF_VD + (h + 1) * 128)
        units.append(dict(nk=1, nq=1, k=k, q=q, v=v))
    off = 0
    sel = []
    for u in units:
        u["base"] = off
        parts = [u["k"], _swap64(u["k"]), u["q"], _swap64(u["q"]), u["v"]]
        u["o_k"] = 0
        u["o_ks"] = len(u["k"])
        u["o_q"] = u["o_ks"] + len(u["k"])
        u["o_qs"] = u["o_q"] + len(u["q"])
        u["o_v"] = u["o_qs"] + len(u["q"])
        u["ncols"] = u["o_v"] + 128
        sel.append(np.concatenate(parts))
        off += u["ncols"]
    return units, np.concatenate(sel)


UNITS, SEL = _unit_cols()
NSEL = len(SEL)
NCH = NSEL // 128


def own_blocks(j):
    return sorted([8 * m + j for m in range(8)] + [8 * m + 7 - j for m in range(8)])


class Tr:
    __slots__ = ("w", "r")

    def __init__(self):
        self.w = {}
        self.r = {}


def trs(n):
    return [Tr() for _ in range(n)]


class Prog:
    ENG = ("pe", "act", "dve", "pool", "sp")

    def __init__(self, nc, stack, n_dma_sems=48):
        self.nc = nc
        self.q = {e: [] for e in self.ENG}
        self.esem = {e: stack.enter_context(nc.semaphore("s_" + e)) for e in self.ENG}
        self.ecnt = {e: 0 for e in self.ENG}
        self.waited = {e: {} for e in self.ENG}
        self.dsem = [stack.enter_context(nc.semaphore("d%d" % i)) for i in range(n_dma_sems)]
        self.dcnt = [0] * n_dma_sems
        self.dpool = {"sp": list(range(0, n_dma_sems - 16)), "pool": list(range(n_dma_sems - 16, n_dma_sems))}
        self.dnext = {"sp": 0, "pool": 0}
        self.in_cond = False
        self.handles = {"pe": nc.tensor, "act": nc.scalar, "dve": nc.vector, "pool": nc.gpsimd, "sp": nc.sync}

    def _need(self, eng, s, v):
        wd = self.waited[eng]
        if wd.get(s, 0) >= v:
            return
        wd[s] = v
        self.q[eng].append(("wait", s, v))

    def _waits(self, eng, reads, writes):
        need = {}
        for t in reads:
            for s, v in t.w.items():
                if need.get(s, 0) < v:
                    need[s] = v
        for t in writes:
            for s, v in t.w.items():
                if need.get(s, 0) < v:
                    need[s] = v
            for s, v in t.r.items():
                if need.get(s, 0) < v:
                    need[s] = v
        for s, v in need.items():
            if eng == "pe" and s is self.esem["pe"]:
                continue
            self._need(eng, s, v)

    def _record(self, ev, reads, writes):
        s, v = ev
        for t in reads:
            if t.r.get(s, 0) < v:
                t.r[s] = v
        for t in writes:
            if self.in_cond:
                if t.w.get(s, 0) < v:
                    t.w[s] = v
            else:
                t.w = {s: v}
                t.r = {}

    def op(self, eng, fn, reads=(), writes=()):
        self._waits(eng, reads, writes)
        self.ecnt[eng] += 1
        ev = (self.esem[eng], self.ecnt[eng])
        self.q[eng].append(("op", fn, self.esem[eng], 1))
        self._record(ev, reads, writes)

    def group(self, eng, fns, reads=(), writes=()):
        self._waits(eng, reads, writes)
        self.ecnt[eng] += 1
        ev = (self.esem[eng], self.ecnt[eng])
        for f in fns[:-1]:
            self.q[eng].append(("op", f, None, 0))
        self.q[eng].append(("op", fns[-1], self.esem[eng], 1))
        self._record(ev, reads, writes)

    def dma(self, eng, fn, reads=(), writes=(), slot=None):
        pl = self.dpool[eng]
        if slot is None:
            i = pl[self.dnext[eng]]
            self.dnext[eng] = (self.dnext[eng] + 1) % (len(pl) - 4)
        else:
            i = pl[len(pl) - 4 + slot]
        s = self.dsem[i]
        if self.dcnt[i]:
            self._need(eng, s, self.dcnt[i])
        self._waits(eng, reads, writes)
        self.dcnt[i] += 16
        ev = (s, self.dcnt[i])
        self.q[eng].append(("op", fn, s, 16))
        self._record(ev, reads, writes)
        return ev

    CENG = ("pe", "act", "dve", "sp")

    def regload(self, ap, reads=()):
        for e in self.CENG:
            self._waits(e, reads, ())
            self.q[e].append(("regload", ap))

    def cond_begin(self, thr):
        if not hasattr(self, "_cstack"):
            self._cstack = []
        self._cstack.append(({e: self.ecnt[e] for e in self.ENG}, list(self.dcnt), {e: dict(self.waited[e]) for e in self.ENG}))
        self.in_cond = True
        for e in self.CENG:
            self.q[e].append(["if", thr, None])

    def cond_end(self):
        ec0, dc0, wd0 = self._cstack.pop()
        assert self.ecnt["pool"] == ec0["pool"], "pool must stay outside conditional regions"
        dd = [(i, self.dcnt[i] - dc0[i]) for i in range(len(self.dcnt)) if self.dcnt[i] != dc0[i]]
        for i, _ in dd:
            assert i in self.dpool["sp"]
        for e in self.CENG:
            comp = []
            if self.ecnt[e] != ec0[e]:
                comp.append((self.esem[e], self.ecnt[e] - ec0[e]))
            if e == "sp":
                comp += [(self.dsem[i], d, dc0[i]) for i, d in dd]
            for it in reversed(self.q[e]):
                if isinstance(it, list) and it[0] == "if" and it[2] is None:
                    it[2] = comp
                    break
            self.q[e].append(("endif",))
            self.waited[e] = wd0[e]
        self.waited["pool"] = wd0["pool"]
        self.in_cond = bool(self._cstack)

    def barrier(self):
        for e in self.ENG:
            for f in self.ENG:
                if f != e and self.ecnt[f]:
                    self._need(e, self.esem[f], self.ecnt[f])
            for i, s in enumerate(self.dsem):
                if self.dcnt[i]:
                    self._need(e, s, self.dcnt[i])

    def emit(self):
        nc = self.nc
        q = self.q
        self.q = {e: [] for e in self.ENG}
        if not hasattr(self, "regs"):
            self.regs = {}
        with nc.Block() as block:
            def run_items(h, ename, items):
                i = 0
                n = len(items)
                while i < n:
                    it = items[i]
                    k = it[0]
                    if k == "wait":
                        h.wait_ge(it[1], it[2])
                    elif k == "op":
                        ins = it[1]()
                        if it[2] is not None:
                            ins.then_inc(it[2], it[3])
                    elif k == "regload":
                        if ename not in self.regs:
                            self.regs[ename] = h.alloc_register("cnt_" + ename)
                        h.reg_load(self.regs[ename], it[1])
                    elif k == "if":
                        depth = 1
                        j = i + 1
                        while True:
                            if items[j][0] == "if":
                                depth += 1
                            elif items[j][0] == "endif":
                                depth -= 1
                                if depth == 0:
                                    break
                            j += 1
                        body = items[i + 1:j]
                        with h.If_lt(self.regs[ename], it[1]):
                            h.drain()
                            for cp in it[2]:
                                if len(cp) == 3 and cp[2]:
                                    h.wait_ge(cp[0], cp[2])
                                h.sem_inc(cp[0], cp[1])
                        with h.Else():
                            run_items(h, ename, body)
                        i = j
                    i += 1

            def run(ename):
                run_items(self.handles[ename], ename, q[ename])

            @block.tensor
            def _(e):
                run("pe")

            @block.scalar
            def _(e):
                run("act")

            @block.vector
            def _(e):
                run("dve")

            @block.gpsimd
            def _(e):
                run("pool")

            @block.sync
            def _(e):
                run("sp")


def build_program(j_core_unused=None, debug=False):
    nc = bass.Bass("TRN2", target_bir_lowering=False)
    din = lambda name, shape, dt=F32: nc.dram_tensor(name, list(shape), dt, kind="ExternalInput").ap()
    x_d = din("x", [SX, D])
    pos_d = din("pos", [1, SX], I32)
    dmask_d = din("dmask", [128, NOWN * 512])
    smask_d = din("smask", [128, NOWN * 256])
    cT_d = din("cT", [128, 8])
    wada_d = din("w_ada", [D, 6 * D])
    bada_d = din("b_ada", [1, 6 * D])
    gmixT_d = din("gmixT", [128, 8])
    gffnT_d = din("gffnT", [128, 8])
    wsel_d = din("w_sel", [D, NSEL])
    bselT_d = din("b_selT", [128, NCH])
    bsel_d = din("b_sel", [1, NSEL])
    sinks_d = din("sinks", [1, 8])
    lam_d = din("lam4", [4, 64])
    gsub_d = din("g_subln", [1, 128])
    wout_d = din("w_out", [D, D])
    bout_d = din("b_out", [1, D])
    wr_d = din("w_router", [D, NE])
    br_d = din("b_router", [1, NE])
    w1_d = din("w1", [NE, D, 2 * D])
    b1_d = din("b1", [NE, 2 * D])
    gffn_d = din("g_ffn", [1, D])
    w2_d = din("w2", [NE, D, D])
    b2_d = din("b2", [NE, D])
    gfin_d = din("g_final", [1, D])
    cst_d = din("consts", [128, NCST])
    out_d = nc.dram_tensor("out", [NOWN * 128, D], F32, kind="ExternalOutput").ap()
    hT_d = nc.dram_tensor("hT_scr", [8, 128, SX], BF16, kind="Internal").ap()
    cos_d = nc.dram_tensor("cos_scr", [128, SX], F32, kind="Internal").ap()
    sin_d = nc.dram_tensor("sin_scr", [128, SX], F32, kind="Internal").ap()
    x1_d = nc.dram_tensor("x1_scr", [NOWN * 128, D], F32, kind="Internal").ap()
    mod_d = nc.dram_tensor("mod_scr", [1, 6 * D], F32, kind="Internal").ap()
    xbuf_d = nc.dram_tensor("xbuf_scr", [NE * CAP + 128, D], BF16, kind="Internal").ap()
    ybuf_d = nc.dram_tensor("ybuf_scr", [NE * CAP, D], F32, kind="Internal").ap()


    with ExitStack() as st:
        P = Prog(nc, st)
        sbuf = lambda stack, name, shape, dt=F32: stack.enter_context(nc.sbuf_tensor(name, list(shape), dt))
        V, A, T, G_ = nc.vector, nc.scalar, nc.tensor, nc.gpsimd

        bank = [st.enter_context(nc.psum_tensor("bank%d" % i, [128, 512], F32)) for i in range(8)]
        tb = trs(8)

        cst = sbuf(st, "cst", [128, NCST]); t_cst = Tr()
        identb = sbuf(st, "identb", [128, 128], BF16)
        mask256 = sbuf(st, "mask256", [128, 256], BF16)
        onesb = sbuf(st, "onesb", [1, 128], BF16)
        trib = sbuf(st, "trib", [128, 128], BF16)
        ones128b = sbuf(st, "ones128b", [128, 128], BF16)
        A1 = sbuf(st, "A1", [128, 8]); S1 = sbuf(st, "S1", [128, 8])
        A2 = sbuf(st, "A2", [128, 8]); S2 = sbuf(st, "S2", [128, 8])
        t_mod = Tr()
        bufA = sbuf(st, "bufA", [128, 16 * 1024], BF16)
        t_mixed = trs(NOWN)
        small = sbuf(st, "small", [128, 64]); t_small = Tr()
        ident = cst[:, 0:128]
        invf = cst[:, 384:385]
        sgn = cst[:, 385:386]
        halfpi = cst[:, 386:387]
        zero_c = cst[:, 387:388]
        one11 = cst[0:1, 388:389]
        ones_row = cst[0:1, 392:520]
        neglam = small[:, 0:1]
        expsink = small[:, 8:16]

        P.dma("sp", lambda: nc.sync.dma_start(out=cst[:], in_=cst_d[:, :]), writes=[t_cst])
        P.op("dve", lambda: V.tensor_copy(out=identb[:], in_=cst[:, 0:128]), reads=[t_cst], writes=[t_cst])
        P.op("dve", lambda: V.tensor_copy(out=mask256[:, 0:128], in_=cst[:, 256:384]), reads=[t_cst], writes=[t_cst])
        P.op("dve", lambda: V.tensor_copy(out=mask256[:, 128:256], in_=cst[:, 128:256]), reads=[t_cst], writes=[t_cst])
        P.op("dve", lambda: V.tensor_copy(out=onesb[:], in_=cst[0:1, 392:520]), reads=[t_cst], writes=[t_cst])
        P.op("dve", lambda: V.tensor_copy(out=trib[:], in_=cst[:, 520:648]), reads=[t_cst], writes=[t_cst])
        P.op("dve", lambda: V.tensor_copy(out=ones128b[:], in_=cst[:, 392:520]), reads=[t_cst], writes=[t_cst])

        with ExitStack() as s0:
            cT = sbuf(s0, "cT_sb", [128, 8]); t_cT = Tr()
            wad = [sbuf(s0, "wad%d" % i, [128, 8, 512]) for i in range(2)]; t_wad = trs(2)
            modrow = sbuf(s0, "modrow", [1, 6 * D]); t_modrow = Tr()
            badar = sbuf(s0, "badar", [1, 6 * D]); t_bada = Tr()
            gT = sbuf(s0, "gT", [128, 16]); t_gT = Tr()
            lamb = sbuf(s0, "lamb", [128, 256]); t_lam = Tr()
            lamp = sbuf(s0, "lamp", [128, 128])
            P.dma("sp", lambda: nc.sync.dma_start(out=cT[:], in_=cT_d[:, :]), writes=[t_cT])
            P.dma("sp", lambda: nc.sync.dma_start(out=badar[:], in_=bada_d[:, :]), writes=[t_bada])
            P.dma("sp", lambda: nc.sync.dma_start(out=gT[:, 0:8], in_=gmixT_d[:, :]), writes=[t_gT])
            P.dma("sp", lambda: nc.sync.dma_start(out=gT[:, 8:16], in_=gffnT_d[:, :]), writes=[t_gT])
            P.dma("sp", lambda: nc.sync.dma_start(out=lamb[:].rearrange("p (a b) -> p a b", a=4),
                                                  in_=lam_d[:, :].partition_broadcast(128)), writes=[t_lam])
            P.dma("sp", lambda: nc.sync.dma_start(out=small[:, 16:24], in_=sinks_d[0:1, :].partition_broadcast(128)), writes=[t_small])
            P.op("act", lambda: A.activation(out=cT[:], in_=cT[:], func=AF.Silu), reads=[t_cT], writes=[t_cT])
            wada_v = wada_d.rearrange("(k p) n -> p k n", p=128)
            for pc in range(12):
                b = pc % 2
                P.dma("sp", lambda pc=pc, b=b: nc.sync.dma_start(out=wad[b][:], in_=wada_v[:, :, pc * 512:(pc + 1) * 512]), writes=[t_wad[b]])
                bk = pc % 2
                P.group("pe", [(lambda kc=kc, b=b, bk=bk: T.matmul(bank[bk][0:1, :], lhsT=cT[:, kc:kc + 1], rhs=wad[b][:, kc, :],
                                                                    start=(kc == 0), stop=(kc == 7))) for kc in range(8)],
                        reads=[t_cT, t_wad[b]], writes=[tb[bk]])
                P.op("dve", lambda pc=pc, bk=bk: V.tensor_tensor(out=modrow[0:1, pc * 512:(pc + 1) * 512], in0=bank[bk][0:1, :],
                                                                 in1=badar[0:1, pc * 512:(pc + 1) * 512], op=ALU.add),
                     reads=[tb[bk], t_bada], writes=[t_modrow])
            cols = [(0, 0), (1, 8), (3, 16), (4, 24)]
            fns = []
            for mi, dc in cols:
                for kc in range(8):
                    fns.append(lambda mi=mi, dc=dc, kc=kc: T.matmul(bank[2][:, dc + kc:dc + kc + 1],
                                                                    lhsT=modrow[0:1, mi * D + kc * 128: mi * D + (kc + 1) * 128],
                                                                    rhs=one11, start=True, stop=True))
            P.group("pe", fns, reads=[t_modrow, t_cst], writes=[tb[2]])
            P.op("dve", lambda: V.tensor_copy(out=S1[:], in_=bank[2][:, 0:8]), reads=[tb[2]], writes=[t_mod])
            P.op("dve", lambda: V.scalar_tensor_tensor(out=A1[:], in0=bank[2][:, 8:16], scalar=1.0, in1=gT[:, 0:8], op0=ALU.add, op1=ALU.mult),
                 reads=[tb[2], t_gT], writes=[t_mod])
            P.op("dve", lambda: V.tensor_copy(out=S2[:], in_=bank[2][:, 16:24]), reads=[tb[2]], writes=[t_mod])
            P.op("dve", lambda: V.scalar_tensor_tensor(out=A2[:], in0=bank[2][:, 24:32], scalar=1.0, in1=gT[:, 8:16], op0=ALU.add, op1=ALU.mult),
                 reads=[tb[2], t_gT], writes=[t_mod])
            P.dma("sp", lambda: nc.sync.dma_start(out=mod_d[:, :], in_=modrow[:]), reads=[t_modrow])
            P.op("dve", lambda: V.tensor_tensor(out=lamp[:, 0:64], in0=lamb[:, 0:64], in1=lamb[:, 64:128], op=ALU.mult), reads=[t_lam], writes=[t_lam])
            P.op("dve", lambda: V.tensor_tensor(out=lamp[:, 64:128], in0=lamb[:, 128:192], in1=lamb[:, 192:256], op=ALU.mult), reads=[t_lam], writes=[t_lam])
            P.op("dve", lambda: V.tensor_reduce(out=small[:, 1:3], in_=lamp[:].rearrange("p (a b) -> p a b", a=2), axis=AX.X, op=ALU.add),
                 reads=[t_lam], writes=[t_small])
            P.op("act", lambda: A.activation(out=small[:, 1:3], in_=small[:, 1:3], func=AF.Exp), reads=[t_small], writes=[t_small])
            P.op("dve", lambda: V.scalar_tensor_tensor(out=small[:, 0:1], in0=small[:, 2:3], scalar=-0.2, in1=small[:, 1:2], op0=ALU.add, op1=ALU.subtract),
                 reads=[t_small], writes=[t_small])
            P.op("act", lambda: A.activation(out=small[:, 8:16], in_=small[:, 16:24], func=AF.Exp), reads=[t_small], writes=[t_small])
            P.barrier()
            P.emit()

        with ExitStack() as s1:
            XB = 8
            xt = [sbuf(s1, "xt%d" % i, [128, D]) for i in range(XB)]; t_xt = trs(XB)
            xn = [sbuf(s1, "xn%d" % i, [128, D], BF16) for i in range(2)]; t_xn = trs(2)
            junk = sbuf(s1, "junk", [128, D], BF16); t_junk = Tr()
            ssq = sbuf(s1, "ssq", [128, 2, 8]); t_ssq = trs(2)
            hTg = [sbuf(s1, "hTg%d" % i, [128, 8, 512], BF16) for i in range(2)]; t_hTg = trs(2)
            posi = sbuf(s1, "posi", [128, 512], I32); t_posi = Tr()
            ang = sbuf(s1, "ang", [128, 512]); t_ang = Tr()
            ki = sbuf(s1, "ki", [128, 512], I32); kf = sbuf(s1, "kf", [128, 512]); t_k = Tr()
            rr = sbuf(s1, "rr", [128, 512]); t_rr = Tr()
            tab = [sbuf(s1, "tab%d" % i, [128, 512]) for i in range(4)]; t_tab = trs(4)
            hT_v = hT_d.rearrange("k p t -> p k t")

            def rope_group(g):
                P.dma("sp", lambda: nc.sync.dma_start(out=posi[:], in_=pos_d[0:1, g * 512:(g + 1) * 512].partition_broadcast(128)), writes=[t_posi])
                P.op("dve", lambda: V.tensor_copy(out=ang[:], in_=posi[:]), reads=[t_posi], writes=[t_ang])
                P.op("dve", lambda: V.tensor_scalar(out=ang[:], in0=ang[:], scalar1=invf, scalar2=None, op0=ALU.mult), reads=[t_ang, t_cst], writes=[t_ang])
                for which in range(2):
                    tbi = (2 * g + which) % 4
                    if which == 0:
                        P.op("dve", lambda: V.tensor_scalar(out=ki[:], in0=ang[:], scalar1=INV2PI, scalar2=None, op0=ALU.mult), reads=[t_ang], writes=[t_k])
                    else:
                        P.op("dve", lambda: V.tensor_scalar(out=ki[:], in0=ang[:], scalar1=INV2PI, scalar2=0.25, op0=ALU.mult, op1=ALU.add), reads=[t_ang], writes=[t_k])
                    P.op("dve", lambda: V.tensor_copy(out=kf[:], in_=ki[:]), reads=[t_k], writes=[t_k])
                    P.op("dve", lambda: V.scalar_tensor_tensor(out=rr[:], in0=kf[:], scalar=-C1, in1=ang[:], op0=ALU.mult, op1=ALU.add), reads=[t_k, t_ang], writes=[t_rr])
                    P.op("dve", lambda: V.scalar_tensor_tensor(out=rr[:], in0=kf[:], scalar=-C2, in1=rr[:], op0=ALU.mult, op1=ALU.add), reads=[t_k, t_rr], writes=[t_rr])
                    if which == 0:
                        P.op("dve", lambda: V.tensor_scalar(out=rr[:], in0=rr[:], scalar1=-3.1415925, scalar2=3.1415925, op0=ALU.max, op1=ALU.min), reads=[t_rr], writes=[t_rr])
                        P.op("act", lambda tbi=tbi: A.activation(out=tab[tbi][:], in_=rr[:], func=AF.Sin, scale=sgn, bias=zero_c), reads=[t_rr, t_cst], writes=[t_tab[tbi]])
                        P.dma("sp", lambda tbi=tbi: nc.sync.dma_start(out=sin_d[:, g * 512:(g + 1) * 512], in_=tab[tbi][:]), reads=[t_tab[tbi]])
                    else:
                        P.op("dve", lambda: V.tensor_scalar(out=rr[:], in0=rr[:], scalar1=-4.712388, scalar2=1.570796, op0=ALU.max, op1=ALU.min), reads=[t_rr], writes=[t_rr])
                        P.op("act", lambda tbi=tbi: A.activation(out=tab[tbi][:], in_=rr[:], func=AF.Sin, scale=1.0, bias=halfpi), reads=[t_rr, t_cst], writes=[t_tab[tbi]])
                        P.dma("sp", lambda tbi=tbi: nc.sync.dma_start(out=cos_d[:, g * 512:(g + 1) * 512], in_=tab[tbi][:]), reads=[t_tab[tbi]])
            for g in range(NGX):
                rope_group(g)

            def stageA(g):
                gp = g % 2
                for tt in range(4):
                    t = 4 * g + tt
                    xb = t % XB
                    P.dma("sp", lambda t=t, xb=xb: nc.sync.dma_start(out=xt[xb][:], in_=x_d[t * 128:(t + 1) * 128, :]), writes=[t_xt[xb]])
                    P.op("act", lambda xb=xb, tt=tt: A.activation(out=junk[:], in_=xt[xb][:], func=AF.Square, accum_out=ssq[:, gp, tt:tt + 1]),
                         reads=[t_xt[xb]], writes=[t_junk, t_ssq[gp]])
                P.op("dve", lambda: V.tensor_scalar(out=ssq[:, gp, 4:8], in0=ssq[:, gp, 0:4], scalar1=1.0 / D, scalar2=EPS, op0=ALU.mult, op1=ALU.add), reads=[t_ssq[gp]], writes=[t_ssq[gp]])
                P.op("act", lambda: A.activation(out=ssq[:, gp, 4:8], in_=ssq[:, gp, 4:8], func=AF.Sqrt), reads=[t_ssq[gp]], writes=[t_ssq[gp]])
                P.op("dve", lambda: V.reciprocal(out=ssq[:, gp, 4:8], in_=ssq[:, gp, 4:8]), reads=[t_ssq[gp]], writes=[t_ssq[gp]])

            def stageB(g):
                gp = g % 2
                hb = g % 2

                def tile_b(tt):
                    t = 4 * g + tt
                    xb = t % XB
                    nb = t % 2
                    P.op("dve", lambda: V.tensor_scalar(out=xn[nb][:], in0=xt[xb][:], scalar1=ssq[:, gp, 4 + tt:5 + tt], scalar2=None, op0=ALU.mult),
                         reads=[t_xt[xb], t_ssq[gp]], writes=[t_xn[nb]])
                    bk = nb
                    pT = bank[bk][:, :].bitcast(BF16)
                    P.group("pe", [(lambda kc=kc: T.transpose(out=pT[:, kc * 128:(kc + 1) * 128], in_=xn[nb][:, kc * 128:(kc + 1) * 128], identity=identb[:]))
                                   for kc in range(8)], reads=[t_xn[nb], t_cst], writes=[tb[bk]])
                    for kc in range(8):
                        if kc % 2 == 0:
                            P.op("act", lambda kc=kc: A.activation(out=hTg[hb][:, kc, tt * 128:(tt + 1) * 128], in_=pT[:, kc * 128:(kc + 1) * 128],
                                                                   func=AF.Identity, scale=A1[:, kc:kc + 1], bias=S1[:, kc:kc + 1]),
                                 reads=[tb[bk], t_mod], writes=[t_hTg[hb]])
                        else:
                            P.op("dve", lambda kc=kc: V.tensor_scalar(out=hTg[hb][:, kc, tt * 128:(tt + 1) * 128], in0=pT[:, kc * 128:(kc + 1) * 128],
                                                                      scalar1=A1[:, kc:kc + 1], scalar2=S1[:, kc:kc + 1], op0=ALU.mult, op1=ALU.add),
                                 reads=[tb[bk], t_mod], writes=[t_hTg[hb]])
                for tt in range(4):
                    tile_b(tt)
                P.dma("sp", lambda: nc.sync.dma_start(out=hT_v[:, :, g * 512:(g + 1) * 512], in_=hTg[hb][:]), reads=[t_hTg[hb]])

            for g in range(NGX + 1):
                if g < NGX:
                    stageA(g)
                if g >= 1:
                    stageB(g - 1)
            P.barrier()
            P.emit()

        mixed = bufA[:].rearrange("p (a b) -> p a b", a=NOWN)
        with ExitStack() as s2:
            Wu = sbuf(s2, "Wu", [128, 8, 1664], BF16); t_Wu = Tr()
            KT = sbuf(s2, "KT", [128, S], BF16); t_KT = Tr()
            Vb = sbuf(s2, "Vb", [128, 64 * 130], BF16); t_V = Tr()
            QT = sbuf(s2, "QT", [128, 4, NOWN * 128], BF16); t_QT = Tr()
            hTg = [sbuf(s2, "hTg2_%d" % i, [128, 8, 512], BF16) for i in range(2)]; t_hTg = trs(2)
            csg = [sbuf(s2, "csg%d" % i, [128, 2, 512]) for i in range(2)]; t_csg = trs(2)
            tm1 = [sbuf(s2, "tm1_%d" % i, [128, 512]) for i in range(2)]; t_tm1 = trs(2)
            tm2 = [sbuf(s2, "tm2_%d" % i, [128, 512]) for i in range(2)]; t_tm2 = trs(2)
            PT = [sbuf(s2, "PT%d" % i, [128, 512], BF16) for i in range(3)]; t_PT = trs(3)
            dmask = sbuf(s2, "dmask_sb", [128, NOWN, 512], BF16); t_dmask = Tr()
            smask = sbuf(s2, "smask_sb", [128, NOWN, 256], BF16); t_smask = Tr()
            bselT = sbuf(s2, "bselT", [128, NCH]); t_bsel = Tr()
            vbias = sbuf(s2, "vbias", [128, 128]); t_vbias = Tr()
            gsub_b = sbuf(s2, "gsub_b", [128, 128]); t_gsub = Tr()
            fin = sbuf(s2, "fin", [128, 8 * 128]); t_fin = Tr()
            fsm = sbuf(s2, "fsm", [128, 32]); t_fsm = Tr()
            junk2 = sbuf(s2, "junk2", [128, 128], BF16)
            hT_v = hT_d.rearrange("k p t -> p k t")
            wsel_v = wsel_d.rearrange("(k p) n -> p k n", p=128)
            for q4 in range(4):
                P.dma("pool", lambda q4=q4: G_.dma_start(out=dmask[:, 4 * q4:4 * q4 + 4, :], in_=dmask_d[:, q4 * 2048:(q4 + 1) * 2048].rearrange("p (a b) -> p a b", a=4)),
                      writes=[t_dmask])
            for q4 in range(2):
                P.dma("pool", lambda q4=q4: G_.dma_start(out=smask[:, 8 * q4:8 * q4 + 8, :], in_=smask_d[:, q4 * 2048:(q4 + 1) * 2048].rearrange("p (a b) -> p a b", a=8)),
                      writes=[t_smask])
            P.dma("sp", lambda: nc.sync.dma_start(out=bselT[:], in_=bselT_d[:, :]), writes=[t_bsel])
            P.dma("sp", lambda: nc.sync.dma_start(out=gsub_b[:], in_=gsub_d[0:1, :].partition_broadcast(128)), writes=[t_gsub])
            P.op("dve", lambda: V.tensor_scalar(out=gsub_b[:], in0=gsub_b[:], scalar1=0.8, scalar2=None, op0=ALU.mult), reads=[t_gsub], writes=[t_gsub])
            gcount = [0]

            def rope_proj(u, wc, wcs, hb, cb, ccol, ncol, dst, t_dst, par):
                bA, bB = bank[2 * par], bank[2 * par + 1]
                ci = (u["base"] + wc) // 128
                cis = (u["base"] + wcs) // 128
                P.group("pe", [(lambda kc=kc: T.matmul(bA[:, 0:ncol], lhsT=Wu[:, kc, wc:wc + 128], rhs=hTg[hb][:, kc, ccol:ccol + ncol], start=(kc == 0), stop=(kc == 7)))
                               for kc in range(8)], reads=[t_Wu, t_hTg[hb]], writes=[tb[2 * par]])
                P.group("pe", [(lambda kc=kc: T.matmul(bB[:, 0:ncol], lhsT=Wu[:, kc, wcs:wcs + 128], rhs=hTg[hb][:, kc, ccol:ccol + ncol], start=(kc == 0), stop=(kc == 7)))
                               for kc in range(8)], reads=[t_Wu, t_hTg[hb]], writes=[tb[2 * par + 1]])
                P.op("dve", lambda: V.scalar_tensor_tensor(out=tm1[par][:, 0:ncol], in0=bA[:, 0:ncol], scalar=bselT[:, ci:ci + 1], in1=csg[cb][:, 0, ccol:ccol + ncol],
                                                           op0=ALU.add, op1=ALU.mult), reads=[tb[2 * par], t_bsel, t_csg[cb]], writes=[t_tm1[par]])
                P.op("dve", lambda: V.scalar_tensor_tensor(out=tm2[par][:, 0:ncol], in0=bB[:, 0:ncol], scalar=bselT[:, cis:cis + 1], in1=csg[cb][:, 1, ccol:ccol + ncol],
                                                           op0=ALU.add, op1=ALU.mult), reads=[tb[2 * par + 1], t_bsel, t_csg[cb]], writes=[t_tm2[par]])
                P.op("pool", lambda: G_.tensor_tensor(out=dst, in0=tm1[par][:, 0:ncol], in1=tm2[par][:, 0:ncol], op=ALU.add),
                     reads=[t_tm1[par], t_tm2[par]], writes=[t_dst])

            def load_group(g):
                hb = gcount[0] % 2
                gcount[0] += 1
                P.dma("sp", lambda: nc.sync.dma_start(out=hTg[hb][:], in_=hT_v[:, :, g * 512:(g + 1) * 512]), writes=[t_hTg[hb]])
                P.dma("sp", lambda: nc.sync.dma_start(out=csg[hb][:, 0, :], in_=cos_d[:, g * 512:(g + 1) * 512]), writes=[t_csg[hb]])
                P.dma("sp", lambda: nc.sync.dma_start(out=csg[hb][:, 1, :], in_=sin_d[:, g * 512:(g + 1) * 512]), writes=[t_csg[hb]])
                return hb

            pcount = [0]

            def v_proj(u, hb, vt0, vw, swa):
                bk = 4 + (pcount[0] % 2)
                pcount[0] += 1
                ov = u["o_v"]
                fns = []
                for tt in range(4):
                    for kc in range(8):
                        fns.append(lambda tt=tt, kc=kc: T.matmul(bank[bk][:, tt * 128:(tt + 1) * 128], lhsT=hTg[hb][:, kc, tt * 128:(tt + 1) * 128],
                                                                 rhs=Wu[:, kc, ov:ov + 128], start=(kc == 0), stop=(kc == 7)))
                P.group("pe", fns, reads=[t_Wu, t_hTg[hb]], writes=[tb[bk]])
                src = bank[bk][:, :].rearrange("p (a b) -> p a b", a=4)
                vb_b = vbias[:].unsqueeze(1).to_broadcast([128, 4, 128])
                if not swa:
                    dst = Vb[:, vt0 * 129:(vt0 + 4) * 129].rearrange("p (a b) -> p a b", a=4)[:, :, 0:128]
                    P.op("dve", lambda: V.tensor_tensor(out=dst, in0=src, in1=vb_b, op=ALU.add), reads=[tb[bk], t_vbias], writes=[t_V])
                else:
                    for kv in range(2):
                        dst = Vb[:, vt0 * 130:(vt0 + 4) * 130].rearrange("p (a b) -> p a b", a=4)[:, :, kv * 65:kv * 65 + 64]
                        P.op("dve", lambda dst=dst, kv=kv: V.tensor_tensor(out=dst, in0=src[:, :, kv * 64:(kv + 1) * 64],
                                                                          in1=vbias[:, kv * 64:(kv + 1) * 64].unsqueeze(1).to_broadcast([128, 4, 64]), op=ALU.add),
                             reads=[tb[bk], t_vbias], writes=[t_V])

            for ui, u in enumerate(UNITS):
                swa = (ui == 0)
                nc_u = u["ncols"]
                P.dma("pool", lambda u=u, nc_u=nc_u: G_.dma_start(out=Wu[:, :, 0:nc_u], in_=wsel_v[:, :, u["base"]:u["base"] + nc_u]), writes=[t_Wu])
                P.dma("sp", lambda u=u: nc.sync.dma_start(out=vbias[:], in_=bsel_d[0:1, u["base"] + u["o_v"]:u["base"] + u["o_v"] + 128].partition_broadcast(128)),
                      writes=[t_vbias])
                if swa:
                    vv = Vb[:, 0:32 * 130].rearrange("p (a b) -> p a b", a=32)
                    P.op("pool", lambda vv=vv: G_.memset(vv[:, :, 64:65], 1.0), writes=[t_V])
                    P.op("pool", lambda vv=vv: G_.memset(vv[:, :, 129:130], 1.0), writes=[t_V])
                    kv_groups = [(20 + i, i * 512, 4 * i) for i in range(4)] + [(16 + i, 2048 + i * 512, 16 + 4 * i) for i in range(4)]
                elif ui == 1:
                    vv = Vb[:, 0:64 * 129].rearrange("p (a b) -> p a b", a=64)
                    P.op("pool", lambda vv=vv: G_.memset(vv[:, :, 128:129], 1.0), writes=[t_V])
                    kv_groups = [(g, g * 512, 4 * g) for g in range(NG)]
                else:
                    kv_groups = [(g, g * 512, 4 * g) for g in range(NG)]
                par = 0
                for (g, kcol, vt0) in kv_groups:
                    hb = load_group(g)
                    for kc_ in range(u["nk"]):
                        rope_proj(u, u["o_k"] + kc_ * 128, u["o_ks"] + kc_ * 128, hb, hb, 0, 512, KT[:, kc_ * 4096 + kcol:kc_ * 4096 + kcol + 512], t_KT, par)
                        par ^= 1
                    v_proj(u, hb, vt0, None, swa)
                    if swa and g < 20:
                        for qc in range(4):
                            rope_proj(u, u["o_q"] + qc * 128, u["o_qs"] + qc * 128, hb, hb, 0, 512, QT[:, qc, (g - 16) * 512:(g - 15) * 512], t_QT, par)
                            par ^= 1
                if not swa:
                    for g in range(16, 20):
                        hb = load_group(g)
                        rope_proj(u, u["o_q"], u["o_qs"], hb, hb, 0, 512, QT[:, 0, (g - 16) * 512:(g - 15) * 512], t_QT, par)
                        par ^= 1

                items = []
                if swa:
                    for oi in range(NOWN):
                        for hh in range(8):
                            items.append((oi, hh, 0, True))
                else:
                    for oi in range(NOWN):
                        nkb = 8 * (oi // 2) + (4 if oi % 2 == 0 else 8)
                        for m in range(2):
                            for c in range(nkb // 4):
                                items.append((oi, m, c, c == nkb // 4 - 1))

                def qk(n):
                    oi, a, c, last = items[n]
                    sb_ = n % 3
                    if swa:
                        hh = a; half = hh % 2; qc = hh // 2; kvg = hh // 4
                        ps = slice(half * 64, half * 64 + 64)
                        fns = [lambda: T.matmul(bank[sb_][:, 0:128], lhsT=KT[ps, kvg * 4096 + oi * 128:kvg * 4096 + (oi + 1) * 128], rhs=QT[ps, qc, oi * 128:(oi + 1) * 128], start=True, stop=True),
                               lambda: T.matmul(bank[sb_][:, 128:256], lhsT=KT[ps, kvg * 4096 + 2048 + oi * 128:kvg * 4096 + 2048 + (oi + 1) * 128], rhs=QT[ps, qc, oi * 128:(oi + 1) * 128], start=True, stop=True)]
                        ncol = 256
                        mk = smask[:, oi, :]
                        t_mk = t_smask
                    else:
                        m = a
                        ps = slice(m * 64, m * 64 + 64)
                        fns = [(lambda i=i: T.matmul(bank[sb_][:, i * 128:(i + 1) * 128], lhsT=KT[ps, (4 * c + i) * 128:(4 * c + i + 1) * 128],
                                                     rhs=QT[ps, 0, oi * 128:(oi + 1) * 128], start=True, stop=True)) for i in range(4)]
                        ncol = 512
                        mk = dmask[:, oi, :]
                        t_mk = t_dmask
                    P.group("pe", fns, reads=[t_KT, t_QT], writes=[tb[sb_]])
                    P.op("act", lambda: A.activation(out=PT[sb_][:, 0:ncol], in_=bank[sb_][:, 0:ncol], func=AF.Exp, scale=0.125), reads=[tb[sb_]], writes=[t_PT[sb_]])
                    if last:
                        P.op("pool", lambda: G_.tensor_tensor(out=PT[sb_][:, 0:ncol], in0=PT[sb_][:, 0:ncol], in1=mk, op=ALU.mult), reads=[t_PT[sb_], t_mk], writes=[t_PT[sb_]])

                def pv(n):
                    oi, a, c, last = items[n]
                    sb_ = n % 3
                    if swa:
                        hh = a; kvg = hh // 4
                        ob = 3 + (oi % 2) * 2 + (hh // 4)
                        oc = (hh % 4) * 65
                        fns = [lambda: T.matmul(bank[ob][:, oc:oc + 65], lhsT=PT[sb_][:, 0:128], rhs=Vb[:, oi * 130 + kvg * 65: oi * 130 + kvg * 65 + 65], start=True, stop=False),
                               lambda: T.matmul(bank[ob][:, oc:oc + 65], lhsT=PT[sb_][:, 128:256], rhs=Vb[:, (16 + oi) * 130 + kvg * 65: (16 + oi) * 130 + kvg * 65 + 65], start=False, stop=True)]
                    else:
                        m = a
                        ob = 3 + (oi % 2) * 2 + m
                        fns = [(lambda i=i: T.matmul(bank[ob][:, 0:129], lhsT=PT[sb_][:, i * 128:(i + 1) * 128], rhs=Vb[:, (4 * c + i) * 129:(4 * c + i + 1) * 129],
                                                     start=(c == 0 and i == 0), stop=(last and i == 3))) for i in range(4)]
                    P.group("pe", fns, reads=[t_PT[sb_], t_V], writes=[tb[ob]])
                    if swa and a == 7:
                        for hh in range(8):
                            ob2 = 3 + (oi % 2) * 2 + (hh // 4)
                            oc2 = (hh % 4) * 65
                            P.op("dve", lambda hh=hh, ob2=ob2, oc2=oc2: V.tensor_tensor(out=fsm[:, hh:hh + 1], in0=bank[ob2][:, oc2 + 64:oc2 + 65], in1=expsink[:, hh:hh + 1], op=ALU.add),
                                 reads=[tb[ob2], t_small], writes=[t_fsm])
                        P.op("dve", lambda: V.reciprocal(out=fsm[:, 0:8], in_=fsm[:, 0:8]), reads=[t_fsm], writes=[t_fsm])
                        for hh in range(8):
                            ob2 = 3 + (oi % 2) * 2 + (hh // 4)
                            oc2 = (hh % 4) * 65
                            P.op("dve", lambda hh=hh, ob2=ob2, oc2=oc2: V.tensor_scalar(out=mixed[:, oi, hh * 64:(hh + 1) * 64], in0=bank[ob2][:, oc2:oc2 + 64],
                                                                                         scalar1=fsm[:, hh:hh + 1], scalar2=None, op0=ALU.mult),
                                 reads=[tb[ob2], t_fsm], writes=[t_mixed[oi]])
                    if (not swa) and a == 1 and last:
                        h = ui - 1
                        o0 = bank[3 + (oi % 2) * 2]
                        o1 = bank[3 + (oi % 2) * 2 + 1]
                        t0, t1 = tb[3 + (oi % 2) * 2], tb[3 + (oi % 2) * 2 + 1]
                        P.op("dve", lambda: V.reciprocal(out=fsm[:, 16:17], in_=o0[:, 128:129]), reads=[t0], writes=[t_fsm])
                        P.op("dve", lambda: V.reciprocal(out=fsm[:, 17:18], in_=o1[:, 128:129]), reads=[t1], writes=[t_fsm])
                        P.op("dve", lambda: V.tensor_tensor(out=fsm[:, 17:18], in0=fsm[:, 17:18], in1=neglam, op=ALU.mult), reads=[t_fsm, t_small], writes=[t_fsm])
                        P.op("dve", lambda: V.tensor_scalar(out=fin[:, 0:128], in0=o1[:, 0:128], scalar1=fsm[:, 17:18], scalar2=None, op0=ALU.mult), reads=[t1, t_fsm], writes=[t_fin])
                        P.op("dve", lambda: V.scalar_tensor_tensor(out=fin[:, 128:256], in0=o0[:, 0:128], scalar=fsm[:, 16:17], in1=fin[:, 0:128], op0=ALU.mult, op1=ALU.add),
                             reads=[t0, t_fsm, t_fin], writes=[t_fin])
                        P.op("act", lambda: A.activation(out=junk2[:], in_=fin[:, 128:256], func=AF.Square, accum_out=fsm[:, 18:19]), reads=[t_fin], writes=[t_fsm])
                        P.op("dve", lambda: V.tensor_scalar(out=fsm[:, 18:19], in0=fsm[:, 18:19], scalar1=1.0 / 128, scalar2=EPS, op0=ALU.mult, op1=ALU.add), reads=[t_fsm], writes=[t_fsm])
                        P.op("act", lambda: A.activation(out=fsm[:, 18:19], in_=fsm[:, 18:19], func=AF.Sqrt), reads=[t_fsm], writes=[t_fsm])
                        P.op("dve", lambda: V.reciprocal(out=fsm[:, 18:19], in_=fsm[:, 18:19]), reads=[t_fsm], writes=[t_fsm])
                        P.op("dve", lambda: V.scalar_tensor_tensor(out=mixed[:, oi, 512 + h * 128:512 + (h + 1) * 128], in0=fin[:, 128:256], scalar=fsm[:, 18:19], in1=gsub_b[:],
                                                                   op0=ALU.mult, op1=ALU.mult), reads=[t_fin, t_fsm, t_gsub], writes=[t_mixed[oi]])

                LAG = 2
                for n in range(len(items) + LAG):
                    if n < len(items):
                        qk(n)
                    if n >= LAG:
                        pv(n - LAG)
            P.barrier()
            P.emit()

        s34 = st.enter_context(ExitStack())
        dest_i = sbuf(s34, "dest_i", [128, 4 * NOWN], I32); t_dest = trs(NOWN)
        gate4 = sbuf(s34, "gate4", [128, 4 * NOWN]); t_gate4 = trs(NOWN)
        maskb = sbuf(s34, "maskb", [128, NOWN, NE], BF16); t_maskb = trs(NOWN)
        cnt_run = sbuf(s34, "cnt_run", [128, NE]); t_cnt = Tr()
        cnt_i = sbuf(s34, "cnt_i", [1, NE], I32); t_cnti = Tr()
        padidx = sbuf(s34, "padidx", [128, NE], I32); t_pad = Tr()
        t_xbuf = Tr()
        iota32 = cst[:, 648:680]
        e2048 = cst[:, 680:712]
        iota_p = cst[:, 712:713]
        with ExitStack() as s3:
            gt1_b = sbuf(s3, "gt1_b", [128, D])
            A2b = sbuf(s3, "A2b", [128, D]); S2b = sbuf(s3, "S2b", [128, D]); t_m2 = Tr()
            P.dma("sp", lambda: nc.sync.dma_start(out=gt1_b[:], in_=mod_d[0:1, 2 * D:3 * D].partition_broadcast(128)), writes=[t_mod])
            P.dma("sp", lambda: nc.sync.dma_start(out=S2b[:], in_=mod_d[0:1, 3 * D:4 * D].partition_broadcast(128)), writes=[t_m2])
            P.dma("sp", lambda: nc.sync.dma_start(out=A2b[:], in_=mod_d[0:1, 4 * D:5 * D].partition_broadcast(128)), writes=[t_m2])
            wout = sbuf(s3, "wout", [128, 8, D], BF16); t_wout = Tr()
            boutb = sbuf(s3, "boutb", [1, D], BF16)
            wr = sbuf(s3, "wr", [128, 8, NE], BF16); t_wr = Tr()
            brb = sbuf(s3, "brb", [1, NE], BF16)
            gfb = sbuf(s3, "gfb", [128, D]); t_gfb = Tr()
            P.dma("sp", lambda: nc.sync.dma_start(out=gfb[:], in_=gffn_d[0:1, :].partition_broadcast(128)), writes=[t_gfb])
            P.op("dve", lambda: V.scalar_tensor_tensor(out=A2b[:], in0=A2b[:], scalar=1.0, in1=gfb[:], op0=ALU.add, op1=ALU.mult), reads=[t_m2, t_gfb], writes=[t_m2])
            P.op("dve", lambda: V.memset(cnt_run[:], 0.0), writes=[t_cnt])
            mixT = [sbuf(s3, "mixT%d" % i, [128, 8, 128], BF16) for i in range(2)]; t_mixT = trs(2)
            xo = [sbuf(s3, "xo%d" % i, [128, D]) for i in range(2)]; t_xo = trs(2)
            x1t = [sbuf(s3, "x1t%d" % i, [128, D]) for i in range(2)]; t_x1t = trs(2)
            h2f = [sbuf(s3, "h2f%d" % i, [128, D]) for i in range(2)]; t_h2f = trs(2)
            h2tok = [sbuf(s3, "h2tok%d" % i, [128, D], BF16) for i in range(2)]; t_h2tok = trs(2)
            h2Tt = [sbuf(s3, "h2Tt%d" % i, [128, 8, 128], BF16) for i in range(2)]; t_h2Tt = trs(2)
            zrow = sbuf(s3, "zrow", [128, D], BF16); t_zrow = Tr()
            junk3 = sbuf(s3, "junk3", [128, D], BF16); t_junk3 = Tr()
            rs = sbuf(s3, "rs", [128, 64]); t_rs = trs(2)
            lg = sbuf(s3, "lg", [128, 2, 4 * NE]); t_lg = trs(2)
            idx8 = sbuf(s3, "idx8", [128, 2, 8], U32)
            posb = sbuf(s3, "posb", [128, 2, NE]); junkp = sbuf(s3, "junkp", [128, 2, NE])
            wout_v = wout_d.rearrange("(k p) n -> p k n", p=128)
            wr_v = wr_d.rearrange("(k p) n -> p k n", p=128)
            P.dma("pool", lambda: G_.dma_start(out=wout[:], in_=wout_v), writes=[t_wout])
            P.dma("pool", lambda: G_.dma_start(out=boutb[:], in_=bout_d[:, :]), writes=[t_wout])
            P.dma("pool", lambda: G_.dma_start(out=wr[:], in_=wr_v), writes=[t_wr])
            P.dma("pool", lambda: G_.dma_start(out=brb[:], in_=br_d[:, :]), writes=[t_wr])
            P.op("pool", lambda: G_.memset(zrow[:], 0.0), writes=[t_zrow])

            def p3(oi):
                b = oi % 2
                P.dma("sp", lambda: nc.sync.dma_start(out=xo[b][:], in_=x_d[S + oi * 128:S + (oi + 1) * 128, :]), writes=[t_xo[b]])
                pT = bank[b][:, :].bitcast(BF16)
                P.group("pe", [(lambda kc=kc: T.transpose(out=pT[:, kc * 128:(kc + 1) * 128], in_=mixed[:, oi, kc * 128:(kc + 1) * 128], identity=identb[:])) for kc in range(8)],
                        reads=[t_mixed[oi], t_cst], writes=[tb[b]])
                P.op("act", lambda: A.activation(out=mixT[b][:].rearrange("p a b -> p (a b)"), in_=pT[:, :], func=AF.Copy), reads=[tb[b]], writes=[t_mixT[b]])
                for hf in range(2):
                    bk = 2 + 2 * b + hf
                    fns = [(lambda kc=kc, hf=hf, bk=bk: T.matmul(bank[bk][:, :], lhsT=mixT[b][:, kc, :], rhs=wout[:, kc, hf * 512:(hf + 1) * 512], start=(kc == 0), stop=False)) for kc in range(8)]
                    fns.append(lambda hf=hf, bk=bk: T.matmul(bank[bk][:, :], lhsT=onesb[0:1, :], rhs=boutb[0:1, hf * 512:(hf + 1) * 512], start=False, stop=True))
                    P.group("pe", fns, reads=[t_mixT[b], t_wout, t_cst], writes=[tb[bk]])
                    P.op("dve", lambda hf=hf, bk=bk: V.tensor_tensor(out=x1t[b][:, hf * 512:(hf + 1) * 512], in0=bank[bk][:, :], in1=gt1_b[:, hf * 512:(hf + 1) * 512], op=ALU.mult),
                         reads=[tb[bk], t_mod], writes=[t_x1t[b]])
                P.op("pool", lambda: G_.tensor_tensor(out=x1t[b][:], in0=x1t[b][:], in1=xo[b][:], op=ALU.add), reads=[t_x1t[b], t_xo[b]], writes=[t_x1t[b]])
                P.dma("sp", lambda: nc.sync.dma_start(out=x1_d[oi * 128:(oi + 1) * 128, :], in_=x1t[b][:]), reads=[t_x1t[b]])
                r0 = 32 * b
                P.op("act", lambda: A.activation(out=junk3[:], in_=x1t[b][:], func=AF.Square, accum_out=rs[:, r0:r0 + 1]), reads=[t_x1t[b]], writes=[t_junk3, t_rs[b]])
                P.op("dve", lambda: V.tensor_scalar(out=rs[:, r0 + 1:r0 + 2], in0=rs[:, r0:r0 + 1], scalar1=1.0 / D, scalar2=EPS, op0=ALU.mult, op1=ALU.add), reads=[t_rs[b]], writes=[t_rs[b]])
                P.op("act", lambda: A.activation(out=rs[:, r0 + 1:r0 + 2], in_=rs[:, r0 + 1:r0 + 2], func=AF.Sqrt), reads=[t_rs[b]], writes=[t_rs[b]])
                P.op("dve", lambda: V.reciprocal(out=rs[:, r0 + 1:r0 + 2], in_=rs[:, r0 + 1:r0 + 2]), reads=[t_rs[b]], writes=[t_rs[b]])
                P.op("dve", lambda: V.scalar_tensor_tensor(out=h2f[b][:], in0=x1t[b][:], scalar=rs[:, r0 + 1:r0 + 2], in1=A2b[:], op0=ALU.mult, op1=ALU.mult),
                     reads=[t_x1t[b], t_rs[b], t_m2], writes=[t_h2f[b]])
                P.op("pool", lambda: G_.tensor_tensor(out=h2tok[b][:], in0=h2f[b][:], in1=S2b[:], op=ALU.add), reads=[t_h2f[b], t_m2], writes=[t_h2tok[b]])
                bk = 6 + b
                pT2 = bank[bk][:, :].bitcast(BF16)
                P.group("pe", [(lambda kc=kc: T.transpose(out=pT2[:, kc * 128:(kc + 1) * 128], in_=h2tok[b][:, kc * 128:(kc + 1) * 128], identity=identb[:])) for kc in range(8)],
                        reads=[t_h2tok[b], t_cst], writes=[tb[bk]])
                P.op("act", lambda: A.activation(out=h2Tt[b][:].rearrange("p a b -> p (a b)"), in_=pT2[:, :], func=AF.Copy), reads=[tb[bk]], writes=[t_h2Tt[b]])
                fns = [(lambda kc=kc: T.matmul(bank[b][:, 0:NE], lhsT=h2Tt[b][:, kc, :], rhs=wr[:, kc, :], start=(kc == 0), stop=False)) for kc in range(8)]
                fns.append(lambda: T.matmul(bank[b][:, 0:NE], lhsT=onesb[0:1, :], rhs=brb[0:1, :], start=False, stop=True))
                P.group("pe", fns, reads=[t_h2Tt[b], t_wr, t_cst], writes=[tb[b]])
                L0, L1, L2, L3 = lg[:, b, 0:NE], lg[:, b, NE:NE + 8], lg[:, b, 2 * NE:3 * NE], lg[:, b, 3 * NE:3 * NE + 8]
                P.op("dve", lambda: V.tensor_copy(out=L0, in_=bank[b][:, 0:NE]), reads=[tb[b]], writes=[t_lg[b]])
                P.op("dve", lambda: V.max(out=L1, in_=L0), reads=[t_lg[b]], writes=[t_lg[b]])
                P.op("dve", lambda: V.max_index(out=idx8[:, b, :], in_max=L1, in_values=L0), reads=[t_lg[b]], writes=[t_lg[b]])
                P.op("dve", lambda: V.tensor_scalar(out=maskb[:, oi, :], in0=L0, scalar1=lg[:, b, NE + 3:NE + 4], scalar2=None, op0=ALU.is_ge), reads=[t_lg[b]], writes=[t_maskb[oi]])
                P.op("dve", lambda: V.tensor_scalar(out=rs[:, r0 + 2:r0 + 3], in0=lg[:, b, NE:NE + 1], scalar1=-1.0, scalar2=None, op0=ALU.mult), reads=[t_lg[b]], writes=[t_rs[b]])
                P.op("act", lambda: A.activation(out=L3[:, 0:4], in_=L1[:, 0:4], func=AF.Exp, bias=rs[:, r0 + 2:r0 + 3], scale=1.0, accum_out=rs[:, r0 + 3:r0 + 4]),
                     reads=[t_lg[b], t_rs[b]], writes=[t_lg[b], t_rs[b]])
                P.op("dve", lambda: V.reciprocal(out=rs[:, r0 + 3:r0 + 4], in_=rs[:, r0 + 3:r0 + 4]), reads=[t_rs[b]], writes=[t_rs[b]])
                P.op("dve", lambda: V.tensor_scalar(out=gate4[:, 4 * oi:4 * oi + 4], in0=L3[:, 0:4], scalar1=rs[:, r0 + 3:r0 + 4], scalar2=None, op0=ALU.mult),
                     reads=[t_lg[b], t_rs[b]], writes=[t_gate4[oi]])
                pb = bank[b]
                P.group("pe", [lambda: T.matmul(pb[:, 64:64 + NE], lhsT=trib[:], rhs=maskb[:, oi, :], start=True, stop=True),
                               lambda: T.matmul(pb[:, 128:128 + NE], lhsT=ones128b[:], rhs=maskb[:, oi, :], start=True, stop=True)],
                        reads=[t_maskb[oi], t_cst, t_lg[b]], writes=[tb[b]])
                P.op("dve", lambda: V.tensor_tensor(out=posb[:, b, :], in0=pb[:, 64:64 + NE], in1=cnt_run[:], op=ALU.add), reads=[tb[b], t_cnt], writes=[t_lg[b]])
                P.op("dve", lambda: V.tensor_tensor(out=cnt_run[:], in0=pb[:, 128:128 + NE], in1=cnt_run[:], op=ALU.add), reads=[tb[b], t_cnt, t_lg[b]], writes=[t_cnt])
                EK = rs[:, r0 + 8:r0 + 12]; PK = rs[:, r0 + 12:r0 + 16]; DF = rs[:, r0 + 16:r0 + 20]
                P.op("dve", lambda: V.tensor_copy(out=EK, in_=idx8[:, b, 0:4]), reads=[t_lg[b]], writes=[t_rs[b]])
                for k in range(4):
                    P.op("dve", lambda k=k: V.scalar_tensor_tensor(out=junkp[:, b, :], in0=iota32, scalar=rs[:, r0 + 8 + k:r0 + 9 + k], in1=posb[:, b, :],
                                                                   op0=ALU.is_equal, op1=ALU.mult, accum_out=rs[:, r0 + 12 + k:r0 + 13 + k]),
                         reads=[t_lg[b], t_rs[b], t_cst], writes=[t_rs[b]])
                P.op("dve", lambda: V.scalar_tensor_tensor(out=DF, in0=EK, scalar=float(CAP), in1=PK, op0=ALU.mult, op1=ALU.add), reads=[t_rs[b]], writes=[t_rs[b]])
                P.op("dve", lambda: V.tensor_copy(out=dest_i[:, 4 * oi:4 * oi + 4], in_=DF), reads=[t_rs[b]], writes=[t_dest[oi]])
                for k in range(4):
                    P.dma("pool", lambda k=k: G_.indirect_dma_start(out=xbuf_d[:, :], out_offset=bass.IndirectOffsetOnAxis(ap=dest_i[:, 4 * oi + k:4 * oi + k + 1], axis=0),
                                                                    in_=h2tok[b][:, :], in_offset=None),
                          reads=[t_h2tok[b], t_dest[oi]], writes=[t_xbuf])
            for oi in range(NOWN):
                p3(oi)
            cf = rs[0:1, 0:NE]
            P.op("dve", lambda: V.tensor_scalar(out=cf, in0=cnt_run[0:1, :], scalar1=127.0, scalar2=1.0 / 128, op0=ALU.add, op1=ALU.mult), reads=[t_cnt] + t_rs, writes=t_rs)
            P.op("dve", lambda: V.tensor_scalar(out=cf, in0=cf, scalar1=-0.496, scalar2=None, op0=ALU.add), reads=t_rs, writes=t_rs)
            P.op("dve", lambda: V.tensor_copy(out=cnt_i[:], in_=cf), reads=t_rs, writes=[t_cnti])
            P.op("dve", lambda: V.tensor_scalar(out=posb[:, 0, :], in0=cnt_run[:], scalar1=iota_p, scalar2=None, op0=ALU.add), reads=[t_cnt, t_cst] + t_lg, writes=t_lg)
            P.op("dve", lambda: V.tensor_scalar(out=posb[:, 1, :], in0=posb[:, 0, :], scalar1=float(CAP), scalar2=None, op0=ALU.is_ge), reads=t_lg, writes=t_lg)
            P.op("dve", lambda: V.tensor_tensor(out=posb[:, 0, :], in0=posb[:, 0, :], in1=e2048, op=ALU.add), reads=t_lg + [t_cst], writes=t_lg)
            P.op("dve", lambda: V.tensor_scalar(out=rs[:, 40:41], in0=iota_p, scalar1=float(NE * CAP), scalar2=None, op0=ALU.add), reads=[t_cst] + t_rs, writes=t_rs)
            P.op("dve", lambda: V.tensor_scalar(out=junkp[:, 0, :], in0=posb[:, 0, :], scalar1=-1.0, scalar2=rs[:, 40:41], op0=ALU.mult, op1=ALU.add), reads=t_lg + t_rs, writes=t_lg)
            P.op("dve", lambda: V.tensor_tensor(out=junkp[:, 0, :], in0=junkp[:, 0, :], in1=posb[:, 1, :], op=ALU.mult), reads=t_lg, writes=t_lg)
            P.op("dve", lambda: V.tensor_tensor(out=posb[:, 0, :], in0=posb[:, 0, :], in1=junkp[:, 0, :], op=ALU.add), reads=t_lg, writes=t_lg)
            P.op("dve", lambda: V.tensor_copy(out=padidx[:], in_=posb[:, 0, :]), reads=t_lg, writes=[t_pad])
            for e in range(NE):
                P.dma("pool", lambda e=e: G_.indirect_dma_start(out=xbuf_d[:, :], out_offset=bass.IndirectOffsetOnAxis(ap=padidx[:, e:e + 1], axis=0),
                                                                in_=zrow[:, :], in_offset=None),
                      reads=[t_zrow, t_pad], writes=[t_xbuf])
            P.barrier()
            P.emit()

        if int(os.environ.get('K_STOP', '9')) <= 3:
            return nc
        with ExitStack() as s4:
            w1b = [sbuf(s4, "w1b%d" % i, [128, 8, 2 * D], BF16) for i in range(2)]
            w2b = [sbuf(s4, "w2b%d" % i, [128, 8, D], BF16) for i in range(2)]
            b1r = [sbuf(s4, "b1r%d" % i, [1, 2 * D], BF16) for i in range(2)]
            b2r = [sbuf(s4, "b2r%d" % i, [1, D], BF16) for i in range(2)]
            t_w = trs(2)
            Xtok = [sbuf(s4, "Xtok%d" % i, [128, D], BF16) for i in range(2)]; t_Xtok = trs(2)
            XT = [sbuf(s4, "XT%d" % i, [128, 8, 128], BF16) for i in range(2)]; t_XT = trs(2)
            gg = [sbuf(s4, "gg%d" % i, [128, 256]) for i in range(2)]; t_gg = trs(2)
            sg = [sbuf(s4, "sg%d" % i, [128, 256]) for i in range(2)]; t_sg = trs(2)
            ll = [sbuf(s4, "ll%d" % i, [128, 256]) for i in range(2)]; t_ll = trs(2)
            atok = [sbuf(s4, "atok%d" % i, [128, D], BF16) for i in range(2)]; t_atok = trs(2)
            aT = [sbuf(s4, "aT%d" % i, [128, 8, 128], BF16) for i in range(2)]; t_aT = trs(2)
            yt = [sbuf(s4, "yt%d" % i, [128, D]) for i in range(2)]; t_yt = trs(2)
            t_ybuf = Tr()
            w1_v = w1_d.rearrange("e (k p) n -> e p k n", p=128)
            w2_v = w2_d.rearrange("e (k p) n -> e p k n", p=128)
            blk = [0]

            def block_body(e, bslot, ws):
                n = blk[0]
                blk[0] += 1
                xb = n % 2
                row0 = e * CAP + bslot * 128
                P.dma("sp", lambda: nc.sync.dma_start(out=Xtok[xb][:], in_=xbuf_d[row0:row0 + 128, :]), reads=[t_xbuf], writes=[t_Xtok[xb]], slot=xb)
                pT = bank[xb][:, :].bitcast(BF16)
                P.group("pe", [(lambda kc=kc: T.transpose(out=pT[:, kc * 128:(kc + 1) * 128], in_=Xtok[xb][:, kc * 128:(kc + 1) * 128], identity=identb[:])) for kc in range(8)],
                        reads=[t_Xtok[xb], t_cst], writes=[tb[xb]])
                P.op("act", lambda: A.activation(out=XT[xb][:].rearrange("p a b -> p (a b)"), in_=pT[:, :], func=AF.Copy), reads=[tb[xb]], writes=[t_XT[xb]])
                def do_cch(cch):
                    bk = 2 + (cch % 2)
                    par = cch % 2
                    fns = [(lambda kc=kc: T.matmul(bank[bk][:, :], lhsT=XT[xb][:, kc, :], rhs=w1b[ws][:, kc, cch * 512:(cch + 1) * 512], start=(kc == 0), stop=False)) for kc in range(8)]
                    fns.append(lambda: T.matmul(bank[bk][:, :], lhsT=onesb[0:1, :], rhs=b1r[ws][0:1, cch * 512:(cch + 1) * 512], start=False, stop=True))
                    P.group("pe", fns, reads=[t_XT[xb], t_w[ws], t_cst], writes=[tb[bk]])
                    P.op("dve", lambda: V.tensor_scalar(out=gg[par][:], in0=bank[bk][:, 0:512:2], scalar1=7.0, scalar2=None, op0=ALU.min), reads=[tb[bk]], writes=[t_gg[par]])
                    P.op("act", lambda: A.activation(out=sg[par][:], in_=gg[par][:], func=AF.Gelu_apprx_sigmoid), reads=[t_gg[par]], writes=[t_sg[par]])
                    P.op("dve", lambda: V.tensor_scalar(out=ll[par][:], in0=bank[bk][:, 1:512:2], scalar1=7.0, scalar2=-7.0, op0=ALU.min, op1=ALU.max), reads=[tb[bk]], writes=[t_ll[par]])
                    P.op("dve", lambda: V.scalar_tensor_tensor(out=atok[xb][:, cch * 256:(cch + 1) * 256], in0=ll[par][:], scalar=1.0, in1=sg[par][:], op0=ALU.add, op1=ALU.mult),
                         reads=[t_ll[par], t_sg[par]], writes=[t_atok[xb]])
                for cch in range(4):
                    do_cch(cch)
                bk = 4 + xb
                pT2 = bank[bk][:, :].bitcast(BF16)
                P.group("pe", [(lambda j=j: T.transpose(out=pT2[:, j * 128:(j + 1) * 128], in_=atok[xb][:, j * 128:(j + 1) * 128], identity=identb[:])) for j in range(8)],
                        reads=[t_atok[xb], t_cst], writes=[tb[bk]])
                P.op("act", lambda: A.activation(out=aT[xb][:].rearrange("p a b -> p (a b)"), in_=pT2[:, :], func=AF.Copy), reads=[tb[bk]], writes=[t_aT[xb]])
                def do_hf(hf):
                    bk2 = 6 + hf
                    fns = [(lambda j=j: T.matmul(bank[bk2][:, :], lhsT=aT[xb][:, j, :], rhs=w2b[ws][:, j, hf * 512:(hf + 1) * 512], start=(j == 0), stop=False)) for j in range(8)]
                    fns.append(lambda: T.matmul(bank[bk2][:, :], lhsT=onesb[0:1, :], rhs=b2r[ws][0:1, hf * 512:(hf + 1) * 512], start=False, stop=True))
                    P.group("pe", fns, reads=[t_aT[xb], t_w[ws], t_cst], writes=[tb[bk2]])
                    if hf == 0:
                        P.op("act", lambda: A.activation(out=yt[xb][:, 0:512], in_=bank[bk2][:, :], func=AF.Copy), reads=[tb[bk2]], writes=[t_yt[xb]])
                    else:
                        P.op("dve", lambda: V.tensor_copy(out=yt[xb][:, 512:1024], in_=bank[bk2][:, :]), reads=[tb[bk2]], writes=[t_yt[xb]])
                for hf in range(2):
                    do_hf(hf)
                P.dma("sp", lambda: nc.sync.dma_start(out=ybuf_d[row0:row0 + 128, :], in_=yt[xb][:]), reads=[t_yt[xb]], writes=[t_ybuf], slot=2 + xb)

            for e in range(int(os.environ.get('K_NE', NE))):
                ws = e % 2
                P.dma("pool", lambda e=e, ws=ws: G_.dma_start(out=w1b[ws][:], in_=w1_v[e]), writes=[t_w[ws]])
                P.dma("pool", lambda e=e, ws=ws: G_.dma_start(out=w2b[ws][:], in_=w2_v[e]), writes=[t_w[ws]])
                P.dma("pool", lambda e=e, ws=ws: G_.dma_start(out=b1r[ws][:], in_=b1_d[e:e + 1, :]), writes=[t_w[ws]])
                P.dma("pool", lambda e=e, ws=ws: G_.dma_start(out=b2r[ws][:], in_=b2_d[e:e + 1, :]), writes=[t_w[ws]])
                P.regload(cnt_i[0:1, e:e + 1], reads=[t_cnti])
                NB = CAP // 128
                for bslot in range(NB):
                    if bslot in (3, 6, 10):
                        P.cond_begin(bslot + 1)
                    P.cond_begin(bslot + 1)
                    block_body(e, bslot, ws)
                    P.cond_end()
                for _ in range(3):
                    P.cond_end()
            P.barrier()
            P.emit()

        if int(os.environ.get('K_STOP', '9')) <= 4:
            return nc
        with ExitStack() as s5:
            gt2_b = sbuf(s5, "gt2_b", [128, D]); gfin_b = sbuf(s5, "gfin_b", [128, D]); t_g5 = Tr()
            yk = [sbuf(s5, "yk%d" % i, [128, D]) for i in range(4)]; t_yk = trs(4)
            acc = [sbuf(s5, "acc%d" % i, [128, D]) for i in range(2)]; t_acc = trs(2)
            x1b = [sbuf(s5, "x1b%d" % i, [128, D]) for i in range(2)]; t_x1b = trs(2)
            ob_ = [sbuf(s5, "ob%d" % i, [128, D]) for i in range(2)]; t_ob = trs(2)
            junk5 = sbuf(s5, "junk5", [128, D], BF16); t_junk5 = Tr()
            fs5 = sbuf(s5, "fs5", [128, 8]); t_fs5 = trs(2)
            P.dma("sp", lambda: nc.sync.dma_start(out=gt2_b[:], in_=mod_d[0:1, 5 * D:6 * D].partition_broadcast(128)), writes=[t_g5])
            P.dma("sp", lambda: nc.sync.dma_start(out=gfin_b[:], in_=gfin_d[0:1, :].partition_broadcast(128)), writes=[t_g5])

            def p5(oi):
                b = oi % 2
                P.dma("sp", lambda: nc.sync.dma_start(out=x1b[b][:], in_=x1_d[oi * 128:(oi + 1) * 128, :]), writes=[t_x1b[b]])
                for k in range(4):
                    P.dma("pool", lambda k=k: G_.indirect_dma_start(out=yk[k][:, :], out_offset=None, in_=ybuf_d[:, :],
                                                                    in_offset=bass.IndirectOffsetOnAxis(ap=dest_i[:, 4 * oi + k:4 * oi + k + 1], axis=0),
                                                                    ), reads=[t_dest[oi]], writes=[t_yk[k]])
                    if k == 0:
                        P.op("dve", lambda: V.tensor_scalar(out=acc[b][:], in0=yk[0][:], scalar1=gate4[:, 4 * oi:4 * oi + 1], scalar2=None, op0=ALU.mult),
                             reads=[t_yk[0], t_gate4[oi]], writes=[t_acc[b]])
                    else:
                        P.op("dve", lambda k=k: V.scalar_tensor_tensor(out=acc[b][:], in0=yk[k][:], scalar=gate4[:, 4 * oi + k:4 * oi + k + 1], in1=acc[b][:], op0=ALU.mult, op1=ALU.add),
                             reads=[t_yk[k], t_gate4[oi], t_acc[b]], writes=[t_acc[b]])
                P.op("pool", lambda: G_.tensor_tensor(out=acc[b][:], in0=acc[b][:], in1=gt2_b[:], op=ALU.mult), reads=[t_acc[b], t_g5], writes=[t_acc[b]])
                P.op("pool", lambda: G_.tensor_tensor(out=x1b[b][:], in0=acc[b][:], in1=x1b[b][:], op=ALU.add), reads=[t_acc[b], t_x1b[b]], writes=[t_x1b[b]])
                P.op("act", lambda: A.activation(out=junk5[:], in_=x1b[b][:], func=AF.Square, accum_out=fs5[:, 4 * b:4 * b + 1]), reads=[t_x1b[b]], writes=[t_junk5, t_fs5[b]])
                P.op("dve", lambda: V.tensor_scalar(out=fs5[:, 4 * b + 1:4 * b + 2], in0=fs5[:, 4 * b:4 * b + 1], scalar1=1.0 / D, scalar2=EPS, op0=ALU.mult, op1=ALU.add),
                     reads=[t_fs5[b]], writes=[t_fs5[b]])
                P.op("act", lambda: A.activation(out=fs5[:, 4 * b + 1:4 * b + 2], in_=fs5[:, 4 * b + 1:4 * b + 2], func=AF.Sqrt), reads=[t_fs5[b]], writes=[t_fs5[b]])
                P.op("dve", lambda: V.reciprocal(out=fs5[:, 4 * b + 1:4 * b + 2], in_=fs5[:, 4 * b + 1:4 * b + 2]), reads=[t_fs5[b]], writes=[t_fs5[b]])
                P.op("dve", lambda: V.scalar_tensor_tensor(out=ob_[b][:], in0=x1b[b][:], scalar=fs5[:, 4 * b + 1:4 * b + 2], in1=gfin_b[:], op0=ALU.mult, op1=ALU.mult),
                     reads=[t_x1b[b], t_fs5[b], t_g5], writes=[t_ob[b]])
                P.dma("sp", lambda: nc.sync.dma_start(out=out_d[oi * 128:(oi + 1) * 128, :], in_=ob_[b][:]), reads=[t_ob[b]])
            for oi in range(NOWN):
                p5(oi)
            P.barrier()
            P.emit()
    return nc


def _consts():
    c = np.zeros((128, NCST), np.float32)
    c[:, 0:128] = np.eye(128, dtype=np.float32)
    k = np.arange(128)[:, None]
    q = np.arange(128)[None, :]
    c[:, 128:256] = (k <= q)
    c[:, 256:384] = (k > q)
    inv = (1.0 / (np.float32(10000.0) ** (np.arange(0, 64, 2, dtype=np.float32) / np.float32(64)))).astype(np.float32)
    p = np.arange(128)
    c[:, 384] = inv[p % 32]
    c[:, 385] = np.where((p % 64) < 32, -1.0, 1.0)
    c[:, 386] = np.float32(math.pi / 2)
    c[:, 387] = 0.0
    c[:, 388] = 1.0
    c[:, 392:520] = 1.0
    c[:, 520:648] = (k < q)
    c[:, 648:680] = np.arange(32)[None, :]
    c[:, 680:712] = (np.arange(32) * CAP)[None, :]
    c[:, 712] = np.arange(128)
    return c


def _core_masks(j):
    own = own_blocks(j)
    k = np.arange(128)[:, None]
    q = np.arange(128)[None, :]
    tri = (k <= q).astype(np.float32)
    low = (k > q).astype(np.float32)
    dm = np.zeros((128, NOWN, 4, 128), np.float32)
    sm = np.zeros((128, NOWN, 2, 128), np.float32)
    for oi, gb in enumerate(own):
        nkb = 8 * (oi // 2) + (4 if oi % 2 == 0 else 8)
        for i in range(4):
            kb = nkb - 4 + i
            if kb < gb:
                dm[:, oi, i, :] = 1.0
            elif kb == gb:
                dm[:, oi, i, :] = tri
        sm[:, oi, 1, :] = tri
        if gb > 0:
            sm[:, oi, 0, :] = low
    return dm.reshape(128, NOWN * 512), sm.reshape(128, NOWN * 256)


_NC_CACHE = {}


def kernel(x, c, positions, w_ada, b_ada, g_mix, w_in, b_in, attn_sinks, lambda_q1, lambda_k1, lambda_q2, lambda_k2,
           g_subln, w_out, b_out, g_ffn, w_router, b_router, w1, b1, w2, b2, g_final):
    f = lambda a: np.ascontiguousarray(np.asarray(a))
    x = f(x); positions = f(positions)
    if "nc" not in _NC_CACHE:
        _NC_CACHE["nc"] = build_program()
    nc = _NC_CACHE["nc"]
    colT = lambda v: f(np.asarray(v).reshape(-1, 128).T)
    w_sel = f(np.asarray(w_in)[0][:, SEL])
    b_sel = f(np.asarray(b_in)[0][SEL])
    b1_ = np.asarray(b1)[0]
    shared = {
        "w_ada": f(np.asarray(w_ada)[0]), "b_ada": f(np.asarray(b_ada)[0][None, :]),
        "gmixT": colT(np.asarray(g_mix)[0]), "gffnT": colT(np.asarray(g_ffn)[0]),
        "w_sel": w_sel, "b_selT": colT(b_sel), "b_sel": f(b_sel[None, :]),
        "sinks": f(np.asarray(attn_sinks)[0][None, :]),
        "lam4": f(np.stack([np.asarray(lambda_q1)[0], np.asarray(lambda_k1)[0], np.asarray(lambda_q2)[0], np.asarray(lambda_k2)[0]])),
        "g_subln": f(np.asarray(g_subln)[0][None, :]),
        "w_out": f(np.asarray(w_out)[0]), "b_out": f(np.asarray(b_out)[0][None, :]),
        "w_router": f(np.asarray(w_router)[0]), "b_router": f(np.asarray(b_router)[0][None, :]),
        "w1": f(np.asarray(w1)[0]), "w2": f(np.asarray(w2)[0]), "b2": f(np.asarray(b2)[0]),
        "b1": f(b1_), "g_ffn": f(np.asarray(g_ffn)[0][None, :]),
        "g_final": f(np.asarray(g_final)[None, :]),
        "consts": _consts(),
    }
    in_maps = []
    rows_all = []
    for core in range(8):
        b, j = core // 4, core % 4
        own = own_blocks(j)
        rows_own = np.concatenate([np.arange(g * 128, (g + 1) * 128) for g in own])
        rows_prev = np.concatenate([np.arange(max(g - 1, 0) * 128, (max(g - 1, 0) + 1) * 128) for g in own])
        rows_all.append(rows_own)
        xb = x[b]
        x_ext = np.concatenate([xb, xb[rows_own], xb[rows_prev]], axis=0)
        pb = positions[b]
        pos_ext = np.concatenate([pb, pb[rows_own], pb[rows_prev]])[None, :].astype(np.int32)
        dm, sm = _core_masks(j)
        m = dict(shared)
        m.update({"x": f(x_ext), "pos": f(pos_ext), "cT": colT(np.asarray(c)[b]), "dmask": dm, "smask": sm})
        in_maps.append(m)
    res = run_bass_kernel_spmd(nc, in_maps, core_ids=list(range(8)))
    out = np.zeros((2, S, D), np.float32)
    for core in range(8):
        out[core // 4, rows_all[core], :] = np.asarray(res.results[core]["out"])
    return out
```

```python
import math
import os
from contextlib import ExitStack

import numpy as np
import concourse.bass as bass
import concourse.mybir as mybir
from concourse.bass_utils import run_bass_kernel_spmd

F32 = mybir.dt.float32
BF16 = mybir.dt.bfloat16
I32 = mybir.dt.int32
ALU = mybir.AluOpType
AF = mybir.ActivationFunctionType
AX = mybir.AxisListType

D = 1024
S = 8192
NT = 64
NG = 16
NOWN = 16
NE = 32
SX = S + 2 * NOWN * 128
NGX = SX // 512
NCST = 720
CAP = 2048
U32 = mybir.dt.uint32
EPS = 1e-5
C1 = 6.28125
C2 = 2 * math.pi - 6.28125
INV2PI = float(1.0 / (2 * math.pi))

OFF_QA, OFF_KA, OFF_VA, OFF_QD, OFF_KD, OFF_VD = 0, 512, 640, 768, 1280, 1792


def _swap64(cols):
    cols = np.asarray(cols).reshape(-1, 64)
    return np.concatenate([cols[:, 32:], cols[:, :32]], axis=1).reshape(-1)


def _unit_cols():
    units = []
    k = np.concatenate([np.tile(np.arange(OFF_KA + g * 64, OFF_KA + (g + 1) * 64), 2) for g in range(2)])
    q = np.arange(OFF_QA, OFF_QA + 512)
    v = np.arange(OFF_VA, OFF_VA + 128)
    units.append(dict(nk=2, nq=4, k=k, q=q, v=v))
    for h in range(4):
        k = np.arange(OFF_KD + h * 128, OFF_KD + (h + 1) * 128)
        q = np.arange(OFF_QD + h * 128, OFF_QD + (h + 1) * 128)
        v = np.arange(OFF_VD + h * 128, OFF_VD + (h + 1) * 128)
        units.append(dict(nk=1, nq=1, k=k, q=q, v=v))
    off = 0
    sel = []
    for u in units:
        u["base"] = off
        parts = [u["k"], _swap64(u["k"]), u["q"], _swap64(u["q"]), u["v"]]
        u["o_k"] = 0
        u["o_ks"] = len(u["k"])
        u["o_q"] = u["o_ks"] + len(u["k"])
        u["o_qs"] = u["o_q"] + len(u["q"])
        u["o_v"] = u["o_qs"] + len(u["q"])
        u["ncols"] = u["o_v"] + 128
        sel.append(np.concatenate(parts))
        off += u["ncols"]
    return units, np.concatenate(sel)


UNITS, SEL = _unit_cols()
NSEL = len(SEL)
NCH = NSEL // 128


def own_blocks(j):
    return sorted([8 * m + j for m in range(8)] + [8 * m + 7 - j for m in range(8)])


class Tr:
    __slots__ = ("w", "r")

    def __init__(self):
        self.w = {}
        self.r = {}


def trs(n):
    return [Tr() for _ in range(n)]


class Prog:
    ENG = ("pe", "act", "dve", "pool", "sp")

    def __init__(self, nc, stack, n_dma_sems=48):
        self.nc = nc
        self.q = {e: [] for e in self.ENG}
        self.esem = {e: stack.enter_context(nc.semaphore("s_" + e)) for e in self.ENG}
        self.ecnt = {e: 0 for e in self.ENG}
        self.waited = {e: {} for e in self.ENG}
        self.dsem = [stack.enter_context(nc.semaphore("d%d" % i)) for i in range(n_dma_sems)]
        self.dcnt = [0] * n_dma_sems
        self.dpool = {"sp": list(range(0, n_dma_sems - 16)), "pool": list(range(n_dma_sems - 16, n_dma_sems))}
        self.dnext = {"sp": 0, "pool": 0}
        self.in_cond = False
        self.handles = {"pe": nc.tensor, "act": nc.scalar, "dve": nc.vector, "pool": nc.gpsimd, "sp": nc.sync}

    def _need(self, eng, s, v):
        wd = self.waited[eng]
        if wd.get(s, 0) >= v:
            return
        wd[s] = v
        self.q[eng].append(("wait", s, v))

    def _waits(self, eng, reads, writes):
        need = {}
        for t in reads:
            for s, v in t.w.items():
                if need.get(s, 0) < v:
                    need[s] = v
        for t in writes:
            for s, v in t.w.items():
                if need.get(s, 0) < v:
                    need[s] = v
            for s, v in t.r.items():
                if need.get(s, 0) < v:
                    need[s] = v
        for s, v in need.items():
            if eng == "pe" and s is self.esem["pe"]:
                continue
            self._need(eng, s, v)

    def _record(self, ev, reads, writes):
        s, v = ev
        for t in reads:
            if t.r.get(s, 0) < v:
                t.r[s] = v
        for t in writes:
            if self.in_cond:
                if t.w.get(s, 0) < v:
                    t.w[s] = v
            else:
                t.w = {s: v}
                t.r = {}

    def op(self, eng, fn, reads=(), writes=()):
        self._waits(eng, reads, writes)
        self.ecnt[eng] += 1
        ev = (self.esem[eng], self.ecnt[eng])
        self.q[eng].append(("op", fn, self.esem[eng], 1))
        self._record(ev, reads, writes)

    def group(self, eng, fns, reads=(), writes=()):
        self._waits(eng, reads, writes)
        self.ecnt[eng] += 1
        ev = (self.esem[eng], self.ecnt[eng])
        for f in fns[:-1]:
            self.q[eng].append(("op", f, None, 0))
        self.q[eng].append(("op", fns[-1], self.esem[eng], 1))
        self._record(ev, reads, writes)

    def dma(self, eng, fn, reads=(), writes=(), slot=None):
        pl = self.dpool[eng]
        if slot is None:
            i = pl[self.dnext[eng]]
            self.dnext[eng] = (self.dnext[eng] + 1) % (len(pl) - 4)
        else:
            i = pl[len(pl) - 4 + slot]
        s = self.dsem[i]
        if self.dcnt[i]:
            self._need(eng, s, self.dcnt[i])
        self._waits(eng, reads, writes)
        self.dcnt[i] += 16
        ev = (s, self.dcnt[i])
        self.q[eng].append(("op", fn, s, 16))
        self._record(ev, reads, writes)
        return ev

    CENG = ("pe", "act", "dve", "sp")

    def regload(self, ap, reads=()):
        for e in self.CENG:
            self._waits(e, reads, ())
            self.q[e].append(("regload", ap))

    def cond_begin(self, thr):
        if not hasattr(self, "_cstack"):
            self._cstack = []
        self._cstack.append(({e: self.ecnt[e] for e in self.ENG}, list(self.dcnt), {e: dict(self.waited[e]) for e in self.ENG}))
        self.in_cond = True
        for e in self.CENG:
            self.q[e].append(["if", thr, None])

    def cond_end(self):
        ec0, dc0, wd0 = self._cstack.pop()
        assert self.ecnt["pool"] == ec0["pool"], "pool must stay outside conditional regions"
        dd = [(i, self.dcnt[i] - dc0[i]) for i in range(len(self.dcnt)) if self.dcnt[i] != dc0[i]]
        for i, _ in dd:
            assert i in self.dpool["sp"]
        for e in self.CENG:
            comp = []
            if self.ecnt[e] != ec0[e]:
                comp.append((self.esem[e], self.ecnt[e] - ec0[e]))
            if e == "sp":
                comp += [(self.dsem[i], d, dc0[i]) for i, d in dd]
            for it in reversed(self.q[e]):
                if isinstance(it, list) and it[0] == "if" and it[2] is None:
                    it[2] = comp
                    break
            self.q[e].append(("endif",))
            self.waited[e] = wd0[e]
        self.waited["pool"] = wd0["pool"]
        self.in_cond = bool(self._cstack)

    def barrier(self):
        for e in self.ENG:
            for f in self.ENG:
                if f != e and self.ecnt[f]:
                    self._need(e, self.esem[f], self.ecnt[f])
            for i, s in enumerate(self.dsem):
                if self.dcnt[i]:
                    self._need(e, s, self.dcnt[i])

    def emit(self):
        nc = self.nc
        q = self.q
        self.q = {e: [] for e in self.ENG}
        if not hasattr(self, "regs"):
            self.regs = {}
        with nc.Block() as block:
            def run_items(h, ename, items):
                i = 0
                n = len(items)
                while i < n:
                    it = items[i]
                    k = it[0]
                    if k == "wait":
                        h.wait_ge(it[1], it[2])
                    elif k == "op":
                        ins = it[1]()
                        if it[2] is not None:
                            ins.then_inc(it[2], it[3])
                    elif k == "regload":
                        if ename not in self.regs:
                            self.regs[ename] = h.alloc_register("cnt_" + ename)
                        h.reg_load(self.regs[ename], it[1])
                    elif k == "if":
                        depth = 1
                        j = i + 1
                        while True:
                            if items[j][0] == "if":
                                depth += 1
                            elif items[j][0] == "endif":
                                depth -= 1
                                if depth == 0:
                                    break
                            j += 1
                        body = items[i + 1:j]
                        with h.If_lt(self.regs[ename], it[1]):
                            h.drain()
                            for cp in it[2]:
                                if len(cp) == 3 and cp[2]:
                                    h.wait_ge(cp[0], cp[2])
                                h.sem_inc(cp[0], cp[1])
                        with h.Else():
                            run_items(h, ename, body)
                        i = j
                    i += 1

            def run(ename):
                run_items(self.handles[ename], ename, q[ename])

            @block.tensor
            def _(e):
                run("pe")

            @block.scalar
            def _(e):
                run("act")

            @block.vector
            def _(e):
                run("dve")

            @block.gpsimd
            def _(e):
                run("pool")

            @block.sync
            def _(e):
                run("sp")


def build_program(j_core_unused=None, debug=False):
    nc = bass.Bass("TRN2", target_bir_lowering=False)
    din = lambda name, shape, dt=F32: nc.dram_tensor(name, list(shape), dt, kind="ExternalInput").ap()
    x_d = din("x", [SX, D])
    pos_d = din("pos", [1, SX], I32)
    dmask_d = din("dmask", [128, NOWN * 512])
    smask_d = din("smask", [128, NOWN * 256])
    cT_d = din("cT", [128, 8])
    wada_d = din("w_ada", [D, 6 * D])
    bada_d = din("b_ada", [1, 6 * D])
    gmixT_d = din("gmixT", [128, 8])
    gffnT_d = din("gffnT", [128, 8])
    wsel_d = din("w_sel", [D, NSEL])
    bselT_d = din("b_selT", [128, NCH])
    bsel_d = din("b_sel", [1, NSEL])
    sinks_d = din("sinks", [1, 8])
    lam_d = din("lam4", [4, 64])
    gsub_d = din("g_subln", [1, 128])
    wout_d = din("w_out", [D, D])
    bout_d = din("b_out", [1, D])
    wr_d = din("w_router", [D, NE])
    br_d = din("b_router", [1, NE])
    w1_d = din("w1", [NE, D, 2 * D])
    b1_d = din("b1", [NE, 2 * D])
    gffn_d = din("g_ffn", [1, D])
    gmix_d = din("g_mix", [1, D])
    w2_d = din("w2", [NE, D, D])
    b2_d = din("b2", [NE, D])
    gfin_d = din("g_final", [1, D])
    cst_d = din("consts", [128, NCST])
    out_d = nc.dram_tensor("out", [NOWN * 128, D], F32, kind="ExternalOutput").ap()
    hT_d = nc.dram_tensor("hT_scr", [8, 128, SX], BF16, kind="Internal").ap()
    cos_d = nc.dram_tensor("cos_scr", [128, SX], F32, kind="Internal").ap()
    sin_d = nc.dram_tensor("sin_scr", [128, SX], F32, kind="Internal").ap()
    x1_d = nc.dram_tensor("x1_scr", [NOWN * 128, D], F32, kind="Internal").ap()
    mod_d = nc.dram_tensor("mod_scr", [1, 6 * D], F32, kind="Internal").ap()
    xbuf_d = nc.dram_tensor("xbuf_scr", [NE * CAP + 128, D], BF16, kind="Internal").ap()
    ybuf_d = nc.dram_tensor("ybuf_scr", [NE * CAP, D], F32, kind="Internal").ap()


    with ExitStack() as st:
        P = Prog(nc, st)
        sbuf = lambda stack, name, shape, dt=F32: stack.enter_context(nc.sbuf_tensor(name, list(shape), dt))
        V, A, T, G_ = nc.vector, nc.scalar, nc.tensor, nc.gpsimd

        bank = [st.enter_context(nc.psum_tensor("bank%d" % i, [128, 512], F32)) for i in range(8)]
        tb = trs(8)

        cst = sbuf(st, "cst", [128, NCST]); t_cst = Tr()
        identb = sbuf(st, "identb", [128, 128], BF16)
        mask256 = sbuf(st, "mask256", [128, 256], BF16)
        onesb = sbuf(st, "onesb", [1, 128], BF16)
        trib = sbuf(st, "trib", [128, 128], BF16)
        ones128b = sbuf(st, "ones128b", [128, 128], BF16)
        A1 = sbuf(st, "A1", [128, 8]); S1 = sbuf(st, "S1", [128, 8])
        A2 = sbuf(st, "A2", [128, 8]); S2 = sbuf(st, "S2", [128, 8])
        t_mod = Tr()
        bufA = sbuf(st, "bufA", [128, 16 * 1024], BF16)
        t_mixed = trs(NOWN)
        small = sbuf(st, "small", [128, 64]); t_small = Tr()
        ident = cst[:, 0:128]
        invf = cst[:, 384:385]
        sgn = cst[:, 385:386]
        halfpi = cst[:, 386:387]
        zero_c = cst[:, 387:388]
        one11 = cst[0:1, 388:389]
        ones_row = cst[0:1, 392:520]
        neglam = small[:, 0:1]
        expsink = small[:, 8:16]

        P.dma("sp", lambda: nc.sync.dma_start(out=cst[:], in_=cst_d[:, :]), writes=[t_cst])
        P.op("dve", lambda: V.tensor_copy(out=identb[:], in_=cst[:, 0:128]), reads=[t_cst], writes=[t_cst])
        P.op("dve", lambda: V.tensor_copy(out=mask256[:, 0:128], in_=cst[:, 256:384]), reads=[t_cst], writes=[t_cst])
        P.op("dve", lambda: V.tensor_copy(out=mask256[:, 128:256], in_=cst[:, 128:256]), reads=[t_cst], writes=[t_cst])
        P.op("dve", lambda: V.tensor_copy(out=onesb[:], in_=cst[0:1, 392:520]), reads=[t_cst], writes=[t_cst])
        P.op("dve", lambda: V.tensor_copy(out=trib[:], in_=cst[:, 520:648]), reads=[t_cst], writes=[t_cst])
        P.op("dve", lambda: V.tensor_copy(out=ones128b[:], in_=cst[:, 392:520]), reads=[t_cst], writes=[t_cst])

        with ExitStack() as s0:
            cT = sbuf(s0, "cT_sb", [128, 8]); t_cT = Tr()
            wad = [sbuf(s0, "wad%d" % i, [128, 8, 512]) for i in range(2)]; t_wad = trs(2)
            modrow = sbuf(s0, "modrow", [1, 6 * D]); t_modrow = Tr()
            badar = sbuf(s0, "badar", [1, 6 * D]); t_bada = Tr()
            gT = sbuf(s0, "gT", [128, 16]); t_gT = Tr()
            lamb = sbuf(s0, "lamb", [128, 256]); t_lam = Tr()
            lamp = sbuf(s0, "lamp", [128, 128])
            P.dma("sp", lambda: nc.sync.dma_start(out=cT[:], in_=cT_d[:, :]), writes=[t_cT])
            P.dma("sp", lambda: nc.sync.dma_start(out=badar[:], in_=bada_d[:, :]), writes=[t_bada])
            P.dma("sp", lambda: nc.sync.dma_start(out=gT[:, 0:8], in_=gmixT_d[:, :]), writes=[t_gT])
            P.dma("sp", lambda: nc.sync.dma_start(out=gT[:, 8:16], in_=gffnT_d[:, :]), writes=[t_gT])
            P.dma("sp", lambda: nc.sync.dma_start(out=lamb[:].rearrange("p (a b) -> p a b", a=4),
                                                  in_=lam_d[:, :].partition_broadcast(128)), writes=[t_lam])
            P.dma("sp", lambda: nc.sync.dma_start(out=small[:, 16:24], in_=sinks_d[0:1, :].partition_broadcast(128)), writes=[t_small])
            P.op("act", lambda: A.activation(out=cT[:], in_=cT[:], func=AF.Silu), reads=[t_cT], writes=[t_cT])
            wada_v = wada_d.rearrange("(k p) n -> p k n", p=128)
            for pc in range(12):
                b = pc % 2
                P.dma("sp", lambda pc=pc, b=b: nc.sync.dma_start(out=wad[b][:], in_=wada_v[:, :, pc * 512:(pc + 1) * 512]), writes=[t_wad[b]])
                bk = pc % 2
                P.group("pe", [(lambda kc=kc, b=b, bk=bk: T.matmul(bank[bk][0:1, :], lhsT=cT[:, kc:kc + 1], rhs=wad[b][:, kc, :],
                                                                    start=(kc == 0), stop=(kc == 7))) for kc in range(8)],
                        reads=[t_cT, t_wad[b]], writes=[tb[bk]])
                P.op("dve", lambda pc=pc, bk=bk: V.tensor_tensor(out=modrow[0:1, pc * 512:(pc + 1) * 512], in0=bank[bk][0:1, :],
                                                                 in1=badar[0:1, pc * 512:(pc + 1) * 512], op=ALU.add),
                     reads=[tb[bk], t_bada], writes=[t_modrow])
            cols = [(0, 0), (1, 8), (3, 16), (4, 24)]
            fns = []
            for mi, dc in cols:
                for kc in range(8):
                    fns.append(lambda mi=mi, dc=dc, kc=kc: T.matmul(bank[2][:, dc + kc:dc + kc + 1],
                                                                    lhsT=modrow[0:1, mi * D + kc * 128: mi * D + (kc + 1) * 128],
                                                                    rhs=one11, start=True, stop=True))
            P.group("pe", fns, reads=[t_modrow, t_cst], writes=[tb[2]])
            P.op("dve", lambda: V.tensor_copy(out=S1[:], in_=bank[2][:, 0:8]), reads=[tb[2]], writes=[t_mod])
            P.op("dve", lambda: V.scalar_tensor_tensor(out=A1[:], in0=bank[2][:, 8:16], scalar=1.0, in1=gT[:, 0:8], op0=ALU.add, op1=ALU.mult),
                 reads=[tb[2], t_gT], writes=[t_mod])
            P.op("dve", lambda: V.tensor_copy(out=S2[:], in_=bank[2][:, 16:24]), reads=[tb[2]], writes=[t_mod])
            P.op("dve", lambda: V.scalar_tensor_tensor(out=A2[:], in0=bank[2][:, 24:32], scalar=1.0, in1=gT[:, 8:16], op0=ALU.add, op1=ALU.mult),
                 reads=[tb[2], t_gT], writes=[t_mod])
            P.dma("sp", lambda: nc.sync.dma_start(out=mod_d[:, :], in_=modrow[:]), reads=[t_modrow])
            P.op("dve", lambda: V.tensor_tensor(out=lamp[:, 0:64], in0=lamb[:, 0:64], in1=lamb[:, 64:128], op=ALU.mult), reads=[t_lam], writes=[t_lam])
            P.op("dve", lambda: V.tensor_tensor(out=lamp[:, 64:128], in0=lamb[:, 128:192], in1=lamb[:, 192:256], op=ALU.mult), reads=[t_lam], writes=[t_lam])
            P.op("dve", lambda: V.tensor_reduce(out=small[:, 1:3], in_=lamp[:].rearrange("p (a b) -> p a b", a=2), axis=AX.X, op=ALU.add),
                 reads=[t_lam], writes=[t_small])
            P.op("act", lambda: A.activation(out=small[:, 1:3], in_=small[:, 1:3], func=AF.Exp), reads=[t_small], writes=[t_small])
            P.op("dve", lambda: V.scalar_tensor_tensor(out=small[:, 0:1], in0=small[:, 2:3], scalar=-0.2, in1=small[:, 1:2], op0=ALU.add, op1=ALU.subtract),
                 reads=[t_small], writes=[t_small])
            P.op("act", lambda: A.activation(out=small[:, 8:16], in_=small[:, 16:24], func=AF.Exp), reads=[t_small], writes=[t_small])
            P.barrier()
            P.emit()

        with ExitStack() as s1:
            XB = 8
            xt = [sbuf(s1, "xt%d" % i, [128, D]) for i in range(XB)]; t_xt = trs(XB)
            xn = [sbuf(s1, "xn%d" % i, [128, D], BF16) for i in range(2)]; t_xn = trs(2)
            junk = sbuf(s1, "junk", [128, D], BF16); t_junk = Tr()
            ssq = sbuf(s1, "ssq", [128, 2, 8]); t_ssq = trs(2)
            hTg = [sbuf(s1, "hTg%d" % i, [128, 8, 512], BF16) for i in range(2)]; t_hTg = trs(2)
            posi = sbuf(s1, "posi", [128, 512], I32); t_posi = Tr()
            ang = sbuf(s1, "ang", [128, 512]); t_ang = Tr()
            ki = sbuf(s1, "ki", [128, 512], I32); kf = sbuf(s1, "kf", [128, 512]); t_k = Tr()
            rr = sbuf(s1, "rr", [128, 512]); t_rr = Tr()
            tab = [sbuf(s1, "tab%d" % i, [128, 512]) for i in range(4)]; t_tab = trs(4)
            hT_v = hT_d.rearrange("k p t -> p k t")
            A1b = sbuf(s1, "A1b", [128, D]); S1b = sbuf(s1, "S1b", [128, D]); gmb = sbuf(s1, "gmb", [128, D]); t_m1 = Tr()
            xm = [sbuf(s1, "xm%d" % i, [128, D]) for i in range(2)]; t_xm = trs(2)
            P.dma("sp", lambda: nc.sync.dma_start(out=S1b[:], in_=mod_d[0:1, 0:D].partition_broadcast(128)), writes=[t_m1])
            P.dma("sp", lambda: nc.sync.dma_start(out=A1b[:], in_=mod_d[0:1, D:2 * D].partition_broadcast(128)), writes=[t_m1])
            P.dma("sp", lambda: nc.sync.dma_start(out=gmb[:], in_=gmix_d[0:1, :].partition_broadcast(128)), writes=[t_m1])
            P.op("dve", lambda: V.scalar_tensor_tensor(out=A1b[:], in0=A1b[:], scalar=1.0, in1=gmb[:], op0=ALU.add, op1=ALU.mult), reads=[t_m1], writes=[t_m1])

            def rope_group(g):
                P.dma("sp", lambda: nc.sync.dma_start(out=posi[:], in_=pos_d[0:1, g * 512:(g + 1) * 512].partition_broadcast(128)), writes=[t_posi])
                P.op("dve", lambda: V.tensor_copy(out=ang[:], in_=posi[:]), reads=[t_posi], writes=[t_ang])
                P.op("dve", lambda: V.tensor_scalar(out=ang[:], in0=ang[:], scalar1=invf, scalar2=None, op0=ALU.mult), reads=[t_ang, t_cst], writes=[t_ang])
                for which in range(2):
                    tbi = (2 * g + which) % 4
                    if which == 0:
                        P.op("dve", lambda: V.tensor_scalar(out=ki[:], in0=ang[:], scalar1=INV2PI, scalar2=None, op0=ALU.mult), reads=[t_ang], writes=[t_k])
                    else:
                        P.op("dve", lambda: V.tensor_scalar(out=ki[:], in0=ang[:], scalar1=INV2PI, scalar2=0.25, op0=ALU.mult, op1=ALU.add), reads=[t_ang], writes=[t_k])
                    P.op("dve", lambda: V.tensor_copy(out=kf[:], in_=ki[:]), reads=[t_k], writes=[t_k])
                    P.op("dve", lambda: V.scalar_tensor_tensor(out=rr[:], in0=kf[:], scalar=-C1, in1=ang[:], op0=ALU.mult, op1=ALU.add), reads=[t_k, t_ang], writes=[t_rr])
                    P.op("dve", lambda: V.scalar_tensor_tensor(out=rr[:], in0=kf[:], scalar=-C2, in1=rr[:], op0=ALU.mult, op1=ALU.add), reads=[t_k, t_rr], writes=[t_rr])
                    if which == 0:
                        P.op("dve", lambda: V.tensor_scalar(out=rr[:], in0=rr[:], scalar1=-3.1415925, scalar2=3.1415925, op0=ALU.max, op1=ALU.min), reads=[t_rr], writes=[t_rr])
                        P.op("act", lambda tbi=tbi: A.activation(out=tab[tbi][:], in_=rr[:], func=AF.Sin, scale=sgn, bias=zero_c), reads=[t_rr, t_cst], writes=[t_tab[tbi]])
                        P.dma("sp", lambda tbi=tbi: nc.sync.dma_start(out=sin_d[:, g * 512:(g + 1) * 512], in_=tab[tbi][:]), reads=[t_tab[tbi]])
                    else:
                        P.op("dve", lambda: V.tensor_scalar(out=rr[:], in0=rr[:], scalar1=-4.712388, scalar2=1.570796, op0=ALU.max, op1=ALU.min), reads=[t_rr], writes=[t_rr])
                        P.op("act", lambda tbi=tbi: A.activation(out=tab[tbi][:], in_=rr[:], func=AF.Sin, scale=1.0, bias=halfpi), reads=[t_rr, t_cst], writes=[t_tab[tbi]])
                        P.dma("sp", lambda tbi=tbi: nc.sync.dma_start(out=cos_d[:, g * 512:(g + 1) * 512], in_=tab[tbi][:]), reads=[t_tab[tbi]])
            for g in range(NGX):
                rope_group(g)

            def stageA(g):
                gp = g % 2
                for tt in range(4):
                    t = 4 * g + tt
                    xb = t % XB
                    P.dma("sp", lambda t=t, xb=xb: nc.sync.dma_start(out=xt[xb][:], in_=x_d[t * 128:(t + 1) * 128, :]), writes=[t_xt[xb]])
                    P.op("act", lambda xb=xb, tt=tt: A.activation(out=junk[:], in_=xt[xb][:], func=AF.Square, accum_out=ssq[:, gp, tt:tt + 1]),
                         reads=[t_xt[xb]], writes=[t_junk, t_ssq[gp]])
                P.op("dve", lambda: V.tensor_scalar(out=ssq[:, gp, 4:8], in0=ssq[:, gp, 0:4], scalar1=1.0 / D, scalar2=EPS, op0=ALU.mult, op1=ALU.add), reads=[t_ssq[gp]], writes=[t_ssq[gp]])
                P.op("act", lambda: A.activation(out=ssq[:, gp, 4:8], in_=ssq[:, gp, 4:8], func=AF.Sqrt), reads=[t_ssq[gp]], writes=[t_ssq[gp]])
                P.op("dve", lambda: V.reciprocal(out=ssq[:, gp, 4:8], in_=ssq[:, gp, 4:8]), reads=[t_ssq[gp]], writes=[t_ssq[gp]])

            def stageB(g):
                gp = g % 2
                hb = g % 2

                def tile_b(tt):
                    t = 4 * g + tt
                    xb = t % XB
                    nb = t % 2
                    P.op("dve", lambda: V.scalar_tensor_tensor(out=xm[nb][:], in0=xt[xb][:], scalar=ssq[:, gp, 4 + tt:5 + tt], in1=A1b[:], op0=ALU.mult, op1=ALU.mult),
                         reads=[t_xt[xb], t_ssq[gp], t_m1], writes=[t_xm[nb]])
                    P.op("dve", lambda: V.tensor_tensor(out=xn[nb][:], in0=xm[nb][:], in1=S1b[:], op=ALU.add), reads=[t_xm[nb], t_m1], writes=[t_xn[nb]])
                    bk = nb
                    pT = bank[bk][:, :].bitcast(BF16)
                    P.group("pe", [(lambda kc=kc: T.transpose(out=pT[:, kc * 128:(kc + 1) * 128], in_=xn[nb][:, kc * 128:(kc + 1) * 128], identity=identb[:]))
                                   for kc in range(8)], reads=[t_xn[nb], t_cst], writes=[tb[bk]])
                    P.op("act", lambda: A.activation(out=hTg[hb][:, :, tt * 128:(tt + 1) * 128], in_=pT[:, :].rearrange("p (a b) -> p a b", a=8), func=AF.Copy),
                         reads=[tb[bk]], writes=[t_hTg[hb]])
                for tt in range(4):
                    tile_b(tt)
                P.dma("sp", lambda: nc.sync.dma_start(out=hT_v[:, :, g * 512:(g + 1) * 512], in_=hTg[hb][:]), reads=[t_hTg[hb]])

            for g in range(NGX + 1):
                if g < NGX:
                    stageA(g)
                if g >= 1:
                    stageB(g - 1)
            P.barrier()
            P.emit()

        mixed = bufA[:].rearrange("p (a b) -> p a b", a=NOWN)
        with ExitStack() as s2:
            Wu = sbuf(s2, "Wu", [128, 8, 1664], BF16); t_Wu = Tr()
            KT = sbuf(s2, "KT", [128, S], BF16); t_KT = Tr()
            Vb = sbuf(s2, "Vb", [128, 64 * 130], BF16); t_V = Tr()
            QT = sbuf(s2, "QT", [128, 4, NOWN * 128], BF16); t_QT = Tr()
            hTg = [sbuf(s2, "hTg2_%d" % i, [128, 8, 512], BF16) for i in range(2)]; t_hTg = trs(2)
            csg = [sbuf(s2, "csg%d" % i, [128, 2, 512]) for i in range(2)]; t_csg = trs(2)
            tm1 = [sbuf(s2, "tm1_%d" % i, [128, 512]) for i in range(2)]; t_tm1 = trs(2)
            tm2 = [sbuf(s2, "tm2_%d" % i, [128, 512]) for i in range(2)]; t_tm2 = trs(2)
            PT = [sbuf(s2, "PT%d" % i, [128, 512], BF16) for i in range(3)]; t_PT = trs(3)
            dmask = sbuf(s2, "dmask_sb", [128, NOWN, 512], BF16); t_dmask = Tr()
            smask = sbuf(s2, "smask_sb", [128, NOWN, 256], BF16); t_smask = Tr()
            bselT = sbuf(s2, "bselT", [128, NCH]); t_bsel = Tr()
            vbias = sbuf(s2, "vbias", [128, 128]); t_vbias = Tr()
            gsub_b = sbuf(s2, "gsub_b", [128, 128]); t_gsub = Tr()
            fin = sbuf(s2, "fin", [128, 8 * 128]); t_fin = Tr()
            fsm = sbuf(s2, "fsm", [128, 32]); t_fsm = Tr()
            junk2 = sbuf(s2, "junk2", [128, 128], BF16)
            hT_v = hT_d.rearrange("k p t -> p k t")
            wsel_v = wsel_d.rearrange("(k p) n -> p k n", p=128)
            for q4 in range(4):
                P.dma("pool", lambda q4=q4: G_.dma_start(out=dmask[:, 4 * q4:4 * q4 + 4, :], in_=dmask_d[:, q4 * 2048:(q4 + 1) * 2048].rearrange("p (a b) -> p a b", a=4)),
                      writes=[t_dmask])
            for q4 in range(2):
                P.dma("pool", lambda q4=q4: G_.dma_start(out=smask[:, 8 * q4:8 * q4 + 8, :], in_=smask_d[:, q4 * 2048:(q4 + 1) * 2048].rearrange("p (a b) -> p a b", a=8)),
                      writes=[t_smask])
            P.dma("sp", lambda: nc.sync.dma_start(out=bselT[:], in_=bselT_d[:, :]), writes=[t_bsel])
            P.dma("sp", lambda: nc.sync.dma_start(out=gsub_b[:], in_=gsub_d[0:1, :].partition_broadcast(128)), writes=[t_gsub])
            P.op("dve", lambda: V.tensor_scalar(out=gsub_b[:], in0=gsub_b[:], scalar1=0.8, scalar2=None, op0=ALU.mult), reads=[t_gsub], writes=[t_gsub])
            gcount = [0]

            def rope_proj(u, wc, wcs, hb, cb, ccol, ncol, dst, t_dst, par):
                bA, bB = bank[2 * par], bank[2 * par + 1]
                ci = (u["base"] + wc) // 128
                cis = (u["base"] + wcs) // 128
                P.group("pe", [(lambda kc=kc: T.matmul(bA[:, 0:ncol], lhsT=Wu[:, kc, wc:wc + 128], rhs=hTg[hb][:, kc, ccol:ccol + ncol], start=(kc == 0), stop=(kc == 7)))
                               for kc in range(8)], reads=[t_Wu, t_hTg[hb]], writes=[tb[2 * par]])
                P.group("pe", [(lambda kc=kc: T.matmul(bB[:, 0:ncol], lhsT=Wu[:, kc, wcs:wcs + 128], rhs=hTg[hb][:, kc, ccol:ccol + ncol], start=(kc == 0), stop=(kc == 7)))
                               for kc in range(8)], reads=[t_Wu, t_hTg[hb]], writes=[tb[2 * par + 1]])
                P.op("dve", lambda: V.scalar_tensor_tensor(out=tm1[par][:, 0:ncol], in0=bA[:, 0:ncol], scalar=bselT[:, ci:ci + 1], in1=csg[cb][:, 0, ccol:ccol + ncol],
                                                           op0=ALU.add, op1=ALU.mult), reads=[tb[2 * par], t_bsel, t_csg[cb]], writes=[t_tm1[par]])
                P.op("dve", lambda: V.scalar_tensor_tensor(out=tm2[par][:, 0:ncol], in0=bB[:, 0:ncol], scalar=bselT[:, cis:cis + 1], in1=csg[cb][:, 1, ccol:ccol + ncol],
                                                           op0=ALU.add, op1=ALU.mult), reads=[tb[2 * par + 1], t_bsel, t_csg[cb]], writes=[t_tm2[par]])
                P.op("pool", lambda: G_.tensor_tensor(out=dst, in0=tm1[par][:, 0:ncol], in1=tm2[par][:, 0:ncol], op=ALU.add),
                     reads=[t_tm1[par], t_tm2[par]], writes=[t_dst])

            def load_group(g):
                hb = gcount[0] % 2
                gcount[0] += 1
                P.dma("sp", lambda: nc.sync.dma_start(out=hTg[hb][:], in_=hT_v[:, :, g * 512:(g + 1) * 512]), writes=[t_hTg[hb]])
                P.dma("sp", lambda: nc.sync.dma_start(out=csg[hb][:, 0, :], in_=cos_d[:, g * 512:(g + 1) * 512]), writes=[t_csg[hb]])
                P.dma("sp", lambda: nc.sync.dma_start(out=csg[hb][:, 1, :], in_=sin_d[:, g * 512:(g + 1) * 512]), writes=[t_csg[hb]])
                return hb

            pcount = [0]

            def v_proj(u, hb, vt0, vw, swa):
                bk = 4 + (pcount[0] % 2)
                pcount[0] += 1
                ov = u["o_v"]
                fns = []
                for tt in range(4):
                    for kc in range(8):
                        fns.append(lambda tt=tt, kc=kc: T.matmul(bank[bk][:, tt * 128:(tt + 1) * 128], lhsT=hTg[hb][:, kc, tt * 128:(tt + 1) * 128],
                                                                 rhs=Wu[:, kc, ov:ov + 128], start=(kc == 0), stop=(kc == 7)))
                P.group("pe", fns, reads=[t_Wu, t_hTg[hb]], writes=[tb[bk]])
                src = bank[bk][:, :].rearrange("p (a b) -> p a b", a=4)
                vb_b = vbias[:].unsqueeze(1).to_broadcast([128, 4, 128])
                if not swa:
                    dst = Vb[:, vt0 * 129:(vt0 + 4) * 129].rearrange("p (a b) -> p a b", a=4)[:, :, 0:128]
                    P.op("dve", lambda: V.tensor_tensor(out=dst, in0=src, in1=vb_b, op=ALU.add), reads=[tb[bk], t_vbias], writes=[t_V])
                else:
                    for kv in range(2):
                        dst = Vb[:, vt0 * 130:(vt0 + 4) * 130].rearrange("p (a b) -> p a b", a=4)[:, :, kv * 65:kv * 65 + 64]
                        P.op("dve", lambda dst=dst, kv=kv: V.tensor_tensor(out=dst, in0=src[:, :, kv * 64:(kv + 1) * 64],
                                                                          in1=vbias[:, kv * 64:(kv + 1) * 64].unsqueeze(1).to_broadcast([128, 4, 64]), op=ALU.add),
                             reads=[tb[bk], t_vbias], writes=[t_V])

            for ui, u in enumerate(UNITS):
                swa = (ui == 0)
                nc_u = u["ncols"]
                P.dma("pool", lambda u=u, nc_u=nc_u: G_.dma_start(out=Wu[:, :, 0:nc_u], in_=wsel_v[:, :, u["base"]:u["base"] + nc_u]), writes=[t_Wu])
                P.dma("sp", lambda u=u: nc.sync.dma_start(out=vbias[:], in_=bsel_d[0:1, u["base"] + u["o_v"]:u["base"] + u["o_v"] + 128].partition_broadcast(128)),
                      writes=[t_vbias])
                if swa:
                    vv = Vb[:, 0:32 * 130].rearrange("p (a b) -> p a b", a=32)
                    P.op("pool", lambda vv=vv: G_.memset(vv[:, :, 64:65], 1.0), writes=[t_V])
                    P.op("pool", lambda vv=vv: G_.memset(vv[:, :, 129:130], 1.0), writes=[t_V])
                    kv_groups = [(20 + i, i * 512, 4 * i) for i in range(4)] + [(16 + i, 2048 + i * 512, 16 + 4 * i) for i in range(4)]
                elif ui == 1:
                    vv = Vb[:, 0:64 * 129].rearrange("p (a b) -> p a b", a=64)
                    P.op("pool", lambda vv=vv: G_.memset(vv[:, :, 128:129], 1.0), writes=[t_V])
                    kv_groups = [(g, g * 512, 4 * g) for g in range(NG)]
                else:
                    kv_groups = [(g, g * 512, 4 * g) for g in range(NG)]
                par = 0
                for (g, kcol, vt0) in kv_groups:
                    hb = load_group(g)
                    for kc_ in range(u["nk"]):
                        rope_proj(u, u["o_k"] + kc_ * 128, u["o_ks"] + kc_ * 128, hb, hb, 0, 512, KT[:, kc_ * 4096 + kcol:kc_ * 4096 + kcol + 512], t_KT, par)
                        par ^= 1
                    v_proj(u, hb, vt0, None, swa)
                    if swa and g < 20:
                        for qc in range(4):
                            rope_proj(u, u["o_q"] + qc * 128, u["o_qs"] + qc * 128, hb, hb, 0, 512, QT[:, qc, (g - 16) * 512:(g - 15) * 512], t_QT, par)
                            par ^= 1
                if not swa:
                    for g in range(16, 20):
                        hb = load_group(g)
                        rope_proj(u, u["o_q"], u["o_qs"], hb, hb, 0, 512, QT[:, 0, (g - 16) * 512:(g - 15) * 512], t_QT, par)
                        par ^= 1

                items = []
                if swa:
                    for oi in range(NOWN):
                        for hh in range(8):
                            items.append((oi, hh, 0, True))
                else:
                    for oi in range(NOWN):
                        nkb = 8 * (oi // 2) + (4 if oi % 2 == 0 else 8)
                        for m in range(2):
                            for c in range(nkb // 4):
                                items.append((oi, m, c, c == nkb // 4 - 1))

                def qk(n):
                    oi, a, c, last = items[n]
                    sb_ = n % 3
                    if swa:
                        hh = a; half = hh % 2; qc = hh // 2; kvg = hh // 4
                        ps = slice(half * 64, half * 64 + 64)
                        fns = [lambda: T.matmul(bank[sb_][:, 0:128], lhsT=KT[ps, kvg * 4096 + oi * 128:kvg * 4096 + (oi + 1) * 128], rhs=QT[ps, qc, oi * 128:(oi + 1) * 128], start=True, stop=True),
                               lambda: T.matmul(bank[sb_][:, 128:256], lhsT=KT[ps, kvg * 4096 + 2048 + oi * 128:kvg * 4096 + 2048 + (oi + 1) * 128], rhs=QT[ps, qc, oi * 128:(oi + 1) * 128], start=True, stop=True)]
                        ncol = 256
                        mk = smask[:, oi, :]
                        t_mk = t_smask
                    else:
                        m = a
                        ps = slice(m * 64, m * 64 + 64)
                        fns = [(lambda i=i: T.matmul(bank[sb_][:, i * 128:(i + 1) * 128], lhsT=KT[ps, (4 * c + i) * 128:(4 * c + i + 1) * 128],
                                                     rhs=QT[ps, 0, oi * 128:(oi + 1) * 128], start=True, stop=True)) for i in range(4)]
                        ncol = 512
                        mk = dmask[:, oi, :]
                        t_mk = t_dmask
                    P.group("pe", fns, reads=[t_KT, t_QT], writes=[tb[sb_]])
                    P.op("act", lambda: A.activation(out=PT[sb_][:, 0:ncol], in_=bank[sb_][:, 0:ncol], func=AF.Exp, scale=0.125), reads=[tb[sb_]], writes=[t_PT[sb_]])
                    if last:
                        P.op("pool", lambda: G_.tensor_tensor(out=PT[sb_][:, 0:ncol], in0=PT[sb_][:, 0:ncol], in1=mk, op=ALU.mult), reads=[t_PT[sb_], t_mk], writes=[t_PT[sb_]])

                def pv(n):
                    oi, a, c, last = items[n]
                    sb_ = n % 3
                    if swa:
                        hh = a; kvg = hh // 4
                        ob = 3 + (oi % 2) * 2 + (hh // 4)
                        oc = (hh % 4) * 65
                        fns = [lambda: T.matmul(bank[ob][:, oc:oc + 65], lhsT=PT[sb_][:, 0:128], rhs=Vb[:, oi * 130 + kvg * 65: oi * 130 + kvg * 65 + 65], start=True, stop=False),
                               lambda: T.matmul(bank[ob][:, oc:oc + 65], lhsT=PT[sb_][:, 128:256], rhs=Vb[:, (16 + oi) * 130 + kvg * 65: (16 + oi) * 130 + kvg * 65 + 65], start=False, stop=True)]
                    else:
                        m = a
                        ob = 3 + (oi % 2) * 2 + m
                        fns = [(lambda i=i: T.matmul(bank[ob][:, 0:129], lhsT=PT[sb_][:, i * 128:(i + 1) * 128], rhs=Vb[:, (4 * c + i) * 129:(4 * c + i + 1) * 129],
                                                     start=(c == 0 and i == 0), stop=(last and i == 3))) for i in range(4)]
                    P.group("pe", fns, reads=[t_PT[sb_], t_V], writes=[tb[ob]])
                    if swa and a == 7:
                        for hh in range(8):
                            ob2 = 3 + (oi % 2) * 2 + (hh // 4)
                            oc2 = (hh % 4) * 65
                            P.op("dve", lambda hh=hh, ob2=ob2, oc2=oc2: V.tensor_tensor(out=fsm[:, hh:hh + 1], in0=bank[ob2][:, oc2 + 64:oc2 + 65], in1=expsink[:, hh:hh + 1], op=ALU.add),
                                 reads=[tb[ob2], t_small], writes=[t_fsm])
                        P.op("dve", lambda: V.reciprocal(out=fsm[:, 0:8], in_=fsm[:, 0:8]), reads=[t_fsm], writes=[t_fsm])
                        for hh in range(8):
                            ob2 = 3 + (oi % 2) * 2 + (hh // 4)
                            oc2 = (hh % 4) * 65
                            P.op("dve", lambda hh=hh, ob2=ob2, oc2=oc2: V.tensor_scalar(out=mixed[:, oi, hh * 64:(hh + 1) * 64], in0=bank[ob2][:, oc2:oc2 + 64],
                                                                                         scalar1=fsm[:, hh:hh + 1], scalar2=None, op0=ALU.mult),
                                 reads=[tb[ob2], t_fsm], writes=[t_mixed[oi]])
                    if (not swa) and a == 1 and last:
                        h = ui - 1
                        o0 = bank[3 + (oi % 2) * 2]
                        o1 = bank[3 + (oi % 2) * 2 + 1]
                        t0, t1 = tb[3 + (oi % 2) * 2], tb[3 + (oi % 2) * 2 + 1]
                        P.op("dve", lambda: V.reciprocal(out=fsm[:, 16:17], in_=o0[:, 128:129]), reads=[t0], writes=[t_fsm])
                        P.op("dve", lambda: V.reciprocal(out=fsm[:, 17:18], in_=o1[:, 128:129]), reads=[t1], writes=[t_fsm])
                        P.op("dve", lambda: V.tensor_tensor(out=fsm[:, 17:18], in0=fsm[:, 17:18], in1=neglam, op=ALU.mult), reads=[t_fsm, t_small], writes=[t_fsm])
                        P.op("dve", lambda: V.tensor_scalar(out=fin[:, 0:128], in0=o1[:, 0:128], scalar1=fsm[:, 17:18], scalar2=None, op0=ALU.mult), reads=[t1, t_fsm], writes=[t_fin])
                        P.op("dve", lambda: V.scalar_tensor_tensor(out=fin[:, 128:256], in0=o0[:, 0:128], scalar=fsm[:, 16:17], in1=fin[:, 0:128], op0=ALU.mult, op1=ALU.add),
                             reads=[t0, t_fsm, t_fin], writes=[t_fin])
                        P.op("act", lambda: A.activation(out=junk2[:], in_=fin[:, 128:256], func=AF.Square, accum_out=fsm[:, 18:19]), reads=[t_fin], writes=[t_fsm])
                        P.op("dve", lambda: V.tensor_scalar(out=fsm[:, 18:19], in0=fsm[:, 18:19], scalar1=1.0 / 128, scalar2=EPS, op0=ALU.mult, op1=ALU.add), reads=[t_fsm], writes=[t_fsm])
                        P.op("act", lambda: A.activation(out=fsm[:, 18:19], in_=fsm[:, 18:19], func=AF.Sqrt), reads=[t_fsm], writes=[t_fsm])
                        P.op("dve", lambda: V.reciprocal(out=fsm[:, 18:19], in_=fsm[:, 18:19]), reads=[t_fsm], writes=[t_fsm])
                        P.op("dve", lambda: V.scalar_tensor_tensor(out=mixed[:, oi, 512 + h * 128:512 + (h + 1) * 128], in0=fin[:, 128:256], scalar=fsm[:, 18:19], in1=gsub_b[:],
                                                                   op0=ALU.mult, op1=ALU.mult), reads=[t_fin, t_fsm, t_gsub], writes=[t_mixed[oi]])

                LAG = 2
                for n in range(len(items) + LAG):
                    if n < len(items):
                        qk(n)
                    if n >= LAG:
                        pv(n - LAG)
            P.barrier()
            P.emit()

        s34 = st.enter_context(ExitStack())
        dest_i = sbuf(s34, "dest_i", [128, 4 * NOWN], I32); t_dest = trs(NOWN)
        gate4 = sbuf(s34, "gate4", [128, 4 * NOWN]); t_gate4 = trs(NOWN)
        maskb = sbuf(s34, "maskb", [128, NOWN, NE], BF16); t_maskb = trs(NOWN)
        cnt_run = sbuf(s34, "cnt_run", [128, NE]); t_cnt = Tr()
        cnt_i = sbuf(s34, "cnt_i", [1, NE], I32); t_cnti = Tr()
        padidx = sbuf(s34, "padidx", [128, NE], I32); t_pad = Tr()
        t_xbuf = Tr()
        iota32 = cst[:, 648:680]
        e2048 = cst[:, 680:712]
        iota_p = cst[:, 712:713]
        with ExitStack() as s3:
            gt1_b = sbuf(s3, "gt1_b", [128, D])
            A2b = sbuf(s3, "A2b", [128, D]); S2b = sbuf(s3, "S2b", [128, D]); t_m2 = Tr()
            P.dma("sp", lambda: nc.sync.dma_start(out=gt1_b[:], in_=mod_d[0:1, 2 * D:3 * D].partition_broadcast(128)), writes=[t_mod])
            P.dma("sp", lambda: nc.sync.dma_start(out=S2b[:], in_=mod_d[0:1, 3 * D:4 * D].partition_broadcast(128)), writes=[t_m2])
            P.dma("sp", lambda: nc.sync.dma_start(out=A2b[:], in_=mod_d[0:1, 4 * D:5 * D].partition_broadcast(128)), writes=[t_m2])
            wout = sbuf(s3, "wout", [128, 8, D], BF16); t_wout = Tr()
            boutb = sbuf(s3, "boutb", [1, D], BF16)
            wr = sbuf(s3, "wr", [128, 8, NE], BF16); t_wr = Tr()
            brb = sbuf(s3, "brb", [1, NE], BF16)
            gfb = sbuf(s3, "gfb", [128, D]); t_gfb = Tr()
            P.dma("sp", lambda: nc.sync.dma_start(out=gfb[:], in_=gffn_d[0:1, :].partition_broadcast(128)), writes=[t_gfb])
            P.op("dve", lambda: V.scalar_tensor_tensor(out=A2b[:], in0=A2b[:], scalar=1.0, in1=gfb[:], op0=ALU.add, op1=ALU.mult), reads=[t_m2, t_gfb], writes=[t_m2])
            P.op("dve", lambda: V.memset(cnt_run[:], 0.0), writes=[t_cnt])
            mixT = [sbuf(s3, "mixT%d" % i, [128, 8, 128], BF16) for i in range(2)]; t_mixT = trs(2)
            xo = [sbuf(s3, "xo%d" % i, [128, D]) for i in range(2)]; t_xo = trs(2)
            x1t = [sbuf(s3, "x1t%d" % i, [128, D]) for i in range(2)]; t_x1t = trs(2)
            h2f = [sbuf(s3, "h2f%d" % i, [128, D]) for i in range(2)]; t_h2f = trs(2)
            h2tok = [sbuf(s3, "h2tok%d" % i, [128, D], BF16) for i in range(2)]; t_h2tok = trs(2)
            h2Tt = [sbuf(s3, "h2Tt%d" % i, [128, 8, 128], BF16) for i in range(2)]; t_h2Tt = trs(2)
            zrow = sbuf(s3, "zrow", [128, D], BF16); t_zrow = Tr()
            junk3 = sbuf(s3, "junk3", [128, D], BF16); t_junk3 = Tr()
            rs = sbuf(s3, "rs", [128, 64]); t_rs = trs(2)
            lg = sbuf(s3, "lg", [128, 2, 4 * NE]); t_lg = trs(2)
            idx8 = sbuf(s3, "idx8", [128, 2, 8], U32)
            posb = sbuf(s3, "posb", [128, 2, NE]); junkp = sbuf(s3, "junkp", [128, 2, NE])
            wout_v = wout_d.rearrange("(k p) n -> p k n", p=128)
            wr_v = wr_d.rearrange("(k p) n -> p k n", p=128)
            P.dma("pool", lambda: G_.dma_start(out=wout[:], in_=wout_v), writes=[t_wout])
            P.dma("pool", lambda: G_.dma_start(out=boutb[:], in_=bout_d[:, :]), writes=[t_wout])
            P.dma("pool", lambda: G_.dma_start(out=wr[:], in_=wr_v), writes=[t_wr])
            P.dma("pool", lambda: G_.dma_start(out=brb[:], in_=br_d[:, :]), writes=[t_wr])
            P.op("pool", lambda: G_.memset(zrow[:], 0.0), writes=[t_zrow])

            def p3(oi):
                b = oi % 2
                P.dma("sp", lambda: nc.sync.dma_start(out=xo[b][:], in_=x_d[S + oi * 128:S + (oi + 1) * 128, :]), writes=[t_xo[b]])
                pT = bank[b][:, :].bitcast(BF16)
                P.group("pe", [(lambda kc=kc: T.transpose(out=pT[:, kc * 128:(kc + 1) * 128], in_=mixed[:, oi, kc * 128:(kc + 1) * 128], identity=identb[:])) for kc in range(8)],
                        reads=[t_mixed[oi], t_cst], writes=[tb[b]])
                P.op("act", lambda: A.activation(out=mixT[b][:].rearrange("p a b -> p (a b)"), in_=pT[:, :], func=AF.Copy), reads=[tb[b]], writes=[t_mixT[b]])
                for hf in range(2):
                    bk = 2 + 2 * b + hf
                    fns = [(lambda kc=kc, hf=hf, bk=bk: T.matmul(bank[bk][:, :], lhsT=mixT[b][:, kc, :], rhs=wout[:, kc, hf * 512:(hf + 1) * 512], start=(kc == 0), stop=False)) for kc in range(8)]
                    fns.append(lambda hf=hf, bk=bk: T.matmul(bank[bk][:, :], lhsT=onesb[0:1, :], rhs=boutb[0:1, hf * 512:(hf + 1) * 512], start=False, stop=True))
                    P.group("pe", fns, reads=[t_mixT[b], t_wout, t_cst], writes=[tb[bk]])
                    P.op("dve", lambda hf=hf, bk=bk: V.tensor_tensor(out=x1t[b][:, hf * 512:(hf + 1) * 512], in0=bank[bk][:, :], in1=gt1_b[:, hf * 512:(hf + 1) * 512], op=ALU.mult),
                         reads=[tb[bk], t_mod], writes=[t_x1t[b]])
                P.op("pool", lambda: G_.tensor_tensor(out=x1t[b][:], in0=x1t[b][:], in1=xo[b][:], op=ALU.add), reads=[t_x1t[b], t_xo[b]], writes=[t_x1t[b]])
                P.dma("sp", lambda: nc.sync.dma_start(out=x1_d[oi * 128:(oi + 1) * 128, :], in_=x1t[b][:]), reads=[t_x1t[b]])
                r0 = 32 * b
                P.op("act", lambda: A.activation(out=junk3[:], in_=x1t[b][:], func=AF.Square, accum_out=rs[:, r0:r0 + 1]), reads=[t_x1t[b]], writes=[t_junk3, t_rs[b]])
                P.op("dve", lambda: V.tensor_scalar(out=rs[:, r0 + 1:r0 + 2], in0=rs[:, r0:r0 + 1], scalar1=1.0 / D, scalar2=EPS, op0=ALU.mult, op1=ALU.add), reads=[t_rs[b]], writes=[t_rs[b]])
                P.op("act", lambda: A.activation(out=rs[:, r0 + 1:r0 + 2], in_=rs[:, r0 + 1:r0 + 2], func=AF.Sqrt), reads=[t_rs[b]], writes=[t_rs[b]])
                P.op("dve", lambda: V.reciprocal(out=rs[:, r0 + 1:r0 + 2], in_=rs[:, r0 + 1:r0 + 2]), reads=[t_rs[b]], writes=[t_rs[b]])
                P.op("dve", lambda: V.scalar_tensor_tensor(out=h2f[b][:], in0=x1t[b][:], scalar=rs[:, r0 + 1:r0 + 2], in1=A2b[:], op0=ALU.mult, op1=ALU.mult),
                     reads=[t_x1t[b], t_rs[b], t_m2], writes=[t_h2f[b]])
                P.op("pool", lambda: G_.tensor_tensor(out=h2tok[b][:], in0=h2f[b][:], in1=S2b[:], op=ALU.add), reads=[t_h2f[b], t_m2], writes=[t_h2tok[b]])
                bk = 6 + b
                pT2 = bank[bk][:, :].bitcast(BF16)
                P.group("pe", [(lambda kc=kc: T.transpose(out=pT2[:, kc * 128:(kc + 1) * 128], in_=h2tok[b][:, kc * 128:(kc + 1) * 128], identity=identb[:])) for kc in range(8)],
                        reads=[t_h2tok[b], t_cst], writes=[tb[bk]])
                P.op("act", lambda: A.activation(out=h2Tt[b][:].rearrange("p a b -> p (a b)"), in_=pT2[:, :], func=AF.Copy), reads=[tb[bk]], writes=[t_h2Tt[b]])
                fns = [(lambda kc=kc: T.matmul(bank[b][:, 0:NE], lhsT=h2Tt[b][:, kc, :], rhs=wr[:, kc, :], start=(kc == 0), stop=False)) for kc in range(8)]
                fns.append(lambda: T.matmul(bank[b][:, 0:NE], lhsT=onesb[0:1, :], rhs=brb[0:1, :], start=False, stop=True))
                P.group("pe", fns, reads=[t_h2Tt[b], t_wr, t_cst], writes=[tb[b]])
                L0, L1, L2, L3 = lg[:, b, 0:NE], lg[:, b, NE:NE + 8], lg[:, b, 2 * NE:3 * NE], lg[:, b, 3 * NE:3 * NE + 8]
                P.op("dve", lambda: V.tensor_copy(out=L0, in_=bank[b][:, 0:NE]), reads=[tb[b]], writes=[t_lg[b]])
                P.op("dve", lambda: V.max(out=L1, in_=L0), reads=[t_lg[b]], writes=[t_lg[b]])
                P.op("dve", lambda: V.max_index(out=idx8[:, b, :], in_max=L1, in_values=L0), reads=[t_lg[b]], writes=[t_lg[b]])
                P.op("dve", lambda: V.tensor_scalar(out=maskb[:, oi, :], in0=L0, scalar1=lg[:, b, NE + 3:NE + 4], scalar2=None, op0=ALU.is_ge), reads=[t_lg[b]], writes=[t_maskb[oi]])
                P.op("dve", lambda: V.tensor_scalar(out=rs[:, r0 + 2:r0 + 3], in0=lg[:, b, NE:NE + 1], scalar1=-1.0, scalar2=None, op0=ALU.mult), reads=[t_lg[b]], writes=[t_rs[b]])
                P.op("act", lambda: A.activation(out=L3[:, 0:4], in_=L1[:, 0:4], func=AF.Exp, bias=rs[:, r0 + 2:r0 + 3], scale=1.0, accum_out=rs[:, r0 + 3:r0 + 4]),
                     reads=[t_lg[b], t_rs[b]], writes=[t_lg[b], t_rs[b]])
                P.op("dve", lambda: V.reciprocal(out=rs[:, r0 + 3:r0 + 4], in_=rs[:, r0 + 3:r0 + 4]), reads=[t_rs[b]], writes=[t_rs[b]])
                P.op("dve", lambda: V.tensor_scalar(out=gate4[:, 4 * oi:4 * oi + 4], in0=L3[:, 0:4], scalar1=rs[:, r0 + 3:r0 + 4], scalar2=None, op0=ALU.mult),
                     reads=[t_lg[b], t_rs[b]], writes=[t_gate4[oi]])
                pb = bank[b]
                P.group("pe", [lambda: T.matmul(pb[:, 64:64 + NE], lhsT=trib[:], rhs=maskb[:, oi, :], start=True, stop=True),
                               lambda: T.matmul(pb[:, 128:128 + NE], lhsT=ones128b[:], rhs=maskb[:, oi, :], start=True, stop=True)],
                        reads=[t_maskb[oi], t_cst, t_lg[b]], writes=[tb[b]])
                P.op("dve", lambda: V.tensor_tensor(out=posb[:, b, :], in0=pb[:, 64:64 + NE], in1=cnt_run[:], op=ALU.add), reads=[tb[b], t_cnt], writes=[t_lg[b]])
                P.op("dve", lambda: V.tensor_tensor(out=cnt_run[:], in0=pb[:, 128:128 + NE], in1=cnt_run[:], op=ALU.add), reads=[tb[b], t_cnt, t_lg[b]], writes=[t_cnt])
                EK = rs[:, r0 + 8:r0 + 12]; PK = rs[:, r0 + 12:r0 + 16]; DF = rs[:, r0 + 16:r0 + 20]
                P.op("dve", lambda: V.tensor_copy(out=EK, in_=idx8[:, b, 0:4]), reads=[t_lg[b]], writes=[t_rs[b]])
                for k in range(4):
                    P.op("dve", lambda k=k: V.scalar_tensor_tensor(out=junkp[:, b, :], in0=iota32, scalar=rs[:, r0 + 8 + k:r0 + 9 + k], in1=posb[:, b, :],
                                                                   op0=ALU.is_equal, op1=ALU.mult, accum_out=rs[:, r0 + 12 + k:r0 + 13 + k]),
                         reads=[t_lg[b], t_rs[b], t_cst], writes=[t_rs[b]])
                P.op("dve", lambda: V.scalar_tensor_tensor(out=DF, in0=EK, scalar=float(CAP), in1=PK, op0=ALU.mult, op1=ALU.add), reads=[t_rs[b]], writes=[t_rs[b]])
                P.op("dve", lambda: V.tensor_copy(out=dest_i[:, 4 * oi:4 * oi + 4], in_=DF), reads=[t_rs[b]], writes=[t_dest[oi]])
                for k in range(4):
                    P.dma("pool", lambda k=k: G_.indirect_dma_start(out=xbuf_d[:, :], out_offset=bass.IndirectOffsetOnAxis(ap=dest_i[:, 4 * oi + k:4 * oi + k + 1], axis=0),
                                                                    in_=h2tok[b][:, :], in_offset=None),
                          reads=[t_h2tok[b], t_dest[oi]], writes=[t_xbuf])
            for oi in range(NOWN):
                p3(oi)
            cf = rs[0:1, 0:NE]
            P.op("dve", lambda: V.tensor_scalar(out=cf, in0=cnt_run[0:1, :], scalar1=127.0, scalar2=1.0 / 128, op0=ALU.add, op1=ALU.mult), reads=[t_cnt] + t_rs, writes=t_rs)
            P.op("dve", lambda: V.tensor_scalar(out=cf, in0=cf, scalar1=-0.496, scalar2=None, op0=ALU.add), reads=t_rs, writes=t_rs)
            P.op("dve", lambda: V.tensor_copy(out=cnt_i[:], in_=cf), reads=t_rs, writes=[t_cnti])
            P.op("dve", lambda: V.tensor_scalar(out=posb[:, 0, :], in0=cnt_run[:], scalar1=iota_p, scalar2=None, op0=ALU.add), reads=[t_cnt, t_cst] + t_lg, writes=t_lg)
            P.op("dve", lambda: V.tensor_scalar(out=posb[:, 1, :], in0=posb[:, 0, :], scalar1=float(CAP), scalar2=None, op0=ALU.is_ge), reads=t_lg, writes=t_lg)
            P.op("dve", lambda: V.tensor_tensor(out=posb[:, 0, :], in0=posb[:, 0, :], in1=e2048, op=ALU.add), reads=t_lg + [t_cst], writes=t_lg)
            P.op("dve", lambda: V.tensor_scalar(out=rs[:, 40:41], in0=iota_p, scalar1=float(NE * CAP), scalar2=None, op0=ALU.add), reads=[t_cst] + t_rs, writes=t_rs)
            P.op("dve", lambda: V.tensor_scalar(out=junkp[:, 0, :], in0=posb[:, 0, :], scalar1=-1.0, scalar2=rs[:, 40:41], op0=ALU.mult, op1=ALU.add), reads=t_lg + t_rs, writes=t_lg)
            P.op("dve", lambda: V.tensor_tensor(out=junkp[:, 0, :], in0=junkp[:, 0, :], in1=posb[:, 1, :], op=ALU.mult), reads=t_lg, writes=t_lg)
            P.op("dve", lambda: V.tensor_tensor(out=posb[:, 0, :], in0=posb[:, 0, :], in1=junkp[:, 0, :], op=ALU.add), reads=t_lg, writes=t_lg)
            P.op("dve", lambda: V.tensor_copy(out=padidx[:], in_=posb[:, 0, :]), reads=t_lg, writes=[t_pad])
            for e in range(NE):
                P.dma("pool", lambda e=e: G_.indirect_dma_start(out=xbuf_d[:, :], out_offset=bass.IndirectOffsetOnAxis(ap=padidx[:, e:e + 1], axis=0),
                                                                in_=zrow[:, :], in_offset=None),
                      reads=[t_zrow, t_pad], writes=[t_xbuf])
            P.barrier()
            P.emit()

        if int(os.environ.get('K_STOP', '9')) <= 3:
            return nc
        with ExitStack() as s4:
            w1b = [sbuf(s4, "w1b%d" % i, [128, 8, 2 * D], BF16) for i in range(2)]
            w2b = [sbuf(s4, "w2b%d" % i, [128, 8, D], BF16) for i in range(2)]
            b1r = [sbuf(s4, "b1r%d" % i, [1, 2 * D], BF16) for i in range(2)]
            b2r = [sbuf(s4, "b2r%d" % i, [1, D], BF16) for i in range(2)]
            t_w = trs(2)
            Xtok = [sbuf(s4, "Xtok%d" % i, [128, D], BF16) for i in range(2)]; t_Xtok = trs(2)
            XT = [sbuf(s4, "XT%d" % i, [128, 8, 128], BF16) for i in range(2)]; t_XT = trs(2)
            gg = [sbuf(s4, "gg%d" % i, [128, 256]) for i in range(2)]; t_gg = trs(2)
            sg = [sbuf(s4, "sg%d" % i, [128, 256]) for i in range(2)]; t_sg = trs(2)
            ll = [sbuf(s4, "ll%d" % i, [128, 256]) for i in range(2)]; t_ll = trs(2)
            atok = [sbuf(s4, "atok%d" % i, [128, D], BF16) for i in range(2)]; t_atok = trs(2)
            aT = [sbuf(s4, "aT%d" % i, [128, 8, 128], BF16) for i in range(2)]; t_aT = trs(2)
            yt = [sbuf(s4, "yt%d" % i, [128, D]) for i in range(2)]; t_yt = trs(2)
            t_ybuf = Tr()
            w1_v = w1_d.rearrange("e (k p) n -> e p k n", p=128)
            w2_v = w2_d.rearrange("e (k p) n -> e p k n", p=128)
            blk = [0]

            def block_body(e, bslot, ws):
                n = blk[0]
                blk[0] += 1
                xb = n % 2
                row0 = e * CAP + bslot * 128
                P.dma("sp", lambda: nc.sync.dma_start(out=Xtok[xb][:], in_=xbuf_d[row0:row0 + 128, :]), reads=[t_xbuf], writes=[t_Xtok[xb]], slot=xb)
                pT = bank[xb][:, :].bitcast(BF16)
                P.group("pe", [(lambda kc=kc: T.transpose(out=pT[:, kc * 128:(kc + 1) * 128], in_=Xtok[xb][:, kc * 128:(kc + 1) * 128], identity=identb[:])) for kc in range(8)],
                        reads=[t_Xtok[xb], t_cst], writes=[tb[xb]])
                P.op("act", lambda: A.activation(out=XT[xb][:].rearrange("p a b -> p (a b)"), in_=pT[:, :], func=AF.Copy), reads=[tb[xb]], writes=[t_XT[xb]])
                def do_cch(cch):
                    bk = 2 + (cch % 2)
                    par = cch % 2
                    fns = [(lambda kc=kc: T.matmul(bank[bk][:, :], lhsT=XT[xb][:, kc, :], rhs=w1b[ws][:, kc, cch * 512:(cch + 1) * 512], start=(kc == 0), stop=False)) for kc in range(8)]
                    fns.append(lambda: T.matmul(bank[bk][:, :], lhsT=onesb[0:1, :], rhs=b1r[ws][0:1, cch * 512:(cch + 1) * 512], start=False, stop=True))
                    P.group("pe", fns, reads=[t_XT[xb], t_w[ws], t_cst], writes=[tb[bk]])
                    P.op("dve", lambda: V.tensor_scalar(out=gg[par][:], in0=bank[bk][:, 0:512:2], scalar1=7.0, scalar2=None, op0=ALU.min), reads=[tb[bk]], writes=[t_gg[par]])
                    P.op("act", lambda: A.activation(out=sg[par][:], in_=gg[par][:], func=AF.Gelu_apprx_sigmoid), reads=[t_gg[par]], writes=[t_sg[par]])
                    P.op("dve", lambda: V.tensor_scalar(out=ll[par][:], in0=bank[bk][:, 1:512:2], scalar1=7.0, scalar2=-7.0, op0=ALU.min, op1=ALU.max), reads=[tb[bk]], writes=[t_ll[par]])
                    P.op("dve", lambda: V.scalar_tensor_tensor(out=atok[xb][:, cch * 256:(cch + 1) * 256], in0=ll[par][:], scalar=1.0, in1=sg[par][:], op0=ALU.add, op1=ALU.mult),
                         reads=[t_ll[par], t_sg[par]], writes=[t_atok[xb]])
                for cch in range(4):
                    do_cch(cch)
                bk = 4 + xb
                pT2 = bank[bk][:, :].bitcast(BF16)
                P.group("pe", [(lambda j=j: T.transpose(out=pT2[:, j * 128:(j + 1) * 128], in_=atok[xb][:, j * 128:(j + 1) * 128], identity=identb[:])) for j in range(8)],
                        reads=[t_atok[xb], t_cst], writes=[tb[bk]])
                P.op("act", lambda: A.activation(out=aT[xb][:].rearrange("p a b -> p (a b)"), in_=pT2[:, :], func=AF.Copy), reads=[tb[bk]], writes=[t_aT[xb]])
                def do_hf(hf):
                    bk2 = 6 + hf
                    fns = [(lambda j=j: T.matmul(bank[bk2][:, :], lhsT=aT[xb][:, j, :], rhs=w2b[ws][:, j, hf * 512:(hf + 1) * 512], start=(j == 0), stop=False)) for j in range(8)]
                    fns.append(lambda: T.matmul(bank[bk2][:, :], lhsT=onesb[0:1, :], rhs=b2r[ws][0:1, hf * 512:(hf + 1) * 512], start=False, stop=True))
                    P.group("pe", fns, reads=[t_aT[xb], t_w[ws], t_cst], writes=[tb[bk2]])
                    if hf == 0:
                        P.op("act", lambda: A.activation(out=yt[xb][:, 0:512], in_=bank[bk2][:, :], func=AF.Copy), reads=[tb[bk2]], writes=[t_yt[xb]])
                    else:
                        P.op("dve", lambda: V.tensor_copy(out=yt[xb][:, 512:1024], in_=bank[bk2][:, :]), reads=[tb[bk2]], writes=[t_yt[xb]])
                for hf in range(2):
                    do_hf(hf)
                P.dma("sp", lambda: nc.sync.dma_start(out=ybuf_d[row0:row0 + 128, :], in_=yt[xb][:]), reads=[t_yt[xb]], writes=[t_ybuf], slot=2 + xb)

            for e in range(int(os.environ.get('K_NE', NE))):
                ws = e % 2
                P.dma("pool", lambda e=e, ws=ws: G_.dma_start(out=w1b[ws][:], in_=w1_v[e]), writes=[t_w[ws]])
                P.dma("pool", lambda e=e, ws=ws: G_.dma_start(out=w2b[ws][:], in_=w2_v[e]), writes=[t_w[ws]])
                P.dma("pool", lambda e=e, ws=ws: G_.dma_start(out=b1r[ws][:], in_=b1_d[e:e + 1, :]), writes=[t_w[ws]])
                P.dma("pool", lambda e=e, ws=ws: G_.dma_start(out=b2r[ws][:], in_=b2_d[e:e + 1, :]), writes=[t_w[ws]])
                P.regload(cnt_i[0:1, e:e + 1], reads=[t_cnti])
                NB = CAP // 128
                for bslot in range(NB):
                    if bslot in (3, 6, 10):
                        P.cond_begin(bslot + 1)
                    P.cond_begin(bslot + 1)
                    block_body(e, bslot, ws)
                    P.cond_end()
                for _ in range(3):
                    P.cond_end()
            P.barrier()
            P.emit()

        if int(os.environ.get('K_STOP', '9')) <= 4:
            return nc
        with ExitStack() as s5:
            gt2_b = sbuf(s5, "gt2_b", [128, D]); gfin_b = sbuf(s5, "gfin_b", [128, D]); t_g5 = Tr()
            yk = [sbuf(s5, "yk%d" % i, [128, D]) for i in range(4)]; t_yk = trs(4)
            acc = [sbuf(s5, "acc%d" % i, [128, D]) for i in range(2)]; t_acc = trs(2)
            x1b = [sbuf(s5, "x1b%d" % i, [128, D]) for i in range(2)]; t_x1b = trs(2)
            ob_ = [sbuf(s5, "ob%d" % i, [128, D]) for i in range(2)]; t_ob = trs(2)
            junk5 = sbuf(s5, "junk5", [128, D], BF16); t_junk5 = Tr()
            fs5 = sbuf(s5, "fs5", [128, 8]); t_fs5 = trs(2)
            P.dma("sp", lambda: nc.sync.dma_start(out=gt2_b[:], in_=mod_d[0:1, 5 * D:6 * D].partition_broadcast(128)), writes=[t_g5])
            P.dma("sp", lambda: nc.sync.dma_start(out=gfin_b[:], in_=gfin_d[0:1, :].partition_broadcast(128)), writes=[t_g5])

            def p5(oi):
                b = oi % 2
                P.dma("sp", lambda: nc.sync.dma_start(out=x1b[b][:], in_=x1_d[oi * 128:(oi + 1) * 128, :]), writes=[t_x1b[b]])
                for k in range(4):
                    P.dma("pool", lambda k=k: G_.indirect_dma_start(out=yk[k][:, :], out_offset=None, in_=ybuf_d[:, :],
                                                                    in_offset=bass.IndirectOffsetOnAxis(ap=dest_i[:, 4 * oi + k:4 * oi + k + 1], axis=0),
                                                                    ), reads=[t_dest[oi]], writes=[t_yk[k]])
                    if k == 0:
                        P.op("dve", lambda: V.tensor_scalar(out=acc[b][:], in0=yk[0][:], scalar1=gate4[:, 4 * oi:4 * oi + 1], scalar2=None, op0=ALU.mult),
                             reads=[t_yk[0], t_gate4[oi]], writes=[t_acc[b]])
                    else:
                        P.op("dve", lambda k=k: V.scalar_tensor_tensor(out=acc[b][:], in0=yk[k][:], scalar=gate4[:, 4 * oi + k:4 * oi + k + 1], in1=acc[b][:], op0=ALU.mult, op1=ALU.add),
                             reads=[t_yk[k], t_gate4[oi], t_acc[b]], writes=[t_acc[b]])
                P.op("pool", lambda: G_.tensor_tensor(out=acc[b][:], in0=acc[b][:], in1=gt2_b[:], op=ALU.mult), reads=[t_acc[b], t_g5], writes=[t_acc[b]])
                P.op("pool", lambda: G_.tensor_tensor(out=x1b[b][:], in0=acc[b][:], in1=x1b[b][:], op=ALU.add), reads=[t_acc[b], t_x1b[b]], writes=[t_x1b[b]])
                P.op("act", lambda: A.activation(out=junk5[:], in_=x1b[b][:], func=AF.Square, accum_out=fs5[:, 4 * b:4 * b + 1]), reads=[t_x1b[b]], writes=[t_junk5, t_fs5[b]])
                P.op("dve", lambda: V.tensor_scalar(out=fs5[:, 4 * b + 1:4 * b + 2], in0=fs5[:, 4 * b:4 * b + 1], scalar1=1.0 / D, scalar2=EPS, op0=ALU.mult, op1=ALU.add),
                     reads=[t_fs5[b]], writes=[t_fs5[b]])
                P.op("act", lambda: A.activation(out=fs5[:, 4 * b + 1:4 * b + 2], in_=fs5[:, 4 * b + 1:4 * b + 2], func=AF.Sqrt), reads=[t_fs5[b]], writes=[t_fs5[b]])
                P.op("dve", lambda: V.reciprocal(out=fs5[:, 4 * b + 1:4 * b + 2], in_=fs5[:, 4 * b + 1:4 * b + 2]), reads=[t_fs5[b]], writes=[t_fs5[b]])
                P.op("dve", lambda: V.scalar_tensor_tensor(out=ob_[b][:], in0=x1b[b][:], scalar=fs5[:, 4 * b + 1:4 * b + 2], in1=gfin_b[:], op0=ALU.mult, op1=ALU.mult),
                     reads=[t_x1b[b], t_fs5[b], t_g5], writes=[t_ob[b]])
                P.dma("sp", lambda: nc.sync.dma_start(out=out_d[oi * 128:(oi + 1) * 128, :], in_=ob_[b][:]), reads=[t_ob[b]])
            for oi in range(NOWN):
                p5(oi)
            P.barrier()
            P.emit()
    return nc


def _consts():
    c = np.zeros((128, NCST), np.float32)
    c[:, 0:128] = np.eye(128, dtype=np.float32)
    k = np.arange(128)[:, None]
    q = np.arange(128)[None, :]
    c[:, 128:256] = (k <= q)
    c[:, 256:384] = (k > q)
    inv = (1.0 / (np.float32(10000.0) ** (np.arange(0, 64, 2, dtype=np.float32) / np.float32(64)))).astype(np.float32)
    p = np.arange(128)
    c[:, 384] = inv[p % 32]
    c[:, 385] = np.where((p % 64) < 32, -1.0, 1.0)
    c[:, 386] = np.float32(math.pi / 2)
    c[:, 387] = 0.0
    c[:, 388] = 1.0
    c[:, 392:520] = 1.0
    c[:, 520:648] = (k < q)
    c[:, 648:680] = np.arange(32)[None, :]
    c[:, 680:712] = (np.arange(32) * CAP)[None, :]
    c[:, 712] = np.arange(128)
    return c


def _core_masks(j):
    own = own_blocks(j)
    k = np.arange(128)[:, None]
    q = np.arange(128)[None, :]
    tri = (k <= q).astype(np.float32)
    low = (k > q).astype(np.float32)
    dm = np.zeros((128, NOWN, 4, 128), np.float32)
    sm = np.zeros((128, NOWN, 2, 128), np.float32)
    for oi, gb in enumerate(own):
        nkb = 8 * (oi // 2) + (4 if oi % 2 == 0 else 8)
        for i in range(4):
            kb = nkb - 4 + i
            if kb < gb:
                dm[:, oi, i, :] = 1.0
            elif kb == gb:
                dm[:, oi, i, :] = tri
        sm[:, oi, 1, :] = tri
        if gb > 0:
            sm[:, oi, 0, :] = low
    return dm.reshape(128, NOWN * 512), sm.reshape(128, NOWN * 256)


_NC_CACHE = {}


def kernel(x, c, positions, w_ada, b_ada, g_mix, w_in, b_in, attn_sinks, lambda_q1, lambda_k1, lambda_q2, lambda_k2,
           g_subln, w_out, b_out, g_ffn, w_router, b_router, w1, b1, w2, b2, g_final):
    f = lambda a: np.ascontiguousarray(np.asarray(a))
    x = f(x); positions = f(positions)
    if "nc" not in _NC_CACHE:
        _NC_CACHE["nc"] = build_program()
    nc = _NC_CACHE["nc"]
    colT = lambda v: f(np.asarray(v).reshape(-1, 128).T)
    w_sel = f(np.asarray(w_in)[0][:, SEL])
    b_sel = f(np.asarray(b_in)[0][SEL])
    b1_ = np.asarray(b1)[0]
    shared = {
        "w_ada": f(np.asarray(w_ada)[0]), "b_ada": f(np.asarray(b_ada)[0][None, :]),
        "gmixT": colT(np.asarray(g_mix)[0]), "gffnT": colT(np.asarray(g_ffn)[0]),
        "w_sel": w_sel, "b_selT": colT(b_sel), "b_sel": f(b_sel[None, :]),
        "sinks": f(np.asarray(attn_sinks)[0][None, :]),
        "lam4": f(np.stack([np.asarray(lambda_q1)[0], np.asarray(lambda_k1)[0], np.asarray(lambda_q2)[0], np.asarray(lambda_k2)[0]])),
        "g_subln": f(np.asarray(g_subln)[0][None, :]),
        "w_out": f(np.asarray(w_out)[0]), "b_out": f(np.asarray(b_out)[0][None, :]),
        "w_router": f(np.asarray(w_router)[0]), "b_router": f(np.asarray(b_router)[0][None, :]),
        "w1": f(np.asarray(w1)[0]), "w2": f(np.asarray(w2)[0]), "b2": f(np.asarray(b2)[0]),
        "b1": f(b1_), "g_ffn": f(np.asarray(g_ffn)[0][None, :]), "g_mix": f(np.asarray(g_mix)[0][None, :]),
        "g_final": f(np.asarray(g_final)[None, :]),
        "consts": _consts(),
    }
    in_maps = []
    rows_all = []
    for core in range(8):
        b, j = core // 4, core % 4
        own = own_blocks(j)
        rows_own = np.concatenate([np.arange(g * 128, (g + 1) * 128) for g in own])
        rows_prev = np.concatenate([np.arange(max(g - 1, 0) * 128, (max(g - 1, 0) + 1) * 128) for g in own])
        rows_all.append(rows_own)
        xb = x[b]
        x_ext = np.concatenate([xb, xb[rows_own], xb[rows_prev]], axis=0)
        pb = positions[b]
        pos_ext = np.concatenate([pb, pb[rows_own], pb[rows_prev]])[None, :].astype(np.int32)
        dm, sm = _core_masks(j)
        m = dict(shared)
        m.update({"x": f(x_ext), "pos": f(pos_ext), "cT": colT(np.asarray(c)[b]), "dmask": dm, "smask": sm})
        in_maps.append(m)
    res = run_bass_kernel_spmd(nc, in_maps, core_ids=list(range(8)))
    out = np.zeros((2, S, D), np.float32)
    for core in range(8):
        out[core // 4, rows_all[core], :] = np.asarray(res.results[core]["out"])
    return out
```

```python
import math
import os
from contextlib import ExitStack

import numpy as np
import concourse.bass as bass
import concourse.mybir as mybir
from concourse.bass_utils import run_bass_kernel_spmd

F32 = mybir.dt.float32
BF16 = mybir.dt.bfloat16
I32 = mybir.dt.int32
ALU = mybir.AluOpType
AF = mybir.ActivationFunctionType
AX = mybir.AxisListType

D = 1024
S = 8192
NT = 64
NG = 16
NOWN = 16
NE = 32
SX = S + 2 * NOWN * 128
NGX = SX // 512
NCST = 720
CAP = 2048
U32 = mybir.dt.uint32
EPS = 1e-5
C1 = 6.28125
C2 = 2 * math.pi - 6.28125
INV2PI = float(1.0 / (2 * math.pi))

OFF_QA, OFF_KA, OFF_VA, OFF_QD, OFF_KD, OFF_VD = 0, 512, 640, 768, 1280, 1792


def _swap64(cols):
    cols = np.asarray(cols).reshape(-1, 64)
    return np.concatenate([cols[:, 32:], cols[:, :32]], axis=1).reshape(-1)


def _unit_cols():
    units = []
    k = np.concatenate([np.tile(np.arange(OFF_KA + g * 64, OFF_KA + (g + 1) * 64), 2) for g in range(2)])
    q = np.arange(OFF_QA, OFF_QA + 512)
    v = np.arange(OFF_VA, OFF_VA + 128)
    units.append(dict(nk=2, nq=4, k=k, q=q, v=v))
    for h in range(4):
        k = np.arange(OFF_KD + h * 128, OFF_KD + (h + 1) * 128)
        q = np.arange(OFF_QD + h * 128, OFF_QD + (h + 1) * 128)
        v = np.arange(OFF_VD + h * 128, OFF_VD + (h + 1) * 128)
        units.append(dict(nk=1, nq=1, k=k, q=q, v=v))
    off = 0
    sel = []
    for u in units:
        u["base"] = off
        parts = [u["k"], _swap64(u["k"]), u["q"], _swap64(u["q"]), u["v"]]
        u["o_k"] = 0
        u["o_ks"] = len(u["k"])
        u["o_q"] = u["o_ks"] + len(u["k"])
        u["o_qs"] = u["o_q"] + len(u["q"])
        u["o_v"] = u["o_qs"] + len(u["q"])
        u["ncols"] = u["o_v"] + 128
        sel.append(np.concatenate(parts))
        off += u["ncols"]
    return units, np.concatenate(sel)


UNITS, SEL = _unit_cols()
NSEL = len(SEL)
NCH = NSEL // 128


def own_blocks(j):
    return sorted([8 * m + j for m in range(8)] + [8 * m + 7 - j for m in range(8)])


class Tr:
    __slots__ = ("w", "r")

    def __init__(self):
        self.w = {}
        self.r = {}


def trs(n):
    return [Tr() for _ in range(n)]


class Prog:
    ENG = ("pe", "act", "dve", "pool", "sp")

    def __init__(self, nc, stack, n_dma_sems=48):
        self.nc = nc
        self.q = {e: [] for e in self.ENG}
        self.esem = {e: stack.enter_context(nc.semaphore("s_" + e)) for e in self.ENG}
        self.ecnt = {e: 0 for e in self.ENG}
        self.waited = {e: {} for e in self.ENG}
        self.dsem = [stack.enter_context(nc.semaphore("d%d" % i)) for i in range(n_dma_sems)]
        self.dcnt = [0] * n_dma_sems
        self.dpool = {"sp": list(range(0, n_dma_sems - 16)), "pool": list(range(n_dma_sems - 16, n_dma_sems))}
        self.dnext = {"sp": 0, "pool": 0}
        self.in_cond = False
        self.handles = {"pe": nc.tensor, "act": nc.scalar, "dve": nc.vector, "pool": nc.gpsimd, "sp": nc.sync}

    def _need(self, eng, s, v):
        wd = self.waited[eng]
        if wd.get(s, 0) >= v:
            return
        wd[s] = v
        self.q[eng].append(("wait", s, v))

    def _waits(self, eng, reads, writes):
        need = {}
        for t in reads:
            for s, v in t.w.items():
                if need.get(s, 0) < v:
                    need[s] = v
        for t in writes:
            for s, v in t.w.items():
                if need.get(s, 0) < v:
                    need[s] = v
            for s, v in t.r.items():
                if need.get(s, 0) < v:
                    need[s] = v
        for s, v in need.items():
            if eng == "pe" and s is self.esem["pe"]:
                continue
            self._need(eng, s, v)

    def _record(self, ev, reads, writes):
        s, v = ev
        for t in reads:
            if t.r.get(s, 0) < v:
                t.r[s] = v
        for t in writes:
            if self.in_cond:
                if t.w.get(s, 0) < v:
                    t.w[s] = v
            else:
                t.w = {s: v}
                t.r = {}

    def op(self, eng, fn, reads=(), writes=()):
        self._waits(eng, reads, writes)
        self.ecnt[eng] += 1
        ev = (self.esem[eng], self.ecnt[eng])
        self.q[eng].append(("op", fn, self.esem[eng], 1))
        self._record(ev, reads, writes)

    def group(self, eng, fns, reads=(), writes=()):
        self._waits(eng, reads, writes)
        self.ecnt[eng] += 1
        ev = (self.esem[eng], self.ecnt[eng])
        for f in fns[:-1]:
            self.q[eng].append(("op", f, None, 0))
        self.q[eng].append(("op", fns[-1], self.esem[eng], 1))
        self._record(ev, reads, writes)

    def dma(self, eng, fn, reads=(), writes=(), slot=None):
        pl = self.dpool[eng]
        if slot is None:
            i = pl[self.dnext[eng]]
            self.dnext[eng] = (self.dnext[eng] + 1) % (len(pl) - 4)
        else:
            i = pl[len(pl) - 4 + slot]
        s = self.dsem[i]
        if self.dcnt[i]:
            self._need(eng, s, self.dcnt[i])
        self._waits(eng, reads, writes)
        self.dcnt[i] += 16
        ev = (s, self.dcnt[i])
        self.q[eng].append(("op", fn, s, 16))
        self._record(ev, reads, writes)
        return ev

    CENG = ("pe", "act", "dve", "sp")

    def regload(self, ap, reads=()):
        for e in self.CENG:
            self._waits(e, reads, ())
            self.q[e].append(("regload", ap))

    def cond_begin(self, thr):
        if not hasattr(self, "_cstack"):
            self._cstack = []
        self._cstack.append(({e: self.ecnt[e] for e in self.ENG}, list(self.dcnt), {e: dict(self.waited[e]) for e in self.ENG}))
        self.in_cond = True
        for e in self.CENG:
            self.q[e].append(["if", thr, None])

    def cond_end(self):
        ec0, dc0, wd0 = self._cstack.pop()
        assert self.ecnt["pool"] == ec0["pool"], "pool must stay outside conditional regions"
        dd = [(i, self.dcnt[i] - dc0[i]) for i in range(len(self.dcnt)) if self.dcnt[i] != dc0[i]]
        for i, _ in dd:
            assert i in self.dpool["sp"]
        for e in self.CENG:
            comp = []
            if self.ecnt[e] != ec0[e]:
                comp.append((self.esem[e], self.ecnt[e] - ec0[e]))
            if e == "sp":
                comp += [(self.dsem[i], d, dc0[i]) for i, d in dd]
            for it in reversed(self.q[e]):
                if isinstance(it, list) and it[0] == "if" and it[2] is None:
                    it[2] = comp
                    break
            self.q[e].append(("endif",))
            self.waited[e] = wd0[e]
        self.waited["pool"] = wd0["pool"]
        self.in_cond = bool(self._cstack)

    def barrier(self):
        for e in self.ENG:
            for f in self.ENG:
                if f != e and self.ecnt[f]:
                    self._need(e, self.esem[f], self.ecnt[f])
            for i, s in enumerate(self.dsem):
                if self.dcnt[i]:
                    self._need(e, s, self.dcnt[i])

    def emit(self):
        nc = self.nc
        q = self.q
        self.q = {e: [] for e in self.ENG}
        if not hasattr(self, "regs"):
            self.regs = {}
        with nc.Block() as block:
            def run_items(h, ename, items):
                i = 0
                n = len(items)
                while i < n:
                    it = items[i]
                    k = it[0]
                    if k == "wait":
                        h.wait_ge(it[1], it[2])
                    elif k == "op":
                        ins = it[1]()
                        if it[2] is not None:
                            ins.then_inc(it[2], it[3])
                    elif k == "regload":
                        if ename not in self.regs:
                            self.regs[ename] = h.alloc_register("cnt_" + ename)
                        h.reg_load(self.regs[ename], it[1])
                    elif k == "if":
                        depth = 1
                        j = i + 1
                        while True:
                            if items[j][0] == "if":
                                depth += 1
                            elif items[j][0] == "endif":
                                depth -= 1
                                if depth == 0:
                                    break
                            j += 1
                        body = items[i + 1:j]
                        with h.If_lt(self.regs[ename], it[1]):
                            h.drain()
                            for cp in it[2]:
                                if len(cp) == 3 and cp[2]:
                                    h.wait_ge(cp[0], cp[2])
                                h.sem_inc(cp[0], cp[1])
                        with h.Else():
                            run_items(h, ename, body)
                        i = j
                    i += 1

            def run(ename):
                run_items(self.handles[ename], ename, q[ename])

            @block.tensor
            def _(e):
                run("pe")

            @block.scalar
            def _(e):
                run("act")

            @block.vector
            def _(e):
                run("dve")

            @block.gpsimd
            def _(e):
                run("pool")

            @block.sync
            def _(e):
                run("sp")


def build_program(j_core_unused=None, debug=False):
    nc = bass.Bass("TRN2", target_bir_lowering=False)
    din = lambda name, shape, dt=F32: nc.dram_tensor(name, list(shape), dt, kind="ExternalInput").ap()
    x_d = din("x", [SX, D])
    pos_d = din("pos", [1, SX], I32)
    dmask_d = din("dmask", [128, NOWN * 512])
    smask_d = din("smask", [128, NOWN * 256])
    cT_d = din("cT", [128, 8])
    wada_d = din("w_ada", [D, 6 * D])
    bada_d = din("b_ada", [1, 6 * D])
    gmixT_d = din("gmixT", [128, 8])
    gffnT_d = din("gffnT", [128, 8])
    wsel_d = din("w_sel", [D, NSEL])
    bselT_d = din("b_selT", [128, NCH])
    bsel_d = din("b_sel", [1, NSEL])
    sinks_d = din("sinks", [1, 8])
    lam_d = din("lam4", [4, 64])
    gsub_d = din("g_subln", [1, 128])
    wout_d = din("w_out", [D, D])
    bout_d = din("b_out", [1, D])
    wr_d = din("w_router", [D, NE])
    br_d = din("b_router", [1, NE])
    w1_d = din("w1", [NE, D, 2 * D])
    b1_d = din("b1", [NE, 2 * D])
    gffn_d = din("g_ffn", [1, D])
    gmix_d = din("g_mix", [1, D])
    w2_d = din("w2", [NE, D, D])
    b2_d = din("b2", [NE, D])
    gfin_d = din("g_final", [1, D])
    cst_d = din("consts", [128, NCST])
    out_d = nc.dram_tensor("out", [NOWN * 128, D], F32, kind="ExternalOutput").ap()
    hT_d = nc.dram_tensor("hT_scr", [8, 128, SX], BF16, kind="Internal").ap()
    cos_d = nc.dram_tensor("cos_scr", [128, SX], F32, kind="Internal").ap()
    sin_d = nc.dram_tensor("sin_scr", [128, SX], F32, kind="Internal").ap()
    x1_d = nc.dram_tensor("x1_scr", [NOWN * 128, D], F32, kind="Internal").ap()
    mod_d = nc.dram_tensor("mod_scr", [1, 6 * D], F32, kind="Internal").ap()
    xbuf_d = nc.dram_tensor("xbuf_scr", [NE * CAP + 128, D], BF16, kind="Internal").ap()
    ybuf_d = nc.dram_tensor("ybuf_scr", [NE * CAP, D], F32, kind="Internal").ap()


    with ExitStack() as st:
        P = Prog(nc, st)
        sbuf = lambda stack, name, shape, dt=F32: stack.enter_context(nc.sbuf_tensor(name, list(shape), dt))
        V, A, T, G_ = nc.vector, nc.scalar, nc.tensor, nc.gpsimd

        bank = [st.enter_context(nc.psum_tensor("bank%d" % i, [128, 512], F32)) for i in range(8)]
        tb = trs(8)

        cst = sbuf(st, "cst", [128, NCST]); t_cst = Tr()
        identb = sbuf(st, "identb", [128, 128], BF16)
        mask256 = sbuf(st, "mask256", [128, 256], BF16)
        onesb = sbuf(st, "onesb", [1, 128], BF16)
        trib = sbuf(st, "trib", [128, 128], BF16)
        ones128b = sbuf(st, "ones128b", [128, 128], BF16)
        A1 = sbuf(st, "A1", [128, 8]); S1 = sbuf(st, "S1", [128, 8])
        A2 = sbuf(st, "A2", [128, 8]); S2 = sbuf(st, "S2", [128, 8])
        t_mod = Tr()
        bufA = sbuf(st, "bufA", [128, 16 * 1024], BF16)
        t_mixed = trs(NOWN)
        small = sbuf(st, "small", [128, 64]); t_small = Tr()
        ident = cst[:, 0:128]
        invf = cst[:, 384:385]
        sgn = cst[:, 385:386]
        halfpi = cst[:, 386:387]
        zero_c = cst[:, 387:388]
        one11 = cst[0:1, 388:389]
        ones_row = cst[0:1, 392:520]
        neglam = small[:, 0:1]
        expsink = small[:, 8:16]

        P.dma("sp", lambda: nc.sync.dma_start(out=cst[:], in_=cst_d[:, :]), writes=[t_cst])
        P.op("dve", lambda: V.tensor_copy(out=identb[:], in_=cst[:, 0:128]), reads=[t_cst], writes=[t_cst])
        P.op("dve", lambda: V.tensor_copy(out=mask256[:, 0:128], in_=cst[:, 256:384]), reads=[t_cst], writes=[t_cst])
        P.op("dve", lambda: V.tensor_copy(out=mask256[:, 128:256], in_=cst[:, 128:256]), reads=[t_cst], writes=[t_cst])
        P.op("dve", lambda: V.tensor_copy(out=onesb[:], in_=cst[0:1, 392:520]), reads=[t_cst], writes=[t_cst])
        P.op("dve", lambda: V.tensor_copy(out=trib[:], in_=cst[:, 520:648]), reads=[t_cst], writes=[t_cst])
        P.op("dve", lambda: V.tensor_copy(out=ones128b[:], in_=cst[:, 392:520]), reads=[t_cst], writes=[t_cst])

        with ExitStack() as s0:
            cT = sbuf(s0, "cT_sb", [128, 8]); t_cT = Tr()
            wad = [sbuf(s0, "wad%d" % i, [128, 8, 512]) for i in range(2)]; t_wad = trs(2)
            modrow = sbuf(s0, "modrow", [1, 6 * D]); t_modrow = Tr()
            badar = sbuf(s0, "badar", [1, 6 * D]); t_bada = Tr()
            gT = sbuf(s0, "gT", [128, 16]); t_gT = Tr()
            lamb = sbuf(s0, "lamb", [128, 256]); t_lam = Tr()
            lamp = sbuf(s0, "lamp", [128, 128])
            P.dma("sp", lambda: nc.sync.dma_start(out=cT[:], in_=cT_d[:, :]), writes=[t_cT])
            P.dma("sp", lambda: nc.sync.dma_start(out=badar[:], in_=bada_d[:, :]), writes=[t_bada])
            P.dma("sp", lambda: nc.sync.dma_start(out=gT[:, 0:8], in_=gmixT_d[:, :]), writes=[t_gT])
            P.dma("sp", lambda: nc.sync.dma_start(out=gT[:, 8:16], in_=gffnT_d[:, :]), writes=[t_gT])
            P.dma("sp", lambda: nc.sync.dma_start(out=lamb[:].rearrange("p (a b) -> p a b", a=4),
                                                  in_=lam_d[:, :].partition_broadcast(128)), writes=[t_lam])
            P.dma("sp", lambda: nc.sync.dma_start(out=small[:, 16:24], in_=sinks_d[0:1, :].partition_broadcast(128)), writes=[t_small])
            P.op("act", lambda: A.activation(out=cT[:], in_=cT[:], func=AF.Silu), reads=[t_cT], writes=[t_cT])
            wada_v = wada_d.rearrange("(k p) n -> p k n", p=128)
            for pc in range(12):
                b = pc % 2
                P.dma("sp", lambda pc=pc, b=b: nc.sync.dma_start(out=wad[b][:], in_=wada_v[:, :, pc * 512:(pc + 1) * 512]), writes=[t_wad[b]])
                bk = pc % 2
                P.group("pe", [(lambda kc=kc, b=b, bk=bk: T.matmul(bank[bk][0:1, :], lhsT=cT[:, kc:kc + 1], rhs=wad[b][:, kc, :],
                                                                    start=(kc == 0), stop=(kc == 7))) for kc in range(8)],
                        reads=[t_cT, t_wad[b]], writes=[tb[bk]])
                P.op("dve", lambda pc=pc, bk=bk: V.tensor_tensor(out=modrow[0:1, pc * 512:(pc + 1) * 512], in0=bank[bk][0:1, :],
                                                                 in1=badar[0:1, pc * 512:(pc + 1) * 512], op=ALU.add),
                     reads=[tb[bk], t_bada], writes=[t_modrow])
            cols = [(0, 0), (1, 8), (3, 16), (4, 24)]
            fns = []
            for mi, dc in cols:
                for kc in range(8):
                    fns.append(lambda mi=mi, dc=dc, kc=kc: T.matmul(bank[2][:, dc + kc:dc + kc + 1],
                                                                    lhsT=modrow[0:1, mi * D + kc * 128: mi * D + (kc + 1) * 128],
                                                                    rhs=one11, start=True, stop=True))
            P.group("pe", fns, reads=[t_modrow, t_cst], writes=[tb[2]])
            P.op("dve", lambda: V.tensor_copy(out=S1[:], in_=bank[2][:, 0:8]), reads=[tb[2]], writes=[t_mod])
            P.op("dve", lambda: V.scalar_tensor_tensor(out=A1[:], in0=bank[2][:, 8:16], scalar=1.0, in1=gT[:, 0:8], op0=ALU.add, op1=ALU.mult),
                 reads=[tb[2], t_gT], writes=[t_mod])
            P.op("dve", lambda: V.tensor_copy(out=S2[:], in_=bank[2][:, 16:24]), reads=[tb[2]], writes=[t_mod])
            P.op("dve", lambda: V.scalar_tensor_tensor(out=A2[:], in0=bank[2][:, 24:32], scalar=1.0, in1=gT[:, 8:16], op0=ALU.add, op1=ALU.mult),
                 reads=[tb[2], t_gT], writes=[t_mod])
            P.dma("sp", lambda: nc.sync.dma_start(out=mod_d[:, :], in_=modrow[:]), reads=[t_modrow])
            P.op("dve", lambda: V.tensor_tensor(out=lamp[:, 0:64], in0=lamb[:, 0:64], in1=lamb[:, 64:128], op=ALU.mult), reads=[t_lam], writes=[t_lam])
            P.op("dve", lambda: V.tensor_tensor(out=lamp[:, 64:128], in0=lamb[:, 128:192], in1=lamb[:, 192:256], op=ALU.mult), reads=[t_lam], writes=[t_lam])
            P.op("dve", lambda: V.tensor_reduce(out=small[:, 1:3], in_=lamp[:].rearrange("p (a b) -> p a b", a=2), axis=AX.X, op=ALU.add),
                 reads=[t_lam], writes=[t_small])
            P.op("act", lambda: A.activation(out=small[:, 1:3], in_=small[:, 1:3], func=AF.Exp), reads=[t_small], writes=[t_small])
            P.op("dve", lambda: V.scalar_tensor_tensor(out=small[:, 0:1], in0=small[:, 2:3], scalar=-0.2, in1=small[:, 1:2], op0=ALU.add, op1=ALU.subtract),
                 reads=[t_small], writes=[t_small])
            P.op("act", lambda: A.activation(out=small[:, 8:16], in_=small[:, 16:24], func=AF.Exp), reads=[t_small], writes=[t_small])
            P.barrier()
            P.emit()

        with ExitStack() as s1:
            XB = 8
            xt = [sbuf(s1, "xt%d" % i, [128, D]) for i in range(XB)]; t_xt = trs(XB)
            xn = [sbuf(s1, "xn%d" % i, [128, D], BF16) for i in range(2)]; t_xn = trs(2)
            junk = sbuf(s1, "junk", [128, D], BF16); t_junk = Tr()
            ssq = sbuf(s1, "ssq", [128, 2, 8]); t_ssq = trs(2)
            hTg = [sbuf(s1, "hTg%d" % i, [128, 8, 512], BF16) for i in range(2)]; t_hTg = trs(2)
            posi = sbuf(s1, "posi", [128, 512], I32); t_posi = Tr()
            ang = sbuf(s1, "ang", [128, 512]); t_ang = Tr()
            ki = sbuf(s1, "ki", [128, 512], I32); kf = sbuf(s1, "kf", [128, 512]); t_k = Tr()
            rr = sbuf(s1, "rr", [128, 512]); t_rr = Tr()
            tab = [sbuf(s1, "tab%d" % i, [128, 512]) for i in range(4)]; t_tab = trs(4)
            hT_v = hT_d.rearrange("k p t -> p k t")
            A1b = sbuf(s1, "A1b", [128, D]); S1b = sbuf(s1, "S1b", [128, D]); gmb = sbuf(s1, "gmb", [128, D]); t_m1 = Tr()
            xm = [sbuf(s1, "xm%d" % i, [128, D]) for i in range(2)]; t_xm = trs(2)
            P.dma("sp", lambda: nc.sync.dma_start(out=S1b[:], in_=mod_d[0:1, 0:D].partition_broadcast(128)), writes=[t_m1])
            P.dma("sp", lambda: nc.sync.dma_start(out=A1b[:], in_=mod_d[0:1, D:2 * D].partition_broadcast(128)), writes=[t_m1])
            P.dma("sp", lambda: nc.sync.dma_start(out=gmb[:], in_=gmix_d[0:1, :].partition_broadcast(128)), writes=[t_m1])
            P.op("dve", lambda: V.scalar_tensor_tensor(out=A1b[:], in0=A1b[:], scalar=1.0, in1=gmb[:], op0=ALU.add, op1=ALU.mult), reads=[t_m1], writes=[t_m1])

            def rope_group(g):
                P.dma("sp", lambda: nc.sync.dma_start(out=posi[:], in_=pos_d[0:1, g * 512:(g + 1) * 512].partition_broadcast(128)), writes=[t_posi])
                P.op("dve", lambda: V.tensor_copy(out=ang[:], in_=posi[:]), reads=[t_posi], writes=[t_ang])
                P.op("dve", lambda: V.tensor_scalar(out=ang[:], in0=ang[:], scalar1=invf, scalar2=None, op0=ALU.mult), reads=[t_ang, t_cst], writes=[t_ang])
                for which in range(2):
                    tbi = (2 * g + which) % 4
                    if which == 0:
                        P.op("dve", lambda: V.tensor_scalar(out=ki[:], in0=ang[:], scalar1=INV2PI, scalar2=None, op0=ALU.mult), reads=[t_ang], writes=[t_k])
                    else:
                        P.op("dve", lambda: V.tensor_scalar(out=ki[:], in0=ang[:], scalar1=INV2PI, scalar2=0.25, op0=ALU.mult, op1=ALU.add), reads=[t_ang], writes=[t_k])
                    P.op("dve", lambda: V.tensor_copy(out=kf[:], in_=ki[:]), reads=[t_k], writes=[t_k])
                    P.op("dve", lambda: V.scalar_tensor_tensor(out=rr[:], in0=kf[:], scalar=-C1, in1=ang[:], op0=ALU.mult, op1=ALU.add), reads=[t_k, t_ang], writes=[t_rr])
                    P.op("dve", lambda: V.scalar_tensor_tensor(out=rr[:], in0=kf[:], scalar=-C2, in1=rr[:], op0=ALU.mult, op1=ALU.add), reads=[t_k, t_rr], writes=[t_rr])
                    if which == 0:
                        P.op("dve", lambda: V.tensor_scalar(out=rr[:], in0=rr[:], scalar1=-3.1415925, scalar2=3.1415925, op0=ALU.max, op1=ALU.min), reads=[t_rr], writes=[t_rr])
                        P.op("act", lambda tbi=tbi: A.activation(out=tab[tbi][:], in_=rr[:], func=AF.Sin, scale=sgn, bias=zero_c), reads=[t_rr, t_cst], writes=[t_tab[tbi]])
                        P.dma("sp", lambda tbi=tbi: nc.sync.dma_start(out=sin_d[:, g * 512:(g + 1) * 512], in_=tab[tbi][:]), reads=[t_tab[tbi]])
                    else:
                        P.op("dve", lambda: V.tensor_scalar(out=rr[:], in0=rr[:], scalar1=-4.712388, scalar2=1.570796, op0=ALU.max, op1=ALU.min), reads=[t_rr], writes=[t_rr])
                        P.op("act", lambda tbi=tbi: A.activation(out=tab[tbi][:], in_=rr[:], func=AF.Sin, scale=1.0, bias=halfpi), reads=[t_rr, t_cst], writes=[t_tab[tbi]])
                        P.dma("sp", lambda tbi=tbi: nc.sync.dma_start(out=cos_d[:, g * 512:(g + 1) * 512], in_=tab[tbi][:]), reads=[t_tab[tbi]])
            for g in range(NGX):
                rope_group(g)

            def stageA(g):
                gp = g % 2
                for tt in range(4):
                    t = 4 * g + tt
                    xb = t % XB
                    P.dma("sp", lambda t=t, xb=xb: nc.sync.dma_start(out=xt[xb][:], in_=x_d[t * 128:(t + 1) * 128, :]), writes=[t_xt[xb]])
                    P.op("act", lambda xb=xb, tt=tt: A.activation(out=junk[:], in_=xt[xb][:], func=AF.Square, accum_out=ssq[:, gp, tt:tt + 1]),
                         reads=[t_xt[xb]], writes=[t_junk, t_ssq[gp]])
                P.op("dve", lambda: V.tensor_scalar(out=ssq[:, gp, 4:8], in0=ssq[:, gp, 0:4], scalar1=1.0 / D, scalar2=EPS, op0=ALU.mult, op1=ALU.add), reads=[t_ssq[gp]], writes=[t_ssq[gp]])
                P.op("act", lambda: A.activation(out=ssq[:, gp, 4:8], in_=ssq[:, gp, 4:8], func=AF.Sqrt), reads=[t_ssq[gp]], writes=[t_ssq[gp]])
                P.op("dve", lambda: V.reciprocal(out=ssq[:, gp, 4:8], in_=ssq[:, gp, 4:8]), reads=[t_ssq[gp]], writes=[t_ssq[gp]])

            def stageB(g):
                gp = g % 2
                hb = g % 2

                def tile_b(tt):
                    t = 4 * g + tt
                    xb = t % XB
                    nb = t % 2
                    P.op("dve", lambda: V.scalar_tensor_tensor(out=xm[nb][:], in0=xt[xb][:], scalar=ssq[:, gp, 4 + tt:5 + tt], in1=A1b[:], op0=ALU.mult, op1=ALU.mult),
                         reads=[t_xt[xb], t_ssq[gp], t_m1], writes=[t_xm[nb]])
                    P.op("dve", lambda: V.tensor_tensor(out=xn[nb][:], in0=xm[nb][:], in1=S1b[:], op=ALU.add), reads=[t_xm[nb], t_m1], writes=[t_xn[nb]])
                    bk = nb
                    pT = bank[bk][:, :].bitcast(BF16)
                    P.group("pe", [(lambda kc=kc: T.transpose(out=pT[:, kc * 128:(kc + 1) * 128], in_=xn[nb][:, kc * 128:(kc + 1) * 128], identity=identb[:]))
                                   for kc in range(8)], reads=[t_xn[nb], t_cst], writes=[tb[bk]])
                    P.op("act", lambda: A.activation(out=hTg[hb][:, :, tt * 128:(tt + 1) * 128], in_=pT[:, :].rearrange("p (a b) -> p a b", a=8), func=AF.Copy),
                         reads=[tb[bk]], writes=[t_hTg[hb]])
                for tt in range(4):
                    tile_b(tt)
                P.dma("sp", lambda: nc.sync.dma_start(out=hT_v[:, :, g * 512:(g + 1) * 512], in_=hTg[hb][:]), reads=[t_hTg[hb]])

            for g in range(NGX + 1):
                if g < NGX:
                    stageA(g)
                if g >= 1:
                    stageB(g - 1)
            P.barrier()
            P.emit()

        mixed = bufA[:].rearrange("p (a b) -> p a b", a=NOWN)
        with ExitStack() as s2:
            Wu = sbuf(s2, "Wu", [128, 8, 1664], BF16); t_Wu = Tr()
            KT = sbuf(s2, "KT", [128, S], BF16); t_KT = Tr()
            Vb = sbuf(s2, "Vb", [128, 64 * 130], BF16); t_V = Tr()
            QT = sbuf(s2, "QT", [128, 4, NOWN * 128], BF16); t_QT = Tr()
            hTg = [sbuf(s2, "hTg2_%d" % i, [128, 8, 512], BF16) for i in range(2)]; t_hTg = trs(2)
            csg = [sbuf(s2, "csg%d" % i, [128, 2, 512]) for i in range(2)]; t_csg = trs(2)
            tm1 = [sbuf(s2, "tm1_%d" % i, [128, 512]) for i in range(2)]; t_tm1 = trs(2)
            tm2 = [sbuf(s2, "tm2_%d" % i, [128, 512]) for i in range(2)]; t_tm2 = trs(2)
            PT = [sbuf(s2, "PT%d" % i, [128, 512], BF16) for i in range(3)]; t_PT = trs(3)
            dmask = sbuf(s2, "dmask_sb", [128, NOWN, 512], BF16); t_dmask = Tr()
            smask = sbuf(s2, "smask_sb", [128, NOWN, 256], BF16); t_smask = Tr()
            bselT = sbuf(s2, "bselT", [128, NCH]); t_bsel = Tr()
            vbias = sbuf(s2, "vbias", [128, 128]); t_vbias = Tr()
            gsub_b = sbuf(s2, "gsub_b", [128, 128]); t_gsub = Tr()
            fin = sbuf(s2, "fin", [128, 8 * 128]); t_fin = Tr()
            fsm = sbuf(s2, "fsm", [128, 32]); t_fsm = Tr()
            junk2 = sbuf(s2, "junk2", [128, 128], BF16)
            hT_v = hT_d.rearrange("k p t -> p k t")
            wsel_v = wsel_d.rearrange("(k p) n -> p k n", p=128)
            for q4 in range(4):
                P.dma("pool", lambda q4=q4: G_.dma_start(out=dmask[:, 4 * q4:4 * q4 + 4, :], in_=dmask_d[:, q4 * 2048:(q4 + 1) * 2048].rearrange("p (a b) -> p a b", a=4)),
                      writes=[t_dmask])
            for q4 in range(2):
                P.dma("pool", lambda q4=q4: G_.dma_start(out=smask[:, 8 * q4:8 * q4 + 8, :], in_=smask_d[:, q4 * 2048:(q4 + 1) * 2048].rearrange("p (a b) -> p a b", a=8)),
                      writes=[t_smask])
            P.dma("sp", lambda: nc.sync.dma_start(out=bselT[:], in_=bselT_d[:, :]), writes=[t_bsel])
            P.dma("sp", lambda: nc.sync.dma_start(out=gsub_b[:], in_=gsub_d[0:1, :].partition_broadcast(128)), writes=[t_gsub])
            P.op("dve", lambda: V.tensor_scalar(out=gsub_b[:], in0=gsub_b[:], scalar1=0.8, scalar2=None, op0=ALU.mult), reads=[t_gsub], writes=[t_gsub])
            gcount = [0]

            def rope_proj(u, wc, wcs, hb, cb, ccol, ncol, dst, t_dst, par):
                bA, bB = bank[2 * par], bank[2 * par + 1]
                ci = (u["base"] + wc) // 128
                cis = (u["base"] + wcs) // 128
                P.group("pe", [(lambda kc=kc: T.matmul(bA[:, 0:ncol], lhsT=Wu[:, kc, wc:wc + 128], rhs=hTg[hb][:, kc, ccol:ccol + ncol], start=(kc == 0), stop=(kc == 7)))
                               for kc in range(8)], reads=[t_Wu, t_hTg[hb]], writes=[tb[2 * par]])
                P.group("pe", [(lambda kc=kc: T.matmul(bB[:, 0:ncol], lhsT=Wu[:, kc, wcs:wcs + 128], rhs=hTg[hb][:, kc, ccol:ccol + ncol], start=(kc == 0), stop=(kc == 7)))
                               for kc in range(8)], reads=[t_Wu, t_hTg[hb]], writes=[tb[2 * par + 1]])
                P.op("dve", lambda: V.scalar_tensor_tensor(out=tm1[par][:, 0:ncol], in0=bA[:, 0:ncol], scalar=bselT[:, ci:ci + 1], in1=csg[cb][:, 0, ccol:ccol + ncol],
                                                           op0=ALU.add, op1=ALU.mult), reads=[tb[2 * par], t_bsel, t_csg[cb]], writes=[t_tm1[par]])
                P.op("dve", lambda: V.scalar_tensor_tensor(out=tm2[par][:, 0:ncol], in0=bB[:, 0:ncol], scalar=bselT[:, cis:cis + 1], in1=csg[cb][:, 1, ccol:ccol + ncol],
                                                           op0=ALU.add, op1=ALU.mult), reads=[tb[2 * par + 1], t_bsel, t_csg[cb]], writes=[t_tm2[par]])
                P.op("dve", lambda: V.tensor_tensor(out=dst, in0=tm1[par][:, 0:ncol], in1=tm2[par][:, 0:ncol], op=ALU.add),
                     reads=[t_tm1[par], t_tm2[par]], writes=[t_dst])

            def load_group(g):
                hb = gcount[0] % 2
                gcount[0] += 1
                P.dma("sp", lambda: nc.sync.dma_start(out=hTg[hb][:], in_=hT_v[:, :, g * 512:(g + 1) * 512]), writes=[t_hTg[hb]])
                P.dma("sp", lambda: nc.sync.dma_start(out=csg[hb][:, 0, :], in_=cos_d[:, g * 512:(g + 1) * 512]), writes=[t_csg[hb]])
                P.dma("sp", lambda: nc.sync.dma_start(out=csg[hb][:, 1, :], in_=sin_d[:, g * 512:(g + 1) * 512]), writes=[t_csg[hb]])
                return hb

            pcount = [0]

            def v_proj(u, hb, vt0, vw, swa):
                bk = 4 + (pcount[0] % 2)
                pcount[0] += 1
                ov = u["o_v"]
                fns = []
                for tt in range(4):
                    for kc in range(8):
                        fns.append(lambda tt=tt, kc=kc: T.matmul(bank[bk][:, tt * 128:(tt + 1) * 128], lhsT=hTg[hb][:, kc, tt * 128:(tt + 1) * 128],
                                                                 rhs=Wu[:, kc, ov:ov + 128], start=(kc == 0), stop=(kc == 7)))
                P.group("pe", fns, reads=[t_Wu, t_hTg[hb]], writes=[tb[bk]])
                src = bank[bk][:, :].rearrange("p (a b) -> p a b", a=4)
                vb_b = vbias[:].unsqueeze(1).to_broadcast([128, 4, 128])
                if not swa:
                    dst = Vb[:, vt0 * 129:(vt0 + 4) * 129].rearrange("p (a b) -> p a b", a=4)[:, :, 0:128]
                    P.op("dve", lambda: V.tensor_tensor(out=dst, in0=src, in1=vb_b, op=ALU.add), reads=[tb[bk], t_vbias], writes=[t_V])
                else:
                    for kv in range(2):
                        dst = Vb[:, vt0 * 130:(vt0 + 4) * 130].rearrange("p (a b) -> p a b", a=4)[:, :, kv * 65:kv * 65 + 64]
                        P.op("dve", lambda dst=dst, kv=kv: V.tensor_tensor(out=dst, in0=src[:, :, kv * 64:(kv + 1) * 64],
                                                                          in1=vbias[:, kv * 64:(kv + 1) * 64].unsqueeze(1).to_broadcast([128, 4, 64]), op=ALU.add),
                             reads=[tb[bk], t_vbias], writes=[t_V])

            for ui, u in enumerate(UNITS):
                swa = (ui == 0)
                nc_u = u["ncols"]
                P.dma("pool", lambda u=u, nc_u=nc_u: G_.dma_start(out=Wu[:, :, 0:nc_u], in_=wsel_v[:, :, u["base"]:u["base"] + nc_u]), writes=[t_Wu])
                P.dma("sp", lambda u=u: nc.sync.dma_start(out=vbias[:], in_=bsel_d[0:1, u["base"] + u["o_v"]:u["base"] + u["o_v"] + 128].partition_broadcast(128)),
                      writes=[t_vbias])
                if swa:
                    vv = Vb[:, 0:32 * 130].rearrange("p (a b) -> p a b", a=32)
                    P.op("pool", lambda vv=vv: G_.memset(vv[:, :, 64:65], 1.0), writes=[t_V])
                    P.op("pool", lambda vv=vv: G_.memset(vv[:, :, 129:130], 1.0), writes=[t_V])
                    kv_groups = [(20 + i, i * 512, 4 * i) for i in range(4)] + [(16 + i, 2048 + i * 512, 16 + 4 * i) for i in range(4)]
                elif ui == 1:
                    vv = Vb[:, 0:64 * 129].rearrange("p (a b) -> p a b", a=64)
                    P.op("pool", lambda vv=vv: G_.memset(vv[:, :, 128:129], 1.0), writes=[t_V])
                    kv_groups = [(g, g * 512, 4 * g) for g in range(NG)]
                else:
                    kv_groups = [(g, g * 512, 4 * g) for g in range(NG)]
                par = 0
                for (g, kcol, vt0) in kv_groups:
                    hb = load_group(g)
                    for kc_ in range(u["nk"]):
                        rope_proj(u, u["o_k"] + kc_ * 128, u["o_ks"] + kc_ * 128, hb, hb, 0, 512, KT[:, kc_ * 4096 + kcol:kc_ * 4096 + kcol + 512], t_KT, par)
                        par ^= 1
                    v_proj(u, hb, vt0, None, swa)
                    if swa and g < 20:
                        for qc in range(4):
                            rope_proj(u, u["o_q"] + qc * 128, u["o_qs"] + qc * 128, hb, hb, 0, 512, QT[:, qc, (g - 16) * 512:(g - 15) * 512], t_QT, par)
                            par ^= 1
                if not swa:
                    for g in range(16, 20):
                        hb = load_group(g)
                        rope_proj(u, u["o_q"], u["o_qs"], hb, hb, 0, 512, QT[:, 0, (g - 16) * 512:(g - 15) * 512], t_QT, par)
                        par ^= 1

                items = []
                if swa:
                    for oi in range(NOWN):
                        for hh in range(8):
                            items.append((oi, hh, 0, True))
                else:
                    for oi in range(NOWN):
                        nkb = 8 * (oi // 2) + (4 if oi % 2 == 0 else 8)
                        for m in range(2):
                            for c in range(nkb // 4):
                                items.append((oi, m, c, c == nkb // 4 - 1))

                def qk(n):
                    oi, a, c, last = items[n]
                    sb_ = n % 3
                    if swa:
                        hh = a; half = hh % 2; qc = hh // 2; kvg = hh // 4
                        ps = slice(half * 64, half * 64 + 64)
                        fns = [lambda: T.matmul(bank[sb_][:, 0:128], lhsT=KT[ps, kvg * 4096 + oi * 128:kvg * 4096 + (oi + 1) * 128], rhs=QT[ps, qc, oi * 128:(oi + 1) * 128], start=True, stop=True),
                               lambda: T.matmul(bank[sb_][:, 128:256], lhsT=KT[ps, kvg * 4096 + 2048 + oi * 128:kvg * 4096 + 2048 + (oi + 1) * 128], rhs=QT[ps, qc, oi * 128:(oi + 1) * 128], start=True, stop=True)]
                        ncol = 256
                        mk = smask[:, oi, :]
                        t_mk = t_smask
                    else:
                        m = a
                        ps = slice(m * 64, m * 64 + 64)
                        fns = [(lambda i=i: T.matmul(bank[sb_][:, i * 128:(i + 1) * 128], lhsT=KT[ps, (4 * c + i) * 128:(4 * c + i + 1) * 128],
                                                     rhs=QT[ps, 0, oi * 128:(oi + 1) * 128], start=True, stop=True)) for i in range(4)]
                        ncol = 512
                        mk = dmask[:, oi, :]
                        t_mk = t_dmask
                    P.group("pe", fns, reads=[t_KT, t_QT], writes=[tb[sb_]])
                    P.op("act", lambda: A.activation(out=PT[sb_][:, 0:ncol], in_=bank[sb_][:, 0:ncol], func=AF.Exp, scale=0.125), reads=[tb[sb_]], writes=[t_PT[sb_]])
                    if last:
                        P.op("pool", lambda: G_.tensor_tensor(out=PT[sb_][:, 0:ncol], in0=PT[sb_][:, 0:ncol], in1=mk, op=ALU.mult), reads=[t_PT[sb_], t_mk], writes=[t_PT[sb_]])

                def pv(n):
                    oi, a, c, last = items[n]
                    sb_ = n % 3
                    if swa:
                        hh = a; kvg = hh // 4
                        ob = 3 + (oi % 2) * 2 + (hh // 4)
                        oc = (hh % 4) * 65
                        fns = [lambda: T.matmul(bank[ob][:, oc:oc + 65], lhsT=PT[sb_][:, 0:128], rhs=Vb[:, oi * 130 + kvg * 65: oi * 130 + kvg * 65 + 65], start=True, stop=False),
                               lambda: T.matmul(bank[ob][:, oc:oc + 65], lhsT=PT[sb_][:, 128:256], rhs=Vb[:, (16 + oi) * 130 + kvg * 65: (16 + oi) * 130 + kvg * 65 + 65], start=False, stop=True)]
                    else:
                        m = a
                        ob = 3 + (oi % 2) * 2 + m
                        fns = [(lambda i=i: T.matmul(bank[ob][:, 0:129], lhsT=PT[sb_][:, i * 128:(i + 1) * 128], rhs=Vb[:, (4 * c + i) * 129:(4 * c + i + 1) * 129],
                                                     start=(c == 0 and i == 0), stop=(last and i == 3))) for i in range(4)]
                    P.group("pe", fns, reads=[t_PT[sb_], t_V], writes=[tb[ob]])
                    if swa and a == 7:
                        for hh in range(8):
                            ob2 = 3 + (oi % 2) * 2 + (hh // 4)
                            oc2 = (hh % 4) * 65
                            P.op("dve", lambda hh=hh, ob2=ob2, oc2=oc2: V.tensor_tensor(out=fsm[:, hh:hh + 1], in0=bank[ob2][:, oc2 + 64:oc2 + 65], in1=expsink[:, hh:hh + 1], op=ALU.add),
                                 reads=[tb[ob2], t_small], writes=[t_fsm])
                        P.op("dve", lambda: V.reciprocal(out=fsm[:, 0:8], in_=fsm[:, 0:8]), reads=[t_fsm], writes=[t_fsm])
                        for hh in range(8):
                            ob2 = 3 + (oi % 2) * 2 + (hh // 4)
                            oc2 = (hh % 4) * 65
                            P.op("dve", lambda hh=hh, ob2=ob2, oc2=oc2: V.tensor_scalar(out=mixed[:, oi, hh * 64:(hh + 1) * 64], in0=bank[ob2][:, oc2:oc2 + 64],
                                                                                         scalar1=fsm[:, hh:hh + 1], scalar2=None, op0=ALU.mult),
                                 reads=[tb[ob2], t_fsm], writes=[t_mixed[oi]])
                    if (not swa) and a == 1 and last:
                        h = ui - 1
                        o0 = bank[3 + (oi % 2) * 2]
                        o1 = bank[3 + (oi % 2) * 2 + 1]
                        t0, t1 = tb[3 + (oi % 2) * 2], tb[3 + (oi % 2) * 2 + 1]
                        P.op("dve", lambda: V.reciprocal(out=fsm[:, 16:17], in_=o0[:, 128:129]), reads=[t0], writes=[t_fsm])
                        P.op("dve", lambda: V.reciprocal(out=fsm[:, 17:18], in_=o1[:, 128:129]), reads=[t1], writes=[t_fsm])
                        P.op("dve", lambda: V.tensor_tensor(out=fsm[:, 17:18], in0=fsm[:, 17:18], in1=neglam, op=ALU.mult), reads=[t_fsm, t_small], writes=[t_fsm])
                        P.op("dve", lambda: V.tensor_scalar(out=fin[:, 0:128], in0=o1[:, 0:128], scalar1=fsm[:, 17:18], scalar2=None, op0=ALU.mult), reads=[t1, t_fsm], writes=[t_fin])
                        P.op("dve", lambda: V.scalar_tensor_tensor(out=fin[:, 128:256], in0=o0[:, 0:128], scalar=fsm[:, 16:17], in1=fin[:, 0:128], op0=ALU.mult, op1=ALU.add),
                             reads=[t0, t_fsm, t_fin], writes=[t_fin])
                        P.op("act", lambda: A.activation(out=junk2[:], in_=fin[:, 128:256], func=AF.Square, accum_out=fsm[:, 18:19]), reads=[t_fin], writes=[t_fsm])
                        P.op("dve", lambda: V.tensor_scalar(out=fsm[:, 18:19], in0=fsm[:, 18:19], scalar1=1.0 / 128, scalar2=EPS, op0=ALU.mult, op1=ALU.add), reads=[t_fsm], writes=[t_fsm])
                        P.op("act", lambda: A.activation(out=fsm[:, 18:19], in_=fsm[:, 18:19], func=AF.Sqrt), reads=[t_fsm], writes=[t_fsm])
                        P.op("dve", lambda: V.reciprocal(out=fsm[:, 18:19], in_=fsm[:, 18:19]), reads=[t_fsm], writes=[t_fsm])
                        P.op("dve", lambda: V.scalar_tensor_tensor(out=mixed[:, oi, 512 + h * 128:512 + (h + 1) * 128], in0=fin[:, 128:256], scalar=fsm[:, 18:19], in1=gsub_b[:],
                                                                   op0=ALU.mult, op1=ALU.mult), reads=[t_fin, t_fsm, t_gsub], writes=[t_mixed[oi]])

                LAG = 2
                for n in range(len(items) + LAG):
                    if n < len(items):
                        qk(n)
                    if n >= LAG:
                        pv(n - LAG)
            P.barrier()
            P.emit()

        s34 = st.enter_context(ExitStack())
        dest_i = sbuf(s34, "dest_i", [128, 4 * NOWN], I32); t_dest = trs(NOWN)
        gate4 = sbuf(s34, "gate4", [128, 4 * NOWN]); t_gate4 = trs(NOWN)
        maskb = sbuf(s34, "maskb", [128, NOWN, NE], BF16); t_maskb = trs(NOWN)
        cnt_run = sbuf(s34, "cnt_run", [128, NE]); t_cnt = Tr()
        cnt_i = sbuf(s34, "cnt_i", [1, NE], I32); t_cnti = Tr()
        padidx = sbuf(s34, "padidx", [128, NE], I32); t_pad = Tr()
        t_xbuf = Tr()
        iota32 = cst[:, 648:680]
        e2048 = cst[:, 680:712]
        iota_p = cst[:, 712:713]
        with ExitStack() as s3:
            gt1_b = sbuf(s3, "gt1_b", [128, D])
            A2b = sbuf(s3, "A2b", [128, D]); S2b = sbuf(s3, "S2b", [128, D]); t_m2 = Tr()
            P.dma("sp", lambda: nc.sync.dma_start(out=gt1_b[:], in_=mod_d[0:1, 2 * D:3 * D].partition_broadcast(128)), writes=[t_mod])
            P.dma("sp", lambda: nc.sync.dma_start(out=S2b[:], in_=mod_d[0:1, 3 * D:4 * D].partition_broadcast(128)), writes=[t_m2])
            P.dma("sp", lambda: nc.sync.dma_start(out=A2b[:], in_=mod_d[0:1, 4 * D:5 * D].partition_broadcast(128)), writes=[t_m2])
            wout = sbuf(s3, "wout", [128, 8, D], BF16); t_wout = Tr()
            boutb = sbuf(s3, "boutb", [1, D], BF16)
            wr = sbuf(s3, "wr", [128, 8, NE], BF16); t_wr = Tr()
            brb = sbuf(s3, "brb", [1, NE], BF16)
            gfb = sbuf(s3, "gfb", [128, D]); t_gfb = Tr()
            P.dma("sp", lambda: nc.sync.dma_start(out=gfb[:], in_=gffn_d[0:1, :].partition_broadcast(128)), writes=[t_gfb])
            P.op("dve", lambda: V.scalar_tensor_tensor(out=A2b[:], in0=A2b[:], scalar=1.0, in1=gfb[:], op0=ALU.add, op1=ALU.mult), reads=[t_m2, t_gfb], writes=[t_m2])
            P.op("dve", lambda: V.memset(cnt_run[:], 0.0), writes=[t_cnt])
            mixT = [sbuf(s3, "mixT%d" % i, [128, 8, 128], BF16) for i in range(2)]; t_mixT = trs(2)
            xo = [sbuf(s3, "xo%d" % i, [128, D]) for i in range(2)]; t_xo = trs(2)
            x1t = [sbuf(s3, "x1t%d" % i, [128, D]) for i in range(2)]; t_x1t = trs(2)
            h2f = [sbuf(s3, "h2f%d" % i, [128, D]) for i in range(2)]; t_h2f = trs(2)
            h2tok = [sbuf(s3, "h2tok%d" % i, [128, D], BF16) for i in range(2)]; t_h2tok = trs(2)
            h2Tt = [sbuf(s3, "h2Tt%d" % i, [128, 8, 128], BF16) for i in range(2)]; t_h2Tt = trs(2)
            zrow = sbuf(s3, "zrow", [128, D], BF16); t_zrow = Tr()
            junk3 = sbuf(s3, "junk3", [128, D], BF16); t_junk3 = Tr()
            rs = sbuf(s3, "rs", [128, 64]); t_rs = trs(2)
            lg = sbuf(s3, "lg", [128, 2, 4 * NE]); t_lg = trs(2)
            idx8 = sbuf(s3, "idx8", [128, 2, 8], U32)
            posb = sbuf(s3, "posb", [128, 2, NE]); junkp = sbuf(s3, "junkp", [128, 2, NE])
            wout_v = wout_d.rearrange("(k p) n -> p k n", p=128)
            wr_v = wr_d.rearrange("(k p) n -> p k n", p=128)
            P.dma("pool", lambda: G_.dma_start(out=wout[:], in_=wout_v), writes=[t_wout])
            P.dma("pool", lambda: G_.dma_start(out=boutb[:], in_=bout_d[:, :]), writes=[t_wout])
            P.dma("pool", lambda: G_.dma_start(out=wr[:], in_=wr_v), writes=[t_wr])
            P.dma("pool", lambda: G_.dma_start(out=brb[:], in_=br_d[:, :]), writes=[t_wr])
            P.op("pool", lambda: G_.memset(zrow[:], 0.0), writes=[t_zrow])

            def p3(oi):
                b = oi % 2
                P.dma("sp", lambda: nc.sync.dma_start(out=xo[b][:], in_=x_d[S + oi * 128:S + (oi + 1) * 128, :]), writes=[t_xo[b]])
                pT = bank[b][:, :].bitcast(BF16)
                P.group("pe", [(lambda kc=kc: T.transpose(out=pT[:, kc * 128:(kc + 1) * 128], in_=mixed[:, oi, kc * 128:(kc + 1) * 128], identity=identb[:])) for kc in range(8)],
                        reads=[t_mixed[oi], t_cst], writes=[tb[b]])
                P.op("act", lambda: A.activation(out=mixT[b][:].rearrange("p a b -> p (a b)"), in_=pT[:, :], func=AF.Copy), reads=[tb[b]], writes=[t_mixT[b]])
                for hf in range(2):
                    bk = 2 + 2 * b + hf
                    fns = [(lambda kc=kc, hf=hf, bk=bk: T.matmul(bank[bk][:, :], lhsT=mixT[b][:, kc, :], rhs=wout[:, kc, hf * 512:(hf + 1) * 512], start=(kc == 0), stop=False)) for kc in range(8)]
                    fns.append(lambda hf=hf, bk=bk: T.matmul(bank[bk][:, :], lhsT=onesb[0:1, :], rhs=boutb[0:1, hf * 512:(hf + 1) * 512], start=False, stop=True))
                    P.group("pe", fns, reads=[t_mixT[b], t_wout, t_cst], writes=[tb[bk]])
                    P.op("dve", lambda hf=hf, bk=bk: V.tensor_tensor(out=x1t[b][:, hf * 512:(hf + 1) * 512], in0=bank[bk][:, :], in1=gt1_b[:, hf * 512:(hf + 1) * 512], op=ALU.mult),
                         reads=[tb[bk], t_mod], writes=[t_x1t[b]])
                P.op("dve", lambda: V.tensor_tensor(out=x1t[b][:], in0=x1t[b][:], in1=xo[b][:], op=ALU.add), reads=[t_x1t[b], t_xo[b]], writes=[t_x1t[b]])
                P.dma("sp", lambda: nc.sync.dma_start(out=x1_d[oi * 128:(oi + 1) * 128, :], in_=x1t[b][:]), reads=[t_x1t[b]])
                r0 = 32 * b
                P.op("act", lambda: A.activation(out=junk3[:], in_=x1t[b][:], func=AF.Square, accum_out=rs[:, r0:r0 + 1]), reads=[t_x1t[b]], writes=[t_junk3, t_rs[b]])
                P.op("dve", lambda: V.tensor_scalar(out=rs[:, r0 + 1:r0 + 2], in0=rs[:, r0:r0 + 1], scalar1=1.0 / D, scalar2=EPS, op0=ALU.mult, op1=ALU.add), reads=[t_rs[b]], writes=[t_rs[b]])
                P.op("act", lambda: A.activation(out=rs[:, r0 + 1:r0 + 2], in_=rs[:, r0 + 1:r0 + 2], func=AF.Sqrt), reads=[t_rs[b]], writes=[t_rs[b]])
                P.op("dve", lambda: V.reciprocal(out=rs[:, r0 + 1:r0 + 2], in_=rs[:, r0 + 1:r0 + 2]), reads=[t_rs[b]], writes=[t_rs[b]])
                P.op("dve", lambda: V.scalar_tensor_tensor(out=h2f[b][:], in0=x1t[b][:], scalar=rs[:, r0 + 1:r0 + 2], in1=A2b[:], op0=ALU.mult, op1=ALU.mult),
                     reads=[t_x1t[b], t_rs[b], t_m2], writes=[t_h2f[b]])
                P.op("dve", lambda: V.tensor_tensor(out=h2tok[b][:], in0=h2f[b][:], in1=S2b[:], op=ALU.add), reads=[t_h2f[b], t_m2], writes=[t_h2tok[b]])
                bk = 6 + b
                pT2 = bank[bk][:, :].bitcast(BF16)
                P.group("pe", [(lambda kc=kc: T.transpose(out=pT2[:, kc * 128:(kc + 1) * 128], in_=h2tok[b][:, kc * 128:(kc + 1) * 128], identity=identb[:])) for kc in range(8)],
                        reads=[t_h2tok[b], t_cst], writes=[tb[bk]])
                P.op("act", lambda: A.activation(out=h2Tt[b][:].rearrange("p a b -> p (a b)"), in_=pT2[:, :], func=AF.Copy), reads=[tb[bk]], writes=[t_h2Tt[b]])
                fns = [(lambda kc=kc: T.matmul(bank[b][:, 0:NE], lhsT=h2Tt[b][:, kc, :], rhs=wr[:, kc, :], start=(kc == 0), stop=False)) for kc in range(8)]
                fns.append(lambda: T.matmul(bank[b][:, 0:NE], lhsT=onesb[0:1, :], rhs=brb[0:1, :], start=False, stop=True))
                P.group("pe", fns, reads=[t_h2Tt[b], t_wr, t_cst], writes=[tb[b]])
                L0, L1, L2, L3 = lg[:, b, 0:NE], lg[:, b, NE:NE + 8], lg[:, b, 2 * NE:3 * NE], lg[:, b, 3 * NE:3 * NE + 8]
                P.op("dve", lambda: V.tensor_copy(out=L0, in_=bank[b][:, 0:NE]), reads=[tb[b]], writes=[t_lg[b]])
                P.op("dve", lambda: V.max(out=L1, in_=L0), reads=[t_lg[b]], writes=[t_lg[b]])
                P.op("dve", lambda: V.max_index(out=idx8[:, b, :], in_max=L1, in_values=L0), reads=[t_lg[b]], writes=[t_lg[b]])
                P.op("dve", lambda: V.tensor_scalar(out=maskb[:, oi, :], in0=L0, scalar1=lg[:, b, NE + 3:NE + 4], scalar2=None, op0=ALU.is_ge), reads=[t_lg[b]], writes=[t_maskb[oi]])
                P.op("dve", lambda: V.tensor_scalar(out=rs[:, r0 + 2:r0 + 3], in0=lg[:, b, NE:NE + 1], scalar1=-1.0, scalar2=None, op0=ALU.mult), reads=[t_lg[b]], writes=[t_rs[b]])
                P.op("act", lambda: A.activation(out=L3[:, 0:4], in_=L1[:, 0:4], func=AF.Exp, bias=rs[:, r0 + 2:r0 + 3], scale=1.0, accum_out=rs[:, r0 + 3:r0 + 4]),
                     reads=[t_lg[b], t_rs[b]], writes=[t_lg[b], t_rs[b]])
                P.op("dve", lambda: V.reciprocal(out=rs[:, r0 + 3:r0 + 4], in_=rs[:, r0 + 3:r0 + 4]), reads=[t_rs[b]], writes=[t_rs[b]])
                P.op("dve", lambda: V.tensor_scalar(out=gate4[:, 4 * oi:4 * oi + 4], in0=L3[:, 0:4], scalar1=rs[:, r0 + 3:r0 + 4], scalar2=None, op0=ALU.mult),
                     reads=[t_lg[b], t_rs[b]], writes=[t_gate4[oi]])
                pb = bank[b]
                P.group("pe", [lambda: T.matmul(pb[:, 64:64 + NE], lhsT=trib[:], rhs=maskb[:, oi, :], start=True, stop=True),
                               lambda: T.matmul(pb[:, 128:128 + NE], lhsT=ones128b[:], rhs=maskb[:, oi, :], start=True, stop=True)],
                        reads=[t_maskb[oi], t_cst, t_lg[b]], writes=[tb[b]])
                P.op("dve", lambda: V.tensor_tensor(out=posb[:, b, :], in0=pb[:, 64:64 + NE], in1=cnt_run[:], op=ALU.add), reads=[tb[b], t_cnt], writes=[t_lg[b]])
                P.op("dve", lambda: V.tensor_tensor(out=cnt_run[:], in0=pb[:, 128:128 + NE], in1=cnt_run[:], op=ALU.add), reads=[tb[b], t_cnt, t_lg[b]], writes=[t_cnt])
                EK = rs[:, r0 + 8:r0 + 12]; PK = rs[:, r0 + 12:r0 + 16]; DF = rs[:, r0 + 16:r0 + 20]
                P.op("dve", lambda: V.tensor_copy(out=EK, in_=idx8[:, b, 0:4]), reads=[t_lg[b]], writes=[t_rs[b]])
                for k in range(4):
                    P.op("dve", lambda k=k: V.scalar_tensor_tensor(out=junkp[:, b, :], in0=iota32, scalar=rs[:, r0 + 8 + k:r0 + 9 + k], in1=posb[:, b, :],
                                                                   op0=ALU.is_equal, op1=ALU.mult, accum_out=rs[:, r0 + 12 + k:r0 + 13 + k]),
                         reads=[t_lg[b], t_rs[b], t_cst], writes=[t_rs[b]])
                P.op("dve", lambda: V.scalar_tensor_tensor(out=DF, in0=EK, scalar=float(CAP), in1=PK, op0=ALU.mult, op1=ALU.add), reads=[t_rs[b]], writes=[t_rs[b]])
                P.op("dve", lambda: V.tensor_copy(out=dest_i[:, 4 * oi:4 * oi + 4], in_=DF), reads=[t_rs[b]], writes=[t_dest[oi]])
                for k in range(4):
                    P.dma("pool", lambda k=k: G_.indirect_dma_start(out=xbuf_d[:, :], out_offset=bass.IndirectOffsetOnAxis(ap=dest_i[:, 4 * oi + k:4 * oi + k + 1], axis=0),
                                                                    in_=h2tok[b][:, :], in_offset=None),
                          reads=[t_h2tok[b], t_dest[oi]], writes=[t_xbuf])
            for oi in range(NOWN):
                p3(oi)
            cf = rs[0:1, 0:NE]
            P.op("dve", lambda: V.tensor_scalar(out=cf, in0=cnt_run[0:1, :], scalar1=127.0, scalar2=1.0 / 128, op0=ALU.add, op1=ALU.mult), reads=[t_cnt] + t_rs, writes=t_rs)
            P.op("dve", lambda: V.tensor_scalar(out=cf, in0=cf, scalar1=-0.496, scalar2=None, op0=ALU.add), reads=t_rs, writes=t_rs)
            P.op("dve", lambda: V.tensor_copy(out=cnt_i[:], in_=cf), reads=t_rs, writes=[t_cnti])
            P.op("dve", lambda: V.tensor_scalar(out=posb[:, 0, :], in0=cnt_run[:], scalar1=iota_p, scalar2=None, op0=ALU.add), reads=[t_cnt, t_cst] + t_lg, writes=t_lg)
            P.op("dve", lambda: V.tensor_scalar(out=posb[:, 1, :], in0=posb[:, 0, :], scalar1=float(CAP), scalar2=None, op0=ALU.is_ge), reads=t_lg, writes=t_lg)
            P.op("dve", lambda: V.tensor_tensor(out=posb[:, 0, :], in0=posb[:, 0, :], in1=e2048, op=ALU.add), reads=t_lg + [t_cst], writes=t_lg)
            P.op("dve", lambda: V.tensor_scalar(out=rs[:, 40:41], in0=iota_p, scalar1=float(NE * CAP), scalar2=None, op0=ALU.add), reads=[t_cst] + t_rs, writes=t_rs)
            P.op("dve", lambda: V.tensor_scalar(out=junkp[:, 0, :], in0=posb[:, 0, :], scalar1=-1.0, scalar2=rs[:, 40:41], op0=ALU.mult, op1=ALU.add), reads=t_lg + t_rs, writes=t_lg)
            P.op("dve", lambda: V.tensor_tensor(out=junkp[:, 0, :], in0=junkp[:, 0, :], in1=posb[:, 1, :], op=ALU.mult), reads=t_lg, writes=t_lg)
            P.op("dve", lambda: V.tensor_tensor(out=posb[:, 0, :], in0=posb[:, 0, :], in1=junkp[:, 0, :], op=ALU.add), reads=t_lg, writes=t_lg)
            P.op("dve", lambda: V.tensor_copy(out=padidx[:], in_=posb[:, 0, :]), reads=t_lg, writes=[t_pad])
            for e in range(NE):
                P.dma("pool", lambda e=e: G_.indirect_dma_start(out=xbuf_d[:, :], out_offset=bass.IndirectOffsetOnAxis(ap=padidx[:, e:e + 1], axis=0),
                                                                in_=zrow[:, :], in_offset=None),
                      reads=[t_zrow, t_pad], writes=[t_xbuf])
            P.barrier()
            P.emit()

        if int(os.environ.get('K_STOP', '9')) <= 3:
            return nc
        with ExitStack() as s4:
            w1b = [sbuf(s4, "w1b%d" % i, [128, 8, 2 * D], BF16) for i in range(2)]
            w2b = [sbuf(s4, "w2b%d" % i, [128, 8, D], BF16) for i in range(2)]
            b1r = [sbuf(s4, "b1r%d" % i, [1, 2 * D], BF16) for i in range(2)]
            b2r = [sbuf(s4, "b2r%d" % i, [1, D], BF16) for i in range(2)]
            t_w = trs(2)
            Xtok = [sbuf(s4, "Xtok%d" % i, [128, D], BF16) for i in range(2)]; t_Xtok = trs(2)
            XT = [sbuf(s4, "XT%d" % i, [128, 8, 128], BF16) for i in range(2)]; t_XT = trs(2)
            gg = [sbuf(s4, "gg%d" % i, [128, 256]) for i in range(2)]; t_gg = trs(2)
            sg = [sbuf(s4, "sg%d" % i, [128, 256]) for i in range(2)]; t_sg = trs(2)
            ll = [sbuf(s4, "ll%d" % i, [128, 256]) for i in range(2)]; t_ll = trs(2)
            atok = [sbuf(s4, "atok%d" % i, [128, D], BF16) for i in range(2)]; t_atok = trs(2)
            aT = [sbuf(s4, "aT%d" % i, [128, 8, 128], BF16) for i in range(2)]; t_aT = trs(2)
            yt = [sbuf(s4, "yt%d" % i, [128, D]) for i in range(2)]; t_yt = trs(2)
            t_ybuf = Tr()
            w1_v = w1_d.rearrange("e (k p) n -> e p k n", p=128)
            w2_v = w2_d.rearrange("e (k p) n -> e p k n", p=128)
            blk = [0]

            def block_body(e, bslot, ws):
                n = blk[0]
                blk[0] += 1
                xb = n % 2
                row0 = e * CAP + bslot * 128
                P.dma("sp", lambda: nc.sync.dma_start(out=Xtok[xb][:], in_=xbuf_d[row0:row0 + 128, :]), reads=[t_xbuf], writes=[t_Xtok[xb]], slot=xb)
                pT = bank[xb][:, :].bitcast(BF16)
                P.group("pe", [(lambda kc=kc: T.transpose(out=pT[:, kc * 128:(kc + 1) * 128], in_=Xtok[xb][:, kc * 128:(kc + 1) * 128], identity=identb[:])) for kc in range(8)],
                        reads=[t_Xtok[xb], t_cst], writes=[tb[xb]])
                P.op("act", lambda: A.activation(out=XT[xb][:].rearrange("p a b -> p (a b)"), in_=pT[:, :], func=AF.Copy), reads=[tb[xb]], writes=[t_XT[xb]])
                def do_cch(cch):
                    bk = 2 + (cch % 2)
                    par = cch % 2
                    fns = [(lambda kc=kc: T.matmul(bank[bk][:, :], lhsT=XT[xb][:, kc, :], rhs=w1b[ws][:, kc, cch * 512:(cch + 1) * 512], start=(kc == 0), stop=False)) for kc in range(8)]
                    fns.append(lambda: T.matmul(bank[bk][:, :], lhsT=onesb[0:1, :], rhs=b1r[ws][0:1, cch * 512:(cch + 1) * 512], start=False, stop=True))
                    P.group("pe", fns, reads=[t_XT[xb], t_w[ws], t_cst], writes=[tb[bk]])
                    P.op("dve", lambda: V.tensor_scalar(out=gg[par][:], in0=bank[bk][:, 0:512:2], scalar1=7.0, scalar2=None, op0=ALU.min), reads=[tb[bk]], writes=[t_gg[par]])
                    P.op("act", lambda: A.activation(out=sg[par][:], in_=gg[par][:], func=AF.Gelu_apprx_sigmoid), reads=[t_gg[par]], writes=[t_sg[par]])
                    P.op("dve", lambda: V.tensor_scalar(out=ll[par][:], in0=bank[bk][:, 1:512:2], scalar1=7.0, scalar2=-7.0, op0=ALU.min, op1=ALU.max), reads=[tb[bk]], writes=[t_ll[par]])
                    P.op("dve", lambda: V.scalar_tensor_tensor(out=atok[xb][:, cch * 256:(cch + 1) * 256], in0=ll[par][:], scalar=1.0, in1=sg[par][:], op0=ALU.add, op1=ALU.mult),
                         reads=[t_ll[par], t_sg[par]], writes=[t_atok[xb]])
                for cch in range(4):
                    do_cch(cch)
                bk = 4 + xb
                pT2 = bank[bk][:, :].bitcast(BF16)
                P.group("pe", [(lambda j=j: T.transpose(out=pT2[:, j * 128:(j + 1) * 128], in_=atok[xb][:, j * 128:(j + 1) * 128], identity=identb[:])) for j in range(8)],
                        reads=[t_atok[xb], t_cst], writes=[tb[bk]])
                P.op("act", lambda: A.activation(out=aT[xb][:].rearrange("p a b -> p (a b)"), in_=pT2[:, :], func=AF.Copy), reads=[tb[bk]], writes=[t_aT[xb]])
                def do_hf(hf):
                    bk2 = 6 + hf
                    fns = [(lambda j=j: T.matmul(bank[bk2][:, :], lhsT=aT[xb][:, j, :], rhs=w2b[ws][:, j, hf * 512:(hf + 1) * 512], start=(j == 0), stop=False)) for j in range(8)]
                    fns.append(lambda: T.matmul(bank[bk2][:, :], lhsT=onesb[0:1, :], rhs=b2r[ws][0:1, hf * 512:(hf + 1) * 512], start=False, stop=True))
                    P.group("pe", fns, reads=[t_aT[xb], t_w[ws], t_cst], writes=[tb[bk2]])
                    if hf == 0:
                        P.op("act", lambda: A.activation(out=yt[xb][:, 0:512], in_=bank[bk2][:, :], func=AF.Copy), reads=[tb[bk2]], writes=[t_yt[xb]])
                    else:
                        P.op("dve", lambda: V.tensor_copy(out=yt[xb][:, 512:1024], in_=bank[bk2][:, :]), reads=[tb[bk2]], writes=[t_yt[xb]])
                for hf in range(2):
                    do_hf(hf)
                P.dma("sp", lambda: nc.sync.dma_start(out=ybuf_d[row0:row0 + 128, :], in_=yt[xb][:]), reads=[t_yt[xb]], writes=[t_ybuf], slot=2 + xb)

            for e in range(int(os.environ.get('K_NE', NE))):
                ws = e % 2
                P.dma("pool", lambda e=e, ws=ws: G_.dma_start(out=w1b[ws][:], in_=w1_v[e]), writes=[t_w[ws]])
                P.dma("pool", lambda e=e, ws=ws: G_.dma_start(out=w2b[ws][:], in_=w2_v[e]), writes=[t_w[ws]])
                P.dma("pool", lambda e=e, ws=ws: G_.dma_start(out=b1r[ws][:], in_=b1_d[e:e + 1, :]), writes=[t_w[ws]])
                P.dma("pool", lambda e=e, ws=ws: G_.dma_start(out=b2r[ws][:], in_=b2_d[e:e + 1, :]), writes=[t_w[ws]])
                P.regload(cnt_i[0:1, e:e + 1], reads=[t_cnti])
                NB = CAP // 128
                for bslot in range(NB):
                    if bslot in (3, 6, 10):
                        P.cond_begin(bslot + 1)
                    P.cond_begin(bslot + 1)
                    block_body(e, bslot, ws)
                    P.cond_end()
                for _ in range(3):
                    P.cond_end()
            P.barrier()
            P.emit()

        if int(os.environ.get('K_STOP', '9')) <= 4:
            return nc
        with ExitStack() as s5:
            gt2_b = sbuf(s5, "gt2_b", [128, D]); gfin_b = sbuf(s5, "gfin_b", [128, D]); t_g5 = Tr()
            yk = [sbuf(s5, "yk%d" % i, [128, D]) for i in range(4)]; t_yk = trs(4)
            acc = [sbuf(s5, "acc%d" % i, [128, D]) for i in range(2)]; t_acc = trs(2)
            x1b = [sbuf(s5, "x1b%d" % i, [128, D]) for i in range(2)]; t_x1b = trs(2)
            ob_ = [sbuf(s5, "ob%d" % i, [128, D]) for i in range(2)]; t_ob = trs(2)
            junk5 = sbuf(s5, "junk5", [128, D], BF16); t_junk5 = Tr()
            fs5 = sbuf(s5, "fs5", [128, 8]); t_fs5 = trs(2)
            P.dma("sp", lambda: nc.sync.dma_start(out=gt2_b[:], in_=mod_d[0:1, 5 * D:6 * D].partition_broadcast(128)), writes=[t_g5])
            P.dma("sp", lambda: nc.sync.dma_start(out=gfin_b[:], in_=gfin_d[0:1, :].partition_broadcast(128)), writes=[t_g5])

            def p5(oi):
                b = oi % 2
                P.dma("sp", lambda: nc.sync.dma_start(out=x1b[b][:], in_=x1_d[oi * 128:(oi + 1) * 128, :]), writes=[t_x1b[b]])
                for k in range(4):
                    P.dma("pool", lambda k=k: G_.indirect_dma_start(out=yk[k][:, :], out_offset=None, in_=ybuf_d[:, :],
                                                                    in_offset=bass.IndirectOffsetOnAxis(ap=dest_i[:, 4 * oi + k:4 * oi + k + 1], axis=0),
                                                                    ), reads=[t_dest[oi]], writes=[t_yk[k]])
                    if k == 0:
                        P.op("dve", lambda: V.tensor_scalar(out=acc[b][:], in0=yk[0][:], scalar1=gate4[:, 4 * oi:4 * oi + 1], scalar2=None, op0=ALU.mult),
                             reads=[t_yk[0], t_gate4[oi]], writes=[t_acc[b]])
                    else:
                        P.op("dve", lambda k=k: V.scalar_tensor_tensor(out=acc[b][:], in0=yk[k][:], scalar=gate4[:, 4 * oi + k:4 * oi + k + 1], in1=acc[b][:], op0=ALU.mult, op1=ALU.add),
                             reads=[t_yk[k], t_gate4[oi], t_acc[b]], writes=[t_acc[b]])
                P.op("dve", lambda: V.tensor_tensor(out=acc[b][:], in0=acc[b][:], in1=gt2_b[:], op=ALU.mult), reads=[t_acc[b], t_g5], writes=[t_acc[b]])
                P.op("dve", lambda: V.tensor_tensor(out=x1b[b][:], in0=acc[b][:], in1=x1b[b][:], op=ALU.add), reads=[t_acc[b], t_x1b[b]], writes=[t_x1b[b]])
                P.op("act", lambda: A.activation(out=junk5[:], in_=x1b[b][:], func=AF.Square, accum_out=fs5[:, 4 * b:4 * b + 1]), reads=[t_x1b[b]], writes=[t_junk5, t_fs5[b]])
                P.op("dve", lambda: V.tensor_scalar(out=fs5[:, 4 * b + 1:4 * b + 2], in0=fs5[:, 4 * b:4 * b + 1], scalar1=1.0 / D, scalar2=EPS, op0=ALU.mult, op1=ALU.add),
                     reads=[t_fs5[b]], writes=[t_fs5[b]])
                P.op("act", lambda: A.activation(out=fs5[:, 4 * b + 1:4 * b + 2], in_=fs5[:, 4 * b + 1:4 * b + 2], func=AF.Sqrt), reads=[t_fs5[b]], writes=[t_fs5[b]])
                P.op("dve", lambda: V.reciprocal(out=fs5[:, 4 * b + 1:4 * b + 2], in_=fs5[:, 4 * b + 1:4 * b + 2]), reads=[t_fs5[b]], writes=[t_fs5[b]])
                P.op("dve", lambda: V.scalar_tensor_tensor(out=ob_[b][:], in0=x1b[b][:], scalar=fs5[:, 4 * b + 1:4 * b + 2], in1=gfin_b[:], op0=ALU.mult, op1=ALU.mult),
                     reads=[t_x1b[b], t_fs5[b], t_g5], writes=[t_ob[b]])
                P.dma("sp", lambda: nc.sync.dma_start(out=out_d[oi * 128:(oi + 1) * 128, :], in_=ob_[b][:]), reads=[t_ob[b]])
            for oi in range(NOWN):
                p5(oi)
            P.barrier()
            P.emit()
    return nc


def _consts():
    c = np.zeros((128, NCST), np.float32)
    c[:, 0:128] = np.eye(128, dtype=np.float32)
    k = np.arange(128)[:, None]
    q = np.arange(128)[None, :]
    c[:, 128:256] = (k <= q)
    c[:, 256:384] = (k > q)
    inv = (1.0 / (np.float32(10000.0) ** (np.arange(0, 64, 2, dtype=np.float32) / np.float32(64)))).astype(np.float32)
    p = np.arange(128)
    c[:, 384] = inv[p % 32]
    c[:, 385] = np.where((p % 64) < 32, -1.0, 1.0)
    c[:, 386] = np.float32(math.pi / 2)
    c[:, 387] = 0.0
    c[:, 388] = 1.0
    c[:, 392:520] = 1.0
    c[:, 520:648] = (k < q)
    c[:, 648:680] = np.arange(32)[None, :]
    c[:, 680:712] = (np.arange(32) * CAP)[None, :]
    c[:, 712] = np.arange(128)
    return c


def _core_masks(j):
    own = own_blocks(j)
    k = np.arange(128)[:, None]
    q = np.arange(128)[None, :]
    tri = (k <= q).astype(np.float32)
    low = (k > q).astype(np.float32)
    dm = np.zeros((128, NOWN, 4, 128), np.float32)
    sm = np.zeros((128, NOWN, 2, 128), np.float32)
    for oi, gb in enumerate(own):
        nkb = 8 * (oi // 2) + (4 if oi % 2 == 0 else 8)
        for i in range(4):
            kb = nkb - 4 + i
            if kb < gb:
                dm[:, oi, i, :] = 1.0
            elif kb == gb:
                dm[:, oi, i, :] = tri
        sm[:, oi, 1, :] = tri
        if gb > 0:
            sm[:, oi, 0, :] = low
    return dm.reshape(128, NOWN * 512), sm.reshape(128, NOWN * 256)


_NC_CACHE = {}


def kernel(x, c, positions, w_ada, b_ada, g_mix, w_in, b_in, attn_sinks, lambda_q1, lambda_k1, lambda_q2, lambda_k2,
           g_subln, w_out, b_out, g_ffn, w_router, b_router, w1, b1, w2, b2, g_final):
    f = lambda a: np.ascontiguousarray(np.asarray(a))
    x = f(x); positions = f(positions)
    if "nc" not in _NC_CACHE:
        _NC_CACHE["nc"] = build_program()
    nc = _NC_CACHE["nc"]
    colT = lambda v: f(np.asarray(v).reshape(-1, 128).T)
    w_sel = f(np.asarray(w_in)[0][:, SEL])
    b_sel = f(np.asarray(b_in)[0][SEL])
    b1_ = np.asarray(b1)[0]
    shared = {
        "w_ada": f(np.asarray(w_ada)[0]), "b_ada": f(np.asarray(b_ada)[0][None, :]),
        "gmixT": colT(np.asarray(g_mix)[0]), "gffnT": colT(np.asarray(g_ffn)[0]),
        "w_sel": w_sel, "b_selT": colT(b_sel), "b_sel": f(b_sel[None, :]),
        "sinks": f(np.asarray(attn_sinks)[0][None, :]),
        "lam4": f(np.stack([np.asarray(lambda_q1)[0], np.asarray(lambda_k1)[0], np.asarray(lambda_q2)[0], np.asarray(lambda_k2)[0]])),
        "g_subln": f(np.asarray(g_subln)[0][None, :]),
        "w_out": f(np.asarray(w_out)[0]), "b_out": f(np.asarray(b_out)[0][None, :]),
        "w_router": f(np.asarray(w_router)[0]), "b_router": f(np.asarray(b_router)[0][None, :]),
        "w1": f(np.asarray(w1)[0]), "w2": f(np.asarray(w2)[0]), "b2": f(np.asarray(b2)[0]),
        "b1": f(b1_), "g_ffn": f(np.asarray(g_ffn)[0][None, :]), "g_mix": f(np.asarray(g_mix)[0][None, :]),
        "g_final": f(np.asarray(g_final)[None, :]),
        "consts": _consts(),
    }
    in_maps = []
    rows_all = []
    for core in range(8):
        b, j = core // 4, core % 4
        own = own_blocks(j)
        rows_own = np.concatenate([np.arange(g * 128, (g + 1) * 128) for g in own])
        rows_prev = np.concatenate([np.arange(max(g - 1, 0) * 128, (max(g - 1, 0) + 1) * 128) for g in own])
        rows_all.append(rows_own)
        xb = x[b]
        x_ext = np.concatenate([xb, xb[rows_own], xb[rows_prev]], axis=0)
        pb = positions[b]
        pos_ext = np.concatenate([pb, pb[rows_own], pb[rows_prev]])[None, :].astype(np.int32)
        dm, sm = _core_masks(j)
        m = dict(shared)
        m.update({"x": f(x_ext), "pos": f(pos_ext), "cT": colT(np.asarray(c)[b]), "dmask": dm, "smask": sm})
        in_maps.append(m)
    res = run_bass_kernel_spmd(nc, in_maps, core_ids=list(range(8)))
    out = np.zeros((2, S, D), np.float32)
    for core in range(8):
        out[core // 4, rows_all[core], :] = np.asarray(res.results[core]["out"])
    return out
```

```python
import math
import os
from contextlib import ExitStack

import numpy as np
import concourse.bass as bass
import concourse.mybir as mybir
from concourse.bass_utils import run_bass_kernel_spmd

F32 = mybir.dt.float32
BF16 = mybir.dt.bfloat16
I32 = mybir.dt.int32
ALU = mybir.AluOpType
AF = mybir.ActivationFunctionType
AX = mybir.AxisListType

D = 1024
S = 8192
NT = 64
NG = 16
NOWN = 16
NE = 32
SX = S + 2 * NOWN * 128
NGX = SX // 512
NCST = 720
CAP = 2048
U32 = mybir.dt.uint32
EPS = 1e-5
C1 = 6.28125
C2 = 2 * math.pi - 6.28125
INV2PI = float(1.0 / (2 * math.pi))

OFF_QA, OFF_KA, OFF_VA, OFF_QD, OFF_KD, OFF_VD = 0, 512, 640, 768, 1280, 1792


def _swap64(cols):
    cols = np.asarray(cols).reshape(-1, 64)
    return np.concatenate([cols[:, 32:], cols[:, :32]], axis=1).reshape(-1)


def _unit_cols():
    units = []
    k = np.concatenate([np.tile(np.arange(OFF_KA + g * 64, OFF_KA + (g + 1) * 64), 2) for g in range(2)])
    q = np.arange(OFF_QA, OFF_QA + 512)
    v = np.arange(OFF_VA, OFF_VA + 128)
    units.append(dict(nk=2, nq=4, k=k, q=q, v=v))
    for h in range(4):
        k = np.arange(OFF_KD + h * 128, OFF_KD + (h + 1) * 128)
        q = np.arange(OFF_QD + h * 128, OFF_QD + (h + 1) * 128)
        v = np.arange(OFF_VD + h * 128, OFF_VD + (h + 1) * 128)
        units.append(dict(nk=1, nq=1, k=k, q=q, v=v))
    off = 0
    sel = []
    for u in units:
        u["base"] = off
        parts = [u["k"], _swap64(u["k"]), u["q"], _swap64(u["q"]), u["v"]]
        u["o_k"] = 0
        u["o_ks"] = len(u["k"])
        u["o_q"] = u["o_ks"] + len(u["k"])
        u["o_qs"] = u["o_q"] + len(u["q"])
        u["o_v"] = u["o_qs"] + len(u["q"])
        u["ncols"] = u["o_v"] + 128
        sel.append(np.concatenate(parts))
        off += u["ncols"]
    return units, np.concatenate(sel)


UNITS, SEL = _unit_cols()
NSEL = len(SEL)
NCH = NSEL // 128


def own_blocks(j):
    return sorted([8 * m + j for m in range(8)] + [8 * m + 7 - j for m in range(8)])


class Tr:
    __slots__ = ("w", "r")

    def __init__(self):
        self.w = {}
        self.r = {}


def trs(n):
    return [Tr() for _ in range(n)]


class Prog:
    ENG = ("pe", "act", "dve", "pool", "sp")

    def __init__(self, nc, stack, n_dma_sems=48):
        self.nc = nc
        self.q = {e: [] for e in self.ENG}
        self.esem = {e: stack.enter_context(nc.semaphore("s_" + e)) for e in self.ENG}
        self.ecnt = {e: 0 for e in self.ENG}
        self.waited = {e: {} for e in self.ENG}
        self.dsem = [stack.enter_context(nc.semaphore("d%d" % i)) for i in range(n_dma_sems)]
        self.dcnt = [0] * n_dma_sems
        self.dpool = {"sp": list(range(0, n_dma_sems - 16)), "pool": list(range(n_dma_sems - 16, n_dma_sems))}
        self.dnext = {"sp": 0, "pool": 0}
        self.in_cond = False
        self.handles = {"pe": nc.tensor, "act": nc.scalar, "dve": nc.vector, "pool": nc.gpsimd, "sp": nc.sync}

    def _need(self, eng, s, v):
        wd = self.waited[eng]
        if wd.get(s, 0) >= v:
            return
        wd[s] = v
        self.q[eng].append(("wait", s, v))

    def _waits(self, eng, reads, writes):
        need = {}
        for t in reads:
            for s, v in t.w.items():
                if need.get(s, 0) < v:
                    need[s] = v
        for t in writes:
            for s, v in t.w.items():
                if need.get(s, 0) < v:
                    need[s] = v
            for s, v in t.r.items():
                if need.get(s, 0) < v:
                    need[s] = v
        for s, v in need.items():
            if eng == "pe" and s is self.esem["pe"]:
                continue
            self._need(eng, s, v)

    def _record(self, ev, reads, writes):
        s, v = ev
        for t in reads:
            if t.r.get(s, 0) < v:
                t.r[s] = v
        for t in writes:
            if self.in_cond:
                if t.w.get(s, 0) < v:
                    t.w[s] = v
            else:
                t.w = {s: v}
                t.r = {}

    def op(self, eng, fn, reads=(), writes=()):
        self._waits(eng, reads, writes)
        self.ecnt[eng] += 1
        ev = (self.esem[eng], self.ecnt[eng])
        self.q[eng].append(("op", fn, self.esem[eng], 1))
        self._record(ev, reads, writes)

    def group(self, eng, fns, reads=(), writes=()):
        self._waits(eng, reads, writes)
        self.ecnt[eng] += 1
        ev = (self.esem[eng], self.ecnt[eng])
        for f in fns[:-1]:
            self.q[eng].append(("op", f, None, 0))
        self.q[eng].append(("op", fns[-1], self.esem[eng], 1))
        self._record(ev, reads, writes)

    def dma(self, eng, fn, reads=(), writes=(), slot=None):
        pl = self.dpool[eng]
        if slot is None:
            i = pl[self.dnext[eng]]
            self.dnext[eng] = (self.dnext[eng] + 1) % (len(pl) - 4)
        else:
            i = pl[len(pl) - 4 + slot]
        s = self.dsem[i]
        if self.dcnt[i]:
            self._need(eng, s, self.dcnt[i])
        self._waits(eng, reads, writes)
        self.dcnt[i] += 16
        ev = (s, self.dcnt[i])
        self.q[eng].append(("op", fn, s, 16))
        self._record(ev, reads, writes)
        return ev

    CENG = ("pe", "act", "dve", "sp")

    def regload(self, ap, reads=()):
        for e in self.CENG:
            self._waits(e, reads, ())
            self.q[e].append(("regload", ap))

    def cond_begin(self, thr):
        if not hasattr(self, "_cstack"):
            self._cstack = []
        self._cstack.append(({e: self.ecnt[e] for e in self.ENG}, list(self.dcnt), {e: dict(self.waited[e]) for e in self.ENG}))
        self.in_cond = True
        for e in self.CENG:
            self.q[e].append(["if", thr, None])

    def cond_end(self):
        ec0, dc0, wd0 = self._cstack.pop()
        assert self.ecnt["pool"] == ec0["pool"], "pool must stay outside conditional regions"
        dd = [(i, self.dcnt[i] - dc0[i]) for i in range(len(self.dcnt)) if self.dcnt[i] != dc0[i]]
        for i, _ in dd:
            assert i in self.dpool["sp"]
        for e in self.CENG:
            comp = []
            if self.ecnt[e] != ec0[e]:
                comp.append((self.esem[e], self.ecnt[e] - ec0[e]))
            if e == "sp":
                comp += [(self.dsem[i], d, dc0[i]) for i, d in dd]
            for it in reversed(self.q[e]):
                if isinstance(it, list) and it[0] == "if" and it[2] is None:
                    it[2] = comp
                    break
            self.q[e].append(("endif",))
            self.waited[e] = wd0[e]
        self.waited["pool"] = wd0["pool"]
        self.in_cond = bool(self._cstack)

    def barrier(self):
        for e in self.ENG:
            for f in self.ENG:
                if f != e and self.ecnt[f]:
                    self._need(e, self.esem[f], self.ecnt[f])
            for i, s in enumerate(self.dsem):
                if self.dcnt[i]:
                    self._need(e, s, self.dcnt[i])

    def emit(self):
        nc = self.nc
        q = self.q
        self.q = {e: [] for e in self.ENG}
        if not hasattr(self, "regs"):
            self.regs = {}
        with nc.Block() as block:
            def run_items(h, ename, items):
                i = 0
                n = len(items)
                while i < n:
                    it = items[i]
                    k = it[0]
                    if k == "wait":
                        h.wait_ge(it[1], it[2])
                    elif k == "op":
                        ins = it[1]()
                        if it[2] is not None:
                            ins.then_inc(it[2], it[3])
                    elif k == "regload":
                        if ename not in self.regs:
                            self.regs[ename] = h.alloc_register("cnt_" + ename)
                        h.reg_load(self.regs[ename], it[1])
                    elif k == "if":
                        depth = 1
                        j = i + 1
                        while True:
                            if items[j][0] == "if":
                                depth += 1
                            elif items[j][0] == "endif":
                                depth -= 1
                                if depth == 0:
                                    break
                            j += 1
                        body = items[i + 1:j]
                        with h.If_lt(self.regs[ename], it[1]):
                            h.drain()
                            for cp in it[2]:
                                if len(cp) == 3 and cp[2]:
                                    h.wait_ge(cp[0], cp[2])
                                h.sem_inc(cp[0], cp[1])
                        with h.Else():
                            run_items(h, ename, body)
                        i = j
                    i += 1

            def run(ename):
                run_items(self.handles[ename], ename, q[ename])

            @block.tensor
            def _(e):
                run("pe")

            @block.scalar
            def _(e):
                run("act")

            @block.vector
            def _(e):
                run("dve")

            @block.gpsimd
            def _(e):
                run("pool")

            @block.sync
            def _(e):
                run("sp")


def build_program(j_core_unused=None, debug=False):
    nc = bass.Bass("TRN2", target_bir_lowering=False)
    din = lambda name, shape, dt=F32: nc.dram_tensor(name, list(shape), dt, kind="ExternalInput").ap()
    x_d = din("x", [SX, D])
    pos_d = din("pos", [1, SX], I32)
    dmask_d = din("dmask", [128, NOWN * 512])
    smask_d = din("smask", [128, NOWN * 256])
    cT_d = din("cT", [128, 8])
    wada_d = din("w_ada", [D, 6 * D])
    bada_d = din("b_ada", [1, 6 * D])
    gmixT_d = din("gmixT", [128, 8])
    gffnT_d = din("gffnT", [128, 8])
    wsel_d = din("w_sel", [D, NSEL])
    bselT_d = din("b_selT", [128, NCH])
    bsel_d = din("b_sel", [1, NSEL])
    sinks_d = din("sinks", [1, 8])
    lam_d = din("lam4", [4, 64])
    gsub_d = din("g_subln", [1, 128])
    wout_d = din("w_out", [D, D])
    bout_d = din("b_out", [1, D])
    wr_d = din("w_router", [D, NE])
    br_d = din("b_router", [1, NE])
    w1_d = din("w1", [NE, D, 2 * D])
    b1_d = din("b1", [NE, 2 * D])
    gffn_d = din("g_ffn", [1, D])
    gmix_d = din("g_mix", [1, D])
    w2_d = din("w2", [NE, D, D])
    b2_d = din("b2", [NE, D])
    gfin_d = din("g_final", [1, D])
    cst_d = din("consts", [128, NCST])
    out_d = nc.dram_tensor("out", [NOWN * 128, D], F32, kind="ExternalOutput").ap()
    hT_d = nc.dram_tensor("hT_scr", [8, 128, SX], BF16, kind="Internal").ap()
    cos_d = nc.dram_tensor("cos_scr", [128, SX], F32, kind="Internal").ap()
    sin_d = nc.dram_tensor("sin_scr", [128, SX], F32, kind="Internal").ap()
    x1_d = nc.dram_tensor("x1_scr", [NOWN * 128, D], F32, kind="Internal").ap()
    mod_d = nc.dram_tensor("mod_scr", [1, 6 * D], F32, kind="Internal").ap()
    xbuf_d = nc.dram_tensor("xbuf_scr", [NE * CAP + 128, D], BF16, kind="Internal").ap()
    ybuf_d = nc.dram_tensor("ybuf_scr", [NE * CAP, D], F32, kind="Internal").ap()


    with ExitStack() as st:
        P = Prog(nc, st)
        sbuf = lambda stack, name, shape, dt=F32: stack.enter_context(nc.sbuf_tensor(name, list(shape), dt))
        V, A, T, G_ = nc.vector, nc.scalar, nc.tensor, nc.gpsimd

        bank = [st.enter_context(nc.psum_tensor("bank%d" % i, [128, 512], F32)) for i in range(8)]
        tb = trs(8)

        cst = sbuf(st, "cst", [128, NCST]); t_cst = Tr()
        identb = sbuf(st, "identb", [128, 128], BF16)
        mask256 = sbuf(st, "mask256", [128, 256], BF16)
        onesb = sbuf(st, "onesb", [1, 128], BF16)
        trib = sbuf(st, "trib", [128, 128], BF16)
        ones128b = sbuf(st, "ones128b", [128, 128], BF16)
        A1 = sbuf(st, "A1", [128, 8]); S1 = sbuf(st, "S1", [128, 8])
        A2 = sbuf(st, "A2", [128, 8]); S2 = sbuf(st, "S2", [128, 8])
        t_mod = Tr()
        t_mixed = trs(NOWN)
        small = sbuf(st, "small", [128, 64]); t_small = Tr()
        ident = cst[:, 0:128]
        invf = cst[:, 384:385]
        sgn = cst[:, 385:386]
        halfpi = cst[:, 386:387]
        zero_c = cst[:, 387:388]
        one11 = cst[0:1, 388:389]
        ones_row = cst[0:1, 392:520]
        neglam = small[:, 0:1]
        expsink = small[:, 8:16]

        P.dma("sp", lambda: nc.sync.dma_start(out=cst[:], in_=cst_d[:, :]), writes=[t_cst])
        P.op("dve", lambda: V.tensor_copy(out=identb[:], in_=cst[:, 0:128]), reads=[t_cst], writes=[t_cst])
        P.op("dve", lambda: V.tensor_copy(out=mask256[:, 0:128], in_=cst[:, 256:384]), reads=[t_cst], writes=[t_cst])
        P.op("dve", lambda: V.tensor_copy(out=mask256[:, 128:256], in_=cst[:, 128:256]), reads=[t_cst], writes=[t_cst])
        P.op("dve", lambda: V.tensor_copy(out=onesb[:], in_=cst[0:1, 392:520]), reads=[t_cst], writes=[t_cst])
        P.op("dve", lambda: V.tensor_copy(out=trib[:], in_=cst[:, 520:648]), reads=[t_cst], writes=[t_cst])
        P.op("dve", lambda: V.tensor_copy(out=ones128b[:], in_=cst[:, 392:520]), reads=[t_cst], writes=[t_cst])

        with ExitStack() as s0:
            cT = sbuf(s0, "cT_sb", [128, 8]); t_cT = Tr()
            wad = [sbuf(s0, "wad%d" % i, [128, 8, 512]) for i in range(2)]; t_wad = trs(2)
            modrow = sbuf(s0, "modrow", [1, 6 * D]); t_modrow = Tr()
            badar = sbuf(s0, "badar", [1, 6 * D]); t_bada = Tr()
            gT = sbuf(s0, "gT", [128, 16]); t_gT = Tr()
            lamb = sbuf(s0, "lamb", [128, 256]); t_lam = Tr()
            lamp = sbuf(s0, "lamp", [128, 128])
            P.dma("sp", lambda: nc.sync.dma_start(out=cT[:], in_=cT_d[:, :]), writes=[t_cT])
            P.dma("sp", lambda: nc.sync.dma_start(out=badar[:], in_=bada_d[:, :]), writes=[t_bada])
            P.dma("sp", lambda: nc.sync.dma_start(out=gT[:, 0:8], in_=gmixT_d[:, :]), writes=[t_gT])
            P.dma("sp", lambda: nc.sync.dma_start(out=gT[:, 8:16], in_=gffnT_d[:, :]), writes=[t_gT])
            P.dma("sp", lambda: nc.sync.dma_start(out=lamb[:].rearrange("p (a b) -> p a b", a=4),
                                                  in_=lam_d[:, :].partition_broadcast(128)), writes=[t_lam])
            P.dma("sp", lambda: nc.sync.dma_start(out=small[:, 16:24], in_=sinks_d[0:1, :].partition_broadcast(128)), writes=[t_small])
            posi = sbuf(s0, "posi", [128, 512], I32); t_posi = Tr()
            ang = sbuf(s0, "ang", [128, 512]); t_ang = Tr()
            ki = sbuf(s0, "ki", [128, 512], I32); kf = sbuf(s0, "kf", [128, 512]); t_k = Tr()
            rr = sbuf(s0, "rr", [128, 512]); t_rr = Tr()
            tab = [sbuf(s0, "tab%d" % i, [128, 512]) for i in range(4)]; t_tab = trs(4)
            def rope_group(g):
                P.dma("sp", lambda: nc.sync.dma_start(out=posi[:], in_=pos_d[0:1, g * 512:(g + 1) * 512].partition_broadcast(128)), writes=[t_posi])
                P.op("dve", lambda: V.tensor_copy(out=ang[:], in_=posi[:]), reads=[t_posi], writes=[t_ang])
                P.op("dve", lambda: V.tensor_scalar(out=ang[:], in0=ang[:], scalar1=invf, scalar2=None, op0=ALU.mult), reads=[t_ang, t_cst], writes=[t_ang])
                for which in range(2):
                    tbi = (2 * g + which) % 4
                    if which == 0:
                        P.op("dve", lambda: V.tensor_scalar(out=ki[:], in0=ang[:], scalar1=INV2PI, scalar2=None, op0=ALU.mult), reads=[t_ang], writes=[t_k])
                    else:
                        P.op("dve", lambda: V.tensor_scalar(out=ki[:], in0=ang[:], scalar1=INV2PI, scalar2=0.25, op0=ALU.mult, op1=ALU.add), reads=[t_ang], writes=[t_k])
                    P.op("dve", lambda: V.tensor_copy(out=kf[:], in_=ki[:]), reads=[t_k], writes=[t_k])
                    P.op("dve", lambda: V.scalar_tensor_tensor(out=rr[:], in0=kf[:], scalar=-C1, in1=ang[:], op0=ALU.mult, op1=ALU.add), reads=[t_k, t_ang], writes=[t_rr])
                    P.op("dve", lambda: V.scalar_tensor_tensor(out=rr[:], in0=kf[:], scalar=-C2, in1=rr[:], op0=ALU.mult, op1=ALU.add), reads=[t_k, t_rr], writes=[t_rr])
                    if which == 0:
                        P.op("dve", lambda: V.tensor_scalar(out=rr[:], in0=rr[:], scalar1=-3.1415925, scalar2=3.1415925, op0=ALU.max, op1=ALU.min), reads=[t_rr], writes=[t_rr])
                        P.op("act", lambda tbi=tbi: A.activation(out=tab[tbi][:], in_=rr[:], func=AF.Sin, scale=sgn, bias=zero_c), reads=[t_rr, t_cst], writes=[t_tab[tbi]])
                        P.dma("sp", lambda tbi=tbi: nc.sync.dma_start(out=sin_d[:, g * 512:(g + 1) * 512], in_=tab[tbi][:]), reads=[t_tab[tbi]])
                    else:
                        P.op("dve", lambda: V.tensor_scalar(out=rr[:], in0=rr[:], scalar1=-4.712388, scalar2=1.570796, op0=ALU.max, op1=ALU.min), reads=[t_rr], writes=[t_rr])
                        P.op("act", lambda tbi=tbi: A.activation(out=tab[tbi][:], in_=rr[:], func=AF.Sin, scale=1.0, bias=halfpi), reads=[t_rr, t_cst], writes=[t_tab[tbi]])
                        P.dma("sp", lambda tbi=tbi: nc.sync.dma_start(out=cos_d[:, g * 512:(g + 1) * 512], in_=tab[tbi][:]), reads=[t_tab[tbi]])
            for g in range(NGX):
                rope_group(g)

            P.op("act", lambda: A.activation(out=cT[:], in_=cT[:], func=AF.Silu), reads=[t_cT], writes=[t_cT])
            wada_v = wada_d.rearrange("(k p) n -> p k n", p=128)
            for pc in range(12):
                b = pc % 2
                P.dma("sp", lambda pc=pc, b=b: nc.sync.dma_start(out=wad[b][:], in_=wada_v[:, :, pc * 512:(pc + 1) * 512]), writes=[t_wad[b]])
                bk = pc % 2
                P.group("pe", [(lambda kc=kc, b=b, bk=bk: T.matmul(bank[bk][0:1, :], lhsT=cT[:, kc:kc + 1], rhs=wad[b][:, kc, :],
                                                                    start=(kc == 0), stop=(kc == 7))) for kc in range(8)],
                        reads=[t_cT, t_wad[b]], writes=[tb[bk]])
                P.op("dve", lambda pc=pc, bk=bk: V.tensor_tensor(out=modrow[0:1, pc * 512:(pc + 1) * 512], in0=bank[bk][0:1, :],
                                                                 in1=badar[0:1, pc * 512:(pc + 1) * 512], op=ALU.add),
                     reads=[tb[bk], t_bada], writes=[t_modrow])
            cols = [(0, 0), (1, 8), (3, 16), (4, 24)]
            fns = []
            for mi, dc in cols:
                for kc in range(8):
                    fns.append(lambda mi=mi, dc=dc, kc=kc: T.matmul(bank[2][:, dc + kc:dc + kc + 1],
                                                                    lhsT=modrow[0:1, mi * D + kc * 128: mi * D + (kc + 1) * 128],
                                                                    rhs=one11, start=True, stop=True))
            P.group("pe", fns, reads=[t_modrow, t_cst], writes=[tb[2]])
            P.op("dve", lambda: V.tensor_copy(out=S1[:], in_=bank[2][:, 0:8]), reads=[tb[2]], writes=[t_mod])
            P.op("dve", lambda: V.scalar_tensor_tensor(out=A1[:], in0=bank[2][:, 8:16], scalar=1.0, in1=gT[:, 0:8], op0=ALU.add, op1=ALU.mult),
                 reads=[tb[2], t_gT], writes=[t_mod])
            P.op("dve", lambda: V.tensor_copy(out=S2[:], in_=bank[2][:, 16:24]), reads=[tb[2]], writes=[t_mod])
            P.op("dve", lambda: V.scalar_tensor_tensor(out=A2[:], in0=bank[2][:, 24:32], scalar=1.0, in1=gT[:, 8:16], op0=ALU.add, op1=ALU.mult),
                 reads=[tb[2], t_gT], writes=[t_mod])
            P.dma("sp", lambda: nc.sync.dma_start(out=mod_d[:, :], in_=modrow[:]), reads=[t_modrow])
            P.op("dve", lambda: V.tensor_tensor(out=lamp[:, 0:64], in0=lamb[:, 0:64], in1=lamb[:, 64:128], op=ALU.mult), reads=[t_lam], writes=[t_lam])
            P.op("dve", lambda: V.tensor_tensor(out=lamp[:, 64:128], in0=lamb[:, 128:192], in1=lamb[:, 192:256], op=ALU.mult), reads=[t_lam], writes=[t_lam])
            P.op("dve", lambda: V.tensor_reduce(out=small[:, 1:3], in_=lamp[:].rearrange("p (a b) -> p a b", a=2), axis=AX.X, op=ALU.add),
                 reads=[t_lam], writes=[t_small])
            P.op("act", lambda: A.activation(out=small[:, 1:3], in_=small[:, 1:3], func=AF.Exp), reads=[t_small], writes=[t_small])
            P.op("dve", lambda: V.scalar_tensor_tensor(out=small[:, 0:1], in0=small[:, 2:3], scalar=-0.2, in1=small[:, 1:2], op0=ALU.add, op1=ALU.subtract),
                 reads=[t_small], writes=[t_small])
            P.op("act", lambda: A.activation(out=small[:, 8:16], in_=small[:, 16:24], func=AF.Exp), reads=[t_small], writes=[t_small])
            P.barrier()
            P.emit()

        with ExitStack() as s1:
            XB = 8
            xt = [sbuf(s1, "xt%d" % i, [128, D]) for i in range(XB)]; t_xt = trs(XB)
            xn = [sbuf(s1, "xn%d" % i, [128, D], BF16) for i in range(2)]; t_xn = trs(2)
            junk = sbuf(s1, "junk", [128, D], BF16); t_junk = Tr()
            ssq = sbuf(s1, "ssq", [128, 2, 8]); t_ssq = trs(2)
            hTg = [sbuf(s1, "hTg%d" % i, [128, 8, 512], BF16) for i in range(2)]; t_hTg = trs(2)
            hT_v = hT_d.rearrange("k p t -> p k t")
            A1b = sbuf(s1, "A1b", [128, D]); S1b = sbuf(s1, "S1b", [128, D]); gmb = sbuf(s1, "gmb", [128, D]); t_m1 = Tr()
            xm = [sbuf(s1, "xm%d" % i, [128, D]) for i in range(2)]; t_xm = trs(2)
            P.dma("sp", lambda: nc.sync.dma_start(out=S1b[:], in_=mod_d[0:1, 0:D].partition_broadcast(128)), writes=[t_m1])
            P.dma("sp", lambda: nc.sync.dma_start(out=A1b[:], in_=mod_d[0:1, D:2 * D].partition_broadcast(128)), writes=[t_m1])
            P.dma("sp", lambda: nc.sync.dma_start(out=gmb[:], in_=gmix_d[0:1, :].partition_broadcast(128)), writes=[t_m1])
            P.op("dve", lambda: V.scalar_tensor_tensor(out=A1b[:], in0=A1b[:], scalar=1.0, in1=gmb[:], op0=ALU.add, op1=ALU.mult), reads=[t_m1], writes=[t_m1])

            def stageA(g):
                gp = g % 2
                for tt in range(4):
                    t = 4 * g + tt
                    xb = t % XB
                    P.dma("sp", lambda t=t, xb=xb: nc.sync.dma_start(out=xt[xb][:], in_=x_d[t * 128:(t + 1) * 128, :]), writes=[t_xt[xb]])
                    P.op("act", lambda xb=xb, tt=tt: A.activation(out=junk[:], in_=xt[xb][:], func=AF.Square, accum_out=ssq[:, gp, tt:tt + 1]),
                         reads=[t_xt[xb]], writes=[t_junk, t_ssq[gp]])
                P.op("dve", lambda: V.tensor_scalar(out=ssq[:, gp, 4:8], in0=ssq[:, gp, 0:4], scalar1=1.0 / D, scalar2=EPS, op0=ALU.mult, op1=ALU.add), reads=[t_ssq[gp]], writes=[t_ssq[gp]])
                P.op("act", lambda: A.activation(out=ssq[:, gp, 4:8], in_=ssq[:, gp, 4:8], func=AF.Sqrt), reads=[t_ssq[gp]], writes=[t_ssq[gp]])
                P.op("dve", lambda: V.reciprocal(out=ssq[:, gp, 4:8], in_=ssq[:, gp, 4:8]), reads=[t_ssq[gp]], writes=[t_ssq[gp]])

            def stageB(g):
                gp = g % 2
                hb = g % 2

                def tile_b(tt):
                    t = 4 * g + tt
                    xb = t % XB
                    nb = t % 2
                    P.op("dve", lambda: V.scalar_tensor_tensor(out=xm[nb][:], in0=xt[xb][:], scalar=ssq[:, gp, 4 + tt:5 + tt], in1=A1b[:], op0=ALU.mult, op1=ALU.mult),
                         reads=[t_xt[xb], t_ssq[gp], t_m1], writes=[t_xm[nb]])
                    P.op("dve", lambda: V.tensor_tensor(out=xn[nb][:], in0=xm[nb][:], in1=S1b[:], op=ALU.add), reads=[t_xm[nb], t_m1], writes=[t_xn[nb]])
                    bk = nb
                    pT = bank[bk][:, :].bitcast(BF16)
                    P.group("pe", [(lambda kc=kc: T.transpose(out=pT[:, kc * 128:(kc + 1) * 128], in_=xn[nb][:, kc * 128:(kc + 1) * 128], identity=identb[:]))
                                   for kc in range(8)], reads=[t_xn[nb], t_cst], writes=[tb[bk]])
                    P.op("act", lambda: A.activation(out=hTg[hb][:, :, tt * 128:(tt + 1) * 128], in_=pT[:, :].rearrange("p (a b) -> p a b", a=8), func=AF.Copy),
                         reads=[tb[bk]], writes=[t_hTg[hb]])
                for tt in range(4):
                    tile_b(tt)
                P.dma("sp", lambda: nc.sync.dma_start(out=hT_v[:, :, g * 512:(g + 1) * 512], in_=hTg[hb][:]), reads=[t_hTg[hb]])

            for g in range(NGX + 1):
                if g < NGX:
                    stageA(g)
                if g >= 1:
                    stageB(g - 1)
            P.barrier()
            P.emit()

        s34 = st.enter_context(ExitStack())
        dest_i = sbuf(s34, "dest_i", [128, 4 * NOWN], I32); t_dest = trs(NOWN)
        gate4 = sbuf(s34, "gate4", [128, 4 * NOWN]); t_gate4 = trs(NOWN)
        maskb = sbuf(s34, "maskb", [128, NOWN, NE], BF16); t_maskb = trs(NOWN)
        cnt_run = sbuf(s34, "cnt_run", [128, NE]); t_cnt = Tr()
        cnt_i = sbuf(s34, "cnt_i", [1, NE], I32); t_cnti = Tr()
        padidx = sbuf(s34, "padidx", [128, NE], I32); t_pad = Tr()
        t_xbuf = Tr()
        iota32 = cst[:, 648:680]
        e2048 = cst[:, 680:712]
        iota_p = cst[:, 712:713]
        sA = ExitStack()
        bufA = sbuf(sA, "bufA", [128, 16 * 1024], BF16)
        mixed = bufA[:].rearrange("p (a b) -> p a b", a=NOWN)
        with ExitStack() as s2:
            Wu = sbuf(s2, "Wu", [128, 8, 1664], BF16); t_Wu = Tr()
            KT = sbuf(s2, "KT", [128, S], BF16); t_KT = Tr()
            Vb = sbuf(s2, "Vb", [128, 64 * 130], BF16); t_V = Tr()
            QT = sbuf(s2, "QT", [128, 4, NOWN * 128], BF16); t_QT = Tr()
            hTg = [sbuf(s2, "hTg2_%d" % i, [128, 8, 512], BF16) for i in range(2)]; t_hTg = trs(2)
            csg = [sbuf(s2, "csg%d" % i, [128, 2, 512]) for i in range(2)]; t_csg = trs(2)
            tm1 = [sbuf(s2, "tm1_%d" % i, [128, 512]) for i in range(2)]; t_tm1 = trs(2)
            tm2 = [sbuf(s2, "tm2_%d" % i, [128, 512]) for i in range(2)]; t_tm2 = trs(2)
            PT = [sbuf(s2, "PT%d" % i, [128, 512], BF16) for i in range(3)]; t_PT = trs(3)
            dmask = sbuf(s2, "dmask_sb", [128, NOWN, 512], BF16); t_dmask = Tr()
            smask = sbuf(s2, "smask_sb", [128, NOWN, 256], BF16); t_smask = Tr()
            bselT = sbuf(s2, "bselT", [128, NCH]); t_bsel = Tr()
            vbias = sbuf(s2, "vbias", [128, 128]); t_vbias = Tr()
            gsub_b = sbuf(s2, "gsub_b", [128, 128]); t_gsub = Tr()
            fin = sbuf(s2, "fin", [128, 8 * 128]); t_fin = Tr()
            fsm = sbuf(s2, "fsm", [128, 32]); t_fsm = Tr()
            junk2 = sbuf(s2, "junk2", [128, 128], BF16)
            hT_v = hT_d.rearrange("k p t -> p k t")
            wsel_v = wsel_d.rearrange("(k p) n -> p k n", p=128)
            for q4 in range(4):
                P.dma("pool", lambda q4=q4: G_.dma_start(out=dmask[:, 4 * q4:4 * q4 + 4, :], in_=dmask_d[:, q4 * 2048:(q4 + 1) * 2048].rearrange("p (a b) -> p a b", a=4)),
                      writes=[t_dmask])
            for q4 in range(2):
                P.dma("pool", lambda q4=q4: G_.dma_start(out=smask[:, 8 * q4:8 * q4 + 8, :], in_=smask_d[:, q4 * 2048:(q4 + 1) * 2048].rearrange("p (a b) -> p a b", a=8)),
                      writes=[t_smask])
            P.dma("sp", lambda: nc.sync.dma_start(out=bselT[:], in_=bselT_d[:, :]), writes=[t_bsel])
            P.dma("sp", lambda: nc.sync.dma_start(out=gsub_b[:], in_=gsub_d[0:1, :].partition_broadcast(128)), writes=[t_gsub])
            P.op("dve", lambda: V.tensor_scalar(out=gsub_b[:], in0=gsub_b[:], scalar1=0.8, scalar2=None, op0=ALU.mult), reads=[t_gsub], writes=[t_gsub])
            gcount = [0]

            def rope_proj(u, wc, wcs, hb, cb, ccol, ncol, dst, t_dst, par):
                bA, bB = bank[2 * par], bank[2 * par + 1]
                ci = (u["base"] + wc) // 128
                cis = (u["base"] + wcs) // 128
                P.group("pe", [(lambda kc=kc: T.matmul(bA[:, 0:ncol], lhsT=Wu[:, kc, wc:wc + 128], rhs=hTg[hb][:, kc, ccol:ccol + ncol], start=(kc == 0), stop=(kc == 7)))
                               for kc in range(8)], reads=[t_Wu, t_hTg[hb]], writes=[tb[2 * par]])
                P.group("pe", [(lambda kc=kc: T.matmul(bB[:, 0:ncol], lhsT=Wu[:, kc, wcs:wcs + 128], rhs=hTg[hb][:, kc, ccol:ccol + ncol], start=(kc == 0), stop=(kc == 7)))
                               for kc in range(8)], reads=[t_Wu, t_hTg[hb]], writes=[tb[2 * par + 1]])
                P.op("dve", lambda: V.scalar_tensor_tensor(out=tm1[par][:, 0:ncol], in0=bA[:, 0:ncol], scalar=bselT[:, ci:ci + 1], in1=csg[cb][:, 0, ccol:ccol + ncol],
                                                           op0=ALU.add, op1=ALU.mult), reads=[tb[2 * par], t_bsel, t_csg[cb]], writes=[t_tm1[par]])
                P.op("dve", lambda: V.scalar_tensor_tensor(out=tm2[par][:, 0:ncol], in0=bB[:, 0:ncol], scalar=bselT[:, cis:cis + 1], in1=csg[cb][:, 1, ccol:ccol + ncol],
                                                           op0=ALU.add, op1=ALU.mult), reads=[tb[2 * par + 1], t_bsel, t_csg[cb]], writes=[t_tm2[par]])
                P.op("dve", lambda: V.tensor_tensor(out=dst, in0=tm1[par][:, 0:ncol], in1=tm2[par][:, 0:ncol], op=ALU.add),
                     reads=[t_tm1[par], t_tm2[par]], writes=[t_dst])

            def load_group(g):
                hb = gcount[0] % 2
                gcount[0] += 1
                P.dma("sp", lambda: nc.sync.dma_start(out=hTg[hb][:], in_=hT_v[:, :, g * 512:(g + 1) * 512]), writes=[t_hTg[hb]])
                P.dma("sp", lambda: nc.sync.dma_start(out=csg[hb][:, 0, :], in_=cos_d[:, g * 512:(g + 1) * 512]), writes=[t_csg[hb]])
                P.dma("sp", lambda: nc.sync.dma_start(out=csg[hb][:, 1, :], in_=sin_d[:, g * 512:(g + 1) * 512]), writes=[t_csg[hb]])
                return hb

            pcount = [0]

            def v_proj(u, hb, vt0, vw, swa):
                bk = 4 + (pcount[0] % 2)
                pcount[0] += 1
                ov = u["o_v"]
                fns = []
                for tt in range(4):
                    for kc in range(8):
                        fns.append(lambda tt=tt, kc=kc: T.matmul(bank[bk][:, tt * 128:(tt + 1) * 128], lhsT=hTg[hb][:, kc, tt * 128:(tt + 1) * 128],
                                                                 rhs=Wu[:, kc, ov:ov + 128], start=(kc == 0), stop=(kc == 7)))
                P.group("pe", fns, reads=[t_Wu, t_hTg[hb]], writes=[tb[bk]])
                src = bank[bk][:, :].rearrange("p (a b) -> p a b", a=4)
                vb_b = vbias[:].unsqueeze(1).to_broadcast([128, 4, 128])
                if not swa:
                    dst = Vb[:, vt0 * 129:(vt0 + 4) * 129].rearrange("p (a b) -> p a b", a=4)[:, :, 0:128]
                    P.op("dve", lambda: V.tensor_tensor(out=dst, in0=src, in1=vb_b, op=ALU.add), reads=[tb[bk], t_vbias], writes=[t_V])
                else:
                    for kv in range(2):
                        dst = Vb[:, vt0 * 130:(vt0 + 4) * 130].rearrange("p (a b) -> p a b", a=4)[:, :, kv * 65:kv * 65 + 64]
                        P.op("dve", lambda dst=dst, kv=kv: V.tensor_tensor(out=dst, in0=src[:, :, kv * 64:(kv + 1) * 64],
                                                                          in1=vbias[:, kv * 64:(kv + 1) * 64].unsqueeze(1).to_broadcast([128, 4, 64]), op=ALU.add),
                             reads=[tb[bk], t_vbias], writes=[t_V])

            for ui, u in enumerate(UNITS):
                swa = (ui == 0)
                nc_u = u["ncols"]
                P.dma("pool", lambda u=u, nc_u=nc_u: G_.dma_start(out=Wu[:, :, 0:nc_u], in_=wsel_v[:, :, u["base"]:u["base"] + nc_u]), writes=[t_Wu])
                P.dma("sp", lambda u=u: nc.sync.dma_start(out=vbias[:], in_=bsel_d[0:1, u["base"] + u["o_v"]:u["base"] + u["o_v"] + 128].partition_broadcast(128)),
                      writes=[t_vbias])
                if swa:
                    vv = Vb[:, 0:32 * 130].rearrange("p (a b) -> p a b", a=32)
                    P.op("pool", lambda vv=vv: G_.memset(vv[:, :, 64:65], 1.0), writes=[t_V])
                    P.op("pool", lambda vv=vv: G_.memset(vv[:, :, 129:130], 1.0), writes=[t_V])
                    kv_groups = [(20 + i, i * 512, 4 * i) for i in range(4)] + [(16 + i, 2048 + i * 512, 16 + 4 * i) for i in range(4)]
                elif ui == 1:
                    vv = Vb[:, 0:64 * 129].rearrange("p (a b) -> p a b", a=64)
                    P.op("pool", lambda vv=vv: G_.memset(vv[:, :, 128:129], 1.0), writes=[t_V])
                    kv_groups = [(g, g * 512, 4 * g) for g in range(NG)]
                else:
                    kv_groups = [(g, g * 512, 4 * g) for g in range(NG)]
                par = 0
                for (g, kcol, vt0) in kv_groups:
                    hb = load_group(g)
                    for kc_ in range(u["nk"]):
                        rope_proj(u, u["o_k"] + kc_ * 128, u["o_ks"] + kc_ * 128, hb, hb, 0, 512, KT[:, kc_ * 4096 + kcol:kc_ * 4096 + kcol + 512], t_KT, par)
                        par ^= 1
                    v_proj(u, hb, vt0, None, swa)
                    if swa and g < 20:
                        for qc in range(4):
                            rope_proj(u, u["o_q"] + qc * 128, u["o_qs"] + qc * 128, hb, hb, 0, 512, QT[:, qc, (g - 16) * 512:(g - 15) * 512], t_QT, par)
                            par ^= 1
                if not swa:
                    for g in range(16, 20):
                        hb = load_group(g)
                        rope_proj(u, u["o_q"], u["o_qs"], hb, hb, 0, 512, QT[:, 0, (g - 16) * 512:(g - 15) * 512], t_QT, par)
                        par ^= 1

                items = []
                if swa:
                    for oi in range(NOWN):
                        for hh in range(8):
                            items.append((oi, hh, 0, True))
                else:
                    for oi in range(NOWN):
                        nkb = 8 * (oi // 2) + (4 if oi % 2 == 0 else 8)
                        for m in range(2):
                            for c in range(nkb // 4):
                                items.append((oi, m, c, c == nkb // 4 - 1))

                def qk(n):
                    oi, a, c, last = items[n]
                    sb_ = n % 3
                    if swa:
                        hh = a; half = hh % 2; qc = hh // 2; kvg = hh // 4
                        ps = slice(half * 64, half * 64 + 64)
                        fns = [lambda: T.matmul(bank[sb_][:, 0:128], lhsT=KT[ps, kvg * 4096 + oi * 128:kvg * 4096 + (oi + 1) * 128], rhs=QT[ps, qc, oi * 128:(oi + 1) * 128], start=True, stop=True),
                               lambda: T.matmul(bank[sb_][:, 128:256], lhsT=KT[ps, kvg * 4096 + 2048 + oi * 128:kvg * 4096 + 2048 + (oi + 1) * 128], rhs=QT[ps, qc, oi * 128:(oi + 1) * 128], start=True, stop=True)]
                        ncol = 256
                        mk = smask[:, oi, :]
                        t_mk = t_smask
                    else:
                        m = a
                        ps = slice(m * 64, m * 64 + 64)
                        fns = [(lambda i=i: T.matmul(bank[sb_][:, i * 128:(i + 1) * 128], lhsT=KT[ps, (4 * c + i) * 128:(4 * c + i + 1) * 128],
                                                     rhs=QT[ps, 0, oi * 128:(oi + 1) * 128], start=True, stop=True)) for i in range(4)]
                        ncol = 512
                        mk = dmask[:, oi, :]
                        t_mk = t_dmask
                    P.group("pe", fns, reads=[t_KT, t_QT], writes=[tb[sb_]])
                    P.op("act", lambda: A.activation(out=PT[sb_][:, 0:ncol], in_=bank[sb_][:, 0:ncol], func=AF.Exp, scale=0.125), reads=[tb[sb_]], writes=[t_PT[sb_]])
                    if last:
                        P.op("pool", lambda: G_.tensor_tensor(out=PT[sb_][:, 0:ncol], in0=PT[sb_][:, 0:ncol], in1=mk, op=ALU.mult), reads=[t_PT[sb_], t_mk], writes=[t_PT[sb_]])

                def pv(n):
                    oi, a, c, last = items[n]
                    sb_ = n % 3
                    if swa:
                        hh = a; kvg = hh // 4
                        ob = 3 + (oi % 2) * 2 + (hh // 4)
                        oc = (hh % 4) * 65
                        fns = [lambda: T.matmul(bank[ob][:, oc:oc + 65], lhsT=PT[sb_][:, 0:128], rhs=Vb[:, oi * 130 + kvg * 65: oi * 130 + kvg * 65 + 65], start=True, stop=False),
                               lambda: T.matmul(bank[ob][:, oc:oc + 65], lhsT=PT[sb_][:, 128:256], rhs=Vb[:, (16 + oi) * 130 + kvg * 65: (16 + oi) * 130 + kvg * 65 + 65], start=False, stop=True)]
                    else:
                        m = a
                        ob = 3 + (oi % 2) * 2 + m
                        fns = [(lambda i=i: T.matmul(bank[ob][:, 0:129], lhsT=PT[sb_][:, i * 128:(i + 1) * 128], rhs=Vb[:, (4 * c + i) * 129:(4 * c + i + 1) * 129],
                                                     start=(c == 0 and i == 0), stop=(last and i == 3))) for i in range(4)]
                    P.group("pe", fns, reads=[t_PT[sb_], t_V], writes=[tb[ob]])
                    if swa and a == 7:
                        for hh in range(8):
                            ob2 = 3 + (oi % 2) * 2 + (hh // 4)
                            oc2 = (hh % 4) * 65
                            P.op("dve", lambda hh=hh, ob2=ob2, oc2=oc2: V.tensor_tensor(out=fsm[:, hh:hh + 1], in0=bank[ob2][:, oc2 + 64:oc2 + 65], in1=expsink[:, hh:hh + 1], op=ALU.add),
                                 reads=[tb[ob2], t_small], writes=[t_fsm])
                        P.op("dve", lambda: V.reciprocal(out=fsm[:, 0:8], in_=fsm[:, 0:8]), reads=[t_fsm], writes=[t_fsm])
                        for hh in range(8):
                            ob2 = 3 + (oi % 2) * 2 + (hh // 4)
                            oc2 = (hh % 4) * 65
                            P.op("dve", lambda hh=hh, ob2=ob2, oc2=oc2: V.tensor_scalar(out=mixed[:, oi, hh * 64:(hh + 1) * 64], in0=bank[ob2][:, oc2:oc2 + 64],
                                                                                         scalar1=fsm[:, hh:hh + 1], scalar2=None, op0=ALU.mult),
                                 reads=[tb[ob2], t_fsm], writes=[t_mixed[oi]])
                    if (not swa) and a == 1 and last:
                        h = ui - 1
                        o0 = bank[3 + (oi % 2) * 2]
                        o1 = bank[3 + (oi % 2) * 2 + 1]
                        t0, t1 = tb[3 + (oi % 2) * 2], tb[3 + (oi % 2) * 2 + 1]
                        P.op("dve", lambda: V.reciprocal(out=fsm[:, 16:17], in_=o0[:, 128:129]), reads=[t0], writes=[t_fsm])
                        P.op("dve", lambda: V.reciprocal(out=fsm[:, 17:18], in_=o1[:, 128:129]), reads=[t1], writes=[t_fsm])
                        P.op("dve", lambda: V.tensor_tensor(out=fsm[:, 17:18], in0=fsm[:, 17:18], in1=neglam, op=ALU.mult), reads=[t_fsm, t_small], writes=[t_fsm])
                        P.op("dve", lambda: V.tensor_scalar(out=fin[:, 0:128], in0=o1[:, 0:128], scalar1=fsm[:, 17:18], scalar2=None, op0=ALU.mult), reads=[t1, t_fsm], writes=[t_fin])
                        P.op("dve", lambda: V.scalar_tensor_tensor(out=fin[:, 128:256], in0=o0[:, 0:128], scalar=fsm[:, 16:17], in1=fin[:, 0:128], op0=ALU.mult, op1=ALU.add),
                             reads=[t0, t_fsm, t_fin], writes=[t_fin])
                        P.op("act", lambda: A.activation(out=junk2[:], in_=fin[:, 128:256], func=AF.Square, accum_out=fsm[:, 18:19]), reads=[t_fin], writes=[t_fsm])
                        P.op("dve", lambda: V.tensor_scalar(out=fsm[:, 18:19], in0=fsm[:, 18:19], scalar1=1.0 / 128, scalar2=EPS, op0=ALU.mult, op1=ALU.add), reads=[t_fsm], writes=[t_fsm])
                        P.op("act", lambda: A.activation(out=fsm[:, 18:19], in_=fsm[:, 18:19], func=AF.Sqrt), reads=[t_fsm], writes=[t_fsm])
                        P.op("dve", lambda: V.reciprocal(out=fsm[:, 18:19], in_=fsm[:, 18:19]), reads=[t_fsm], writes=[t_fsm])
                        P.op("dve", lambda: V.scalar_tensor_tensor(out=mixed[:, oi, 512 + h * 128:512 + (h + 1) * 128], in0=fin[:, 128:256], scalar=fsm[:, 18:19], in1=gsub_b[:],
                                                                   op0=ALU.mult, op1=ALU.mult), reads=[t_fin, t_fsm, t_gsub], writes=[t_mixed[oi]])

                LAG = 2
                for n in range(len(items) + LAG):
                    if n < len(items):
                        qk(n)
                    if n >= LAG:
                        pv(n - LAG)
            P.barrier()
            P.emit()

        with ExitStack() as s3:
            gt1_b = sbuf(s3, "gt1_b", [128, D])
            A2b = sbuf(s3, "A2b", [128, D]); S2b = sbuf(s3, "S2b", [128, D]); t_m2 = Tr()
            P.dma("sp", lambda: nc.sync.dma_start(out=gt1_b[:], in_=mod_d[0:1, 2 * D:3 * D].partition_broadcast(128)), writes=[t_mod])
            P.dma("sp", lambda: nc.sync.dma_start(out=S2b[:], in_=mod_d[0:1, 3 * D:4 * D].partition_broadcast(128)), writes=[t_m2])
            P.dma("sp", lambda: nc.sync.dma_start(out=A2b[:], in_=mod_d[0:1, 4 * D:5 * D].partition_broadcast(128)), writes=[t_m2])
            wout = sbuf(s3, "wout", [128, 8, D], BF16); t_wout = Tr()
            boutb = sbuf(s3, "boutb", [1, D], BF16)
            wr = sbuf(s3, "wr", [128, 8, NE], BF16); t_wr = Tr()
            brb = sbuf(s3, "brb", [1, NE], BF16)
            gfb = sbuf(s3, "gfb", [128, D]); t_gfb = Tr()
            P.dma("sp", lambda: nc.sync.dma_start(out=gfb[:], in_=gffn_d[0:1, :].partition_broadcast(128)), writes=[t_gfb])
            P.op("dve", lambda: V.scalar_tensor_tensor(out=A2b[:], in0=A2b[:], scalar=1.0, in1=gfb[:], op0=ALU.add, op1=ALU.mult), reads=[t_m2, t_gfb], writes=[t_m2])
            P.op("dve", lambda: V.memset(cnt_run[:], 0.0), writes=[t_cnt])
            mixT = [sbuf(s3, "mixT%d" % i, [128, 8, 128], BF16) for i in range(2)]; t_mixT = trs(2)
            xo = [sbuf(s3, "xo%d" % i, [128, D]) for i in range(2)]; t_xo = trs(2)
            x1t = [sbuf(s3, "x1t%d" % i, [128, D]) for i in range(2)]; t_x1t = trs(2)
            h2f = [sbuf(s3, "h2f%d" % i, [128, D]) for i in range(2)]; t_h2f = trs(2)
            h2tok = [sbuf(s3, "h2tok%d" % i, [128, D], BF16) for i in range(2)]; t_h2tok = trs(2)
            h2Tt = [sbuf(s3, "h2Tt%d" % i, [128, 8, 128], BF16) for i in range(2)]; t_h2Tt = trs(2)
            zrow = sbuf(s3, "zrow", [128, D], BF16); t_zrow = Tr()
            junk3 = sbuf(s3, "junk3", [128, D], BF16); t_junk3 = Tr()
            rs = sbuf(s3, "rs", [128, 64]); t_rs = trs(2)
            lg = sbuf(s3, "lg", [128, 2, 4 * NE]); t_lg = trs(2)
            idx8 = sbuf(s3, "idx8", [128, 2, 8], U32)
            posb = sbuf(s3, "posb", [128, 2, NE]); junkp = sbuf(s3, "junkp", [128, 2, NE])
            wout_v = wout_d.rearrange("(k p) n -> p k n", p=128)
            wr_v = wr_d.rearrange("(k p) n -> p k n", p=128)
            P.dma("pool", lambda: G_.dma_start(out=wout[:], in_=wout_v), writes=[t_wout])
            P.dma("pool", lambda: G_.dma_start(out=boutb[:], in_=bout_d[:, :]), writes=[t_wout])
            P.dma("pool", lambda: G_.dma_start(out=wr[:], in_=wr_v), writes=[t_wr])
            P.dma("pool", lambda: G_.dma_start(out=brb[:], in_=br_d[:, :]), writes=[t_wr])
            P.op("pool", lambda: G_.memset(zrow[:], 0.0), writes=[t_zrow])

            def p3(oi):
                b = oi % 2
                P.dma("sp", lambda: nc.sync.dma_start(out=xo[b][:], in_=x_d[S + oi * 128:S + (oi + 1) * 128, :]), writes=[t_xo[b]])
                pT = bank[b][:, :].bitcast(BF16)
                P.group("pe", [(lambda kc=kc: T.transpose(out=pT[:, kc * 128:(kc + 1) * 128], in_=mixed[:, oi, kc * 128:(kc + 1) * 128], identity=identb[:])) for kc in range(8)],
                        reads=[t_mixed[oi], t_cst], writes=[tb[b]])
                P.op("act", lambda: A.activation(out=mixT[b][:].rearrange("p a b -> p (a b)"), in_=pT[:, :], func=AF.Copy), reads=[tb[b]], writes=[t_mixT[b]])
                for hf in range(2):
                    bk = 2 + 2 * b + hf
                    fns = [(lambda kc=kc, hf=hf, bk=bk: T.matmul(bank[bk][:, :], lhsT=mixT[b][:, kc, :], rhs=wout[:, kc, hf * 512:(hf + 1) * 512], start=(kc == 0), stop=False)) for kc in range(8)]
                    fns.append(lambda hf=hf, bk=bk: T.matmul(bank[bk][:, :], lhsT=onesb[0:1, :], rhs=boutb[0:1, hf * 512:(hf + 1) * 512], start=False, stop=True))
                    P.group("pe", fns, reads=[t_mixT[b], t_wout, t_cst], writes=[tb[bk]])
                    P.op("dve", lambda hf=hf, bk=bk: V.tensor_tensor(out=x1t[b][:, hf * 512:(hf + 1) * 512], in0=bank[bk][:, :], in1=gt1_b[:, hf * 512:(hf + 1) * 512], op=ALU.mult),
                         reads=[tb[bk], t_mod], writes=[t_x1t[b]])
                P.op("dve", lambda: V.tensor_tensor(out=x1t[b][:], in0=x1t[b][:], in1=xo[b][:], op=ALU.add), reads=[t_x1t[b], t_xo[b]], writes=[t_x1t[b]])
                P.dma("sp", lambda: nc.sync.dma_start(out=x1_d[oi * 128:(oi + 1) * 128, :], in_=x1t[b][:]), reads=[t_x1t[b]])
                r0 = 32 * b
                P.op("act", lambda: A.activation(out=junk3[:], in_=x1t[b][:], func=AF.Square, accum_out=rs[:, r0:r0 + 1]), reads=[t_x1t[b]], writes=[t_junk3, t_rs[b]])
                P.op("dve", lambda: V.tensor_scalar(out=rs[:, r0 + 1:r0 + 2], in0=rs[:, r0:r0 + 1], scalar1=1.0 / D, scalar2=EPS, op0=ALU.mult, op1=ALU.add), reads=[t_rs[b]], writes=[t_rs[b]])
                P.op("act", lambda: A.activation(out=rs[:, r0 + 1:r0 + 2], in_=rs[:, r0 + 1:r0 + 2], func=AF.Sqrt), reads=[t_rs[b]], writes=[t_rs[b]])
                P.op("dve", lambda: V.reciprocal(out=rs[:, r0 + 1:r0 + 2], in_=rs[:, r0 + 1:r0 + 2]), reads=[t_rs[b]], writes=[t_rs[b]])
                P.op("dve", lambda: V.scalar_tensor_tensor(out=h2f[b][:], in0=x1t[b][:], scalar=rs[:, r0 + 1:r0 + 2], in1=A2b[:], op0=ALU.mult, op1=ALU.mult),
                     reads=[t_x1t[b], t_rs[b], t_m2], writes=[t_h2f[b]])
                P.op("dve", lambda: V.tensor_tensor(out=h2tok[b][:], in0=h2f[b][:], in1=S2b[:], op=ALU.add), reads=[t_h2f[b], t_m2], writes=[t_h2tok[b]])
                bk = 6 + b
                pT2 = bank[bk][:, :].bitcast(BF16)
                P.group("pe", [(lambda kc=kc: T.transpose(out=pT2[:, kc * 128:(kc + 1) * 128], in_=h2tok[b][:, kc * 128:(kc + 1) * 128], identity=identb[:])) for kc in range(8)],
                        reads=[t_h2tok[b], t_cst], writes=[tb[bk]])
                P.op("act", lambda: A.activation(out=h2Tt[b][:].rearrange("p a b -> p (a b)"), in_=pT2[:, :], func=AF.Copy), reads=[tb[bk]], writes=[t_h2Tt[b]])
                fns = [(lambda kc=kc: T.matmul(bank[b][:, 0:NE], lhsT=h2Tt[b][:, kc, :], rhs=wr[:, kc, :], start=(kc == 0), stop=False)) for kc in range(8)]
                fns.append(lambda: T.matmul(bank[b][:, 0:NE], lhsT=onesb[0:1, :], rhs=brb[0:1, :], start=False, stop=True))
                P.group("pe", fns, reads=[t_h2Tt[b], t_wr, t_cst], writes=[tb[b]])
                L0, L1, L2, L3 = lg[:, b, 0:NE], lg[:, b, NE:NE + 8], lg[:, b, 2 * NE:3 * NE], lg[:, b, 3 * NE:3 * NE + 8]
                P.op("dve", lambda: V.tensor_copy(out=L0, in_=bank[b][:, 0:NE]), reads=[tb[b]], writes=[t_lg[b]])
                P.op("dve", lambda: V.max(out=L1, in_=L0), reads=[t_lg[b]], writes=[t_lg[b]])
                P.op("dve", lambda: V.max_index(out=idx8[:, b, :], in_max=L1, in_values=L0), reads=[t_lg[b]], writes=[t_lg[b]])
                P.op("dve", lambda: V.tensor_scalar(out=maskb[:, oi, :], in0=L0, scalar1=lg[:, b, NE + 3:NE + 4], scalar2=None, op0=ALU.is_ge), reads=[t_lg[b]], writes=[t_maskb[oi]])
                P.op("dve", lambda: V.tensor_scalar(out=rs[:, r0 + 2:r0 + 3], in0=lg[:, b, NE:NE + 1], scalar1=-1.0, scalar2=None, op0=ALU.mult), reads=[t_lg[b]], writes=[t_rs[b]])
                P.op("act", lambda: A.activation(out=L3[:, 0:4], in_=L1[:, 0:4], func=AF.Exp, bias=rs[:, r0 + 2:r0 + 3], scale=1.0, accum_out=rs[:, r0 + 3:r0 + 4]),
                     reads=[t_lg[b], t_rs[b]], writes=[t_lg[b], t_rs[b]])
                P.op("dve", lambda: V.reciprocal(out=rs[:, r0 + 3:r0 + 4], in_=rs[:, r0 + 3:r0 + 4]), reads=[t_rs[b]], writes=[t_rs[b]])
                P.op("dve", lambda: V.tensor_scalar(out=gate4[:, 4 * oi:4 * oi + 4], in0=L3[:, 0:4], scalar1=rs[:, r0 + 3:r0 + 4], scalar2=None, op0=ALU.mult),
                     reads=[t_lg[b], t_rs[b]], writes=[t_gate4[oi]])
                pb = bank[b]
                P.group("pe", [lambda: T.matmul(pb[:, 64:64 + NE], lhsT=trib[:], rhs=maskb[:, oi, :], start=True, stop=True),
                               lambda: T.matmul(pb[:, 128:128 + NE], lhsT=ones128b[:], rhs=maskb[:, oi, :], start=True, stop=True)],
                        reads=[t_maskb[oi], t_cst, t_lg[b]], writes=[tb[b]])
                P.op("dve", lambda: V.tensor_tensor(out=posb[:, b, :], in0=pb[:, 64:64 + NE], in1=cnt_run[:], op=ALU.add), reads=[tb[b], t_cnt], writes=[t_lg[b]])
                P.op("dve", lambda: V.tensor_tensor(out=cnt_run[:], in0=pb[:, 128:128 + NE], in1=cnt_run[:], op=ALU.add), reads=[tb[b], t_cnt, t_lg[b]], writes=[t_cnt])
                EK = rs[:, r0 + 8:r0 + 12]; PK = rs[:, r0 + 12:r0 + 16]; DF = rs[:, r0 + 16:r0 + 20]
                P.op("dve", lambda: V.tensor_copy(out=EK, in_=idx8[:, b, 0:4]), reads=[t_lg[b]], writes=[t_rs[b]])
                for k in range(4):
                    P.op("dve", lambda k=k: V.scalar_tensor_tensor(out=junkp[:, b, :], in0=iota32, scalar=rs[:, r0 + 8 + k:r0 + 9 + k], in1=posb[:, b, :],
                                                                   op0=ALU.is_equal, op1=ALU.mult, accum_out=rs[:, r0 + 12 + k:r0 + 13 + k]),
                         reads=[t_lg[b], t_rs[b], t_cst], writes=[t_rs[b]])
                P.op("dve", lambda: V.scalar_tensor_tensor(out=DF, in0=EK, scalar=float(CAP), in1=PK, op0=ALU.mult, op1=ALU.add), reads=[t_rs[b]], writes=[t_rs[b]])
                P.op("dve", lambda: V.tensor_copy(out=dest_i[:, 4 * oi:4 * oi + 4], in_=DF), reads=[t_rs[b]], writes=[t_dest[oi]])
                for k in range(4):
                    P.dma("pool", lambda k=k: G_.indirect_dma_start(out=xbuf_d[:, :], out_offset=bass.IndirectOffsetOnAxis(ap=dest_i[:, 4 * oi + k:4 * oi + k + 1], axis=0),
                                                                    in_=h2tok[b][:, :], in_offset=None),
                          reads=[t_h2tok[b], t_dest[oi]], writes=[t_xbuf])
            for oi in range(NOWN):
                p3(oi)
            cf = rs[0:1, 0:NE]
            P.op("dve", lambda: V.tensor_scalar(out=cf, in0=cnt_run[0:1, :], scalar1=127.0, scalar2=1.0 / 128, op0=ALU.add, op1=ALU.mult), reads=[t_cnt] + t_rs, writes=t_rs)
            P.op("dve", lambda: V.tensor_scalar(out=cf, in0=cf, scalar1=-0.496, scalar2=None, op0=ALU.add), reads=t_rs, writes=t_rs)
            P.op("dve", lambda: V.tensor_copy(out=cnt_i[:], in_=cf), reads=t_rs, writes=[t_cnti])
            P.op("dve", lambda: V.tensor_scalar(out=posb[:, 0, :], in0=cnt_run[:], scalar1=iota_p, scalar2=None, op0=ALU.add), reads=[t_cnt, t_cst] + t_lg, writes=t_lg)
            P.op("dve", lambda: V.tensor_scalar(out=posb[:, 1, :], in0=posb[:, 0, :], scalar1=float(CAP), scalar2=None, op0=ALU.is_ge), reads=t_lg, writes=t_lg)
            P.op("dve", lambda: V.tensor_tensor(out=posb[:, 0, :], in0=posb[:, 0, :], in1=e2048, op=ALU.add), reads=t_lg + [t_cst], writes=t_lg)
            P.op("dve", lambda: V.tensor_scalar(out=rs[:, 40:41], in0=iota_p, scalar1=float(NE * CAP), scalar2=None, op0=ALU.add), reads=[t_cst] + t_rs, writes=t_rs)
            P.op("dve", lambda: V.tensor_scalar(out=junkp[:, 0, :], in0=posb[:, 0, :], scalar1=-1.0, scalar2=rs[:, 40:41], op0=ALU.mult, op1=ALU.add), reads=t_lg + t_rs, writes=t_lg)
            P.op("dve", lambda: V.tensor_tensor(out=junkp[:, 0, :], in0=junkp[:, 0, :], in1=posb[:, 1, :], op=ALU.mult), reads=t_lg, writes=t_lg)
            P.op("dve", lambda: V.tensor_tensor(out=posb[:, 0, :], in0=posb[:, 0, :], in1=junkp[:, 0, :], op=ALU.add), reads=t_lg, writes=t_lg)
            P.op("dve", lambda: V.tensor_copy(out=padidx[:], in_=posb[:, 0, :]), reads=t_lg, writes=[t_pad])
            for e in range(NE):
                P.dma("pool", lambda e=e: G_.indirect_dma_start(out=xbuf_d[:, :], out_offset=bass.IndirectOffsetOnAxis(ap=padidx[:, e:e + 1], axis=0),
                                                                in_=zrow[:, :], in_offset=None),
                      reads=[t_zrow, t_pad], writes=[t_xbuf])
            P.barrier()
            P.emit()

        sA.close()
        if int(os.environ.get('K_STOP', '9')) <= 3:
            return nc
        with ExitStack() as s4:
            NW = 3
            w1b = [sbuf(s4, "w1b%d" % i, [128, 8, 2 * D], BF16) for i in range(NW)]
            w2b = [sbuf(s4, "w2b%d" % i, [128, 8, D], BF16) for i in range(NW)]
            b1r = [sbuf(s4, "b1r%d" % i, [1, 2 * D], BF16) for i in range(NW)]
            b2r = [sbuf(s4, "b2r%d" % i, [1, D], BF16) for i in range(NW)]
            t_w = trs(NW)
            Xtok = [sbuf(s4, "Xtok%d" % i, [128, D], BF16) for i in range(2)]; t_Xtok = trs(2)
            XT = [sbuf(s4, "XT%d" % i, [128, 8, 128], BF16) for i in range(2)]; t_XT = trs(2)
            gg = [sbuf(s4, "gg%d" % i, [128, 256]) for i in range(2)]; t_gg = trs(2)
            sg = [sbuf(s4, "sg%d" % i, [128, 256]) for i in range(2)]; t_sg = trs(2)
            ll = [sbuf(s4, "ll%d" % i, [128, 256]) for i in range(2)]; t_ll = trs(2)
            atok = [sbuf(s4, "atok%d" % i, [128, D], BF16) for i in range(2)]; t_atok = trs(2)
            aT = [sbuf(s4, "aT%d" % i, [128, 8, 128], BF16) for i in range(2)]; t_aT = trs(2)
            yt = [sbuf(s4, "yt%d" % i, [128, D]) for i in range(2)]; t_yt = trs(2)
            t_ybuf = Tr()
            w1_v = w1_d.rearrange("e (k p) n -> e p k n", p=128)
            w2_v = w2_d.rearrange("e (k p) n -> e p k n", p=128)
            blk = [0]

            def block_body(e, bslot, ws):
                n = blk[0]
                blk[0] += 1
                xb = n % 2
                row0 = e * CAP + bslot * 128
                P.dma("sp", lambda: nc.sync.dma_start(out=Xtok[xb][:], in_=xbuf_d[row0:row0 + 128, :]), reads=[t_xbuf], writes=[t_Xtok[xb]], slot=xb)
                pT = bank[xb][:, :].bitcast(BF16)
                P.group("pe", [(lambda kc=kc: T.transpose(out=pT[:, kc * 128:(kc + 1) * 128], in_=Xtok[xb][:, kc * 128:(kc + 1) * 128], identity=identb[:])) for kc in range(8)],
                        reads=[t_Xtok[xb], t_cst], writes=[tb[xb]])
                P.op("act", lambda: A.activation(out=XT[xb][:].rearrange("p a b -> p (a b)"), in_=pT[:, :], func=AF.Copy), reads=[tb[xb]], writes=[t_XT[xb]])
                def do_cch(cch):
                    bk = 2 + (cch % 2)
                    par = cch % 2
                    fns = [(lambda kc=kc: T.matmul(bank[bk][:, :], lhsT=XT[xb][:, kc, :], rhs=w1b[ws][:, kc, cch * 512:(cch + 1) * 512], start=(kc == 0), stop=False)) for kc in range(8)]
                    fns.append(lambda: T.matmul(bank[bk][:, :], lhsT=onesb[0:1, :], rhs=b1r[ws][0:1, cch * 512:(cch + 1) * 512], start=False, stop=True))
                    P.group("pe", fns, reads=[t_XT[xb], t_w[ws], t_cst], writes=[tb[bk]])
                    P.op("dve", lambda: V.tensor_scalar(out=gg[par][:], in0=bank[bk][:, 0:512:2], scalar1=7.0, scalar2=None, op0=ALU.min), reads=[tb[bk]], writes=[t_gg[par]])
                    P.op("act", lambda: A.activation(out=sg[par][:], in_=gg[par][:], func=AF.Gelu_apprx_sigmoid), reads=[t_gg[par]], writes=[t_sg[par]])
                    P.op("dve", lambda: V.tensor_scalar(out=ll[par][:], in0=bank[bk][:, 1:512:2], scalar1=7.0, scalar2=-7.0, op0=ALU.min, op1=ALU.max), reads=[tb[bk]], writes=[t_ll[par]])
                    P.op("dve", lambda: V.scalar_tensor_tensor(out=atok[xb][:, cch * 256:(cch + 1) * 256], in0=ll[par][:], scalar=1.0, in1=sg[par][:], op0=ALU.add, op1=ALU.mult),
                         reads=[t_ll[par], t_sg[par]], writes=[t_atok[xb]])
                for cch in range(4):
                    do_cch(cch)
                bk = 4 + xb
                pT2 = bank[bk][:, :].bitcast(BF16)
                P.group("pe", [(lambda j=j: T.transpose(out=pT2[:, j * 128:(j + 1) * 128], in_=atok[xb][:, j * 128:(j + 1) * 128], identity=identb[:])) for j in range(8)],
                        reads=[t_atok[xb], t_cst], writes=[tb[bk]])
                P.op("act", lambda: A.activation(out=aT[xb][:].rearrange("p a b -> p (a b)"), in_=pT2[:, :], func=AF.Copy), reads=[tb[bk]], writes=[t_aT[xb]])
                def do_hf(hf):
                    bk2 = 6 + hf
                    fns = [(lambda j=j: T.matmul(bank[bk2][:, :], lhsT=aT[xb][:, j, :], rhs=w2b[ws][:, j, hf * 512:(hf + 1) * 512], start=(j == 0), stop=False)) for j in range(8)]
                    fns.append(lambda: T.matmul(bank[bk2][:, :], lhsT=onesb[0:1, :], rhs=b2r[ws][0:1, hf * 512:(hf + 1) * 512], start=False, stop=True))
                    P.group("pe", fns, reads=[t_aT[xb], t_w[ws], t_cst], writes=[tb[bk2]])
                    if hf == 0:
                        P.op("act", lambda: A.activation(out=yt[xb][:, 0:512], in_=bank[bk2][:, :], func=AF.Copy), reads=[tb[bk2]], writes=[t_yt[xb]])
                    else:
                        P.op("dve", lambda: V.tensor_copy(out=yt[xb][:, 512:1024], in_=bank[bk2][:, :]), reads=[tb[bk2]], writes=[t_yt[xb]])
                for hf in range(2):
                    do_hf(hf)
                P.dma("sp", lambda: nc.sync.dma_start(out=ybuf_d[row0:row0 + 128, :], in_=yt[xb][:]), reads=[t_yt[xb]], writes=[t_ybuf], slot=2 + xb)

            for e in range(int(os.environ.get('K_NE', NE))):
                ws = e % NW
                P.dma("pool", lambda e=e, ws=ws: G_.dma_start(out=w1b[ws][:], in_=w1_v[e]), writes=[t_w[ws]])
                P.dma("pool", lambda e=e, ws=ws: G_.dma_start(out=w2b[ws][:], in_=w2_v[e]), writes=[t_w[ws]])
                P.dma("pool", lambda e=e, ws=ws: G_.dma_start(out=b1r[ws][:], in_=b1_d[e:e + 1, :]), writes=[t_w[ws]])
                P.dma("pool", lambda e=e, ws=ws: G_.dma_start(out=b2r[ws][:], in_=b2_d[e:e + 1, :]), writes=[t_w[ws]])
                P.regload(cnt_i[0:1, e:e + 1], reads=[t_cnti])
                NB = CAP // 128
                for bslot in range(NB):
                    if bslot in (3, 6, 10):
                        P.cond_begin(bslot + 1)
                    P.cond_begin(bslot + 1)
                    block_body(e, bslot, ws)
                    P.cond_end()
                for _ in range(3):
                    P.cond_end()
            P.barrier()
            P.emit()

        if int(os.environ.get('K_STOP', '9')) <= 4:
            return nc
        with ExitStack() as s5:
            gt2_b = sbuf(s5, "gt2_b", [128, D]); gfin_b = sbuf(s5, "gfin_b", [128, D]); t_g5 = Tr()
            yk = [sbuf(s5, "yk%d" % i, [128, D]) for i in range(4)]; t_yk = trs(4)
            acc = [sbuf(s5, "acc%d" % i, [128, D]) for i in range(2)]; t_acc = trs(2)
            x1b = [sbuf(s5, "x1b%d" % i, [128, D]) for i in range(2)]; t_x1b = trs(2)
            ob_ = [sbuf(s5, "ob%d" % i, [128, D]) for i in range(2)]; t_ob = trs(2)
            junk5 = sbuf(s5, "junk5", [128, D], BF16); t_junk5 = Tr()
            fs5 = sbuf(s5, "fs5", [128, 8]); t_fs5 = trs(2)
            P.dma("sp", lambda: nc.sync.dma_start(out=gt2_b[:], in_=mod_d[0:1, 5 * D:6 * D].partition_broadcast(128)), writes=[t_g5])
            P.dma("sp", lambda: nc.sync.dma_start(out=gfin_b[:], in_=gfin_d[0:1, :].partition_broadcast(128)), writes=[t_g5])

            def p5(oi):
                b = oi % 2
                P.dma("sp", lambda: nc.sync.dma_start(out=x1b[b][:], in_=x1_d[oi * 128:(oi + 1) * 128, :]), writes=[t_x1b[b]])
                for k in range(4):
                    P.dma("pool", lambda k=k: G_.indirect_dma_start(out=yk[k][:, :], out_offset=None, in_=ybuf_d[:, :],
                                                                    in_offset=bass.IndirectOffsetOnAxis(ap=dest_i[:, 4 * oi + k:4 * oi + k + 1], axis=0),
                                                                    ), reads=[t_dest[oi]], writes=[t_yk[k]])
                    if k == 0:
                        P.op("dve", lambda: V.tensor_scalar(out=acc[b][:], in0=yk[0][:], scalar1=gate4[:, 4 * oi:4 * oi + 1], scalar2=None, op0=ALU.mult),
                             reads=[t_yk[0], t_gate4[oi]], writes=[t_acc[b]])
                    else:
                        P.op("dve", lambda k=k: V.scalar_tensor_tensor(out=acc[b][:], in0=yk[k][:], scalar=gate4[:, 4 * oi + k:4 * oi + k + 1], in1=acc[b][:], op0=ALU.mult, op1=ALU.add),
                             reads=[t_yk[k], t_gate4[oi], t_acc[b]], writes=[t_acc[b]])
                P.op("dve", lambda: V.tensor_tensor(out=acc[b][:], in0=acc[b][:], in1=gt2_b[:], op=ALU.mult), reads=[t_acc[b], t_g5], writes=[t_acc[b]])
                P.op("dve", lambda: V.tensor_tensor(out=x1b[b][:], in0=acc[b][:], in1=x1b[b][:], op=ALU.add), reads=[t_acc[b], t_x1b[b]], writes=[t_x1b[b]])
                P.op("act", lambda: A.activation(out=junk5[:], in_=x1b[b][:], func=AF.Square, accum_out=fs5[:, 4 * b:4 * b + 1]), reads=[t_x1b[b]], writes=[t_junk5, t_fs5[b]])
                P.op("dve", lambda: V.tensor_scalar(out=fs5[:, 4 * b + 1:4 * b + 2], in0=fs5[:, 4 * b:4 * b + 1], scalar1=1.0 / D, scalar2=EPS, op0=ALU.mult, op1=ALU.add),
                     reads=[t_fs5[b]], writes=[t_fs5[b]])
                P.op("act", lambda: A.activation(out=fs5[:, 4 * b + 1:4 * b + 2], in_=fs5[:, 4 * b + 1:4 * b + 2], func=AF.Sqrt), reads=[t_fs5[b]], writes=[t_fs5[b]])
                P.op("dve", lambda: V.reciprocal(out=fs5[:, 4 * b + 1:4 * b + 2], in_=fs5[:, 4 * b + 1:4 * b + 2]), reads=[t_fs5[b]], writes=[t_fs5[b]])
                P.op("dve", lambda: V.scalar_tensor_tensor(out=ob_[b][:], in0=x1b[b][:], scalar=fs5[:, 4 * b + 1:4 * b + 2], in1=gfin_b[:], op0=ALU.mult, op1=ALU.mult),
                     reads=[t_x1b[b], t_fs5[b], t_g5], writes=[t_ob[b]])
                P.dma("sp", lambda: nc.sync.dma_start(out=out_d[oi * 128:(oi + 1) * 128, :], in_=ob_[b][:]), reads=[t_ob[b]])
            for oi in range(NOWN):
                p5(oi)
            P.barrier()
            P.emit()
    return nc


def _consts():
    c = np.zeros((128, NCST), np.float32)
    c[:, 0:128] = np.eye(128, dtype=np.float32)
    k = np.arange(128)[:, None]
    q = np.arange(128)[None, :]
    c[:, 128:256] = (k <= q)
    c[:, 256:384] = (k > q)
    inv = (1.0 / (np.float32(10000.0) ** (np.arange(0, 64, 2, dtype=np.float32) / np.float32(64)))).astype(np.float32)
    p = np.arange(128)
    c[:, 384] = inv[p % 32]
    c[:, 385] = np.where((p % 64) < 32, -1.0, 1.0)
    c[:, 386] = np.float32(math.pi / 2)
    c[:, 387] = 0.0
    c[:, 388] = 1.0
    c[:, 392:520] = 1.0
    c[:, 520:648] = (k < q)
    c[:, 648:680] = np.arange(32)[None, :]
    c[:, 680:712] = (np.arange(32) * CAP)[None, :]
    c[:, 712] = np.arange(128)
    return c


def _core_masks(j):
    own = own_blocks(j)
    k = np.arange(128)[:, None]
    q = np.arange(128)[None, :]
    tri = (k <= q).astype(np.float32)
    low = (k > q).astype(np.float32)
    dm = np.zeros((128, NOWN, 4, 128), np.float32)
    sm = np.zeros((128, NOWN, 2, 128), np.float32)
    for oi, gb in enumerate(own):
        nkb = 8 * (oi // 2) + (4 if oi % 2 == 0 else 8)
        for i in range(4):
            kb = nkb - 4 + i
            if kb < gb:
                dm[:, oi, i, :] = 1.0
            elif kb == gb:
                dm[:, oi, i, :] = tri
        sm[:, oi, 1, :] = tri
        if gb > 0:
            sm[:, oi, 0, :] = low
    return dm.reshape(128, NOWN * 512), sm.reshape(128, NOWN * 256)


_NC_CACHE = {}


def kernel(x, c, positions, w_ada, b_ada, g_mix, w_in, b_in, attn_sinks, lambda_q1, lambda_k1, lambda_q2, lambda_k2,
           g_subln, w_out, b_out, g_ffn, w_router, b_router, w1, b1, w2, b2, g_final):
    f = lambda a: np.ascontiguousarray(np.asarray(a))
    x = f(x); positions = f(positions)
    if "nc" not in _NC_CACHE:
        _NC_CACHE["nc"] = build_program()
    nc = _NC_CACHE["nc"]
    colT = lambda v: f(np.asarray(v).reshape(-1, 128).T)
    w_sel = f(np.asarray(w_in)[0][:, SEL])
    b_sel = f(np.asarray(b_in)[0][SEL])
    b1_ = np.asarray(b1)[0]
    shared = {
        "w_ada": f(np.asarray(w_ada)[0]), "b_ada": f(np.asarray(b_ada)[0][None, :]),
        "gmixT": colT(np.asarray(g_mix)[0]), "gffnT": colT(np.asarray(g_ffn)[0]),
        "w_sel": w_sel, "b_selT": colT(b_sel), "b_sel": f(b_sel[None, :]),
        "sinks": f(np.asarray(attn_sinks)[0][None, :]),
        "lam4": f(np.stack([np.asarray(lambda_q1)[0], np.asarray(lambda_k1)[0], np.asarray(lambda_q2)[0], np.asarray(lambda_k2)[0]])),
        "g_subln": f(np.asarray(g_subln)[0][None, :]),
        "w_out": f(np.asarray(w_out)[0]), "b_out": f(np.asarray(b_out)[0][None, :]),
        "w_router": f(np.asarray(w_router)[0]), "b_router": f(np.asarray(b_router)[0][None, :]),
        "w1": f(np.asarray(w1)[0]), "w2": f(np.asarray(w2)[0]), "b2": f(np.asarray(b2)[0]),
        "b1": f(b1_), "g_ffn": f(np.asarray(g_ffn)[0][None, :]), "g_mix": f(np.asarray(g_mix)[0][None, :]),
        "g_final": f(np.asarray(g_final)[None, :]),
        "consts": _consts(),
    }
    in_maps = []
    rows_all = []
    for core in range(8):
        b, j = core // 4, core % 4
        own = own_blocks(j)
        rows_own = np.concatenate([np.arange(g * 128, (g + 1) * 128) for g in own])
        rows_prev = np.concatenate([np.arange(max(g - 1, 0) * 128, (max(g - 1, 0) + 1) * 128) for g in own])
        rows_all.append(rows_own)
        xb = x[b]
        x_ext = np.concatenate([xb, xb[rows_own], xb[rows_prev]], axis=0)
        pb = positions[b]
        pos_ext = np.concatenate([pb, pb[rows_own], pb[rows_prev]])[None, :].astype(np.int32)
        dm, sm = _core_masks(j)
        m = dict(shared)
        m.update({"x": f(x_ext), "pos": f(pos_ext), "cT": colT(np.asarray(c)[b]), "dmask": dm, "smask": sm})
        in_maps.append(m)
    res = run_bass_kernel_spmd(nc, in_maps, core_ids=list(range(8)))
    out = np.zeros((2, S, D), np.float32)
    for core in range(8):
        out[core // 4, rows_all[core], :] = np.asarray(res.results[core]["out"])
    return out
```

```python
import math
import os
from contextlib import ExitStack

import numpy as np
import concourse.bass as bass
import concourse.mybir as mybir
from concourse.bass_utils import run_bass_kernel_spmd

F32 = mybir.dt.float32
BF16 = mybir.dt.bfloat16
I32 = mybir.dt.int32
ALU = mybir.AluOpType
AF = mybir.ActivationFunctionType
AX = mybir.AxisListType

D = 1024
S = 8192
NT = 64
NG = 16
NOWN = 16
NE = 32
SX = S + 2 * NOWN * 128
NGX = SX // 512
NCST = 720
CAP = 2048
U32 = mybir.dt.uint32
EPS = 1e-5
C1 = 6.28125
C2 = 2 * math.pi - 6.28125
INV2PI = float(1.0 / (2 * math.pi))

OFF_QA, OFF_KA, OFF_VA, OFF_QD, OFF_KD, OFF_VD = 0, 512, 640, 768, 1280, 1792


def _swap64(cols):
    cols = np.asarray(cols).reshape(-1, 64)
    return np.concatenate([cols[:, 32:], cols[:, :32]], axis=1).reshape(-1)


def _unit_cols():
    units = []
    k = np.concatenate([np.tile(np.arange(OFF_KA + g * 64, OFF_KA + (g + 1) * 64), 2) for g in range(2)])
    q = np.arange(OFF_QA, OFF_QA + 512)
    v = np.arange(OFF_VA, OFF_VA + 128)
    units.append(dict(nk=2, nq=4, k=k, q=q, v=v))
    for h in range(4):
        k = np.arange(OFF_KD + h * 128, OFF_KD + (h + 1) * 128)
        q = np.arange(OFF_QD + h * 128, OFF_QD + (h + 1) * 128)
        v = np.arange(OFF_VD + h * 128, OFF_VD + (h + 1) * 128)
        units.append(dict(nk=1, nq=1, k=k, q=q, v=v))
    off = 0
    sel = []
    for u in units:
        u["base"] = off
        parts = [u["k"], _swap64(u["k"]), u["q"], _swap64(u["q"]), u["v"]]
        u["o_k"] = 0
        u["o_ks"] = len(u["k"])
        u["o_q"] = u["o_ks"] + len(u["k"])
        u["o_qs"] = u["o_q"] + len(u["q"])
        u["o_v"] = u["o_qs"] + len(u["q"])
        u["ncols"] = u["o_v"] + 128
        sel.append(np.concatenate(parts))
        off += u["ncols"]
    return units, np.concatenate(sel)


UNITS, SEL = _unit_cols()
NSEL = len(SEL)
NCH = NSEL // 128


def own_blocks(j):
    return sorted([8 * m + j for m in range(8)] + [8 * m + 7 - j for m in range(8)])


class Tr:
    __slots__ = ("w", "r")

    def __init__(self):
        self.w = {}
        self.r = {}


def trs(n):
    return [Tr() for _ in range(n)]


class Prog:
    ENG = ("pe", "act", "dve", "pool", "sp")

    def __init__(self, nc, stack, n_dma_sems=48):
        self.nc = nc
        self.q = {e: [] for e in self.ENG}
        self.esem = {e: stack.enter_context(nc.semaphore("s_" + e)) for e in self.ENG}
        self.ecnt = {e: 0 for e in self.ENG}
        self.waited = {e: {} for e in self.ENG}
        self.dsem = [stack.enter_context(nc.semaphore("d%d" % i)) for i in range(n_dma_sems)]
        self.dcnt = [0] * n_dma_sems
        self.dpool = {"sp": list(range(0, n_dma_sems - 16)), "pool": list(range(n_dma_sems - 16, n_dma_sems))}
        self.dnext = {"sp": 0, "pool": 0}
        self.in_cond = False
        self.handles = {"pe": nc.tensor, "act": nc.scalar, "dve": nc.vector, "pool": nc.gpsimd, "sp": nc.sync}

    def _need(self, eng, s, v):
        wd = self.waited[eng]
        if wd.get(s, 0) >= v:
            return
        wd[s] = v
        self.q[eng].append(("wait", s, v))

    def _waits(self, eng, reads, writes):
        need = {}
        for t in reads:
            for s, v in t.w.items():
                if need.get(s, 0) < v:
                    need[s] = v
        for t in writes:
            for s, v in t.w.items():
                if need.get(s, 0) < v:
                    need[s] = v
            for s, v in t.r.items():
                if need.get(s, 0) < v:
                    need[s] = v
        for s, v in need.items():
            if eng == "pe" and s is self.esem["pe"]:
                continue
            self._need(eng, s, v)

    def _record(self, ev, reads, writes):
        s, v = ev
        for t in reads:
            if t.r.get(s, 0) < v:
                t.r[s] = v
        for t in writes:
            if self.in_cond:
                if t.w.get(s, 0) < v:
                    t.w[s] = v
            else:
                t.w = {s: v}
                t.r = {}

    def op(self, eng, fn, reads=(), writes=()):
        self._waits(eng, reads, writes)
        self.ecnt[eng] += 1
        ev = (self.esem[eng], self.ecnt[eng])
        self.q[eng].append(("op", fn, self.esem[eng], 1))
        self._record(ev, reads, writes)

    def group(self, eng, fns, reads=(), writes=()):
        self._waits(eng, reads, writes)
        self.ecnt[eng] += 1
        ev = (self.esem[eng], self.ecnt[eng])
        for f in fns[:-1]:
            self.q[eng].append(("op", f, None, 0))
        self.q[eng].append(("op", fns[-1], self.esem[eng], 1))
        self._record(ev, reads, writes)

    def dma(self, eng, fn, reads=(), writes=(), slot=None):
        pl = self.dpool[eng]
        if slot is None:
            i = pl[self.dnext[eng]]
            self.dnext[eng] = (self.dnext[eng] + 1) % (len(pl) - 4)
        else:
            i = pl[len(pl) - 4 + slot]
        s = self.dsem[i]
        if self.dcnt[i]:
            self._need(eng, s, self.dcnt[i])
        self._waits(eng, reads, writes)
        self.dcnt[i] += 16
        ev = (s, self.dcnt[i])
        self.q[eng].append(("op", fn, s, 16))
        self._record(ev, reads, writes)
        return ev

    CENG = ("pe", "act", "dve", "sp")

    def regload(self, ap, reads=()):
        for e in self.CENG:
            self._waits(e, reads, ())
            self.q[e].append(("regload", ap))

    def cond_begin(self, thr):
        if not hasattr(self, "_cstack"):
            self._cstack = []
        self._cstack.append(({e: self.ecnt[e] for e in self.ENG}, list(self.dcnt), {e: dict(self.waited[e]) for e in self.ENG}))
        self.in_cond = True
        for e in self.CENG:
            self.q[e].append(["if", thr, None])

    def cond_end(self):
        ec0, dc0, wd0 = self._cstack.pop()
        assert self.ecnt["pool"] == ec0["pool"], "pool must stay outside conditional regions"
        dd = [(i, self.dcnt[i] - dc0[i]) for i in range(len(self.dcnt)) if self.dcnt[i] != dc0[i]]
        for i, _ in dd:
            assert i in self.dpool["sp"]
        for e in self.CENG:
            comp = []
            if self.ecnt[e] != ec0[e]:
                comp.append((self.esem[e], self.ecnt[e] - ec0[e]))
            if e == "sp":
                comp += [(self.dsem[i], d, dc0[i]) for i, d in dd]
            for it in reversed(self.q[e]):
                if isinstance(it, list) and it[0] == "if" and it[2] is None:
                    it[2] = comp
                    break
            self.q[e].append(("endif",))
            self.waited[e] = wd0[e]
        self.waited["pool"] = wd0["pool"]
        self.in_cond = bool(self._cstack)

    def barrier(self):
        for e in self.ENG:
            for f in self.ENG:
                if f != e and self.ecnt[f]:
                    self._need(e, self.esem[f], self.ecnt[f])
            for i, s in enumerate(self.dsem):
                if self.dcnt[i]:
                    self._need(e, s, self.dcnt[i])

    def emit(self):
        nc = self.nc
        q = self.q
        self.q = {e: [] for e in self.ENG}
        if not hasattr(self, "regs"):
            self.regs = {}
        with nc.Block() as block:
            def run_items(h, ename, items):
                i = 0
                n = len(items)
                while i < n:
                    it = items[i]
                    k = it[0]
                    if k == "wait":
                        h.wait_ge(it[1], it[2])
                    elif k == "op":
                        ins = it[1]()
                        if it[2] is not None:
                            ins.then_inc(it[2], it[3])
                    elif k == "regload":
                        if ename not in self.regs:
                            self.regs[ename] = h.alloc_register("cnt_" + ename)
                        h.reg_load(self.regs[ename], it[1])
                    elif k == "if":
                        depth = 1
                        j = i + 1
                        while True:
                            if items[j][0] == "if":
                                depth += 1
                            elif items[j][0] == "endif":
                                depth -= 1
                                if depth == 0:
                                    break
                            j += 1
                        body = items[i + 1:j]
                        with h.If_lt(self.regs[ename], it[1]):
                            h.drain()
                            for cp in it[2]:
                                if len(cp) == 3 and cp[2]:
                                    h.wait_ge(cp[0], cp[2])
                                h.sem_inc(cp[0], cp[1])
                        with h.Else():
                            run_items(h, ename, body)
                        i = j
                    i += 1

            def run(ename):
                run_items(self.handles[ename], ename, q[ename])

            @block.tensor
            def _(e):
                run("pe")

            @block.scalar
            def _(e):
                run("act")

            @block.vector
            def _(e):
                run("dve")

            @block.gpsimd
            def _(e):
                run("pool")

            @block.sync
            def _(e):
                run("sp")


def build_program(j_core_unused=None, debug=False):
    nc = bass.Bass("TRN2", target_bir_lowering=False)
    din = lambda name, shape, dt=F32: nc.dram_tensor(name, list(shape), dt, kind="ExternalInput").ap()
    x_d = din("x", [SX, D])
    pos_d = din("pos", [1, SX], I32)
    dmask_d = din("dmask", [128, NOWN * 512])
    smask_d = din("smask", [128, NOWN * 256])
    cT_d = din("cT", [128, 8])
    wada_d = din("w_ada", [D, 6 * D])
    bada_d = din("b_ada", [1, 6 * D])
    gmixT_d = din("gmixT", [128, 8])
    gffnT_d = din("gffnT", [128, 8])
    wsel_d = din("w_sel", [D, NSEL])
    bselT_d = din("b_selT", [128, NCH])
    bsel_d = din("b_sel", [1, NSEL])
    sinks_d = din("sinks", [1, 8])
    lam_d = din("lam4", [4, 64])
    gsub_d = din("g_subln", [1, 128])
    wout_d = din("w_out", [D, D])
    bout_d = din("b_out", [1, D])
    wr_d = din("w_router", [D, NE])
    br_d = din("b_router", [1, NE])
    w1_d = din("w1", [NE, D, 2 * D])
    b1_d = din("b1", [NE, 2 * D])
    gffn_d = din("g_ffn", [1, D])
    gmix_d = din("g_mix", [1, D])
    w2_d = din("w2", [NE, D, D])
    b2_d = din("b2", [NE, D])
    gfin_d = din("g_final", [1, D])
    cst_d = din("consts", [128, NCST])
    out_d = nc.dram_tensor("out", [NOWN * 128, D], F32, kind="ExternalOutput").ap()
    hT_d = nc.dram_tensor("hT_scr", [8, 128, SX], BF16, kind="Internal").ap()
    cos_d = nc.dram_tensor("cos_scr", [128, SX], F32, kind="Internal").ap()
    sin_d = nc.dram_tensor("sin_scr", [128, SX], F32, kind="Internal").ap()
    x1_d = nc.dram_tensor("x1_scr", [NOWN * 128, D], F32, kind="Internal").ap()
    mod_d = nc.dram_tensor("mod_scr", [1, 6 * D], F32, kind="Internal").ap()
    xbuf_d = nc.dram_tensor("xbuf_scr", [NE * CAP + 128, D], BF16, kind="Internal").ap()
    ybuf_d = nc.dram_tensor("ybuf_scr", [NE * CAP, D], F32, kind="Internal").ap()


    with ExitStack() as st:
        P = Prog(nc, st)
        sbuf = lambda stack, name, shape, dt=F32: stack.enter_context(nc.sbuf_tensor(name, list(shape), dt))
        V, A, T, G_ = nc.vector, nc.scalar, nc.tensor, nc.gpsimd

        bank = [st.enter_context(nc.psum_tensor("bank%d" % i, [128, 512], F32)) for i in range(8)]
        tb = trs(8)

        cst = sbuf(st, "cst", [128, NCST]); t_cst = Tr()
        identb = sbuf(st, "identb", [128, 128], BF16)
        mask256 = sbuf(st, "mask256", [128, 256], BF16)
        onesb = sbuf(st, "onesb", [1, 128], BF16)
        trib = sbuf(st, "trib", [128, 128], BF16)
        ones128b = sbuf(st, "ones128b", [128, 128], BF16)
        A1 = sbuf(st, "A1", [128, 8]); S1 = sbuf(st, "S1", [128, 8])
        A2 = sbuf(st, "A2", [128, 8]); S2 = sbuf(st, "S2", [128, 8])
        t_mod = Tr()
        t_mixed = trs(NOWN)
        small = sbuf(st, "small", [128, 64]); t_small = Tr()
        ident = cst[:, 0:128]
        invf = cst[:, 384:385]
        sgn = cst[:, 385:386]
        halfpi = cst[:, 386:387]
        zero_c = cst[:, 387:388]
        one11 = cst[0:1, 388:389]
        ones_row = cst[0:1, 392:520]
        neglam = small[:, 0:1]
        expsink = small[:, 8:16]

        P.dma("sp", lambda: nc.sync.dma_start(out=cst[:], in_=cst_d[:, :]), writes=[t_cst])
        P.op("dve", lambda: V.tensor_copy(out=identb[:], in_=cst[:, 0:128]), reads=[t_cst], writes=[t_cst])
        P.op("dve", lambda: V.tensor_copy(out=mask256[:, 0:128], in_=cst[:, 256:384]), reads=[t_cst], writes=[t_cst])
        P.op("dve", lambda: V.tensor_copy(out=mask256[:, 128:256], in_=cst[:, 128:256]), reads=[t_cst], writes=[t_cst])
        P.op("dve", lambda: V.tensor_copy(out=onesb[:], in_=cst[0:1, 392:520]), reads=[t_cst], writes=[t_cst])
        P.op("dve", lambda: V.tensor_copy(out=trib[:], in_=cst[:, 520:648]), reads=[t_cst], writes=[t_cst])
        P.op("dve", lambda: V.tensor_copy(out=ones128b[:], in_=cst[:, 392:520]), reads=[t_cst], writes=[t_cst])

        with ExitStack() as s0:
            cT = sbuf(s0, "cT_sb", [128, 8]); t_cT = Tr()
            wad = [sbuf(s0, "wad%d" % i, [128, 8, 512]) for i in range(2)]; t_wad = trs(2)
            modrow = sbuf(s0, "modrow", [1, 6 * D]); t_modrow = Tr()
            badar = sbuf(s0, "badar", [1, 6 * D]); t_bada = Tr()
            gT = sbuf(s0, "gT", [128, 16]); t_gT = Tr()
            lamb = sbuf(s0, "lamb", [128, 256]); t_lam = Tr()
            lamp = sbuf(s0, "lamp", [128, 128])
            P.dma("sp", lambda: nc.sync.dma_start(out=cT[:], in_=cT_d[:, :]), writes=[t_cT])
            P.dma("sp", lambda: nc.sync.dma_start(out=badar[:], in_=bada_d[:, :]), writes=[t_bada])
            P.dma("sp", lambda: nc.sync.dma_start(out=gT[:, 0:8], in_=gmixT_d[:, :]), writes=[t_gT])
            P.dma("sp", lambda: nc.sync.dma_start(out=gT[:, 8:16], in_=gffnT_d[:, :]), writes=[t_gT])
            P.dma("sp", lambda: nc.sync.dma_start(out=lamb[:].rearrange("p (a b) -> p a b", a=4),
                                                  in_=lam_d[:, :].partition_broadcast(128)), writes=[t_lam])
            P.dma("sp", lambda: nc.sync.dma_start(out=small[:, 16:24], in_=sinks_d[0:1, :].partition_broadcast(128)), writes=[t_small])
            posi = sbuf(s0, "posi", [128, 512], I32); t_posi = Tr()
            ang = sbuf(s0, "ang", [128, 512]); t_ang = Tr()
            ki = sbuf(s0, "ki", [128, 512], I32); kf = sbuf(s0, "kf", [128, 512]); t_k = Tr()
            rr = sbuf(s0, "rr", [128, 512]); t_rr = Tr()
            tab = [sbuf(s0, "tab%d" % i, [128, 512]) for i in range(4)]; t_tab = trs(4)
            def rope_group(g):
                P.dma("sp", lambda: nc.sync.dma_start(out=posi[:], in_=pos_d[0:1, g * 512:(g + 1) * 512].partition_broadcast(128)), writes=[t_posi])
                P.op("dve", lambda: V.tensor_copy(out=ang[:], in_=posi[:]), reads=[t_posi], writes=[t_ang])
                P.op("dve", lambda: V.tensor_scalar(out=ang[:], in0=ang[:], scalar1=invf, scalar2=None, op0=ALU.mult), reads=[t_ang, t_cst], writes=[t_ang])
                for which in range(2):
                    tbi = (2 * g + which) % 4
                    if which == 0:
                        P.op("dve", lambda: V.tensor_scalar(out=ki[:], in0=ang[:], scalar1=INV2PI, scalar2=None, op0=ALU.mult), reads=[t_ang], writes=[t_k])
                    else:
                        P.op("dve", lambda: V.tensor_scalar(out=ki[:], in0=ang[:], scalar1=INV2PI, scalar2=0.25, op0=ALU.mult, op1=ALU.add), reads=[t_ang], writes=[t_k])
                    P.op("dve", lambda: V.tensor_copy(out=kf[:], in_=ki[:]), reads=[t_k], writes=[t_k])
                    P.op("dve", lambda: V.scalar_tensor_tensor(out=rr[:], in0=kf[:], scalar=-C1, in1=ang[:], op0=ALU.mult, op1=ALU.add), reads=[t_k, t_ang], writes=[t_rr])
                    P.op("dve", lambda: V.scalar_tensor_tensor(out=rr[:], in0=kf[:], scalar=-C2, in1=rr[:], op0=ALU.mult, op1=ALU.add), reads=[t_k, t_rr], writes=[t_rr])
                    if which == 0:
                        P.op("dve", lambda: V.tensor_scalar(out=rr[:], in0=rr[:], scalar1=-3.1415925, scalar2=3.1415925, op0=ALU.max, op1=ALU.min), reads=[t_rr], writes=[t_rr])
                        P.op("act", lambda tbi=tbi: A.activation(out=tab[tbi][:], in_=rr[:], func=AF.Sin, scale=sgn, bias=zero_c), reads=[t_rr, t_cst], writes=[t_tab[tbi]])
                        P.dma("sp", lambda tbi=tbi: nc.sync.dma_start(out=sin_d[:, g * 512:(g + 1) * 512], in_=tab[tbi][:]), reads=[t_tab[tbi]])
                    else:
                        P.op("dve", lambda: V.tensor_scalar(out=rr[:], in0=rr[:], scalar1=-4.712388, scalar2=1.570796, op0=ALU.max, op1=ALU.min), reads=[t_rr], writes=[t_rr])
                        P.op("act", lambda tbi=tbi: A.activation(out=tab[tbi][:], in_=rr[:], func=AF.Sin, scale=1.0, bias=halfpi), reads=[t_rr, t_cst], writes=[t_tab[tbi]])
                        P.dma("sp", lambda tbi=tbi: nc.sync.dma_start(out=cos_d[:, g * 512:(g + 1) * 512], in_=tab[tbi][:]), reads=[t_tab[tbi]])
            for g in range(NGX):
                rope_group(g)

            P.op("act", lambda: A.activation(out=cT[:], in_=cT[:], func=AF.Silu), reads=[t_cT], writes=[t_cT])
            wada_v = wada_d.rearrange("(k p) n -> p k n", p=128)
            for pc in range(12):
                b = pc % 2
                P.dma("sp", lambda pc=pc, b=b: nc.sync.dma_start(out=wad[b][:], in_=wada_v[:, :, pc * 512:(pc + 1) * 512]), writes=[t_wad[b]])
                bk = pc % 2
                P.group("pe", [(lambda kc=kc, b=b, bk=bk: T.matmul(bank[bk][0:1, :], lhsT=cT[:, kc:kc + 1], rhs=wad[b][:, kc, :],
                                                                    start=(kc == 0), stop=(kc == 7))) for kc in range(8)],
                        reads=[t_cT, t_wad[b]], writes=[tb[bk]])
                P.op("dve", lambda pc=pc, bk=bk: V.tensor_tensor(out=modrow[0:1, pc * 512:(pc + 1) * 512], in0=bank[bk][0:1, :],
                                                                 in1=badar[0:1, pc * 512:(pc + 1) * 512], op=ALU.add),
                     reads=[tb[bk], t_bada], writes=[t_modrow])
            cols = [(0, 0), (1, 8), (3, 16), (4, 24)]
            fns = []
            for mi, dc in cols:
                for kc in range(8):
                    fns.append(lambda mi=mi, dc=dc, kc=kc: T.matmul(bank[2][:, dc + kc:dc + kc + 1],
                                                                    lhsT=modrow[0:1, mi * D + kc * 128: mi * D + (kc + 1) * 128],
                                                                    rhs=one11, start=True, stop=True))
            P.group("pe", fns, reads=[t_modrow, t_cst], writes=[tb[2]])
            P.op("dve", lambda: V.tensor_copy(out=S1[:], in_=bank[2][:, 0:8]), reads=[tb[2]], writes=[t_mod])
            P.op("dve", lambda: V.scalar_tensor_tensor(out=A1[:], in0=bank[2][:, 8:16], scalar=1.0, in1=gT[:, 0:8], op0=ALU.add, op1=ALU.mult),
                 reads=[tb[2], t_gT], writes=[t_mod])
            P.op("dve", lambda: V.tensor_copy(out=S2[:], in_=bank[2][:, 16:24]), reads=[tb[2]], writes=[t_mod])
            P.op("dve", lambda: V.scalar_tensor_tensor(out=A2[:], in0=bank[2][:, 24:32], scalar=1.0, in1=gT[:, 8:16], op0=ALU.add, op1=ALU.mult),
                 reads=[tb[2], t_gT], writes=[t_mod])
            P.dma("sp", lambda: nc.sync.dma_start(out=mod_d[:, :], in_=modrow[:]), reads=[t_modrow])
            P.op("dve", lambda: V.tensor_tensor(out=lamp[:, 0:64], in0=lamb[:, 0:64], in1=lamb[:, 64:128], op=ALU.mult), reads=[t_lam], writes=[t_lam])
            P.op("dve", lambda: V.tensor_tensor(out=lamp[:, 64:128], in0=lamb[:, 128:192], in1=lamb[:, 192:256], op=ALU.mult), reads=[t_lam], writes=[t_lam])
            P.op("dve", lambda: V.tensor_reduce(out=small[:, 1:3], in_=lamp[:].rearrange("p (a b) -> p a b", a=2), axis=AX.X, op=ALU.add),
                 reads=[t_lam], writes=[t_small])
            P.op("act", lambda: A.activation(out=small[:, 1:3], in_=small[:, 1:3], func=AF.Exp), reads=[t_small], writes=[t_small])
            P.op("dve", lambda: V.scalar_tensor_tensor(out=small[:, 0:1], in0=small[:, 2:3], scalar=-0.2, in1=small[:, 1:2], op0=ALU.add, op1=ALU.subtract),
                 reads=[t_small], writes=[t_small])
            P.op("act", lambda: A.activation(out=small[:, 8:16], in_=small[:, 16:24], func=AF.Exp), reads=[t_small], writes=[t_small])
            P.barrier()
            P.emit()

        with ExitStack() as s1:
            XB = 8
            xt = [sbuf(s1, "xt%d" % i, [128, D]) for i in range(XB)]; t_xt = trs(XB)
            xn = [sbuf(s1, "xn%d" % i, [128, D], BF16) for i in range(2)]; t_xn = trs(2)
            junk = sbuf(s1, "junk", [128, D], BF16); t_junk = Tr()
            ssq = sbuf(s1, "ssq", [128, 2, 8]); t_ssq = trs(2)
            hTg = [sbuf(s1, "hTg%d" % i, [128, 8, 512], BF16) for i in range(2)]; t_hTg = trs(2)
            hT_v = hT_d.rearrange("k p t -> p k t")
            A1b = sbuf(s1, "A1b", [128, D]); S1b = sbuf(s1, "S1b", [128, D]); gmb = sbuf(s1, "gmb", [128, D]); t_m1 = Tr()
            xm = [sbuf(s1, "xm%d" % i, [128, D]) for i in range(2)]; t_xm = trs(2)
            P.dma("sp", lambda: nc.sync.dma_start(out=S1b[:], in_=mod_d[0:1, 0:D].partition_broadcast(128)), writes=[t_m1])
            P.dma("sp", lambda: nc.sync.dma_start(out=A1b[:], in_=mod_d[0:1, D:2 * D].partition_broadcast(128)), writes=[t_m1])
            P.dma("sp", lambda: nc.sync.dma_start(out=gmb[:], in_=gmix_d[0:1, :].partition_broadcast(128)), writes=[t_m1])
            P.op("dve", lambda: V.scalar_tensor_tensor(out=A1b[:], in0=A1b[:], scalar=1.0, in1=gmb[:], op0=ALU.add, op1=ALU.mult), reads=[t_m1], writes=[t_m1])

            def stageA(g):
                gp = g % 2
                for tt in range(4):
                    t = 4 * g + tt
                    xb = t % XB
                    P.dma("sp", lambda t=t, xb=xb: nc.sync.dma_start(out=xt[xb][:], in_=x_d[t * 128:(t + 1) * 128, :]), writes=[t_xt[xb]])
                    P.op("act", lambda xb=xb, tt=tt: A.activation(out=junk[:], in_=xt[xb][:], func=AF.Square, accum_out=ssq[:, gp, tt:tt + 1]),
                         reads=[t_xt[xb]], writes=[t_junk, t_ssq[gp]])

            def stageA2(g):
                gp = g % 2
                P.op("dve", lambda: V.tensor_scalar(out=ssq[:, gp, 4:8], in0=ssq[:, gp, 0:4], scalar1=1.0 / D, scalar2=EPS, op0=ALU.mult, op1=ALU.add), reads=[t_ssq[gp]], writes=[t_ssq[gp]])
                P.op("act", lambda: A.activation(out=ssq[:, gp, 4:8], in_=ssq[:, gp, 4:8], func=AF.Sqrt), reads=[t_ssq[gp]], writes=[t_ssq[gp]])
                P.op("dve", lambda: V.reciprocal(out=ssq[:, gp, 4:8], in_=ssq[:, gp, 4:8]), reads=[t_ssq[gp]], writes=[t_ssq[gp]])

            def stageB(g):
                gp = g % 2
                hb = g % 2

                def tile_b(tt):
                    t = 4 * g + tt
                    xb = t % XB
                    nb = t % 2
                    P.op("dve", lambda: V.scalar_tensor_tensor(out=xm[nb][:], in0=xt[xb][:], scalar=ssq[:, gp, 4 + tt:5 + tt], in1=A1b[:], op0=ALU.mult, op1=ALU.mult),
                         reads=[t_xt[xb], t_ssq[gp], t_m1], writes=[t_xm[nb]])
                    P.op("dve", lambda: V.tensor_tensor(out=xn[nb][:], in0=xm[nb][:], in1=S1b[:], op=ALU.add), reads=[t_xm[nb], t_m1], writes=[t_xn[nb]])
                    bk = nb
                    pT = bank[bk][:, :].bitcast(BF16)
                    P.group("pe", [(lambda kc=kc: T.transpose(out=pT[:, kc * 128:(kc + 1) * 128], in_=xn[nb][:, kc * 128:(kc + 1) * 128], identity=identb[:]))
                                   for kc in range(8)], reads=[t_xn[nb], t_cst], writes=[tb[bk]])
                    P.op("act", lambda: A.activation(out=hTg[hb][:, :, tt * 128:(tt + 1) * 128], in_=pT[:, :].rearrange("p (a b) -> p a b", a=8), func=AF.Copy),
                         reads=[tb[bk]], writes=[t_hTg[hb]])
                for tt in range(4):
                    tile_b(tt)
                P.dma("sp", lambda: nc.sync.dma_start(out=hT_v[:, :, g * 512:(g + 1) * 512], in_=hTg[hb][:]), reads=[t_hTg[hb]])

            for g in range(NGX + 1):
                if g < NGX:
                    stageA(g)
                if g >= 1:
                    stageB(g - 1)
                if g < NGX:
                    stageA2(g)
            P.barrier()
            P.emit()

        s34 = st.enter_context(ExitStack())
        dest_i = sbuf(s34, "dest_i", [128, 4 * NOWN], I32); t_dest = trs(NOWN)
        gate4 = sbuf(s34, "gate4", [128, 4 * NOWN]); t_gate4 = trs(NOWN)
        maskb = sbuf(s34, "maskb", [128, NOWN, NE], BF16); t_maskb = trs(NOWN)
        cnt_run = sbuf(s34, "cnt_run", [128, NE]); t_cnt = Tr()
        cnt_i = sbuf(s34, "cnt_i", [1, NE], I32); t_cnti = Tr()
        padidx = sbuf(s34, "padidx", [128, NE], I32); t_pad = Tr()
        t_xbuf = Tr()
        iota32 = cst[:, 648:680]
        e2048 = cst[:, 680:712]
        iota_p = cst[:, 712:713]
        sA = ExitStack()
        bufA = sbuf(sA, "bufA", [128, 16 * 1024], BF16)
        mixed = bufA[:].rearrange("p (a b) -> p a b", a=NOWN)
        with ExitStack() as s2:
            Wu = sbuf(s2, "Wu", [128, 8, 1664], BF16); t_Wu = Tr()
            KT = sbuf(s2, "KT", [128, S], BF16); t_KT = Tr()
            Vb = sbuf(s2, "Vb", [128, 64 * 130], BF16); t_V = Tr()
            QT = sbuf(s2, "QT", [128, 4, NOWN * 128], BF16); t_QT = Tr()
            hTg = [sbuf(s2, "hTg2_%d" % i, [128, 8, 512], BF16) for i in range(2)]; t_hTg = trs(2)
            csg = [sbuf(s2, "csg%d" % i, [128, 2, 512]) for i in range(2)]; t_csg = trs(2)
            tm1 = [sbuf(s2, "tm1_%d" % i, [128, 512]) for i in range(2)]; t_tm1 = trs(2)
            tm2 = [sbuf(s2, "tm2_%d" % i, [128, 512]) for i in range(2)]; t_tm2 = trs(2)
            PT = [sbuf(s2, "PT%d" % i, [128, 512], BF16) for i in range(3)]; t_PT = trs(3)
            dmask = sbuf(s2, "dmask_sb", [128, NOWN, 512], BF16); t_dmask = Tr()
            smask = sbuf(s2, "smask_sb", [128, NOWN, 256], BF16); t_smask = Tr()
            bselT = sbuf(s2, "bselT", [128, NCH]); t_bsel = Tr()
            vbias = sbuf(s2, "vbias", [128, 128]); t_vbias = Tr()
            gsub_b = sbuf(s2, "gsub_b", [128, 128]); t_gsub = Tr()
            fin = sbuf(s2, "fin", [128, 8 * 128]); t_fin = Tr()
            fsm = sbuf(s2, "fsm", [128, 32]); t_fsm = Tr()
            junk2 = sbuf(s2, "junk2", [128, 128], BF16)
            hT_v = hT_d.rearrange("k p t -> p k t")
            wsel_v = wsel_d.rearrange("(k p) n -> p k n", p=128)
            for q4 in range(4):
                P.dma("pool", lambda q4=q4: G_.dma_start(out=dmask[:, 4 * q4:4 * q4 + 4, :], in_=dmask_d[:, q4 * 2048:(q4 + 1) * 2048].rearrange("p (a b) -> p a b", a=4)),
                      writes=[t_dmask])
            for q4 in range(2):
                P.dma("pool", lambda q4=q4: G_.dma_start(out=smask[:, 8 * q4:8 * q4 + 8, :], in_=smask_d[:, q4 * 2048:(q4 + 1) * 2048].rearrange("p (a b) -> p a b", a=8)),
                      writes=[t_smask])
            P.dma("sp", lambda: nc.sync.dma_start(out=bselT[:], in_=bselT_d[:, :]), writes=[t_bsel])
            P.dma("sp", lambda: nc.sync.dma_start(out=gsub_b[:], in_=gsub_d[0:1, :].partition_broadcast(128)), writes=[t_gsub])
            P.op("dve", lambda: V.tensor_scalar(out=gsub_b[:], in0=gsub_b[:], scalar1=0.8, scalar2=None, op0=ALU.mult), reads=[t_gsub], writes=[t_gsub])
            gcount = [0]

            def rope_proj(u, wc, wcs, hb, cb, ccol, ncol, dst, t_dst, par):
                bA, bB = bank[2 * par], bank[2 * par + 1]
                ci = (u["base"] + wc) // 128
                cis = (u["base"] + wcs) // 128
                P.group("pe", [(lambda kc=kc: T.matmul(bA[:, 0:ncol], lhsT=Wu[:, kc, wc:wc + 128], rhs=hTg[hb][:, kc, ccol:ccol + ncol], start=(kc == 0), stop=(kc == 7)))
                               for kc in range(8)], reads=[t_Wu, t_hTg[hb]], writes=[tb[2 * par]])
                P.group("pe", [(lambda kc=kc: T.matmul(bB[:, 0:ncol], lhsT=Wu[:, kc, wcs:wcs + 128], rhs=hTg[hb][:, kc, ccol:ccol + ncol], start=(kc == 0), stop=(kc == 7)))
                               for kc in range(8)], reads=[t_Wu, t_hTg[hb]], writes=[tb[2 * par + 1]])
                P.op("dve", lambda: V.scalar_tensor_tensor(out=tm1[par][:, 0:ncol], in0=bA[:, 0:ncol], scalar=bselT[:, ci:ci + 1], in1=csg[cb][:, 0, ccol:ccol + ncol],
                                                           op0=ALU.add, op1=ALU.mult), reads=[tb[2 * par], t_bsel, t_csg[cb]], writes=[t_tm1[par]])
                P.op("dve", lambda: V.scalar_tensor_tensor(out=tm2[par][:, 0:ncol], in0=bB[:, 0:ncol], scalar=bselT[:, cis:cis + 1], in1=csg[cb][:, 1, ccol:ccol + ncol],
                                                           op0=ALU.add, op1=ALU.mult), reads=[tb[2 * par + 1], t_bsel, t_csg[cb]], writes=[t_tm2[par]])
                P.op("dve", lambda: V.tensor_tensor(out=dst, in0=tm1[par][:, 0:ncol], in1=tm2[par][:, 0:ncol], op=ALU.add),
                     reads=[t_tm1[par], t_tm2[par]], writes=[t_dst])

            def load_group(g):
                hb = gcount[0] % 2
                gcount[0] += 1
                P.dma("sp", lambda: nc.sync.dma_start(out=hTg[hb][:], in_=hT_v[:, :, g * 512:(g + 1) * 512]), writes=[t_hTg[hb]])
                P.dma("sp", lambda: nc.sync.dma_start(out=csg[hb][:, 0, :], in_=cos_d[:, g * 512:(g + 1) * 512]), writes=[t_csg[hb]])
                P.dma("sp", lambda: nc.sync.dma_start(out=csg[hb][:, 1, :], in_=sin_d[:, g * 512:(g + 1) * 512]), writes=[t_csg[hb]])
                return hb

            pcount = [0]

            def v_proj(u, hb, vt0, vw, swa):
                bk = 4 + (pcount[0] % 2)
                pcount[0] += 1
                ov = u["o_v"]
                fns = []
                for tt in range(4):
                    for kc in range(8):
                        fns.append(lambda tt=tt, kc=kc: T.matmul(bank[bk][:, tt * 128:(tt + 1) * 128], lhsT=hTg[hb][:, kc, tt * 128:(tt + 1) * 128],
                                                                 rhs=Wu[:, kc, ov:ov + 128], start=(kc == 0), stop=(kc == 7)))
                P.group("pe", fns, reads=[t_Wu, t_hTg[hb]], writes=[tb[bk]])
                src = bank[bk][:, :].rearrange("p (a b) -> p a b", a=4)
                vb_b = vbias[:].unsqueeze(1).to_broadcast([128, 4, 128])
                if not swa:
                    dst = Vb[:, vt0 * 129:(vt0 + 4) * 129].rearrange("p (a b) -> p a b", a=4)[:, :, 0:128]
                    P.op("dve", lambda: V.tensor_tensor(out=dst, in0=src, in1=vb_b, op=ALU.add), reads=[tb[bk], t_vbias], writes=[t_V])
                else:
                    for kv in range(2):
                        dst = Vb[:, vt0 * 130:(vt0 + 4) * 130].rearrange("p (a b) -> p a b", a=4)[:, :, kv * 65:kv * 65 + 64]
                        P.op("dve", lambda dst=dst, kv=kv: V.tensor_tensor(out=dst, in0=src[:, :, kv * 64:(kv + 1) * 64],
                                                                          in1=vbias[:, kv * 64:(kv + 1) * 64].unsqueeze(1).to_broadcast([128, 4, 64]), op=ALU.add),
                             reads=[tb[bk], t_vbias], writes=[t_V])

            for ui, u in enumerate(UNITS):
                swa = (ui == 0)
                nc_u = u["ncols"]
                P.dma("pool", lambda u=u, nc_u=nc_u: G_.dma_start(out=Wu[:, :, 0:nc_u], in_=wsel_v[:, :, u["base"]:u["base"] + nc_u]), writes=[t_Wu])
                P.dma("sp", lambda u=u: nc.sync.dma_start(out=vbias[:], in_=bsel_d[0:1, u["base"] + u["o_v"]:u["base"] + u["o_v"] + 128].partition_broadcast(128)),
                      writes=[t_vbias])
                if swa:
                    vv = Vb[:, 0:32 * 130].rearrange("p (a b) -> p a b", a=32)
                    P.op("pool", lambda vv=vv: G_.memset(vv[:, :, 64:65], 1.0), writes=[t_V])
                    P.op("pool", lambda vv=vv: G_.memset(vv[:, :, 129:130], 1.0), writes=[t_V])
                    kv_groups = [(20 + i, i * 512, 4 * i) for i in range(4)] + [(16 + i, 2048 + i * 512, 16 + 4 * i) for i in range(4)]
                elif ui == 1:
                    vv = Vb[:, 0:64 * 129].rearrange("p (a b) -> p a b", a=64)
                    P.op("pool", lambda vv=vv: G_.memset(vv[:, :, 128:129], 1.0), writes=[t_V])
                    kv_groups = [(g, g * 512, 4 * g) for g in range(NG)]
                else:
                    kv_groups = [(g, g * 512, 4 * g) for g in range(NG)]
                par = 0
                for (g, kcol, vt0) in kv_groups:
                    hb = load_group(g)
                    for kc_ in range(u["nk"]):
                        rope_proj(u, u["o_k"] + kc_ * 128, u["o_ks"] + kc_ * 128, hb, hb, 0, 512, KT[:, kc_ * 4096 + kcol:kc_ * 4096 + kcol + 512], t_KT, par)
                        par ^= 1
                    v_proj(u, hb, vt0, None, swa)
                    if swa and g < 20:
                        for qc in range(4):
                            rope_proj(u, u["o_q"] + qc * 128, u["o_qs"] + qc * 128, hb, hb, 0, 512, QT[:, qc, (g - 16) * 512:(g - 15) * 512], t_QT, par)
                            par ^= 1
                if not swa:
                    for g in range(16, 20):
                        hb = load_group(g)
                        rope_proj(u, u["o_q"], u["o_qs"], hb, hb, 0, 512, QT[:, 0, (g - 16) * 512:(g - 15) * 512], t_QT, par)
                        par ^= 1

                items = []
                if swa:
                    for oi in range(NOWN):
                        for hh in range(8):
                            items.append((oi, hh, 0, True))
                else:
                    for oi in range(NOWN):
                        nkb = 8 * (oi // 2) + (4 if oi % 2 == 0 else 8)
                        for m in range(2):
                            for c in range(nkb // 4):
                                items.append((oi, m, c, c == nkb // 4 - 1))

                def qk(n):
                    oi, a, c, last = items[n]
                    sb_ = n % 3
                    if swa:
                        hh = a; half = hh % 2; qc = hh // 2; kvg = hh // 4
                        ps = slice(half * 64, half * 64 + 64)
                        fns = [lambda: T.matmul(bank[sb_][:, 0:128], lhsT=KT[ps, kvg * 4096 + oi * 128:kvg * 4096 + (oi + 1) * 128], rhs=QT[ps, qc, oi * 128:(oi + 1) * 128], start=True, stop=True),
                               lambda: T.matmul(bank[sb_][:, 128:256], lhsT=KT[ps, kvg * 4096 + 2048 + oi * 128:kvg * 4096 + 2048 + (oi + 1) * 128], rhs=QT[ps, qc, oi * 128:(oi + 1) * 128], start=True, stop=True)]
                        ncol = 256
                        mk = smask[:, oi, :]
                        t_mk = t_smask
                    else:
                        m = a
                        ps = slice(m * 64, m * 64 + 64)
                        fns = [(lambda i=i: T.matmul(bank[sb_][:, i * 128:(i + 1) * 128], lhsT=KT[ps, (4 * c + i) * 128:(4 * c + i + 1) * 128],
                                                     rhs=QT[ps, 0, oi * 128:(oi + 1) * 128], start=True, stop=True)) for i in range(4)]
                        ncol = 512
                        mk = dmask[:, oi, :]
                        t_mk = t_dmask
                    P.group("pe", fns, reads=[t_KT, t_QT], writes=[tb[sb_]])
                    P.op("act", lambda: A.activation(out=PT[sb_][:, 0:ncol], in_=bank[sb_][:, 0:ncol], func=AF.Exp, scale=0.125), reads=[tb[sb_]], writes=[t_PT[sb_]])
                    if last:
                        P.op("pool", lambda: G_.tensor_tensor(out=PT[sb_][:, 0:ncol], in0=PT[sb_][:, 0:ncol], in1=mk, op=ALU.mult), reads=[t_PT[sb_], t_mk], writes=[t_PT[sb_]])

                def pv(n):
                    oi, a, c, last = items[n]
                    sb_ = n % 3
                    if swa:
                        hh = a; kvg = hh // 4
                        ob = 3 + (oi % 2) * 2 + (hh // 4)
                        oc = (hh % 4) * 65
                        fns = [lambda: T.matmul(bank[ob][:, oc:oc + 65], lhsT=PT[sb_][:, 0:128], rhs=Vb[:, oi * 130 + kvg * 65: oi * 130 + kvg * 65 + 65], start=True, stop=False),
                               lambda: T.matmul(bank[ob][:, oc:oc + 65], lhsT=PT[sb_][:, 128:256], rhs=Vb[:, (16 + oi) * 130 + kvg * 65: (16 + oi) * 130 + kvg * 65 + 65], start=False, stop=True)]
                    else:
                        m = a
                        ob = 3 + (oi % 2) * 2 + m
                        fns = [(lambda i=i: T.matmul(bank[ob][:, 0:129], lhsT=PT[sb_][:, i * 128:(i + 1) * 128], rhs=Vb[:, (4 * c + i) * 129:(4 * c + i + 1) * 129],
                                                     start=(c == 0 and i == 0), stop=(last and i == 3))) for i in range(4)]
                    P.group("pe", fns, reads=[t_PT[sb_], t_V], writes=[tb[ob]])
                    if swa and a == 7:
                        for hh in range(8):
                            ob2 = 3 + (oi % 2) * 2 + (hh // 4)
                            oc2 = (hh % 4) * 65
                            P.op("dve", lambda hh=hh, ob2=ob2, oc2=oc2: V.tensor_tensor(out=fsm[:, hh:hh + 1], in0=bank[ob2][:, oc2 + 64:oc2 + 65], in1=expsink[:, hh:hh + 1], op=ALU.add),
                                 reads=[tb[ob2], t_small], writes=[t_fsm])
                        P.op("dve", lambda: V.reciprocal(out=fsm[:, 0:8], in_=fsm[:, 0:8]), reads=[t_fsm], writes=[t_fsm])
                        for hh in range(8):
                            ob2 = 3 + (oi % 2) * 2 + (hh // 4)
                            oc2 = (hh % 4) * 65
                            P.op("dve", lambda hh=hh, ob2=ob2, oc2=oc2: V.tensor_scalar(out=mixed[:, oi, hh * 64:(hh + 1) * 64], in0=bank[ob2][:, oc2:oc2 + 64],
                                                                                         scalar1=fsm[:, hh:hh + 1], scalar2=None, op0=ALU.mult),
                                 reads=[tb[ob2], t_fsm], writes=[t_mixed[oi]])
                    if (not swa) and a == 1 and last:
                        h = ui - 1
                        o0 = bank[3 + (oi % 2) * 2]
                        o1 = bank[3 + (oi % 2) * 2 + 1]
                        t0, t1 = tb[3 + (oi % 2) * 2], tb[3 + (oi % 2) * 2 + 1]
                        P.op("dve", lambda: V.reciprocal(out=fsm[:, 16:17], in_=o0[:, 128:129]), reads=[t0], writes=[t_fsm])
                        P.op("dve", lambda: V.reciprocal(out=fsm[:, 17:18], in_=o1[:, 128:129]), reads=[t1], writes=[t_fsm])
                        P.op("dve", lambda: V.tensor_tensor(out=fsm[:, 17:18], in0=fsm[:, 17:18], in1=neglam, op=ALU.mult), reads=[t_fsm, t_small], writes=[t_fsm])
                        P.op("dve", lambda: V.tensor_scalar(out=fin[:, 0:128], in0=o1[:, 0:128], scalar1=fsm[:, 17:18], scalar2=None, op0=ALU.mult), reads=[t1, t_fsm], writes=[t_fin])
                        P.op("dve", lambda: V.scalar_tensor_tensor(out=fin[:, 128:256], in0=o0[:, 0:128], scalar=fsm[:, 16:17], in1=fin[:, 0:128], op0=ALU.mult, op1=ALU.add),
                             reads=[t0, t_fsm, t_fin], writes=[t_fin])
                        P.op("act", lambda: A.activation(out=junk2[:], in_=fin[:, 128:256], func=AF.Square, accum_out=fsm[:, 18:19]), reads=[t_fin], writes=[t_fsm])
                        P.op("dve", lambda: V.tensor_scalar(out=fsm[:, 18:19], in0=fsm[:, 18:19], scalar1=1.0 / 128, scalar2=EPS, op0=ALU.mult, op1=ALU.add), reads=[t_fsm], writes=[t_fsm])
                        P.op("act", lambda: A.activation(out=fsm[:, 18:19], in_=fsm[:, 18:19], func=AF.Sqrt), reads=[t_fsm], writes=[t_fsm])
                        P.op("dve", lambda: V.reciprocal(out=fsm[:, 18:19], in_=fsm[:, 18:19]), reads=[t_fsm], writes=[t_fsm])
                        P.op("dve", lambda: V.scalar_tensor_tensor(out=mixed[:, oi, 512 + h * 128:512 + (h + 1) * 128], in0=fin[:, 128:256], scalar=fsm[:, 18:19], in1=gsub_b[:],
                                                                   op0=ALU.mult, op1=ALU.mult), reads=[t_fin, t_fsm, t_gsub], writes=[t_mixed[oi]])

                LAG = 2
                for n in range(len(items) + LAG):
                    if n < len(items):
                        qk(n)
                    if n >= LAG:
                        pv(n - LAG)
            P.barrier()
            P.emit()

        with ExitStack() as s3:
            gt1_b = sbuf(s3, "gt1_b", [128, D])
            A2b = sbuf(s3, "A2b", [128, D]); S2b = sbuf(s3, "S2b", [128, D]); t_m2 = Tr()
            P.dma("sp", lambda: nc.sync.dma_start(out=gt1_b[:], in_=mod_d[0:1, 2 * D:3 * D].partition_broadcast(128)), writes=[t_mod])
            P.dma("sp", lambda: nc.sync.dma_start(out=S2b[:], in_=mod_d[0:1, 3 * D:4 * D].partition_broadcast(128)), writes=[t_m2])
            P.dma("sp", lambda: nc.sync.dma_start(out=A2b[:], in_=mod_d[0:1, 4 * D:5 * D].partition_broadcast(128)), writes=[t_m2])
            wout = sbuf(s3, "wout", [128, 8, D], BF16); t_wout = Tr()
            boutb = sbuf(s3, "boutb", [1, D], BF16)
            wr = sbuf(s3, "wr", [128, 8, NE], BF16); t_wr = Tr()
            brb = sbuf(s3, "brb", [1, NE], BF16)
            gfb = sbuf(s3, "gfb", [128, D]); t_gfb = Tr()
            P.dma("sp", lambda: nc.sync.dma_start(out=gfb[:], in_=gffn_d[0:1, :].partition_broadcast(128)), writes=[t_gfb])
            P.op("dve", lambda: V.scalar_tensor_tensor(out=A2b[:], in0=A2b[:], scalar=1.0, in1=gfb[:], op0=ALU.add, op1=ALU.mult), reads=[t_m2, t_gfb], writes=[t_m2])
            P.op("dve", lambda: V.memset(cnt_run[:], 0.0), writes=[t_cnt])
            mixT = [sbuf(s3, "mixT%d" % i, [128, 8, 128], BF16) for i in range(2)]; t_mixT = trs(2)
            xo = [sbuf(s3, "xo%d" % i, [128, D]) for i in range(2)]; t_xo = trs(2)
            x1t = [sbuf(s3, "x1t%d" % i, [128, D]) for i in range(2)]; t_x1t = trs(2)
            h2f = [sbuf(s3, "h2f%d" % i, [128, D]) for i in range(2)]; t_h2f = trs(2)
            h2tok = [sbuf(s3, "h2tok%d" % i, [128, D], BF16) for i in range(2)]; t_h2tok = trs(2)
            h2Tt = [sbuf(s3, "h2Tt%d" % i, [128, 8, 128], BF16) for i in range(2)]; t_h2Tt = trs(2)
            zrow = sbuf(s3, "zrow", [128, D], BF16); t_zrow = Tr()
            junk3 = sbuf(s3, "junk3", [128, D], BF16); t_junk3 = Tr()
            rs = sbuf(s3, "rs", [128, 64]); t_rs = trs(2)
            lg = sbuf(s3, "lg", [128, 2, 4 * NE]); t_lg = trs(2)
            idx8 = sbuf(s3, "idx8", [128, 2, 8], U32)
            posb = sbuf(s3, "posb", [128, 2, NE]); junkp = sbuf(s3, "junkp", [128, 2, NE])
            wout_v = wout_d.rearrange("(k p) n -> p k n", p=128)
            wr_v = wr_d.rearrange("(k p) n -> p k n", p=128)
            P.dma("pool", lambda: G_.dma_start(out=wout[:], in_=wout_v), writes=[t_wout])
            P.dma("pool", lambda: G_.dma_start(out=boutb[:], in_=bout_d[:, :]), writes=[t_wout])
            P.dma("pool", lambda: G_.dma_start(out=wr[:], in_=wr_v), writes=[t_wr])
            P.dma("pool", lambda: G_.dma_start(out=brb[:], in_=br_d[:, :]), writes=[t_wr])
            P.op("pool", lambda: G_.memset(zrow[:], 0.0), writes=[t_zrow])

            def p3(oi):
                b = oi % 2
                P.dma("sp", lambda: nc.sync.dma_start(out=xo[b][:], in_=x_d[S + oi * 128:S + (oi + 1) * 128, :]), writes=[t_xo[b]])
                pT = bank[b][:, :].bitcast(BF16)
                P.group("pe", [(lambda kc=kc: T.transpose(out=pT[:, kc * 128:(kc + 1) * 128], in_=mixed[:, oi, kc * 128:(kc + 1) * 128], identity=identb[:])) for kc in range(8)],
                        reads=[t_mixed[oi], t_cst], writes=[tb[b]])
                P.op("act", lambda: A.activation(out=mixT[b][:].rearrange("p a b -> p (a b)"), in_=pT[:, :], func=AF.Copy), reads=[tb[b]], writes=[t_mixT[b]])
                for hf in range(2):
                    bk = 2 + 2 * b + hf
                    fns = [(lambda kc=kc, hf=hf, bk=bk: T.matmul(bank[bk][:, :], lhsT=mixT[b][:, kc, :], rhs=wout[:, kc, hf * 512:(hf + 1) * 512], start=(kc == 0), stop=False)) for kc in range(8)]
                    fns.append(lambda hf=hf, bk=bk: T.matmul(bank[bk][:, :], lhsT=onesb[0:1, :], rhs=boutb[0:1, hf * 512:(hf + 1) * 512], start=False, stop=True))
                    P.group("pe", fns, reads=[t_mixT[b], t_wout, t_cst], writes=[tb[bk]])
                    P.op("dve", lambda hf=hf, bk=bk: V.tensor_tensor(out=x1t[b][:, hf * 512:(hf + 1) * 512], in0=bank[bk][:, :], in1=gt1_b[:, hf * 512:(hf + 1) * 512], op=ALU.mult),
                         reads=[tb[bk], t_mod], writes=[t_x1t[b]])
                P.op("dve", lambda: V.tensor_tensor(out=x1t[b][:], in0=x1t[b][:], in1=xo[b][:], op=ALU.add), reads=[t_x1t[b], t_xo[b]], writes=[t_x1t[b]])
                P.dma("sp", lambda: nc.sync.dma_start(out=x1_d[oi * 128:(oi + 1) * 128, :], in_=x1t[b][:]), reads=[t_x1t[b]])
                r0 = 32 * b
                P.op("act", lambda: A.activation(out=junk3[:], in_=x1t[b][:], func=AF.Square, accum_out=rs[:, r0:r0 + 1]), reads=[t_x1t[b]], writes=[t_junk3, t_rs[b]])
                P.op("dve", lambda: V.tensor_scalar(out=rs[:, r0 + 1:r0 + 2], in0=rs[:, r0:r0 + 1], scalar1=1.0 / D, scalar2=EPS, op0=ALU.mult, op1=ALU.add), reads=[t_rs[b]], writes=[t_rs[b]])
                P.op("act", lambda: A.activation(out=rs[:, r0 + 1:r0 + 2], in_=rs[:, r0 + 1:r0 + 2], func=AF.Sqrt), reads=[t_rs[b]], writes=[t_rs[b]])
                P.op("dve", lambda: V.reciprocal(out=rs[:, r0 + 1:r0 + 2], in_=rs[:, r0 + 1:r0 + 2]), reads=[t_rs[b]], writes=[t_rs[b]])
                P.op("dve", lambda: V.scalar_tensor_tensor(out=h2f[b][:], in0=x1t[b][:], scalar=rs[:, r0 + 1:r0 + 2], in1=A2b[:], op0=ALU.mult, op1=ALU.mult),
                     reads=[t_x1t[b], t_rs[b], t_m2], writes=[t_h2f[b]])
                P.op("dve", lambda: V.tensor_tensor(out=h2tok[b][:], in0=h2f[b][:], in1=S2b[:], op=ALU.add), reads=[t_h2f[b], t_m2], writes=[t_h2tok[b]])
                bk = 6 + b
                pT2 = bank[bk][:, :].bitcast(BF16)
                P.group("pe", [(lambda kc=kc: T.transpose(out=pT2[:, kc * 128:(kc + 1) * 128], in_=h2tok[b][:, kc * 128:(kc + 1) * 128], identity=identb[:])) for kc in range(8)],
                        reads=[t_h2tok[b], t_cst], writes=[tb[bk]])
                P.op("act", lambda: A.activation(out=h2Tt[b][:].rearrange("p a b -> p (a b)"), in_=pT2[:, :], func=AF.Copy), reads=[tb[bk]], writes=[t_h2Tt[b]])
                fns = [(lambda kc=kc: T.matmul(bank[b][:, 0:NE], lhsT=h2Tt[b][:, kc, :], rhs=wr[:, kc, :], start=(kc == 0), stop=False)) for kc in range(8)]
                fns.append(lambda: T.matmul(bank[b][:, 0:NE], lhsT=onesb[0:1, :], rhs=brb[0:1, :], start=False, stop=True))
                P.group("pe", fns, reads=[t_h2Tt[b], t_wr, t_cst], writes=[tb[b]])
                L0, L1, L2, L3 = lg[:, b, 0:NE], lg[:, b, NE:NE + 8], lg[:, b, 2 * NE:3 * NE], lg[:, b, 3 * NE:3 * NE + 8]
                P.op("dve", lambda: V.tensor_copy(out=L0, in_=bank[b][:, 0:NE]), reads=[tb[b]], writes=[t_lg[b]])
                P.op("dve", lambda: V.max(out=L1, in_=L0), reads=[t_lg[b]], writes=[t_lg[b]])
                P.op("dve", lambda: V.max_index(out=idx8[:, b, :], in_max=L1, in_values=L0), reads=[t_lg[b]], writes=[t_lg[b]])
                P.op("dve", lambda: V.tensor_scalar(out=maskb[:, oi, :], in0=L0, scalar1=lg[:, b, NE + 3:NE + 4], scalar2=None, op0=ALU.is_ge), reads=[t_lg[b]], writes=[t_maskb[oi]])
                P.op("dve", lambda: V.tensor_scalar(out=rs[:, r0 + 2:r0 + 3], in0=lg[:, b, NE:NE + 1], scalar1=-1.0, scalar2=None, op0=ALU.mult), reads=[t_lg[b]], writes=[t_rs[b]])
                P.op("act", lambda: A.activation(out=L3[:, 0:4], in_=L1[:, 0:4], func=AF.Exp, bias=rs[:, r0 + 2:r0 + 3], scale=1.0, accum_out=rs[:, r0 + 3:r0 + 4]),
                     reads=[t_lg[b], t_rs[b]], writes=[t_lg[b], t_rs[b]])
                P.op("dve", lambda: V.reciprocal(out=rs[:, r0 + 3:r0 + 4], in_=rs[:, r0 + 3:r0 + 4]), reads=[t_rs[b]], writes=[t_rs[b]])
                P.op("dve", lambda: V.tensor_scalar(out=gate4[:, 4 * oi:4 * oi + 4], in0=L3[:, 0:4], scalar1=rs[:, r0 + 3:r0 + 4], scalar2=None, op0=ALU.mult),
                     reads=[t_lg[b], t_rs[b]], writes=[t_gate4[oi]])
                pb = bank[b]
                P.group("pe", [lambda: T.matmul(pb[:, 64:64 + NE], lhsT=trib[:], rhs=maskb[:, oi, :], start=True, stop=True),
                               lambda: T.matmul(pb[:, 128:128 + NE], lhsT=ones128b[:], rhs=maskb[:, oi, :], start=True, stop=True)],
                        reads=[t_maskb[oi], t_cst, t_lg[b]], writes=[tb[b]])
                P.op("dve", lambda: V.tensor_tensor(out=posb[:, b, :], in0=pb[:, 64:64 + NE], in1=cnt_run[:], op=ALU.add), reads=[tb[b], t_cnt], writes=[t_lg[b]])
                P.op("dve", lambda: V.tensor_tensor(out=cnt_run[:], in0=pb[:, 128:128 + NE], in1=cnt_run[:], op=ALU.add), reads=[tb[b], t_cnt, t_lg[b]], writes=[t_cnt])
                EK = rs[:, r0 + 8:r0 + 12]; PK = rs[:, r0 + 12:r0 + 16]; DF = rs[:, r0 + 16:r0 + 20]
                P.op("dve", lambda: V.tensor_copy(out=EK, in_=idx8[:, b, 0:4]), reads=[t_lg[b]], writes=[t_rs[b]])
                for k in range(4):
                    P.op("dve", lambda k=k: V.scalar_tensor_tensor(out=junkp[:, b, :], in0=iota32, scalar=rs[:, r0 + 8 + k:r0 + 9 + k], in1=posb[:, b, :],
                                                                   op0=ALU.is_equal, op1=ALU.mult, accum_out=rs[:, r0 + 12 + k:r0 + 13 + k]),
                         reads=[t_lg[b], t_rs[b], t_cst], writes=[t_rs[b]])
                P.op("dve", lambda: V.scalar_tensor_tensor(out=DF, in0=EK, scalar=float(CAP), in1=PK, op0=ALU.mult, op1=ALU.add), reads=[t_rs[b]], writes=[t_rs[b]])
                P.op("dve", lambda: V.tensor_copy(out=dest_i[:, 4 * oi:4 * oi + 4], in_=DF), reads=[t_rs[b]], writes=[t_dest[oi]])
                for k in range(4):
                    P.dma("pool", lambda k=k: G_.indirect_dma_start(out=xbuf_d[:, :], out_offset=bass.IndirectOffsetOnAxis(ap=dest_i[:, 4 * oi + k:4 * oi + k + 1], axis=0),
                                                                    in_=h2tok[b][:, :], in_offset=None),
                          reads=[t_h2tok[b], t_dest[oi]], writes=[t_xbuf])
            for oi in range(NOWN):
                p3(oi)
            cf = rs[0:1, 0:NE]
            P.op("dve", lambda: V.tensor_scalar(out=cf, in0=cnt_run[0:1, :], scalar1=127.0, scalar2=1.0 / 128, op0=ALU.add, op1=ALU.mult), reads=[t_cnt] + t_rs, writes=t_rs)
            P.op("dve", lambda: V.tensor_scalar(out=cf, in0=cf, scalar1=-0.496, scalar2=None, op0=ALU.add), reads=t_rs, writes=t_rs)
            P.op("dve", lambda: V.tensor_copy(out=cnt_i[:], in_=cf), reads=t_rs, writes=[t_cnti])
            P.op("dve", lambda: V.tensor_scalar(out=posb[:, 0, :], in0=cnt_run[:], scalar1=iota_p, scalar2=None, op0=ALU.add), reads=[t_cnt, t_cst] + t_lg, writes=t_lg)
            P.op("dve", lambda: V.tensor_scalar(out=posb[:, 1, :], in0=posb[:, 0, :], scalar1=float(CAP), scalar2=None, op0=ALU.is_ge), reads=t_lg, writes=t_lg)
            P.op("dve", lambda: V.tensor_tensor(out=posb[:, 0, :], in0=posb[:, 0, :], in1=e2048, op=ALU.add), reads=t_lg + [t_cst], writes=t_lg)
            P.op("dve", lambda: V.tensor_scalar(out=rs[:, 40:41], in0=iota_p, scalar1=float(NE * CAP), scalar2=None, op0=ALU.add), reads=[t_cst] + t_rs, writes=t_rs)
            P.op("dve", lambda: V.tensor_scalar(out=junkp[:, 0, :], in0=posb[:, 0, :], scalar1=-1.0, scalar2=rs[:, 40:41], op0=ALU.mult, op1=ALU.add), reads=t_lg + t_rs, writes=t_lg)
            P.op("dve", lambda: V.tensor_tensor(out=junkp[:, 0, :], in0=junkp[:, 0, :], in1=posb[:, 1, :], op=ALU.mult), reads=t_lg, writes=t_lg)
            P.op("dve", lambda: V.tensor_tensor(out=posb[:, 0, :], in0=posb[:, 0, :], in1=junkp[:, 0, :], op=ALU.add), reads=t_lg, writes=t_lg)
            P.op("dve", lambda: V.tensor_copy(out=padidx[:], in_=posb[:, 0, :]), reads=t_lg, writes=[t_pad])
            for e in range(NE):
                P.dma("pool", lambda e=e: G_.indirect_dma_start(out=xbuf_d[:, :], out_offset=bass.IndirectOffsetOnAxis(ap=padidx[:, e:e + 1], axis=0),
                                                                in_=zrow[:, :], in_offset=None),
                      reads=[t_zrow, t_pad], writes=[t_xbuf])
            P.barrier()
            P.emit()

        sA.close()
        if int(os.environ.get('K_STOP', '9')) <= 3:
            return nc
        with ExitStack() as s4:
            NW = 3
            w1b = [sbuf(s4, "w1b%d" % i, [128, 8, 2 * D], BF16) for i in range(NW)]
            w2b = [sbuf(s4, "w2b%d" % i, [128, 8, D], BF16) for i in range(NW)]
            b1r = [sbuf(s4, "b1r%d" % i, [1, 2 * D], BF16) for i in range(NW)]
            b2r = [sbuf(s4, "b2r%d" % i, [1, D], BF16) for i in range(NW)]
            t_w = trs(NW)
            Xtok = [sbuf(s4, "Xtok%d" % i, [128, D], BF16) for i in range(2)]; t_Xtok = trs(2)
            XT = [sbuf(s4, "XT%d" % i, [128, 8, 128], BF16) for i in range(2)]; t_XT = trs(2)
            gg = [sbuf(s4, "gg%d" % i, [128, 256]) for i in range(2)]; t_gg = trs(2)
            sg = [sbuf(s4, "sg%d" % i, [128, 256]) for i in range(2)]; t_sg = trs(2)
            ll = [sbuf(s4, "ll%d" % i, [128, 256]) for i in range(2)]; t_ll = trs(2)
            atok = [sbuf(s4, "atok%d" % i, [128, D], BF16) for i in range(2)]; t_atok = trs(2)
            aT = [sbuf(s4, "aT%d" % i, [128, 8, 128], BF16) for i in range(2)]; t_aT = trs(2)
            yt = [sbuf(s4, "yt%d" % i, [128, D]) for i in range(2)]; t_yt = trs(2)
            t_ybuf = Tr()
            w1_v = w1_d.rearrange("e (k p) n -> e p k n", p=128)
            w2_v = w2_d.rearrange("e (k p) n -> e p k n", p=128)
            blk = [0]

            def block_body(e, bslot, ws):
                n = blk[0]
                blk[0] += 1
                xb = n % 2
                row0 = e * CAP + bslot * 128
                P.dma("sp", lambda: nc.sync.dma_start(out=Xtok[xb][:], in_=xbuf_d[row0:row0 + 128, :]), reads=[t_xbuf], writes=[t_Xtok[xb]], slot=xb)
                pT = bank[xb][:, :].bitcast(BF16)
                P.group("pe", [(lambda kc=kc: T.transpose(out=pT[:, kc * 128:(kc + 1) * 128], in_=Xtok[xb][:, kc * 128:(kc + 1) * 128], identity=identb[:])) for kc in range(8)],
                        reads=[t_Xtok[xb], t_cst], writes=[tb[xb]])
                P.op("act", lambda: A.activation(out=XT[xb][:].rearrange("p a b -> p (a b)"), in_=pT[:, :], func=AF.Copy), reads=[tb[xb]], writes=[t_XT[xb]])
                def do_cch(cch):
                    bk = 2 + (cch % 2)
                    par = cch % 2
                    fns = [(lambda kc=kc: T.matmul(bank[bk][:, :], lhsT=XT[xb][:, kc, :], rhs=w1b[ws][:, kc, cch * 512:(cch + 1) * 512], start=(kc == 0), stop=False)) for kc in range(8)]
                    fns.append(lambda: T.matmul(bank[bk][:, :], lhsT=onesb[0:1, :], rhs=b1r[ws][0:1, cch * 512:(cch + 1) * 512], start=False, stop=True))
                    P.group("pe", fns, reads=[t_XT[xb], t_w[ws], t_cst], writes=[tb[bk]])
                    P.op("dve", lambda: V.tensor_scalar(out=gg[par][:], in0=bank[bk][:, 0:512:2], scalar1=7.0, scalar2=None, op0=ALU.min), reads=[tb[bk]], writes=[t_gg[par]])
                    P.op("act", lambda: A.activation(out=sg[par][:], in_=gg[par][:], func=AF.Gelu_apprx_sigmoid), reads=[t_gg[par]], writes=[t_sg[par]])
                    P.op("dve", lambda: V.tensor_scalar(out=ll[par][:], in0=bank[bk][:, 1:512:2], scalar1=7.0, scalar2=-7.0, op0=ALU.min, op1=ALU.max), reads=[tb[bk]], writes=[t_ll[par]])
                    P.op("dve", lambda: V.scalar_tensor_tensor(out=atok[xb][:, cch * 256:(cch + 1) * 256], in0=ll[par][:], scalar=1.0, in1=sg[par][:], op0=ALU.add, op1=ALU.mult),
                         reads=[t_ll[par], t_sg[par]], writes=[t_atok[xb]])
                for cch in range(4):
                    do_cch(cch)
                bk = 4 + xb
                pT2 = bank[bk][:, :].bitcast(BF16)
                P.group("pe", [(lambda j=j: T.transpose(out=pT2[:, j * 128:(j + 1) * 128], in_=atok[xb][:, j * 128:(j + 1) * 128], identity=identb[:])) for j in range(8)],
                        reads=[t_atok[xb], t_cst], writes=[tb[bk]])
                P.op("act", lambda: A.activation(out=aT[xb][:].rearrange("p a b -> p (a b)"), in_=pT2[:, :], func=AF.Copy), reads=[tb[bk]], writes=[t_aT[xb]])
                def do_hf(hf):
                    bk2 = 6 + hf
                    fns = [(lambda j=j: T.matmul(bank[bk2][:, :], lhsT=aT[xb][:, j, :], rhs=w2b[ws][:, j, hf * 512:(hf + 1) * 512], start=(j == 0), stop=False)) for j in range(8)]
                    fns.append(lambda: T.matmul(bank[bk2][:, :], lhsT=onesb[0:1, :], rhs=b2r[ws][0:1, hf * 512:(hf + 1) * 512], start=False, stop=True))
                    P.group("pe", fns, reads=[t_aT[xb], t_w[ws], t_cst], writes=[tb[bk2]])
                    if hf == 0:
                        P.op("act", lambda: A.activation(out=yt[xb][:, 0:512], in_=bank[bk2][:, :], func=AF.Copy), reads=[tb[bk2]], writes=[t_yt[xb]])
                    else:
                        P.op("dve", lambda: V.tensor_copy(out=yt[xb][:, 512:1024], in_=bank[bk2][:, :]), reads=[tb[bk2]], writes=[t_yt[xb]])
                for hf in range(2):
                    do_hf(hf)
                P.dma("sp", lambda: nc.sync.dma_start(out=ybuf_d[row0:row0 + 128, :], in_=yt[xb][:]), reads=[t_yt[xb]], writes=[t_ybuf], slot=2 + xb)

            for e in range(int(os.environ.get('K_NE', NE))):
                ws = e % NW
                P.dma("pool", lambda e=e, ws=ws: G_.dma_start(out=w1b[ws][:], in_=w1_v[e]), writes=[t_w[ws]])
                P.dma("pool", lambda e=e, ws=ws: G_.dma_start(out=w2b[ws][:], in_=w2_v[e]), writes=[t_w[ws]])
                P.dma("pool", lambda e=e, ws=ws: G_.dma_start(out=b1r[ws][:], in_=b1_d[e:e + 1, :]), writes=[t_w[ws]])
                P.dma("pool", lambda e=e, ws=ws: G_.dma_start(out=b2r[ws][:], in_=b2_d[e:e + 1, :]), writes=[t_w[ws]])
                P.regload(cnt_i[0:1, e:e + 1], reads=[t_cnti])
                NB = CAP // 128
                for bslot in range(NB):
                    if bslot in (3, 6, 10):
                        P.cond_begin(bslot + 1)
                    P.cond_begin(bslot + 1)
                    block_body(e, bslot, ws)
                    P.cond_end()
                for _ in range(3):
                    P.cond_end()
            P.barrier()
            P.emit()

        if int(os.environ.get('K_STOP', '9')) <= 4:
            return nc
        with ExitStack() as s5:
            gt2_b = sbuf(s5, "gt2_b", [128, D]); gfin_b = sbuf(s5, "gfin_b", [128, D]); t_g5 = Tr()
            yk = [sbuf(s5, "yk%d" % i, [128, D]) for i in range(4)]; t_yk = trs(4)
            acc = [sbuf(s5, "acc%d" % i, [128, D]) for i in range(2)]; t_acc = trs(2)
            x1b = [sbuf(s5, "x1b%d" % i, [128, D]) for i in range(2)]; t_x1b = trs(2)
            ob_ = [sbuf(s5, "ob%d" % i, [128, D]) for i in range(2)]; t_ob = trs(2)
            junk5 = sbuf(s5, "junk5", [128, D], BF16); t_junk5 = Tr()
            fs5 = sbuf(s5, "fs5", [128, 8]); t_fs5 = trs(2)
            P.dma("sp", lambda: nc.sync.dma_start(out=gt2_b[:], in_=mod_d[0:1, 5 * D:6 * D].partition_broadcast(128)), writes=[t_g5])
            P.dma("sp", lambda: nc.sync.dma_start(out=gfin_b[:], in_=gfin_d[0:1, :].partition_broadcast(128)), writes=[t_g5])

            def p5(oi):
                b = oi % 2
                P.dma("sp", lambda: nc.sync.dma_start(out=x1b[b][:], in_=x1_d[oi * 128:(oi + 1) * 128, :]), writes=[t_x1b[b]])
                for k in range(4):
                    P.dma("pool", lambda k=k: G_.indirect_dma_start(out=yk[k][:, :], out_offset=None, in_=ybuf_d[:, :],
                                                                    in_offset=bass.IndirectOffsetOnAxis(ap=dest_i[:, 4 * oi + k:4 * oi + k + 1], axis=0),
                                                                    ), reads=[t_dest[oi]], writes=[t_yk[k]])
                    if k == 0:
                        P.op("dve", lambda: V.tensor_scalar(out=acc[b][:], in0=yk[0][:], scalar1=gate4[:, 4 * oi:4 * oi + 1], scalar2=None, op0=ALU.mult),
                             reads=[t_yk[0], t_gate4[oi]], writes=[t_acc[b]])
                    else:
                        P.op("dve", lambda k=k: V.scalar_tensor_tensor(out=acc[b][:], in0=yk[k][:], scalar=gate4[:, 4 * oi + k:4 * oi + k + 1], in1=acc[b][:], op0=ALU.mult, op1=ALU.add),
                             reads=[t_yk[k], t_gate4[oi], t_acc[b]], writes=[t_acc[b]])
                P.op("dve", lambda: V.tensor_tensor(out=acc[b][:], in0=acc[b][:], in1=gt2_b[:], op=ALU.mult), reads=[t_acc[b], t_g5], writes=[t_acc[b]])
                P.op("dve", lambda: V.tensor_tensor(out=x1b[b][:], in0=acc[b][:], in1=x1b[b][:], op=ALU.add), reads=[t_acc[b], t_x1b[b]], writes=[t_x1b[b]])
                P.op("act", lambda: A.activation(out=junk5[:], in_=x1b[b][:], func=AF.Square, accum_out=fs5[:, 4 * b:4 * b + 1]), reads=[t_x1b[b]], writes=[t_junk5, t_fs5[b]])
                P.op("dve", lambda: V.tensor_scalar(out=fs5[:, 4 * b + 1:4 * b + 2], in0=fs5[:, 4 * b:4 * b + 1], scalar1=1.0 / D, scalar2=EPS, op0=ALU.mult, op1=ALU.add),
                     reads=[t_fs5[b]], writes=[t_fs5[b]])
                P.op("act", lambda: A.activation(out=fs5[:, 4 * b + 1:4 * b + 2], in_=fs5[:, 4 * b + 1:4 * b + 2], func=AF.Sqrt), reads=[t_fs5[b]], writes=[t_fs5[b]])
                P.op("dve", lambda: V.reciprocal(out=fs5[:, 4 * b + 1:4 * b + 2], in_=fs5[:, 4 * b + 1:4 * b + 2]), reads=[t_fs5[b]], writes=[t_fs5[b]])
                P.op("dve", lambda: V.scalar_tensor_tensor(out=ob_[b][:], in0=x1b[b][:], scalar=fs5[:, 4 * b + 1:4 * b + 2], in1=gfin_b[:], op0=ALU.mult, op1=ALU.mult),
                     reads=[t_x1b[b], t_fs5[b], t_g5], writes=[t_ob[b]])
                P.dma("sp", lambda: nc.sync.dma_start(out=out_d[oi * 128:(oi + 1) * 128, :], in_=ob_[b][:]), reads=[t_ob[b]])
            for oi in range(NOWN):
                p5(oi)
            P.barrier()
            P.emit()
    return nc


def _consts():
    c = np.zeros((128, NCST), np.float32)
    c[:, 0:128] = np.eye(128, dtype=np.float32)
    k = np.arange(128)[:, None]
    q = np.arange(128)[None, :]
    c[:, 128:256] = (k <= q)
    c[:, 256:384] = (k > q)
    inv = (1.0 / (np.float32(10000.0) ** (np.arange(0, 64, 2, dtype=np.float32) / np.float32(64)))).astype(np.float32)
    p = np.arange(128)
    c[:, 384] = inv[p % 32]
    c[:, 385] = np.where((p % 64) < 32, -1.0, 1.0)
    c[:, 386] = np.float32(math.pi / 2)
    c[:, 387] = 0.0
    c[:, 388] = 1.0
    c[:, 392:520] = 1.0
    c[:, 520:648] = (k < q)
    c[:, 648:680] = np.arange(32)[None, :]
    c[:, 680:712] = (np.arange(32) * CAP)[None, :]
    c[:, 712] = np.arange(128)
    return c


def _core_masks(j):
    own = own_blocks(j)
    k = np.arange(128)[:, None]
    q = np.arange(128)[None, :]
    tri = (k <= q).astype(np.float32)
    low = (k > q).astype(np.float32)
    dm = np.zeros((128, NOWN, 4, 128), np.float32)
    sm = np.zeros((128, NOWN, 2, 128), np.float32)
    for oi, gb in enumerate(own):
        nkb = 8 * (oi // 2) + (4 if oi % 2 == 0 else 8)
        for i in range(4):
            kb = nkb - 4 + i
            if kb < gb:
                dm[:, oi, i, :] = 1.0
            elif kb == gb:
                dm[:, oi, i, :] = tri
        sm[:, oi, 1, :] = tri
        if gb > 0:
            sm[:, oi, 0, :] = low
    return dm.reshape(128, NOWN * 512), sm.reshape(128, NOWN * 256)


_NC_CACHE = {}


def kernel(x, c, positions, w_ada, b_ada, g_mix, w_in, b_in, attn_sinks, lambda_q1, lambda_k1, lambda_q2, lambda_k2,
           g_subln, w_out, b_out, g_ffn, w_router, b_router, w1, b1, w2, b2, g_final):
    f = lambda a: np.ascontiguousarray(np.asarray(a))
    x = f(x); positions = f(positions)
    if "nc" not in _NC_CACHE:
        _NC_CACHE["nc"] = build_program()
    nc = _NC_CACHE["nc"]
    colT = lambda v: f(np.asarray(v).reshape(-1, 128).T)
    w_sel = f(np.asarray(w_in)[0][:, SEL])
    b_sel = f(np.asarray(b_in)[0][SEL])
    b1_ = np.asarray(b1)[0]
    shared = {
        "w_ada": f(np.asarray(w_ada)[0]), "b_ada": f(np.asarray(b_ada)[0][None, :]),
        "gmixT": colT(np.asarray(g_mix)[0]), "gffnT": colT(np.asarray(g_ffn)[0]),
        "w_sel": w_sel, "b_selT": colT(b_sel), "b_sel": f(b_sel[None, :]),
        "sinks": f(np.asarray(attn_sinks)[0][None, :]),
        "lam4": f(np.stack([np.asarray(lambda_q1)[0], np.asarray(lambda_k1)[0], np.asarray(lambda_q2)[0], np.asarray(lambda_k2)[0]])),
        "g_subln": f(np.asarray(g_subln)[0][None, :]),
        "w_out": f(np.asarray(w_out)[0]), "b_out": f(np.asarray(b_out)[0][None, :]),
        "w_router": f(np.asarray(w_router)[0]), "b_router": f(np.asarray(b_router)[0][None, :]),
        "w1": f(np.asarray(w1)[0]), "w2": f(np.asarray(w2)[0]), "b2": f(np.asarray(b2)[0]),
        "b1": f(b1_), "g_ffn": f(np.asarray(g_ffn)[0][None, :]), "g_mix": f(np.asarray(g_mix)[0][None, :]),
        "g_final": f(np.asarray(g_final)[None, :]),
        "consts": _consts(),
    }
    in_maps = []
    rows_all = []
    for core in range(8):
        b, j = core // 4, core % 4
        own = own_blocks(j)
        rows_own = np.concatenate([np.arange(g * 128, (g + 1) * 128) for g in own])
        rows_prev = np.concatenate([np.arange(max(g - 1, 0) * 128, (max(g - 1, 0) + 1) * 128) for g in own])
        rows_all.append(rows_own)
        xb = x[b]
        x_ext = np.concatenate([xb, xb[rows_own], xb[rows_prev]], axis=0)
        pb = positions[b]
        pos_ext = np.concatenate([pb, pb[rows_own], pb[rows_prev]])[None, :].astype(np.int32)
        dm, sm = _core_masks(j)
        m = dict(shared)
        m.update({"x": f(x_ext), "pos": f(pos_ext), "cT": colT(np.asarray(c)[b]), "dmask": dm, "smask": sm})
        in_maps.append(m)
    res = run_bass_kernel_spmd(nc, in_maps, core_ids=list(range(8)))
    out = np.zeros((2, S, D), np.float32)
    for core in range(8):
        out[core // 4, rows_all[core], :] = np.asarray(res.results[core]["out"])
    return out
```

```python
import math
import os
from contextlib import ExitStack

import numpy as np
import concourse.bass as bass
import concourse.mybir as mybir
from concourse.bass_utils import run_bass_kernel_spmd

F32 = mybir.dt.float32
BF16 = mybir.dt.bfloat16
I32 = mybir.dt.int32
ALU = mybir.AluOpType
AF = mybir.ActivationFunctionType
AX = mybir.AxisListType

D = 1024
S = 8192
NT = 64
NG = 16
NOWN = 16
NE = 32
SX = S + 2 * NOWN * 128
NGX = SX // 512
NCST = 720
CAP = 2048
U32 = mybir.dt.uint32
EPS = 1e-5
C1 = 6.28125
C2 = 2 * math.pi - 6.28125
INV2PI = float(1.0 / (2 * math.pi))

OFF_QA, OFF_KA, OFF_VA, OFF_QD, OFF_KD, OFF_VD = 0, 512, 640, 768, 1280, 1792


def _swap64(cols):
    cols = np.asarray(cols).reshape(-1, 64)
    return np.concatenate([cols[:, 32:], cols[:, :32]], axis=1).reshape(-1)


def _unit_cols():
    units = []
    k = np.concatenate([np.tile(np.arange(OFF_KA + g * 64, OFF_KA + (g + 1) * 64), 2) for g in range(2)])
    q = np.arange(OFF_QA, OFF_QA + 512)
    v = np.arange(OFF_VA, OFF_VA + 128)
    units.append(dict(nk=2, nq=4, k=k, q=q, v=v))
    for h in range(4):
        k = np.arange(OFF_KD + h * 128, OFF_KD + (h + 1) * 128)
        q = np.arange(OFF_QD + h * 128, OFF_QD + (h + 1) * 128)
        v = np.arange(OFF_VD + h * 128, OFF_VD + (h + 1) * 128)
        units.append(dict(nk=1, nq=1, k=k, q=q, v=v))
    off = 0
    sel = []
    for u in units:
        u["base"] = off
        parts = [u["k"], _swap64(u["k"]), u["q"], _swap64(u["q"]), u["v"]]
        u["o_k"] = 0
        u["o_ks"] = len(u["k"])
        u["o_q"] = u["o_ks"] + len(u["k"])
        u["o_qs"] = u["o_q"] + len(u["q"])
        u["o_v"] = u["o_qs"] + len(u["q"])
        u["ncols"] = u["o_v"] + 128
        sel.append(np.concatenate(parts))
        off += u["ncols"]
    return units, np.concatenate(sel)


UNITS, SEL = _unit_cols()
NSEL = len(SEL)
NCH = NSEL // 128


def own_blocks(j):
    return sorted([8 * m + j for m in range(8)] + [8 * m + 7 - j for m in range(8)])


class Tr:
    __slots__ = ("w", "r")

    def __init__(self):
        self.w = {}
        self.r = {}


def trs(n):
    return [Tr() for _ in range(n)]


class Prog:
    ENG = ("pe", "act", "dve", "pool", "sp")

    def __init__(self, nc, stack, n_dma_sems=48):
        self.nc = nc
        self.q = {e: [] for e in self.ENG}
        self.esem = {e: stack.enter_context(nc.semaphore("s_" + e)) for e in self.ENG}
        self.ecnt = {e: 0 for e in self.ENG}
        self.waited = {e: {} for e in self.ENG}
        self.dsem = [stack.enter_context(nc.semaphore("d%d" % i)) for i in range(n_dma_sems)]
        self.dcnt = [0] * n_dma_sems
        self.dpool = {"sp": list(range(0, n_dma_sems - 16)), "pool": list(range(n_dma_sems - 16, n_dma_sems))}
        self.dnext = {"sp": 0, "pool": 0}
        self.in_cond = False
        self.handles = {"pe": nc.tensor, "act": nc.scalar, "dve": nc.vector, "pool": nc.gpsimd, "sp": nc.sync}

    def _need(self, eng, s, v):
        wd = self.waited[eng]
        if wd.get(s, 0) >= v:
            return
        wd[s] = v
        self.q[eng].append(("wait", s, v))

    def _waits(self, eng, reads, writes):
        need = {}
        for t in reads:
            for s, v in t.w.items():
                if need.get(s, 0) < v:
                    need[s] = v
        for t in writes:
            for s, v in t.w.items():
                if need.get(s, 0) < v:
                    need[s] = v
            for s, v in t.r.items():
                if need.get(s, 0) < v:
                    need[s] = v
        for s, v in need.items():
            if eng == "pe" and s is self.esem["pe"]:
                continue
            self._need(eng, s, v)

    def _record(self, ev, reads, writes):
        s, v = ev
        for t in reads:
            if t.r.get(s, 0) < v:
                t.r[s] = v
        for t in writes:
            if self.in_cond:
                if t.w.get(s, 0) < v:
                    t.w[s] = v
            else:
                t.w = {s: v}
                t.r = {}

    def op(self, eng, fn, reads=(), writes=()):
        self._waits(eng, reads, writes)
        self.ecnt[eng] += 1
        ev = (self.esem[eng], self.ecnt[eng])
        self.q[eng].append(("op", fn, self.esem[eng], 1))
        self._record(ev, reads, writes)

    def group(self, eng, fns, reads=(), writes=()):
        self._waits(eng, reads, writes)
        self.ecnt[eng] += 1
        ev = (self.esem[eng], self.ecnt[eng])
        for f in fns[:-1]:
            self.q[eng].append(("op", f, None, 0))
        self.q[eng].append(("op", fns[-1], self.esem[eng], 1))
        self._record(ev, reads, writes)

    def dma(self, eng, fn, reads=(), writes=(), slot=None):
        pl = self.dpool[eng]
        if slot is None:
            i = pl[self.dnext[eng]]
            self.dnext[eng] = (self.dnext[eng] + 1) % (len(pl) - 4)
        else:
            i = pl[len(pl) - 4 + slot]
        s = self.dsem[i]
        if self.dcnt[i]:
            self._need(eng, s, self.dcnt[i])
        self._waits(eng, reads, writes)
        self.dcnt[i] += 16
        ev = (s, self.dcnt[i])
        self.q[eng].append(("op", fn, s, 16))
        self._record(ev, reads, writes)
        return ev

    CENG = ("pe", "act", "dve", "sp")

    def regload(self, ap, reads=()):
        for e in self.CENG:
            self._waits(e, reads, ())
            self.q[e].append(("regload", ap))

    def cond_begin(self, thr):
        if not hasattr(self, "_cstack"):
            self._cstack = []
        self._cstack.append(({e: self.ecnt[e] for e in self.ENG}, list(self.dcnt), {e: dict(self.waited[e]) for e in self.ENG}))
        self.in_cond = True
        for e in self.CENG:
            self.q[e].append(["if", thr, None])

    def cond_end(self):
        ec0, dc0, wd0 = self._cstack.pop()
        assert self.ecnt["pool"] == ec0["pool"], "pool must stay outside conditional regions"
        dd = [(i, self.dcnt[i] - dc0[i]) for i in range(len(self.dcnt)) if self.dcnt[i] != dc0[i]]
        for i, _ in dd:
            assert i in self.dpool["sp"]
        for e in self.CENG:
            comp = []
            if self.ecnt[e] != ec0[e]:
                comp.append((self.esem[e], self.ecnt[e] - ec0[e]))
            if e == "sp":
                comp += [(self.dsem[i], d, dc0[i]) for i, d in dd]
            for it in reversed(self.q[e]):
                if isinstance(it, list) and it[0] == "if" and it[2] is None:
                    it[2] = comp
                    break
            self.q[e].append(("endif",))
            self.waited[e] = wd0[e]
        self.waited["pool"] = wd0["pool"]
        self.in_cond = bool(self._cstack)

    def barrier(self):
        for e in self.ENG:
            for f in self.ENG:
                if f != e and self.ecnt[f]:
                    self._need(e, self.esem[f], self.ecnt[f])
            for i, s in enumerate(self.dsem):
                if self.dcnt[i]:
                    self._need(e, s, self.dcnt[i])

    def emit(self):
        nc = self.nc
        q = self.q
        self.q = {e: [] for e in self.ENG}
        if not hasattr(self, "regs"):
            self.regs = {}
        with nc.Block() as block:
            def run_items(h, ename, items):
                i = 0
                n = len(items)
                while i < n:
                    it = items[i]
                    k = it[0]
                    if k == "wait":
                        h.wait_ge(it[1], it[2])
                    elif k == "op":
                        ins = it[1]()
                        if it[2] is not None:
                            ins.then_inc(it[2], it[3])
                    elif k == "regload":
                        if ename not in self.regs:
                            self.regs[ename] = h.alloc_register("cnt_" + ename)
                        h.reg_load(self.regs[ename], it[1])
                    elif k == "if":
                        depth = 1
                        j = i + 1
                        while True:
                            if items[j][0] == "if":
                                depth += 1
                            elif items[j][0] == "endif":
                                depth -= 1
                                if depth == 0:
                                    break
                            j += 1
                        body = items[i + 1:j]
                        with h.If_lt(self.regs[ename], it[1]):
                            h.drain()
                            for cp in it[2]:
                                if len(cp) == 3 and cp[2]:
                                    h.wait_ge(cp[0], cp[2])
                                h.sem_inc(cp[0], cp[1])
                        with h.Else():
                            run_items(h, ename, body)
                        i = j
                    i += 1

            def run(ename):
                run_items(self.handles[ename], ename, q[ename])

            @block.tensor
            def _(e):
                run("pe")

            @block.scalar
            def _(e):
                run("act")

            @block.vector
            def _(e):
                run("dve")

            @block.gpsimd
            def _(e):
                run("pool")

            @block.sync
            def _(e):
                run("sp")


def build_program(j_core_unused=None, debug=False):
    nc = bass.Bass("TRN2", target_bir_lowering=False)
    din = lambda name, shape, dt=F32: nc.dram_tensor(name, list(shape), dt, kind="ExternalInput").ap()
    x_d = din("x", [SX, D])
    pos_d = din("pos", [1, SX], I32)
    dmask_d = din("dmask", [128, NOWN * 512])
    smask_d = din("smask", [128, NOWN * 256])
    cT_d = din("cT", [128, 8])
    wada_d = din("w_ada", [D, 6 * D])
    bada_d = din("b_ada", [1, 6 * D])
    gmixT_d = din("gmixT", [128, 8])
    gffnT_d = din("gffnT", [128, 8])
    wsel_d = din("w_sel", [D, NSEL])
    bselT_d = din("b_selT", [128, NCH])
    bsel_d = din("b_sel", [1, NSEL])
    sinks_d = din("sinks", [1, 8])
    lam_d = din("lam4", [4, 64])
    gsub_d = din("g_subln", [1, 128])
    wout_d = din("w_out", [D, D])
    bout_d = din("b_out", [1, D])
    wr_d = din("w_router", [D, NE])
    br_d = din("b_router", [1, NE])
    w1_d = din("w1", [NE, D, 2 * D])
    b1_d = din("b1", [NE, 2 * D])
    gffn_d = din("g_ffn", [1, D])
    gmix_d = din("g_mix", [1, D])
    w2_d = din("w2", [NE, D, D])
    b2_d = din("b2", [NE, D])
    gfin_d = din("g_final", [1, D])
    cst_d = din("consts", [128, NCST])
    out_d = nc.dram_tensor("out", [NOWN * 128, D], F32, kind="ExternalOutput").ap()
    hT_d = nc.dram_tensor("hT_scr", [8, 128, SX], BF16, kind="Internal").ap()
    cos_d = nc.dram_tensor("cos_scr", [128, SX], F32, kind="Internal").ap()
    sin_d = nc.dram_tensor("sin_scr", [128, SX], F32, kind="Internal").ap()
    x1_d = nc.dram_tensor("x1_scr", [NOWN * 128, D], F32, kind="Internal").ap()
    mod_d = nc.dram_tensor("mod_scr", [1, 6 * D], F32, kind="Internal").ap()
    xbuf_d = nc.dram_tensor("xbuf_scr", [NE * CAP + 128, D], BF16, kind="Internal").ap()
    ybuf_d = nc.dram_tensor("ybuf_scr", [NE * CAP, D], F32, kind="Internal").ap()


    with ExitStack() as st:
        P = Prog(nc, st)
        sbuf = lambda stack, name, shape, dt=F32: stack.enter_context(nc.sbuf_tensor(name, list(shape), dt))
        V, A, T, G_ = nc.vector, nc.scalar, nc.tensor, nc.gpsimd

        bank = [st.enter_context(nc.psum_tensor("bank%d" % i, [128, 512], F32)) for i in range(8)]
        tb = trs(8)

        cst = sbuf(st, "cst", [128, NCST]); t_cst = Tr()
        identb = sbuf(st, "identb", [128, 128], BF16)
        mask256 = sbuf(st, "mask256", [128, 256], BF16)
        onesb = sbuf(st, "onesb", [1, 128], BF16)
        trib = sbuf(st, "trib", [128, 128], BF16)
        ones128b = sbuf(st, "ones128b", [128, 128], BF16)
        A1 = sbuf(st, "A1", [128, 8]); S1 = sbuf(st, "S1", [128, 8])
        A2 = sbuf(st, "A2", [128, 8]); S2 = sbuf(st, "S2", [128, 8])
        t_mod = Tr()
        t_mixed = trs(NOWN)
        small = sbuf(st, "small", [128, 64]); t_small = Tr()
        ident = cst[:, 0:128]
        invf = cst[:, 384:385]
        sgn = cst[:, 385:386]
        halfpi = cst[:, 386:387]
        zero_c = cst[:, 387:388]
        one11 = cst[0:1, 388:389]
        ones_row = cst[0:1, 392:520]
        neglam = small[:, 0:1]
        expsink = small[:, 8:16]

        P.dma("sp", lambda: nc.sync.dma_start(out=cst[:], in_=cst_d[:, :]), writes=[t_cst])
        P.op("dve", lambda: V.tensor_copy(out=identb[:], in_=cst[:, 0:128]), reads=[t_cst], writes=[t_cst])
        P.op("dve", lambda: V.tensor_copy(out=mask256[:, 0:128], in_=cst[:, 256:384]), reads=[t_cst], writes=[t_cst])
        P.op("dve", lambda: V.tensor_copy(out=mask256[:, 128:256], in_=cst[:, 128:256]), reads=[t_cst], writes=[t_cst])
        P.op("dve", lambda: V.tensor_copy(out=onesb[:], in_=cst[0:1, 392:520]), reads=[t_cst], writes=[t_cst])
        P.op("dve", lambda: V.tensor_copy(out=trib[:], in_=cst[:, 520:648]), reads=[t_cst], writes=[t_cst])
        P.op("dve", lambda: V.tensor_copy(out=ones128b[:], in_=cst[:, 392:520]), reads=[t_cst], writes=[t_cst])

        with ExitStack() as s0:
            cT = sbuf(s0, "cT_sb", [128, 8]); t_cT = Tr()
            wad = [sbuf(s0, "wad%d" % i, [128, 8, 512]) for i in range(2)]; t_wad = trs(2)
            modrow = sbuf(s0, "modrow", [1, 6 * D]); t_modrow = Tr()
            badar = sbuf(s0, "badar", [1, 6 * D]); t_bada = Tr()
            gT = sbuf(s0, "gT", [128, 16]); t_gT = Tr()
            lamb = sbuf(s0, "lamb", [128, 256]); t_lam = Tr()
            lamp = sbuf(s0, "lamp", [128, 128])
            P.dma("sp", lambda: nc.sync.dma_start(out=cT[:], in_=cT_d[:, :]), writes=[t_cT])
            P.dma("sp", lambda: nc.sync.dma_start(out=badar[:], in_=bada_d[:, :]), writes=[t_bada])
            P.dma("sp", lambda: nc.sync.dma_start(out=gT[:, 0:8], in_=gmixT_d[:, :]), writes=[t_gT])
            P.dma("sp", lambda: nc.sync.dma_start(out=gT[:, 8:16], in_=gffnT_d[:, :]), writes=[t_gT])
            P.dma("sp", lambda: nc.sync.dma_start(out=lamb[:].rearrange("p (a b) -> p a b", a=4),
                                                  in_=lam_d[:, :].partition_broadcast(128)), writes=[t_lam])
            P.dma("sp", lambda: nc.sync.dma_start(out=small[:, 16:24], in_=sinks_d[0:1, :].partition_broadcast(128)), writes=[t_small])
            posi = sbuf(s0, "posi", [128, 512], I32); t_posi = Tr()
            ang = sbuf(s0, "ang", [128, 512]); t_ang = Tr()
            ki = sbuf(s0, "ki", [128, 512], I32); kf = sbuf(s0, "kf", [128, 512]); t_k = Tr()
            rr = sbuf(s0, "rr", [128, 512]); t_rr = Tr()
            tab = [sbuf(s0, "tab%d" % i, [128, 512]) for i in range(4)]; t_tab = trs(4)
            def rope_group(g):
                P.dma("sp", lambda: nc.sync.dma_start(out=posi[:], in_=pos_d[0:1, g * 512:(g + 1) * 512].partition_broadcast(128)), writes=[t_posi])
                P.op("dve", lambda: V.tensor_copy(out=ang[:], in_=posi[:]), reads=[t_posi], writes=[t_ang])
                P.op("dve", lambda: V.tensor_scalar(out=ang[:], in0=ang[:], scalar1=invf, scalar2=None, op0=ALU.mult), reads=[t_ang, t_cst], writes=[t_ang])
                for which in range(2):
                    tbi = (2 * g + which) % 4
                    if which == 0:
                        P.op("dve", lambda: V.tensor_scalar(out=ki[:], in0=ang[:], scalar1=INV2PI, scalar2=None, op0=ALU.mult), reads=[t_ang], writes=[t_k])
                    else:
                        P.op("dve", lambda: V.tensor_scalar(out=ki[:], in0=ang[:], scalar1=INV2PI, scalar2=0.25, op0=ALU.mult, op1=ALU.add), reads=[t_ang], writes=[t_k])
                    P.op("dve", lambda: V.tensor_copy(out=kf[:], in_=ki[:]), reads=[t_k], writes=[t_k])
                    P.op("dve", lambda: V.scalar_tensor_tensor(out=rr[:], in0=kf[:], scalar=-C1, in1=ang[:], op0=ALU.mult, op1=ALU.add), reads=[t_k, t_ang], writes=[t_rr])
                    P.op("dve", lambda: V.scalar_tensor_tensor(out=rr[:], in0=kf[:], scalar=-C2, in1=rr[:], op0=ALU.mult, op1=ALU.add), reads=[t_k, t_rr], writes=[t_rr])
                    if which == 0:
                        P.op("dve", lambda: V.tensor_scalar(out=rr[:], in0=rr[:], scalar1=-3.1415925, scalar2=3.1415925, op0=ALU.max, op1=ALU.min), reads=[t_rr], writes=[t_rr])
                        P.op("act", lambda tbi=tbi: A.activation(out=tab[tbi][:], in_=rr[:], func=AF.Sin, scale=sgn, bias=zero_c), reads=[t_rr, t_cst], writes=[t_tab[tbi]])
                        P.dma("sp", lambda tbi=tbi: nc.sync.dma_start(out=sin_d[:, g * 512:(g + 1) * 512], in_=tab[tbi][:]), reads=[t_tab[tbi]])
                    else:
                        P.op("dve", lambda: V.tensor_scalar(out=rr[:], in0=rr[:], scalar1=-4.712388, scalar2=1.570796, op0=ALU.max, op1=ALU.min), reads=[t_rr], writes=[t_rr])
                        P.op("act", lambda tbi=tbi: A.activation(out=tab[tbi][:], in_=rr[:], func=AF.Sin, scale=1.0, bias=halfpi), reads=[t_rr, t_cst], writes=[t_tab[tbi]])
                        P.dma("sp", lambda tbi=tbi: nc.sync.dma_start(out=cos_d[:, g * 512:(g + 1) * 512], in_=tab[tbi][:]), reads=[t_tab[tbi]])
            for g in range(NGX):
                rope_group(g)

            P.op("act", lambda: A.activation(out=cT[:], in_=cT[:], func=AF.Silu), reads=[t_cT], writes=[t_cT])
            wada_v = wada_d.rearrange("(k p) n -> p k n", p=128)
            for pc in range(12):
                b = pc % 2
                P.dma("sp", lambda pc=pc, b=b: nc.sync.dma_start(out=wad[b][:], in_=wada_v[:, :, pc * 512:(pc + 1) * 512]), writes=[t_wad[b]])
                bk = pc % 2
                P.group("pe", [(lambda kc=kc, b=b, bk=bk: T.matmul(bank[bk][0:1, :], lhsT=cT[:, kc:kc + 1], rhs=wad[b][:, kc, :],
                                                                    start=(kc == 0), stop=(kc == 7))) for kc in range(8)],
                        reads=[t_cT, t_wad[b]], writes=[tb[bk]])
                P.op("dve", lambda pc=pc, bk=bk: V.tensor_tensor(out=modrow[0:1, pc * 512:(pc + 1) * 512], in0=bank[bk][0:1, :],
                                                                 in1=badar[0:1, pc * 512:(pc + 1) * 512], op=ALU.add),
                     reads=[tb[bk], t_bada], writes=[t_modrow])
            cols = [(0, 0), (1, 8), (3, 16), (4, 24)]
            fns = []
            for mi, dc in cols:
                for kc in range(8):
                    fns.append(lambda mi=mi, dc=dc, kc=kc: T.matmul(bank[2][:, dc + kc:dc + kc + 1],
                                                                    lhsT=modrow[0:1, mi * D + kc * 128: mi * D + (kc + 1) * 128],
                                                                    rhs=one11, start=True, stop=True))
            P.group("pe", fns, reads=[t_modrow, t_cst], writes=[tb[2]])
            P.op("dve", lambda: V.tensor_copy(out=S1[:], in_=bank[2][:, 0:8]), reads=[tb[2]], writes=[t_mod])
            P.op("dve", lambda: V.scalar_tensor_tensor(out=A1[:], in0=bank[2][:, 8:16], scalar=1.0, in1=gT[:, 0:8], op0=ALU.add, op1=ALU.mult),
                 reads=[tb[2], t_gT], writes=[t_mod])
            P.op("dve", lambda: V.tensor_copy(out=S2[:], in_=bank[2][:, 16:24]), reads=[tb[2]], writes=[t_mod])
            P.op("dve", lambda: V.scalar_tensor_tensor(out=A2[:], in0=bank[2][:, 24:32], scalar=1.0, in1=gT[:, 8:16], op0=ALU.add, op1=ALU.mult),
                 reads=[tb[2], t_gT], writes=[t_mod])
            P.dma("sp", lambda: nc.sync.dma_start(out=mod_d[:, :], in_=modrow[:]), reads=[t_modrow])
            P.op("dve", lambda: V.tensor_tensor(out=lamp[:, 0:64], in0=lamb[:, 0:64], in1=lamb[:, 64:128], op=ALU.mult), reads=[t_lam], writes=[t_lam])
            P.op("dve", lambda: V.tensor_tensor(out=lamp[:, 64:128], in0=lamb[:, 128:192], in1=lamb[:, 192:256], op=ALU.mult), reads=[t_lam], writes=[t_lam])
            P.op("dve", lambda: V.tensor_reduce(out=small[:, 1:3], in_=lamp[:].rearrange("p (a b) -> p a b", a=2), axis=AX.X, op=ALU.add),
                 reads=[t_lam], writes=[t_small])
            P.op("act", lambda: A.activation(out=small[:, 1:3], in_=small[:, 1:3], func=AF.Exp), reads=[t_small], writes=[t_small])
            P.op("dve", lambda: V.scalar_tensor_tensor(out=small[:, 0:1], in0=small[:, 2:3], scalar=-0.2, in1=small[:, 1:2], op0=ALU.add, op1=ALU.subtract),
                 reads=[t_small], writes=[t_small])
            P.op("act", lambda: A.activation(out=small[:, 8:16], in_=small[:, 16:24], func=AF.Exp), reads=[t_small], writes=[t_small])
            P.barrier()
            P.emit()

        with ExitStack() as s1:
            XB = 8
            xt = [sbuf(s1, "xt%d" % i, [128, D]) for i in range(XB)]; t_xt = trs(XB)
            xn = [sbuf(s1, "xn%d" % i, [128, D], BF16) for i in range(2)]; t_xn = trs(2)
            junk = sbuf(s1, "junk", [128, D], BF16); t_junk = Tr()
            ssq = sbuf(s1, "ssq", [128, 2, 8]); t_ssq = trs(2)
            hTg = [sbuf(s1, "hTg%d" % i, [128, 8, 512], BF16) for i in range(2)]; t_hTg = trs(2)
            hT_v = hT_d.rearrange("k p t -> p k t")
            A1b = sbuf(s1, "A1b", [128, D]); S1b = sbuf(s1, "S1b", [128, D]); gmb = sbuf(s1, "gmb", [128, D]); t_m1 = Tr()
            xm = [sbuf(s1, "xm%d" % i, [128, D]) for i in range(2)]; t_xm = trs(2)
            P.dma("sp", lambda: nc.sync.dma_start(out=S1b[:], in_=mod_d[0:1, 0:D].partition_broadcast(128)), writes=[t_m1])
            P.dma("sp", lambda: nc.sync.dma_start(out=A1b[:], in_=mod_d[0:1, D:2 * D].partition_broadcast(128)), writes=[t_m1])
            P.dma("sp", lambda: nc.sync.dma_start(out=gmb[:], in_=gmix_d[0:1, :].partition_broadcast(128)), writes=[t_m1])
            P.op("dve", lambda: V.scalar_tensor_tensor(out=A1b[:], in0=A1b[:], scalar=1.0, in1=gmb[:], op0=ALU.add, op1=ALU.mult), reads=[t_m1], writes=[t_m1])

            def stageA(g):
                gp = g % 2
                for tt in range(4):
                    t = 4 * g + tt
                    xb = t % XB
                    P.dma("sp", lambda t=t, xb=xb: nc.sync.dma_start(out=xt[xb][:], in_=x_d[t * 128:(t + 1) * 128, :]), writes=[t_xt[xb]])
                    P.op("act", lambda xb=xb, tt=tt: A.activation(out=junk[:], in_=xt[xb][:], func=AF.Square, accum_out=ssq[:, gp, tt:tt + 1]),
                         reads=[t_xt[xb]], writes=[t_junk, t_ssq[gp]])

            def stageA2(g):
                gp = g % 2
                P.op("dve", lambda: V.tensor_scalar(out=ssq[:, gp, 4:8], in0=ssq[:, gp, 0:4], scalar1=1.0 / D, scalar2=EPS, op0=ALU.mult, op1=ALU.add), reads=[t_ssq[gp]], writes=[t_ssq[gp]])
                P.op("act", lambda: A.activation(out=ssq[:, gp, 4:8], in_=ssq[:, gp, 4:8], func=AF.Sqrt), reads=[t_ssq[gp]], writes=[t_ssq[gp]])
                P.op("dve", lambda: V.reciprocal(out=ssq[:, gp, 4:8], in_=ssq[:, gp, 4:8]), reads=[t_ssq[gp]], writes=[t_ssq[gp]])

            def stageB(g):
                gp = g % 2
                hb = g % 2

                def tile_b(tt):
                    t = 4 * g + tt
                    xb = t % XB
                    nb = t % 2
                    P.op("dve", lambda: V.scalar_tensor_tensor(out=xm[nb][:], in0=xt[xb][:], scalar=ssq[:, gp, 4 + tt:5 + tt], in1=A1b[:], op0=ALU.mult, op1=ALU.mult),
                         reads=[t_xt[xb], t_ssq[gp], t_m1], writes=[t_xm[nb]])
                    P.op("dve", lambda: V.tensor_tensor(out=xn[nb][:], in0=xm[nb][:], in1=S1b[:], op=ALU.add), reads=[t_xm[nb], t_m1], writes=[t_xn[nb]])
                    bk = nb
                    pT = bank[bk][:, :].bitcast(BF16)
                    P.group("pe", [(lambda kc=kc: T.transpose(out=pT[:, kc * 128:(kc + 1) * 128], in_=xn[nb][:, kc * 128:(kc + 1) * 128], identity=identb[:]))
                                   for kc in range(8)], reads=[t_xn[nb], t_cst], writes=[tb[bk]])
                    P.op("act", lambda: A.activation(out=hTg[hb][:, :, tt * 128:(tt + 1) * 128], in_=pT[:, :].rearrange("p (a b) -> p a b", a=8), func=AF.Copy),
                         reads=[tb[bk]], writes=[t_hTg[hb]])
                for tt in range(4):
                    tile_b(tt)
                P.dma("sp", lambda: nc.sync.dma_start(out=hT_v[:, :, g * 512:(g + 1) * 512], in_=hTg[hb][:]), reads=[t_hTg[hb]])

            for g in range(NGX + 1):
                if g < NGX:
                    stageA(g)
                if g >= 1:
                    stageB(g - 1)
                if g < NGX:
                    stageA2(g)
            P.barrier()
            P.emit()

        s34 = st.enter_context(ExitStack())
        dest_i = sbuf(s34, "dest_i", [128, 4 * NOWN], I32); t_dest = trs(NOWN)
        gate4 = sbuf(s34, "gate4", [128, 4 * NOWN]); t_gate4 = trs(NOWN)
        maskb = sbuf(s34, "maskb", [128, NOWN, NE], BF16); t_maskb = trs(NOWN)
        cnt_run = sbuf(s34, "cnt_run", [128, NE]); t_cnt = Tr()
        cnt_i = sbuf(s34, "cnt_i", [1, NE], I32); t_cnti = Tr()
        padidx = sbuf(s34, "padidx", [128, NE], I32); t_pad = Tr()
        t_xbuf = Tr()
        iota32 = cst[:, 648:680]
        e2048 = cst[:, 680:712]
        iota_p = cst[:, 712:713]
        sA = ExitStack()
        bufA = sbuf(sA, "bufA", [128, 16 * 1024], BF16)
        mixed = bufA[:].rearrange("p (a b) -> p a b", a=NOWN)
        with ExitStack() as s2:
            Wu = sbuf(s2, "Wu", [128, 8, 1664], BF16); t_Wu = Tr()
            KT = sbuf(s2, "KT", [128, S], BF16); t_KT = Tr()
            Vb = sbuf(s2, "Vb", [128, 64 * 130], BF16); t_V = Tr()
            QT = sbuf(s2, "QT", [128, 4, NOWN * 128], BF16); t_QT = Tr()
            hTg = [sbuf(s2, "hTg2_%d" % i, [128, 8, 512], BF16) for i in range(2)]; t_hTg = trs(2)
            csg = [sbuf(s2, "csg%d" % i, [128, 2, 512]) for i in range(2)]; t_csg = trs(2)
            tm1 = [sbuf(s2, "tm1_%d" % i, [128, 512]) for i in range(2)]; t_tm1 = trs(2)
            tm2 = [sbuf(s2, "tm2_%d" % i, [128, 512]) for i in range(2)]; t_tm2 = trs(2)
            PT = [sbuf(s2, "PT%d" % i, [128, 512], BF16) for i in range(3)]; t_PT = trs(3)
            dmask = sbuf(s2, "dmask_sb", [128, NOWN, 512], BF16); t_dmask = Tr()
            smask = sbuf(s2, "smask_sb", [128, NOWN, 256], BF16); t_smask = Tr()
            bselT = sbuf(s2, "bselT", [128, NCH]); t_bsel = Tr()
            vbias = sbuf(s2, "vbias", [128, 128]); t_vbias = Tr()
            gsub_b = sbuf(s2, "gsub_b", [128, 128]); t_gsub = Tr()
            fin = sbuf(s2, "fin", [128, 8 * 128]); t_fin = Tr()
            fsm = sbuf(s2, "fsm", [128, 32]); t_fsm = Tr()
            junk2 = sbuf(s2, "junk2", [128, 128], BF16)
            hT_v = hT_d.rearrange("k p t -> p k t")
            wsel_v = wsel_d.rearrange("(k p) n -> p k n", p=128)
            for q4 in range(4):
                P.dma("pool", lambda q4=q4: G_.dma_start(out=dmask[:, 4 * q4:4 * q4 + 4, :], in_=dmask_d[:, q4 * 2048:(q4 + 1) * 2048].rearrange("p (a b) -> p a b", a=4)),
                      writes=[t_dmask])
            for q4 in range(2):
                P.dma("pool", lambda q4=q4: G_.dma_start(out=smask[:, 8 * q4:8 * q4 + 8, :], in_=smask_d[:, q4 * 2048:(q4 + 1) * 2048].rearrange("p (a b) -> p a b", a=8)),
                      writes=[t_smask])
            P.dma("sp", lambda: nc.sync.dma_start(out=bselT[:], in_=bselT_d[:, :]), writes=[t_bsel])
            P.dma("sp", lambda: nc.sync.dma_start(out=gsub_b[:], in_=gsub_d[0:1, :].partition_broadcast(128)), writes=[t_gsub])
            P.op("dve", lambda: V.tensor_scalar(out=gsub_b[:], in0=gsub_b[:], scalar1=0.8, scalar2=None, op0=ALU.mult), reads=[t_gsub], writes=[t_gsub])
            gcount = [0]

            def rope_proj(u, wc, wcs, hb, cb, ccol, ncol, dst, t_dst, par):
                bA, bB = bank[2 * par], bank[2 * par + 1]
                ci = (u["base"] + wc) // 128
                cis = (u["base"] + wcs) // 128
                P.group("pe", [(lambda kc=kc: T.matmul(bA[:, 0:ncol], lhsT=Wu[:, kc, wc:wc + 128], rhs=hTg[hb][:, kc, ccol:ccol + ncol], start=(kc == 0), stop=(kc == 7)))
                               for kc in range(8)], reads=[t_Wu, t_hTg[hb]], writes=[tb[2 * par]])
                P.group("pe", [(lambda kc=kc: T.matmul(bB[:, 0:ncol], lhsT=Wu[:, kc, wcs:wcs + 128], rhs=hTg[hb][:, kc, ccol:ccol + ncol], start=(kc == 0), stop=(kc == 7)))
                               for kc in range(8)], reads=[t_Wu, t_hTg[hb]], writes=[tb[2 * par + 1]])
                P.op("dve", lambda: V.scalar_tensor_tensor(out=tm1[par][:, 0:ncol], in0=bA[:, 0:ncol], scalar=bselT[:, ci:ci + 1], in1=csg[cb][:, 0, ccol:ccol + ncol],
                                                           op0=ALU.add, op1=ALU.mult), reads=[tb[2 * par], t_bsel, t_csg[cb]], writes=[t_tm1[par]])
                P.op("dve", lambda: V.scalar_tensor_tensor(out=tm2[par][:, 0:ncol], in0=bB[:, 0:ncol], scalar=bselT[:, cis:cis + 1], in1=csg[cb][:, 1, ccol:ccol + ncol],
                                                           op0=ALU.add, op1=ALU.mult), reads=[tb[2 * par + 1], t_bsel, t_csg[cb]], writes=[t_tm2[par]])
                P.op("dve", lambda: V.tensor_tensor(out=dst, in0=tm1[par][:, 0:ncol], in1=tm2[par][:, 0:ncol], op=ALU.add),
                     reads=[t_tm1[par], t_tm2[par]], writes=[t_dst])

            def load_group(g):
                hb = gcount[0] % 2
                gcount[0] += 1
                P.dma("sp", lambda: nc.sync.dma_start(out=hTg[hb][:], in_=hT_v[:, :, g * 512:(g + 1) * 512]), writes=[t_hTg[hb]])
                P.dma("sp", lambda: nc.sync.dma_start(out=csg[hb][:, 0, :], in_=cos_d[:, g * 512:(g + 1) * 512]), writes=[t_csg[hb]])
                P.dma("sp", lambda: nc.sync.dma_start(out=csg[hb][:, 1, :], in_=sin_d[:, g * 512:(g + 1) * 512]), writes=[t_csg[hb]])
                return hb

            pcount = [0]

            def v_proj(u, hb, vt0, vw, swa):
                bk = 4 + (pcount[0] % 2)
                pcount[0] += 1
                ov = u["o_v"]
                fns = []
                for tt in range(4):
                    for kc in range(8):
                        fns.append(lambda tt=tt, kc=kc: T.matmul(bank[bk][:, tt * 128:(tt + 1) * 128], lhsT=hTg[hb][:, kc, tt * 128:(tt + 1) * 128],
                                                                 rhs=Wu[:, kc, ov:ov + 128], start=(kc == 0), stop=(kc == 7)))
                P.group("pe", fns, reads=[t_Wu, t_hTg[hb]], writes=[tb[bk]])
                src = bank[bk][:, :].rearrange("p (a b) -> p a b", a=4)
                vb_b = vbias[:].unsqueeze(1).to_broadcast([128, 4, 128])
                if not swa:
                    dst = Vb[:, vt0 * 129:(vt0 + 4) * 129].rearrange("p (a b) -> p a b", a=4)[:, :, 0:128]
                    P.op("dve", lambda: V.tensor_tensor(out=dst, in0=src, in1=vb_b, op=ALU.add), reads=[tb[bk], t_vbias], writes=[t_V])
                else:
                    for kv in range(2):
                        dst = Vb[:, vt0 * 130:(vt0 + 4) * 130].rearrange("p (a b) -> p a b", a=4)[:, :, kv * 65:kv * 65 + 64]
                        P.op("dve", lambda dst=dst, kv=kv: V.tensor_tensor(out=dst, in0=src[:, :, kv * 64:(kv + 1) * 64],
                                                                          in1=vbias[:, kv * 64:(kv + 1) * 64].unsqueeze(1).to_broadcast([128, 4, 64]), op=ALU.add),
                             reads=[tb[bk], t_vbias], writes=[t_V])

            for ui, u in enumerate(UNITS):
                swa = (ui == 0)
                nc_u = u["ncols"]
                P.dma("pool", lambda u=u, nc_u=nc_u: G_.dma_start(out=Wu[:, :, 0:nc_u], in_=wsel_v[:, :, u["base"]:u["base"] + nc_u]), writes=[t_Wu])
                P.dma("sp", lambda u=u: nc.sync.dma_start(out=vbias[:], in_=bsel_d[0:1, u["base"] + u["o_v"]:u["base"] + u["o_v"] + 128].partition_broadcast(128)),
                      writes=[t_vbias])
                if swa:
                    vv = Vb[:, 0:32 * 130].rearrange("p (a b) -> p a b", a=32)
                    P.op("pool", lambda vv=vv: G_.memset(vv[:, :, 64:65], 1.0), writes=[t_V])
                    P.op("pool", lambda vv=vv: G_.memset(vv[:, :, 129:130], 1.0), writes=[t_V])
                    kv_groups = [(20 + i, i * 512, 4 * i) for i in range(4)] + [(16 + i, 2048 + i * 512, 16 + 4 * i) for i in range(4)]
                elif ui == 1:
                    vv = Vb[:, 0:64 * 129].rearrange("p (a b) -> p a b", a=64)
                    P.op("pool", lambda vv=vv: G_.memset(vv[:, :, 128:129], 1.0), writes=[t_V])
                    kv_groups = [(g, g * 512, 4 * g) for g in range(NG)]
                else:
                    kv_groups = [(g, g * 512, 4 * g) for g in range(NG)]
                par = 0
                for (g, kcol, vt0) in kv_groups:
                    hb = load_group(g)
                    for kc_ in range(u["nk"]):
                        rope_proj(u, u["o_k"] + kc_ * 128, u["o_ks"] + kc_ * 128, hb, hb, 0, 512, KT[:, kc_ * 4096 + kcol:kc_ * 4096 + kcol + 512], t_KT, par)
                        par ^= 1
                    v_proj(u, hb, vt0, None, swa)
                    if swa and g < 20:
                        for qc in range(4):
                            rope_proj(u, u["o_q"] + qc * 128, u["o_qs"] + qc * 128, hb, hb, 0, 512, QT[:, qc, (g - 16) * 512:(g - 15) * 512], t_QT, par)
                            par ^= 1
                if not swa:
                    for g in range(16, 20):
                        hb = load_group(g)
                        rope_proj(u, u["o_q"], u["o_qs"], hb, hb, 0, 512, QT[:, 0, (g - 16) * 512:(g - 15) * 512], t_QT, par)
                        par ^= 1

                items = []
                if swa:
                    for oi in range(NOWN):
                        for hh in range(8):
                            items.append((oi, hh, 0, True))
                else:
                    for oi in range(NOWN):
                        nkb = 8 * (oi // 2) + (4 if oi % 2 == 0 else 8)
                        for m in range(2):
                            for c in range(nkb // 4):
                                items.append((oi, m, c, c == nkb // 4 - 1))

                def qk(n):
                    oi, a, c, last = items[n]
                    sb_ = n % 3
                    if swa:
                        hh = a; half = hh % 2; qc = hh // 2; kvg = hh // 4
                        ps = slice(half * 64, half * 64 + 64)
                        fns = [lambda: T.matmul(bank[sb_][:, 0:128], lhsT=KT[ps, kvg * 4096 + oi * 128:kvg * 4096 + (oi + 1) * 128], rhs=QT[ps, qc, oi * 128:(oi + 1) * 128], start=True, stop=True),
                               lambda: T.matmul(bank[sb_][:, 128:256], lhsT=KT[ps, kvg * 4096 + 2048 + oi * 128:kvg * 4096 + 2048 + (oi + 1) * 128], rhs=QT[ps, qc, oi * 128:(oi + 1) * 128], start=True, stop=True)]
                        ncol = 256
                        mk = smask[:, oi, :]
                        t_mk = t_smask
                    else:
                        m = a
                        ps = slice(m * 64, m * 64 + 64)
                        fns = [(lambda i=i: T.matmul(bank[sb_][:, i * 128:(i + 1) * 128], lhsT=KT[ps, (4 * c + i) * 128:(4 * c + i + 1) * 128],
                                                     rhs=QT[ps, 0, oi * 128:(oi + 1) * 128], start=True, stop=True)) for i in range(4)]
                        ncol = 512
                        mk = dmask[:, oi, :]
                        t_mk = t_dmask
                    P.group("pe", fns, reads=[t_KT, t_QT], writes=[tb[sb_]])
                    P.op("act", lambda: A.activation(out=PT[sb_][:, 0:ncol], in_=bank[sb_][:, 0:ncol], func=AF.Exp, scale=0.125), reads=[tb[sb_]], writes=[t_PT[sb_]])
                    if last:
                        P.op("pool", lambda: G_.tensor_tensor(out=PT[sb_][:, 0:ncol], in0=PT[sb_][:, 0:ncol], in1=mk, op=ALU.mult), reads=[t_PT[sb_], t_mk], writes=[t_PT[sb_]])

                def pv(n):
                    oi, a, c, last = items[n]
                    sb_ = n % 3
                    if swa:
                        hh = a; kvg = hh // 4
                        ob = 3 + (oi % 2) * 2 + (hh // 4)
                        oc = (hh % 4) * 65
                        fns = [lambda: T.matmul(bank[ob][:, oc:oc + 65], lhsT=PT[sb_][:, 0:128], rhs=Vb[:, oi * 130 + kvg * 65: oi * 130 + kvg * 65 + 65], start=True, stop=False),
                               lambda: T.matmul(bank[ob][:, oc:oc + 65], lhsT=PT[sb_][:, 128:256], rhs=Vb[:, (16 + oi) * 130 + kvg * 65: (16 + oi) * 130 + kvg * 65 + 65], start=False, stop=True)]
                    else:
                        m = a
                        ob = 3 + (oi % 2) * 2 + m
                        fns = [(lambda i=i: T.matmul(bank[ob][:, 0:129], lhsT=PT[sb_][:, i * 128:(i + 1) * 128], rhs=Vb[:, (4 * c + i) * 129:(4 * c + i + 1) * 129],
                                                     start=(c == 0 and i == 0), stop=(last and i == 3))) for i in range(4)]
                    P.group("pe", fns, reads=[t_PT[sb_], t_V], writes=[tb[ob]])
                    if swa and a == 7:
                        for hh in range(8):
                            ob2 = 3 + (oi % 2) * 2 + (hh // 4)
                            oc2 = (hh % 4) * 65
                            P.op("dve", lambda hh=hh, ob2=ob2, oc2=oc2: V.tensor_tensor(out=fsm[:, hh:hh + 1], in0=bank[ob2][:, oc2 + 64:oc2 + 65], in1=expsink[:, hh:hh + 1], op=ALU.add),
                                 reads=[tb[ob2], t_small], writes=[t_fsm])
                        P.op("dve", lambda: V.reciprocal(out=fsm[:, 0:8], in_=fsm[:, 0:8]), reads=[t_fsm], writes=[t_fsm])
                        for hh in range(8):
                            ob2 = 3 + (oi % 2) * 2 + (hh // 4)
                            oc2 = (hh % 4) * 65
                            P.op("dve", lambda hh=hh, ob2=ob2, oc2=oc2: V.tensor_scalar(out=mixed[:, oi, hh * 64:(hh + 1) * 64], in0=bank[ob2][:, oc2:oc2 + 64],
                                                                                         scalar1=fsm[:, hh:hh + 1], scalar2=None, op0=ALU.mult),
                                 reads=[tb[ob2], t_fsm], writes=[t_mixed[oi]])
                    if (not swa) and a == 1 and last:
                        h = ui - 1
                        o0 = bank[3 + (oi % 2) * 2]
                        o1 = bank[3 + (oi % 2) * 2 + 1]
                        t0, t1 = tb[3 + (oi % 2) * 2], tb[3 + (oi % 2) * 2 + 1]
                        P.op("dve", lambda: V.reciprocal(out=fsm[:, 16:17], in_=o0[:, 128:129]), reads=[t0], writes=[t_fsm])
                        P.op("dve", lambda: V.reciprocal(out=fsm[:, 17:18], in_=o1[:, 128:129]), reads=[t1], writes=[t_fsm])
                        P.op("dve", lambda: V.tensor_tensor(out=fsm[:, 17:18], in0=fsm[:, 17:18], in1=neglam, op=ALU.mult), reads=[t_fsm, t_small], writes=[t_fsm])
                        P.op("dve", lambda: V.tensor_scalar(out=fin[:, 0:128], in0=o1[:, 0:128], scalar1=fsm[:, 17:18], scalar2=None, op0=ALU.mult), reads=[t1, t_fsm], writes=[t_fin])
                        P.op("dve", lambda: V.scalar_tensor_tensor(out=fin[:, 128:256], in0=o0[:, 0:128], scalar=fsm[:, 16:17], in1=fin[:, 0:128], op0=ALU.mult, op1=ALU.add),
                             reads=[t0, t_fsm, t_fin], writes=[t_fin])
                        P.op("act", lambda: A.activation(out=junk2[:], in_=fin[:, 128:256], func=AF.Square, accum_out=fsm[:, 18:19]), reads=[t_fin], writes=[t_fsm])
                        P.op("dve", lambda: V.tensor_scalar(out=fsm[:, 18:19], in0=fsm[:, 18:19], scalar1=1.0 / 128, scalar2=EPS, op0=ALU.mult, op1=ALU.add), reads=[t_fsm], writes=[t_fsm])
                        P.op("act", lambda: A.activation(out=fsm[:, 18:19], in_=fsm[:, 18:19], func=AF.Sqrt), reads=[t_fsm], writes=[t_fsm])
                        P.op("dve", lambda: V.reciprocal(out=fsm[:, 18:19], in_=fsm[:, 18:19]), reads=[t_fsm], writes=[t_fsm])
                        P.op("dve", lambda: V.scalar_tensor_tensor(out=mixed[:, oi, 512 + h * 128:512 + (h + 1) * 128], in0=fin[:, 128:256], scalar=fsm[:, 18:19], in1=gsub_b[:],
                                                                   op0=ALU.mult, op1=ALU.mult), reads=[t_fin, t_fsm, t_gsub], writes=[t_mixed[oi]])

                LAG = 2
                for n in range(len(items) + LAG):
                    if n < len(items):
                        qk(n)
                    if n >= LAG:
                        pv(n - LAG)
            P.barrier()
            P.emit()

        with ExitStack() as s3:
            gt1_b = sbuf(s3, "gt1_b", [128, D])
            A2b = sbuf(s3, "A2b", [128, D]); S2b = sbuf(s3, "S2b", [128, D]); t_m2 = Tr()
            P.dma("sp", lambda: nc.sync.dma_start(out=gt1_b[:], in_=mod_d[0:1, 2 * D:3 * D].partition_broadcast(128)), writes=[t_mod])
            P.dma("sp", lambda: nc.sync.dma_start(out=S2b[:], in_=mod_d[0:1, 3 * D:4 * D].partition_broadcast(128)), writes=[t_m2])
            P.dma("sp", lambda: nc.sync.dma_start(out=A2b[:], in_=mod_d[0:1, 4 * D:5 * D].partition_broadcast(128)), writes=[t_m2])
            wout = sbuf(s3, "wout", [128, 8, D], BF16); t_wout = Tr()
            boutb = sbuf(s3, "boutb", [1, D], BF16)
            wr = sbuf(s3, "wr", [128, 8, NE], BF16); t_wr = Tr()
            brb = sbuf(s3, "brb", [1, NE], BF16)
            gfb = sbuf(s3, "gfb", [128, D]); t_gfb = Tr()
            P.dma("sp", lambda: nc.sync.dma_start(out=gfb[:], in_=gffn_d[0:1, :].partition_broadcast(128)), writes=[t_gfb])
            P.op("dve", lambda: V.scalar_tensor_tensor(out=A2b[:], in0=A2b[:], scalar=1.0, in1=gfb[:], op0=ALU.add, op1=ALU.mult), reads=[t_m2, t_gfb], writes=[t_m2])
            P.op("dve", lambda: V.memset(cnt_run[:], 0.0), writes=[t_cnt])
            mixT = [sbuf(s3, "mixT%d" % i, [128, 8, 128], BF16) for i in range(2)]; t_mixT = trs(2)
            xo = [sbuf(s3, "xo%d" % i, [128, D]) for i in range(2)]; t_xo = trs(2)
            x1t = [sbuf(s3, "x1t%d" % i, [128, D]) for i in range(2)]; t_x1t = trs(2)
            h2f = [sbuf(s3, "h2f%d" % i, [128, D]) for i in range(2)]; t_h2f = trs(2)
            h2tok = [sbuf(s3, "h2tok%d" % i, [128, D], BF16) for i in range(2)]; t_h2tok = trs(2)
            h2Tt = [sbuf(s3, "h2Tt%d" % i, [128, 8, 128], BF16) for i in range(2)]; t_h2Tt = trs(2)
            zrow = sbuf(s3, "zrow", [128, D], BF16); t_zrow = Tr()
            junk3 = sbuf(s3, "junk3", [128, D], BF16); t_junk3 = Tr()
            rs = sbuf(s3, "rs", [128, 64]); t_rs = trs(2)
            lg = sbuf(s3, "lg", [128, 2, 4 * NE]); t_lg = trs(2)
            idx8 = sbuf(s3, "idx8", [128, 2, 8], U32)
            posb = sbuf(s3, "posb", [128, 2, NE]); junkp = sbuf(s3, "junkp", [128, 2, NE])
            wout_v = wout_d.rearrange("(k p) n -> p k n", p=128)
            wr_v = wr_d.rearrange("(k p) n -> p k n", p=128)
            P.dma("pool", lambda: G_.dma_start(out=wout[:], in_=wout_v), writes=[t_wout])
            P.dma("pool", lambda: G_.dma_start(out=boutb[:], in_=bout_d[:, :]), writes=[t_wout])
            P.dma("pool", lambda: G_.dma_start(out=wr[:], in_=wr_v), writes=[t_wr])
            P.dma("pool", lambda: G_.dma_start(out=brb[:], in_=br_d[:, :]), writes=[t_wr])
            P.op("pool", lambda: G_.memset(zrow[:], 0.0), writes=[t_zrow])

            def p3(oi):
                b = oi % 2
                P.dma("sp", lambda: nc.sync.dma_start(out=xo[b][:], in_=x_d[S + oi * 128:S + (oi + 1) * 128, :]), writes=[t_xo[b]])
                pT = bank[b][:, :].bitcast(BF16)
                P.group("pe", [(lambda kc=kc: T.transpose(out=pT[:, kc * 128:(kc + 1) * 128], in_=mixed[:, oi, kc * 128:(kc + 1) * 128], identity=identb[:])) for kc in range(8)],
                        reads=[t_mixed[oi], t_cst], writes=[tb[b]])
                P.op("act", lambda: A.activation(out=mixT[b][:].rearrange("p a b -> p (a b)"), in_=pT[:, :], func=AF.Copy), reads=[tb[b]], writes=[t_mixT[b]])
                for hf in range(2):
                    bk = 2 + 2 * b + hf
                    fns = [(lambda kc=kc, hf=hf, bk=bk: T.matmul(bank[bk][:, :], lhsT=mixT[b][:, kc, :], rhs=wout[:, kc, hf * 512:(hf + 1) * 512], start=(kc == 0), stop=False)) for kc in range(8)]
                    fns.append(lambda hf=hf, bk=bk: T.matmul(bank[bk][:, :], lhsT=onesb[0:1, :], rhs=boutb[0:1, hf * 512:(hf + 1) * 512], start=False, stop=True))
                    P.group("pe", fns, reads=[t_mixT[b], t_wout, t_cst], writes=[tb[bk]])
                    P.op("dve", lambda hf=hf, bk=bk: V.tensor_tensor(out=x1t[b][:, hf * 512:(hf + 1) * 512], in0=bank[bk][:, :], in1=gt1_b[:, hf * 512:(hf + 1) * 512], op=ALU.mult),
                         reads=[tb[bk], t_mod], writes=[t_x1t[b]])
                P.op("dve", lambda: V.tensor_tensor(out=x1t[b][:], in0=x1t[b][:], in1=xo[b][:], op=ALU.add), reads=[t_x1t[b], t_xo[b]], writes=[t_x1t[b]])
                P.dma("sp", lambda: nc.sync.dma_start(out=x1_d[oi * 128:(oi + 1) * 128, :], in_=x1t[b][:]), reads=[t_x1t[b]])
                r0 = 32 * b
                P.op("act", lambda: A.activation(out=junk3[:], in_=x1t[b][:], func=AF.Square, accum_out=rs[:, r0:r0 + 1]), reads=[t_x1t[b]], writes=[t_junk3, t_rs[b]])
                P.op("dve", lambda: V.tensor_scalar(out=rs[:, r0 + 1:r0 + 2], in0=rs[:, r0:r0 + 1], scalar1=1.0 / D, scalar2=EPS, op0=ALU.mult, op1=ALU.add), reads=[t_rs[b]], writes=[t_rs[b]])
                P.op("act", lambda: A.activation(out=rs[:, r0 + 1:r0 + 2], in_=rs[:, r0 + 1:r0 + 2], func=AF.Sqrt), reads=[t_rs[b]], writes=[t_rs[b]])
                P.op("dve", lambda: V.reciprocal(out=rs[:, r0 + 1:r0 + 2], in_=rs[:, r0 + 1:r0 + 2]), reads=[t_rs[b]], writes=[t_rs[b]])
                P.op("dve", lambda: V.scalar_tensor_tensor(out=h2f[b][:], in0=x1t[b][:], scalar=rs[:, r0 + 1:r0 + 2], in1=A2b[:], op0=ALU.mult, op1=ALU.mult),
                     reads=[t_x1t[b], t_rs[b], t_m2], writes=[t_h2f[b]])
                P.op("dve", lambda: V.tensor_tensor(out=h2tok[b][:], in0=h2f[b][:], in1=S2b[:], op=ALU.add), reads=[t_h2f[b], t_m2], writes=[t_h2tok[b]])
                bk = 6 + b
                pT2 = bank[bk][:, :].bitcast(BF16)
                P.group("pe", [(lambda kc=kc: T.transpose(out=pT2[:, kc * 128:(kc + 1) * 128], in_=h2tok[b][:, kc * 128:(kc + 1) * 128], identity=identb[:])) for kc in range(8)],
                        reads=[t_h2tok[b], t_cst], writes=[tb[bk]])
                P.op("act", lambda: A.activation(out=h2Tt[b][:].rearrange("p a b -> p (a b)"), in_=pT2[:, :], func=AF.Copy), reads=[tb[bk]], writes=[t_h2Tt[b]])
                fns = [(lambda kc=kc: T.matmul(bank[b][:, 0:NE], lhsT=h2Tt[b][:, kc, :], rhs=wr[:, kc, :], start=(kc == 0), stop=False)) for kc in range(8)]
                fns.append(lambda: T.matmul(bank[b][:, 0:NE], lhsT=onesb[0:1, :], rhs=brb[0:1, :], start=False, stop=True))
                P.group("pe", fns, reads=[t_h2Tt[b], t_wr, t_cst], writes=[tb[b]])
                L0, L1, L2, L3 = lg[:, b, 0:NE], lg[:, b, NE:NE + 8], lg[:, b, 2 * NE:3 * NE], lg[:, b, 3 * NE:3 * NE + 8]
                P.op("dve", lambda: V.tensor_copy(out=L0, in_=bank[b][:, 0:NE]), reads=[tb[b]], writes=[t_lg[b]])
                P.op("dve", lambda: V.max(out=L1, in_=L0), reads=[t_lg[b]], writes=[t_lg[b]])
                P.op("dve", lambda: V.max_index(out=idx8[:, b, :], in_max=L1, in_values=L0), reads=[t_lg[b]], writes=[t_lg[b]])
                P.op("dve", lambda: V.tensor_scalar(out=maskb[:, oi, :], in0=L0, scalar1=lg[:, b, NE + 3:NE + 4], scalar2=None, op0=ALU.is_ge), reads=[t_lg[b]], writes=[t_maskb[oi]])
                P.op("dve", lambda: V.tensor_scalar(out=rs[:, r0 + 2:r0 + 3], in0=lg[:, b, NE:NE + 1], scalar1=-1.0, scalar2=None, op0=ALU.mult), reads=[t_lg[b]], writes=[t_rs[b]])
                P.op("act", lambda: A.activation(out=L3[:, 0:4], in_=L1[:, 0:4], func=AF.Exp, bias=rs[:, r0 + 2:r0 + 3], scale=1.0, accum_out=rs[:, r0 + 3:r0 + 4]),
                     reads=[t_lg[b], t_rs[b]], writes=[t_lg[b], t_rs[b]])
                P.op("dve", lambda: V.reciprocal(out=rs[:, r0 + 3:r0 + 4], in_=rs[:, r0 + 3:r0 + 4]), reads=[t_rs[b]], writes=[t_rs[b]])
                P.op("dve", lambda: V.tensor_scalar(out=gate4[:, 4 * oi:4 * oi + 4], in0=L3[:, 0:4], scalar1=rs[:, r0 + 3:r0 + 4], scalar2=None, op0=ALU.mult),
                     reads=[t_lg[b], t_rs[b]], writes=[t_gate4[oi]])
                pb = bank[b]
                P.group("pe", [lambda: T.matmul(pb[:, 64:64 + NE], lhsT=trib[:], rhs=maskb[:, oi, :], start=True, stop=True),
                               lambda: T.matmul(pb[:, 128:128 + NE], lhsT=ones128b[:], rhs=maskb[:, oi, :], start=True, stop=True)],
                        reads=[t_maskb[oi], t_cst, t_lg[b]], writes=[tb[b]])
                P.op("dve", lambda: V.tensor_tensor(out=posb[:, b, :], in0=pb[:, 64:64 + NE], in1=cnt_run[:], op=ALU.add), reads=[tb[b], t_cnt], writes=[t_lg[b]])
                P.op("dve", lambda: V.tensor_tensor(out=cnt_run[:], in0=pb[:, 128:128 + NE], in1=cnt_run[:], op=ALU.add), reads=[tb[b], t_cnt, t_lg[b]], writes=[t_cnt])
                EK = rs[:, r0 + 8:r0 + 12]; PK = rs[:, r0 + 12:r0 + 16]; DF = rs[:, r0 + 16:r0 + 20]
                P.op("dve", lambda: V.tensor_copy(out=EK, in_=idx8[:, b, 0:4]), reads=[t_lg[b]], writes=[t_rs[b]])
                for k in range(4):
                    P.op("dve", lambda k=k: V.scalar_tensor_tensor(out=junkp[:, b, :], in0=iota32, scalar=rs[:, r0 + 8 + k:r0 + 9 + k], in1=posb[:, b, :],
                                                                   op0=ALU.is_equal, op1=ALU.mult, accum_out=rs[:, r0 + 12 + k:r0 + 13 + k]),
                         reads=[t_lg[b], t_rs[b], t_cst], writes=[t_rs[b]])
                P.op("dve", lambda: V.scalar_tensor_tensor(out=DF, in0=EK, scalar=float(CAP), in1=PK, op0=ALU.mult, op1=ALU.add), reads=[t_rs[b]], writes=[t_rs[b]])
                P.op("dve", lambda: V.tensor_copy(out=dest_i[:, 4 * oi:4 * oi + 4], in_=DF), reads=[t_rs[b]], writes=[t_dest[oi]])
                for k in range(4):
                    P.dma("pool", lambda k=k: G_.indirect_dma_start(out=xbuf_d[:, :], out_offset=bass.IndirectOffsetOnAxis(ap=dest_i[:, 4 * oi + k:4 * oi + k + 1], axis=0),
                                                                    in_=h2tok[b][:, :], in_offset=None),
                          reads=[t_h2tok[b], t_dest[oi]], writes=[t_xbuf])
            for oi in range(NOWN):
                p3(oi)
            cf = rs[0:1, 0:NE]
            P.op("dve", lambda: V.tensor_scalar(out=cf, in0=cnt_run[0:1, :], scalar1=127.0, scalar2=1.0 / 128, op0=ALU.add, op1=ALU.mult), reads=[t_cnt] + t_rs, writes=t_rs)
            P.op("dve", lambda: V.tensor_scalar(out=cf, in0=cf, scalar1=-0.496, scalar2=None, op0=ALU.add), reads=t_rs, writes=t_rs)
            P.op("dve", lambda: V.tensor_copy(out=cnt_i[:], in_=cf), reads=t_rs, writes=[t_cnti])
            P.op("dve", lambda: V.tensor_scalar(out=posb[:, 0, :], in0=cnt_run[:], scalar1=iota_p, scalar2=None, op0=ALU.add), reads=[t_cnt, t_cst] + t_lg, writes=t_lg)
            P.op("dve", lambda: V.tensor_scalar(out=posb[:, 1, :], in0=posb[:, 0, :], scalar1=float(CAP), scalar2=None, op0=ALU.is_ge), reads=t_lg, writes=t_lg)
            P.op("dve", lambda: V.tensor_tensor(out=posb[:, 0, :], in0=posb[:, 0, :], in1=e2048, op=ALU.add), reads=t_lg + [t_cst], writes=t_lg)
            P.op("dve", lambda: V.tensor_scalar(out=rs[:, 40:41], in0=iota_p, scalar1=float(NE * CAP), scalar2=None, op0=ALU.add), reads=[t_cst] + t_rs, writes=t_rs)
            P.op("dve", lambda: V.tensor_scalar(out=junkp[:, 0, :], in0=posb[:, 0, :], scalar1=-1.0, scalar2=rs[:, 40:41], op0=ALU.mult, op1=ALU.add), reads=t_lg + t_rs, writes=t_lg)
            P.op("dve", lambda: V.tensor_tensor(out=junkp[:, 0, :], in0=junkp[:, 0, :], in1=posb[:, 1, :], op=ALU.mult), reads=t_lg, writes=t_lg)
            P.op("dve", lambda: V.tensor_tensor(out=posb[:, 0, :], in0=posb[:, 0, :], in1=junkp[:, 0, :], op=ALU.add), reads=t_lg, writes=t_lg)
            P.op("dve", lambda: V.tensor_copy(out=padidx[:], in_=posb[:, 0, :]), reads=t_lg, writes=[t_pad])
            for e in range(NE):
                P.dma("pool", lambda e=e: G_.indirect_dma_start(out=xbuf_d[:, :], out_offset=bass.IndirectOffsetOnAxis(ap=padidx[:, e:e + 1], axis=0),
                                                                in_=zrow[:, :], in_offset=None),
                      reads=[t_zrow, t_pad], writes=[t_xbuf])
            P.barrier()
            P.emit()

        sA.close()
        if int(os.environ.get('K_STOP', '9')) <= 3:
            return nc
        with ExitStack() as s4:
            NW = 2
            w1b = [sbuf(s4, "w1b%d" % i, [128, 8, 2 * D], BF16) for i in range(NW)]
            w2b = [sbuf(s4, "w2b%d" % i, [128, 8, D], BF16) for i in range(NW)]
            b1r = [sbuf(s4, "b1r%d" % i, [1, 2 * D], BF16) for i in range(NW)]
            b2r = [sbuf(s4, "b2r%d" % i, [1, D], BF16) for i in range(NW)]
            t_w = trs(NW)
            Xtok = [sbuf(s4, "Xtok%d" % i, [128, D], BF16) for i in range(2)]; t_Xtok = trs(2)
            XT = [sbuf(s4, "XT%d" % i, [128, 8, 128], BF16) for i in range(2)]; t_XT = trs(2)
            gg = [sbuf(s4, "gg%d" % i, [128, 256]) for i in range(4)]; t_gg = trs(4)
            sg = [sbuf(s4, "sg%d" % i, [128, 256]) for i in range(4)]; t_sg = trs(4)
            ll = [sbuf(s4, "ll%d" % i, [128, 256]) for i in range(4)]; t_ll = trs(4)
            atok = [sbuf(s4, "atok%d" % i, [128, D], BF16) for i in range(2)]; t_atok = trs(2)
            aT = [sbuf(s4, "aT%d" % i, [128, 8, 128], BF16) for i in range(2)]; t_aT = trs(2)
            yt = [sbuf(s4, "yt%d" % i, [128, D]) for i in range(2)]; t_yt = trs(2)
            t_ybuf = Tr()
            w1_v = w1_d.rearrange("e (k p) n -> e p k n", p=128)
            w2_v = w2_d.rearrange("e (k p) n -> e p k n", p=128)
            blk = [0]

            def prologue(e, bslot, n):
                xb = n % 2
                row0 = e * CAP + bslot * 128
                P.dma("sp", lambda: nc.sync.dma_start(out=Xtok[xb][:], in_=xbuf_d[row0:row0 + 128, :]), reads=[t_xbuf], writes=[t_Xtok[xb]], slot=xb)
                pT = bank[xb][:, :].bitcast(BF16)
                P.group("pe", [(lambda kc=kc: T.transpose(out=pT[:, kc * 128:(kc + 1) * 128], in_=Xtok[xb][:, kc * 128:(kc + 1) * 128], identity=identb[:])) for kc in range(8)],
                        reads=[t_Xtok[xb], t_cst], writes=[tb[xb]])
                P.op("act", lambda: A.activation(out=XT[xb][:].rearrange("p a b -> p (a b)"), in_=pT[:, :], func=AF.Copy), reads=[tb[xb]], writes=[t_XT[xb]])

            def block_body(e, bslot, ws, n, nxt):
                xb = n % 2
                row0 = e * CAP + bslot * 128
                def do_cch(cch):
                    bk = 2 + (cch % 2)
                    par = cch
                    fns = [(lambda kc=kc: T.matmul(bank[bk][:, :], lhsT=XT[xb][:, kc, :], rhs=w1b[ws][:, kc, cch * 512:(cch + 1) * 512], start=(kc == 0), stop=False)) for kc in range(8)]
                    fns.append(lambda: T.matmul(bank[bk][:, :], lhsT=onesb[0:1, :], rhs=b1r[ws][0:1, cch * 512:(cch + 1) * 512], start=False, stop=True))
                    P.group("pe", fns, reads=[t_XT[xb], t_w[ws], t_cst], writes=[tb[bk]])
                    P.op("dve", lambda: V.tensor_scalar(out=gg[par][:], in0=bank[bk][:, 0:512:2], scalar1=7.0, scalar2=None, op0=ALU.min), reads=[tb[bk]], writes=[t_gg[par]])
                    P.op("act", lambda: A.activation(out=sg[par][:], in_=gg[par][:], func=AF.Gelu_apprx_sigmoid), reads=[t_gg[par]], writes=[t_sg[par]])
                    P.op("dve", lambda: V.tensor_scalar(out=ll[par][:], in0=bank[bk][:, 1:512:2], scalar1=7.0, scalar2=-7.0, op0=ALU.min, op1=ALU.max), reads=[tb[bk]], writes=[t_ll[par]])

                def fin_cch(cch):
                    par = cch
                    P.op("dve", lambda: V.scalar_tensor_tensor(out=atok[xb][:, cch * 256:(cch + 1) * 256], in0=ll[par][:], scalar=1.0, in1=sg[par][:], op0=ALU.add, op1=ALU.mult),
                         reads=[t_ll[par], t_sg[par]], writes=[t_atok[xb]])
                for cch in range(4):
                    do_cch(cch)
                    if cch >= 1:
                        fin_cch(cch - 1)
                if nxt is not None:
                    prologue(*nxt)
                fin_cch(3)
                bk = 4 + xb
                pT2 = bank[bk][:, :].bitcast(BF16)
                P.group("pe", [(lambda j=j: T.transpose(out=pT2[:, j * 128:(j + 1) * 128], in_=atok[xb][:, j * 128:(j + 1) * 128], identity=identb[:])) for j in range(8)],
                        reads=[t_atok[xb], t_cst], writes=[tb[bk]])
                P.op("act", lambda: A.activation(out=aT[xb][:].rearrange("p a b -> p (a b)"), in_=pT2[:, :], func=AF.Copy), reads=[tb[bk]], writes=[t_aT[xb]])
                def do_hf(hf):
                    bk2 = 6 + hf
                    fns = [(lambda j=j: T.matmul(bank[bk2][:, :], lhsT=aT[xb][:, j, :], rhs=w2b[ws][:, j, hf * 512:(hf + 1) * 512], start=(j == 0), stop=False)) for j in range(8)]
                    fns.append(lambda: T.matmul(bank[bk2][:, :], lhsT=onesb[0:1, :], rhs=b2r[ws][0:1, hf * 512:(hf + 1) * 512], start=False, stop=True))
                    P.group("pe", fns, reads=[t_aT[xb], t_w[ws], t_cst], writes=[tb[bk2]])
                    if hf == 0:
                        P.op("act", lambda: A.activation(out=yt[xb][:, 0:512], in_=bank[bk2][:, :], func=AF.Copy), reads=[tb[bk2]], writes=[t_yt[xb]])
                    else:
                        P.op("dve", lambda: V.tensor_copy(out=yt[xb][:, 512:1024], in_=bank[bk2][:, :]), reads=[tb[bk2]], writes=[t_yt[xb]])
                for hf in range(2):
                    do_hf(hf)
                P.dma("sp", lambda: nc.sync.dma_start(out=ybuf_d[row0:row0 + 128, :], in_=yt[xb][:]), reads=[t_yt[xb]], writes=[t_ybuf], slot=2 + xb)

            for e in range(int(os.environ.get('K_NE', NE))):
                ws = e % NW
                P.dma("pool", lambda e=e, ws=ws: G_.dma_start(out=w1b[ws][:], in_=w1_v[e]), writes=[t_w[ws]])
                P.dma("pool", lambda e=e, ws=ws: G_.dma_start(out=w2b[ws][:], in_=w2_v[e]), writes=[t_w[ws]])
                P.dma("pool", lambda e=e, ws=ws: G_.dma_start(out=b1r[ws][:], in_=b1_d[e:e + 1, :]), writes=[t_w[ws]])
                P.dma("pool", lambda e=e, ws=ws: G_.dma_start(out=b2r[ws][:], in_=b2_d[e:e + 1, :]), writes=[t_w[ws]])
                P.regload(cnt_i[0:1, e:e + 1], reads=[t_cnti])
                NB = CAP // 128
                n0 = blk[0]
                blk[0] += NB
                prologue(e, 0, n0)
                for bslot in range(NB):
                    if bslot in (3, 6, 10):
                        P.cond_begin(bslot + 1)
                    P.cond_begin(bslot + 1)
                    block_body(e, bslot, ws, n0 + bslot, (e, bslot + 1, n0 + bslot + 1) if bslot + 1 < NB else None)
                    P.cond_end()
                for _ in range(3):
                    P.cond_end()
            P.barrier()
            P.emit()

        if int(os.environ.get('K_STOP', '9')) <= 4:
            return nc
        with ExitStack() as s5:
            gt2_b = sbuf(s5, "gt2_b", [128, D]); gfin_b = sbuf(s5, "gfin_b", [128, D]); t_g5 = Tr()
            yk = [sbuf(s5, "yk%d" % i, [128, D]) for i in range(4)]; t_yk = trs(4)
            acc = [sbuf(s5, "acc%d" % i, [128, D]) for i in range(2)]; t_acc = trs(2)
            x1b = [sbuf(s5, "x1b%d" % i, [128, D]) for i in range(2)]; t_x1b = trs(2)
            ob_ = [sbuf(s5, "ob%d" % i, [128, D]) for i in range(2)]; t_ob = trs(2)
            junk5 = sbuf(s5, "junk5", [128, D], BF16); t_junk5 = Tr()
            fs5 = sbuf(s5, "fs5", [128, 8]); t_fs5 = trs(2)
            P.dma("sp", lambda: nc.sync.dma_start(out=gt2_b[:], in_=mod_d[0:1, 5 * D:6 * D].partition_broadcast(128)), writes=[t_g5])
            P.dma("sp", lambda: nc.sync.dma_start(out=gfin_b[:], in_=gfin_d[0:1, :].partition_broadcast(128)), writes=[t_g5])

            def p5(oi):
                b = oi % 2
                P.dma("sp", lambda: nc.sync.dma_start(out=x1b[b][:], in_=x1_d[oi * 128:(oi + 1) * 128, :]), writes=[t_x1b[b]])
                for k in range(4):
                    P.dma("pool", lambda k=k: G_.indirect_dma_start(out=yk[k][:, :], out_offset=None, in_=ybuf_d[:, :],
                                                                    in_offset=bass.IndirectOffsetOnAxis(ap=dest_i[:, 4 * oi + k:4 * oi + k + 1], axis=0),
                                                                    ), reads=[t_dest[oi]], writes=[t_yk[k]])
                    if k == 0:
                        P.op("dve", lambda: V.tensor_scalar(out=acc[b][:], in0=yk[0][:], scalar1=gate4[:, 4 * oi:4 * oi + 1], scalar2=None, op0=ALU.mult),
                             reads=[t_yk[0], t_gate4[oi]], writes=[t_acc[b]])
                    else:
                        P.op("dve", lambda k=k: V.scalar_tensor_tensor(out=acc[b][:], in0=yk[k][:], scalar=gate4[:, 4 * oi + k:4 * oi + k + 1], in1=acc[b][:], op0=ALU.mult, op1=ALU.add),
                             reads=[t_yk[k], t_gate4[oi], t_acc[b]], writes=[t_acc[b]])
                P.op("dve", lambda: V.tensor_tensor(out=acc[b][:], in0=acc[b][:], in1=gt2_b[:], op=ALU.mult), reads=[t_acc[b], t_g5], writes=[t_acc[b]])
                P.op("dve", lambda: V.tensor_tensor(out=x1b[b][:], in0=acc[b][:], in1=x1b[b][:], op=ALU.add), reads=[t_acc[b], t_x1b[b]], writes=[t_x1b[b]])
                P.op("act", lambda: A.activation(out=junk5[:], in_=x1b[b][:], func=AF.Square, accum_out=fs5[:, 4 * b:4 * b + 1]), reads=[t_x1b[b]], writes=[t_junk5, t_fs5[b]])
                P.op("dve", lambda: V.tensor_scalar(out=fs5[:, 4 * b + 1:4 * b + 2], in0=fs5[:, 4 * b:4 * b + 1], scalar1=1.0 / D, scalar2=EPS, op0=ALU.mult, op1=ALU.add),
                     reads=[t_fs5[b]], writes=[t_fs5[b]])
                P.op("act", lambda: A.activation(out=fs5[:, 4 * b + 1:4 * b + 2], in_=fs5[:, 4 * b + 1:4 * b + 2], func=AF.Sqrt), reads=[t_fs5[b]], writes=[t_fs5[b]])
                P.op("dve", lambda: V.reciprocal(out=fs5[:, 4 * b + 1:4 * b + 2], in_=fs5[:, 4 * b + 1:4 * b + 2]), reads=[t_fs5[b]], writes=[t_fs5[b]])
                P.op("dve", lambda: V.scalar_tensor_tensor(out=ob_[b][:], in0=x1b[b][:], scalar=fs5[:, 4 * b + 1:4 * b + 2], in1=gfin_b[:], op0=ALU.mult, op1=ALU.mult),
                     reads=[t_x1b[b], t_fs5[b], t_g5], writes=[t_ob[b]])
                P.dma("sp", lambda: nc.sync.dma_start(out=out_d[oi * 128:(oi + 1) * 128, :], in_=ob_[b][:]), reads=[t_ob[b]])
            for oi in range(NOWN):
                p5(oi)
            P.barrier()
            P.emit()
    return nc


def _consts():
    c = np.zeros((128, NCST), np.float32)
    c[:, 0:128] = np.eye(128, dtype=np.float32)
    k = np.arange(128)[:, None]
    q = np.arange(128)[None, :]
    c[:, 128:256] = (k <= q)
    c[:, 256:384] = (k > q)
    inv = (1.0 / (np.float32(10000.0) ** (np.arange(0, 64, 2, dtype=np.float32) / np.float32(64)))).astype(np.float32)
    p = np.arange(128)
    c[:, 384] = inv[p % 32]
    c[:, 385] = np.where((p % 64) < 32, -1.0, 1.0)
    c[:, 386] = np.float32(math.pi / 2)
    c[:, 387] = 0.0
    c[:, 388] = 1.0
    c[:, 392:520] = 1.0
    c[:, 520:648] = (k < q)
    c[:, 648:680] = np.arange(32)[None, :]
    c[:, 680:712] = (np.arange(32) * CAP)[None, :]
    c[:, 712] = np.arange(128)
    return c


def _core_masks(j):
    own = own_blocks(j)
    k = np.arange(128)[:, None]
    q = np.arange(128)[None, :]
    tri = (k <= q).astype(np.float32)
    low = (k > q).astype(np.float32)
    dm = np.zeros((128, NOWN, 4, 128), np.float32)
    sm = np.zeros((128, NOWN, 2, 128), np.float32)
    for oi, gb in enumerate(own):
        nkb = 8 * (oi // 2) + (4 if oi % 2 == 0 else 8)
        for i in range(4):
            kb = nkb - 4 + i
            if kb < gb:
                dm[:, oi, i, :] = 1.0
            elif kb == gb:
                dm[:, oi, i, :] = tri
        sm[:, oi, 1, :] = tri
        if gb > 0:
            sm[:, oi, 0, :] = low
    return dm.reshape(128, NOWN * 512), sm.reshape(128, NOWN * 256)


_NC_CACHE = {}


def kernel(x, c, positions, w_ada, b_ada, g_mix, w_in, b_in, attn_sinks, lambda_q1, lambda_k1, lambda_q2, lambda_k2,
           g_subln, w_out, b_out, g_ffn, w_router, b_router, w1, b1, w2, b2, g_final):
    f = lambda a: np.ascontiguousarray(np.asarray(a))
    x = f(x); positions = f(positions)
    if "nc" not in _NC_CACHE:
        _NC_CACHE["nc"] = build_program()
    nc = _NC_CACHE["nc"]
    colT = lambda v: f(np.asarray(v).reshape(-1, 128).T)
    w_sel = f(np.asarray(w_in)[0][:, SEL])
    b_sel = f(np.asarray(b_in)[0][SEL])
    b1_ = np.asarray(b1)[0]
    shared = {
        "w_ada": f(np.asarray(w_ada)[0]), "b_ada": f(np.asarray(b_ada)[0][None, :]),
        "gmixT": colT(np.asarray(g_mix)[0]), "gffnT": colT(np.asarray(g_ffn)[0]),
        "w_sel": w_sel, "b_selT": colT(b_sel), "b_sel": f(b_sel[None, :]),
        "sinks": f(np.asarray(attn_sinks)[0][None, :]),
        "lam4": f(np.stack([np.asarray(lambda_q1)[0], np.asarray(lambda_k1)[0], np.asarray(lambda_q2)[0], np.asarray(lambda_k2)[0]])),
        "g_subln": f(np.asarray(g_subln)[0][None, :]),
        "w_out": f(np.asarray(w_out)[0]), "b_out": f(np.asarray(b_out)[0][None, :]),
        "w_router": f(np.asarray(w_router)[0]), "b_router": f(np.asarray(b_router)[0][None, :]),
        "w1": f(np.asarray(w1)[0]), "w2": f(np.asarray(w2)[0]), "b2": f(np.asarray(b2)[0]),
        "b1": f(b1_), "g_ffn": f(np.asarray(g_ffn)[0][None, :]), "g_mix": f(np.asarray(g_mix)[0][None, :]),
        "g_final": f(np.asarray(g_final)[None, :]),
        "consts": _consts(),
    }
    in_maps = []
    rows_all = []
    for core in range(8):
        b, j = core // 4, core % 4
        own = own_blocks(j)
        rows_own = np.concatenate([np.arange(g * 128, (g + 1) * 128) for g in own])
        rows_prev = np.concatenate([np.arange(max(g - 1, 0) * 128, (max(g - 1, 0) + 1) * 128) for g in own])
        rows_all.append(rows_own)
        xb = x[b]
        x_ext = np.concatenate([xb, xb[rows_own], xb[rows_prev]], axis=0)
        pb = positions[b]
        pos_ext = np.concatenate([pb, pb[rows_own], pb[rows_prev]])[None, :].astype(np.int32)
        dm, sm = _core_masks(j)
        m = dict(shared)
        m.update({"x": f(x_ext), "pos": f(pos_ext), "cT": colT(np.asarray(c)[b]), "dmask": dm, "smask": sm})
        in_maps.append(m)
    res = run_bass_kernel_spmd(nc, in_maps, core_ids=list(range(8)))
    out = np.zeros((2, S, D), np.float32)
    for core in range(8):
        out[core // 4, rows_all[core], :] = np.asarray(res.results[core]["out"])
    return out
```

```python
import math
import os
from contextlib import ExitStack

import numpy as np
import concourse.bass as bass
import concourse.mybir as mybir
from concourse.bass_utils import run_bass_kernel_spmd

F32 = mybir.dt.float32
BF16 = mybir.dt.bfloat16
I32 = mybir.dt.int32
ALU = mybir.AluOpType
AF = mybir.ActivationFunctionType
AX = mybir.AxisListType

D = 1024
S = 8192
NT = 64
NG = 16
NOWN = 16
NE = 32
SX = S + 2 * NOWN * 128
NGX = SX // 512
NCST = 720
CAP = 2048
U32 = mybir.dt.uint32
EPS = 1e-5
C1 = 6.28125
C2 = 2 * math.pi - 6.28125
INV2PI = float(1.0 / (2 * math.pi))

OFF_QA, OFF_KA, OFF_VA, OFF_QD, OFF_KD, OFF_VD = 0, 512, 640, 768, 1280, 1792


def _swap64(cols):
    cols = np.asarray(cols).reshape(-1, 64)
    return np.concatenate([cols[:, 32:], cols[:, :32]], axis=1).reshape(-1)


def _unit_cols():
    units = []
    k = np.concatenate([np.tile(np.arange(OFF_KA + g * 64, OFF_KA + (g + 1) * 64), 2) for g in range(2)])
    q = np.arange(OFF_QA, OFF_QA + 512)
    v = np.arange(OFF_VA, OFF_VA + 128)
    units.append(dict(nk=2, nq=4, k=k, q=q, v=v))
    for h in range(4):
        k = np.arange(OFF_KD + h * 128, OFF_KD + (h + 1) * 128)
        q = np.arange(OFF_QD + h * 128, OFF_QD + (h + 1) * 128)
        v = np.arange(OFF_VD + h * 128, OFF_VD + (h + 1) * 128)
        units.append(dict(nk=1, nq=1, k=k, q=q, v=v))
    off = 0
    sel = []
    for u in units:
        u["base"] = off
        parts = [u["k"], _swap64(u["k"]), u["q"], _swap64(u["q"]), u["v"]]
        u["o_k"] = 0
        u["o_ks"] = len(u["k"])
        u["o_q"] = u["o_ks"] + len(u["k"])
        u["o_qs"] = u["o_q"] + len(u["q"])
        u["o_v"] = u["o_qs"] + len(u["q"])
        u["ncols"] = u["o_v"] + 128
        sel.append(np.concatenate(parts))
        off += u["ncols"]
    return units, np.concatenate(sel)


UNITS, SEL = _unit_cols()
NSEL = len(SEL)
NCH = NSEL // 128


def own_blocks(j):
    return sorted([8 * m + j for m in range(8)] + [8 * m + 7 - j for m in range(8)])


class Tr:
    __slots__ = ("w", "r")

    def __init__(self):
        self.w = {}
        self.r = {}


def trs(n):
    return [Tr() for _ in range(n)]


class Prog:
    ENG = ("pe", "act", "dve", "pool", "sp")

    def __init__(self, nc, stack, n_dma_sems=48):
        self.nc = nc
        self.q = {e: [] for e in self.ENG}
        self.esem = {e: stack.enter_context(nc.semaphore("s_" + e)) for e in self.ENG}
        self.ecnt = {e: 0 for e in self.ENG}
        self.waited = {e: {} for e in self.ENG}
        self.dsem = [stack.enter_context(nc.semaphore("d%d" % i)) for i in range(n_dma_sems)]
        self.dcnt = [0] * n_dma_sems
        self.dpool = {"sp": list(range(0, n_dma_sems - 16)), "pool": list(range(n_dma_sems - 16, n_dma_sems))}
        self.dnext = {"sp": 0, "pool": 0}
        self.in_cond = False
        self.handles = {"pe": nc.tensor, "act": nc.scalar, "dve": nc.vector, "pool": nc.gpsimd, "sp": nc.sync}

    def _need(self, eng, s, v):
        wd = self.waited[eng]
        if wd.get(s, 0) >= v:
            return
        wd[s] = v
        self.q[eng].append(("wait", s, v))

    def _waits(self, eng, reads, writes):
        need = {}
        for t in reads:
            for s, v in t.w.items():
                if need.get(s, 0) < v:
                    need[s] = v
        for t in writes:
            for s, v in t.w.items():
                if need.get(s, 0) < v:
                    need[s] = v
            for s, v in t.r.items():
                if need.get(s, 0) < v:
                    need[s] = v
        for s, v in need.items():
            if eng == "pe" and s is self.esem["pe"]:
                continue
            self._need(eng, s, v)

    def _record(self, ev, reads, writes):
        s, v = ev
        for t in reads:
            if t.r.get(s, 0) < v:
                t.r[s] = v
        for t in writes:
            if self.in_cond:
                if t.w.get(s, 0) < v:
                    t.w[s] = v
            else:
                t.w = {s: v}
                t.r = {}

    def op(self, eng, fn, reads=(), writes=()):
        self._waits(eng, reads, writes)
        self.ecnt[eng] += 1
        ev = (self.esem[eng], self.ecnt[eng])
        self.q[eng].append(("op", fn, self.esem[eng], 1))
        self._record(ev, reads, writes)

    def group(self, eng, fns, reads=(), writes=()):
        self._waits(eng, reads, writes)
        self.ecnt[eng] += 1
        ev = (self.esem[eng], self.ecnt[eng])
        for f in fns[:-1]:
            self.q[eng].append(("op", f, None, 0))
        self.q[eng].append(("op", fns[-1], self.esem[eng], 1))
        self._record(ev, reads, writes)

    def dma(self, eng, fn, reads=(), writes=(), slot=None):
        pl = self.dpool[eng]
        if slot is None:
            i = pl[self.dnext[eng]]
            self.dnext[eng] = (self.dnext[eng] + 1) % (len(pl) - 4)
        else:
            i = pl[len(pl) - 4 + slot]
        s = self.dsem[i]
        if self.dcnt[i]:
            self._need(eng, s, self.dcnt[i])
        self._waits(eng, reads, writes)
        self.dcnt[i] += 16
        ev = (s, self.dcnt[i])
        self.q[eng].append(("op", fn, s, 16))
        self._record(ev, reads, writes)
        return ev

    CENG = ("pe", "act", "dve", "sp")

    def regload(self, ap, reads=()):
        for e in self.CENG:
            self._waits(e, reads, ())
            self.q[e].append(("regload", ap))

    def cond_begin(self, thr):
        if not hasattr(self, "_cstack"):
            self._cstack = []
        self._cstack.append(({e: self.ecnt[e] for e in self.ENG}, list(self.dcnt), {e: dict(self.waited[e]) for e in self.ENG}))
        self.in_cond = True
        for e in self.CENG:
            self.q[e].append(["if", thr, None])

    def cond_end(self):
        ec0, dc0, wd0 = self._cstack.pop()
        assert self.ecnt["pool"] == ec0["pool"], "pool must stay outside conditional regions"
        dd = [(i, self.dcnt[i] - dc0[i]) for i in range(len(self.dcnt)) if self.dcnt[i] != dc0[i]]
        for i, _ in dd:
            assert i in self.dpool["sp"]
        for e in self.CENG:
            comp = []
            if self.ecnt[e] != ec0[e]:
                comp.append((self.esem[e], self.ecnt[e] - ec0[e]))
            if e == "sp":
                comp += [(self.dsem[i], d, dc0[i]) for i, d in dd]
            for it in reversed(self.q[e]):
                if isinstance(it, list) and it[0] == "if" and it[2] is None:
                    it[2] = comp
                    break
            self.q[e].append(("endif",))
            self.waited[e] = wd0[e]
        self.waited["pool"] = wd0["pool"]
        self.in_cond = bool(self._cstack)

    def barrier(self):
        for e in self.ENG:
            for f in self.ENG:
                if f != e and self.ecnt[f]:
                    self._need(e, self.esem[f], self.ecnt[f])
            for i, s in enumerate(self.dsem):
                if self.dcnt[i]:
                    self._need(e, s, self.dcnt[i])

    def emit(self):
        nc = self.nc
        q = self.q
        self.q = {e: [] for e in self.ENG}
        if not hasattr(self, "regs"):
            self.regs = {}
        with nc.Block() as block:
            def run_items(h, ename, items):
                i = 0
                n = len(items)
                while i < n:
                    it = items[i]
                    k = it[0]
                    if k == "wait":
                        h.wait_ge(it[1], it[2])
                    elif k == "op":
                        ins = it[1]()
                        if it[2] is not None:
                            ins.then_inc(it[2], it[3])
                    elif k == "regload":
                        if ename not in self.regs:
                            self.regs[ename] = h.alloc_register("cnt_" + ename)
                        h.reg_load(self.regs[ename], it[1])
                    elif k == "if":
                        depth = 1
                        j = i + 1
                        while True:
                            if items[j][0] == "if":
                                depth += 1
                            elif items[j][0] == "endif":
                                depth -= 1
                                if depth == 0:
                                    break
                            j += 1
                        body = items[i + 1:j]
                        with h.If_lt(self.regs[ename], it[1]):
                            h.drain()
                            for cp in it[2]:
                                if len(cp) == 3 and cp[2]:
                                    h.wait_ge(cp[0], cp[2])
                                h.sem_inc(cp[0], cp[1])
                        with h.Else():
                            run_items(h, ename, body)
                        i = j
                    i += 1

            def run(ename):
                run_items(self.handles[ename], ename, q[ename])

            @block.tensor
            def _(e):
                run("pe")

            @block.scalar
            def _(e):
                run("act")

            @block.vector
            def _(e):
                run("dve")

            @block.gpsimd
            def _(e):
                run("pool")

            @block.sync
            def _(e):
                run("sp")


def build_program(j_core_unused=None, debug=False):
    nc = bass.Bass("TRN2", target_bir_lowering=False)
    din = lambda name, shape, dt=F32: nc.dram_tensor(name, list(shape), dt, kind="ExternalInput").ap()
    x_d = din("x", [SX, D])
    pos_d = din("pos", [1, SX], I32)
    dmask_d = din("dmask", [128, NOWN * 512])
    smask_d = din("smask", [128, NOWN * 256])
    cT_d = din("cT", [128, 8])
    wada_d = din("w_ada", [D, 6 * D])
    bada_d = din("b_ada", [1, 6 * D])
    gmixT_d = din("gmixT", [128, 8])
    gffnT_d = din("gffnT", [128, 8])
    wsel_d = din("w_sel", [D, NSEL])
    bselT_d = din("b_selT", [128, NCH])
    bsel_d = din("b_sel", [1, NSEL])
    sinks_d = din("sinks", [1, 8])
    lam_d = din("lam4", [4, 64])
    gsub_d = din("g_subln", [1, 128])
    wout_d = din("w_out", [D, D])
    bout_d = din("b_out", [1, D])
    wr_d = din("w_router", [D, NE])
    br_d = din("b_router", [1, NE])
    w1_d = din("w1", [NE, D, 2 * D])
    b1_d = din("b1", [NE, 2 * D])
    gffn_d = din("g_ffn", [1, D])
    gmix_d = din("g_mix", [1, D])
    w2_d = din("w2", [NE, D, D])
    b2_d = din("b2", [NE, D])
    gfin_d = din("g_final", [1, D])
    cst_d = din("consts", [128, NCST])
    out_d = nc.dram_tensor("out", [NOWN * 128, D], F32, kind="ExternalOutput").ap()
    hT_d = nc.dram_tensor("hT_scr", [8, 128, SX], BF16, kind="Internal").ap()
    cos_d = nc.dram_tensor("cos_scr", [128, SX], F32, kind="Internal").ap()
    sin_d = nc.dram_tensor("sin_scr", [128, SX], F32, kind="Internal").ap()
    x1_d = nc.dram_tensor("x1_scr", [NOWN * 128, D], F32, kind="Internal").ap()
    mod_d = nc.dram_tensor("mod_scr", [1, 6 * D], F32, kind="Internal").ap()
    xbuf_d = nc.dram_tensor("xbuf_scr", [NE * CAP + 128, D], BF16, kind="Internal").ap()
    ybuf_d = nc.dram_tensor("ybuf_scr", [NE * CAP, D], F32, kind="Internal").ap()


    with ExitStack() as st:
        P = Prog(nc, st)
        sbuf = lambda stack, name, shape, dt=F32: stack.enter_context(nc.sbuf_tensor(name, list(shape), dt))
        V, A, T, G_ = nc.vector, nc.scalar, nc.tensor, nc.gpsimd

        bank = [st.enter_context(nc.psum_tensor("bank%d" % i, [128, 512], F32)) for i in range(8)]
        tb = trs(8)

        cst = sbuf(st, "cst", [128, NCST]); t_cst = Tr()
        identb = sbuf(st, "identb", [128, 128], BF16)
        mask256 = sbuf(st, "mask256", [128, 256], BF16)
        onesb = sbuf(st, "onesb", [1, 128], BF16)
        trib = sbuf(st, "trib", [128, 128], BF16)
        ones128b = sbuf(st, "ones128b", [128, 128], BF16)
        A1 = sbuf(st, "A1", [128, 8]); S1 = sbuf(st, "S1", [128, 8])
        A2 = sbuf(st, "A2", [128, 8]); S2 = sbuf(st, "S2", [128, 8])
        t_mod = Tr()
        t_mixed = trs(NOWN)
        small = sbuf(st, "small", [128, 64]); t_small = Tr()
        ident = cst[:, 0:128]
        invf = cst[:, 384:385]
        sgn = cst[:, 385:386]
        halfpi = cst[:, 386:387]
        zero_c = cst[:, 387:388]
        one11 = cst[0:1, 388:389]
        ones_row = cst[0:1, 392:520]
        neglam = small[:, 0:1]
        expsink = small[:, 8:16]

        P.dma("sp", lambda: nc.sync.dma_start(out=cst[:], in_=cst_d[:, :]), writes=[t_cst])
        P.op("dve", lambda: V.tensor_copy(out=identb[:], in_=cst[:, 0:128]), reads=[t_cst], writes=[t_cst])
        P.op("dve", lambda: V.tensor_copy(out=mask256[:, 0:128], in_=cst[:, 256:384]), reads=[t_cst], writes=[t_cst])
        P.op("dve", lambda: V.tensor_copy(out=mask256[:, 128:256], in_=cst[:, 128:256]), reads=[t_cst], writes=[t_cst])
        P.op("dve", lambda: V.tensor_copy(out=onesb[:], in_=cst[0:1, 392:520]), reads=[t_cst], writes=[t_cst])
        P.op("dve", lambda: V.tensor_copy(out=trib[:], in_=cst[:, 520:648]), reads=[t_cst], writes=[t_cst])
        P.op("dve", lambda: V.tensor_copy(out=ones128b[:], in_=cst[:, 392:520]), reads=[t_cst], writes=[t_cst])

        with ExitStack() as s0:
            cT = sbuf(s0, "cT_sb", [128, 8]); t_cT = Tr()
            wad = [sbuf(s0, "wad%d" % i, [128, 8, 512]) for i in range(2)]; t_wad = trs(2)
            modrow = sbuf(s0, "modrow", [1, 6 * D]); t_modrow = Tr()
            badar = sbuf(s0, "badar", [1, 6 * D]); t_bada = Tr()
            gT = sbuf(s0, "gT", [128, 16]); t_gT = Tr()
            lamb = sbuf(s0, "lamb", [128, 256]); t_lam = Tr()
            lamp = sbuf(s0, "lamp", [128, 128])
            P.dma("sp", lambda: nc.sync.dma_start(out=cT[:], in_=cT_d[:, :]), writes=[t_cT])
            P.dma("sp", lambda: nc.sync.dma_start(out=badar[:], in_=bada_d[:, :]), writes=[t_bada])
            P.dma("sp", lambda: nc.sync.dma_start(out=gT[:, 0:8], in_=gmixT_d[:, :]), writes=[t_gT])
            P.dma("sp", lambda: nc.sync.dma_start(out=gT[:, 8:16], in_=gffnT_d[:, :]), writes=[t_gT])
            P.dma("sp", lambda: nc.sync.dma_start(out=lamb[:].rearrange("p (a b) -> p a b", a=4),
                                                  in_=lam_d[:, :].partition_broadcast(128)), writes=[t_lam])
            P.dma("sp", lambda: nc.sync.dma_start(out=small[:, 16:24], in_=sinks_d[0:1, :].partition_broadcast(128)), writes=[t_small])
            posi = sbuf(s0, "posi", [128, 512], I32); t_posi = Tr()
            ang = sbuf(s0, "ang", [128, 512]); t_ang = Tr()
            ki = sbuf(s0, "ki", [128, 512], I32); kf = sbuf(s0, "kf", [128, 512]); t_k = Tr()
            rr = sbuf(s0, "rr", [128, 512]); t_rr = Tr()
            tab = [sbuf(s0, "tab%d" % i, [128, 512]) for i in range(4)]; t_tab = trs(4)
            def rope_group(g):
                P.dma("sp", lambda: nc.sync.dma_start(out=posi[:], in_=pos_d[0:1, g * 512:(g + 1) * 512].partition_broadcast(128)), writes=[t_posi])
                P.op("dve", lambda: V.tensor_copy(out=ang[:], in_=posi[:]), reads=[t_posi], writes=[t_ang])
                P.op("dve", lambda: V.tensor_scalar(out=ang[:], in0=ang[:], scalar1=invf, scalar2=None, op0=ALU.mult), reads=[t_ang, t_cst], writes=[t_ang])
                for which in range(2):
                    tbi = (2 * g + which) % 4
                    if which == 0:
                        P.op("dve", lambda: V.tensor_scalar(out=ki[:], in0=ang[:], scalar1=INV2PI, scalar2=None, op0=ALU.mult), reads=[t_ang], writes=[t_k])
                    else:
                        P.op("dve", lambda: V.tensor_scalar(out=ki[:], in0=ang[:], scalar1=INV2PI, scalar2=0.25, op0=ALU.mult, op1=ALU.add), reads=[t_ang], writes=[t_k])
                    P.op("dve", lambda: V.tensor_copy(out=kf[:], in_=ki[:]), reads=[t_k], writes=[t_k])
                    P.op("dve", lambda: V.scalar_tensor_tensor(out=rr[:], in0=kf[:], scalar=-C1, in1=ang[:], op0=ALU.mult, op1=ALU.add), reads=[t_k, t_ang], writes=[t_rr])
                    P.op("dve", lambda: V.scalar_tensor_tensor(out=rr[:], in0=kf[:], scalar=-C2, in1=rr[:], op0=ALU.mult, op1=ALU.add), reads=[t_k, t_rr], writes=[t_rr])
                    if which == 0:
                        P.op("dve", lambda: V.tensor_scalar(out=rr[:], in0=rr[:], scalar1=-3.1415925, scalar2=3.1415925, op0=ALU.max, op1=ALU.min), reads=[t_rr], writes=[t_rr])
                        P.op("act", lambda tbi=tbi: A.activation(out=tab[tbi][:], in_=rr[:], func=AF.Sin, scale=sgn, bias=zero_c), reads=[t_rr, t_cst], writes=[t_tab[tbi]])
                        P.dma("sp", lambda tbi=tbi: nc.sync.dma_start(out=sin_d[:, g * 512:(g + 1) * 512], in_=tab[tbi][:]), reads=[t_tab[tbi]])
                    else:
                        P.op("dve", lambda: V.tensor_scalar(out=rr[:], in0=rr[:], scalar1=-4.712388, scalar2=1.570796, op0=ALU.max, op1=ALU.min), reads=[t_rr], writes=[t_rr])
                        P.op("act", lambda tbi=tbi: A.activation(out=tab[tbi][:], in_=rr[:], func=AF.Sin, scale=1.0, bias=halfpi), reads=[t_rr, t_cst], writes=[t_tab[tbi]])
                        P.dma("sp", lambda tbi=tbi: nc.sync.dma_start(out=cos_d[:, g * 512:(g + 1) * 512], in_=tab[tbi][:]), reads=[t_tab[tbi]])
            for g in range(NGX):
                rope_group(g)

            P.op("act", lambda: A.activation(out=cT[:], in_=cT[:], func=AF.Silu), reads=[t_cT], writes=[t_cT])
            wada_v = wada_d.rearrange("(k p) n -> p k n", p=128)
            for pc in range(12):
                b = pc % 2
                P.dma("sp", lambda pc=pc, b=b: nc.sync.dma_start(out=wad[b][:], in_=wada_v[:, :, pc * 512:(pc + 1) * 512]), writes=[t_wad[b]])
                bk = pc % 2
                P.group("pe", [(lambda kc=kc, b=b, bk=bk: T.matmul(bank[bk][0:1, :], lhsT=cT[:, kc:kc + 1], rhs=wad[b][:, kc, :],
                                                                    start=(kc == 0), stop=(kc == 7))) for kc in range(8)],
                        reads=[t_cT, t_wad[b]], writes=[tb[bk]])
                P.op("dve", lambda pc=pc, bk=bk: V.tensor_tensor(out=modrow[0:1, pc * 512:(pc + 1) * 512], in0=bank[bk][0:1, :],
                                                                 in1=badar[0:1, pc * 512:(pc + 1) * 512], op=ALU.add),
                     reads=[tb[bk], t_bada], writes=[t_modrow])
            cols = [(0, 0), (1, 8), (3, 16), (4, 24)]
            fns = []
            for mi, dc in cols:
                for kc in range(8):
                    fns.append(lambda mi=mi, dc=dc, kc=kc: T.matmul(bank[2][:, dc + kc:dc + kc + 1],
                                                                    lhsT=modrow[0:1, mi * D + kc * 128: mi * D + (kc + 1) * 128],
                                                                    rhs=one11, start=True, stop=True))
            P.group("pe", fns, reads=[t_modrow, t_cst], writes=[tb[2]])
            P.op("dve", lambda: V.tensor_copy(out=S1[:], in_=bank[2][:, 0:8]), reads=[tb[2]], writes=[t_mod])
            P.op("dve", lambda: V.scalar_tensor_tensor(out=A1[:], in0=bank[2][:, 8:16], scalar=1.0, in1=gT[:, 0:8], op0=ALU.add, op1=ALU.mult),
                 reads=[tb[2], t_gT], writes=[t_mod])
            P.op("dve", lambda: V.tensor_copy(out=S2[:], in_=bank[2][:, 16:24]), reads=[tb[2]], writes=[t_mod])
            P.op("dve", lambda: V.scalar_tensor_tensor(out=A2[:], in0=bank[2][:, 24:32], scalar=1.0, in1=gT[:, 8:16], op0=ALU.add, op1=ALU.mult),
                 reads=[tb[2], t_gT], writes=[t_mod])
            P.dma("sp", lambda: nc.sync.dma_start(out=mod_d[:, :], in_=modrow[:]), reads=[t_modrow])
            P.op("dve", lambda: V.tensor_tensor(out=lamp[:, 0:64], in0=lamb[:, 0:64], in1=lamb[:, 64:128], op=ALU.mult), reads=[t_lam], writes=[t_lam])
            P.op("dve", lambda: V.tensor_tensor(out=lamp[:, 64:128], in0=lamb[:, 128:192], in1=lamb[:, 192:256], op=ALU.mult), reads=[t_lam], writes=[t_lam])
            P.op("dve", lambda: V.tensor_reduce(out=small[:, 1:3], in_=lamp[:].rearrange("p (a b) -> p a b", a=2), axis=AX.X, op=ALU.add),
                 reads=[t_lam], writes=[t_small])
            P.op("act", lambda: A.activation(out=small[:, 1:3], in_=small[:, 1:3], func=AF.Exp), reads=[t_small], writes=[t_small])
            P.op("dve", lambda: V.scalar_tensor_tensor(out=small[:, 0:1], in0=small[:, 2:3], scalar=-0.2, in1=small[:, 1:2], op0=ALU.add, op1=ALU.subtract),
                 reads=[t_small], writes=[t_small])
            P.op("act", lambda: A.activation(out=small[:, 8:16], in_=small[:, 16:24], func=AF.Exp), reads=[t_small], writes=[t_small])
            P.barrier()
            P.emit()

        with ExitStack() as s1:
            XB = 8
            xt = [sbuf(s1, "xt%d" % i, [128, D]) for i in range(XB)]; t_xt = trs(XB)
            xn = [sbuf(s1, "xn%d" % i, [128, D], BF16) for i in range(2)]; t_xn = trs(2)
            junk = sbuf(s1, "junk", [128, D], BF16); t_junk = Tr()
            ssq = sbuf(s1, "ssq", [128, 2, 8]); t_ssq = trs(2)
            hTg = [sbuf(s1, "hTg%d" % i, [128, 8, 512], BF16) for i in range(2)]; t_hTg = trs(2)
            hT_v = hT_d.rearrange("k p t -> p k t")
            A1b = sbuf(s1, "A1b", [128, D]); S1b = sbuf(s1, "S1b", [128, D]); gmb = sbuf(s1, "gmb", [128, D]); t_m1 = Tr()
            xm = [sbuf(s1, "xm%d" % i, [128, D]) for i in range(2)]; t_xm = trs(2)
            P.dma("sp", lambda: nc.sync.dma_start(out=S1b[:], in_=mod_d[0:1, 0:D].partition_broadcast(128)), writes=[t_m1])
            P.dma("sp", lambda: nc.sync.dma_start(out=A1b[:], in_=mod_d[0:1, D:2 * D].partition_broadcast(128)), writes=[t_m1])
            P.dma("sp", lambda: nc.sync.dma_start(out=gmb[:], in_=gmix_d[0:1, :].partition_broadcast(128)), writes=[t_m1])
            P.op("dve", lambda: V.scalar_tensor_tensor(out=A1b[:], in0=A1b[:], scalar=1.0, in1=gmb[:], op0=ALU.add, op1=ALU.mult), reads=[t_m1], writes=[t_m1])

            def stageA(g):
                gp = g % 2
                for tt in range(4):
                    t = 4 * g + tt
                    xb = t % XB
                    P.dma("sp", lambda t=t, xb=xb: nc.sync.dma_start(out=xt[xb][:], in_=x_d[t * 128:(t + 1) * 128, :]), writes=[t_xt[xb]])
                    P.op("act", lambda xb=xb, tt=tt: A.activation(out=junk[:], in_=xt[xb][:], func=AF.Square, accum_out=ssq[:, gp, tt:tt + 1]),
                         reads=[t_xt[xb]], writes=[t_junk, t_ssq[gp]])

            def stageA2(g):
                gp = g % 2
                P.op("dve", lambda: V.tensor_scalar(out=ssq[:, gp, 4:8], in0=ssq[:, gp, 0:4], scalar1=1.0 / D, scalar2=EPS, op0=ALU.mult, op1=ALU.add), reads=[t_ssq[gp]], writes=[t_ssq[gp]])
                P.op("act", lambda: A.activation(out=ssq[:, gp, 4:8], in_=ssq[:, gp, 4:8], func=AF.Sqrt), reads=[t_ssq[gp]], writes=[t_ssq[gp]])
                P.op("dve", lambda: V.reciprocal(out=ssq[:, gp, 4:8], in_=ssq[:, gp, 4:8]), reads=[t_ssq[gp]], writes=[t_ssq[gp]])

            def stageB(g):
                gp = g % 2
                hb = g % 2

                def tile_b(tt):
                    t = 4 * g + tt
                    xb = t % XB
                    nb = t % 2
                    P.op("dve", lambda: V.scalar_tensor_tensor(out=xm[nb][:], in0=xt[xb][:], scalar=ssq[:, gp, 4 + tt:5 + tt], in1=A1b[:], op0=ALU.mult, op1=ALU.mult),
                         reads=[t_xt[xb], t_ssq[gp], t_m1], writes=[t_xm[nb]])
                    P.op("dve", lambda: V.tensor_tensor(out=xn[nb][:], in0=xm[nb][:], in1=S1b[:], op=ALU.add), reads=[t_xm[nb], t_m1], writes=[t_xn[nb]])
                    bk = nb
                    pT = bank[bk][:, :].bitcast(BF16)
                    P.group("pe", [(lambda kc=kc: T.transpose(out=pT[:, kc * 128:(kc + 1) * 128], in_=xn[nb][:, kc * 128:(kc + 1) * 128], identity=identb[:]))
                                   for kc in range(8)], reads=[t_xn[nb], t_cst], writes=[tb[bk]])
                    P.op("act", lambda: A.activation(out=hTg[hb][:, :, tt * 128:(tt + 1) * 128], in_=pT[:, :].rearrange("p (a b) -> p a b", a=8), func=AF.Copy),
                         reads=[tb[bk]], writes=[t_hTg[hb]])
                for tt in range(4):
                    tile_b(tt)
                P.dma("sp", lambda: nc.sync.dma_start(out=hT_v[:, :, g * 512:(g + 1) * 512], in_=hTg[hb][:]), reads=[t_hTg[hb]])

            for g in range(NGX + 1):
                if g < NGX:
                    stageA(g)
                if g >= 1:
                    stageB(g - 1)
                if g < NGX:
                    stageA2(g)
            P.barrier()
            P.emit()

        s34 = st.enter_context(ExitStack())
        dest_i = sbuf(s34, "dest_i", [128, 4 * NOWN], I32); t_dest = trs(NOWN)
        gate4 = sbuf(s34, "gate4", [128, 4 * NOWN]); t_gate4 = trs(NOWN)
        maskb = sbuf(s34, "maskb", [128, NOWN, NE], BF16); t_maskb = trs(NOWN)
        cnt_run = sbuf(s34, "cnt_run", [128, NE]); t_cnt = Tr()
        cnt_i = sbuf(s34, "cnt_i", [1, NE], I32); t_cnti = Tr()
        padidx = sbuf(s34, "padidx", [128, NE], I32); t_pad = Tr()
        t_xbuf = Tr()
        iota32 = cst[:, 648:680]
        e2048 = cst[:, 680:712]
        iota_p = cst[:, 712:713]
        sA = ExitStack()
        bufA = sbuf(sA, "bufA", [128, 16 * 1024], BF16)
        mixed = bufA[:].rearrange("p (a b) -> p a b", a=NOWN)
        with ExitStack() as s2:
            Wu = sbuf(s2, "Wu", [128, 8, 1664], BF16); t_Wu = Tr()
            KT = sbuf(s2, "KT", [128, S], BF16); t_KT = Tr()
            Vb = sbuf(s2, "Vb", [128, 64 * 130], BF16); t_V = Tr()
            QT = sbuf(s2, "QT", [128, 4, NOWN * 128], BF16); t_QT = Tr()
            hTg = [sbuf(s2, "hTg2_%d" % i, [128, 8, 512], BF16) for i in range(2)]; t_hTg = trs(2)
            csg = [sbuf(s2, "csg%d" % i, [128, 2, 512]) for i in range(2)]; t_csg = trs(2)
            tm1 = [sbuf(s2, "tm1_%d" % i, [128, 512]) for i in range(2)]; t_tm1 = trs(2)
            tm2 = [sbuf(s2, "tm2_%d" % i, [128, 512]) for i in range(2)]; t_tm2 = trs(2)
            PT = [sbuf(s2, "PT%d" % i, [128, 512], BF16) for i in range(3)]; t_PT = trs(3)
            dmask = sbuf(s2, "dmask_sb", [128, NOWN, 512], BF16); t_dmask = Tr()
            smask = sbuf(s2, "smask_sb", [128, NOWN, 256], BF16); t_smask = Tr()
            bselT = sbuf(s2, "bselT", [128, NCH]); t_bsel = Tr()
            vbias = sbuf(s2, "vbias", [128, 128]); t_vbias = Tr()
            gsub_b = sbuf(s2, "gsub_b", [128, 128]); t_gsub = Tr()
            fin = sbuf(s2, "fin", [128, 8 * 128]); t_fin = Tr()
            fsm = sbuf(s2, "fsm", [128, 32]); t_fsm = Tr()
            junk2 = sbuf(s2, "junk2", [128, 128], BF16)
            hT_v = hT_d.rearrange("k p t -> p k t")
            wsel_v = wsel_d.rearrange("(k p) n -> p k n", p=128)
            for q4 in range(4):
                P.dma("pool", lambda q4=q4: G_.dma_start(out=dmask[:, 4 * q4:4 * q4 + 4, :], in_=dmask_d[:, q4 * 2048:(q4 + 1) * 2048].rearrange("p (a b) -> p a b", a=4)),
                      writes=[t_dmask])
            for q4 in range(2):
                P.dma("pool", lambda q4=q4: G_.dma_start(out=smask[:, 8 * q4:8 * q4 + 8, :], in_=smask_d[:, q4 * 2048:(q4 + 1) * 2048].rearrange("p (a b) -> p a b", a=8)),
                      writes=[t_smask])
            P.dma("sp", lambda: nc.sync.dma_start(out=bselT[:], in_=bselT_d[:, :]), writes=[t_bsel])
            P.dma("sp", lambda: nc.sync.dma_start(out=gsub_b[:], in_=gsub_d[0:1, :].partition_broadcast(128)), writes=[t_gsub])
            P.op("dve", lambda: V.tensor_scalar(out=gsub_b[:], in0=gsub_b[:], scalar1=0.8, scalar2=None, op0=ALU.mult), reads=[t_gsub], writes=[t_gsub])
            gcount = [0]

            def rope_proj(u, wc, wcs, hb, cb, ccol, ncol, dst, t_dst, par):
                bA, bB = bank[2 * par], bank[2 * par + 1]
                ci = (u["base"] + wc) // 128
                cis = (u["base"] + wcs) // 128
                P.group("pe", [(lambda kc=kc: T.matmul(bA[:, 0:ncol], lhsT=Wu[:, kc, wc:wc + 128], rhs=hTg[hb][:, kc, ccol:ccol + ncol], start=(kc == 0), stop=(kc == 7)))
                               for kc in range(8)], reads=[t_Wu, t_hTg[hb]], writes=[tb[2 * par]])
                P.group("pe", [(lambda kc=kc: T.matmul(bB[:, 0:ncol], lhsT=Wu[:, kc, wcs:wcs + 128], rhs=hTg[hb][:, kc, ccol:ccol + ncol], start=(kc == 0), stop=(kc == 7)))
                               for kc in range(8)], reads=[t_Wu, t_hTg[hb]], writes=[tb[2 * par + 1]])
                P.op("dve", lambda: V.scalar_tensor_tensor(out=tm1[par][:, 0:ncol], in0=bA[:, 0:ncol], scalar=bselT[:, ci:ci + 1], in1=csg[cb][:, 0, ccol:ccol + ncol],
                                                           op0=ALU.add, op1=ALU.mult), reads=[tb[2 * par], t_bsel, t_csg[cb]], writes=[t_tm1[par]])
                P.op("dve", lambda: V.scalar_tensor_tensor(out=tm2[par][:, 0:ncol], in0=bB[:, 0:ncol], scalar=bselT[:, cis:cis + 1], in1=csg[cb][:, 1, ccol:ccol + ncol],
                                                           op0=ALU.add, op1=ALU.mult), reads=[tb[2 * par + 1], t_bsel, t_csg[cb]], writes=[t_tm2[par]])
                P.op("dve", lambda: V.tensor_tensor(out=dst, in0=tm1[par][:, 0:ncol], in1=tm2[par][:, 0:ncol], op=ALU.add),
                     reads=[t_tm1[par], t_tm2[par]], writes=[t_dst])

            def load_group(g):
                hb = gcount[0] % 2
                gcount[0] += 1
                P.dma("sp", lambda: nc.sync.dma_start(out=hTg[hb][:], in_=hT_v[:, :, g * 512:(g + 1) * 512]), writes=[t_hTg[hb]])
                P.dma("sp", lambda: nc.sync.dma_start(out=csg[hb][:, 0, :], in_=cos_d[:, g * 512:(g + 1) * 512]), writes=[t_csg[hb]])
                P.dma("sp", lambda: nc.sync.dma_start(out=csg[hb][:, 1, :], in_=sin_d[:, g * 512:(g + 1) * 512]), writes=[t_csg[hb]])
                return hb

            pcount = [0]

            def v_proj(u, hb, vt0, vw, swa):
                bk = 4 + (pcount[0] % 2)
                pcount[0] += 1
                ov = u["o_v"]
                fns = []
                for tt in range(4):
                    for kc in range(8):
                        fns.append(lambda tt=tt, kc=kc: T.matmul(bank[bk][:, tt * 128:(tt + 1) * 128], lhsT=hTg[hb][:, kc, tt * 128:(tt + 1) * 128],
                                                                 rhs=Wu[:, kc, ov:ov + 128], start=(kc == 0), stop=(kc == 7)))
                P.group("pe", fns, reads=[t_Wu, t_hTg[hb]], writes=[tb[bk]])
                src = bank[bk][:, :].rearrange("p (a b) -> p a b", a=4)
                vb_b = vbias[:].unsqueeze(1).to_broadcast([128, 4, 128])
                if not swa:
                    dst = Vb[:, vt0 * 129:(vt0 + 4) * 129].rearrange("p (a b) -> p a b", a=4)[:, :, 0:128]
                    P.op("dve", lambda: V.tensor_tensor(out=dst, in0=src, in1=vb_b, op=ALU.add), reads=[tb[bk], t_vbias], writes=[t_V])
                else:
                    for kv in range(2):
                        dst = Vb[:, vt0 * 130:(vt0 + 4) * 130].rearrange("p (a b) -> p a b", a=4)[:, :, kv * 65:kv * 65 + 64]
                        P.op("dve", lambda dst=dst, kv=kv: V.tensor_tensor(out=dst, in0=src[:, :, kv * 64:(kv + 1) * 64],
                                                                          in1=vbias[:, kv * 64:(kv + 1) * 64].unsqueeze(1).to_broadcast([128, 4, 64]), op=ALU.add),
                             reads=[tb[bk], t_vbias], writes=[t_V])

            for ui, u in enumerate(UNITS):
                swa = (ui == 0)
                nc_u = u["ncols"]
                P.dma("pool", lambda u=u, nc_u=nc_u: G_.dma_start(out=Wu[:, :, 0:nc_u], in_=wsel_v[:, :, u["base"]:u["base"] + nc_u]), writes=[t_Wu])
                P.dma("sp", lambda u=u: nc.sync.dma_start(out=vbias[:], in_=bsel_d[0:1, u["base"] + u["o_v"]:u["base"] + u["o_v"] + 128].partition_broadcast(128)),
                      writes=[t_vbias])
                if swa:
                    vv = Vb[:, 0:32 * 130].rearrange("p (a b) -> p a b", a=32)
                    P.op("pool", lambda vv=vv: G_.memset(vv[:, :, 64:65], 1.0), writes=[t_V])
                    P.op("pool", lambda vv=vv: G_.memset(vv[:, :, 129:130], 1.0), writes=[t_V])
                    kv_groups = [(20 + i, i * 512, 4 * i) for i in range(4)] + [(16 + i, 2048 + i * 512, 16 + 4 * i) for i in range(4)]
                elif ui == 1:
                    vv = Vb[:, 0:64 * 129].rearrange("p (a b) -> p a b", a=64)
                    P.op("pool", lambda vv=vv: G_.memset(vv[:, :, 128:129], 1.0), writes=[t_V])
                    kv_groups = [(g, g * 512, 4 * g) for g in range(NG)]
                else:
                    kv_groups = [(g, g * 512, 4 * g) for g in range(NG)]
                par = 0
                for (g, kcol, vt0) in kv_groups:
                    hb = load_group(g)
                    for kc_ in range(u["nk"]):
                        rope_proj(u, u["o_k"] + kc_ * 128, u["o_ks"] + kc_ * 128, hb, hb, 0, 512, KT[:, kc_ * 4096 + kcol:kc_ * 4096 + kcol + 512], t_KT, par)
                        par ^= 1
                    v_proj(u, hb, vt0, None, swa)
                    if swa and g < 20:
                        for qc in range(4):
                            rope_proj(u, u["o_q"] + qc * 128, u["o_qs"] + qc * 128, hb, hb, 0, 512, QT[:, qc, (g - 16) * 512:(g - 15) * 512], t_QT, par)
                            par ^= 1
                if not swa:
                    for g in range(16, 20):
                        hb = load_group(g)
                        rope_proj(u, u["o_q"], u["o_qs"], hb, hb, 0, 512, QT[:, 0, (g - 16) * 512:(g - 15) * 512], t_QT, par)
                        par ^= 1

                items = []
                if swa:
                    for oi in range(NOWN):
                        for hh in range(8):
                            items.append((oi, hh, 0, True))
                else:
                    for oi in range(NOWN):
                        nkb = 8 * (oi // 2) + (4 if oi % 2 == 0 else 8)
                        for m in range(2):
                            for c in range(nkb // 4):
                                items.append((oi, m, c, c == nkb // 4 - 1))

                def qk(n):
                    oi, a, c, last = items[n]
                    sb_ = n % 3
                    if swa:
                        hh = a; half = hh % 2; qc = hh // 2; kvg = hh // 4
                        ps = slice(half * 64, half * 64 + 64)
                        fns = [lambda: T.matmul(bank[sb_][:, 0:128], lhsT=KT[ps, kvg * 4096 + oi * 128:kvg * 4096 + (oi + 1) * 128], rhs=QT[ps, qc, oi * 128:(oi + 1) * 128], start=True, stop=True),
                               lambda: T.matmul(bank[sb_][:, 128:256], lhsT=KT[ps, kvg * 4096 + 2048 + oi * 128:kvg * 4096 + 2048 + (oi + 1) * 128], rhs=QT[ps, qc, oi * 128:(oi + 1) * 128], start=True, stop=True)]
                        ncol = 256
                        mk = smask[:, oi, :]
                        t_mk = t_smask
                    else:
                        m = a
                        ps = slice(m * 64, m * 64 + 64)
                        fns = [(lambda i=i: T.matmul(bank[sb_][:, i * 128:(i + 1) * 128], lhsT=KT[ps, (4 * c + i) * 128:(4 * c + i + 1) * 128],
                                                     rhs=QT[ps, 0, oi * 128:(oi + 1) * 128], start=True, stop=True)) for i in range(4)]
                        ncol = 512
                        mk = dmask[:, oi, :]
                        t_mk = t_dmask
                    P.group("pe", fns, reads=[t_KT, t_QT], writes=[tb[sb_]])
                    P.op("act", lambda: A.activation(out=PT[sb_][:, 0:ncol], in_=bank[sb_][:, 0:ncol], func=AF.Exp, scale=0.125), reads=[tb[sb_]], writes=[t_PT[sb_]])
                    if last:
                        P.op("pool", lambda: G_.tensor_tensor(out=PT[sb_][:, 0:ncol], in0=PT[sb_][:, 0:ncol], in1=mk, op=ALU.mult), reads=[t_PT[sb_], t_mk], writes=[t_PT[sb_]])

                def pv(n):
                    oi, a, c, last = items[n]
                    sb_ = n % 3
                    if swa:
                        hh = a; kvg = hh // 4
                        ob = 3 + (oi % 2) * 2 + (hh // 4)
                        oc = (hh % 4) * 65
                        fns = [lambda: T.matmul(bank[ob][:, oc:oc + 65], lhsT=PT[sb_][:, 0:128], rhs=Vb[:, oi * 130 + kvg * 65: oi * 130 + kvg * 65 + 65], start=True, stop=False),
                               lambda: T.matmul(bank[ob][:, oc:oc + 65], lhsT=PT[sb_][:, 128:256], rhs=Vb[:, (16 + oi) * 130 + kvg * 65: (16 + oi) * 130 + kvg * 65 + 65], start=False, stop=True)]
                    else:
                        m = a
                        ob = 3 + (oi % 2) * 2 + m
                        fns = [(lambda i=i: T.matmul(bank[ob][:, 0:129], lhsT=PT[sb_][:, i * 128:(i + 1) * 128], rhs=Vb[:, (4 * c + i) * 129:(4 * c + i + 1) * 129],
                                                     start=(c == 0 and i == 0), stop=(last and i == 3))) for i in range(4)]
                    P.group("pe", fns, reads=[t_PT[sb_], t_V], writes=[tb[ob]])
                    if swa and a == 7:
                        for hh in range(8):
                            ob2 = 3 + (oi % 2) * 2 + (hh // 4)
                            oc2 = (hh % 4) * 65
                            P.op("dve", lambda hh=hh, ob2=ob2, oc2=oc2: V.tensor_tensor(out=fsm[:, hh:hh + 1], in0=bank[ob2][:, oc2 + 64:oc2 + 65], in1=expsink[:, hh:hh + 1], op=ALU.add),
                                 reads=[tb[ob2], t_small], writes=[t_fsm])
                        P.op("dve", lambda: V.reciprocal(out=fsm[:, 0:8], in_=fsm[:, 0:8]), reads=[t_fsm], writes=[t_fsm])
                        for hh in range(8):
                            ob2 = 3 + (oi % 2) * 2 + (hh // 4)
                            oc2 = (hh % 4) * 65
                            P.op("dve", lambda hh=hh, ob2=ob2, oc2=oc2: V.tensor_scalar(out=mixed[:, oi, hh * 64:(hh + 1) * 64], in0=bank[ob2][:, oc2:oc2 + 64],
                                                                                         scalar1=fsm[:, hh:hh + 1], scalar2=None, op0=ALU.mult),
                                 reads=[tb[ob2], t_fsm], writes=[t_mixed[oi]])
                    if (not swa) and a == 1 and last:
                        h = ui - 1
                        o0 = bank[3 + (oi % 2) * 2]
                        o1 = bank[3 + (oi % 2) * 2 + 1]
                        t0, t1 = tb[3 + (oi % 2) * 2], tb[3 + (oi % 2) * 2 + 1]
                        P.op("dve", lambda: V.reciprocal(out=fsm[:, 16:17], in_=o0[:, 128:129]), reads=[t0], writes=[t_fsm])
                        P.op("dve", lambda: V.reciprocal(out=fsm[:, 17:18], in_=o1[:, 128:129]), reads=[t1], writes=[t_fsm])
                        P.op("dve", lambda: V.tensor_tensor(out=fsm[:, 17:18], in0=fsm[:, 17:18], in1=neglam, op=ALU.mult), reads=[t_fsm, t_small], writes=[t_fsm])
                        P.op("dve", lambda: V.tensor_scalar(out=fin[:, 0:128], in0=o1[:, 0:128], scalar1=fsm[:, 17:18], scalar2=None, op0=ALU.mult), reads=[t1, t_fsm], writes=[t_fin])
                        P.op("dve", lambda: V.scalar_tensor_tensor(out=fin[:, 128:256], in0=o0[:, 0:128], scalar=fsm[:, 16:17], in1=fin[:, 0:128], op0=ALU.mult, op1=ALU.add),
                             reads=[t0, t_fsm, t_fin], writes=[t_fin])
                        P.op("act", lambda: A.activation(out=junk2[:], in_=fin[:, 128:256], func=AF.Square, accum_out=fsm[:, 18:19]), reads=[t_fin], writes=[t_fsm])
                        P.op("dve", lambda: V.tensor_scalar(out=fsm[:, 18:19], in0=fsm[:, 18:19], scalar1=1.0 / 128, scalar2=EPS, op0=ALU.mult, op1=ALU.add), reads=[t_fsm], writes=[t_fsm])
                        P.op("act", lambda: A.activation(out=fsm[:, 18:19], in_=fsm[:, 18:19], func=AF.Sqrt), reads=[t_fsm], writes=[t_fsm])
                        P.op("dve", lambda: V.reciprocal(out=fsm[:, 18:19], in_=fsm[:, 18:19]), reads=[t_fsm], writes=[t_fsm])
                        P.op("dve", lambda: V.scalar_tensor_tensor(out=mixed[:, oi, 512 + h * 128:512 + (h + 1) * 128], in0=fin[:, 128:256], scalar=fsm[:, 18:19], in1=gsub_b[:],
                                                                   op0=ALU.mult, op1=ALU.mult), reads=[t_fin, t_fsm, t_gsub], writes=[t_mixed[oi]])

                LAG = 2
                for n in range(len(items) + LAG):
                    if n < len(items):
                        qk(n)
                    if n >= LAG:
                        pv(n - LAG)
            P.barrier()
            P.emit()

        with ExitStack() as s3:
            gt1_b = sbuf(s3, "gt1_b", [128, D])
            A2b = sbuf(s3, "A2b", [128, D]); S2b = sbuf(s3, "S2b", [128, D]); t_m2 = Tr()
            P.dma("sp", lambda: nc.sync.dma_start(out=gt1_b[:], in_=mod_d[0:1, 2 * D:3 * D].partition_broadcast(128)), writes=[t_mod])
            P.dma("sp", lambda: nc.sync.dma_start(out=S2b[:], in_=mod_d[0:1, 3 * D:4 * D].partition_broadcast(128)), writes=[t_m2])
            P.dma("sp", lambda: nc.sync.dma_start(out=A2b[:], in_=mod_d[0:1, 4 * D:5 * D].partition_broadcast(128)), writes=[t_m2])
            wout = sbuf(s3, "wout", [128, 8, D], BF16); t_wout = Tr()
            boutb = sbuf(s3, "boutb", [1, D], BF16)
            wr = sbuf(s3, "wr", [128, 8, NE], BF16); t_wr = Tr()
            brb = sbuf(s3, "brb", [1, NE], BF16)
            gfb = sbuf(s3, "gfb", [128, D]); t_gfb = Tr()
            P.dma("sp", lambda: nc.sync.dma_start(out=gfb[:], in_=gffn_d[0:1, :].partition_broadcast(128)), writes=[t_gfb])
            P.op("dve", lambda: V.scalar_tensor_tensor(out=A2b[:], in0=A2b[:], scalar=1.0, in1=gfb[:], op0=ALU.add, op1=ALU.mult), reads=[t_m2, t_gfb], writes=[t_m2])
            P.op("dve", lambda: V.memset(cnt_run[:], 0.0), writes=[t_cnt])
            mixT = [sbuf(s3, "mixT%d" % i, [128, 8, 128], BF16) for i in range(2)]; t_mixT = trs(2)
            xo = [sbuf(s3, "xo%d" % i, [128, D]) for i in range(2)]; t_xo = trs(2)
            x1t = [sbuf(s3, "x1t%d" % i, [128, D]) for i in range(2)]; t_x1t = trs(2)
            h2f = [sbuf(s3, "h2f%d" % i, [128, D]) for i in range(2)]; t_h2f = trs(2)
            h2tok = [sbuf(s3, "h2tok%d" % i, [128, D], BF16) for i in range(2)]; t_h2tok = trs(2)
            h2Tt = [sbuf(s3, "h2Tt%d" % i, [128, 8, 128], BF16) for i in range(2)]; t_h2Tt = trs(2)
            zrow = sbuf(s3, "zrow", [128, D], BF16); t_zrow = Tr()
            junk3 = sbuf(s3, "junk3", [128, D], BF16); t_junk3 = Tr()
            rs = sbuf(s3, "rs", [128, 64]); t_rs = trs(2)
            lg = sbuf(s3, "lg", [128, 2, 4 * NE]); t_lg = trs(2)
            idx8 = sbuf(s3, "idx8", [128, 2, 8], U32)
            posb = sbuf(s3, "posb", [128, 2, NE]); junkp = sbuf(s3, "junkp", [128, 2, NE])
            wout_v = wout_d.rearrange("(k p) n -> p k n", p=128)
            wr_v = wr_d.rearrange("(k p) n -> p k n", p=128)
            P.dma("pool", lambda: G_.dma_start(out=wout[:], in_=wout_v), writes=[t_wout])
            P.dma("pool", lambda: G_.dma_start(out=boutb[:], in_=bout_d[:, :]), writes=[t_wout])
            P.dma("pool", lambda: G_.dma_start(out=wr[:], in_=wr_v), writes=[t_wr])
            P.dma("pool", lambda: G_.dma_start(out=brb[:], in_=br_d[:, :]), writes=[t_wr])
            P.op("pool", lambda: G_.memset(zrow[:], 0.0), writes=[t_zrow])

            def p3(oi):
                b = oi % 2
                P.dma("sp", lambda: nc.sync.dma_start(out=xo[b][:], in_=x_d[S + oi * 128:S + (oi + 1) * 128, :]), writes=[t_xo[b]])
                pT = bank[b][:, :].bitcast(BF16)
                P.group("pe", [(lambda kc=kc: T.transpose(out=pT[:, kc * 128:(kc + 1) * 128], in_=mixed[:, oi, kc * 128:(kc + 1) * 128], identity=identb[:])) for kc in range(8)],
                        reads=[t_mixed[oi], t_cst], writes=[tb[b]])
                P.op("act", lambda: A.activation(out=mixT[b][:].rearrange("p a b -> p (a b)"), in_=pT[:, :], func=AF.Copy), reads=[tb[b]], writes=[t_mixT[b]])
                for hf in range(2):
                    bk = 2 + 2 * b + hf
                    fns = [(lambda kc=kc, hf=hf, bk=bk: T.matmul(bank[bk][:, :], lhsT=mixT[b][:, kc, :], rhs=wout[:, kc, hf * 512:(hf + 1) * 512], start=(kc == 0), stop=False)) for kc in range(8)]
                    fns.append(lambda hf=hf, bk=bk: T.matmul(bank[bk][:, :], lhsT=onesb[0:1, :], rhs=boutb[0:1, hf * 512:(hf + 1) * 512], start=False, stop=True))
                    P.group("pe", fns, reads=[t_mixT[b], t_wout, t_cst], writes=[tb[bk]])
                    P.op("dve", lambda hf=hf, bk=bk: V.tensor_tensor(out=x1t[b][:, hf * 512:(hf + 1) * 512], in0=bank[bk][:, :], in1=gt1_b[:, hf * 512:(hf + 1) * 512], op=ALU.mult),
                         reads=[tb[bk], t_mod], writes=[t_x1t[b]])
                P.op("dve", lambda: V.tensor_tensor(out=x1t[b][:], in0=x1t[b][:], in1=xo[b][:], op=ALU.add), reads=[t_x1t[b], t_xo[b]], writes=[t_x1t[b]])
                P.dma("sp", lambda: nc.sync.dma_start(out=x1_d[oi * 128:(oi + 1) * 128, :], in_=x1t[b][:]), reads=[t_x1t[b]])
                r0 = 32 * b
                P.op("act", lambda: A.activation(out=junk3[:], in_=x1t[b][:], func=AF.Square, accum_out=rs[:, r0:r0 + 1]), reads=[t_x1t[b]], writes=[t_junk3, t_rs[b]])
                P.op("dve", lambda: V.tensor_scalar(out=rs[:, r0 + 1:r0 + 2], in0=rs[:, r0:r0 + 1], scalar1=1.0 / D, scalar2=EPS, op0=ALU.mult, op1=ALU.add), reads=[t_rs[b]], writes=[t_rs[b]])
                P.op("act", lambda: A.activation(out=rs[:, r0 + 1:r0 + 2], in_=rs[:, r0 + 1:r0 + 2], func=AF.Sqrt), reads=[t_rs[b]], writes=[t_rs[b]])
                P.op("dve", lambda: V.reciprocal(out=rs[:, r0 + 1:r0 + 2], in_=rs[:, r0 + 1:r0 + 2]), reads=[t_rs[b]], writes=[t_rs[b]])
                P.op("dve", lambda: V.scalar_tensor_tensor(out=h2f[b][:], in0=x1t[b][:], scalar=rs[:, r0 + 1:r0 + 2], in1=A2b[:], op0=ALU.mult, op1=ALU.mult),
                     reads=[t_x1t[b], t_rs[b], t_m2], writes=[t_h2f[b]])
                P.op("dve", lambda: V.tensor_tensor(out=h2tok[b][:], in0=h2f[b][:], in1=S2b[:], op=ALU.add), reads=[t_h2f[b], t_m2], writes=[t_h2tok[b]])
                bk = 6 + b
                pT2 = bank[bk][:, :].bitcast(BF16)
                P.group("pe", [(lambda kc=kc: T.transpose(out=pT2[:, kc * 128:(kc + 1) * 128], in_=h2tok[b][:, kc * 128:(kc + 1) * 128], identity=identb[:])) for kc in range(8)],
                        reads=[t_h2tok[b], t_cst], writes=[tb[bk]])
                P.op("act", lambda: A.activation(out=h2Tt[b][:].rearrange("p a b -> p (a b)"), in_=pT2[:, :], func=AF.Copy), reads=[tb[bk]], writes=[t_h2Tt[b]])
                fns = [(lambda kc=kc: T.matmul(bank[b][:, 0:NE], lhsT=h2Tt[b][:, kc, :], rhs=wr[:, kc, :], start=(kc == 0), stop=False)) for kc in range(8)]
                fns.append(lambda: T.matmul(bank[b][:, 0:NE], lhsT=onesb[0:1, :], rhs=brb[0:1, :], start=False, stop=True))
                P.group("pe", fns, reads=[t_h2Tt[b], t_wr, t_cst], writes=[tb[b]])

            def p3c(oi):
                b = oi % 2
                r0 = 32 * b
                L0, L1, L2, L3 = lg[:, b, 0:NE], lg[:, b, NE:NE + 8], lg[:, b, 2 * NE:3 * NE], lg[:, b, 3 * NE:3 * NE + 8]
                P.op("dve", lambda: V.tensor_copy(out=L0, in_=bank[b][:, 0:NE]), reads=[tb[b]], writes=[t_lg[b]])
                P.op("dve", lambda: V.max(out=L1, in_=L0), reads=[t_lg[b]], writes=[t_lg[b]])
                P.op("dve", lambda: V.max_index(out=idx8[:, b, :], in_max=L1, in_values=L0), reads=[t_lg[b]], writes=[t_lg[b]])
                P.op("dve", lambda: V.tensor_scalar(out=maskb[:, oi, :], in0=L0, scalar1=lg[:, b, NE + 3:NE + 4], scalar2=None, op0=ALU.is_ge), reads=[t_lg[b]], writes=[t_maskb[oi]])
                P.op("dve", lambda: V.tensor_scalar(out=rs[:, r0 + 2:r0 + 3], in0=lg[:, b, NE:NE + 1], scalar1=-1.0, scalar2=None, op0=ALU.mult), reads=[t_lg[b]], writes=[t_rs[b]])
                P.op("act", lambda: A.activation(out=L3[:, 0:4], in_=L1[:, 0:4], func=AF.Exp, bias=rs[:, r0 + 2:r0 + 3], scale=1.0, accum_out=rs[:, r0 + 3:r0 + 4]),
                     reads=[t_lg[b], t_rs[b]], writes=[t_lg[b], t_rs[b]])
                P.op("dve", lambda: V.reciprocal(out=rs[:, r0 + 3:r0 + 4], in_=rs[:, r0 + 3:r0 + 4]), reads=[t_rs[b]], writes=[t_rs[b]])
                P.op("dve", lambda: V.tensor_scalar(out=gate4[:, 4 * oi:4 * oi + 4], in0=L3[:, 0:4], scalar1=rs[:, r0 + 3:r0 + 4], scalar2=None, op0=ALU.mult),
                     reads=[t_lg[b], t_rs[b]], writes=[t_gate4[oi]])
                pb = bank[b]
                P.group("pe", [lambda: T.matmul(pb[:, 64:64 + NE], lhsT=trib[:], rhs=maskb[:, oi, :], start=True, stop=True),
                               lambda: T.matmul(pb[:, 128:128 + NE], lhsT=ones128b[:], rhs=maskb[:, oi, :], start=True, stop=True)],
                        reads=[t_maskb[oi], t_cst, t_lg[b]], writes=[tb[b]])
                P.op("dve", lambda: V.tensor_tensor(out=posb[:, b, :], in0=pb[:, 64:64 + NE], in1=cnt_run[:], op=ALU.add), reads=[tb[b], t_cnt], writes=[t_lg[b]])
                P.op("dve", lambda: V.tensor_tensor(out=cnt_run[:], in0=pb[:, 128:128 + NE], in1=cnt_run[:], op=ALU.add), reads=[tb[b], t_cnt, t_lg[b]], writes=[t_cnt])
                EK = rs[:, r0 + 8:r0 + 12]; PK = rs[:, r0 + 12:r0 + 16]; DF = rs[:, r0 + 16:r0 + 20]
                P.op("dve", lambda: V.tensor_copy(out=EK, in_=idx8[:, b, 0:4]), reads=[t_lg[b]], writes=[t_rs[b]])
                for k in range(4):
                    P.op("dve", lambda k=k: V.scalar_tensor_tensor(out=junkp[:, b, :], in0=iota32, scalar=rs[:, r0 + 8 + k:r0 + 9 + k], in1=posb[:, b, :],
                                                                   op0=ALU.is_equal, op1=ALU.mult, accum_out=rs[:, r0 + 12 + k:r0 + 13 + k]),
                         reads=[t_lg[b], t_rs[b], t_cst], writes=[t_rs[b]])
                P.op("dve", lambda: V.scalar_tensor_tensor(out=DF, in0=EK, scalar=float(CAP), in1=PK, op0=ALU.mult, op1=ALU.add), reads=[t_rs[b]], writes=[t_rs[b]])
                P.op("dve", lambda: V.tensor_copy(out=dest_i[:, 4 * oi:4 * oi + 4], in_=DF), reads=[t_rs[b]], writes=[t_dest[oi]])
                for k in range(4):
                    P.dma("pool", lambda k=k: G_.indirect_dma_start(out=xbuf_d[:, :], out_offset=bass.IndirectOffsetOnAxis(ap=dest_i[:, 4 * oi + k:4 * oi + k + 1], axis=0),
                                                                    in_=h2tok[b][:, :], in_offset=None),
                          reads=[t_h2tok[b], t_dest[oi]], writes=[t_xbuf])
            p3(0)
            for oi in range(NOWN):
                if oi + 1 < NOWN:
                    p3(oi + 1)
                p3c(oi)
            cf = rs[0:1, 0:NE]
            P.op("dve", lambda: V.tensor_scalar(out=cf, in0=cnt_run[0:1, :], scalar1=127.0, scalar2=1.0 / 128, op0=ALU.add, op1=ALU.mult), reads=[t_cnt] + t_rs, writes=t_rs)
            P.op("dve", lambda: V.tensor_scalar(out=cf, in0=cf, scalar1=-0.496, scalar2=None, op0=ALU.add), reads=t_rs, writes=t_rs)
            P.op("dve", lambda: V.tensor_copy(out=cnt_i[:], in_=cf), reads=t_rs, writes=[t_cnti])
            P.op("dve", lambda: V.tensor_scalar(out=posb[:, 0, :], in0=cnt_run[:], scalar1=iota_p, scalar2=None, op0=ALU.add), reads=[t_cnt, t_cst] + t_lg, writes=t_lg)
            P.op("dve", lambda: V.tensor_scalar(out=posb[:, 1, :], in0=posb[:, 0, :], scalar1=float(CAP), scalar2=None, op0=ALU.is_ge), reads=t_lg, writes=t_lg)
            P.op("dve", lambda: V.tensor_tensor(out=posb[:, 0, :], in0=posb[:, 0, :], in1=e2048, op=ALU.add), reads=t_lg + [t_cst], writes=t_lg)
            P.op("dve", lambda: V.tensor_scalar(out=rs[:, 40:41], in0=iota_p, scalar1=float(NE * CAP), scalar2=None, op0=ALU.add), reads=[t_cst] + t_rs, writes=t_rs)
            P.op("dve", lambda: V.tensor_scalar(out=junkp[:, 0, :], in0=posb[:, 0, :], scalar1=-1.0, scalar2=rs[:, 40:41], op0=ALU.mult, op1=ALU.add), reads=t_lg + t_rs, writes=t_lg)
            P.op("dve", lambda: V.tensor_tensor(out=junkp[:, 0, :], in0=junkp[:, 0, :], in1=posb[:, 1, :], op=ALU.mult), reads=t_lg, writes=t_lg)
            P.op("dve", lambda: V.tensor_tensor(out=posb[:, 0, :], in0=posb[:, 0, :], in1=junkp[:, 0, :], op=ALU.add), reads=t_lg, writes=t_lg)
            P.op("dve", lambda: V.tensor_copy(out=padidx[:], in_=posb[:, 0, :]), reads=t_lg, writes=[t_pad])
            for e in range(NE):
                P.dma("pool", lambda e=e: G_.indirect_dma_start(out=xbuf_d[:, :], out_offset=bass.IndirectOffsetOnAxis(ap=padidx[:, e:e + 1], axis=0),
                                                                in_=zrow[:, :], in_offset=None),
                      reads=[t_zrow, t_pad], writes=[t_xbuf])
            P.barrier()
            P.emit()

        sA.close()
        if int(os.environ.get('K_STOP', '9')) <= 3:
            return nc
        with ExitStack() as s4:
            NW = 2
            w1b = [sbuf(s4, "w1b%d" % i, [128, 8, 2 * D], BF16) for i in range(NW)]
            w2b = [sbuf(s4, "w2b%d" % i, [128, 8, D], BF16) for i in range(NW)]
            b1r = [sbuf(s4, "b1r%d" % i, [1, 2 * D], BF16) for i in range(NW)]
            b2r = [sbuf(s4, "b2r%d" % i, [1, D], BF16) for i in range(NW)]
            t_w = trs(NW)
            Xtok = [sbuf(s4, "Xtok%d" % i, [128, D], BF16) for i in range(2)]; t_Xtok = trs(2)
            XT = [sbuf(s4, "XT%d" % i, [128, 8, 128], BF16) for i in range(2)]; t_XT = trs(2)
            gg = [sbuf(s4, "gg%d" % i, [128, 256]) for i in range(4)]; t_gg = trs(4)
            sg = [sbuf(s4, "sg%d" % i, [128, 256]) for i in range(4)]; t_sg = trs(4)
            ll = [sbuf(s4, "ll%d" % i, [128, 256]) for i in range(4)]; t_ll = trs(4)
            atok = [sbuf(s4, "atok%d" % i, [128, D], BF16) for i in range(2)]; t_atok = trs(2)
            aT = [sbuf(s4, "aT%d" % i, [128, 8, 128], BF16) for i in range(2)]; t_aT = trs(2)
            yt = [sbuf(s4, "yt%d" % i, [128, D]) for i in range(2)]; t_yt = trs(2)
            t_ybuf = Tr()
            w1_v = w1_d.rearrange("e (k p) n -> e p k n", p=128)
            w2_v = w2_d.rearrange("e (k p) n -> e p k n", p=128)
            blk = [0]

            def prologue(e, bslot, n):
                xb = n % 2
                row0 = e * CAP + bslot * 128
                P.dma("sp", lambda: nc.sync.dma_start(out=Xtok[xb][:], in_=xbuf_d[row0:row0 + 128, :]), reads=[t_xbuf], writes=[t_Xtok[xb]], slot=xb)
                pT = bank[xb][:, :].bitcast(BF16)
                P.group("pe", [(lambda kc=kc: T.transpose(out=pT[:, kc * 128:(kc + 1) * 128], in_=Xtok[xb][:, kc * 128:(kc + 1) * 128], identity=identb[:])) for kc in range(8)],
                        reads=[t_Xtok[xb], t_cst], writes=[tb[xb]])
                P.op("act", lambda: A.activation(out=XT[xb][:].rearrange("p a b -> p (a b)"), in_=pT[:, :], func=AF.Copy), reads=[tb[xb]], writes=[t_XT[xb]])

            def block_body(e, bslot, ws, n, nxt):
                xb = n % 2
                row0 = e * CAP + bslot * 128
                def do_cch(cch):
                    bk = 2 + (cch % 2)
                    par = cch
                    fns = [(lambda kc=kc: T.matmul(bank[bk][:, :], lhsT=XT[xb][:, kc, :], rhs=w1b[ws][:, kc, cch * 512:(cch + 1) * 512], start=(kc == 0), stop=False)) for kc in range(8)]
                    fns.append(lambda: T.matmul(bank[bk][:, :], lhsT=onesb[0:1, :], rhs=b1r[ws][0:1, cch * 512:(cch + 1) * 512], start=False, stop=True))
                    P.group("pe", fns, reads=[t_XT[xb], t_w[ws], t_cst], writes=[tb[bk]])
                    P.op("dve", lambda: V.tensor_scalar(out=gg[par][:], in0=bank[bk][:, 0:512:2], scalar1=7.0, scalar2=None, op0=ALU.min), reads=[tb[bk]], writes=[t_gg[par]])
                    P.op("act", lambda: A.activation(out=sg[par][:], in_=gg[par][:], func=AF.Gelu_apprx_sigmoid), reads=[t_gg[par]], writes=[t_sg[par]])
                    P.op("dve", lambda: V.tensor_scalar(out=ll[par][:], in0=bank[bk][:, 1:512:2], scalar1=7.0, scalar2=-7.0, op0=ALU.min, op1=ALU.max), reads=[tb[bk]], writes=[t_ll[par]])

                def fin_cch(cch):
                    par = cch
                    P.op("dve", lambda: V.scalar_tensor_tensor(out=atok[xb][:, cch * 256:(cch + 1) * 256], in0=ll[par][:], scalar=1.0, in1=sg[par][:], op0=ALU.add, op1=ALU.mult),
                         reads=[t_ll[par], t_sg[par]], writes=[t_atok[xb]])
                for cch in range(4):
                    do_cch(cch)
                    if cch >= 1:
                        fin_cch(cch - 1)
                if nxt is not None:
                    prologue(*nxt)
                fin_cch(3)
                bk = 4 + xb
                pT2 = bank[bk][:, :].bitcast(BF16)
                P.group("pe", [(lambda j=j: T.transpose(out=pT2[:, j * 128:(j + 1) * 128], in_=atok[xb][:, j * 128:(j + 1) * 128], identity=identb[:])) for j in range(8)],
                        reads=[t_atok[xb], t_cst], writes=[tb[bk]])
                P.op("act", lambda: A.activation(out=aT[xb][:].rearrange("p a b -> p (a b)"), in_=pT2[:, :], func=AF.Copy), reads=[tb[bk]], writes=[t_aT[xb]])
                def do_hf(hf):
                    bk2 = 6 + hf
                    fns = [(lambda j=j: T.matmul(bank[bk2][:, :], lhsT=aT[xb][:, j, :], rhs=w2b[ws][:, j, hf * 512:(hf + 1) * 512], start=(j == 0), stop=False)) for j in range(8)]
                    fns.append(lambda: T.matmul(bank[bk2][:, :], lhsT=onesb[0:1, :], rhs=b2r[ws][0:1, hf * 512:(hf + 1) * 512], start=False, stop=True))
                    P.group("pe", fns, reads=[t_aT[xb], t_w[ws], t_cst], writes=[tb[bk2]])
                    if hf == 0:
                        P.op("act", lambda: A.activation(out=yt[xb][:, 0:512], in_=bank[bk2][:, :], func=AF.Copy), reads=[tb[bk2]], writes=[t_yt[xb]])
                    else:
                        P.op("dve", lambda: V.tensor_copy(out=yt[xb][:, 512:1024], in_=bank[bk2][:, :]), reads=[tb[bk2]], writes=[t_yt[xb]])
                for hf in range(2):
                    do_hf(hf)
                P.dma("sp", lambda: nc.sync.dma_start(out=ybuf_d[row0:row0 + 128, :], in_=yt[xb][:]), reads=[t_yt[xb]], writes=[t_ybuf], slot=2 + xb)

            for e in range(int(os.environ.get('K_NE', NE))):
                ws = e % NW
                P.dma("pool", lambda e=e, ws=ws: G_.dma_start(out=w1b[ws][:], in_=w1_v[e]), writes=[t_w[ws]])
                P.dma("pool", lambda e=e, ws=ws: G_.dma_start(out=w2b[ws][:], in_=w2_v[e]), writes=[t_w[ws]])
                P.dma("pool", lambda e=e, ws=ws: G_.dma_start(out=b1r[ws][:], in_=b1_d[e:e + 1, :]), writes=[t_w[ws]])
                P.dma("pool", lambda e=e, ws=ws: G_.dma_start(out=b2r[ws][:], in_=b2_d[e:e + 1, :]), writes=[t_w[ws]])
                P.regload(cnt_i[0:1, e:e + 1], reads=[t_cnti])
                NB = CAP // 128
                n0 = blk[0]
                blk[0] += NB
                prologue(e, 0, n0)
                for bslot in range(NB):
                    if bslot in (3, 6, 10):
                        P.cond_begin(bslot + 1)
                    P.cond_begin(bslot + 1)
                    block_body(e, bslot, ws, n0 + bslot, (e, bslot + 1, n0 + bslot + 1) if bslot + 1 < NB else None)
                    P.cond_end()
                for _ in range(3):
                    P.cond_end()
            P.barrier()
            P.emit()

        if int(os.environ.get('K_STOP', '9')) <= 4:
            return nc
        with ExitStack() as s5:
            gt2_b = sbuf(s5, "gt2_b", [128, D]); gfin_b = sbuf(s5, "gfin_b", [128, D]); t_g5 = Tr()
            yk = [sbuf(s5, "yk%d" % i, [128, D]) for i in range(8)]; t_yk = trs(8)
            acc = [sbuf(s5, "acc%d" % i, [128, D]) for i in range(2)]; t_acc = trs(2)
            x1b = [sbuf(s5, "x1b%d" % i, [128, D]) for i in range(2)]; t_x1b = trs(2)
            ob_ = [sbuf(s5, "ob%d" % i, [128, D]) for i in range(2)]; t_ob = trs(2)
            junk5 = sbuf(s5, "junk5", [128, D], BF16); t_junk5 = Tr()
            fs5 = sbuf(s5, "fs5", [128, 8]); t_fs5 = trs(2)
            P.dma("sp", lambda: nc.sync.dma_start(out=gt2_b[:], in_=mod_d[0:1, 5 * D:6 * D].partition_broadcast(128)), writes=[t_g5])
            P.dma("sp", lambda: nc.sync.dma_start(out=gfin_b[:], in_=gfin_d[0:1, :].partition_broadcast(128)), writes=[t_g5])

            def p5(oi):
                b = oi % 2
                kb = 4 * (oi % 2)
                P.dma("sp", lambda: nc.sync.dma_start(out=x1b[b][:], in_=x1_d[oi * 128:(oi + 1) * 128, :]), writes=[t_x1b[b]])
                for k in range(4):
                    P.dma("pool", lambda k=k: G_.indirect_dma_start(out=yk[kb + k][:, :], out_offset=None, in_=ybuf_d[:, :],
                                                                    in_offset=bass.IndirectOffsetOnAxis(ap=dest_i[:, 4 * oi + k:4 * oi + k + 1], axis=0),
                                                                    ), reads=[t_dest[oi]], writes=[t_yk[kb + k]])
                    if k == 0:
                        P.op("dve", lambda: V.tensor_scalar(out=acc[b][:], in0=yk[kb][:], scalar1=gate4[:, 4 * oi:4 * oi + 1], scalar2=None, op0=ALU.mult),
                             reads=[t_yk[kb], t_gate4[oi]], writes=[t_acc[b]])
                    else:
                        P.op("dve", lambda k=k: V.scalar_tensor_tensor(out=acc[b][:], in0=yk[kb + k][:], scalar=gate4[:, 4 * oi + k:4 * oi + k + 1], in1=acc[b][:], op0=ALU.mult, op1=ALU.add),
                             reads=[t_yk[kb + k], t_gate4[oi], t_acc[b]], writes=[t_acc[b]])
                P.op("dve", lambda: V.tensor_tensor(out=acc[b][:], in0=acc[b][:], in1=gt2_b[:], op=ALU.mult), reads=[t_acc[b], t_g5], writes=[t_acc[b]])
                P.op("dve", lambda: V.tensor_tensor(out=x1b[b][:], in0=acc[b][:], in1=x1b[b][:], op=ALU.add), reads=[t_acc[b], t_x1b[b]], writes=[t_x1b[b]])
                P.op("act", lambda: A.activation(out=junk5[:], in_=x1b[b][:], func=AF.Square, accum_out=fs5[:, 4 * b:4 * b + 1]), reads=[t_x1b[b]], writes=[t_junk5, t_fs5[b]])
                P.op("dve", lambda: V.tensor_scalar(out=fs5[:, 4 * b + 1:4 * b + 2], in0=fs5[:, 4 * b:4 * b + 1], scalar1=1.0 / D, scalar2=EPS, op0=ALU.mult, op1=ALU.add),
                     reads=[t_fs5[b]], writes=[t_fs5[b]])
                P.op("act", lambda: A.activation(out=fs5[:, 4 * b + 1:4 * b + 2], in_=fs5[:, 4 * b + 1:4 * b + 2], func=AF.Sqrt), reads=[t_fs5[b]], writes=[t_fs5[b]])
                P.op("dve", lambda: V.reciprocal(out=fs5[:, 4 * b + 1:4 * b + 2], in_=fs5[:, 4 * b + 1:4 * b + 2]), reads=[t_fs5[b]], writes=[t_fs5[b]])
                P.op("dve", lambda: V.scalar_tensor_tensor(out=ob_[b][:], in0=x1b[b][:], scalar=fs5[:, 4 * b + 1:4 * b + 2], in1=gfin_b[:], op0=ALU.mult, op1=ALU.mult),
                     reads=[t_x1b[b], t_fs5[b], t_g5], writes=[t_ob[b]])
                P.dma("sp", lambda: nc.sync.dma_start(out=out_d[oi * 128:(oi + 1) * 128, :], in_=ob_[b][:]), reads=[t_ob[b]])
            for oi in range(NOWN):
                p5(oi)
            P.barrier()
            P.emit()
    return nc


def _consts():
    c = np.zeros((128, NCST), np.float32)
    c[:, 0:128] = np.eye(128, dtype=np.float32)
    k = np.arange(128)[:, None]
    q = np.arange(128)[None, :]
    c[:, 128:256] = (k <= q)
    c[:, 256:384] = (k > q)
    inv = (1.0 / (np.float32(10000.0) ** (np.arange(0, 64, 2, dtype=np.float32) / np.float32(64)))).astype(np.float32)
    p = np.arange(128)
    c[:, 384] = inv[p % 32]
    c[:, 385] = np.where((p % 64) < 32, -1.0, 1.0)
    c[:, 386] = np.float32(math.pi / 2)
    c[:, 387] = 0.0
    c[:, 388] = 1.0
    c[:, 392:520] = 1.0
    c[:, 520:648] = (k < q)
    c[:, 648:680] = np.arange(32)[None, :]
    c[:, 680:712] = (np.arange(32) * CAP)[None, :]
    c[:, 712] = np.arange(128)
    return c


def _core_masks(j):
    own = own_blocks(j)
    k = np.arange(128)[:, None]
    q = np.arange(128)[None, :]
    tri = (k <= q).astype(np.float32)
    low = (k > q).astype(np.float32)
    dm = np.zeros((128, NOWN, 4, 128), np.float32)
    sm = np.zeros((128, NOWN, 2, 128), np.float32)
    for oi, gb in enumerate(own):
        nkb = 8 * (oi // 2) + (4 if oi % 2 == 0 else 8)
        for i in range(4):
            kb = nkb - 4 + i
            if kb < gb:
                dm[:, oi, i, :] = 1.0
            elif kb == gb:
                dm[:, oi, i, :] = tri
        sm[:, oi, 1, :] = tri
        if gb > 0:
            sm[:, oi, 0, :] = low
    return dm.reshape(128, NOWN * 512), sm.reshape(128, NOWN * 256)


_NC_CACHE = {}


def kernel(x, c, positions, w_ada, b_ada, g_mix, w_in, b_in, attn_sinks, lambda_q1, lambda_k1, lambda_q2, lambda_k2,
           g_subln, w_out, b_out, g_ffn, w_router, b_router, w1, b1, w2, b2, g_final):
    f = lambda a: np.ascontiguousarray(np.asarray(a))
    x = f(x); positions = f(positions)
    if "nc" not in _NC_CACHE:
        _NC_CACHE["nc"] = build_program()
    nc = _NC_CACHE["nc"]
    colT = lambda v: f(np.asarray(v).reshape(-1, 128).T)
    w_sel = f(np.asarray(w_in)[0][:, SEL])
    b_sel = f(np.asarray(b_in)[0][SEL])
    b1_ = np.asarray(b1)[0]
    shared = {
        "w_ada": f(np.asarray(w_ada)[0]), "b_ada": f(np.asarray(b_ada)[0][None, :]),
        "gmixT": colT(np.asarray(g_mix)[0]), "gffnT": colT(np.asarray(g_ffn)[0]),
        "w_sel": w_sel, "b_selT": colT(b_sel), "b_sel": f(b_sel[None, :]),
        "sinks": f(np.asarray(attn_sinks)[0][None, :]),
        "lam4": f(np.stack([np.asarray(lambda_q1)[0], np.asarray(lambda_k1)[0], np.asarray(lambda_q2)[0], np.asarray(lambda_k2)[0]])),
        "g_subln": f(np.asarray(g_subln)[0][None, :]),
        "w_out": f(np.asarray(w_out)[0]), "b_out": f(np.asarray(b_out)[0][None, :]),
        "w_router": f(np.asarray(w_router)[0]), "b_router": f(np.asarray(b_router)[0][None, :]),
        "w1": f(np.asarray(w1)[0]), "w2": f(np.asarray(w2)[0]), "b2": f(np.asarray(b2)[0]),
        "b1": f(b1_), "g_ffn": f(np.asarray(g_ffn)[0][None, :]), "g_mix": f(np.asarray(g_mix)[0][None, :]),
        "g_final": f(np.asarray(g_final)[None, :]),
        "consts": _consts(),
    }
    in_maps = []
    rows_all = []
    for core in range(8):
        b, j = core // 4, core % 4
        own = own_blocks(j)
        rows_own = np.concatenate([np.arange(g * 128, (g + 1) * 128) for g in own])
        rows_prev = np.concatenate([np.arange(max(g - 1, 0) * 128, (max(g - 1, 0) + 1) * 128) for g in own])
        rows_all.append(rows_own)
        xb = x[b]
        x_ext = np.concatenate([xb, xb[rows_own], xb[rows_prev]], axis=0)
        pb = positions[b]
        pos_ext = np.concatenate([pb, pb[rows_own], pb[rows_prev]])[None, :].astype(np.int32)
        dm, sm = _core_masks(j)
        m = dict(shared)
        m.update({"x": f(x_ext), "pos": f(pos_ext), "cT": colT(np.asarray(c)[b]), "dmask": dm, "smask": sm})
        in_maps.append(m)
    res = run_bass_kernel_spmd(nc, in_maps, core_ids=list(range(8)))
    out = np.zeros((2, S, D), np.float32)
    for core in range(8):
        out[core // 4, rows_all[core], :] = np.asarray(res.results[core]["out"])
    return out
```

```python
import math
import os
from contextlib import ExitStack

import numpy as np
import concourse.bass as bass
import concourse.mybir as mybir
from concourse.bass_utils import run_bass_kernel_spmd

F32 = mybir.dt.float32
BF16 = mybir.dt.bfloat16
I32 = mybir.dt.int32
ALU = mybir.AluOpType
AF = mybir.ActivationFunctionType
AX = mybir.AxisListType

D = 1024
S = 8192
NT = 64
NG = 16
NOWN = 16
NE = 32
SX = S + 2 * NOWN * 128
NGX = SX // 512
NCST = 720
CAP = 2048
U32 = mybir.dt.uint32
EPS = 1e-5
C1 = 6.28125
C2 = 2 * math.pi - 6.28125
INV2PI = float(1.0 / (2 * math.pi))

OFF_QA, OFF_KA, OFF_VA, OFF_QD, OFF_KD, OFF_VD = 0, 512, 640, 768, 1280, 1792


def _swap64(cols):
    cols = np.asarray(cols).reshape(-1, 64)
    return np.concatenate([cols[:, 32:], cols[:, :32]], axis=1).reshape(-1)


def _unit_cols():
    units = []
    k = np.concatenate([np.tile(np.arange(OFF_KA + g * 64, OFF_KA + (g + 1) * 64), 2) for g in range(2)])
    q = np.arange(OFF_QA, OFF_QA + 512)
    v = np.arange(OFF_VA, OFF_VA + 128)
    units.append(dict(nk=2, nq=4, k=k, q=q, v=v))
    for h in range(4):
        k = np.arange(OFF_KD + h * 128, OFF_KD + (h + 1) * 128)
        q = np.arange(OFF_QD + h * 128, OFF_QD + (h + 1) * 128)
        v = np.arange(OFF_VD + h * 128, OFF_VD + (h + 1) * 128)
        units.append(dict(nk=1, nq=1, k=k, q=q, v=v))
    off = 0
    sel = []
    for u in units:
        u["base"] = off
        parts = [u["k"], _swap64(u["k"]), u["q"], _swap64(u["q"]), u["v"]]
        u["o_k"] = 0
        u["o_ks"] = len(u["k"])
        u["o_q"] = u["o_ks"] + len(u["k"])
        u["o_qs"] = u["o_q"] + len(u["q"])
        u["o_v"] = u["o_qs"] + len(u["q"])
        u["ncols"] = u["o_v"] + 128
        sel.append(np.concatenate(parts))
        off += u["ncols"]
    return units, np.concatenate(sel)


UNITS, SEL = _unit_cols()
NSEL = len(SEL)
NCH = NSEL // 128


def own_blocks(j):
    return sorted([8 * m + j for m in range(8)] + [8 * m + 7 - j for m in range(8)])


class Tr:
    __slots__ = ("w", "r")

    def __init__(self):
        self.w = {}
        self.r = {}


def trs(n):
    return [Tr() for _ in range(n)]


class Prog:
    ENG = ("pe", "act", "dve", "pool", "sp")

    def __init__(self, nc, stack, n_dma_sems=48):
        self.nc = nc
        self.q = {e: [] for e in self.ENG}
        self.esem = {e: stack.enter_context(nc.semaphore("s_" + e)) for e in self.ENG}
        self.ecnt = {e: 0 for e in self.ENG}
        self.waited = {e: {} for e in self.ENG}
        self.dsem = [stack.enter_context(nc.semaphore("d%d" % i)) for i in range(n_dma_sems)]
        self.dcnt = [0] * n_dma_sems
        self.dpool = {"sp": list(range(0, n_dma_sems - 16)), "pool": list(range(n_dma_sems - 16, n_dma_sems))}
        self.dnext = {"sp": 0, "pool": 0}
        self.in_cond = False
        self.handles = {"pe": nc.tensor, "act": nc.scalar, "dve": nc.vector, "pool": nc.gpsimd, "sp": nc.sync}

    def _need(self, eng, s, v):
        wd = self.waited[eng]
        if wd.get(s, 0) >= v:
            return
        wd[s] = v
        self.q[eng].append(("wait", s, v))

    def _waits(self, eng, reads, writes):
        need = {}
        for t in reads:
            for s, v in t.w.items():
                if need.get(s, 0) < v:
                    need[s] = v
        for t in writes:
            for s, v in t.w.items():
                if need.get(s, 0) < v:
                    need[s] = v
            for s, v in t.r.items():
                if need.get(s, 0) < v:
                    need[s] = v
        for s, v in need.items():
            if eng == "pe" and s is self.esem["pe"]:
                continue
            self._need(eng, s, v)

    def _record(self, ev, reads, writes):
        s, v = ev
        for t in reads:
            if t.r.get(s, 0) < v:
                t.r[s] = v
        for t in writes:
            if self.in_cond:
                if t.w.get(s, 0) < v:
                    t.w[s] = v
            else:
                t.w = {s: v}
                t.r = {}

    def op(self, eng, fn, reads=(), writes=()):
        self._waits(eng, reads, writes)
        self.ecnt[eng] += 1
        ev = (self.esem[eng], self.ecnt[eng])
        self.q[eng].append(("op", fn, self.esem[eng], 1))
        self._record(ev, reads, writes)

    def group(self, eng, fns, reads=(), writes=()):
        self._waits(eng, reads, writes)
        self.ecnt[eng] += 1
        ev = (self.esem[eng], self.ecnt[eng])
        for f in fns[:-1]:
            self.q[eng].append(("op", f, None, 0))
        self.q[eng].append(("op", fns[-1], self.esem[eng], 1))
        self._record(ev, reads, writes)

    def dma(self, eng, fn, reads=(), writes=(), slot=None):
        pl = self.dpool[eng]
        if slot is None:
            i = pl[self.dnext[eng]]
            self.dnext[eng] = (self.dnext[eng] + 1) % (len(pl) - 4)
        else:
            i = pl[len(pl) - 4 + slot]
        s = self.dsem[i]
        if self.dcnt[i]:
            self._need(eng, s, self.dcnt[i])
        self._waits(eng, reads, writes)
        self.dcnt[i] += 16
        ev = (s, self.dcnt[i])
        self.q[eng].append(("op", fn, s, 16))
        self._record(ev, reads, writes)
        return ev

    CENG = ("pe", "act", "dve", "sp")

    def regload(self, ap, reads=()):
        for e in self.CENG:
            self._waits(e, reads, ())
            self.q[e].append(("regload", ap))

    def cond_begin(self, thr):
        if not hasattr(self, "_cstack"):
            self._cstack = []
        self._cstack.append(({e: self.ecnt[e] for e in self.ENG}, list(self.dcnt), {e: dict(self.waited[e]) for e in self.ENG}))
        self.in_cond = True
        for e in self.CENG:
            self.q[e].append(["if", thr, None])

    def cond_end(self):
        ec0, dc0, wd0 = self._cstack.pop()
        assert self.ecnt["pool"] == ec0["pool"], "pool must stay outside conditional regions"
        dd = [(i, self.dcnt[i] - dc0[i]) for i in range(len(self.dcnt)) if self.dcnt[i] != dc0[i]]
        for i, _ in dd:
            assert i in self.dpool["sp"]
        for e in self.CENG:
            comp = []
            if self.ecnt[e] != ec0[e]:
                comp.append((self.esem[e], self.ecnt[e] - ec0[e]))
            if e == "sp":
                comp += [(self.dsem[i], d, dc0[i]) for i, d in dd]
            for it in reversed(self.q[e]):
                if isinstance(it, list) and it[0] == "if" and it[2] is None:
                    it[2] = comp
                    break
            self.q[e].append(("endif",))
            self.waited[e] = wd0[e]
        self.waited["pool"] = wd0["pool"]
        self.in_cond = bool(self._cstack)

    def barrier(self):
        for e in self.ENG:
            for f in self.ENG:
                if f != e and self.ecnt[f]:
                    self._need(e, self.esem[f], self.ecnt[f])
            for i, s in enumerate(self.dsem):
                if self.dcnt[i]:
                    self._need(e, s, self.dcnt[i])

    def emit(self):
        nc = self.nc
        q = self.q
        self.q = {e: [] for e in self.ENG}
        if not hasattr(self, "regs"):
            self.regs = {}
        with nc.Block() as block:
            def run_items(h, ename, items):
                i = 0
                n = len(items)
                while i < n:
                    it = items[i]
                    k = it[0]
                    if k == "wait":
                        h.wait_ge(it[1], it[2])
                    elif k == "op":
                        ins = it[1]()
                        if it[2] is not None:
                            ins.then_inc(it[2], it[3])
                    elif k == "regload":
                        if ename not in self.regs:
                            self.regs[ename] = h.alloc_register("cnt_" + ename)
                        h.reg_load(self.regs[ename], it[1])
                    elif k == "if":
                        depth = 1
                        j = i + 1
                        while True:
                            if items[j][0] == "if":
                                depth += 1
                            elif items[j][0] == "endif":
                                depth -= 1
                                if depth == 0:
                                    break
                            j += 1
                        body = items[i + 1:j]
                        with h.If_lt(self.regs[ename], it[1]):
                            h.drain()
                            for cp in it[2]:
                                if len(cp) == 3 and cp[2]:
                                    h.wait_ge(cp[0], cp[2])
                                h.sem_inc(cp[0], cp[1])
                        with h.Else():
                            run_items(h, ename, body)
                        i = j
                    i += 1

            def run(ename):
                run_items(self.handles[ename], ename, q[ename])

            @block.tensor
            def _(e):
                run("pe")

            @block.scalar
            def _(e):
                run("act")

            @block.vector
            def _(e):
                run("dve")

            @block.gpsimd
            def _(e):
                run("pool")

            @block.sync
            def _(e):
                run("sp")


def build_program(j_core_unused=None, debug=False):
    nc = bass.Bass("TRN2", target_bir_lowering=False)
    din = lambda name, shape, dt=F32: nc.dram_tensor(name, list(shape), dt, kind="ExternalInput").ap()
    x_d = din("x", [SX, D])
    pos_d = din("pos", [1, SX], I32)
    dmask_d = din("dmask", [128, NOWN * 512])
    smask_d = din("smask", [128, NOWN * 256])
    cT_d = din("cT", [128, 8])
    wada_d = din("w_ada", [D, 6 * D])
    bada_d = din("b_ada", [1, 6 * D])
    gmixT_d = din("gmixT", [128, 8])
    gffnT_d = din("gffnT", [128, 8])
    wsel_d = din("w_sel", [D, NSEL])
    bselT_d = din("b_selT", [128, NCH])
    bsel_d = din("b_sel", [1, NSEL])
    sinks_d = din("sinks", [1, 8])
    lam_d = din("lam4", [4, 64])
    gsub_d = din("g_subln", [1, 128])
    wout_d = din("w_out", [D, D])
    bout_d = din("b_out", [1, D])
    wr_d = din("w_router", [D, NE])
    br_d = din("b_router", [1, NE])
    w1_d = din("w1", [NE, D, 2 * D])
    b1_d = din("b1", [NE, 2 * D])
    gffn_d = din("g_ffn", [1, D])
    gmix_d = din("g_mix", [1, D])
    w2_d = din("w2", [NE, D, D])
    b2_d = din("b2", [NE, D])
    gfin_d = din("g_final", [1, D])
    cst_d = din("consts", [128, NCST])
    out_d = nc.dram_tensor("out", [NOWN * 128, D], F32, kind="ExternalOutput").ap()
    hT_d = nc.dram_tensor("hT_scr", [8, 128, SX], BF16, kind="Internal").ap()
    cos_d = nc.dram_tensor("cos_scr", [128, SX], F32, kind="Internal").ap()
    sin_d = nc.dram_tensor("sin_scr", [128, SX], F32, kind="Internal").ap()
    x1_d = nc.dram_tensor("x1_scr", [NOWN * 128, D], F32, kind="Internal").ap()
    mod_d = nc.dram_tensor("mod_scr", [1, 6 * D], F32, kind="Internal").ap()
    xbuf_d = nc.dram_tensor("xbuf_scr", [NE * CAP + 128, D], BF16, kind="Internal").ap()
    ybuf_d = nc.dram_tensor("ybuf_scr", [NE * CAP, D], F32, kind="Internal").ap()


    with ExitStack() as st:
        P = Prog(nc, st)
        sbuf = lambda stack, name, shape, dt=F32: stack.enter_context(nc.sbuf_tensor(name, list(shape), dt))
        V, A, T, G_ = nc.vector, nc.scalar, nc.tensor, nc.gpsimd

        bank = [st.enter_context(nc.psum_tensor("bank%d" % i, [128, 512], F32)) for i in range(8)]
        tb = trs(8)

        cst = sbuf(st, "cst", [128, NCST]); t_cst = Tr()
        identb = sbuf(st, "identb", [128, 128], BF16)
        mask256 = sbuf(st, "mask256", [128, 256], BF16)
        onesb = sbuf(st, "onesb", [1, 128], BF16)
        trib = sbuf(st, "trib", [128, 128], BF16)
        ones128b = sbuf(st, "ones128b", [128, 128], BF16)
        A1 = sbuf(st, "A1", [128, 8]); S1 = sbuf(st, "S1", [128, 8])
        A2 = sbuf(st, "A2", [128, 8]); S2 = sbuf(st, "S2", [128, 8])
        t_mod = Tr()
        t_mixed = trs(NOWN)
        small = sbuf(st, "small", [128, 64]); t_small = Tr()
        ident = cst[:, 0:128]
        invf = cst[:, 384:385]
        sgn = cst[:, 385:386]
        halfpi = cst[:, 386:387]
        zero_c = cst[:, 387:388]
        one11 = cst[0:1, 388:389]
        ones_row = cst[0:1, 392:520]
        neglam = small[:, 0:1]
        expsink = small[:, 8:16]

        P.dma("sp", lambda: nc.sync.dma_start(out=cst[:], in_=cst_d[:, :]), writes=[t_cst])
        P.op("dve", lambda: V.tensor_copy(out=identb[:], in_=cst[:, 0:128]), reads=[t_cst], writes=[t_cst])
        P.op("dve", lambda: V.tensor_copy(out=mask256[:, 0:128], in_=cst[:, 256:384]), reads=[t_cst], writes=[t_cst])
        P.op("dve", lambda: V.tensor_copy(out=mask256[:, 128:256], in_=cst[:, 128:256]), reads=[t_cst], writes=[t_cst])
        P.op("dve", lambda: V.tensor_copy(out=onesb[:], in_=cst[0:1, 392:520]), reads=[t_cst], writes=[t_cst])
        P.op("dve", lambda: V.tensor_copy(out=trib[:], in_=cst[:, 520:648]), reads=[t_cst], writes=[t_cst])
        P.op("dve", lambda: V.tensor_copy(out=ones128b[:], in_=cst[:, 392:520]), reads=[t_cst], writes=[t_cst])

        with ExitStack() as s0:
            cT = sbuf(s0, "cT_sb", [128, 8]); t_cT = Tr()
            wad = [sbuf(s0, "wad%d" % i, [128, 8, 512]) for i in range(2)]; t_wad = trs(2)
            modrow = sbuf(s0, "modrow", [1, 6 * D]); t_modrow = Tr()
            badar = sbuf(s0, "badar", [1, 6 * D]); t_bada = Tr()
            gT = sbuf(s0, "gT", [128, 16]); t_gT = Tr()
            lamb = sbuf(s0, "lamb", [128, 256]); t_lam = Tr()
            lamp = sbuf(s0, "lamp", [128, 128])
            P.dma("sp", lambda: nc.sync.dma_start(out=cT[:], in_=cT_d[:, :]), writes=[t_cT])
            P.dma("sp", lambda: nc.sync.dma_start(out=badar[:], in_=bada_d[:, :]), writes=[t_bada])
            P.dma("sp", lambda: nc.sync.dma_start(out=gT[:, 0:8], in_=gmixT_d[:, :]), writes=[t_gT])
            P.dma("sp", lambda: nc.sync.dma_start(out=gT[:, 8:16], in_=gffnT_d[:, :]), writes=[t_gT])
            P.dma("sp", lambda: nc.sync.dma_start(out=lamb[:].rearrange("p (a b) -> p a b", a=4),
                                                  in_=lam_d[:, :].partition_broadcast(128)), writes=[t_lam])
            P.dma("sp", lambda: nc.sync.dma_start(out=small[:, 16:24], in_=sinks_d[0:1, :].partition_broadcast(128)), writes=[t_small])
            posi = sbuf(s0, "posi", [128, 512], I32); t_posi = Tr()
            ang = sbuf(s0, "ang", [128, 512]); t_ang = Tr()
            ki2 = [sbuf(s0, "ki%d" % i, [128, 512], I32) for i in range(2)]; kf2 = [sbuf(s0, "kf%d" % i, [128, 512]) for i in range(2)]; t_k2 = trs(2)
            rr2 = [sbuf(s0, "rr%d" % i, [128, 512]) for i in range(2)]; t_rr2 = trs(2)
            tab = [sbuf(s0, "tab%d" % i, [128, 512]) for i in range(4)]; t_tab = trs(4)
            def rope_group(g):
                P.dma("sp", lambda: nc.sync.dma_start(out=posi[:], in_=pos_d[0:1, g * 512:(g + 1) * 512].partition_broadcast(128)), writes=[t_posi])
                P.op("dve", lambda: V.tensor_copy(out=ang[:], in_=posi[:]), reads=[t_posi], writes=[t_ang])
                P.op("dve", lambda: V.tensor_scalar(out=ang[:], in0=ang[:], scalar1=invf, scalar2=None, op0=ALU.mult), reads=[t_ang, t_cst], writes=[t_ang])
                def chain(which):
                    tbi = (2 * g + which) % 4
                    ki, kf, rr, t_k, t_rr = ki2[which], kf2[which], rr2[which], t_k2[which], t_rr2[which]
                    if which == 0:
                        P.op("dve", lambda: V.tensor_scalar(out=ki[:], in0=ang[:], scalar1=INV2PI, scalar2=None, op0=ALU.mult), reads=[t_ang], writes=[t_k])
                    else:
                        P.op("dve", lambda: V.tensor_scalar(out=ki[:], in0=ang[:], scalar1=INV2PI, scalar2=0.25, op0=ALU.mult, op1=ALU.add), reads=[t_ang], writes=[t_k])
                    P.op("dve", lambda: V.tensor_copy(out=kf[:], in_=ki[:]), reads=[t_k], writes=[t_k])
                    P.op("dve", lambda: V.scalar_tensor_tensor(out=rr[:], in0=kf[:], scalar=-C1, in1=ang[:], op0=ALU.mult, op1=ALU.add), reads=[t_k, t_ang], writes=[t_rr])
                    P.op("dve", lambda: V.scalar_tensor_tensor(out=rr[:], in0=kf[:], scalar=-C2, in1=rr[:], op0=ALU.mult, op1=ALU.add), reads=[t_k, t_rr], writes=[t_rr])
                    if which == 0:
                        P.op("dve", lambda: V.tensor_scalar(out=rr[:], in0=rr[:], scalar1=-3.1415925, scalar2=3.1415925, op0=ALU.max, op1=ALU.min), reads=[t_rr], writes=[t_rr])
                        P.op("act", lambda tbi=tbi: A.activation(out=tab[tbi][:], in_=rr[:], func=AF.Sin, scale=sgn, bias=zero_c), reads=[t_rr, t_cst], writes=[t_tab[tbi]])
                        P.dma("sp", lambda tbi=tbi: nc.sync.dma_start(out=sin_d[:, g * 512:(g + 1) * 512], in_=tab[tbi][:]), reads=[t_tab[tbi]])
                    else:
                        P.op("dve", lambda: V.tensor_scalar(out=rr[:], in0=rr[:], scalar1=-4.712388, scalar2=1.570796, op0=ALU.max, op1=ALU.min), reads=[t_rr], writes=[t_rr])
                        P.op("act", lambda tbi=tbi: A.activation(out=tab[tbi][:], in_=rr[:], func=AF.Sin, scale=1.0, bias=halfpi), reads=[t_rr, t_cst], writes=[t_tab[tbi]])
                        P.dma("sp", lambda tbi=tbi: nc.sync.dma_start(out=cos_d[:, g * 512:(g + 1) * 512], in_=tab[tbi][:]), reads=[t_tab[tbi]])
                chain(0)
                chain(1)
            for g in range(NGX):
                rope_group(g)

            P.op("act", lambda: A.activation(out=cT[:], in_=cT[:], func=AF.Silu), reads=[t_cT], writes=[t_cT])
            wada_v = wada_d.rearrange("(k p) n -> p k n", p=128)
            for pc in range(12):
                b = pc % 2
                P.dma("sp", lambda pc=pc, b=b: nc.sync.dma_start(out=wad[b][:], in_=wada_v[:, :, pc * 512:(pc + 1) * 512]), writes=[t_wad[b]])
                bk = pc % 2
                P.group("pe", [(lambda kc=kc, b=b, bk=bk: T.matmul(bank[bk][0:1, :], lhsT=cT[:, kc:kc + 1], rhs=wad[b][:, kc, :],
                                                                    start=(kc == 0), stop=(kc == 7))) for kc in range(8)],
                        reads=[t_cT, t_wad[b]], writes=[tb[bk]])
                P.op("dve", lambda pc=pc, bk=bk: V.tensor_tensor(out=modrow[0:1, pc * 512:(pc + 1) * 512], in0=bank[bk][0:1, :],
                                                                 in1=badar[0:1, pc * 512:(pc + 1) * 512], op=ALU.add),
                     reads=[tb[bk], t_bada], writes=[t_modrow])
            cols = [(0, 0), (1, 8), (3, 16), (4, 24)]
            fns = []
            for mi, dc in cols:
                for kc in range(8):
                    fns.append(lambda mi=mi, dc=dc, kc=kc: T.matmul(bank[2][:, dc + kc:dc + kc + 1],
                                                                    lhsT=modrow[0:1, mi * D + kc * 128: mi * D + (kc + 1) * 128],
                                                                    rhs=one11, start=True, stop=True))
            P.group("pe", fns, reads=[t_modrow, t_cst], writes=[tb[2]])
            P.op("dve", lambda: V.tensor_copy(out=S1[:], in_=bank[2][:, 0:8]), reads=[tb[2]], writes=[t_mod])
            P.op("dve", lambda: V.scalar_tensor_tensor(out=A1[:], in0=bank[2][:, 8:16], scalar=1.0, in1=gT[:, 0:8], op0=ALU.add, op1=ALU.mult),
                 reads=[tb[2], t_gT], writes=[t_mod])
            P.op("dve", lambda: V.tensor_copy(out=S2[:], in_=bank[2][:, 16:24]), reads=[tb[2]], writes=[t_mod])
            P.op("dve", lambda: V.scalar_tensor_tensor(out=A2[:], in0=bank[2][:, 24:32], scalar=1.0, in1=gT[:, 8:16], op0=ALU.add, op1=ALU.mult),
                 reads=[tb[2], t_gT], writes=[t_mod])
            P.dma("sp", lambda: nc.sync.dma_start(out=mod_d[:, :], in_=modrow[:]), reads=[t_modrow])
            P.op("dve", lambda: V.tensor_tensor(out=lamp[:, 0:64], in0=lamb[:, 0:64], in1=lamb[:, 64:128], op=ALU.mult), reads=[t_lam], writes=[t_lam])
            P.op("dve", lambda: V.tensor_tensor(out=lamp[:, 64:128], in0=lamb[:, 128:192], in1=lamb[:, 192:256], op=ALU.mult), reads=[t_lam], writes=[t_lam])
            P.op("dve", lambda: V.tensor_reduce(out=small[:, 1:3], in_=lamp[:].rearrange("p (a b) -> p a b", a=2), axis=AX.X, op=ALU.add),
                 reads=[t_lam], writes=[t_small])
            P.op("act", lambda: A.activation(out=small[:, 1:3], in_=small[:, 1:3], func=AF.Exp), reads=[t_small], writes=[t_small])
            P.op("dve", lambda: V.scalar_tensor_tensor(out=small[:, 0:1], in0=small[:, 2:3], scalar=-0.2, in1=small[:, 1:2], op0=ALU.add, op1=ALU.subtract),
                 reads=[t_small], writes=[t_small])
            P.op("act", lambda: A.activation(out=small[:, 8:16], in_=small[:, 16:24], func=AF.Exp), reads=[t_small], writes=[t_small])
            P.barrier()
            P.emit()

        with ExitStack() as s1:
            XB = 8
            xt = [sbuf(s1, "xt%d" % i, [128, D]) for i in range(XB)]; t_xt = trs(XB)
            xn = [sbuf(s1, "xn%d" % i, [128, D], BF16) for i in range(2)]; t_xn = trs(2)
            junk = sbuf(s1, "junk", [128, D], BF16); t_junk = Tr()
            ssq = sbuf(s1, "ssq", [128, 2, 8]); t_ssq = trs(2)
            hTg = [sbuf(s1, "hTg%d" % i, [128, 8, 512], BF16) for i in range(2)]; t_hTg = trs(2)
            hT_v = hT_d.rearrange("k p t -> p k t")
            A1b = sbuf(s1, "A1b", [128, D]); S1b = sbuf(s1, "S1b", [128, D]); gmb = sbuf(s1, "gmb", [128, D]); t_m1 = Tr()
            xm = [sbuf(s1, "xm%d" % i, [128, D]) for i in range(2)]; t_xm = trs(2)
            P.dma("sp", lambda: nc.sync.dma_start(out=S1b[:], in_=mod_d[0:1, 0:D].partition_broadcast(128)), writes=[t_m1])
            P.dma("sp", lambda: nc.sync.dma_start(out=A1b[:], in_=mod_d[0:1, D:2 * D].partition_broadcast(128)), writes=[t_m1])
            P.dma("sp", lambda: nc.sync.dma_start(out=gmb[:], in_=gmix_d[0:1, :].partition_broadcast(128)), writes=[t_m1])
            P.op("dve", lambda: V.scalar_tensor_tensor(out=A1b[:], in0=A1b[:], scalar=1.0, in1=gmb[:], op0=ALU.add, op1=ALU.mult), reads=[t_m1], writes=[t_m1])

            def stageA(g):
                gp = g % 2
                for tt in range(4):
                    t = 4 * g + tt
                    xb = t % XB
                    P.dma("sp", lambda t=t, xb=xb: nc.sync.dma_start(out=xt[xb][:], in_=x_d[t * 128:(t + 1) * 128, :]), writes=[t_xt[xb]])
                    P.op("act", lambda xb=xb, tt=tt: A.activation(out=junk[:], in_=xt[xb][:], func=AF.Square, accum_out=ssq[:, gp, tt:tt + 1]),
                         reads=[t_xt[xb]], writes=[t_junk, t_ssq[gp]])

            def stageA2(g):
                gp = g % 2
                P.op("dve", lambda: V.tensor_scalar(out=ssq[:, gp, 4:8], in0=ssq[:, gp, 0:4], scalar1=1.0 / D, scalar2=EPS, op0=ALU.mult, op1=ALU.add), reads=[t_ssq[gp]], writes=[t_ssq[gp]])
                P.op("act", lambda: A.activation(out=ssq[:, gp, 4:8], in_=ssq[:, gp, 4:8], func=AF.Sqrt), reads=[t_ssq[gp]], writes=[t_ssq[gp]])
                P.op("dve", lambda: V.reciprocal(out=ssq[:, gp, 4:8], in_=ssq[:, gp, 4:8]), reads=[t_ssq[gp]], writes=[t_ssq[gp]])

            def stageB(g):
                gp = g % 2
                hb = g % 2

                def tile_b(tt):
                    t = 4 * g + tt
                    xb = t % XB
                    nb = t % 2
                    P.op("dve", lambda: V.scalar_tensor_tensor(out=xm[nb][:], in0=xt[xb][:], scalar=ssq[:, gp, 4 + tt:5 + tt], in1=A1b[:], op0=ALU.mult, op1=ALU.mult),
                         reads=[t_xt[xb], t_ssq[gp], t_m1], writes=[t_xm[nb]])
                    P.op("dve", lambda: V.tensor_tensor(out=xn[nb][:], in0=xm[nb][:], in1=S1b[:], op=ALU.add), reads=[t_xm[nb], t_m1], writes=[t_xn[nb]])
                    bk = nb
                    pT = bank[bk][:, :].bitcast(BF16)
                    P.group("pe", [(lambda kc=kc: T.transpose(out=pT[:, kc * 128:(kc + 1) * 128], in_=xn[nb][:, kc * 128:(kc + 1) * 128], identity=identb[:]))
                                   for kc in range(8)], reads=[t_xn[nb], t_cst], writes=[tb[bk]])
                    P.op("act", lambda: A.activation(out=hTg[hb][:, :, tt * 128:(tt + 1) * 128], in_=pT[:, :].rearrange("p (a b) -> p a b", a=8), func=AF.Copy),
                         reads=[tb[bk]], writes=[t_hTg[hb]])
                for tt in range(4):
                    tile_b(tt)
                P.dma("sp", lambda: nc.sync.dma_start(out=hT_v[:, :, g * 512:(g + 1) * 512], in_=hTg[hb][:]), reads=[t_hTg[hb]])

            for g in range(NGX + 1):
                if g < NGX:
                    stageA(g)
                if g >= 1:
                    stageB(g - 1)
                if g < NGX:
                    stageA2(g)
            P.barrier()
            P.emit()

        s34 = st.enter_context(ExitStack())
        dest_i = sbuf(s34, "dest_i", [128, 4 * NOWN], I32); t_dest = trs(NOWN)
        gate4 = sbuf(s34, "gate4", [128, 4 * NOWN]); t_gate4 = trs(NOWN)
        maskb = sbuf(s34, "maskb", [128, NOWN, NE], BF16); t_maskb = trs(NOWN)
        cnt_run = sbuf(s34, "cnt_run", [128, NE]); t_cnt = Tr()
        cnt_i = sbuf(s34, "cnt_i", [1, NE], I32); t_cnti = Tr()
        padidx = sbuf(s34, "padidx", [128, NE], I32); t_pad = Tr()
        t_xbuf = Tr()
        iota32 = cst[:, 648:680]
        e2048 = cst[:, 680:712]
        iota_p = cst[:, 712:713]
        sA = ExitStack()
        bufA = sbuf(sA, "bufA", [128, 16 * 1024], BF16)
        mixed = bufA[:].rearrange("p (a b) -> p a b", a=NOWN)
        with ExitStack() as s2:
            Wu = sbuf(s2, "Wu", [128, 8, 1664], BF16); t_Wu = Tr()
            KT = sbuf(s2, "KT", [128, S], BF16); t_KT = Tr()
            Vb = sbuf(s2, "Vb", [128, 64 * 130], BF16); t_V = Tr()
            QT = sbuf(s2, "QT", [128, 4, NOWN * 128], BF16); t_QT = Tr()
            hTg = [sbuf(s2, "hTg2_%d" % i, [128, 8, 512], BF16) for i in range(2)]; t_hTg = trs(2)
            csg = [sbuf(s2, "csg%d" % i, [128, 2, 512]) for i in range(2)]; t_csg = trs(2)
            tm1 = [sbuf(s2, "tm1_%d" % i, [128, 512]) for i in range(2)]; t_tm1 = trs(2)
            tm2 = [sbuf(s2, "tm2_%d" % i, [128, 512]) for i in range(2)]; t_tm2 = trs(2)
            PT = [sbuf(s2, "PT%d" % i, [128, 512], BF16) for i in range(3)]; t_PT = trs(3)
            dmask = sbuf(s2, "dmask_sb", [128, NOWN, 512], BF16); t_dmask = Tr()
            smask = sbuf(s2, "smask_sb", [128, NOWN, 256], BF16); t_smask = Tr()
            bselT = sbuf(s2, "bselT", [128, NCH]); t_bsel = Tr()
            vbias = sbuf(s2, "vbias", [128, 128]); t_vbias = Tr()
            gsub_b = sbuf(s2, "gsub_b", [128, 128]); t_gsub = Tr()
            fin = sbuf(s2, "fin", [128, 8 * 128]); t_fin = Tr()
            fsm = sbuf(s2, "fsm", [128, 32]); t_fsm = Tr()
            junk2 = sbuf(s2, "junk2", [128, 128], BF16)
            hT_v = hT_d.rearrange("k p t -> p k t")
            wsel_v = wsel_d.rearrange("(k p) n -> p k n", p=128)
            for q4 in range(4):
                P.dma("pool", lambda q4=q4: G_.dma_start(out=dmask[:, 4 * q4:4 * q4 + 4, :], in_=dmask_d[:, q4 * 2048:(q4 + 1) * 2048].rearrange("p (a b) -> p a b", a=4)),
                      writes=[t_dmask])
            for q4 in range(2):
                P.dma("pool", lambda q4=q4: G_.dma_start(out=smask[:, 8 * q4:8 * q4 + 8, :], in_=smask_d[:, q4 * 2048:(q4 + 1) * 2048].rearrange("p (a b) -> p a b", a=8)),
                      writes=[t_smask])
            P.dma("sp", lambda: nc.sync.dma_start(out=bselT[:], in_=bselT_d[:, :]), writes=[t_bsel])
            P.dma("sp", lambda: nc.sync.dma_start(out=gsub_b[:], in_=gsub_d[0:1, :].partition_broadcast(128)), writes=[t_gsub])
            P.op("dve", lambda: V.tensor_scalar(out=gsub_b[:], in0=gsub_b[:], scalar1=0.8, scalar2=None, op0=ALU.mult), reads=[t_gsub], writes=[t_gsub])
            gcount = [0]

            def rope_proj(u, wc, wcs, hb, cb, ccol, ncol, dst, t_dst, par):
                bA, bB = bank[2 * par], bank[2 * par + 1]
                ci = (u["base"] + wc) // 128
                cis = (u["base"] + wcs) // 128
                P.group("pe", [(lambda kc=kc: T.matmul(bA[:, 0:ncol], lhsT=Wu[:, kc, wc:wc + 128], rhs=hTg[hb][:, kc, ccol:ccol + ncol], start=(kc == 0), stop=(kc == 7)))
                               for kc in range(8)], reads=[t_Wu, t_hTg[hb]], writes=[tb[2 * par]])
                P.group("pe", [(lambda kc=kc: T.matmul(bB[:, 0:ncol], lhsT=Wu[:, kc, wcs:wcs + 128], rhs=hTg[hb][:, kc, ccol:ccol + ncol], start=(kc == 0), stop=(kc == 7)))
                               for kc in range(8)], reads=[t_Wu, t_hTg[hb]], writes=[tb[2 * par + 1]])
                P.op("dve", lambda: V.scalar_tensor_tensor(out=tm1[par][:, 0:ncol], in0=bA[:, 0:ncol], scalar=bselT[:, ci:ci + 1], in1=csg[cb][:, 0, ccol:ccol + ncol],
                                                           op0=ALU.add, op1=ALU.mult), reads=[tb[2 * par], t_bsel, t_csg[cb]], writes=[t_tm1[par]])
                P.op("dve", lambda: V.scalar_tensor_tensor(out=tm2[par][:, 0:ncol], in0=bB[:, 0:ncol], scalar=bselT[:, cis:cis + 1], in1=csg[cb][:, 1, ccol:ccol + ncol],
                                                           op0=ALU.add, op1=ALU.mult), reads=[tb[2 * par + 1], t_bsel, t_csg[cb]], writes=[t_tm2[par]])
                P.op("dve", lambda: V.tensor_tensor(out=dst, in0=tm1[par][:, 0:ncol], in1=tm2[par][:, 0:ncol], op=ALU.add),
                     reads=[t_tm1[par], t_tm2[par]], writes=[t_dst])

            def load_group(g):
                hb = gcount[0] % 2
                gcount[0] += 1
                P.dma("sp", lambda: nc.sync.dma_start(out=hTg[hb][:], in_=hT_v[:, :, g * 512:(g + 1) * 512]), writes=[t_hTg[hb]])
                P.dma("sp", lambda: nc.sync.dma_start(out=csg[hb][:, 0, :], in_=cos_d[:, g * 512:(g + 1) * 512]), writes=[t_csg[hb]])
                P.dma("sp", lambda: nc.sync.dma_start(out=csg[hb][:, 1, :], in_=sin_d[:, g * 512:(g + 1) * 512]), writes=[t_csg[hb]])
                return hb

            pcount = [0]

            def v_proj(u, hb, vt0, vw, swa):
                bk = 4 + (pcount[0] % 2)
                pcount[0] += 1
                ov = u["o_v"]
                fns = []
                for tt in range(4):
                    for kc in range(8):
                        fns.append(lambda tt=tt, kc=kc: T.matmul(bank[bk][:, tt * 128:(tt + 1) * 128], lhsT=hTg[hb][:, kc, tt * 128:(tt + 1) * 128],
                                                                 rhs=Wu[:, kc, ov:ov + 128], start=(kc == 0), stop=(kc == 7)))
                P.group("pe", fns, reads=[t_Wu, t_hTg[hb]], writes=[tb[bk]])
                src = bank[bk][:, :].rearrange("p (a b) -> p a b", a=4)
                vb_b = vbias[:].unsqueeze(1).to_broadcast([128, 4, 128])
                if not swa:
                    dst = Vb[:, vt0 * 129:(vt0 + 4) * 129].rearrange("p (a b) -> p a b", a=4)[:, :, 0:128]
                    P.op("dve", lambda: V.tensor_tensor(out=dst, in0=src, in1=vb_b, op=ALU.add), reads=[tb[bk], t_vbias], writes=[t_V])
                else:
                    for kv in range(2):
                        dst = Vb[:, vt0 * 130:(vt0 + 4) * 130].rearrange("p (a b) -> p a b", a=4)[:, :, kv * 65:kv * 65 + 64]
                        P.op("dve", lambda dst=dst, kv=kv: V.tensor_tensor(out=dst, in0=src[:, :, kv * 64:(kv + 1) * 64],
                                                                          in1=vbias[:, kv * 64:(kv + 1) * 64].unsqueeze(1).to_broadcast([128, 4, 64]), op=ALU.add),
                             reads=[tb[bk], t_vbias], writes=[t_V])

            for ui, u in enumerate(UNITS):
                swa = (ui == 0)
                nc_u = u["ncols"]
                P.dma("pool", lambda u=u, nc_u=nc_u: G_.dma_start(out=Wu[:, :, 0:nc_u], in_=wsel_v[:, :, u["base"]:u["base"] + nc_u]), writes=[t_Wu])
                P.dma("sp", lambda u=u: nc.sync.dma_start(out=vbias[:], in_=bsel_d[0:1, u["base"] + u["o_v"]:u["base"] + u["o_v"] + 128].partition_broadcast(128)),
                      writes=[t_vbias])
                if swa:
                    vv = Vb[:, 0:32 * 130].rearrange("p (a b) -> p a b", a=32)
                    P.op("pool", lambda vv=vv: G_.memset(vv[:, :, 64:65], 1.0), writes=[t_V])
                    P.op("pool", lambda vv=vv: G_.memset(vv[:, :, 129:130], 1.0), writes=[t_V])
                    kv_groups = [(20 + i, i * 512, 4 * i) for i in range(4)] + [(16 + i, 2048 + i * 512, 16 + 4 * i) for i in range(4)]
                elif ui == 1:
                    vv = Vb[:, 0:64 * 129].rearrange("p (a b) -> p a b", a=64)
                    P.op("pool", lambda vv=vv: G_.memset(vv[:, :, 128:129], 1.0), writes=[t_V])
                    kv_groups = [(g, g * 512, 4 * g) for g in range(NG)]
                else:
                    kv_groups = [(g, g * 512, 4 * g) for g in range(NG)]
                par = 0
                for (g, kcol, vt0) in kv_groups:
                    hb = load_group(g)
                    for kc_ in range(u["nk"]):
                        rope_proj(u, u["o_k"] + kc_ * 128, u["o_ks"] + kc_ * 128, hb, hb, 0, 512, KT[:, kc_ * 4096 + kcol:kc_ * 4096 + kcol + 512], t_KT, par)
                        par ^= 1
                    v_proj(u, hb, vt0, None, swa)
                    if swa and g < 20:
                        for qc in range(4):
                            rope_proj(u, u["o_q"] + qc * 128, u["o_qs"] + qc * 128, hb, hb, 0, 512, QT[:, qc, (g - 16) * 512:(g - 15) * 512], t_QT, par)
                            par ^= 1
                if not swa:
                    for g in range(16, 20):
                        hb = load_group(g)
                        rope_proj(u, u["o_q"], u["o_qs"], hb, hb, 0, 512, QT[:, 0, (g - 16) * 512:(g - 15) * 512], t_QT, par)
                        par ^= 1

                items = []
                if swa:
                    for oi in range(NOWN):
                        for hh in range(8):
                            items.append((oi, hh, 0, True))
                else:
                    for oi in range(NOWN):
                        nkb = 8 * (oi // 2) + (4 if oi % 2 == 0 else 8)
                        for m in range(2):
                            for c in range(nkb // 4):
                                items.append((oi, m, c, c == nkb // 4 - 1))

                def qk(n):
                    oi, a, c, last = items[n]
                    sb_ = n % 3
                    if swa:
                        hh = a; half = hh % 2; qc = hh // 2; kvg = hh // 4
                        ps = slice(half * 64, half * 64 + 64)
                        fns = [lambda: T.matmul(bank[sb_][:, 0:128], lhsT=KT[ps, kvg * 4096 + oi * 128:kvg * 4096 + (oi + 1) * 128], rhs=QT[ps, qc, oi * 128:(oi + 1) * 128], start=True, stop=True),
                               lambda: T.matmul(bank[sb_][:, 128:256], lhsT=KT[ps, kvg * 4096 + 2048 + oi * 128:kvg * 4096 + 2048 + (oi + 1) * 128], rhs=QT[ps, qc, oi * 128:(oi + 1) * 128], start=True, stop=True)]
                        ncol = 256
                        mk = smask[:, oi, :]
                        t_mk = t_smask
                    else:
                        m = a
                        ps = slice(m * 64, m * 64 + 64)
                        fns = [(lambda i=i: T.matmul(bank[sb_][:, i * 128:(i + 1) * 128], lhsT=KT[ps, (4 * c + i) * 128:(4 * c + i + 1) * 128],
                                                     rhs=QT[ps, 0, oi * 128:(oi + 1) * 128], start=True, stop=True)) for i in range(4)]
                        ncol = 512
                        mk = dmask[:, oi, :]
                        t_mk = t_dmask
                    P.group("pe", fns, reads=[t_KT, t_QT], writes=[tb[sb_]])
                    P.op("act", lambda: A.activation(out=PT[sb_][:, 0:ncol], in_=bank[sb_][:, 0:ncol], func=AF.Exp, scale=0.125), reads=[tb[sb_]], writes=[t_PT[sb_]])
                    if last:
                        P.op("pool", lambda: G_.tensor_tensor(out=PT[sb_][:, 0:ncol], in0=PT[sb_][:, 0:ncol], in1=mk, op=ALU.mult), reads=[t_PT[sb_], t_mk], writes=[t_PT[sb_]])

                def pv(n):
                    oi, a, c, last = items[n]
                    sb_ = n % 3
                    if swa:
                        hh = a; kvg = hh // 4
                        ob = 3 + (oi % 2) * 2 + (hh // 4)
                        oc = (hh % 4) * 65
                        fns = [lambda: T.matmul(bank[ob][:, oc:oc + 65], lhsT=PT[sb_][:, 0:128], rhs=Vb[:, oi * 130 + kvg * 65: oi * 130 + kvg * 65 + 65], start=True, stop=False),
                               lambda: T.matmul(bank[ob][:, oc:oc + 65], lhsT=PT[sb_][:, 128:256], rhs=Vb[:, (16 + oi) * 130 + kvg * 65: (16 + oi) * 130 + kvg * 65 + 65], start=False, stop=True)]
                    else:
                        m = a
                        ob = 3 + (oi % 2) * 2 + m
                        fns = [(lambda i=i: T.matmul(bank[ob][:, 0:129], lhsT=PT[sb_][:, i * 128:(i + 1) * 128], rhs=Vb[:, (4 * c + i) * 129:(4 * c + i + 1) * 129],
                                                     start=(c == 0 and i == 0), stop=(last and i == 3))) for i in range(4)]
                    P.group("pe", fns, reads=[t_PT[sb_], t_V], writes=[tb[ob]])
                    if swa and a == 7:
                        for hh in range(8):
                            ob2 = 3 + (oi % 2) * 2 + (hh // 4)
                            oc2 = (hh % 4) * 65
                            P.op("dve", lambda hh=hh, ob2=ob2, oc2=oc2: V.tensor_tensor(out=fsm[:, hh:hh + 1], in0=bank[ob2][:, oc2 + 64:oc2 + 65], in1=expsink[:, hh:hh + 1], op=ALU.add),
                                 reads=[tb[ob2], t_small], writes=[t_fsm])
                        P.op("dve", lambda: V.reciprocal(out=fsm[:, 0:8], in_=fsm[:, 0:8]), reads=[t_fsm], writes=[t_fsm])
                        for hh in range(8):
                            ob2 = 3 + (oi % 2) * 2 + (hh // 4)
                            oc2 = (hh % 4) * 65
                            P.op("dve", lambda hh=hh, ob2=ob2, oc2=oc2: V.tensor_scalar(out=mixed[:, oi, hh * 64:(hh + 1) * 64], in0=bank[ob2][:, oc2:oc2 + 64],
                                                                                         scalar1=fsm[:, hh:hh + 1], scalar2=None, op0=ALU.mult),
                                 reads=[tb[ob2], t_fsm], writes=[t_mixed[oi]])
                    if (not swa) and a == 1 and last:
                        h = ui - 1
                        o0 = bank[3 + (oi % 2) * 2]
                        o1 = bank[3 + (oi % 2) * 2 + 1]
                        t0, t1 = tb[3 + (oi % 2) * 2], tb[3 + (oi % 2) * 2 + 1]
                        P.op("dve", lambda: V.reciprocal(out=fsm[:, 16:17], in_=o0[:, 128:129]), reads=[t0], writes=[t_fsm])
                        P.op("dve", lambda: V.reciprocal(out=fsm[:, 17:18], in_=o1[:, 128:129]), reads=[t1], writes=[t_fsm])
                        P.op("dve", lambda: V.tensor_tensor(out=fsm[:, 17:18], in0=fsm[:, 17:18], in1=neglam, op=ALU.mult), reads=[t_fsm, t_small], writes=[t_fsm])
                        P.op("dve", lambda: V.tensor_scalar(out=fin[:, 0:128], in0=o1[:, 0:128], scalar1=fsm[:, 17:18], scalar2=None, op0=ALU.mult), reads=[t1, t_fsm], writes=[t_fin])
                        P.op("dve", lambda: V.scalar_tensor_tensor(out=fin[:, 128:256], in0=o0[:, 0:128], scalar=fsm[:, 16:17], in1=fin[:, 0:128], op0=ALU.mult, op1=ALU.add),
                             reads=[t0, t_fsm, t_fin], writes=[t_fin])
                        P.op("act", lambda: A.activation(out=junk2[:], in_=fin[:, 128:256], func=AF.Square, accum_out=fsm[:, 18:19]), reads=[t_fin], writes=[t_fsm])
                        P.op("dve", lambda: V.tensor_scalar(out=fsm[:, 18:19], in0=fsm[:, 18:19], scalar1=1.0 / 128, scalar2=EPS, op0=ALU.mult, op1=ALU.add), reads=[t_fsm], writes=[t_fsm])
                        P.op("act", lambda: A.activation(out=fsm[:, 18:19], in_=fsm[:, 18:19], func=AF.Sqrt), reads=[t_fsm], writes=[t_fsm])
                        P.op("dve", lambda: V.reciprocal(out=fsm[:, 18:19], in_=fsm[:, 18:19]), reads=[t_fsm], writes=[t_fsm])
                        P.op("dve", lambda: V.scalar_tensor_tensor(out=mixed[:, oi, 512 + h * 128:512 + (h + 1) * 128], in0=fin[:, 128:256], scalar=fsm[:, 18:19], in1=gsub_b[:],
                                                                   op0=ALU.mult, op1=ALU.mult), reads=[t_fin, t_fsm, t_gsub], writes=[t_mixed[oi]])

                LAG = 2
                for n in range(len(items) + LAG):
                    if n < len(items):
                        qk(n)
                    if n >= LAG:
                        pv(n - LAG)
            P.barrier()
            P.emit()

        with ExitStack() as s3:
            gt1_b = sbuf(s3, "gt1_b", [128, D])
            A2b = sbuf(s3, "A2b", [128, D]); S2b = sbuf(s3, "S2b", [128, D]); t_m2 = Tr()
            P.dma("sp", lambda: nc.sync.dma_start(out=gt1_b[:], in_=mod_d[0:1, 2 * D:3 * D].partition_broadcast(128)), writes=[t_mod])
            P.dma("sp", lambda: nc.sync.dma_start(out=S2b[:], in_=mod_d[0:1, 3 * D:4 * D].partition_broadcast(128)), writes=[t_m2])
            P.dma("sp", lambda: nc.sync.dma_start(out=A2b[:], in_=mod_d[0:1, 4 * D:5 * D].partition_broadcast(128)), writes=[t_m2])
            wout = sbuf(s3, "wout", [128, 8, D], BF16); t_wout = Tr()
            boutb = sbuf(s3, "boutb", [1, D], BF16)
            wr = sbuf(s3, "wr", [128, 8, NE], BF16); t_wr = Tr()
            brb = sbuf(s3, "brb", [1, NE], BF16)
            gfb = sbuf(s3, "gfb", [128, D]); t_gfb = Tr()
            P.dma("sp", lambda: nc.sync.dma_start(out=gfb[:], in_=gffn_d[0:1, :].partition_broadcast(128)), writes=[t_gfb])
            P.op("dve", lambda: V.scalar_tensor_tensor(out=A2b[:], in0=A2b[:], scalar=1.0, in1=gfb[:], op0=ALU.add, op1=ALU.mult), reads=[t_m2, t_gfb], writes=[t_m2])
            P.op("dve", lambda: V.memset(cnt_run[:], 0.0), writes=[t_cnt])
            mixT = [sbuf(s3, "mixT%d" % i, [128, 8, 128], BF16) for i in range(2)]; t_mixT = trs(2)
            xo = [sbuf(s3, "xo%d" % i, [128, D]) for i in range(2)]; t_xo = trs(2)
            x1t = [sbuf(s3, "x1t%d" % i, [128, D]) for i in range(2)]; t_x1t = trs(2)
            h2f = [sbuf(s3, "h2f%d" % i, [128, D]) for i in range(2)]; t_h2f = trs(2)
            h2tok = [sbuf(s3, "h2tok%d" % i, [128, D], BF16) for i in range(2)]; t_h2tok = trs(2)
            h2Tt = [sbuf(s3, "h2Tt%d" % i, [128, 8, 128], BF16) for i in range(2)]; t_h2Tt = trs(2)
            zrow = sbuf(s3, "zrow", [128, D], BF16); t_zrow = Tr()
            junk3 = sbuf(s3, "junk3", [128, D], BF16); t_junk3 = Tr()
            rs = sbuf(s3, "rs", [128, 64]); t_rs = trs(2)
            lg = sbuf(s3, "lg", [128, 2, 4 * NE]); t_lg = trs(2)
            idx8 = sbuf(s3, "idx8", [128, 2, 8], U32)
            posb = sbuf(s3, "posb", [128, 2, NE]); junkp = sbuf(s3, "junkp", [128, 2, NE])
            wout_v = wout_d.rearrange("(k p) n -> p k n", p=128)
            wr_v = wr_d.rearrange("(k p) n -> p k n", p=128)
            P.dma("pool", lambda: G_.dma_start(out=wout[:], in_=wout_v), writes=[t_wout])
            P.dma("pool", lambda: G_.dma_start(out=boutb[:], in_=bout_d[:, :]), writes=[t_wout])
            P.dma("pool", lambda: G_.dma_start(out=wr[:], in_=wr_v), writes=[t_wr])
            P.dma("pool", lambda: G_.dma_start(out=brb[:], in_=br_d[:, :]), writes=[t_wr])
            P.op("pool", lambda: G_.memset(zrow[:], 0.0), writes=[t_zrow])

            def p3(oi):
                b = oi % 2
                P.dma("sp", lambda: nc.sync.dma_start(out=xo[b][:], in_=x_d[S + oi * 128:S + (oi + 1) * 128, :]), writes=[t_xo[b]])
                pT = bank[b][:, :].bitcast(BF16)
                P.group("pe", [(lambda kc=kc: T.transpose(out=pT[:, kc * 128:(kc + 1) * 128], in_=mixed[:, oi, kc * 128:(kc + 1) * 128], identity=identb[:])) for kc in range(8)],
                        reads=[t_mixed[oi], t_cst], writes=[tb[b]])
                P.op("act", lambda: A.activation(out=mixT[b][:].rearrange("p a b -> p (a b)"), in_=pT[:, :], func=AF.Copy), reads=[tb[b]], writes=[t_mixT[b]])
                for hf in range(2):
                    bk = 2 + 2 * b + hf
                    fns = [(lambda kc=kc, hf=hf, bk=bk: T.matmul(bank[bk][:, :], lhsT=mixT[b][:, kc, :], rhs=wout[:, kc, hf * 512:(hf + 1) * 512], start=(kc == 0), stop=False)) for kc in range(8)]
                    fns.append(lambda hf=hf, bk=bk: T.matmul(bank[bk][:, :], lhsT=onesb[0:1, :], rhs=boutb[0:1, hf * 512:(hf + 1) * 512], start=False, stop=True))
                    P.group("pe", fns, reads=[t_mixT[b], t_wout, t_cst], writes=[tb[bk]])
                    P.op("dve", lambda hf=hf, bk=bk: V.tensor_tensor(out=x1t[b][:, hf * 512:(hf + 1) * 512], in0=bank[bk][:, :], in1=gt1_b[:, hf * 512:(hf + 1) * 512], op=ALU.mult),
                         reads=[tb[bk], t_mod], writes=[t_x1t[b]])
                P.op("dve", lambda: V.tensor_tensor(out=x1t[b][:], in0=x1t[b][:], in1=xo[b][:], op=ALU.add), reads=[t_x1t[b], t_xo[b]], writes=[t_x1t[b]])
                P.dma("sp", lambda: nc.sync.dma_start(out=x1_d[oi * 128:(oi + 1) * 128, :], in_=x1t[b][:]), reads=[t_x1t[b]])
                r0 = 32 * b
                P.op("act", lambda: A.activation(out=junk3[:], in_=x1t[b][:], func=AF.Square, accum_out=rs[:, r0:r0 + 1]), reads=[t_x1t[b]], writes=[t_junk3, t_rs[b]])
                P.op("dve", lambda: V.tensor_scalar(out=rs[:, r0 + 1:r0 + 2], in0=rs[:, r0:r0 + 1], scalar1=1.0 / D, scalar2=EPS, op0=ALU.mult, op1=ALU.add), reads=[t_rs[b]], writes=[t_rs[b]])
                P.op("act", lambda: A.activation(out=rs[:, r0 + 1:r0 + 2], in_=rs[:, r0 + 1:r0 + 2], func=AF.Sqrt), reads=[t_rs[b]], writes=[t_rs[b]])
                P.op("dve", lambda: V.reciprocal(out=rs[:, r0 + 1:r0 + 2], in_=rs[:, r0 + 1:r0 + 2]), reads=[t_rs[b]], writes=[t_rs[b]])
                P.op("dve", lambda: V.scalar_tensor_tensor(out=h2f[b][:], in0=x1t[b][:], scalar=rs[:, r0 + 1:r0 + 2], in1=A2b[:], op0=ALU.mult, op1=ALU.mult),
                     reads=[t_x1t[b], t_rs[b], t_m2], writes=[t_h2f[b]])
                P.op("dve", lambda: V.tensor_tensor(out=h2tok[b][:], in0=h2f[b][:], in1=S2b[:], op=ALU.add), reads=[t_h2f[b], t_m2], writes=[t_h2tok[b]])
                bk = 6 + b
                pT2 = bank[bk][:, :].bitcast(BF16)
                P.group("pe", [(lambda kc=kc: T.transpose(out=pT2[:, kc * 128:(kc + 1) * 128], in_=h2tok[b][:, kc * 128:(kc + 1) * 128], identity=identb[:])) for kc in range(8)],
                        reads=[t_h2tok[b], t_cst], writes=[tb[bk]])
                P.op("act", lambda: A.activation(out=h2Tt[b][:].rearrange("p a b -> p (a b)"), in_=pT2[:, :], func=AF.Copy), reads=[tb[bk]], writes=[t_h2Tt[b]])
                fns = [(lambda kc=kc: T.matmul(bank[b][:, 0:NE], lhsT=h2Tt[b][:, kc, :], rhs=wr[:, kc, :], start=(kc == 0), stop=False)) for kc in range(8)]
                fns.append(lambda: T.matmul(bank[b][:, 0:NE], lhsT=onesb[0:1, :], rhs=brb[0:1, :], start=False, stop=True))
                P.group("pe", fns, reads=[t_h2Tt[b], t_wr, t_cst], writes=[tb[b]])
                L0, L1, L2, L3 = lg[:, b, 0:NE], lg[:, b, NE:NE + 8], lg[:, b, 2 * NE:3 * NE], lg[:, b, 3 * NE:3 * NE + 8]
                P.op("dve", lambda: V.tensor_copy(out=L0, in_=bank[b][:, 0:NE]), reads=[tb[b]], writes=[t_lg[b]])
                P.op("dve", lambda: V.max(out=L1, in_=L0), reads=[t_lg[b]], writes=[t_lg[b]])
                P.op("dve", lambda: V.max_index(out=idx8[:, b, :], in_max=L1, in_values=L0), reads=[t_lg[b]], writes=[t_lg[b]])
                P.op("dve", lambda: V.tensor_scalar(out=maskb[:, oi, :], in0=L0, scalar1=lg[:, b, NE + 3:NE + 4], scalar2=None, op0=ALU.is_ge), reads=[t_lg[b]], writes=[t_maskb[oi]])
                P.op("dve", lambda: V.tensor_scalar(out=rs[:, r0 + 2:r0 + 3], in0=lg[:, b, NE:NE + 1], scalar1=-1.0, scalar2=None, op0=ALU.mult), reads=[t_lg[b]], writes=[t_rs[b]])
                P.op("act", lambda: A.activation(out=L3[:, 0:4], in_=L1[:, 0:4], func=AF.Exp, bias=rs[:, r0 + 2:r0 + 3], scale=1.0, accum_out=rs[:, r0 + 3:r0 + 4]),
                     reads=[t_lg[b], t_rs[b]], writes=[t_lg[b], t_rs[b]])
                P.op("dve", lambda: V.reciprocal(out=rs[:, r0 + 3:r0 + 4], in_=rs[:, r0 + 3:r0 + 4]), reads=[t_rs[b]], writes=[t_rs[b]])
                P.op("dve", lambda: V.tensor_scalar(out=gate4[:, 4 * oi:4 * oi + 4], in0=L3[:, 0:4], scalar1=rs[:, r0 + 3:r0 + 4], scalar2=None, op0=ALU.mult),
                     reads=[t_lg[b], t_rs[b]], writes=[t_gate4[oi]])
                pb = bank[b]
                P.group("pe", [lambda: T.matmul(pb[:, 64:64 + NE], lhsT=trib[:], rhs=maskb[:, oi, :], start=True, stop=True),
                               lambda: T.matmul(pb[:, 128:128 + NE], lhsT=ones128b[:], rhs=maskb[:, oi, :], start=True, stop=True)],
                        reads=[t_maskb[oi], t_cst, t_lg[b]], writes=[tb[b]])
                P.op("dve", lambda: V.tensor_tensor(out=posb[:, b, :], in0=pb[:, 64:64 + NE], in1=cnt_run[:], op=ALU.add), reads=[tb[b], t_cnt], writes=[t_lg[b]])
                P.op("dve", lambda: V.tensor_tensor(out=cnt_run[:], in0=pb[:, 128:128 + NE], in1=cnt_run[:], op=ALU.add), reads=[tb[b], t_cnt, t_lg[b]], writes=[t_cnt])
                EK = rs[:, r0 + 8:r0 + 12]; PK = rs[:, r0 + 12:r0 + 16]; DF = rs[:, r0 + 16:r0 + 20]
                P.op("dve", lambda: V.tensor_copy(out=EK, in_=idx8[:, b, 0:4]), reads=[t_lg[b]], writes=[t_rs[b]])
                for k in range(4):
                    P.op("dve", lambda k=k: V.scalar_tensor_tensor(out=junkp[:, b, :], in0=iota32, scalar=rs[:, r0 + 8 + k:r0 + 9 + k], in1=posb[:, b, :],
                                                                   op0=ALU.is_equal, op1=ALU.mult, accum_out=rs[:, r0 + 12 + k:r0 + 13 + k]),
                         reads=[t_lg[b], t_rs[b], t_cst], writes=[t_rs[b]])
                P.op("dve", lambda: V.scalar_tensor_tensor(out=DF, in0=EK, scalar=float(CAP), in1=PK, op0=ALU.mult, op1=ALU.add), reads=[t_rs[b]], writes=[t_rs[b]])
                P.op("dve", lambda: V.tensor_copy(out=dest_i[:, 4 * oi:4 * oi + 4], in_=DF), reads=[t_rs[b]], writes=[t_dest[oi]])
                for k in range(4):
                    P.dma("pool", lambda k=k: G_.indirect_dma_start(out=xbuf_d[:, :], out_offset=bass.IndirectOffsetOnAxis(ap=dest_i[:, 4 * oi + k:4 * oi + k + 1], axis=0),
                                                                    in_=h2tok[b][:, :], in_offset=None),
                          reads=[t_h2tok[b], t_dest[oi]], writes=[t_xbuf])
            for oi in range(NOWN):
                p3(oi)
            cf = rs[0:1, 0:NE]
            P.op("dve", lambda: V.tensor_scalar(out=cf, in0=cnt_run[0:1, :], scalar1=127.0, scalar2=1.0 / 128, op0=ALU.add, op1=ALU.mult), reads=[t_cnt] + t_rs, writes=t_rs)
            P.op("dve", lambda: V.tensor_scalar(out=cf, in0=cf, scalar1=-0.496, scalar2=None, op0=ALU.add), reads=t_rs, writes=t_rs)
            P.op("dve", lambda: V.tensor_copy(out=cnt_i[:], in_=cf), reads=t_rs, writes=[t_cnti])
            P.op("dve", lambda: V.tensor_scalar(out=posb[:, 0, :], in0=cnt_run[:], scalar1=iota_p, scalar2=None, op0=ALU.add), reads=[t_cnt, t_cst] + t_lg, writes=t_lg)
            P.op("dve", lambda: V.tensor_scalar(out=posb[:, 1, :], in0=posb[:, 0, :], scalar1=float(CAP), scalar2=None, op0=ALU.is_ge), reads=t_lg, writes=t_lg)
            P.op("dve", lambda: V.tensor_tensor(out=posb[:, 0, :], in0=posb[:, 0, :], in1=e2048, op=ALU.add), reads=t_lg + [t_cst], writes=t_lg)
            P.op("dve", lambda: V.tensor_scalar(out=rs[:, 40:41], in0=iota_p, scalar1=float(NE * CAP), scalar2=None, op0=ALU.add), reads=[t_cst] + t_rs, writes=t_rs)
            P.op("dve", lambda: V.tensor_scalar(out=junkp[:, 0, :], in0=posb[:, 0, :], scalar1=-1.0, scalar2=rs[:, 40:41], op0=ALU.mult, op1=ALU.add), reads=t_lg + t_rs, writes=t_lg)
            P.op("dve", lambda: V.tensor_tensor(out=junkp[:, 0, :], in0=junkp[:, 0, :], in1=posb[:, 1, :], op=ALU.mult), reads=t_lg, writes=t_lg)
            P.op("dve", lambda: V.tensor_tensor(out=posb[:, 0, :], in0=posb[:, 0, :], in1=junkp[:, 0, :], op=ALU.add), reads=t_lg, writes=t_lg)
            P.op("dve", lambda: V.tensor_copy(out=padidx[:], in_=posb[:, 0, :]), reads=t_lg, writes=[t_pad])
            for e in range(NE):
                P.dma("pool", lambda e=e: G_.indirect_dma_start(out=xbuf_d[:, :], out_offset=bass.IndirectOffsetOnAxis(ap=padidx[:, e:e + 1], axis=0),
                                                                in_=zrow[:, :], in_offset=None),
                      reads=[t_zrow, t_pad], writes=[t_xbuf])
            P.barrier()
            P.emit()

        sA.close()
        if int(os.environ.get('K_STOP', '9')) <= 3:
            return nc
        with ExitStack() as s4:
            NW = 2
            w1b = [sbuf(s4, "w1b%d" % i, [128, 8, 2 * D], BF16) for i in range(NW)]
            w2b = [sbuf(s4, "w2b%d" % i, [128, 8, D], BF16) for i in range(NW)]
            b1r = [sbuf(s4, "b1r%d" % i, [1, 2 * D], BF16) for i in range(NW)]
            b2r = [sbuf(s4, "b2r%d" % i, [1, D], BF16) for i in range(NW)]
            t_w = trs(NW)
            Xtok = [sbuf(s4, "Xtok%d" % i, [128, D], BF16) for i in range(2)]; t_Xtok = trs(2)
            XT = [sbuf(s4, "XT%d" % i, [128, 8, 128], BF16) for i in range(2)]; t_XT = trs(2)
            gg = [sbuf(s4, "gg%d" % i, [128, 256]) for i in range(4)]; t_gg = trs(4)
            sg = [sbuf(s4, "sg%d" % i, [128, 256]) for i in range(4)]; t_sg = trs(4)
            ll = [sbuf(s4, "ll%d" % i, [128, 256]) for i in range(4)]; t_ll = trs(4)
            atok = [sbuf(s4, "atok%d" % i, [128, D], BF16) for i in range(2)]; t_atok = trs(2)
            aT = [sbuf(s4, "aT%d" % i, [128, 8, 128], BF16) for i in range(2)]; t_aT = trs(2)
            yt = [sbuf(s4, "yt%d" % i, [128, D]) for i in range(2)]; t_yt = trs(2)
            t_ybuf = Tr()
            w1_v = w1_d.rearrange("e (k p) n -> e p k n", p=128)
            w2_v = w2_d.rearrange("e (k p) n -> e p k n", p=128)
            blk = [0]

            def prologue(e, bslot, n):
                xb = n % 2
                row0 = e * CAP + bslot * 128
                P.dma("sp", lambda: nc.sync.dma_start(out=Xtok[xb][:], in_=xbuf_d[row0:row0 + 128, :]), reads=[t_xbuf], writes=[t_Xtok[xb]], slot=xb)
                pT = bank[xb][:, :].bitcast(BF16)
                P.group("pe", [(lambda kc=kc: T.transpose(out=pT[:, kc * 128:(kc + 1) * 128], in_=Xtok[xb][:, kc * 128:(kc + 1) * 128], identity=identb[:])) for kc in range(8)],
                        reads=[t_Xtok[xb], t_cst], writes=[tb[xb]])
                P.op("act", lambda: A.activation(out=XT[xb][:].rearrange("p a b -> p (a b)"), in_=pT[:, :], func=AF.Copy), reads=[tb[xb]], writes=[t_XT[xb]])

            def block_body(e, bslot, ws, n, nxt):
                xb = n % 2
                row0 = e * CAP + bslot * 128
                def do_cch(cch):
                    bk = 2 + (cch % 2)
                    par = cch
                    fns = [(lambda kc=kc: T.matmul(bank[bk][:, :], lhsT=XT[xb][:, kc, :], rhs=w1b[ws][:, kc, cch * 512:(cch + 1) * 512], start=(kc == 0), stop=False)) for kc in range(8)]
                    fns.append(lambda: T.matmul(bank[bk][:, :], lhsT=onesb[0:1, :], rhs=b1r[ws][0:1, cch * 512:(cch + 1) * 512], start=False, stop=True))
                    P.group("pe", fns, reads=[t_XT[xb], t_w[ws], t_cst], writes=[tb[bk]])
                    P.op("dve", lambda: V.tensor_scalar(out=gg[par][:], in0=bank[bk][:, 0:512:2], scalar1=7.0, scalar2=None, op0=ALU.min), reads=[tb[bk]], writes=[t_gg[par]])
                    P.op("act", lambda: A.activation(out=sg[par][:], in_=gg[par][:], func=AF.Gelu_apprx_sigmoid), reads=[t_gg[par]], writes=[t_sg[par]])
                    P.op("dve", lambda: V.tensor_scalar(out=ll[par][:], in0=bank[bk][:, 1:512:2], scalar1=7.0, scalar2=-7.0, op0=ALU.min, op1=ALU.max), reads=[tb[bk]], writes=[t_ll[par]])

                def fin_cch(cch):
                    par = cch
                    P.op("dve", lambda: V.scalar_tensor_tensor(out=atok[xb][:, cch * 256:(cch + 1) * 256], in0=ll[par][:], scalar=1.0, in1=sg[par][:], op0=ALU.add, op1=ALU.mult),
                         reads=[t_ll[par], t_sg[par]], writes=[t_atok[xb]])
                for cch in range(4):
                    do_cch(cch)
                    if cch >= 1:
                        fin_cch(cch - 1)
                if nxt is not None:
                    prologue(*nxt)
                fin_cch(3)
                bk = 4 + xb
                pT2 = bank[bk][:, :].bitcast(BF16)
                P.group("pe", [(lambda j=j: T.transpose(out=pT2[:, j * 128:(j + 1) * 128], in_=atok[xb][:, j * 128:(j + 1) * 128], identity=identb[:])) for j in range(8)],
                        reads=[t_atok[xb], t_cst], writes=[tb[bk]])
                P.op("act", lambda: A.activation(out=aT[xb][:].rearrange("p a b -> p (a b)"), in_=pT2[:, :], func=AF.Copy), reads=[tb[bk]], writes=[t_aT[xb]])
                def do_hf(hf):
                    bk2 = 6 + hf
                    fns = [(lambda j=j: T.matmul(bank[bk2][:, :], lhsT=aT[xb][:, j, :], rhs=w2b[ws][:, j, hf * 512:(hf + 1) * 512], start=(j == 0), stop=False)) for j in range(8)]
                    fns.append(lambda: T.matmul(bank[bk2][:, :], lhsT=onesb[0:1, :], rhs=b2r[ws][0:1, hf * 512:(hf + 1) * 512], start=False, stop=True))
                    P.group("pe", fns, reads=[t_aT[xb], t_w[ws], t_cst], writes=[tb[bk2]])
                    if hf == 0:
                        P.op("act", lambda: A.activation(out=yt[xb][:, 0:512], in_=bank[bk2][:, :], func=AF.Copy), reads=[tb[bk2]], writes=[t_yt[xb]])
                    else:
                        P.op("dve", lambda: V.tensor_copy(out=yt[xb][:, 512:1024], in_=bank[bk2][:, :]), reads=[tb[bk2]], writes=[t_yt[xb]])
                for hf in range(2):
                    do_hf(hf)
                P.dma("sp", lambda: nc.sync.dma_start(out=ybuf_d[row0:row0 + 128, :], in_=yt[xb][:]), reads=[t_yt[xb]], writes=[t_ybuf], slot=2 + xb)

            for e in range(int(os.environ.get('K_NE', NE))):
                ws = e % NW
                P.dma("pool", lambda e=e, ws=ws: G_.dma_start(out=w1b[ws][:], in_=w1_v[e]), writes=[t_w[ws]])
                P.dma("pool", lambda e=e, ws=ws: G_.dma_start(out=w2b[ws][:], in_=w2_v[e]), writes=[t_w[ws]])
                P.dma("pool", lambda e=e, ws=ws: G_.dma_start(out=b1r[ws][:], in_=b1_d[e:e + 1, :]), writes=[t_w[ws]])
                P.dma("pool", lambda e=e, ws=ws: G_.dma_start(out=b2r[ws][:], in_=b2_d[e:e + 1, :]), writes=[t_w[ws]])
                P.regload(cnt_i[0:1, e:e + 1], reads=[t_cnti])
                NB = CAP // 128
                n0 = blk[0]
                blk[0] += NB
                prologue(e, 0, n0)
                for bslot in range(NB):
                    if bslot in (3, 6, 10):
                        P.cond_begin(bslot + 1)
                    P.cond_begin(bslot + 1)
                    block_body(e, bslot, ws, n0 + bslot, (e, bslot + 1, n0 + bslot + 1) if bslot + 1 < NB else None)
                    P.cond_end()
                for _ in range(3):
                    P.cond_end()
            P.barrier()
            P.emit()

        if int(os.environ.get('K_STOP', '9')) <= 4:
            return nc
        with ExitStack() as s5:
            gt2_b = sbuf(s5, "gt2_b", [128, D]); gfin_b = sbuf(s5, "gfin_b", [128, D]); t_g5 = Tr()
            yk = [sbuf(s5, "yk%d" % i, [128, D]) for i in range(4)]; t_yk = trs(4)
            acc = [sbuf(s5, "acc%d" % i, [128, D]) for i in range(2)]; t_acc = trs(2)
            x1b = [sbuf(s5, "x1b%d" % i, [128, D]) for i in range(2)]; t_x1b = trs(2)
            ob_ = [sbuf(s5, "ob%d" % i, [128, D]) for i in range(2)]; t_ob = trs(2)
            junk5 = sbuf(s5, "junk5", [128, D], BF16); t_junk5 = Tr()
            fs5 = sbuf(s5, "fs5", [128, 8]); t_fs5 = trs(2)
            P.dma("sp", lambda: nc.sync.dma_start(out=gt2_b[:], in_=mod_d[0:1, 5 * D:6 * D].partition_broadcast(128)), writes=[t_g5])
            P.dma("sp", lambda: nc.sync.dma_start(out=gfin_b[:], in_=gfin_d[0:1, :].partition_broadcast(128)), writes=[t_g5])

            def p5(oi):
                b = oi % 2
                P.dma("sp", lambda: nc.sync.dma_start(out=x1b[b][:], in_=x1_d[oi * 128:(oi + 1) * 128, :]), writes=[t_x1b[b]])
                for k in range(4):
                    P.dma("pool", lambda k=k: G_.indirect_dma_start(out=yk[k][:, :], out_offset=None, in_=ybuf_d[:, :],
                                                                    in_offset=bass.IndirectOffsetOnAxis(ap=dest_i[:, 4 * oi + k:4 * oi + k + 1], axis=0),
                                                                    ), reads=[t_dest[oi]], writes=[t_yk[k]])
                    if k == 0:
                        P.op("dve", lambda: V.tensor_scalar(out=acc[b][:], in0=yk[0][:], scalar1=gate4[:, 4 * oi:4 * oi + 1], scalar2=None, op0=ALU.mult),
                             reads=[t_yk[0], t_gate4[oi]], writes=[t_acc[b]])
                    else:
                        P.op("dve", lambda k=k: V.scalar_tensor_tensor(out=acc[b][:], in0=yk[k][:], scalar=gate4[:, 4 * oi + k:4 * oi + k + 1], in1=acc[b][:], op0=ALU.mult, op1=ALU.add),
                             reads=[t_yk[k], t_gate4[oi], t_acc[b]], writes=[t_acc[b]])
                P.op("dve", lambda: V.tensor_tensor(out=acc[b][:], in0=acc[b][:], in1=gt2_b[:], op=ALU.mult), reads=[t_acc[b], t_g5], writes=[t_acc[b]])
                P.op("dve", lambda: V.tensor_tensor(out=x1b[b][:], in0=acc[b][:], in1=x1b[b][:], op=ALU.add), reads=[t_acc[b], t_x1b[b]], writes=[t_x1b[b]])
                P.op("act", lambda: A.activation(out=junk5[:], in_=x1b[b][:], func=AF.Square, accum_out=fs5[:, 4 * b:4 * b + 1]), reads=[t_x1b[b]], writes=[t_junk5, t_fs5[b]])
                P.op("dve", lambda: V.tensor_scalar(out=fs5[:, 4 * b + 1:4 * b + 2], in0=fs5[:, 4 * b:4 * b + 1], scalar1=1.0 / D, scalar2=EPS, op0=ALU.mult, op1=ALU.add),
                     reads=[t_fs5[b]], writes=[t_fs5[b]])
                P.op("act", lambda: A.activation(out=fs5[:, 4 * b + 1:4 * b + 2], in_=fs5[:, 4 * b + 1:4 * b + 2], func=AF.Sqrt), reads=[t_fs5[b]], writes=[t_fs5[b]])
                P.op("dve", lambda: V.reciprocal(out=fs5[:, 4 * b + 1:4 * b + 2], in_=fs5[:, 4 * b + 1:4 * b + 2]), reads=[t_fs5[b]], writes=[t_fs5[b]])
                P.op("dve", lambda: V.scalar_tensor_tensor(out=ob_[b][:], in0=x1b[b][:], scalar=fs5[:, 4 * b + 1:4 * b + 2], in1=gfin_b[:], op0=ALU.mult, op1=ALU.mult),
                     reads=[t_x1b[b], t_fs5[b], t_g5], writes=[t_ob[b]])
                P.dma("sp", lambda: nc.sync.dma_start(out=out_d[oi * 128:(oi + 1) * 128, :], in_=ob_[b][:]), reads=[t_ob[b]])
            for oi in range(NOWN):
                p5(oi)
            P.barrier()
            P.emit()
    return nc


def _consts():
    c = np.zeros((128, NCST), np.float32)
    c[:, 0:128] = np.eye(128, dtype=np.float32)
    k = np.arange(128)[:, None]
    q = np.arange(128)[None, :]
    c[:, 128:256] = (k <= q)
    c[:, 256:384] = (k > q)
    inv = (1.0 / (np.float32(10000.0) ** (np.arange(0, 64, 2, dtype=np.float32) / np.float32(64)))).astype(np.float32)
    p = np.arange(128)
    c[:, 384] = inv[p % 32]
    c[:, 385] = np.where((p % 64) < 32, -1.0, 1.0)
    c[:, 386] = np.float32(math.pi / 2)
    c[:, 387] = 0.0
    c[:, 388] = 1.0
    c[:, 392:520] = 1.0
    c[:, 520:648] = (k < q)
    c[:, 648:680] = np.arange(32)[None, :]
    c[:, 680:712] = (np.arange(32) * CAP)[None, :]
    c[:, 712] = np.arange(128)
    return c


def _core_masks(j):
    own = own_blocks(j)
    k = np.arange(128)[:, None]
    q = np.arange(128)[None, :]
    tri = (k <= q).astype(np.float32)
    low = (k > q).astype(np.float32)
    dm = np.zeros((128, NOWN, 4, 128), np.float32)
    sm = np.zeros((128, NOWN, 2, 128), np.float32)
    for oi, gb in enumerate(own):
        nkb = 8 * (oi // 2) + (4 if oi % 2 == 0 else 8)
        for i in range(4):
            kb = nkb - 4 + i
            if kb < gb:
                dm[:, oi, i, :] = 1.0
            elif kb == gb:
                dm[:, oi, i, :] = tri
        sm[:, oi, 1, :] = tri
        if gb > 0:
            sm[:, oi, 0, :] = low
    return dm.reshape(128, NOWN * 512), sm.reshape(128, NOWN * 256)


_NC_CACHE = {}


def kernel(x, c, positions, w_ada, b_ada, g_mix, w_in, b_in, attn_sinks, lambda_q1, lambda_k1, lambda_q2, lambda_k2,
           g_subln, w_out, b_out, g_ffn, w_router, b_router, w1, b1, w2, b2, g_final):
    f = lambda a: np.ascontiguousarray(np.asarray(a))
    x = f(x); positions = f(positions)
    if "nc" not in _NC_CACHE:
        _NC_CACHE["nc"] = build_program()
    nc = _NC_CACHE["nc"]
    colT = lambda v: f(np.asarray(v).reshape(-1, 128).T)
    w_sel = f(np.asarray(w_in)[0][:, SEL])
    b_sel = f(np.asarray(b_in)[0][SEL])
    b1_ = np.asarray(b1)[0]
    shared = {
        "w_ada": f(np.asarray(w_ada)[0]), "b_ada": f(np.asarray(b_ada)[0][None, :]),
        "gmixT": colT(np.asarray(g_mix)[0]), "gffnT": colT(np.asarray(g_ffn)[0]),
        "w_sel": w_sel, "b_selT": colT(b_sel), "b_sel": f(b_sel[None, :]),
        "sinks": f(np.asarray(attn_sinks)[0][None, :]),
        "lam4": f(np.stack([np.asarray(lambda_q1)[0], np.asarray(lambda_k1)[0], np.asarray(lambda_q2)[0], np.asarray(lambda_k2)[0]])),
        "g_subln": f(np.asarray(g_subln)[0][None, :]),
        "w_out": f(np.asarray(w_out)[0]), "b_out": f(np.asarray(b_out)[0][None, :]),
        "w_router": f(np.asarray(w_router)[0]), "b_router": f(np.asarray(b_router)[0][None, :]),
        "w1": f(np.asarray(w1)[0]), "w2": f(np.asarray(w2)[0]), "b2": f(np.asarray(b2)[0]),
        "b1": f(b1_), "g_ffn": f(np.asarray(g_ffn)[0][None, :]), "g_mix": f(np.asarray(g_mix)[0][None, :]),
        "g_final": f(np.asarray(g_final)[None, :]),
        "consts": _consts(),
    }
    in_maps = []
    rows_all = []
    for core in range(8):
        b, j = core // 4, core % 4
        own = own_blocks(j)
        rows_own = np.concatenate([np.arange(g * 128, (g + 1) * 128) for g in own])
        rows_prev = np.concatenate([np.arange(max(g - 1, 0) * 128, (max(g - 1, 0) + 1) * 128) for g in own])
        rows_all.append(rows_own)
        xb = x[b]
        x_ext = np.concatenate([xb, xb[rows_own], xb[rows_prev]], axis=0)
        pb = positions[b]
        pos_ext = np.concatenate([pb, pb[rows_own], pb[rows_prev]])[None, :].astype(np.int32)
        dm, sm = _core_masks(j)
        m = dict(shared)
        m.update({"x": f(x_ext), "pos": f(pos_ext), "cT": colT(np.asarray(c)[b]), "dmask": dm, "smask": sm})
        in_maps.append(m)
    res = run_bass_kernel_spmd(nc, in_maps, core_ids=list(range(8)))
    out = np.zeros((2, S, D), np.float32)
    for core in range(8):
        out[core // 4, rows_all[core], :] = np.asarray(res.results[core]["out"])
    return out
```
